# Optimizing a Trainium2 kernel written in Bass

```python
import math
import jax
import jax.numpy as jnp
from jax import lax
import numpy as np

D_MODEL = 1024
BATCH = 4
SEQ = 4096
DEPTH = 2

GRID_W = 64
CTX_LEN = 256
EPS = 1e-6

ATT_HEADS = 6
ATT_KV_HEADS = 2
HEAD_DIM = 64
ATT_W = ATT_HEADS * HEAD_DIM
KV_W = ATT_KV_HEADS * HEAD_DIM
ROPE_AXIS_DIM = HEAD_DIM // 2
ROPE_THETA = 10000.0
Q_BLOCK = 128

SSD_HEADS = 6
SSD_HEAD_DIM = 64
SSD_W = SSD_HEADS * SSD_HEAD_DIM
SSD_GROUPS = 2
SSD_STATE = 64
SSD_CONV = 3
SSD_CHUNK = 128
SSD_CONV_CH = SSD_W + 2 * SSD_GROUPS * SSD_STATE

HY_W = 256
HY_ORDER = 2
HY_SHORT = 3
HY_BANDS = 16
HY_POS_DIM = 1 + 2 * HY_BANDS
HY_FILTER_HID = 64
HY_FAST_DECAY = 0.3
HY_SLOW_DECAY = 1.5
HY_TARGET = 1e-2

MIX_W = ATT_W + SSD_W + HY_W
D_IN = ATT_W + 2 * KV_W + SSD_W + SSD_CONV_CH + 2 * SSD_HEADS + (HY_ORDER + 1) * HY_W

N_EXPERTS = 16
N_GROUPS = 4
EXPERTS_PER_GROUP = N_EXPERTS // N_GROUPS
GROUP_SCORE_TOPK = 2
TOP_K = 2
D_FF = 256

kernel_name = 'hybrid_ssd_hyena_gqa_moe_prefix_dit'


def rmsnorm(x, g):
    xf = x.astype(jnp.float32)
    y = xf * lax.rsqrt(jnp.mean(xf * xf, axis=-1, keepdims=True) + EPS)
    return (y * g.astype(jnp.float32)).astype(x.dtype)


def modulate(h, shift, scale):
    return h * (1 + scale[:, None, :]) + shift[:, None, :]


def adaln(cvec, w_mod, b_mod):
    m = jax.nn.silu(cvec) @ w_mod + b_mod
    return jnp.split(m, 6, axis=-1)


def split_proj(u):
    cuts = np.cumsum([ATT_W, KV_W, KV_W, SSD_W, SSD_CONV_CH, 2 * SSD_HEADS])
    return jnp.split(u, [int(i) for i in cuts], axis=-1)


def rev(t):
    return jnp.flip(t, axis=1)


def dwconv_centred(u, w, b):
    k = w.shape[0]
    out = lax.conv_general_dilated(u, w[:, None, :].astype(u.dtype), (1,), [(k // 2, k // 2)],
                                   dimension_numbers=('NWC', 'WIO', 'NWC'),
                                   feature_group_count=u.shape[-1])
    return out + b.astype(u.dtype)


def axial_rope(rows):
    row = jnp.broadcast_to(jnp.arange(rows)[:, None], (rows, GRID_W)).reshape(-1).astype(jnp.float32)
    col = jnp.broadcast_to(jnp.arange(GRID_W)[None, :], (rows, GRID_W)).reshape(-1).astype(jnp.float32)
    inv = ROPE_THETA ** (-jnp.arange(0, ROPE_AXIS_DIM, 2, dtype=jnp.float32) / ROPE_AXIS_DIM)
    ang = jnp.concatenate([row[:, None] * inv, col[:, None] * inv], axis=-1)
    return jnp.cos(ang), jnp.sin(ang)


def apply_rope(x, cos, sin):
    half = HEAD_DIM // 2
    xf = x.astype(jnp.float32)
    x1, x2 = xf[..., :half], xf[..., half:]
    cs, sn = cos[None, :, None, :], sin[None, :, None, :]
    return jnp.concatenate([x1 * cs - x2 * sn, x2 * cs + x1 * sn], axis=-1).astype(x.dtype)


def heads(t, n_heads):
    return t.reshape(t.shape[0], t.shape[1], n_heads, HEAD_DIM)


def gqa_blocks(q, k, v):
    bsz, n = q.shape[:2]
    rep = ATT_HEADS // ATT_KV_HEADS
    qb = q.reshape(bsz, n // Q_BLOCK, Q_BLOCK, ATT_KV_HEADS, rep, HEAD_DIM).swapaxes(0, 1)
    scale = HEAD_DIM ** -0.5

    def one_block(qi):
        s = jnp.einsum('bqgrd,bkgd->bgrqk', qi, k).astype(jnp.float32) * scale
        p = jax.nn.softmax(s, axis=-1).astype(v.dtype)
        return jnp.einsum('bgrqk,bkgd->bqgrd', p, v)

    o = lax.map(one_block, qb)
    return o.swapaxes(0, 1).reshape(bsz, n, ATT_W)


def ssd_chunked(x, dt, a, bmat, cmat, h0):
    bsz, L = x.shape[:2]
    nc = L // SSD_CHUNK
    rep = SSD_HEADS // SSD_GROUPS
    f = jnp.float32
    xc = x.astype(f).reshape(bsz, nc, SSD_CHUNK, SSD_HEADS, SSD_HEAD_DIM)
    dtc = dt.astype(f).reshape(bsz, nc, SSD_CHUNK, SSD_HEADS)
    bc = jnp.repeat(bmat.astype(f), rep, axis=2).reshape(bsz, nc, SSD_CHUNK, SSD_HEADS, SSD_STATE)
    cc = jnp.repeat(cmat.astype(f), rep, axis=2).reshape(bsz, nc, SSD_CHUNK, SSD_HEADS, SSD_STATE)
    acum = jnp.cumsum(dtc * a, axis=2)
    seg = acum[:, :, :, None, :] - acum[:, :, None, :, :]
    earlier = jnp.tril(jnp.ones((SSD_CHUNK, SSD_CHUNK), dtype=bool))[None, None, :, :, None]
    decay = jnp.exp(jnp.where(earlier, seg, -jnp.inf))
    cb = jnp.einsum('bcthn,bcshn->bctsh', cc, bc)
    y_in = jnp.einsum('bctsh,bcsh,bcshp->bcthp', cb * decay, dtc, xc)
    w_end = jnp.exp(acum[:, :, -1:, :] - acum) * dtc
    states = jnp.einsum('bcshn,bcsh,bcshp->bchpn', bc, w_end, xc)
    chunk_decay = jnp.exp(acum[:, :, -1, :])

    def step(hprev, inp):
        st, dec = inp
        return hprev * dec[:, :, None, None] + st, hprev

    h_last, h_enter = lax.scan(step, h0.astype(f),
                               (jnp.moveaxis(states, 1, 0), jnp.moveaxis(chunk_decay, 1, 0)))
    h_enter = jnp.moveaxis(h_enter, 0, 1)
    y_out = jnp.einsum('bcthn,bchpn,bcth->bcthp', cc, h_enter, jnp.exp(acum))
    return (y_in + y_out).reshape(bsz, L, SSD_HEADS, SSD_HEAD_DIM), h_last


def ssd_last_state(x, dt, a, bmat):
    rep = SSD_HEADS // SSD_GROUPS
    f = jnp.float32
    dtf = dt.astype(f)
    acum = jnp.cumsum(dtf * a, axis=1)
    w = jnp.exp(acum[:, -1:, :] - acum) * dtf
    bh = jnp.repeat(bmat.astype(f), rep, axis=2)
    return jnp.einsum('blhn,blh,blhp->bhpn', bh, w, x.astype(f))


def bidir_ssd(xs, dt, a, bm, cm, h0_f, h0_b):
    y_f, h_f = ssd_chunked(xs, dt[:, :, 0], a[0], bm, cm, h0_f)
    y_b, h_b = ssd_chunked(rev(xs), rev(dt[:, :, 1]), a[1], rev(bm), rev(cm), h0_b)
    return y_f + rev(y_b), h_f, h_b


def ssd_prep(xbc, dt_raw, conv_w, conv_b, dt_bias):
    bsz, L = xbc.shape[:2]
    xbc = jax.nn.silu(dwconv_centred(xbc, conv_w, conv_b))
    xs, bm, cm = jnp.split(xbc, [SSD_W, SSD_W + SSD_GROUPS * SSD_STATE], axis=-1)
    dt = jax.nn.softplus(dt_raw.astype(jnp.float32).reshape(bsz, L, 2, SSD_HEADS) + dt_bias.astype(jnp.float32))
    return (xs.reshape(bsz, L, SSD_HEADS, SSD_HEAD_DIM),
            bm.reshape(bsz, L, SSD_GROUPS, SSD_STATE),
            cm.reshape(bsz, L, SSD_GROUPS, SSD_STATE), dt)


def ssd_out(y, xs, z, d_skip, norm_w):
    bsz, L = y.shape[:2]
    y = y + xs.astype(jnp.float32) * d_skip.astype(jnp.float32)[:, None]
    g = y.reshape(bsz, L, SSD_W) * jax.nn.silu(z.astype(jnp.float32))
    g = rmsnorm(g.reshape(bsz, L, SSD_GROUPS, SSD_W // SSD_GROUPS), norm_w.reshape(SSD_GROUPS, -1))
    return g.reshape(bsz, L, SSD_W).astype(z.dtype)


def hyena_filters(L, w1, b1, freq, w2, b2, w3):
    f = jnp.float32
    n = jnp.arange(L, dtype=f)
    t = n / max(L - 1, 1)
    bands = jnp.linspace(1e-4, HY_BANDS - 1, HY_BANDS, dtype=f)
    wpos = (2 * math.pi / L) * n
    feats = jnp.concatenate([t[:, None], jnp.cos(wpos[:, None] * bands), -jnp.sin(wpos[:, None] * bands)], axis=-1)
    fr = freq.astype(f)
    hid = jnp.sin(fr * (feats @ w1.astype(f) + b1.astype(f)))
    hid = jnp.sin(fr * (hid @ w2.astype(f) + b2.astype(f)))
    h = (hid @ w3.astype(f)).reshape(L, 2, HY_ORDER, HY_W)
    deltas = jnp.abs(jnp.linspace(math.log(HY_TARGET) / HY_SLOW_DECAY, math.log(HY_TARGET) / HY_FAST_DECAY, HY_W, dtype=f))
    window = jnp.exp(-t[:, None] * deltas)
    return h * window[:, None, None, :]


def bidir_long_conv(z, hf, hb, bias):
    L = z.shape[1]
    kern = jnp.concatenate([hf, jnp.zeros_like(hf[:1]), hb[:0:-1]], axis=0)
    zf = z.astype(jnp.float32)
    y = jnp.fft.irfft(jnp.fft.rfft(zf, n=2 * L, axis=1) * jnp.fft.rfft(kern, axis=0)[None], n=2 * L, axis=1)[:, :L]
    return y + zf * bias.astype(jnp.float32)


def hyena_mixer(u, conv_w, conv_b, w1, b1, freq, w2, b2, w3, bias):
    u = dwconv_centred(u, conv_w, conv_b)
    v, x1, x2 = jnp.split(u, 3, axis=-1)
    h = hyena_filters(u.shape[1], w1, b1, freq, w2, b2, w3)
    zz = x1.astype(jnp.float32) * bidir_long_conv(v, h[:, 0, 0], h[:, 1, 0], bias[0])
    zz = x2.astype(jnp.float32) * bidir_long_conv(zz, h[:, 0, 1], h[:, 1, 1], bias[1])
    return zz.astype(u.dtype)


def grouped_moe(h, w_router, router_bias, w_gate, w_up, w_down):
    bsz, n, d = h.shape
    t = h.reshape(bsz * n, d)
    scores = jax.nn.sigmoid((t @ w_router).astype(jnp.float32))
    sel = scores + router_bias.astype(jnp.float32)
    grp = lax.top_k(sel.reshape(-1, N_GROUPS, EXPERTS_PER_GROUP), GROUP_SCORE_TOPK)[0].sum(-1)
    best = jnp.argmax(grp, axis=-1)
    in_grp = (jnp.arange(N_EXPERTS) // EXPERTS_PER_GROUP)[None, :] == best[:, None]
    _, idx = lax.top_k(jnp.where(in_grp, sel, -jnp.inf), TOP_K)
    wts = jnp.take_along_axis(scores, idx, axis=-1)
    wts = wts / jnp.sum(wts, axis=-1, keepdims=True)
    combine = jnp.sum(jax.nn.one_hot(idx, N_EXPERTS, dtype=jnp.float32) * wts[..., None], axis=1).astype(t.dtype)
    out = jnp.zeros_like(t)
    for e in range(N_EXPERTS):
        y = (jax.nn.silu(t @ w_gate[e]) * (t @ w_up[e])) @ w_down[e]
        out = out + combine[:, e:e + 1] * y
    return out.reshape(bsz, n, d)


def setup_inputs(seed: int = 0) -> dict:
    key = jax.random.key(seed)
    ks = iter(jax.random.split(key, 48))

    def nrm(shape, s):
        return jax.random.normal(next(ks), shape, jnp.float32) * s

    def gain(shape):
        return 1.0 + nrm(shape, 0.01)

    dt0 = jnp.exp(jax.random.uniform(next(ks), (DEPTH, 2, SSD_HEADS), jnp.float32, math.log(1e-3), math.log(1e-1)))
    return {
        'x': nrm((BATCH, SEQ, D_MODEL), 1.0),
        'c': nrm((BATCH, D_MODEL), 1.0),
        'ctx': nrm((BATCH, CTX_LEN, D_MODEL), 1.0),
        'c_ctx': nrm((D_MODEL,), 1.0),
        'w_mod': nrm((DEPTH, D_MODEL, 6 * D_MODEL), 0.5 * D_MODEL ** -0.5),
        'b_mod': nrm((DEPTH, 6 * D_MODEL), 0.01),
        'g_mix': gain((DEPTH, D_MODEL)),
        'g_ffn': gain((DEPTH, D_MODEL)),
        'w_in': nrm((DEPTH, D_MODEL, D_IN), D_MODEL ** -0.5),
        'q_norm': gain((DEPTH, HEAD_DIM)),
        'k_norm': gain((DEPTH, HEAD_DIM)),
        'ssd_conv_w': nrm((DEPTH, SSD_CONV, SSD_CONV_CH), SSD_CONV ** -0.5),
        'ssd_conv_b': nrm((DEPTH, SSD_CONV_CH), 0.01),
        'ssd_dt_bias': dt0 + jnp.log(-jnp.expm1(-dt0)),
        'ssd_a_log': jnp.log(jax.random.uniform(next(ks), (DEPTH, 2, SSD_HEADS), jnp.float32, 1.0, 16.0)),
        'ssd_d': gain((DEPTH, SSD_HEADS)),
        'ssd_norm': gain((DEPTH, SSD_W)),
        'hy_conv_w': nrm((DEPTH, HY_SHORT, (HY_ORDER + 1) * HY_W), HY_SHORT ** -0.5),
        'hy_conv_b': nrm((DEPTH, (HY_ORDER + 1) * HY_W), 0.01),
        'hy_w1': nrm((DEPTH, HY_POS_DIM, HY_FILTER_HID), HY_POS_DIM ** -0.5),
        'hy_b1': nrm((DEPTH, HY_FILTER_HID), 0.1),
        'hy_freq': gain((DEPTH, HY_FILTER_HID)),
        'hy_w2': nrm((DEPTH, HY_FILTER_HID, HY_FILTER_HID), HY_FILTER_HID ** -0.5),
        'hy_b2': nrm((DEPTH, HY_FILTER_HID), 0.1),
        'hy_w3': nrm((DEPTH, HY_FILTER_HID, 2 * HY_ORDER * HY_W), 0.05 * HY_FILTER_HID ** -0.5),
        'hy_bias': nrm((DEPTH, HY_ORDER, HY_W), 1.0),
        'w_out': nrm((DEPTH, MIX_W, D_MODEL), MIX_W ** -0.5),
        'w_router': nrm((D_MODEL, N_EXPERTS), D_MODEL ** -0.5),
        'router_bias': nrm((N_EXPERTS,), 0.01),
        'w_gate': nrm((DEPTH, N_EXPERTS, D_MODEL, D_FF), D_MODEL ** -0.5),
        'w_up': nrm((DEPTH, N_EXPERTS, D_MODEL, D_FF), D_MODEL ** -0.5),
        'w_down': nrm((DEPTH, N_EXPERTS, D_FF, D_MODEL), D_FF ** -0.5),
        'g_final': gain((D_MODEL,)),
    }


def reference(x, c, ctx, c_ctx, w_mod, b_mod, g_mix, g_ffn, w_in, q_norm, k_norm,
              ssd_conv_w, ssd_conv_b, ssd_dt_bias, ssd_a_log, ssd_d, ssd_norm,
              hy_conv_w, hy_conv_b, hy_w1, hy_b1, hy_freq, hy_w2, hy_b2, hy_w3, hy_bias,
              w_out, w_router, router_bias, w_gate, w_up, w_down, g_final):
    bsz, n_lat = x.shape[0], x.shape[1]
    rows = n_lat // GRID_W
    cos, sin = axial_rope(rows)
    xl, xc = x, ctx
    for i in range(DEPTH):
        need_ctx = i < DEPTH - 1
        sh1, sc1, gt1, sh2, sc2, gt2 = adaln(c, w_mod[i], b_mod[i])
        csh1, csc1, cgt1, csh2, csc2, cgt2 = adaln(c_ctx[None], w_mod[i], b_mod[i])

        hl = modulate(rmsnorm(xl, g_mix[i]), sh1, sc1)
        hc = modulate(rmsnorm(xc, g_mix[i]), csh1, csc1)
        q_l, k_l, v_l, z_l, xbc_l, dt_l, hy_l = split_proj(hl @ w_in[i])
        q_c, k_c, v_c, z_c, xbc_c, dt_c, hy_c = split_proj(hc @ w_in[i])

        ql = apply_rope(rmsnorm(heads(q_l, ATT_HEADS), q_norm[i]), cos, sin)
        kl = apply_rope(rmsnorm(heads(k_l, ATT_KV_HEADS), k_norm[i]), cos, sin)
        kc = rmsnorm(heads(k_c, ATT_KV_HEADS), k_norm[i])
        vl, vc = heads(v_l, ATT_KV_HEADS), heads(v_c, ATT_KV_HEADS)
        att_l = gqa_blocks(ql, jnp.concatenate([kc, kl], axis=1), jnp.concatenate([vc, vl], axis=1))

        a = -jnp.exp(ssd_a_log[i].astype(jnp.float32))
        xs_c, b_c, c_c, dtc = ssd_prep(xbc_c, dt_c, ssd_conv_w[i], ssd_conv_b[i], ssd_dt_bias[i])
        xs_l, b_l, c_l, dtl = ssd_prep(xbc_l, dt_l, ssd_conv_w[i], ssd_conv_b[i], ssd_dt_bias[i])
        if need_ctx:
            zeros = jnp.zeros((bsz, SSD_HEADS, SSD_HEAD_DIM, SSD_STATE), jnp.float32)
            yc, hc_f, hc_b = bidir_ssd(xs_c, dtc, a, b_c, c_c, zeros, zeros)
        else:
            hc_f = ssd_last_state(xs_c, dtc[:, :, 0], a[0], b_c)
            hc_b = ssd_last_state(rev(xs_c), rev(dtc[:, :, 1]), a[1], rev(b_c))
        yl, _, _ = bidir_ssd(xs_l, dtl, a, b_l, c_l, hc_f, hc_b)
        ssd_l = ssd_out(yl, xs_l, z_l, ssd_d[i], ssd_norm[i])

        hyn_l = hyena_mixer(hy_l, hy_conv_w[i], hy_conv_b[i], hy_w1[i], hy_b1[i], hy_freq[i],
                            hy_w2[i], hy_b2[i], hy_w3[i], hy_bias[i])

        mix_l = jnp.concatenate([att_l, ssd_l, hyn_l], axis=-1) @ w_out[i]
        xl = xl + gt1[:, None, :] * mix_l
        if need_ctx:
            qc = rmsnorm(heads(q_c, ATT_HEADS), q_norm[i])
            att_c = gqa_blocks(qc, kc, vc)
            ssd_c = ssd_out(yc, xs_c, z_c, ssd_d[i], ssd_norm[i])
            hyn_c = hyena_mixer(hy_c, hy_conv_w[i], hy_conv_b[i], hy_w1[i], hy_b1[i], hy_freq[i],
                                hy_w2[i], hy_b2[i], hy_w3[i], hy_bias[i])
            mix_c = jnp.concatenate([att_c, ssd_c, hyn_c], axis=-1) @ w_out[i]
            xc = xc + cgt1[:, None, :] * mix_c

        hl2 = modulate(rmsnorm(xl, g_ffn[i]), sh2, sc2)
        if need_ctx:
            hc2 = modulate(rmsnorm(xc, g_ffn[i]), csh2, csc2)
            n_ctx = hc2.shape[1]
            hc2 = jnp.broadcast_to(hc2, (bsz,) + hc2.shape[1:])
            moe_all = grouped_moe(jnp.concatenate([hc2, hl2], axis=1), w_router, router_bias,
                                  w_gate[i], w_up[i], w_down[i])
            xc = xc + cgt2[:, None, :] * moe_all[:, :n_ctx]
            xl = xl + gt2[:, None, :] * moe_all[:, n_ctx:]
        else:
            xl = xl + gt2[:, None, :] * grouped_moe(hl2, w_router, router_bias, w_gate[i], w_up[i], w_down[i])
    return rmsnorm(xl, g_final)
```

```python
import math
from contextlib import ExitStack
import numpy as np
import ml_dtypes
import concourse.bass as bass
import concourse.mybir as mybir
from concourse.bass_utils import run_bass_kernel_spmd

F32 = mybir.dt.float32
BF16 = mybir.dt.bfloat16
AF = mybir.ActivationFunctionType
ALU = mybir.AluOpType
AX = mybir.AxisListType

D = 1024
NCTX = 256
NLAT = 4096
NTOK = NCTX + NLAT
NT = NTOK // 128
DEPTH = 2
D_IN = 2444
EPS = 1e-6
HC = NTOK + 3
NE = 16
DFF = 256
DEBUG = False


def colof(tile):
    return 1 + 128 * tile if tile < 2 else 258 + 128 * (tile - 2)


BLOCKS = [(1, 0, 256, 0, 2)] + [(258 + 512 * j, 256 + 512 * j, 512, 2 + 4 * j, 4) for j in range(8)]


class Tok:
    __slots__ = ("w", "r")

    def __init__(self):
        self.w = None
        self.r = {}


class Sched:
    def __init__(self, nc, ndma=8, same_engine_sync=True):
        self.nc = nc
        self.eng = {"pe": nc.tensor, "act": nc.scalar, "dve": nc.vector, "pool": nc.gpsimd, "sp": nc.sync}
        self.semh = {}
        self.cnt = {}
        self.seen = {k: {} for k in self.eng}
        self.same = same_engine_sync
        for k in self.eng:
            self.semh[k] = nc.alloc_semaphore("s_" + k)
            self.cnt[k] = 0
        self.ndma = ndma
        self.dslot = {}
        self.dval = {}
        for q in ("sp", "pool"):
            self.dslot[q] = 0
            for i in range(ndma):
                key = ("dma", q, i)
                self.semh[key] = nc.alloc_semaphore("d_%s_%d" % (q, i))
                self.dval[key] = 0
        self.ninst = 0

    def _wait(self, e, deps):
        for (k, v) in sorted(deps, key=str):
            if k == e and (e == "pe" or not self.same):
                continue
            if self.seen[e].get(k, 0) >= v:
                continue
            self.eng[e].wait_ge(self.semh[k], v)
            self.seen[e][k] = v

    @staticmethod
    def _deps(reads, writes):
        deps = set()
        for t in reads:
            if t.w is not None:
                deps.add(t.w)
        for t in writes:
            if t.w is not None:
                deps.add(t.w)
            for kv in t.r.items():
                deps.add(kv)
        return deps

    @staticmethod
    def _mark(ev, reads, writes):
        k, v = ev
        for t in reads:
            if t.r.get(k, 0) < v:
                t.r[k] = v
        for t in writes:
            t.w = ev
            t.r = {}

    def op(self, e, fn, reads=(), writes=()):
        self._wait(e, self._deps(reads, writes))
        ins = fn(self.eng[e])
        self.cnt[e] += 1
        ins.then_inc(self.semh[e], 1)
        self._mark((e, self.cnt[e]), reads, writes)
        self.ninst += 1
        return ins

    def dma(self, out, in_, reads=(), writes=(), q="sp", **kw):
        i = self.dslot[q]
        self.dslot[q] = (i + 1) % self.ndma
        key = ("dma", q, i)
        deps = self._deps(reads, writes)
        if self.dval[key] > 0:
            deps.add((key, self.dval[key]))
        self._wait(q, deps)
        ins = self.eng[q].dma_start(out=out, in_=in_, **kw)
        self.dval[key] += 16
        ins.then_inc(self.semh[key], 16)
        self._mark((key, self.dval[key]), reads, writes)
        self.ninst += 1
        return ins

    def barrier(self):
        deps = set()
        for key, v in self.dval.items():
            if v > 0:
                deps.add((key, v))
        for k in self.eng:
            if self.cnt[k] > 0:
                deps.add((k, self.cnt[k]))
        for e in self.eng:
            self._wait(e, deps)


class Ctx:
    pass


_UID = [0]


def SB(nc, name, shape, dt):
    _UID[0] += 1
    return nc.sbuf_tensor("%s_%d" % (name, _UID[0]), shape, dt)


def PS(nc, name, shape, dt):
    _UID[0] += 1
    return nc.psum_tensor("%s_%d" % (name, _UID[0]), shape, dt)


def build(dbg=None):
    nc = bass.Bass("TRN2", target_bir_lowering=False)
    S = Sched(nc)
    G = Ctx()
    G.nc, G.S = nc, S

    def din(name, shape, dt=F32):
        return nc.dram_tensor(name, list(shape), dt, kind="ExternalInput").ap()

    def dscr(name, shape, dt):
        return nc.dram_tensor(name, list(shape), dt, kind="Internal").ap()

    I = {}
    I["x"] = din("x", [NLAT, D])
    I["ctx"] = din("ctx", [NCTX, D])
    I["w_mod"] = din("w_mod", [DEPTH, D, 6 * D])
    I["b_mod"] = din("b_mod", [DEPTH, 6 * D])
    I["w_in"] = din("w_in", [DEPTH, D, D_IN])
    I["w_out"] = din("w_out", [DEPTH, D, D])
    I["w_router"] = din("w_router", [D, NE])
    I["router_bias"] = din("router_bias", [NE])
    I["w_gate"] = din("w_gate", [DEPTH, NE, D, DFF])
    I["w_up"] = din("w_up", [DEPTH, NE, D, DFF])
    I["w_down"] = din("w_down", [DEPTH, NE, DFF, D])
    I["g_final"] = din("g_final", [D])
    I["cols"] = din("cols", [DEPTH, 128, NCOLS])
    I["cmat"] = din("cmat", [9, 128, 128])
    I["rope"] = din("rope", [2, 128, NLAT])
    for sname, P in SEGS.items():
        I["ft_" + sname] = din("ft_" + sname, [P["KC"], 128, P["NC"], 2, 128], BF16)
        I["it_" + sname] = din("it_" + sname, [P["NC"], 128, P["KC"], 2, 128], BF16)
        I["fe_" + sname] = din("fe_" + sname, [33, P["L"]])
        I["win_" + sname] = din("win_" + sname, [P["NC"], 128, 2, 256])
        I["mh_" + sname] = din("mh_" + sname, [128, P["KC"]])
    for nm, shp in (("hy_conv_w", [DEPTH, 3, 768]), ("hy_conv_b", [DEPTH, 768]), ("hy_w1", [DEPTH, 33, 64]), ("hy_w2", [DEPTH, 64, 64]),
                    ("hy_w3", [DEPTH, 64, 1024]), ("hy_bias", [DEPTH, 2, 256])):
        I[nm] = din(nm, shp)
    I["ssdmask"] = din("ssdmask", [2, 4, 128, 512], BF16)
    for nm, shp in (("ssd_conv_w", [DEPTH, 3, 640]), ("ssd_dt_bias", [DEPTH, 2, 6]), ("ssd_a_log", [DEPTH, 2, 6]),
                    ("ssd_d", [DEPTH, 6]), ("ssd_norm", [DEPTH, 384])):
        I[nm] = din(nm, shp)
    out = nc.dram_tensor("out", [NLAT, D], F32, kind="ExternalOutput").ap()
    G.I, G.out = I, out
    G.dbg = {}
    if dbg:
        for name, shape in dbg.items():
            G.dbg[name] = nc.dram_tensor("dbg_" + name, list(shape), F32, kind="ExternalOutput").ap()

    G.xres = dscr("xres", [NTOK, D], F32)
    G.t_xres = [Tok() for _ in range(NT)]
    G.wb_in = [dscr("wb_in%d" % i, [D, D_IN], BF16) for i in range(DEPTH)]
    G.wb_out = [dscr("wb_out%d" % i, [D, D], BF16) for i in range(DEPTH)]
    G.wb_gate = [dscr("wb_gate%d" % i, [NE, D, DFF], BF16) for i in range(DEPTH)]
    G.wb_up = [dscr("wb_up%d" % i, [NE, D, DFF], BF16) for i in range(DEPTH)]
    G.wb_down = [dscr("wb_down%d" % i, [NE, DFF, D], BF16) for i in range(DEPTH)]
    G.t_wb = Tok()
    G.mixT = dscr("mixT", [D, NTOK], BF16)
    G.hyv = dscr("hyv", [3, NTOK, 256], BF16)
    G.ssd_yb = dscr("ssd_yb", [NTOK, 384], F32)
    G.t_ssdyb = [Tok() for _ in range(NT)]
    G.t_hyv = Tok()
    G.Kf = {sn: dscr("Kf_" + sn, [2, P["KC"], 128, 2, 2, 256], BF16) for sn, P in SEGS.items()}
    G.t_mix = Tok()

    cm = nc.alloc_sbuf_tensor("cm", [128, 9, 128], F32)
    cmb = nc.alloc_sbuf_tensor("cmb", [128, 9, 128], BF16)
    ones = nc.alloc_sbuf_tensor("ones", [128, 128], F32)
    epsc = nc.alloc_sbuf_tensor("epsc", [128, 1], F32)
    G.t_c = Tok()
    S.dma(cm[:], I["cmat"].rearrange("a p c -> p a c"), writes=[G.t_c])
    S.op("dve", lambda e: e.tensor_copy(out=cmb[:], in_=cm[:]), reads=[G.t_c], writes=[G.t_c])
    S.op("dve", lambda e: e.memset(ones[:], 1.0), writes=[G.t_c])
    S.op("dve", lambda e: e.memset(epsc[:], EPS), writes=[G.t_c])
    G.cm, G.cmb, G.ones, G.epsc = cm, cmb, ones, epsc
    G.negpi = nc.alloc_sbuf_tensor("negpi", [128, 1], F32)
    S.op("dve", lambda e: e.memset(G.negpi[:], -math.pi), writes=[G.t_c])
    G.identF, G.identB = cm[:, 0, :], cmb[:, 0, :]

    S.dma(G.xres[0:NCTX, :], I["ctx"][:, :], writes=G.t_xres[0:2])
    for j in range(4):
        S.dma(G.xres[NCTX + 1024 * j:NCTX + 1024 * (j + 1), :], I["x"][1024 * j:1024 * (j + 1), :],
              writes=G.t_xres[2 + 8 * j:2 + 8 * (j + 1)])

    convert_weights(G)
    S.barrier()
    for li in range(1 if DEBUG else DEPTH):
        layer(G, li)
    S.barrier()
    return nc


def convert_weights(G):
    nc, S, I = G.nc, G.S, G.I
    CH = 4096
    with ExitStack() as _es:
        cf = _es.enter_context(SB(nc, "cv_f", [128, 2, CH], F32))
        cb = _es.enter_context(SB(nc, "cv_b", [128, 2, CH], BF16))
        tf = [Tok(), Tok()]
        tb = [Tok(), Tok()]
        n = 0
        engs = ["dve", "pool", "act"]
        for li in range(DEPTH):
            pairs = [(I["w_in"][li], G.wb_in[li], "a b -> (a b)"), (I["w_out"][li], G.wb_out[li], "a b -> (a b)"),
                     (I["w_gate"][li], G.wb_gate[li], "e a b -> (e a b)"), (I["w_up"][li], G.wb_up[li], "e a b -> (e a b)"),
                     (I["w_down"][li], G.wb_down[li], "e a b -> (e a b)")]
            for src, dst, pat in pairs:
                s1 = src.rearrange(pat).rearrange("(p m) -> p m", p=128)
                d1 = dst.rearrange(pat).rearrange("(p m) -> p m", p=128)
                M = s1.shape[1]
                for c0 in range(0, M, CH):
                    w = min(CH, M - c0)
                    k = n % 2
                    S.dma(cf[:, k, 0:w], s1[:, c0:c0 + w], writes=[tf[k]])
                    en = engs[n % 3]
                    if en == "act":
                        S.op("act", lambda e: e.copy(out=cb[:, k, 0:w], in_=cf[:, k, 0:w]), reads=[tf[k]], writes=[tb[k]])
                    else:
                        S.op(en, lambda e: e.tensor_copy(out=cb[:, k, 0:w], in_=cf[:, k, 0:w]), reads=[tf[k]], writes=[tb[k]])
                    S.dma(d1[:, c0:c0 + w], cb[:, k, 0:w], reads=[tb[k]], writes=[G.t_wb], q="pool")
                    n += 1


COLS = {}
_o = 0
for _name, _n in [("cc", 16), ("bmod", 32), ("g_mix", 8), ("g_ffn", 8), ("ssd_conv_b", 5), ("qg", 1), ("kg", 1),
                  ("ssd_d", 3), ("ssd_norm", 3), ("hy_b1", 1), ("hy_freq", 1), ("hy_b2", 1)]:
    COLS[_name] = (_o, _n)
    _o += _n
NCOLS = _o


def layer(G, li):
    nc, S, I = G.nc, G.S, G.I
    with ExitStack() as _es:
        cols = _es.enter_context(SB(nc, "cols", [128, NCOLS], F32))
        modc = _es.enter_context(SB(nc, "modc", [128, 4, 8, 2], F32))
        gtb = _es.enter_context(SB(nc, "gtb", [128, 2, 2, D], F32))
        G.cols, G.modc, G.gtb = cols, modc, gtb
        G.t_cols, G.t_modc, G.t_gtb = Tok(), Tok(), Tok()
        S.dma(cols[:], I["cols"][li], writes=[G.t_cols])
        adaln(G, li)
        S.barrier()
        with ExitStack() as _es:
            hT = _es.enter_context(SB(nc, "hT", [128, 8, HC], BF16))
            G.hT, G.t_hT = hT, Tok()
            norm_in(G, li)
            S.barrier()
            if "hy" in STAGES:
                hyena_inproj(G, li)
                S.barrier()
            if "att" in STAGES:
                attention(G, li)
                S.barrier()
            if "ssd" in STAGES:
                ssd(G, li)
                S.barrier()
        if "hy" in STAGES:
            hyena_fft(G, li)
            S.barrier()
        if "moe" in STAGES:
            with ExitStack() as _es:
                h2T = _es.enter_context(SB(nc, "h2T", [128, 8, NTOK], BF16))
                rl = _es.enter_context(SB(nc, "rl", [128, NT, NE], F32))
                G.h2T, G.t_h2T, G.rl, G.t_rl = h2T, Tok(), rl, Tok()
                outproj(G, li)
                S.barrier()
                moe(G, li)
                S.barrier()


STAGES = ("att", "ssd", "hy", "moe")


def colap(G, name, j=0, n=1, p0=0, p1=128):
    o, _ = COLS[name]
    return G.cols[p0:p1, o + j:o + j + n]


def adaln(G, li):
    nc, S, I = G.nc, G.S, G.I
    cols, modc, gtb = G.cols, G.modc, G.gtb
    with ExitStack() as _es:
        sc = _es.enter_context(SB(nc, "sc", [128, 8, 2], F32))
        screp = _es.enter_context(SB(nc, "screp", [128, 8, 2, 128], F32))
        wm = _es.enter_context(SB(nc, "wm", [128, 2, 8, 512], F32))
        brow = _es.enter_context(SB(nc, "brow", [128, 2, D], F32))
        ps_a = _es.enter_context(PS(nc, "ps_a", [128, 4, 2], F32))
        ps_g = _es.enter_context(PS(nc, "ps_g", [128, 2, 512], F32))
        t_sc, t_wm, t_pa, t_pg, t_brow = Tok(), [Tok(), Tok()], Tok(), Tok(), Tok()
        o = COLS["cc"][0]
        S.op("act", lambda e: e.activation(out=sc[:].rearrange("p k j -> p (k j)"), in_=cols[:, o:o + 16], func=AF.Silu),
             reads=[G.t_cols], writes=[t_sc])
        S.op("dve", lambda e: e.tensor_copy(out=screp[:].rearrange("p k j c -> p (k j) c"),
                                            in_=sc[:].rearrange("p k j -> p (k j)").unsqueeze(2).to_broadcast([128, 16, 128])),
             reads=[t_sc], writes=[t_sc])
        for g in range(2):
            S.dma(brow[:, g, :], I["b_mod"][li, (2 + 3 * g) * D:(3 + 3 * g) * D].partition_broadcast(128), writes=[t_brow])
        wv = I["w_mod"][li].rearrange("(k p) c -> p k c", p=128)
        ob = COLS["bmod"][0]
        for cj in range(12):
            b = cj % 2
            S.dma(wm[:, b], wv[:, :, cj * 512:(cj + 1) * 512], writes=[t_wm[b]])
            vec = cj // 2
            half = cj % 2
            if vec in (2, 5):
                g = 0 if vec == 2 else 1
                for j in range(2):
                    for kd in range(8):
                        S.op("pe", lambda e: e.matmul(ps_g[:, j, :], lhsT=screp[:, kd, j, :], rhs=wm[:, b, kd, :],
                                                      start=(kd == 0), stop=(kd == 7)), reads=[t_sc, t_wm[b]], writes=[t_pg])
                    S.op("dve", lambda e: e.tensor_tensor(out=gtb[:, g, j, half * 512:(half + 1) * 512], in0=ps_g[:, j, :],
                                                          in1=brow[:, g, half * 512:(half + 1) * 512], op=ALU.add),
                         reads=[t_pg, t_brow], writes=[G.t_gtb])
            else:
                v = {0: 0, 1: 1, 3: 2, 4: 3}[vec]
                for fc in range(4):
                    for kd in range(8):
                        S.op("pe", lambda e: e.matmul(ps_a[:, fc, :], lhsT=wm[:, b, kd, fc * 128:(fc + 1) * 128], rhs=sc[:, kd, :],
                                                      start=(kd == 0), stop=(kd == 7)), reads=[t_sc, t_wm[b]], writes=[t_pa])
                k0 = half * 4
                S.op("dve", lambda e: e.tensor_tensor(out=modc[:, v, k0:k0 + 4, :], in0=ps_a[:],
                                                      in1=cols[:, ob + v * 8 + k0:ob + v * 8 + k0 + 4].unsqueeze(2).to_broadcast([128, 4, 2]),
                                                      op=ALU.add), reads=[t_pa, G.t_cols], writes=[G.t_modc])
        for v, gname in ((1, "g_mix"), (3, "g_ffn")):
            og = COLS[gname][0]
            S.op("dve", lambda e: e.scalar_tensor_tensor(out=modc[:, v], in0=modc[:, v], scalar=1.0,
                                                         in1=cols[:, og:og + 8].unsqueeze(2).to_broadcast([128, 8, 2]),
                                                         op0=ALU.add, op1=ALU.mult), reads=[G.t_modc, G.t_cols], writes=[G.t_modc])


def rms_to_T(G, xt, t_x, tile, vA, vB, dstT, t_dst, dcol, pool):
    nc, S = G.nc, G.S
    sq, ss, xn, ps_t, toks = pool
    t_sq, t_ss, t_xn, t_ps = toks
    j = 1 if tile < 2 else 0
    S.op("act", lambda e: e.activation(out=sq[:], in_=xt, func=AF.Square, accum_out=ss[:, 0:1]), reads=[t_x], writes=[t_sq, t_ss])
    S.op("act", lambda e: e.activation(out=ss[:, 1:2], in_=ss[:, 0:1], func=AF.Sqrt, bias=G.epsc[:], scale=1.0 / D),
         reads=[t_ss, G.t_c], writes=[t_ss])
    S.op("dve", lambda e: e.reciprocal(out=ss[:, 2:3], in_=ss[:, 1:2]), reads=[t_ss], writes=[t_ss])
    S.op("dve", lambda e: e.tensor_scalar(out=xn[:], in0=xt, scalar1=ss[:, 2:3], scalar2=None, op0=ALU.mult),
         reads=[t_x, t_ss], writes=[t_xn])
    for k in range(8):
        S.op("pe", lambda e: e.transpose(out=ps_t[:, k, :], in_=xn[:, k * 128:(k + 1) * 128], identity=G.identB),
             reads=[t_xn, G.t_c], writes=[t_ps])
    S.op("dve", lambda e: e.tensor_tensor(out=sq[:].rearrange("p (k c) -> p k c", k=8), in0=ps_t[:],
                                          in1=G.modc[:, vA, :, j:j + 1].to_broadcast([128, 8, 128]), op=ALU.mult),
         reads=[t_ps, G.t_modc], writes=[t_sq])
    S.op("dve", lambda e: e.tensor_tensor(out=dstT[:, :, dcol:dcol + 128], in0=sq[:].rearrange("p (k c) -> p k c", k=8),
                                          in1=G.modc[:, vB, :, j:j + 1].to_broadcast([128, 8, 128]), op=ALU.add),
         reads=[t_sq, G.t_modc], writes=[t_dst])


def norm_in(G, li):
    nc, S = G.nc, G.S
    hT = G.hT
    with ExitStack() as _es:
        xt = _es.enter_context(SB(nc, "xt", [128, 2, D], F32))
        sq = _es.enter_context(SB(nc, "sq", [128, D], F32))
        ss = _es.enter_context(SB(nc, "ss", [128, 4], F32))
        xn = _es.enter_context(SB(nc, "xn", [128, D], BF16))
        ps_t = _es.enter_context(PS(nc, "ps_t", [128, 8, 128], BF16))
        t_x = [Tok(), Tok()]
        pool = (sq, ss, xn, ps_t, (Tok(), Tok(), Tok(), Tok()))
        for c in (0, 257, HC - 1):
            S.op("pool", lambda e: e.memset(hT[:, :, c:c + 1], 0.0), writes=[G.t_hT])
        for tile in range(NT):
            b = tile % 2
            S.dma(xt[:, b, :], G.xres[tile * 128:(tile + 1) * 128, :], reads=[G.t_xres[tile]], writes=[t_x[b]])
            rms_to_T(G, xt[:, b, :], t_x[b], tile, 1, 0, hT, G.t_hT, colof(tile), pool)
        if "hT" in G.dbg and li == 0:
            with ExitStack() as _es:
                dh = _es.enter_context(SB(nc, "dbgh", [128, 8, 512], F32))
                t = Tok()
                S.op("dve", lambda e: e.tensor_copy(out=dh[:], in_=hT[:, :, 0:512]), reads=[G.t_hT], writes=[t])
                S.dma(G.dbg["hT"].rearrange("(k p) c -> p k c", p=128), dh[:], reads=[t])


def attention(G, li):
    nc, S, I = G.nc, G.S, G.I
    hT = G.hT
    need_ctx = li < DEPTH - 1
    wv = G.wb_in[li].rearrange("(k p) c -> p k c", p=128)
    scale = 64 ** -0.5
    with ExitStack() as _es:
        wq = _es.enter_context(SB(nc, "wqkv", [128, 8, 640], BF16))
        qT = _es.enter_context(SB(nc, "qT", [128, 3, NTOK], BF16))
        kT = _es.enter_context(SB(nc, "kT", [128, NTOK], BF16))
        vp = _es.enter_context(SB(nc, "vp", [128, NT, 2, 128], BF16))
        rp = _es.enter_context(SB(nc, "rp", [128, 2, 2, 512], F32))
        qs = _es.enter_context(SB(nc, "qs", [128, 512], F32))
        q2 = _es.enter_context(SB(nc, "q2", [128, 512], F32))
        qn = _es.enter_context(SB(nc, "qn", [128, 512], F32))
        qnb = _es.enter_context(SB(nc, "qnb", [128, 512], BF16))
        pT = _es.enter_context(SB(nc, "pT", [128, 2, 512], BF16))
        rd = _es.enter_context(SB(nc, "rd", [128, 2, 512], F32))
        ao = _es.enter_context(SB(nc, "ao", [128, 2, 512], BF16))
        ps_q = _es.enter_context(PS(nc, "ps_q", [128, 512], F32))
        ps_r = _es.enter_context(PS(nc, "ps_r", [128, 512], F32))
        ps_s = _es.enter_context(PS(nc, "ps_s", [128, 2, 512], F32))
        ps_o = _es.enter_context(PS(nc, "ps_o", [128, 2, 512], F32))
        t_w, t_q, t_k, t_v, t_rp = Tok(), Tok(), Tok(), Tok(), [Tok(), Tok()]
        t_qs, t_q2, t_qn, t_qnb, t_psq, t_psr = Tok(), Tok(), Tok(), Tok(), Tok(), Tok()
        t_pT, t_pss, t_pso, t_rd, t_ao = [Tok(), Tok()], [Tok(), Tok()], [Tok(), Tok()], [Tok(), Tok()], [Tok(), Tok()]
        for j in range(3):
            S.dma(wq[:, :, j * 128:j * 128 + 64], wv[:, :, j * 64:(j + 1) * 64], reads=[G.t_wb], writes=[t_w])
            S.dma(wq[:, :, j * 128 + 64:(j + 1) * 128], wv[:, :, (3 + j) * 64:(4 + j) * 64], reads=[G.t_wb], writes=[t_w])
        S.dma(wq[:, :, 384:640], wv[:, :, 384:640], reads=[G.t_wb], writes=[t_w])
        S.op("pool", lambda e: e.memset(vp[:, :, :, 64:128], 1.0), writes=[t_v])
        og = {0: COLS["qg"][0], 1: COLS["qg"][0], 2: COLS["qg"][0], 3: COLS["kg"][0]}
        for bi, (c0, t0, n, tile0, ntile) in enumerate(BLOCKS):
            if bi > 0:
                b = bi % 2
                S.dma(rp[:, b, :, :], I["rope"][:, :, t0 - NCTX:t0 - NCTX + 512].rearrange("a p c -> p a c"), writes=[t_rp[b]])
            for ch in range(4):
                for k in range(8):
                    S.op("pe", lambda e: e.matmul(ps_q[:, 0:n], lhsT=wq[:, k, ch * 128:(ch + 1) * 128], rhs=hT[:, k, c0:c0 + n],
                                                  start=(k == 0), stop=(k == 7)), reads=[t_w, G.t_hT], writes=[t_psq])
                S.op("act", lambda e: e.copy(out=qs[:, 0:n], in_=ps_q[:, 0:n]), reads=[t_psq], writes=[t_qs])
                S.op("act", lambda e: e.activation(out=q2[:, 0:n], in_=qs[:, 0:n], func=AF.Square), reads=[t_qs], writes=[t_q2])
                S.op("pe", lambda e: e.matmul(ps_r[:, 0:n], lhsT=G.cm[:, 3, :], rhs=q2[:, 0:n], start=True, stop=True),
                     reads=[t_q2, G.t_c], writes=[t_psr])
                S.op("act", lambda e: e.activation(out=q2[:, 0:n], in_=ps_r[:, 0:n], func=AF.Sqrt, bias=G.epsc[:], scale=1.0 / 64),
                     reads=[t_psr, G.t_c], writes=[t_q2])
                S.op("dve", lambda e: e.reciprocal(out=q2[:, 0:n], in_=q2[:, 0:n]), reads=[t_q2], writes=[t_q2])
                S.op("dve", lambda e: e.scalar_tensor_tensor(out=qn[:, 0:n], in0=qs[:, 0:n], scalar=G.cols[:, og[ch]:og[ch] + 1],
                                                             in1=q2[:, 0:n], op0=ALU.mult, op1=ALU.mult),
                     reads=[t_qs, t_q2, G.t_cols], writes=[t_qn])
                dst = qT[:, ch, t0:t0 + n] if ch < 3 else kT[:, t0:t0 + n]
                t_dst = t_q if ch < 3 else t_k
                if bi == 0:
                    S.op("dve", lambda e: e.tensor_copy(out=dst, in_=qn[:, 0:n]), reads=[t_qn], writes=[t_dst])
                else:
                    b = bi % 2
                    S.op("dve", lambda e: e.tensor_copy(out=qnb[:, 0:n], in_=qn[:, 0:n]), reads=[t_qn], writes=[t_qnb])
                    S.op("pe", lambda e: e.matmul(ps_r[:, 0:n], lhsT=G.cmb[:, 4, :], rhs=qnb[:, 0:n], start=True, stop=True),
                         reads=[t_qnb, G.t_c], writes=[t_psr])
                    S.op("dve", lambda e: e.tensor_tensor(out=qs[:, 0:n], in0=ps_r[:, 0:n], in1=rp[:, b, 1, 0:n], op=ALU.mult),
                         reads=[t_psr, t_rp[b]], writes=[t_qs])
                    S.op("dve", lambda e: e.tensor_tensor(out=qn[:, 0:n], in0=qn[:, 0:n], in1=rp[:, b, 0, 0:n], op=ALU.mult),
                         reads=[t_qn, t_rp[b]], writes=[t_qn])
                    S.op("dve", lambda e: e.tensor_tensor(out=dst, in0=qn[:, 0:n], in1=qs[:, 0:n], op=ALU.add),
                         reads=[t_qn, t_qs], writes=[t_dst])
            for tl in range(tile0, tile0 + ntile):
                cc = colof(tl)
                for k in range(8):
                    S.op("pe", lambda e: e.matmul(ps_q[:, 0:128], lhsT=hT[:, k, cc:cc + 128], rhs=wq[:, k, 512:640],
                                                  start=(k == 0), stop=(k == 7)), reads=[t_w, G.t_hT], writes=[t_psq])
                S.op("act", lambda e: e.copy(out=vp[:, tl, :, 0:64], in_=ps_q[:, 0:128].rearrange("p (a d) -> p a d", a=2)),
                     reads=[t_psq], writes=[t_v])
        steps = []
        oi = 0
        for h in range(6):
            for bi, (c0, t0, n, tile0, ntile) in enumerate(BLOCKS):
                if bi == 0 and not need_ctx:
                    continue
                kcs = list(range(2)) if bi == 0 else list(range(NT))
                for ki, kc in enumerate(kcs):
                    steps.append((h, t0, n, kc, ki == 0, ki == len(kcs) - 1, oi % 2))
                oi += 1

        def qk(i):
            h, t0, n, kc, first, last, ob = steps[i]
            pb = (h // 3) * 64
            sb = i % 2
            S.op("pe", lambda e: e.matmul(ps_s[:, sb, 0:n], lhsT=kT[pb:pb + 64, kc * 128:(kc + 1) * 128],
                                          rhs=qT[pb:pb + 64, h % 3, t0:t0 + n], start=True, stop=True),
                 reads=[t_q, t_k], writes=[t_pss[sb]])

        qk(0)
        for i, (h, t0, n, kc, first, last, ob) in enumerate(steps):
            sb = i % 2
            kv = h // 3
            if i + 1 < len(steps):
                qk(i + 1)
            S.op("act", lambda e: e.activation(out=pT[:, sb, 0:n], in_=ps_s[:, sb, 0:n], func=AF.Exp, scale=scale),
                 reads=[t_pss[sb]], writes=[t_pT[sb]])
            S.op("pe", lambda e: e.matmul(ps_o[:, ob, 0:n], lhsT=vp[:, kc, kv, :], rhs=pT[:, sb, 0:n], start=first, stop=last),
                 reads=[t_v, t_pT[sb]], writes=[t_pso[ob]])
            if last:
                S.op("dve", lambda e: e.reciprocal(out=rd[0:64, ob, 0:n], in_=ps_o[64:128, ob, 0:n]), reads=[t_pso[ob]], writes=[t_rd[ob]])
                S.op("dve", lambda e: e.tensor_tensor(out=ao[0:64, ob, 0:n], in0=ps_o[0:64, ob, 0:n], in1=rd[0:64, ob, 0:n], op=ALU.mult),
                     reads=[t_pso[ob], t_rd[ob]], writes=[t_ao[ob]])
                S.dma(G.mixT[h * 64:(h + 1) * 64, t0:t0 + n], ao[0:64, ob, 0:n], reads=[t_ao[ob]], writes=[G.t_mix])
        if "att" in G.dbg and li == 0:
            dump_bf16(G, G.mixT[0:128, 256:768], G.dbg["att"], [G.t_mix])


def dump_bf16(G, src, dst, reads):
    nc, S = G.nc, G.S
    p, n = src.shape
    with ExitStack() as _es:
        a = _es.enter_context(SB(nc, "dmpb", [p, n], BF16))
        b = _es.enter_context(SB(nc, "dmpf", [p, n], F32))
        t = Tok()
        S.dma(a[:], src, reads=reads, writes=[t])
        S.op("dve", lambda e: e.tensor_copy(out=b[:], in_=a[:]), reads=[t], writes=[t])
        S.dma(dst, b[:], reads=[t])
        S.barrier()


def _cols_pack(inp, li, b):
    def colform(v, n):
        return np.ascontiguousarray(v.reshape(n, 128).T)
    parts = {}
    cc = np.zeros((128, 8, 2), np.float32)
    cc[:, :, 0] = colform(inp["c"][b], 8)
    cc[:, :, 1] = colform(inp["c_ctx"], 8)
    parts["cc"] = cc.reshape(128, 16)
    bm = inp["b_mod"][li].reshape(6, 8, 128)
    parts["bmod"] = np.concatenate([bm[v].T for v in (0, 1, 3, 4)], axis=1)
    parts["g_mix"] = colform(inp["g_mix"][li], 8)
    parts["g_ffn"] = colform(inp["g_ffn"][li], 8)
    parts["ssd_conv_b"] = colform(inp["ssd_conv_b"][li], 5)
    parts["qg"] = np.tile(inp["q_norm"][li], 2)[:, None]
    parts["kg"] = np.tile(inp["k_norm"][li], 2)[:, None]
    parts["ssd_d"] = colform(np.repeat(inp["ssd_d"][li], 64), 3)
    parts["ssd_norm"] = colform(inp["ssd_norm"][li], 3)
    for nm in ("hy_b1", "hy_freq", "hy_b2"):
        parts[nm] = np.tile(inp[nm][li], 2)[:, None]
    out = np.zeros((128, NCOLS), np.float32)
    for nm, (o, n) in COLS.items():
        out[:, o:o + n] = parts[nm]
    return out


def _consts():
    ident = np.eye(128, dtype=np.float32)
    s = np.arange(128)
    U = (s[:, None] <= s[None, :]).astype(np.float32)
    Lo = (s[:, None] >= s[None, :]).astype(np.float32)
    bo = np.kron(np.eye(2, dtype=np.float32), np.ones((64, 64), np.float32))
    rot = np.zeros((128, 128), np.float32)
    for hb in (0, 64):
        for d in range(32):
            rot[hb + d + 32, hb + d] = -1.0
            rot[hb + d, hb + d + 32] = 1.0
    top = np.zeros((128, 128), np.float32); top[:64] = 1.0
    bot = np.zeros((128, 128), np.float32); bot[64:] = 1.0
    cmat = np.stack([ident, U, Lo, bo, rot, top, bot, U - ident, Lo - ident])
    rows = NLAT // 64
    row = np.repeat(np.arange(rows), 64).astype(np.float32)
    col = np.tile(np.arange(64), rows).astype(np.float32)
    inv = (10000.0 ** (-np.arange(0, 32, 2, dtype=np.float32) / 32)).astype(np.float32)
    ang = np.concatenate([row[:, None] * inv, col[:, None] * inv], axis=-1).astype(np.float32)
    cs = np.cos(ang).astype(np.float32).T
    sn = np.sin(ang).astype(np.float32).T
    rope = np.stack([np.tile(cs, (4, 1)), np.tile(sn, (4, 1))]).astype(np.float32)
    tt = np.arange(512)[None, None, :]
    ss_ = np.arange(128)[None, :, None]
    jj = np.arange(4)[:, None, None]
    mf = (tt >= 128 * jj + ss_).astype(np.float32)
    mb = (tt <= 128 * jj + ss_).astype(np.float32)
    hyt = {}
    for sname, P in SEGS.items():
        ft, it_, fe, wn, mh = _hy_tables(P["L"])
        hyt["ft_" + sname], hyt["it_" + sname], hyt["fe_" + sname], hyt["win_" + sname], hyt["mh_" + sname] = ft, it_, fe, wn, mh
    return {**hyt, "cmat": cmat, "rope": rope, "ssdmask": np.stack([mf, mb]).astype(ml_dtypes.bfloat16)}


_CONSTS = None


def kernel(**inp):
    global _CONSTS
    inp = {k: np.asarray(v) for k, v in inp.items()}
    if _CONSTS is None:
        _CONSTS = _consts()
    dbg = kernel.dbg if hasattr(kernel, "dbg") else None
    nc = build(dbg)
    ncores = 8
    in_maps = []
    for core in range(ncores):
        b = core % 4
        m = {"x": np.ascontiguousarray(inp["x"][b]), "ctx": np.ascontiguousarray(inp["ctx"][b])}
        for k in ("w_mod", "b_mod", "w_in", "w_out", "w_router", "router_bias", "w_gate", "w_up", "w_down", "g_final",
                  "ssd_conv_w", "ssd_dt_bias", "ssd_a_log", "ssd_d", "ssd_norm", "hy_conv_w", "hy_conv_b", "hy_w1", "hy_w2", "hy_w3", "hy_bias"):
            m[k] = inp[k]
        m["cols"] = np.stack([_cols_pack(inp, li, b) for li in range(DEPTH)])
        m.update(_CONSTS)
        in_maps.append(m)
    res = run_bass_kernel_spmd(nc, in_maps, core_ids=list(range(ncores)))
    kernel.last = res
    return np.stack([res.results[b]["out"] for b in range(4)]).astype(np.float32)


def outproj(G, li):
    nc, S, I = G.nc, G.S, G.I
    need_ctx = li < DEPTH - 1
    with ExitStack() as _es:
        wo = _es.enter_context(SB(nc, "wo", [128, 8, D], BF16))
        mx = _es.enter_context(SB(nc, "mx", [128, 2, 8, 128], BF16))
        xt = _es.enter_context(SB(nc, "xt", [128, 2, D], F32))
        tmp = _es.enter_context(SB(nc, "tmp", [128, D], F32))
        sq = _es.enter_context(SB(nc, "sq", [128, D], F32))
        ss = _es.enter_context(SB(nc, "ss", [128, 4], F32))
        xn = _es.enter_context(SB(nc, "xn", [128, D], F32))
        h2f = _es.enter_context(SB(nc, "h2f", [128, 8, 128], F32))
        wr = _es.enter_context(SB(nc, "wr", [128, 8, NE], F32))
        po = _es.enter_context(PS(nc, "po", [128, 2, 512], F32))
        pt = _es.enter_context(PS(nc, "pt", [128, 8, 128], F32))
        pr = _es.enter_context(PS(nc, "pr", [128, NE], F32))
        t_wo, t_mx, t_x, t_tmp, t_po = Tok(), [Tok(), Tok()], [Tok(), Tok()], Tok(), Tok()
        t_sq, t_ss, t_xn, t_pt, t_h2f, t_wr, t_pr = Tok(), Tok(), Tok(), Tok(), Tok(), Tok(), Tok()
        S.dma(wo[:], G.wb_out[li].rearrange("(k p) c -> p k c", p=128), reads=[G.t_wb], writes=[t_wo])
        S.dma(wr[:], I["w_router"].rearrange("(k p) c -> p k c", p=128), writes=[t_wr])
        mv = G.mixT.rearrange("(k p) t -> p k t", p=128)
        for tile in range(NT):
            if tile < 2 and not need_ctx:
                continue
            b = tile % 2
            j = 1 if tile < 2 else 0
            S.dma(mx[:, b], mv[:, :, tile * 128:(tile + 1) * 128], reads=[G.t_mix], writes=[t_mx[b]])
            S.dma(xt[:, b, :], G.xres[tile * 128:(tile + 1) * 128, :], reads=[G.t_xres[tile]], writes=[t_x[b]])
            for half in range(2):
                for k in range(8):
                    S.op("pe", lambda e: e.matmul(po[:, half, :], lhsT=mx[:, b, k, :], rhs=wo[:, k, half * 512:(half + 1) * 512],
                                                  start=(k == 0), stop=(k == 7)), reads=[t_mx[b], t_wo], writes=[t_po])
            S.op("dve", lambda e: e.tensor_tensor(out=tmp[:], in0=po[:].rearrange("p a c -> p (a c)"), in1=G.gtb[:, 0, j, :], op=ALU.mult),
                 reads=[t_po, G.t_gtb], writes=[t_tmp])
            S.op("dve", lambda e: e.tensor_tensor(out=xt[:, b, :], in0=tmp[:], in1=xt[:, b, :], op=ALU.add),
                 reads=[t_tmp, t_x[b]], writes=[t_x[b]])
            S.dma(G.xres[tile * 128:(tile + 1) * 128, :], xt[:, b, :], reads=[t_x[b]], writes=[G.t_xres[tile]])
            xv = xt[:, b, :]
            S.op("act", lambda e: e.activation(out=sq[:], in_=xv, func=AF.Square, accum_out=ss[:, 0:1]), reads=[t_x[b]], writes=[t_sq, t_ss])
            S.op("act", lambda e: e.activation(out=ss[:, 1:2], in_=ss[:, 0:1], func=AF.Sqrt, bias=G.epsc[:], scale=1.0 / D),
                 reads=[t_ss, G.t_c], writes=[t_ss])
            S.op("dve", lambda e: e.reciprocal(out=ss[:, 2:3], in_=ss[:, 1:2]), reads=[t_ss], writes=[t_ss])
            S.op("dve", lambda e: e.tensor_scalar(out=xn[:], in0=xv, scalar1=ss[:, 2:3], scalar2=None, op0=ALU.mult),
                 reads=[t_x[b], t_ss], writes=[t_xn])
            for k in range(8):
                S.op("pe", lambda e: e.transpose(out=pt[:, k, :], in_=xn[:, k * 128:(k + 1) * 128], identity=G.identF),
                     reads=[t_xn, G.t_c], writes=[t_pt])
            S.op("dve", lambda e: e.tensor_tensor(out=h2f[:], in0=pt[:], in1=G.modc[:, 3, :, j:j + 1].to_broadcast([128, 8, 128]), op=ALU.mult),
                 reads=[t_pt, G.t_modc], writes=[t_h2f])
            S.op("dve", lambda e: e.tensor_tensor(out=h2f[:], in0=h2f[:], in1=G.modc[:, 2, :, j:j + 1].to_broadcast([128, 8, 128]), op=ALU.add),
                 reads=[t_h2f, G.t_modc], writes=[t_h2f])
            S.op("act", lambda e: e.copy(out=G.h2T[:, :, tile * 128:(tile + 1) * 128], in_=h2f[:]), reads=[t_h2f], writes=[G.t_h2T])
            for k in range(8):
                S.op("pe", lambda e: e.matmul(pr[:], lhsT=h2f[:, k, :], rhs=wr[:, k, :], start=(k == 0), stop=(k == 7)),
                     reads=[t_h2f, t_wr], writes=[t_pr])
            S.op("dve", lambda e: e.tensor_copy(out=G.rl[:, tile, :], in_=pr[:]), reads=[t_pr], writes=[G.t_rl])


def moe(G, li):
    nc, S, I = G.nc, G.S, G.I
    need_ctx = li < DEPTH - 1
    last = li == DEPTH - 1
    h2T, rl = G.h2T, G.rl
    T0 = 0 if need_ctx else 2
    NTl = NT - T0
    BIG = 1.0e9
    with ExitStack() as _es:
        comb = _es.enter_context(SB(nc, "comb", [128, NT, NE], F32))
        t_comb = Tok()
        with ExitStack() as _es:
            sc = _es.enter_context(SB(nc, "r_sc", [128, NT, NE], F32))
            sel = _es.enter_context(SB(nc, "r_sel", [128, NT, NE], F32))
            ra = _es.enter_context(SB(nc, "r_a", [128, NT, NE], F32))
            rb = _es.enter_context(SB(nc, "r_b", [128, NT, NE], F32))
            rm = _es.enter_context(SB(nc, "r_m", [128, NT * 4], F32))
            rm2 = _es.enter_context(SB(nc, "r_m2", [128, NT * 4], F32))
            rg = _es.enter_context(SB(nc, "r_g", [128, NT], F32))
            rbias = _es.enter_context(SB(nc, "rbias", [128, NE], F32))
            t = Tok()
            if T0 > 0:
                S.op("dve", lambda e: e.memset(rl[:, 0:T0, :], 0.0), reads=[G.t_rl], writes=[G.t_rl])
            S.dma(rbias[:], I["router_bias"].partition_broadcast(128), writes=[t])
            v3 = lambda a: a[:].rearrange("p n (g x) -> p (n g) x", x=4)
            S.op("act", lambda e: e.activation(out=sc[:], in_=rl[:], func=AF.Sigmoid), reads=[G.t_rl], writes=[t])
            S.op("dve", lambda e: e.tensor_tensor(out=sel[:], in0=sc[:], in1=rbias[:].unsqueeze(1).to_broadcast([128, NT, NE]), op=ALU.add),
                 reads=[t], writes=[t])
            S.op("dve", lambda e: e.tensor_reduce(out=rm[:], in_=v3(sel), axis=AX.X, op=ALU.max), reads=[t], writes=[t])
            S.op("dve", lambda e: e.tensor_tensor(out=v3(ra), in0=v3(sel), in1=rm[:].unsqueeze(2).to_broadcast([128, NT * 4, 4]), op=ALU.is_equal),
                 reads=[t], writes=[t])
            S.op("dve", lambda e: e.scalar_tensor_tensor(out=rb[:], in0=ra[:], scalar=-BIG, in1=sel[:], op0=ALU.mult, op1=ALU.add),
                 reads=[t], writes=[t])
            S.op("dve", lambda e: e.tensor_reduce(out=rm2[:], in_=v3(rb), axis=AX.X, op=ALU.max), reads=[t], writes=[t])
            S.op("dve", lambda e: e.tensor_tensor(out=rm[:], in0=rm[:], in1=rm2[:], op=ALU.add), reads=[t], writes=[t])
            S.op("dve", lambda e: e.tensor_reduce(out=rg[:], in_=rm[:].rearrange("p (n g) -> p n g", g=4), axis=AX.X, op=ALU.max),
                 reads=[t], writes=[t])
            S.op("dve", lambda e: e.tensor_tensor(out=rm2[:].rearrange("p (n g) -> p n g", g=4), in0=rm[:].rearrange("p (n g) -> p n g", g=4),
                                                  in1=rg[:].unsqueeze(2).to_broadcast([128, NT, 4]), op=ALU.is_equal), reads=[t], writes=[t])
            S.op("dve", lambda e: e.tensor_scalar(out=rm2[:], in0=rm2[:], scalar1=1.0, scalar2=BIG, op0=ALU.subtract, op1=ALU.mult),
                 reads=[t], writes=[t])
            S.op("dve", lambda e: e.tensor_tensor(out=v3(sel), in0=v3(sel), in1=rm2[:].unsqueeze(2).to_broadcast([128, NT * 4, 4]), op=ALU.add),
                 reads=[t], writes=[t])
            S.op("dve", lambda e: e.tensor_reduce(out=rg[:], in_=sel[:], axis=AX.X, op=ALU.max), reads=[t], writes=[t])
            S.op("dve", lambda e: e.tensor_tensor(out=ra[:], in0=sel[:], in1=rg[:].unsqueeze(2).to_broadcast([128, NT, NE]), op=ALU.is_equal),
                 reads=[t], writes=[t])
            S.op("dve", lambda e: e.scalar_tensor_tensor(out=sel[:], in0=ra[:], scalar=-BIG, in1=sel[:], op0=ALU.mult, op1=ALU.add),
                 reads=[t], writes=[t])
            S.op("dve", lambda e: e.tensor_reduce(out=rg[:], in_=sel[:], axis=AX.X, op=ALU.max), reads=[t], writes=[t])
            S.op("dve", lambda e: e.tensor_tensor(out=rb[:], in0=sel[:], in1=rg[:].unsqueeze(2).to_broadcast([128, NT, NE]), op=ALU.is_equal),
                 reads=[t], writes=[t])
            S.op("dve", lambda e: e.tensor_tensor(out=ra[:], in0=ra[:], in1=rb[:], op=ALU.add), reads=[t], writes=[t])
            S.op("dve", lambda e: e.tensor_tensor(out=ra[:], in0=ra[:], in1=sc[:], op=ALU.mult), reads=[t], writes=[t])
            S.op("dve", lambda e: e.tensor_reduce(out=rg[:], in_=ra[:], axis=AX.X, op=ALU.add), reads=[t], writes=[t])
            S.op("dve", lambda e: e.reciprocal(out=rg[:], in_=rg[:]), reads=[t], writes=[t])
            S.op("dve", lambda e: e.tensor_tensor(out=comb[:], in0=ra[:], in1=rg[:].unsqueeze(2).to_broadcast([128, NT, NE]), op=ALU.mult),
                 reads=[t], writes=[t_comb])
            S.barrier()
        SGT = 12
        with ExitStack() as _es:
            acc = _es.enter_context(SB(nc, "acc", [128, SGT, D], F32))
            wg = _es.enter_context(SB(nc, "wg", [128, 2, 8, DFF], BF16))
            wu = _es.enter_context(SB(nc, "wu", [128, 2, 8, DFF], BF16))
            wd = _es.enter_context(SB(nc, "wd", [128, 2, 2, D], BF16))
            sgl = _es.enter_context(SB(nc, "sgl", [128, 2, 512], F32))
            aa = _es.enter_context(SB(nc, "aa", [128, 2, 512], BF16))
            xt = _es.enter_context(SB(nc, "xt", [128, 2, D], F32))
            gfb = _es.enter_context(SB(nc, "gfb", [128, D], F32))
            ss = _es.enter_context(SB(nc, "ss", [128, 4], F32))
            sq = _es.enter_context(SB(nc, "sq", [128, D], F32))
            pgu = _es.enter_context(PS(nc, "pgu", [128, 4, 512], F32))
            py = _es.enter_context(PS(nc, "py", [128, 2, 2, 512], F32))
            t_acc, t_w, t_sgl, t_aa, t_pgu, t_py, t_x, t_gf, t_ss, t_sq = Tok(), [Tok(), Tok()], Tok(), Tok(), Tok(), [Tok(), Tok()], [Tok(), Tok()], Tok(), Tok(), Tok()
            if last:
                S.dma(gfb[:], I["g_final"].partition_broadcast(128), writes=[t_gf])
            yi = 0
            for s0 in range(T0, NT, SGT):
                tiles = list(range(s0, min(NT, s0 + SGT)))
                for ex in range(NE):
                    wb_ = ex % 2
                    S.dma(wg[:, wb_], G.wb_gate[li][ex].rearrange("(k p) f -> p k f", p=128), reads=[G.t_wb], writes=[t_w[wb_]])
                    S.dma(wu[:, wb_], G.wb_up[li][ex].rearrange("(k p) f -> p k f", p=128), reads=[G.t_wb], writes=[t_w[wb_]])
                    S.dma(wd[:, wb_], G.wb_down[li][ex].rearrange("(j p) c -> p j c", p=128), reads=[G.t_wb], writes=[t_w[wb_]])
                    for b0 in range(0, len(tiles), 4):
                        bt = tiles[b0:b0 + 4]
                        n = len(bt) * 128
                        c0 = bt[0] * 128
                        for wi, wt in enumerate((wg, wu)):
                            for jj in range(2):
                                for k in range(8):
                                    S.op("pe", lambda e: e.matmul(pgu[:, wi * 2 + jj, 0:n], lhsT=wt[:, wb_, k, jj * 128:(jj + 1) * 128],
                                                                  rhs=h2T[:, k, c0:c0 + n], start=(k == 0), stop=(k == 7)),
                                         reads=[t_w[wb_], G.t_h2T], writes=[t_pgu])
                        S.op("act", lambda e: e.activation(out=sgl[:, :, 0:n], in_=pgu[:, 0:2, 0:n], func=AF.Silu), reads=[t_pgu], writes=[t_sgl])
                        S.op("dve", lambda e: e.tensor_tensor(out=aa[:, :, 0:n], in0=sgl[:, :, 0:n], in1=pgu[:, 2:4, 0:n], op=ALU.mult),
                             reads=[t_sgl, t_pgu], writes=[t_aa])
                        for ti, tl in enumerate(bt):
                            yb = yi % 2
                            yi += 1
                            for half in range(2):
                                for jj in range(2):
                                    S.op("pe", lambda e: e.matmul(py[:, yb, half, :], lhsT=aa[:, jj, ti * 128:(ti + 1) * 128],
                                                                  rhs=wd[:, wb_, jj, half * 512:(half + 1) * 512], start=(jj == 0), stop=(jj == 1)),
                                         reads=[t_aa, t_w[wb_]], writes=[t_py[yb]])
                            al = acc[:, tl - s0, :]
                            pyv = py[:, yb].rearrange("p a c -> p (a c)")
                            if ex == 0:
                                S.op("dve", lambda e: e.tensor_scalar(out=al, in0=pyv, scalar1=comb[:, tl, ex:ex + 1], scalar2=None, op0=ALU.mult),
                                     reads=[t_py[yb], t_comb], writes=[t_acc])
                            else:
                                S.op("dve", lambda e: e.scalar_tensor_tensor(out=al, in0=pyv, scalar=comb[:, tl, ex:ex + 1], in1=al,
                                                                             op0=ALU.mult, op1=ALU.add), reads=[t_py[yb], t_comb, t_acc], writes=[t_acc])
                for tl in tiles:
                    b = tl % 2
                    j = 1 if tl < 2 else 0
                    S.dma(xt[:, b, :], G.xres[tl * 128:(tl + 1) * 128, :], reads=[G.t_xres[tl]], writes=[t_x[b]])
                    al = acc[:, tl - s0, :]
                    S.op("dve", lambda e: e.tensor_tensor(out=al, in0=al, in1=G.gtb[:, 1, j, :], op=ALU.mult), reads=[t_acc, G.t_gtb], writes=[t_acc])
                    S.op("dve", lambda e: e.tensor_tensor(out=xt[:, b, :], in0=al, in1=xt[:, b, :], op=ALU.add), reads=[t_acc, t_x[b]], writes=[t_x[b]])
                    if not last:
                        S.dma(G.xres[tl * 128:(tl + 1) * 128, :], xt[:, b, :], reads=[t_x[b]], writes=[G.t_xres[tl]])
                    else:
                        xv = xt[:, b, :]
                        S.op("act", lambda e: e.activation(out=sq[:], in_=xv, func=AF.Square, accum_out=ss[:, 0:1]), reads=[t_x[b]], writes=[t_sq, t_ss])
                        S.op("act", lambda e: e.activation(out=ss[:, 1:2], in_=ss[:, 0:1], func=AF.Sqrt, bias=G.epsc[:], scale=1.0 / D),
                             reads=[t_ss, G.t_c], writes=[t_ss])
                        S.op("dve", lambda e: e.reciprocal(out=ss[:, 2:3], in_=ss[:, 1:2]), reads=[t_ss], writes=[t_ss])
                        S.op("dve", lambda e: e.scalar_tensor_tensor(out=xv, in0=xv, scalar=ss[:, 2:3], in1=gfb[:], op0=ALU.mult, op1=ALU.mult),
                             reads=[t_x[b], t_ss, t_gf], writes=[t_x[b]])
                        S.dma(G.out[(tl - 2) * 128:(tl - 1) * 128, :], xv, reads=[t_x[b]], writes=[])


def ssd(G, li):
    nc, S, I = G.nc, G.S, G.I
    hT = G.hT
    need_ctx = li < DEPTH - 1
    wv32 = I["w_in"][li].rearrange("(k p) c -> p k c", p=128)
    wvb = G.wb_in[li].rearrange("(k p) c -> p k c", p=128)
    XB0 = 1024
    with ExitStack() as _es:
        xbcT = _es.enter_context(SB(nc, "xbcT", [128, 5, NTOK], BF16))
        xs_tok = _es.enter_context(SB(nc, "xs_tok", [128, NT, 384], BF16))
        B_tok = _es.enter_context(SB(nc, "B_tok", [128, NT, 128], BF16))
        lndt = _es.enter_context(SB(nc, "lndt", [128, NT, 12], F32))
        dta = _es.enter_context(SB(nc, "dta", [128, NT, 12], F32))
        ea = _es.enter_context(SB(nc, "ea", [128, NT, 12], F32))
        ww = _es.enter_context(SB(nc, "ww", [128, NT, 12], F32))
        eT = _es.enter_context(SB(nc, "eT", [128, NT, 12], F32))
        t_xbc, t_xs, t_dt = Tok(), Tok(), Tok()
        with ExitStack() as _es2:
            dts = _es2.enter_context(SB(nc, "dts", [128, NT, 12], F32))
            wst = _es2.enter_context(SB(nc, "wst", [128, 8, 128], F32))
            cwb = _es2.enter_context(SB(nc, "cwb", [128, 3, 128], F32))
            wj = _es2.enter_context(SB(nc, "wj", [128, 3, 8, 128], BF16))
            wdt = _es2.enter_context(SB(nc, "wdt", [128, 8, 12], BF16))
            dtb = _es2.enter_context(SB(nc, "dtb", [128, 2, 12], F32))
            tot = _es2.enter_context(SB(nc, "tot", [128, NT, 12], F32))
            wcol = _es2.enter_context(SB(nc, "wcol", [128, NT, 12], F32))
            pp = _es2.enter_context(PS(nc, "pp", [128, 512], F32))
            pdt = _es2.enter_context(PS(nc, "pdt", [128, NT, 12], F32))
            ptb = _es2.enter_context(PS(nc, "ptb", [128, 4, 128], BF16))
            pc = _es2.enter_context(PS(nc, "pc", [128, NT, 12], F32))
            t_wst, t_cwb, t_wj, t_pp, t_wdt, t_pdt, t_ptb, t_a, t_pc = Tok(), Tok(), Tok(), Tok(), Tok(), Tok(), Tok(), Tok(), Tok()
            ocb = COLS["ssd_conv_b"][0]
            for ch in range(5):
                S.dma(wst[:], wv32[:, :, XB0 + ch * 128:XB0 + (ch + 1) * 128], writes=[t_wst])
                for j in range(3):
                    S.dma(cwb[:, j, :], I["ssd_conv_w"][li, j, ch * 128:(ch + 1) * 128].partition_broadcast(128), writes=[t_cwb])
                for j in range(3):
                    S.op("dve", lambda e: e.tensor_tensor(out=wj[:, j], in0=wst[:], in1=cwb[:, j:j + 1, :].to_broadcast([128, 8, 128]), op=ALU.mult),
                         reads=[t_wst, t_cwb], writes=[t_wj])
                for (c0, t0, n, tile0, ntile) in BLOCKS:
                    for j in range(3):
                        for k in range(8):
                            S.op("pe", lambda e: e.matmul(pp[:, 0:n], lhsT=wj[:, j, k, :], rhs=hT[:, k, c0 + j - 1:c0 + j - 1 + n],
                                                          start=(j == 0 and k == 0), stop=(j == 2 and k == 7)), reads=[t_wj, G.t_hT], writes=[t_pp])
                    S.op("act", lambda e: e.activation(out=xbcT[:, ch, t0:t0 + n], in_=pp[:, 0:n], func=AF.Silu, bias=G.cols[:, ocb + ch:ocb + ch + 1]),
                         reads=[t_pp, G.t_cols], writes=[t_xbc])
            S.dma(wdt[:], wvb[:, :, 1664:1676], reads=[G.t_wb], writes=[t_wdt])
            S.dma(dtb[:, 0, :], I["ssd_dt_bias"][li].rearrange("a h -> (a h)").partition_broadcast(128), writes=[t_wdt])
            S.dma(dtb[:, 1, :], I["ssd_a_log"][li].rearrange("a h -> (a h)").partition_broadcast(128), writes=[t_wdt])
            for tl in range(NT):
                cc = colof(tl)
                for k in range(8):
                    S.op("pe", lambda e: e.matmul(pdt[:, tl, :], lhsT=hT[:, k, cc:cc + 128], rhs=wdt[:, k, :], start=(k == 0), stop=(k == 7)),
                         reads=[t_wdt, G.t_hT], writes=[t_pdt])
            S.op("dve", lambda e: e.tensor_tensor(out=dts[:], in0=pdt[:], in1=dtb[:, 0:1, :].to_broadcast([128, NT, 12]), op=ALU.add),
                 reads=[t_pdt, t_wdt], writes=[t_dt])
            S.op("act", lambda e: e.activation(out=dts[:], in_=dts[:], func=AF.Exp), reads=[t_dt], writes=[t_dt])
            S.op("act", lambda e: e.activation(out=dts[:], in_=dts[:], func=AF.Ln, bias=1.0), reads=[t_dt], writes=[t_dt])
            S.op("act", lambda e: e.activation(out=lndt[:], in_=dts[:], func=AF.Ln), reads=[t_dt], writes=[t_dt])
            S.op("act", lambda e: e.activation(out=dtb[:, 1, :], in_=dtb[:, 1, :], func=AF.Exp), reads=[t_wdt], writes=[t_wdt])
            S.op("dve", lambda e: e.scalar_tensor_tensor(out=dta[:], in0=dts[:], scalar=-1.0, in1=dtb[:, 1:2, :].to_broadcast([128, NT, 12]),
                                                         op0=ALU.mult, op1=ALU.mult), reads=[t_dt, t_wdt], writes=[t_a])
            for tl in range(NT):
                for c in range(4):
                    S.op("pe", lambda e: e.transpose(out=ptb[:, c, :], in_=xbcT[:, c, tl * 128:(tl + 1) * 128], identity=G.identB),
                         reads=[t_xbc, G.t_c], writes=[t_ptb])
                S.op("dve", lambda e: e.tensor_copy(out=xs_tok[:, tl, :], in_=ptb[:, 0:3, :].rearrange("p c t -> p (c t)")), reads=[t_ptb], writes=[t_xs])
                S.op("dve", lambda e: e.tensor_copy(out=B_tok[:, tl, :], in_=ptb[:, 3, :]), reads=[t_ptb], writes=[t_xs])
            for dr in range(2):
                S.op("pe", lambda e: e.matmul(pc[:].rearrange("p n h -> p (n h)"), lhsT=G.cm[:, 1 + dr, :], rhs=dta[:].rearrange("p n h -> p (n h)"),
                                              start=True, stop=True), reads=[t_a, G.t_c, t_dt], writes=[t_pc])
                S.op("dve", lambda e: e.tensor_copy(out=wcol[:, :, dr * 6:(dr + 1) * 6], in_=pc[:, :, dr * 6:(dr + 1) * 6]), reads=[t_pc], writes=[t_dt])
            S.op("pe", lambda e: e.matmul(pc[:].rearrange("p n h -> p (n h)"), lhsT=G.ones[:], rhs=dta[:].rearrange("p n h -> p (n h)"), start=True, stop=True),
                 reads=[t_a, G.t_c, t_dt], writes=[t_pc])
            S.op("dve", lambda e: e.tensor_copy(out=tot[:], in_=pc[:]), reads=[t_pc], writes=[t_dt])
            S.op("act", lambda e: e.activation(out=ea[:], in_=wcol[:], func=AF.Exp), reads=[t_dt], writes=[t_dt])
            S.op("act", lambda e: e.activation(out=eT[:], in_=tot[:], func=AF.Exp), reads=[t_dt], writes=[t_dt])
            S.op("dve", lambda e: e.tensor_tensor(out=ww[:], in0=tot[:], in1=wcol[:], op=ALU.subtract), reads=[t_dt], writes=[t_dt])
            S.op("dve", lambda e: e.tensor_tensor(out=ww[:], in0=ww[:], in1=lndt[:], op=ALU.add), reads=[t_dt], writes=[t_dt])
            S.op("act", lambda e: e.activation(out=ww[:], in_=ww[:], func=AF.Exp), reads=[t_dt], writes=[t_dt])
            S.barrier()
        with ExitStack() as _es2:
            wz = _es2.enter_context(SB(nc, "wz", [128, 8, 384], BF16))
            dbc = _es2.enter_context(SB(nc, "dbc", [128, 6, 64], F32))
            d6 = _es2.enter_context(SB(nc, "d6", [128, 6], F32))
            nwb = _es2.enter_context(SB(nc, "nwb", [128, 384], F32))
            hst = _es2.enter_context(SB(nc, "hst", [128, 192], F32))
            hsb = _es2.enter_context(SB(nc, "hsb", [128, 192], BF16))
            xw = _es2.enter_context(SB(nc, "xw", [128, 2, 192], BF16))
            ybt = _es2.enter_context(SB(nc, "ybt", [128, 2, 384], F32))
            Sm = _es2.enter_context(SB(nc, "Sm", [128, 2, 2, 128], F32))
            rA = _es2.enter_context(SB(nc, "rA", [128, 4, 128], F32))
            Dd = _es2.enter_context(SB(nc, "Dd", [128, 4, 128], F32))
            Mt = _es2.enter_context(SB(nc, "Mt", [128, 4, 128], BF16))
            acc = _es2.enter_context(SB(nc, "acc", [128, 384], F32))
            tmp = _es2.enter_context(SB(nc, "tmp", [128, 384], F32))
            zs = _es2.enter_context(SB(nc, "zs", [128, 384], F32))
            ssq = _es2.enter_context(SB(nc, "ssq", [128, 4], F32))
            ob = _es2.enter_context(SB(nc, "ob", [128, 384], BF16))
            oT = _es2.enter_context(SB(nc, "oT", [128, 2, 3, 128], BF16))
            pis = _es2.enter_context(PS(nc, "pis", [128, 2, 192], F32))
            ps_st = _es2.enter_context(PS(nc, "ps_st", [128, 2, 512], F32))
            pseg = _es2.enter_context(PS(nc, "pseg", [128, 3, 512], F32))
            py = _es2.enter_context(PS(nc, "py", [128, 384], F32))
            pz = py
            ptr = _es2.enter_context(PS(nc, "ptr", [128, 3, 128], BF16))
            t_wz, t_db, t_h, t_xw, t_yb, t_Sm, t_acc, t_tmp, t_zs, t_ssq, t_ob, t_oT = Tok(), Tok(), Tok(), Tok(), [Tok(), Tok()], Tok(), Tok(), Tok(), Tok(), Tok(), Tok(), [Tok(), Tok()]
            t_rA, t_Dd, t_Mt, t_pseg = [Tok() for _ in range(4)], [Tok() for _ in range(4)], [Tok() for _ in range(4)], [Tok() for _ in range(3)]
            t_pis, t_pst, t_py, t_ptr = Tok(), Tok(), Tok(), Tok()
            t_pz = t_py
            S.dma(wz[:], wvb[:, :, 640:1024], reads=[G.t_wb], writes=[t_wz])
            S.dma(d6[:], I["ssd_d"][li].partition_broadcast(128), writes=[t_db])
            S.dma(nwb[:], I["ssd_norm"][li].partition_broadcast(128), writes=[t_db])
            S.op("dve", lambda e: e.tensor_copy(out=dbc[:], in_=d6[:].unsqueeze(2).to_broadcast([128, 6, 64])), reads=[t_db], writes=[t_db])
            yb_d = G.ssd_yb

            def carry_step(c, dr, want_out, dst):
                for g in range(2):
                    hd0 = dr * 6 + 3 * g
                    if want_out:
                        S.op("pe", lambda e: e.matmul(pis[:, 0, :], lhsT=xbcT[g * 64:(g + 1) * 64, 4, c * 128:(c + 1) * 128], rhs=hsb[g * 64:(g + 1) * 64, :],
                                                      start=True, stop=True), reads=[t_xbc, t_h], writes=[t_pis])
                        S.op("dve", lambda e: e.tensor_tensor(out=dst[:, g * 192:(g + 1) * 192].rearrange("p (a d) -> p a d", a=3),
                                                              in0=pis[:, 0, :].rearrange("p (a d) -> p a d", a=3),
                                                              in1=ea[:, c, hd0:hd0 + 3].unsqueeze(2).to_broadcast([128, 3, 64]), op=ALU.mult),
                             reads=[t_pis, t_dt], writes=[dst_tok[0]])
                    S.op("dve", lambda e: e.tensor_tensor(out=xw[:, g, :].rearrange("p (a d) -> p a d", a=3),
                                                          in0=xs_tok[:, c, g * 192:(g + 1) * 192].rearrange("p (a d) -> p a d", a=3),
                                                          in1=ww[:, c, hd0:hd0 + 3].unsqueeze(2).to_broadcast([128, 3, 64]), op=ALU.mult),
                         reads=[t_xs, t_dt], writes=[t_xw])
                    gs = slice(g * 64, (g + 1) * 64)
                    S.op("pe", lambda e: e.matmul(pis[:, 1, :], lhsT=B_tok[:, c, :], rhs=xw[:, g, :], start=True, stop=True),
                         reads=[t_xs, t_xw], writes=[t_pis])
                    S.op("dve", lambda e: e.tensor_tensor(out=hst[gs, :].rearrange("p (a d) -> p a d", a=3),
                                                          in0=hst[gs, :].rearrange("p (a d) -> p a d", a=3),
                                                          in1=eT[gs, c, hd0:hd0 + 3].unsqueeze(2).to_broadcast([64, 3, 64]), op=ALU.mult),
                         reads=[t_h, t_dt], writes=[t_h])
                    S.op("dve", lambda e: e.tensor_tensor(out=hst[gs, :], in0=hst[gs, :], in1=pis[gs, 1, :], op=ALU.add),
                         reads=[t_h, t_pis], writes=[t_h])
                    S.op("act", lambda e: e.copy(out=hsb[gs, :], in_=hst[gs, :]), reads=[t_h], writes=[t_h])

            S.op("dve", lambda e: e.memset(hst[:], 0.0), writes=[t_h])
            S.op("dve", lambda e: e.memset(hsb[:], 0.0), writes=[t_h])
            order_b = [1, 0] + list(range(NT - 1, 1, -1))
            for ci, c in enumerate(order_b):
                want = need_ctx or c >= 2
                b = ci % 2
                dst_tok = [t_yb[b]]
                carry_step(c, 1, want, ybt[:, b, :])
                if want:
                    S.dma(yb_d[c * 128:(c + 1) * 128, :], ybt[:, b, :], reads=[t_yb[b]], writes=[G.t_ssdyb[c]])
            S.op("dve", lambda e: e.memset(hst[:], 0.0), reads=[t_h], writes=[t_h])
            S.op("dve", lambda e: e.memset(hsb[:], 0.0), reads=[t_h], writes=[t_h])
            on = COLS["ssd_norm"][0]
            it = 0
            for c in range(NT):
                want = need_ctx or c >= 2
                dst_tok = [t_acc]
                carry_step(c, 0, want, acc[:])
                if not want:
                    continue
                b = c % 2
                tc0 = c * 128
                S.dma(ybt[:, b, :], yb_d[c * 128:(c + 1) * 128, :], reads=[G.t_ssdyb[c]], writes=[t_yb[b]])
                cc = colof(c)
                for k in range(8):
                    S.op("pe", lambda e: e.matmul(pz[:], lhsT=hT[:, k, cc:cc + 128], rhs=wz[:, k, :], start=(k == 0), stop=(k == 7)),
                         reads=[t_wz, G.t_hT], writes=[t_pz])
                S.op("act", lambda e: e.activation(out=zs[:], in_=pz[:], func=AF.Silu), reads=[t_pz], writes=[t_zs])
                for g in range(2):
                    S.op("pe", lambda e: e.matmul(ps_st[:, g, 0:128], lhsT=xbcT[g * 64:(g + 1) * 64, 3, tc0:tc0 + 128], rhs=xbcT[g * 64:(g + 1) * 64, 4, tc0:tc0 + 128],
                                                  start=True, stop=True), reads=[t_xbc], writes=[t_pst])
                for g in range(2):
                    for dr in range(2):
                        S.op("dve", lambda e: e.tensor_tensor(out=Sm[:, g, dr, :], in0=ps_st[:, g, 0:128], in1=G.cm[:, 1 + dr, :], op=ALU.mult),
                             reads=[t_pst, G.t_c], writes=[t_Sm])
                steps = [(h, dr) for h in range(6) for dr in range(2)]

                def st_a(i):
                    h, dr = steps[i]
                    hd = dr * 6 + h
                    r4, p3 = (it + i) % 4, (it + i) % 3
                    S.op("dve", lambda e: e.tensor_scalar(out=rA[:, r4, :], in0=G.cm[:, 1 + dr, :], scalar1=dta[:, c, hd:hd + 1], scalar2=None, op0=ALU.mult),
                         reads=[G.t_c, t_dt], writes=[t_rA[r4]])
                    S.op("pe", lambda e: e.matmul(pseg[:, p3, 0:128], lhsT=G.cm[:, 8 - dr, :], rhs=rA[:, r4, :], start=True, stop=True),
                         reads=[t_rA[r4], G.t_c], writes=[t_pseg[p3]])
                    S.op("act", lambda e: e.activation(out=Dd[:, r4, :], in_=pseg[:, p3, 0:128], func=AF.Exp, bias=lndt[:, c, hd:hd + 1]),
                         reads=[t_pseg[p3], t_dt], writes=[t_Dd[r4]])

                st_a(0)
                st_a(1)
                for i, (h, dr) in enumerate(steps):
                    r4 = (it + i) % 4
                    if i + 2 < len(steps):
                        st_a(i + 2)
                    S.op("dve", lambda e: e.tensor_tensor(out=Mt[:, r4, :], in0=Dd[:, r4, :], in1=Sm[:, h // 3, dr, :], op=ALU.mult),
                         reads=[t_Dd[r4], t_Sm], writes=[t_Mt[r4]])
                    S.op("pe", lambda e: e.matmul(py[:, h * 64:(h + 1) * 64], lhsT=Mt[:, r4, :], rhs=xs_tok[:, c, h * 64:(h + 1) * 64],
                                                  start=(dr == 0), stop=(dr == 1)), reads=[t_Mt[r4], t_xs], writes=[t_py])
                it += len(steps)
                S.op("dve", lambda e: e.tensor_tensor(out=acc[:], in0=acc[:], in1=py[:], op=ALU.add), reads=[t_acc, t_py], writes=[t_acc])
                S.op("dve", lambda e: e.tensor_tensor(out=acc[:], in0=acc[:], in1=ybt[:, b, :], op=ALU.add), reads=[t_acc, t_yb[b]], writes=[t_acc])
                S.op("dve", lambda e: e.tensor_tensor(out=tmp[:], in0=xs_tok[:, c, :], in1=dbc[:].rearrange("p a d -> p (a d)"), op=ALU.mult),
                     reads=[t_xs, t_db], writes=[t_tmp])
                S.op("dve", lambda e: e.tensor_tensor(out=acc[:], in0=acc[:], in1=tmp[:], op=ALU.add), reads=[t_acc, t_tmp], writes=[t_acc])
                S.op("dve", lambda e: e.tensor_tensor(out=acc[:], in0=acc[:], in1=zs[:], op=ALU.mult), reads=[t_acc, t_zs], writes=[t_acc])
                for g in range(2):
                    S.op("act", lambda e: e.activation(out=tmp[:, g * 192:(g + 1) * 192], in_=acc[:, g * 192:(g + 1) * 192], func=AF.Square, accum_out=ssq[:, g:g + 1]),
                         reads=[t_acc], writes=[t_tmp, t_ssq])
                S.op("act", lambda e: e.activation(out=ssq[:, 2:4], in_=ssq[:, 0:2], func=AF.Sqrt, bias=G.epsc[:], scale=1.0 / 192), reads=[t_ssq, G.t_c], writes=[t_ssq])
                S.op("dve", lambda e: e.reciprocal(out=ssq[:, 2:4], in_=ssq[:, 2:4]), reads=[t_ssq], writes=[t_ssq])
                for g in range(2):
                    S.op("dve", lambda e: e.scalar_tensor_tensor(out=ob[:, g * 192:(g + 1) * 192], in0=acc[:, g * 192:(g + 1) * 192], scalar=ssq[:, 2 + g:3 + g],
                                                                 in1=nwb[:, g * 192:(g + 1) * 192], op0=ALU.mult, op1=ALU.mult), reads=[t_acc, t_ssq, t_db], writes=[t_ob])
                for c3 in range(3):
                    S.op("pe", lambda e: e.transpose(out=ptr[:, c3, :], in_=ob[:, c3 * 128:(c3 + 1) * 128], identity=G.identB), reads=[t_ob, G.t_c], writes=[t_ptr])
                S.op("act", lambda e: e.copy(out=oT[:, b], in_=ptr[:]), reads=[t_ptr], writes=[t_oT[b]])
                S.dma(G.mixT[384:768, tc0:tc0 + 128].rearrange("(c p) t -> p c t", p=128), oT[:, b], reads=[t_oT[b]], writes=[G.t_mix])
        if "ssd" in G.dbg and li == 0:
            dump_bf16(G, G.mixT[384:512, 256:768], G.dbg["ssd"], [G.t_mix])


HY0 = 1676
SEGS = {"lat": dict(L=4096, NC=32, KC=17, off=NCTX), "ctx": dict(L=256, NC=2, KC=2, off=0)}


def hyena_inproj(G, li):
    nc, S, I = G.nc, G.S, G.I
    hT = G.hT
    need_ctx = li < DEPTH - 1
    wv32 = I["w_in"][li].rearrange("(k p) c -> p k c", p=128)
    with ExitStack() as _es:
        wst = _es.enter_context(SB(nc, "wst", [128, 8, 128], F32))
        cwb = _es.enter_context(SB(nc, "cwb", [128, 3, 128], F32))
        wj = _es.enter_context(SB(nc, "wjh", [128, 3, 8, 768], BF16))
        hb = _es.enter_context(SB(nc, "hb", [128, 768], F32))
        hv = _es.enter_context(SB(nc, "hv", [128, 2, 768], BF16))
        ph = _es.enter_context(PS(nc, "ph", [128, 2, 512], F32))
        t_wst, t_cwb, t_wj, t_hb, t_hv, t_ph = Tok(), Tok(), Tok(), Tok(), [Tok(), Tok()], Tok()
        S.dma(hb[:], I["hy_conv_b"][li].partition_broadcast(128), writes=[t_hb])
        for cc in range(6):
            S.dma(wst[:], wv32[:, :, HY0 + cc * 128:HY0 + (cc + 1) * 128], writes=[t_wst])
            for j in range(3):
                S.dma(cwb[:, j, :], I["hy_conv_w"][li, j, cc * 128:(cc + 1) * 128].partition_broadcast(128), writes=[t_cwb])
            for j in range(3):
                S.op("dve", lambda e: e.tensor_tensor(out=wj[:, j, :, cc * 128:(cc + 1) * 128], in0=wst[:],
                                                      in1=cwb[:, j:j + 1, :].to_broadcast([128, 8, 128]), op=ALU.mult),
                     reads=[t_wst, t_cwb], writes=[t_wj])
        dv = G.hyv.rearrange("m t c -> t m c")
        for tl in range(NT):
            if tl < 2 and not need_ctx:
                continue
            col = colof(tl)
            b = tl % 2
            for half in range(2):
                for j in range(3):
                    for k in range(8):
                        S.op("pe", lambda e: e.matmul(ph[:, half, 0:384], lhsT=hT[:, k, col + j - 1:col + j - 1 + 128],
                                                      rhs=wj[:, j, k, half * 384:(half + 1) * 384], start=(j == 0 and k == 0), stop=(j == 2 and k == 7)),
                             reads=[t_wj, G.t_hT], writes=[t_ph])
            S.op("dve", lambda e: e.tensor_tensor(out=hv[:, b, :].rearrange("p (a c) -> p a c", a=2), in0=ph[:, :, 0:384],
                                                  in1=hb[:].rearrange("p (a c) -> p a c", a=2), op=ALU.add), reads=[t_ph, t_hb], writes=[t_hv[b]])
            S.dma(dv[tl * 128:(tl + 1) * 128], hv[:, b, :].rearrange("p (m c) -> p m c", m=3), reads=[t_hv[b]], writes=[G.t_hyv])


def hyena_fft(G, li):
    nc, S, I = G.nc, G.S, G.I
    need_ctx = li < DEPTH - 1
    for sname in (("lat", "ctx") if need_ctx else ("lat",)):
        P = SEGS[sname]
        L, NCk, KC, off = P["L"], P["NC"], P["KC"], P["off"]
        NH = NCk // 2
        FT, IT, FE, WIN, MH = I["ft_" + sname], I["it_" + sname], I["fe_" + sname], I["win_" + sname], I["mh_" + sname]
        Kf = G.Kf[sname]
        t_kf = Tok()
        with ExitStack() as _es:
            hk = _es.enter_context(SB(nc, "hk", [128, NCk, 1024], BF16))
            fe = _es.enter_context(SB(nc, "fe", [33, L], F32))
            h1 = _es.enter_context(SB(nc, "h1", [64, L], F32))
            h2 = _es.enter_context(SB(nc, "h2", [64, L], F32))
            w1 = _es.enter_context(SB(nc, "w1", [33, 64], F32))
            w2 = _es.enter_context(SB(nc, "w2", [64, 64], F32))
            w3 = _es.enter_context(SB(nc, "w3", [64, 1024], F32))
            pre = _es.enter_context(SB(nc, "pre", [64, 512], F32))
            pr2 = _es.enter_context(SB(nc, "pr2", [64, 512], F32))
            t_pr2 = Tok()
            MAGIC = 1.5 * 2 ** 23
            wint = _es.enter_context(SB(nc, "wint", [128, 2, 2, 256], F32))
            hbias = _es.enter_context(SB(nc, "hbias", [128, 512], F32))
            mh = _es.enter_context(SB(nc, "mh", [128, KC], F32))
            slab = _es.enter_context(SB(nc, "slab", [128, 2, NCk, 2, 128], BF16))
            xo = _es.enter_context(SB(nc, "xo", [128, 2, 512], F32))
            sd = _es.enter_context(SB(nc, "sd", [128, 2, 2, 2, 512], F32))
            kf = _es.enter_context(SB(nc, "kf", [128, 2, 2, 2, 256], BF16))
            pm = _es.enter_context(PS(nc, "pm", [64, 512], F32))
            phh = _es.enter_context(PS(nc, "phh", [128, 2, 512], F32))
            psk = _es.enter_context(PS(nc, "psk", [128, 2, 2, 512], F32))
            t_hk, t_fe, t_h1, t_h2, t_w, t_pre, t_win, t_slab, t_eo, t_sd, t_kft, t_pm, t_phh, t_psk = \
                Tok(), Tok(), Tok(), Tok(), Tok(), Tok(), [Tok(), Tok()], [Tok(), Tok()], Tok(), Tok(), Tok(), Tok(), Tok(), Tok()
            S.dma(fe[:], FE[:, :], writes=[t_fe])
            S.dma(w1[:], I["hy_w1"][li], writes=[t_w])
            S.dma(w2[:], I["hy_w2"][li], writes=[t_w])
            S.dma(w3[:], I["hy_w3"][li], writes=[t_w])
            S.dma(hbias[:], I["hy_bias"][li].rearrange("o c -> (o c)").partition_broadcast(128), writes=[t_w])
            S.dma(mh[:], MH[:, :], writes=[t_w])
            ob1, ofr, ob2 = COLS["hy_b1"][0], COLS["hy_freq"][0], COLS["hy_b2"][0]
            for (src, t_src, wt, kk, bcol, dst, t_dst) in ((fe, t_fe, w1, 33, ob1, h1, t_h1), (h1, t_h1, w2, 64, ob2, h2, t_h2)):
                for c0 in range(0, L, 512):
                    n = min(512, L - c0)
                    S.op("pe", lambda e: e.matmul(pm[:, 0:n], lhsT=wt[0:kk, :], rhs=src[0:kk, c0:c0 + n], start=True, stop=True),
                         reads=[t_w, t_src], writes=[t_pm])
                    S.op("dve", lambda e: e.tensor_scalar(out=pre[:, 0:n], in0=pm[:, 0:n], scalar1=G.cols[0:64, bcol:bcol + 1],
                                                          scalar2=G.cols[0:64, ofr:ofr + 1], op0=ALU.add, op1=ALU.mult),
                         reads=[t_pm, G.t_cols], writes=[t_pre])
                    S.op("dve", lambda e: e.tensor_scalar(out=pr2[:, 0:n], in0=pre[:, 0:n], scalar1=1.0 / (2.0 * math.pi), scalar2=MAGIC, op0=ALU.mult, op1=ALU.add),
                         reads=[t_pre], writes=[t_pr2])
                    S.op("dve", lambda e: e.tensor_scalar(out=pr2[:, 0:n], in0=pr2[:, 0:n], scalar1=-MAGIC, scalar2=None, op0=ALU.add),
                         reads=[t_pr2], writes=[t_pr2])
                    S.op("dve", lambda e: e.scalar_tensor_tensor(out=pre[:, 0:n], in0=pr2[:, 0:n], scalar=-2.0 * math.pi, in1=pre[:, 0:n], op0=ALU.mult, op1=ALU.add),
                         reads=[t_pr2, t_pre], writes=[t_pre])
                    S.op("act", lambda e: e.activation(out=dst[:, c0:c0 + n], in_=pre[:, 0:n], func=AF.Sin),
                         reads=[t_pre], writes=[t_dst])
            for c in range(NCk):
                b = c % 2
                S.dma(wint[:, b], WIN[c], writes=[t_win[b]])
                for half in range(2):
                    S.op("pe", lambda e: e.matmul(phh[:, half, :], lhsT=h2[:, c * 128:(c + 1) * 128], rhs=w3[:, half * 512:(half + 1) * 512], start=True, stop=True),
                         reads=[t_h2, t_w], writes=[t_phh])
                for dr in range(2):
                    S.op("dve", lambda e: e.tensor_tensor(out=hk[:, c, dr * 512:(dr + 1) * 512].rearrange("p (o c) -> p o c", o=2),
                                                          in0=phh[:, dr, :].rearrange("p (o c) -> p o c", o=2),
                                                          in1=wint[:, b, dr:dr + 1, :].to_broadcast([128, 2, 256]), op=ALU.mult),
                         reads=[t_phh, t_win[b]], writes=[t_hk])
            for kc in range(KC):
                b = kc % 2
                S.dma(slab[:, b], FT[kc], writes=[t_slab[b]])
                for dr in range(2):
                    for eo_ in range(2):
                        for ri in range(2):
                            for c in range(NH):
                                cc = eo_ * NH + c
                                S.op("pe", lambda e: e.matmul(psk[:, eo_, ri, :], lhsT=slab[:, b, cc, ri, :], rhs=hk[:, cc, dr * 512:(dr + 1) * 512],
                                                              start=(c == 0), stop=(c == NH - 1)), reads=[t_slab[b], t_hk], writes=[t_psk])
                    S.op("act", lambda e: e.copy(out=xo[:], in_=psk[:, 1]), reads=[t_psk], writes=[t_eo])
                    S.op("dve", lambda e: e.tensor_tensor(out=sd[:, dr, 0], in0=psk[:, 0], in1=xo[:], op=ALU.add), reads=[t_psk, t_eo], writes=[t_sd])
                    S.op("dve", lambda e: e.tensor_tensor(out=sd[:, dr, 1], in0=psk[:, 0], in1=xo[:], op=ALU.subtract), reads=[t_psk, t_eo], writes=[t_sd])
                v = lambda ap: ap.rearrange("p (o c) -> p o c", o=2)
                S.op("dve", lambda e: e.tensor_tensor(out=sd[:, 0, 0, 0, :], in0=sd[:, 0, 0, 0, :], in1=hbias[:], op=ALU.add), reads=[t_sd, t_w], writes=[t_sd])
                S.op("dve", lambda e: e.tensor_tensor(out=sd[:, 0, 1, 0, :], in0=sd[:, 0, 1, 0, :], in1=hbias[:], op=ALU.add), reads=[t_sd, t_w], writes=[t_sd])
                S.op("dve", lambda e: e.tensor_tensor(out=kf[:, :, 0, 0, :], in0=v(sd[:, 0, 0, 0, :]), in1=v(sd[:, 1, 0, 0, :]), op=ALU.add), reads=[t_sd], writes=[t_kft])
                S.op("dve", lambda e: e.tensor_tensor(out=kf[:, :, 0, 1, :], in0=v(sd[:, 0, 0, 1, :]), in1=v(sd[:, 1, 0, 1, :]), op=ALU.subtract), reads=[t_sd], writes=[t_kft])
                S.op("dve", lambda e: e.tensor_tensor(out=v(xo[:, 0, :]), in0=v(sd[:, 0, 1, 0, :]), in1=v(sd[:, 1, 1, 0, :]), op=ALU.add), reads=[t_sd], writes=[t_eo])
                S.op("dve", lambda e: e.tensor_tensor(out=v(xo[:, 1, :]), in0=v(sd[:, 1, 1, 1, :]), in1=v(sd[:, 0, 1, 1, :]), op=ALU.subtract), reads=[t_sd], writes=[t_eo])
                S.op("dve", lambda e: e.tensor_scalar(out=kf[:, :, 1, 0, :], in0=v(xo[:, 0, :]), scalar1=mh[:, kc:kc + 1], scalar2=None, op0=ALU.mult),
                     reads=[t_eo, t_w], writes=[t_kft])
                S.op("dve", lambda e: e.tensor_scalar(out=kf[:, :, 1, 1, :], in0=v(xo[:, 1, :]), scalar1=mh[:, kc:kc + 1], scalar2=None, op0=ALU.mult),
                     reads=[t_eo, t_w], writes=[t_kft])
                S.dma(Kf[:, kc].rearrange("o p l r c -> p o l r c"), kf[:], reads=[t_kft], writes=[t_kf])
            S.barrier()
        with ExitStack() as _es:
            vt = _es.enter_context(SB(nc, "vt", [128, NCk, 256], BF16))
            zz1 = _es.enter_context(SB(nc, "zz1", [128, NCk, 256], BF16))
            Y = _es.enter_context(SB(nc, "Y", [128, 2, KC, 2, 256], BF16))
            fsl = _es.enter_context(SB(nc, "fsl", [128, 2, NCk, 2, 128], BF16))
            isl = _es.enter_context(SB(nc, "isl", [128, 2, KC, 2, 128], BF16))
            kft = _es.enter_context(SB(nc, "kft", [128, 2, 2, 2, 256], BF16))
            xo = _es.enter_context(SB(nc, "xo", [128, 2, 256], F32))
            xs_ = _es.enter_context(SB(nc, "xs_", [128, 2, 2, 256], F32))
            ta = _es.enter_context(SB(nc, "ta", [128, 4, 256], F32))
            yl = _es.enter_context(SB(nc, "yl", [128, 2, 2, 256], F32))
            xg = _es.enter_context(SB(nc, "xg", [128, 2, 256], BF16))
            zt = _es.enter_context(SB(nc, "zt", [128, 256], BF16))
            zT = _es.enter_context(SB(nc, "zT", [128, 2, 2, 256], BF16))
            psx = _es.enter_context(PS(nc, "psx", [128, 2, 4, 256], F32))
            psy = _es.enter_context(PS(nc, "psy", [128, 2, 512], F32))
            ptr = _es.enter_context(PS(nc, "ptr", [128, 2, 128], BF16))
            t_vt, t_zz1, t_Y, t_fsl, t_isl, t_kft2, t_ta, t_xg, t_zt, t_zT, t_psx, t_psy, t_ptr, t_xo, t_xs, t_yl = \
                Tok(), Tok(), Tok(), [Tok(), Tok()], [Tok(), Tok()], [Tok(), Tok()], Tok(), [Tok(), Tok()], Tok(), [Tok(), Tok()], [Tok(), Tok()], [Tok(), Tok()], Tok(), Tok(), Tok(), Tok()
            hsrc = lambda m: G.hyv[m, off:off + L, :].rearrange("(c p two) ch -> two p c ch", p=128, two=2)
            for par in range(2):
                S.dma(vt[:, par * NH:(par + 1) * NH, :], hsrc(0)[par], reads=[G.t_hyv], writes=[t_vt])
            mo = G.mixT[768:1024, :].rearrange("(c p) t -> p c t", p=128)
            for order in range(2):
                src, t_src = (vt, t_vt) if order == 0 else (zz1, t_zz1)
                for kc in range(KC):
                    b = kc % 2
                    S.dma(fsl[:, b], FT[kc], writes=[t_fsl[b]])
                    S.dma(kft[:, b], Kf[order, kc], reads=[t_kf], writes=[t_kft2[b]])
                    for eo_ in range(2):
                        for ri in range(2):
                            for c in range(NH):
                                cc = eo_ * NH + c
                                S.op("pe", lambda e: e.matmul(psx[:, b, eo_ * 2 + ri, :], lhsT=fsl[:, b, cc, ri, :], rhs=src[:, cc, :], start=(c == 0), stop=(c == NH - 1)),
                                     reads=[t_fsl[b], t_src], writes=[t_psx[b]])
                    S.op("act", lambda e: e.copy(out=xo[:], in_=psx[:, b, 2:4, :]), reads=[t_psx[b]], writes=[t_xo])
                    S.op("dve", lambda e: e.tensor_tensor(out=xs_[:, 0], in0=psx[:, b, 0:2, :], in1=xo[:], op=ALU.add), reads=[t_psx[b], t_xo], writes=[t_xs])
                    S.op("dve", lambda e: e.tensor_tensor(out=xs_[:, 1, 0, :], in0=psx[:, b, 0, :], in1=xo[:, 0, :], op=ALU.subtract), reads=[t_psx[b], t_xo], writes=[t_xs])
                    S.op("dve", lambda e: e.scalar_tensor_tensor(out=xs_[:, 1, 1, :], in0=psx[:, b, 1, :], scalar=-1.0, in1=xo[:, 1, :], op0=ALU.mult, op1=ALU.add),
                         reads=[t_psx[b], t_xo], writes=[t_xs])
                    S.op("dve", lambda e: e.tensor_tensor(out=ta[:, 0:2, :], in0=xs_[:, :, 0, :], in1=kft[:, b, :, 0, :], op=ALU.mult), reads=[t_xs, t_kft2[b]], writes=[t_ta])
                    S.op("pool", lambda e: e.tensor_tensor(out=ta[:, 2:4, :], in0=xs_[:, :, 1, :], in1=kft[:, b, :, 1, :], op=ALU.mult), reads=[t_xs, t_kft2[b]], writes=[t_ta])
                    S.op("dve", lambda e: e.tensor_tensor(out=yl[:, :, 0, :], in0=ta[:, 0:2, :], in1=ta[:, 2:4, :], op=ALU.subtract), reads=[t_ta], writes=[t_yl])
                    S.op("dve", lambda e: e.tensor_tensor(out=ta[:, 0:2, :], in0=xs_[:, :, 0, :], in1=kft[:, b, :, 1, :], op=ALU.mult), reads=[t_xs, t_kft2[b], t_yl], writes=[t_ta])
                    S.op("pool", lambda e: e.tensor_tensor(out=ta[:, 2:4, :], in0=xs_[:, :, 1, :], in1=kft[:, b, :, 0, :], op=ALU.mult), reads=[t_xs, t_kft2[b], t_yl], writes=[t_ta])
                    S.op("dve", lambda e: e.tensor_tensor(out=yl[:, :, 1, :], in0=ta[:, 0:2, :], in1=ta[:, 2:4, :], op=ALU.add), reads=[t_ta], writes=[t_yl])
                    S.op("dve", lambda e: e.tensor_tensor(out=Y[:, 0, kc, 0, :], in0=yl[:, 0, 0, :], in1=yl[:, 1, 0, :], op=ALU.add), reads=[t_yl], writes=[t_Y])
                    S.op("pool", lambda e: e.tensor_tensor(out=Y[:, 0, kc, 1, :], in0=yl[:, 0, 1, :], in1=yl[:, 1, 1, :], op=ALU.subtract), reads=[t_yl], writes=[t_Y])
                    S.op("dve", lambda e: e.tensor_tensor(out=Y[:, 1, kc, 0, :], in0=yl[:, 0, 0, :], in1=yl[:, 1, 0, :], op=ALU.subtract), reads=[t_yl], writes=[t_Y])
                    S.op("pool", lambda e: e.tensor_tensor(out=Y[:, 1, kc, 1, :], in0=yl[:, 0, 1, :], in1=yl[:, 1, 1, :], op=ALU.add), reads=[t_yl], writes=[t_Y])
                oi = 0
                for c2 in range(NH):
                    for par in range(2):
                        cc = par * NH + c2
                        b = oi % 2
                        oi += 1
                        zb = c2 % 2
                        S.dma(isl[:, b], IT[cc], writes=[t_isl[b]])
                        S.dma(xg[:, b], hsrc(1 + order)[par, :, c2, :], reads=[G.t_hyv], writes=[t_xg[b]])
                        for kc in range(KC):
                            for ri in range(2):
                                S.op("pe", lambda e: e.matmul(psy[:, b, 0:256], lhsT=isl[:, b, kc, ri, :], rhs=Y[:, par, kc, ri, :],
                                                              start=(kc == 0 and ri == 0), stop=(kc == KC - 1 and ri == 1)), reads=[t_isl[b], t_Y], writes=[t_psy[b]])
                        if order == 0:
                            S.op("dve", lambda e: e.tensor_tensor(out=zz1[:, cc, :], in0=psy[:, b, 0:256], in1=xg[:, b, :], op=ALU.mult),
                                 reads=[t_psy[b], t_xg[b]], writes=[t_zz1])
                        else:
                            S.op("dve", lambda e: e.tensor_tensor(out=zt[:], in0=psy[:, b, 0:256], in1=xg[:, b, :], op=ALU.mult),
                                 reads=[t_psy[b], t_xg[b]], writes=[t_zt])
                            for hh in range(2):
                                S.op("pe", lambda e: e.transpose(out=ptr[:, hh, :], in_=zt[:, hh * 128:(hh + 1) * 128], identity=G.identB),
                                     reads=[t_zt, G.t_c], writes=[t_ptr])
                            S.op("act", lambda e: e.copy(out=zT[:, zb].rearrange("p h (t two) -> p h t two", two=2)[:, :, :, par], in_=ptr[:]),
                                 reads=[t_ptr], writes=[t_zT[zb]])
                            if par == 1:
                                S.dma(mo[:, :, off + c2 * 256:off + (c2 + 1) * 256], zT[:, zb], reads=[t_zT[zb]], writes=[G.t_mix])
            S.barrier()
    if "hy" in G.dbg and li == 0:
        dump_bf16(G, G.mixT[768:896, 256:768], G.dbg["hy"], [G.t_mix])


def _hy_tables(L):
    N = 2 * L
    NCk = L // 128
    KC = (L // 2 + 1 + 127) // 128
    perm = np.concatenate([np.arange(0, L, 2), np.arange(1, L, 2)])
    n = perm.astype(np.int64)
    k = np.arange(KC * 128, dtype=np.int64)
    ang = ((n[:, None] * k[None, :]) % N).astype(np.float64) * (2 * np.pi / N)
    valid = (k <= L // 2).astype(np.float64)
    w = np.where(k == 0, 1.0, 2.0) / N * valid
    c, s_ = np.cos(ang), np.sin(ang)
    ft = np.stack([c * valid, -s_ * valid], axis=0)
    ft = ft.reshape(2, NCk, 128, KC, 128).transpose(3, 2, 1, 0, 4)
    it = np.stack([c * w, -s_ * w], axis=0)
    it = it.reshape(2, NCk, 128, KC, 128).transpose(1, 4, 3, 0, 2)
    f = np.float32
    nn = np.arange(L, dtype=f)
    t = nn / f(max(L - 1, 1))
    bands = np.linspace(1e-4, 15, 16, dtype=f)
    wpos = (f(2 * math.pi / L) * nn).astype(f)
    feats = np.concatenate([t[:, None], np.cos(wpos[:, None] * bands), -np.sin(wpos[:, None] * bands)], axis=-1).astype(f)
    deltas = np.abs(np.linspace(math.log(1e-2) / 1.5, math.log(1e-2) / 0.3, 256, dtype=f))
    win = np.exp(-t[:, None] * deltas).astype(f)
    winb = win.copy()
    winb[0] = 0.0
    feats = feats[perm]
    wn = np.stack([win, winb], axis=1)[perm].reshape(NCk, 128, 2, 256)
    mh = ((k != L // 2) & (k <= L // 2)).astype(f).reshape(KC, 128).T
    bf = ml_dtypes.bfloat16
    return (np.ascontiguousarray(ft).astype(bf), np.ascontiguousarray(it).astype(bf),
            np.ascontiguousarray(feats.T), np.ascontiguousarray(wn), np.ascontiguousarray(mh))
```

```python
import math
from contextlib import ExitStack
import numpy as np
import ml_dtypes
import concourse.bass as bass
import concourse.mybir as mybir
from concourse.bass_utils import run_bass_kernel_spmd

F32 = mybir.dt.float32
BF16 = mybir.dt.bfloat16
AF = mybir.ActivationFunctionType
ALU = mybir.AluOpType
AX = mybir.AxisListType

D = 1024
NCTX = 256
NLAT = 4096
NTOK = NCTX + NLAT
NT = NTOK // 128
DEPTH = 2
D_IN = 2444
EPS = 1e-6
HC = NTOK + 3
NE = 16
DFF = 256
DEBUG = False


def colof(tile):
    return 1 + 128 * tile if tile < 2 else 258 + 128 * (tile - 2)


BLOCKS = [(1, 0, 256, 0, 2)] + [(258 + 512 * j, 256 + 512 * j, 512, 2 + 4 * j, 4) for j in range(8)]


class Tok:
    __slots__ = ("w", "r")

    def __init__(self):
        self.w = None
        self.r = {}


class Sched:
    def __init__(self, nc, ndma=8, same_engine_sync=True):
        self.nc = nc
        self.eng = {"pe": nc.tensor, "act": nc.scalar, "dve": nc.vector, "pool": nc.gpsimd, "sp": nc.sync}
        self.semh = {}
        self.cnt = {}
        self.seen = {k: {} for k in self.eng}
        self.same = same_engine_sync
        for k in self.eng:
            self.semh[k] = nc.alloc_semaphore("s_" + k)
            self.cnt[k] = 0
        self.ndma = ndma
        self.dslot = {}
        self.dval = {}
        for q in ("sp", "pool"):
            self.dslot[q] = 0
            for i in range(ndma):
                key = ("dma", q, i)
                self.semh[key] = nc.alloc_semaphore("d_%s_%d" % (q, i))
                self.dval[key] = 0
        self.ninst = 0

    def _wait(self, e, deps):
        for (k, v) in sorted(deps, key=str):
            if k == e and (e == "pe" or not self.same):
                continue
            if self.seen[e].get(k, 0) >= v:
                continue
            self.eng[e].wait_ge(self.semh[k], v)
            self.seen[e][k] = v

    @staticmethod
    def _deps(reads, writes):
        deps = set()
        for t in reads:
            if t.w is not None:
                deps.add(t.w)
        for t in writes:
            if t.w is not None:
                deps.add(t.w)
            for kv in t.r.items():
                deps.add(kv)
        return deps

    @staticmethod
    def _mark(ev, reads, writes):
        k, v = ev
        for t in reads:
            if t.r.get(k, 0) < v:
                t.r[k] = v
        for t in writes:
            t.w = ev
            t.r = {}

    def op(self, e, fn, reads=(), writes=()):
        self._wait(e, self._deps(reads, writes))
        ins = fn(self.eng[e])
        self.cnt[e] += 1
        ins.then_inc(self.semh[e], 1)
        self._mark((e, self.cnt[e]), reads, writes)
        self.ninst += 1
        return ins

    def dma(self, out, in_, reads=(), writes=(), q="sp", **kw):
        i = self.dslot[q]
        self.dslot[q] = (i + 1) % self.ndma
        key = ("dma", q, i)
        deps = self._deps(reads, writes)
        if self.dval[key] > 0:
            deps.add((key, self.dval[key]))
        self._wait(q, deps)
        ins = self.eng[q].dma_start(out=out, in_=in_, **kw)
        self.dval[key] += 16
        ins.then_inc(self.semh[key], 16)
        self._mark((key, self.dval[key]), reads, writes)
        self.ninst += 1
        return ins

    def barrier(self):
        deps = set()
        for key, v in self.dval.items():
            if v > 0:
                deps.add((key, v))
        for k in self.eng:
            if self.cnt[k] > 0:
                deps.add((k, self.cnt[k]))
        for e in self.eng:
            self._wait(e, deps)


class Ctx:
    pass


_UID = [0]


def SB(nc, name, shape, dt):
    _UID[0] += 1
    return nc.sbuf_tensor("%s_%d" % (name, _UID[0]), shape, dt)


def PS(nc, name, shape, dt):
    _UID[0] += 1
    return nc.psum_tensor("%s_%d" % (name, _UID[0]), shape, dt)


def build(dbg=None):
    nc = bass.Bass("TRN2", target_bir_lowering=False)
    S = Sched(nc)
    G = Ctx()
    G.nc, G.S = nc, S

    def din(name, shape, dt=F32):
        return nc.dram_tensor(name, list(shape), dt, kind="ExternalInput").ap()

    def dscr(name, shape, dt):
        return nc.dram_tensor(name, list(shape), dt, kind="Internal").ap()

    I = {}
    I["x"] = din("x", [NLAT, D])
    I["ctx"] = din("ctx", [NCTX, D])
    I["w_mod"] = din("w_mod", [DEPTH, D, 6 * D])
    I["b_mod"] = din("b_mod", [DEPTH, 6 * D])
    I["w_in"] = din("w_in", [DEPTH, D, D_IN])
    I["w_out"] = din("w_out", [DEPTH, D, D])
    I["w_router"] = din("w_router", [D, NE])
    I["router_bias"] = din("router_bias", [NE])
    I["w_gate"] = din("w_gate", [DEPTH, NE, D, DFF])
    I["w_up"] = din("w_up", [DEPTH, NE, D, DFF])
    I["w_down"] = din("w_down", [DEPTH, NE, DFF, D])
    I["g_final"] = din("g_final", [D])
    I["cols"] = din("cols", [DEPTH, 128, NCOLS])
    I["cmat"] = din("cmat", [9, 128, 128])
    I["rope"] = din("rope", [2, 128, NLAT])
    for sname, P in SEGS.items():
        I["ft_" + sname] = din("ft_" + sname, [P["KC"], 128, P["NC"], 2, 128], BF16)
        I["it_" + sname] = din("it_" + sname, [P["NC"], 128, P["KC"], 2, 128], BF16)
        I["fe_" + sname] = din("fe_" + sname, [33, P["L"]])
        I["win_" + sname] = din("win_" + sname, [P["NC"], 128, 2, 256])
        I["mh_" + sname] = din("mh_" + sname, [128, P["KC"]])
    for nm, shp in (("hy_conv_w", [DEPTH, 3, 768]), ("hy_conv_b", [DEPTH, 768]), ("hy_w1", [DEPTH, 33, 64]), ("hy_w2", [DEPTH, 64, 64]),
                    ("hy_w3", [DEPTH, 64, 1024]), ("hy_bias", [DEPTH, 2, 256])):
        I[nm] = din(nm, shp)
    I["ssdmask"] = din("ssdmask", [2, 4, 128, 512], BF16)
    for nm, shp in (("ssd_conv_w", [DEPTH, 3, 640]), ("ssd_dt_bias", [DEPTH, 2, 6]), ("ssd_a_log", [DEPTH, 2, 6]),
                    ("ssd_d", [DEPTH, 6]), ("ssd_norm", [DEPTH, 384])):
        I[nm] = din(nm, shp)
    out = nc.dram_tensor("out", [NLAT, D], F32, kind="ExternalOutput").ap()
    G.I, G.out = I, out
    G.dbg = {}
    if dbg:
        for name, shape in dbg.items():
            G.dbg[name] = nc.dram_tensor("dbg_" + name, list(shape), F32, kind="ExternalOutput").ap()

    G.xres = dscr("xres", [NTOK, D], F32)
    G.t_xres = [Tok() for _ in range(NT)]
    G.wb_in = [dscr("wb_in%d" % i, [D, D_IN], BF16) for i in range(DEPTH)]
    G.wb_out = [dscr("wb_out%d" % i, [D, D], BF16) for i in range(DEPTH)]
    G.wb_gate = [dscr("wb_gate%d" % i, [NE, D, DFF], BF16) for i in range(DEPTH)]
    G.wb_up = [dscr("wb_up%d" % i, [NE, D, DFF], BF16) for i in range(DEPTH)]
    G.wb_down = [dscr("wb_down%d" % i, [NE, DFF, D], BF16) for i in range(DEPTH)]
    G.t_wb = Tok()
    G.mixT = dscr("mixT", [D, NTOK], BF16)
    G.hyv = dscr("hyv", [3, NTOK, 256], BF16)
    G.ssd_yb = dscr("ssd_yb", [NTOK, 384], F32)
    G.t_ssdyb = [Tok() for _ in range(NT)]
    G.t_hyv = Tok()
    G.Kf = {sn: dscr("Kf_" + sn, [2, P["KC"], 128, 2, 2, 256], BF16) for sn, P in SEGS.items()}
    G.t_mix = Tok()

    cm = nc.alloc_sbuf_tensor("cm", [128, 9, 128], F32)
    cmb = nc.alloc_sbuf_tensor("cmb", [128, 9, 128], BF16)
    ones = nc.alloc_sbuf_tensor("ones", [128, 128], F32)
    epsc = nc.alloc_sbuf_tensor("epsc", [128, 1], F32)
    G.t_c = Tok()
    S.dma(cm[:], I["cmat"].rearrange("a p c -> p a c"), writes=[G.t_c])
    S.op("dve", lambda e: e.tensor_copy(out=cmb[:], in_=cm[:]), reads=[G.t_c], writes=[G.t_c])
    S.op("dve", lambda e: e.memset(ones[:], 1.0), writes=[G.t_c])
    S.op("dve", lambda e: e.memset(epsc[:], EPS), writes=[G.t_c])
    G.cm, G.cmb, G.ones, G.epsc = cm, cmb, ones, epsc
    G.negpi = nc.alloc_sbuf_tensor("negpi", [128, 1], F32)
    S.op("dve", lambda e: e.memset(G.negpi[:], -math.pi), writes=[G.t_c])
    G.identF, G.identB = cm[:, 0, :], cmb[:, 0, :]

    S.dma(G.xres[0:NCTX, :], I["ctx"][:, :], writes=G.t_xres[0:2])
    for j in range(4):
        S.dma(G.xres[NCTX + 1024 * j:NCTX + 1024 * (j + 1), :], I["x"][1024 * j:1024 * (j + 1), :],
              writes=G.t_xres[2 + 8 * j:2 + 8 * (j + 1)])

    convert_weights(G)
    S.barrier()
    for li in range(1 if DEBUG else DEPTH):
        layer(G, li)
    S.barrier()
    return nc


def convert_weights(G):
    nc, S, I = G.nc, G.S, G.I
    CH = 4096
    with ExitStack() as _es:
        cf = _es.enter_context(SB(nc, "cv_f", [128, 2, CH], F32))
        cb = _es.enter_context(SB(nc, "cv_b", [128, 2, CH], BF16))
        tf = [Tok(), Tok()]
        tb = [Tok(), Tok()]
        n = 0
        engs = ["dve", "pool", "act"]
        for li in range(DEPTH):
            pairs = [(I["w_in"][li], G.wb_in[li], "a b -> (a b)"), (I["w_out"][li], G.wb_out[li], "a b -> (a b)"),
                     (I["w_gate"][li], G.wb_gate[li], "e a b -> (e a b)"), (I["w_up"][li], G.wb_up[li], "e a b -> (e a b)"),
                     (I["w_down"][li], G.wb_down[li], "e a b -> (e a b)")]
            for src, dst, pat in pairs:
                s1 = src.rearrange(pat).rearrange("(p m) -> p m", p=128)
                d1 = dst.rearrange(pat).rearrange("(p m) -> p m", p=128)
                M = s1.shape[1]
                for c0 in range(0, M, CH):
                    w = min(CH, M - c0)
                    k = n % 2
                    S.dma(cf[:, k, 0:w], s1[:, c0:c0 + w], writes=[tf[k]])
                    en = engs[n % 3]
                    if en == "act":
                        S.op("act", lambda e: e.copy(out=cb[:, k, 0:w], in_=cf[:, k, 0:w]), reads=[tf[k]], writes=[tb[k]])
                    else:
                        S.op(en, lambda e: e.tensor_copy(out=cb[:, k, 0:w], in_=cf[:, k, 0:w]), reads=[tf[k]], writes=[tb[k]])
                    S.dma(d1[:, c0:c0 + w], cb[:, k, 0:w], reads=[tb[k]], writes=[G.t_wb], q="pool")
                    n += 1


COLS = {}
_o = 0
for _name, _n in [("cc", 16), ("bmod", 32), ("g_mix", 8), ("g_ffn", 8), ("ssd_conv_b", 5), ("qg", 1), ("kg", 1),
                  ("ssd_d", 3), ("ssd_norm", 3), ("hy_b1", 1), ("hy_freq", 1), ("hy_b2", 1)]:
    COLS[_name] = (_o, _n)
    _o += _n
NCOLS = _o


def layer(G, li):
    nc, S, I = G.nc, G.S, G.I
    with ExitStack() as _es:
        cols = _es.enter_context(SB(nc, "cols", [128, NCOLS], F32))
        modc = _es.enter_context(SB(nc, "modc", [128, 4, 8, 2], F32))
        gtb = _es.enter_context(SB(nc, "gtb", [128, 2, 2, D], F32))
        G.cols, G.modc, G.gtb = cols, modc, gtb
        G.t_cols, G.t_modc, G.t_gtb = Tok(), Tok(), Tok()
        S.dma(cols[:], I["cols"][li], writes=[G.t_cols])
        adaln(G, li)
        S.barrier()
        with ExitStack() as _es:
            hT = _es.enter_context(SB(nc, "hT", [128, 8, HC], BF16))
            G.hT, G.t_hT = hT, Tok()
            norm_in(G, li)
            S.barrier()
            if "hy" in STAGES:
                hyena_inproj(G, li)
                S.barrier()
            if "att" in STAGES:
                attention(G, li)
                S.barrier()
            if "ssd" in STAGES:
                ssd(G, li)
                S.barrier()
        if "hy" in STAGES:
            hyena_fft(G, li)
            S.barrier()
        if "moe" in STAGES:
            with ExitStack() as _es:
                h2T = _es.enter_context(SB(nc, "h2T", [128, 8, NTOK], BF16))
                rl = _es.enter_context(SB(nc, "rl", [128, NT, NE], F32))
                G.h2T, G.t_h2T, G.rl, G.t_rl = h2T, Tok(), rl, Tok()
                outproj(G, li)
                S.barrier()
                moe(G, li)
                S.barrier()


STAGES = ("att", "ssd", "hy", "moe")


def colap(G, name, j=0, n=1, p0=0, p1=128):
    o, _ = COLS[name]
    return G.cols[p0:p1, o + j:o + j + n]


def adaln(G, li):
    nc, S, I = G.nc, G.S, G.I
    cols, modc, gtb = G.cols, G.modc, G.gtb
    with ExitStack() as _es:
        sc = _es.enter_context(SB(nc, "sc", [128, 8, 2], F32))
        screp = _es.enter_context(SB(nc, "screp", [128, 8, 2, 128], F32))
        wm = _es.enter_context(SB(nc, "wm", [128, 2, 8, 512], F32))
        brow = _es.enter_context(SB(nc, "brow", [128, 2, D], F32))
        ps_a = _es.enter_context(PS(nc, "ps_a", [128, 4, 2], F32))
        ps_g = _es.enter_context(PS(nc, "ps_g", [128, 2, 512], F32))
        t_sc, t_wm, t_pa, t_pg, t_brow = Tok(), [Tok(), Tok()], Tok(), Tok(), Tok()
        o = COLS["cc"][0]
        S.op("act", lambda e: e.activation(out=sc[:].rearrange("p k j -> p (k j)"), in_=cols[:, o:o + 16], func=AF.Silu),
             reads=[G.t_cols], writes=[t_sc])
        S.op("dve", lambda e: e.tensor_copy(out=screp[:].rearrange("p k j c -> p (k j) c"),
                                            in_=sc[:].rearrange("p k j -> p (k j)").unsqueeze(2).to_broadcast([128, 16, 128])),
             reads=[t_sc], writes=[t_sc])
        for g in range(2):
            S.dma(brow[:, g, :], I["b_mod"][li, (2 + 3 * g) * D:(3 + 3 * g) * D].partition_broadcast(128), writes=[t_brow])
        wv = I["w_mod"][li].rearrange("(k p) c -> p k c", p=128)
        ob = COLS["bmod"][0]
        for cj in range(12):
            b = cj % 2
            S.dma(wm[:, b], wv[:, :, cj * 512:(cj + 1) * 512], writes=[t_wm[b]])
            vec = cj // 2
            half = cj % 2
            if vec in (2, 5):
                g = 0 if vec == 2 else 1
                for j in range(2):
                    for kd in range(8):
                        S.op("pe", lambda e: e.matmul(ps_g[:, j, :], lhsT=screp[:, kd, j, :], rhs=wm[:, b, kd, :],
                                                      start=(kd == 0), stop=(kd == 7)), reads=[t_sc, t_wm[b]], writes=[t_pg])
                    S.op("dve", lambda e: e.tensor_tensor(out=gtb[:, g, j, half * 512:(half + 1) * 512], in0=ps_g[:, j, :],
                                                          in1=brow[:, g, half * 512:(half + 1) * 512], op=ALU.add),
                         reads=[t_pg, t_brow], writes=[G.t_gtb])
            else:
                v = {0: 0, 1: 1, 3: 2, 4: 3}[vec]
                for fc in range(4):
                    for kd in range(8):
                        S.op("pe", lambda e: e.matmul(ps_a[:, fc, :], lhsT=wm[:, b, kd, fc * 128:(fc + 1) * 128], rhs=sc[:, kd, :],
                                                      start=(kd == 0), stop=(kd == 7)), reads=[t_sc, t_wm[b]], writes=[t_pa])
                k0 = half * 4
                S.op("dve", lambda e: e.tensor_tensor(out=modc[:, v, k0:k0 + 4, :], in0=ps_a[:],
                                                      in1=cols[:, ob + v * 8 + k0:ob + v * 8 + k0 + 4].unsqueeze(2).to_broadcast([128, 4, 2]),
                                                      op=ALU.add), reads=[t_pa, G.t_cols], writes=[G.t_modc])
        for v, gname in ((1, "g_mix"), (3, "g_ffn")):
            og = COLS[gname][0]
            S.op("dve", lambda e: e.scalar_tensor_tensor(out=modc[:, v], in0=modc[:, v], scalar=1.0,
                                                         in1=cols[:, og:og + 8].unsqueeze(2).to_broadcast([128, 8, 2]),
                                                         op0=ALU.add, op1=ALU.mult), reads=[G.t_modc, G.t_cols], writes=[G.t_modc])


def rms_to_T(G, xt, t_x, tile, vA, vB, dstT, t_dst, dcol, pool):
    nc, S = G.nc, G.S
    sq, ss, xn, ps_t, toks = pool
    t_sq, t_ss, t_xn, t_ps = toks
    j = 1 if tile < 2 else 0
    S.op("act", lambda e: e.activation(out=sq[:], in_=xt, func=AF.Square, accum_out=ss[:, 0:1]), reads=[t_x], writes=[t_sq, t_ss])
    S.op("act", lambda e: e.activation(out=ss[:, 1:2], in_=ss[:, 0:1], func=AF.Sqrt, bias=G.epsc[:], scale=1.0 / D),
         reads=[t_ss, G.t_c], writes=[t_ss])
    S.op("dve", lambda e: e.reciprocal(out=ss[:, 2:3], in_=ss[:, 1:2]), reads=[t_ss], writes=[t_ss])
    S.op("dve", lambda e: e.tensor_scalar(out=xn[:], in0=xt, scalar1=ss[:, 2:3], scalar2=None, op0=ALU.mult),
         reads=[t_x, t_ss], writes=[t_xn])
    for k in range(8):
        S.op("pe", lambda e: e.transpose(out=ps_t[:, k, :], in_=xn[:, k * 128:(k + 1) * 128], identity=G.identB),
             reads=[t_xn, G.t_c], writes=[t_ps])
    S.op("dve", lambda e: e.tensor_tensor(out=sq[:].rearrange("p (k c) -> p k c", k=8), in0=ps_t[:],
                                          in1=G.modc[:, vA, :, j:j + 1].to_broadcast([128, 8, 128]), op=ALU.mult),
         reads=[t_ps, G.t_modc], writes=[t_sq])
    S.op("dve", lambda e: e.tensor_tensor(out=dstT[:, :, dcol:dcol + 128], in0=sq[:].rearrange("p (k c) -> p k c", k=8),
                                          in1=G.modc[:, vB, :, j:j + 1].to_broadcast([128, 8, 128]), op=ALU.add),
         reads=[t_sq, G.t_modc], writes=[t_dst])


def norm_in(G, li):
    nc, S = G.nc, G.S
    hT = G.hT
    with ExitStack() as _es:
        xt = _es.enter_context(SB(nc, "xt", [128, 2, D], F32))
        sq = _es.enter_context(SB(nc, "sq", [128, D], F32))
        ss = _es.enter_context(SB(nc, "ss", [128, 4], F32))
        xn = _es.enter_context(SB(nc, "xn", [128, D], BF16))
        ps_t = _es.enter_context(PS(nc, "ps_t", [128, 8, 128], BF16))
        t_x = [Tok(), Tok()]
        pool = (sq, ss, xn, ps_t, (Tok(), Tok(), Tok(), Tok()))
        for c in (0, 257, HC - 1):
            S.op("pool", lambda e: e.memset(hT[:, :, c:c + 1], 0.0), writes=[G.t_hT])
        for tile in range(NT):
            b = tile % 2
            S.dma(xt[:, b, :], G.xres[tile * 128:(tile + 1) * 128, :], reads=[G.t_xres[tile]], writes=[t_x[b]])
            rms_to_T(G, xt[:, b, :], t_x[b], tile, 1, 0, hT, G.t_hT, colof(tile), pool)
        if "hT" in G.dbg and li == 0:
            with ExitStack() as _es:
                dh = _es.enter_context(SB(nc, "dbgh", [128, 8, 512], F32))
                t = Tok()
                S.op("dve", lambda e: e.tensor_copy(out=dh[:], in_=hT[:, :, 0:512]), reads=[G.t_hT], writes=[t])
                S.dma(G.dbg["hT"].rearrange("(k p) c -> p k c", p=128), dh[:], reads=[t])


def attention(G, li):
    nc, S, I = G.nc, G.S, G.I
    hT = G.hT
    need_ctx = li < DEPTH - 1
    wv = G.wb_in[li].rearrange("(k p) c -> p k c", p=128)
    scale = 64 ** -0.5
    with ExitStack() as _es:
        wq = _es.enter_context(SB(nc, "wqkv", [128, 8, 640], BF16))
        qT = _es.enter_context(SB(nc, "qT", [128, 3, NTOK], BF16))
        kT = _es.enter_context(SB(nc, "kT", [128, NTOK], BF16))
        vp = _es.enter_context(SB(nc, "vp", [128, NT, 2, 128], BF16))
        rp = _es.enter_context(SB(nc, "rp", [128, 2, 2, 512], F32))
        qs = _es.enter_context(SB(nc, "qs", [128, 512], F32))
        q2 = _es.enter_context(SB(nc, "q2", [128, 512], F32))
        qn = _es.enter_context(SB(nc, "qn", [128, 512], F32))
        qnb = _es.enter_context(SB(nc, "qnb", [128, 512], BF16))
        pT = _es.enter_context(SB(nc, "pT", [128, 2, 2, 512], BF16))
        rd = _es.enter_context(SB(nc, "rd", [128, 2, 512], F32))
        ao = _es.enter_context(SB(nc, "ao", [128, 2, 512], BF16))
        ps_q = _es.enter_context(PS(nc, "ps_q", [128, 512], F32))
        ps_r = _es.enter_context(PS(nc, "ps_r", [128, 512], F32))
        ps_s = _es.enter_context(PS(nc, "ps_s", [128, 2, 2, 512], F32))
        ps_o = _es.enter_context(PS(nc, "ps_o", [128, 2, 512], F32))
        t_w, t_q, t_k, t_v, t_rp = Tok(), Tok(), Tok(), Tok(), [Tok(), Tok()]
        t_qs, t_q2, t_qn, t_qnb, t_psq, t_psr = Tok(), Tok(), Tok(), Tok(), Tok(), Tok()
        t_pT, t_pss, t_pso, t_rd, t_ao = [Tok(), Tok()], [Tok(), Tok()], [Tok(), Tok()], [Tok(), Tok()], [Tok(), Tok()]
        for j in range(3):
            S.dma(wq[:, :, j * 128:j * 128 + 64], wv[:, :, j * 64:(j + 1) * 64], reads=[G.t_wb], writes=[t_w])
            S.dma(wq[:, :, j * 128 + 64:(j + 1) * 128], wv[:, :, (3 + j) * 64:(4 + j) * 64], reads=[G.t_wb], writes=[t_w])
        S.dma(wq[:, :, 384:640], wv[:, :, 384:640], reads=[G.t_wb], writes=[t_w])
        S.op("pool", lambda e: e.memset(vp[:, :, :, 64:128], 1.0), writes=[t_v])
        og = {0: COLS["qg"][0], 1: COLS["qg"][0], 2: COLS["qg"][0], 3: COLS["kg"][0]}
        for bi, (c0, t0, n, tile0, ntile) in enumerate(BLOCKS):
            if bi > 0:
                b = bi % 2
                S.dma(rp[:, b, :, :], I["rope"][:, :, t0 - NCTX:t0 - NCTX + 512].rearrange("a p c -> p a c"), writes=[t_rp[b]])
            for ch in range(4):
                for k in range(8):
                    S.op("pe", lambda e: e.matmul(ps_q[:, 0:n], lhsT=wq[:, k, ch * 128:(ch + 1) * 128], rhs=hT[:, k, c0:c0 + n],
                                                  start=(k == 0), stop=(k == 7)), reads=[t_w, G.t_hT], writes=[t_psq])
                S.op("act", lambda e: e.copy(out=qs[:, 0:n], in_=ps_q[:, 0:n]), reads=[t_psq], writes=[t_qs])
                S.op("act", lambda e: e.activation(out=q2[:, 0:n], in_=qs[:, 0:n], func=AF.Square), reads=[t_qs], writes=[t_q2])
                S.op("pe", lambda e: e.matmul(ps_r[:, 0:n], lhsT=G.cm[:, 3, :], rhs=q2[:, 0:n], start=True, stop=True),
                     reads=[t_q2, G.t_c], writes=[t_psr])
                S.op("act", lambda e: e.activation(out=q2[:, 0:n], in_=ps_r[:, 0:n], func=AF.Sqrt, bias=G.epsc[:], scale=1.0 / 64),
                     reads=[t_psr, G.t_c], writes=[t_q2])
                S.op("dve", lambda e: e.reciprocal(out=q2[:, 0:n], in_=q2[:, 0:n]), reads=[t_q2], writes=[t_q2])
                S.op("dve", lambda e: e.scalar_tensor_tensor(out=qn[:, 0:n], in0=qs[:, 0:n], scalar=G.cols[:, og[ch]:og[ch] + 1],
                                                             in1=q2[:, 0:n], op0=ALU.mult, op1=ALU.mult),
                     reads=[t_qs, t_q2, G.t_cols], writes=[t_qn])
                dst = qT[:, ch, t0:t0 + n] if ch < 3 else kT[:, t0:t0 + n]
                t_dst = t_q if ch < 3 else t_k
                if bi == 0:
                    S.op("dve", lambda e: e.tensor_copy(out=dst, in_=qn[:, 0:n]), reads=[t_qn], writes=[t_dst])
                else:
                    b = bi % 2
                    S.op("dve", lambda e: e.tensor_copy(out=qnb[:, 0:n], in_=qn[:, 0:n]), reads=[t_qn], writes=[t_qnb])
                    S.op("pe", lambda e: e.matmul(ps_r[:, 0:n], lhsT=G.cmb[:, 4, :], rhs=qnb[:, 0:n], start=True, stop=True),
                         reads=[t_qnb, G.t_c], writes=[t_psr])
                    S.op("dve", lambda e: e.tensor_tensor(out=qs[:, 0:n], in0=ps_r[:, 0:n], in1=rp[:, b, 1, 0:n], op=ALU.mult),
                         reads=[t_psr, t_rp[b]], writes=[t_qs])
                    S.op("dve", lambda e: e.tensor_tensor(out=qn[:, 0:n], in0=qn[:, 0:n], in1=rp[:, b, 0, 0:n], op=ALU.mult),
                         reads=[t_qn, t_rp[b]], writes=[t_qn])
                    S.op("dve", lambda e: e.tensor_tensor(out=dst, in0=qn[:, 0:n], in1=qs[:, 0:n], op=ALU.add),
                         reads=[t_qn, t_qs], writes=[t_dst])
            for tl in range(tile0, tile0 + ntile):
                cc = colof(tl)
                for k in range(8):
                    S.op("pe", lambda e: e.matmul(ps_q[:, 0:128], lhsT=hT[:, k, cc:cc + 128], rhs=wq[:, k, 512:640],
                                                  start=(k == 0), stop=(k == 7)), reads=[t_w, G.t_hT], writes=[t_psq])
                S.op("act", lambda e: e.copy(out=vp[:, tl, :, 0:64], in_=ps_q[:, 0:128].rearrange("p (a d) -> p a d", a=2)),
                     reads=[t_psq], writes=[t_v])
        pairs = []
        oi = 0
        for h in range(6):
            for bi, (c0, t0, n, tile0, ntile) in enumerate(BLOCKS):
                if bi == 0 and not need_ctx:
                    continue
                kcs = list(range(2)) if bi == 0 else list(range(NT))
                for ki in range(0, len(kcs), 2):
                    pairs.append((h, t0, n, kcs[ki], ki == 0, ki + 2 >= len(kcs), oi % 2))
                oi += 1

        def qk(j):
            h, t0, n, kc, first, last, ob = pairs[j]
            pb = (h // 3) * 64
            sb = j % 2
            for u in range(2):
                S.op("pe", lambda e: e.matmul(ps_s[:, sb, u, 0:n], lhsT=kT[pb:pb + 64, (kc + u) * 128:(kc + u + 1) * 128],
                                              rhs=qT[pb:pb + 64, h % 3, t0:t0 + n], start=True, stop=True),
                     reads=[t_q, t_k], writes=[t_pss[sb]])

        qk(0)
        for j, (h, t0, n, kc, first, last, ob) in enumerate(pairs):
            sb = j % 2
            kv = h // 3
            if j + 1 < len(pairs):
                qk(j + 1)
            S.op("act", lambda e: e.activation(out=pT[:, sb, :, 0:n], in_=ps_s[:, sb, :, 0:n], func=AF.Exp, scale=scale),
                 reads=[t_pss[sb]], writes=[t_pT[sb]])
            for u in range(2):
                S.op("pe", lambda e: e.matmul(ps_o[:, ob, 0:n], lhsT=vp[:, kc + u, kv, :], rhs=pT[:, sb, u, 0:n],
                                              start=(first and u == 0), stop=(last and u == 1)),
                     reads=[t_v, t_pT[sb]], writes=[t_pso[ob]])
            if last:
                S.op("dve", lambda e: e.reciprocal(out=rd[0:64, ob, 0:n], in_=ps_o[64:128, ob, 0:n]), reads=[t_pso[ob]], writes=[t_rd[ob]])
                S.op("dve", lambda e: e.tensor_tensor(out=ao[0:64, ob, 0:n], in0=ps_o[0:64, ob, 0:n], in1=rd[0:64, ob, 0:n], op=ALU.mult),
                     reads=[t_pso[ob], t_rd[ob]], writes=[t_ao[ob]])
                S.dma(G.mixT[h * 64:(h + 1) * 64, t0:t0 + n], ao[0:64, ob, 0:n], reads=[t_ao[ob]], writes=[G.t_mix])
        if "att" in G.dbg and li == 0:
            dump_bf16(G, G.mixT[0:128, 256:768], G.dbg["att"], [G.t_mix])


def dump_bf16(G, src, dst, reads):
    nc, S = G.nc, G.S
    p, n = src.shape
    with ExitStack() as _es:
        a = _es.enter_context(SB(nc, "dmpb", [p, n], BF16))
        b = _es.enter_context(SB(nc, "dmpf", [p, n], F32))
        t = Tok()
        S.dma(a[:], src, reads=reads, writes=[t])
        S.op("dve", lambda e: e.tensor_copy(out=b[:], in_=a[:]), reads=[t], writes=[t])
        S.dma(dst, b[:], reads=[t])
        S.barrier()


def _cols_pack(inp, li, b):
    def colform(v, n):
        return np.ascontiguousarray(v.reshape(n, 128).T)
    parts = {}
    cc = np.zeros((128, 8, 2), np.float32)
    cc[:, :, 0] = colform(inp["c"][b], 8)
    cc[:, :, 1] = colform(inp["c_ctx"], 8)
    parts["cc"] = cc.reshape(128, 16)
    bm = inp["b_mod"][li].reshape(6, 8, 128)
    parts["bmod"] = np.concatenate([bm[v].T for v in (0, 1, 3, 4)], axis=1)
    parts["g_mix"] = colform(inp["g_mix"][li], 8)
    parts["g_ffn"] = colform(inp["g_ffn"][li], 8)
    parts["ssd_conv_b"] = colform(inp["ssd_conv_b"][li], 5)
    parts["qg"] = np.tile(inp["q_norm"][li], 2)[:, None]
    parts["kg"] = np.tile(inp["k_norm"][li], 2)[:, None]
    parts["ssd_d"] = colform(np.repeat(inp["ssd_d"][li], 64), 3)
    parts["ssd_norm"] = colform(inp["ssd_norm"][li], 3)
    for nm in ("hy_b1", "hy_freq", "hy_b2"):
        parts[nm] = np.tile(inp[nm][li], 2)[:, None]
    out = np.zeros((128, NCOLS), np.float32)
    for nm, (o, n) in COLS.items():
        out[:, o:o + n] = parts[nm]
    return out


def _consts():
    ident = np.eye(128, dtype=np.float32)
    s = np.arange(128)
    U = (s[:, None] <= s[None, :]).astype(np.float32)
    Lo = (s[:, None] >= s[None, :]).astype(np.float32)
    bo = np.kron(np.eye(2, dtype=np.float32), np.ones((64, 64), np.float32))
    rot = np.zeros((128, 128), np.float32)
    for hb in (0, 64):
        for d in range(32):
            rot[hb + d + 32, hb + d] = -1.0
            rot[hb + d, hb + d + 32] = 1.0
    top = np.zeros((128, 128), np.float32); top[:64] = 1.0
    bot = np.zeros((128, 128), np.float32); bot[64:] = 1.0
    cmat = np.stack([ident, U, Lo, bo, rot, top, bot, U - ident, Lo - ident])
    rows = NLAT // 64
    row = np.repeat(np.arange(rows), 64).astype(np.float32)
    col = np.tile(np.arange(64), rows).astype(np.float32)
    inv = (10000.0 ** (-np.arange(0, 32, 2, dtype=np.float32) / 32)).astype(np.float32)
    ang = np.concatenate([row[:, None] * inv, col[:, None] * inv], axis=-1).astype(np.float32)
    cs = np.cos(ang).astype(np.float32).T
    sn = np.sin(ang).astype(np.float32).T
    rope = np.stack([np.tile(cs, (4, 1)), np.tile(sn, (4, 1))]).astype(np.float32)
    tt = np.arange(512)[None, None, :]
    ss_ = np.arange(128)[None, :, None]
    jj = np.arange(4)[:, None, None]
    mf = (tt >= 128 * jj + ss_).astype(np.float32)
    mb = (tt <= 128 * jj + ss_).astype(np.float32)
    hyt = {}
    for sname, P in SEGS.items():
        ft, it_, fe, wn, mh = _hy_tables(P["L"])
        hyt["ft_" + sname], hyt["it_" + sname], hyt["fe_" + sname], hyt["win_" + sname], hyt["mh_" + sname] = ft, it_, fe, wn, mh
    return {**hyt, "cmat": cmat, "rope": rope, "ssdmask": np.stack([mf, mb]).astype(ml_dtypes.bfloat16)}


_CONSTS = None


def kernel(**inp):
    global _CONSTS
    inp = {k: np.asarray(v) for k, v in inp.items()}
    if _CONSTS is None:
        _CONSTS = _consts()
    dbg = kernel.dbg if hasattr(kernel, "dbg") else None
    nc = build(dbg)
    ncores = 8
    in_maps = []
    for core in range(ncores):
        b = core % 4
        m = {"x": np.ascontiguousarray(inp["x"][b]), "ctx": np.ascontiguousarray(inp["ctx"][b])}
        for k in ("w_mod", "b_mod", "w_in", "w_out", "w_router", "router_bias", "w_gate", "w_up", "w_down", "g_final",
                  "ssd_conv_w", "ssd_dt_bias", "ssd_a_log", "ssd_d", "ssd_norm", "hy_conv_w", "hy_conv_b", "hy_w1", "hy_w2", "hy_w3", "hy_bias"):
            m[k] = inp[k]
        m["cols"] = np.stack([_cols_pack(inp, li, b) for li in range(DEPTH)])
        m.update(_CONSTS)
        in_maps.append(m)
    res = run_bass_kernel_spmd(nc, in_maps, core_ids=list(range(ncores)))
    kernel.last = res
    return np.stack([res.results[b]["out"] for b in range(4)]).astype(np.float32)


def outproj(G, li):
    nc, S, I = G.nc, G.S, G.I
    need_ctx = li < DEPTH - 1
    with ExitStack() as _es:
        wo = _es.enter_context(SB(nc, "wo", [128, 8, D], BF16))
        mx = _es.enter_context(SB(nc, "mx", [128, 2, 8, 128], BF16))
        xt = _es.enter_context(SB(nc, "xt", [128, 2, D], F32))
        tmp = _es.enter_context(SB(nc, "tmp", [128, D], F32))
        sq = _es.enter_context(SB(nc, "sq", [128, D], F32))
        ss = _es.enter_context(SB(nc, "ss", [128, 4], F32))
        xn = _es.enter_context(SB(nc, "xn", [128, D], F32))
        h2f = _es.enter_context(SB(nc, "h2f", [128, 8, 128], F32))
        wr = _es.enter_context(SB(nc, "wr", [128, 8, NE], F32))
        po = _es.enter_context(PS(nc, "po", [128, 2, 512], F32))
        pt = _es.enter_context(PS(nc, "pt", [128, 8, 128], F32))
        pr = _es.enter_context(PS(nc, "pr", [128, NE], F32))
        t_wo, t_mx, t_x, t_tmp, t_po = Tok(), [Tok(), Tok()], [Tok(), Tok()], Tok(), Tok()
        t_sq, t_ss, t_xn, t_pt, t_h2f, t_wr, t_pr = Tok(), Tok(), Tok(), Tok(), Tok(), Tok(), Tok()
        S.dma(wo[:], G.wb_out[li].rearrange("(k p) c -> p k c", p=128), reads=[G.t_wb], writes=[t_wo])
        S.dma(wr[:], I["w_router"].rearrange("(k p) c -> p k c", p=128), writes=[t_wr])
        mv = G.mixT.rearrange("(k p) t -> p k t", p=128)
        tiles = [t for t in range(NT) if need_ctx or t >= 2]

        def mm(tile):
            b = tile % 2
            S.dma(mx[:, b], mv[:, :, tile * 128:(tile + 1) * 128], reads=[G.t_mix], writes=[t_mx[b]])
            S.dma(xt[:, b, :], G.xres[tile * 128:(tile + 1) * 128, :], reads=[G.t_xres[tile]], writes=[t_x[b]])
            for half in range(2):
                for k in range(8):
                    S.op("pe", lambda e: e.matmul(po[:, half, :], lhsT=mx[:, b, k, :], rhs=wo[:, k, half * 512:(half + 1) * 512],
                                                  start=(k == 0), stop=(k == 7)), reads=[t_mx[b], t_wo], writes=[t_po])

        mm(tiles[0])
        for ti, tile in enumerate(tiles):
            b = tile % 2
            j = 1 if tile < 2 else 0
            S.op("dve", lambda e: e.tensor_tensor(out=tmp[:], in0=po[:].rearrange("p a c -> p (a c)"), in1=G.gtb[:, 0, j, :], op=ALU.mult),
                 reads=[t_po, G.t_gtb], writes=[t_tmp])
            if ti + 1 < len(tiles):
                mm(tiles[ti + 1])
            S.op("dve", lambda e: e.tensor_tensor(out=xt[:, b, :], in0=tmp[:], in1=xt[:, b, :], op=ALU.add),
                 reads=[t_tmp, t_x[b]], writes=[t_x[b]])
            S.dma(G.xres[tile * 128:(tile + 1) * 128, :], xt[:, b, :], reads=[t_x[b]], writes=[G.t_xres[tile]])
            xv = xt[:, b, :]
            S.op("act", lambda e: e.activation(out=sq[:], in_=xv, func=AF.Square, accum_out=ss[:, 0:1]), reads=[t_x[b]], writes=[t_sq, t_ss])
            S.op("act", lambda e: e.activation(out=ss[:, 1:2], in_=ss[:, 0:1], func=AF.Sqrt, bias=G.epsc[:], scale=1.0 / D),
                 reads=[t_ss, G.t_c], writes=[t_ss])
            S.op("dve", lambda e: e.reciprocal(out=ss[:, 2:3], in_=ss[:, 1:2]), reads=[t_ss], writes=[t_ss])
            S.op("dve", lambda e: e.tensor_scalar(out=xn[:], in0=xv, scalar1=ss[:, 2:3], scalar2=None, op0=ALU.mult),
                 reads=[t_x[b], t_ss], writes=[t_xn])
            for k in range(8):
                S.op("pe", lambda e: e.transpose(out=pt[:, k, :], in_=xn[:, k * 128:(k + 1) * 128], identity=G.identF),
                     reads=[t_xn, G.t_c], writes=[t_pt])
            S.op("dve", lambda e: e.tensor_tensor(out=h2f[:], in0=pt[:], in1=G.modc[:, 3, :, j:j + 1].to_broadcast([128, 8, 128]), op=ALU.mult),
                 reads=[t_pt, G.t_modc], writes=[t_h2f])
            S.op("dve", lambda e: e.tensor_tensor(out=h2f[:], in0=h2f[:], in1=G.modc[:, 2, :, j:j + 1].to_broadcast([128, 8, 128]), op=ALU.add),
                 reads=[t_h2f, G.t_modc], writes=[t_h2f])
            S.op("act", lambda e: e.copy(out=G.h2T[:, :, tile * 128:(tile + 1) * 128], in_=h2f[:]), reads=[t_h2f], writes=[G.t_h2T])
            for k in range(8):
                S.op("pe", lambda e: e.matmul(pr[:], lhsT=h2f[:, k, :], rhs=wr[:, k, :], start=(k == 0), stop=(k == 7)),
                     reads=[t_h2f, t_wr], writes=[t_pr])
            S.op("dve", lambda e: e.tensor_copy(out=G.rl[:, tile, :], in_=pr[:]), reads=[t_pr], writes=[G.t_rl])


def moe(G, li):
    nc, S, I = G.nc, G.S, G.I
    need_ctx = li < DEPTH - 1
    last = li == DEPTH - 1
    h2T, rl = G.h2T, G.rl
    T0 = 0 if need_ctx else 2
    NTl = NT - T0
    BIG = 1.0e9
    with ExitStack() as _es:
        comb = _es.enter_context(SB(nc, "comb", [128, NT, NE], F32))
        t_comb = Tok()
        with ExitStack() as _es:
            sc = _es.enter_context(SB(nc, "r_sc", [128, NT, NE], F32))
            sel = _es.enter_context(SB(nc, "r_sel", [128, NT, NE], F32))
            ra = _es.enter_context(SB(nc, "r_a", [128, NT, NE], F32))
            rb = _es.enter_context(SB(nc, "r_b", [128, NT, NE], F32))
            rm = _es.enter_context(SB(nc, "r_m", [128, NT * 4], F32))
            rm2 = _es.enter_context(SB(nc, "r_m2", [128, NT * 4], F32))
            rg = _es.enter_context(SB(nc, "r_g", [128, NT], F32))
            rbias = _es.enter_context(SB(nc, "rbias", [128, NE], F32))
            t = Tok()
            if T0 > 0:
                S.op("dve", lambda e: e.memset(rl[:, 0:T0, :], 0.0), reads=[G.t_rl], writes=[G.t_rl])
            S.dma(rbias[:], I["router_bias"].partition_broadcast(128), writes=[t])
            v3 = lambda a: a[:].rearrange("p n (g x) -> p (n g) x", x=4)
            S.op("act", lambda e: e.activation(out=sc[:], in_=rl[:], func=AF.Sigmoid), reads=[G.t_rl], writes=[t])
            S.op("dve", lambda e: e.tensor_tensor(out=sel[:], in0=sc[:], in1=rbias[:].unsqueeze(1).to_broadcast([128, NT, NE]), op=ALU.add),
                 reads=[t], writes=[t])
            S.op("dve", lambda e: e.tensor_reduce(out=rm[:], in_=v3(sel), axis=AX.X, op=ALU.max), reads=[t], writes=[t])
            S.op("dve", lambda e: e.tensor_tensor(out=v3(ra), in0=v3(sel), in1=rm[:].unsqueeze(2).to_broadcast([128, NT * 4, 4]), op=ALU.is_equal),
                 reads=[t], writes=[t])
            S.op("dve", lambda e: e.scalar_tensor_tensor(out=rb[:], in0=ra[:], scalar=-BIG, in1=sel[:], op0=ALU.mult, op1=ALU.add),
                 reads=[t], writes=[t])
            S.op("dve", lambda e: e.tensor_reduce(out=rm2[:], in_=v3(rb), axis=AX.X, op=ALU.max), reads=[t], writes=[t])
            S.op("dve", lambda e: e.tensor_tensor(out=rm[:], in0=rm[:], in1=rm2[:], op=ALU.add), reads=[t], writes=[t])
            S.op("dve", lambda e: e.tensor_reduce(out=rg[:], in_=rm[:].rearrange("p (n g) -> p n g", g=4), axis=AX.X, op=ALU.max),
                 reads=[t], writes=[t])
            S.op("dve", lambda e: e.tensor_tensor(out=rm2[:].rearrange("p (n g) -> p n g", g=4), in0=rm[:].rearrange("p (n g) -> p n g", g=4),
                                                  in1=rg[:].unsqueeze(2).to_broadcast([128, NT, 4]), op=ALU.is_equal), reads=[t], writes=[t])
            S.op("dve", lambda e: e.tensor_scalar(out=rm2[:], in0=rm2[:], scalar1=1.0, scalar2=BIG, op0=ALU.subtract, op1=ALU.mult),
                 reads=[t], writes=[t])
            S.op("dve", lambda e: e.tensor_tensor(out=v3(sel), in0=v3(sel), in1=rm2[:].unsqueeze(2).to_broadcast([128, NT * 4, 4]), op=ALU.add),
                 reads=[t], writes=[t])
            S.op("dve", lambda e: e.tensor_reduce(out=rg[:], in_=sel[:], axis=AX.X, op=ALU.max), reads=[t], writes=[t])
            S.op("dve", lambda e: e.tensor_tensor(out=ra[:], in0=sel[:], in1=rg[:].unsqueeze(2).to_broadcast([128, NT, NE]), op=ALU.is_equal),
                 reads=[t], writes=[t])
            S.op("dve", lambda e: e.scalar_tensor_tensor(out=sel[:], in0=ra[:], scalar=-BIG, in1=sel[:], op0=ALU.mult, op1=ALU.add),
                 reads=[t], writes=[t])
            S.op("dve", lambda e: e.tensor_reduce(out=rg[:], in_=sel[:], axis=AX.X, op=ALU.max), reads=[t], writes=[t])
            S.op("dve", lambda e: e.tensor_tensor(out=rb[:], in0=sel[:], in1=rg[:].unsqueeze(2).to_broadcast([128, NT, NE]), op=ALU.is_equal),
                 reads=[t], writes=[t])
            S.op("dve", lambda e: e.tensor_tensor(out=ra[:], in0=ra[:], in1=rb[:], op=ALU.add), reads=[t], writes=[t])
            S.op("dve", lambda e: e.tensor_tensor(out=ra[:], in0=ra[:], in1=sc[:], op=ALU.mult), reads=[t], writes=[t])
            S.op("dve", lambda e: e.tensor_reduce(out=rg[:], in_=ra[:], axis=AX.X, op=ALU.add), reads=[t], writes=[t])
            S.op("dve", lambda e: e.reciprocal(out=rg[:], in_=rg[:]), reads=[t], writes=[t])
            S.op("dve", lambda e: e.tensor_tensor(out=comb[:], in0=ra[:], in1=rg[:].unsqueeze(2).to_broadcast([128, NT, NE]), op=ALU.mult),
                 reads=[t], writes=[t_comb])
            S.barrier()
        SGT = 12
        with ExitStack() as _es:
            acc = _es.enter_context(SB(nc, "acc", [128, SGT, D], F32))
            wg = _es.enter_context(SB(nc, "wg", [128, 2, 8, DFF], BF16))
            wu = _es.enter_context(SB(nc, "wu", [128, 2, 8, DFF], BF16))
            wd = _es.enter_context(SB(nc, "wd", [128, 2, 2, D], BF16))
            sgl = _es.enter_context(SB(nc, "sgl", [128, 2, 512], F32))
            aa = _es.enter_context(SB(nc, "aa", [128, 2, 512], BF16))
            xt = _es.enter_context(SB(nc, "xt", [128, 2, D], F32))
            gfb = _es.enter_context(SB(nc, "gfb", [128, D], F32))
            ss = _es.enter_context(SB(nc, "ss", [128, 4], F32))
            sq = _es.enter_context(SB(nc, "sq", [128, D], F32))
            pgu = _es.enter_context(PS(nc, "pgu", [128, 4, 512], F32))
            py = _es.enter_context(PS(nc, "py", [128, 2, 2, 512], F32))
            t_acc, t_w, t_sgl, t_aa, t_pgu, t_py, t_x, t_gf, t_ss, t_sq = Tok(), [Tok(), Tok()], Tok(), Tok(), Tok(), [Tok(), Tok()], [Tok(), Tok()], Tok(), Tok(), Tok()
            if last:
                S.dma(gfb[:], I["g_final"].partition_broadcast(128), writes=[t_gf])
            yi = 0
            for s0 in range(T0, NT, SGT):
                tiles = list(range(s0, min(NT, s0 + SGT)))
                for ex in range(NE):
                    wb_ = ex % 2
                    S.dma(wg[:, wb_], G.wb_gate[li][ex].rearrange("(k p) f -> p k f", p=128), reads=[G.t_wb], writes=[t_w[wb_]])
                    S.dma(wu[:, wb_], G.wb_up[li][ex].rearrange("(k p) f -> p k f", p=128), reads=[G.t_wb], writes=[t_w[wb_]])
                    S.dma(wd[:, wb_], G.wb_down[li][ex].rearrange("(j p) c -> p j c", p=128), reads=[G.t_wb], writes=[t_w[wb_]])
                    for b0 in range(0, len(tiles), 4):
                        bt = tiles[b0:b0 + 4]
                        n = len(bt) * 128
                        c0 = bt[0] * 128
                        for wi, wt in enumerate((wg, wu)):
                            for jj in range(2):
                                for k in range(8):
                                    S.op("pe", lambda e: e.matmul(pgu[:, wi * 2 + jj, 0:n], lhsT=wt[:, wb_, k, jj * 128:(jj + 1) * 128],
                                                                  rhs=h2T[:, k, c0:c0 + n], start=(k == 0), stop=(k == 7)),
                                         reads=[t_w[wb_], G.t_h2T], writes=[t_pgu])
                        S.op("act", lambda e: e.activation(out=sgl[:, :, 0:n], in_=pgu[:, 0:2, 0:n], func=AF.Silu), reads=[t_pgu], writes=[t_sgl])
                        S.op("dve", lambda e: e.tensor_tensor(out=aa[:, :, 0:n], in0=sgl[:, :, 0:n], in1=pgu[:, 2:4, 0:n], op=ALU.mult),
                             reads=[t_sgl, t_pgu], writes=[t_aa])
                        for ti, tl in enumerate(bt):
                            yb = yi % 2
                            yi += 1
                            for half in range(2):
                                for jj in range(2):
                                    S.op("pe", lambda e: e.matmul(py[:, yb, half, :], lhsT=aa[:, jj, ti * 128:(ti + 1) * 128],
                                                                  rhs=wd[:, wb_, jj, half * 512:(half + 1) * 512], start=(jj == 0), stop=(jj == 1)),
                                         reads=[t_aa, t_w[wb_]], writes=[t_py[yb]])
                            al = acc[:, tl - s0, :]
                            pyv = py[:, yb].rearrange("p a c -> p (a c)")
                            if ex == 0:
                                S.op("dve", lambda e: e.tensor_scalar(out=al, in0=pyv, scalar1=comb[:, tl, ex:ex + 1], scalar2=None, op0=ALU.mult),
                                     reads=[t_py[yb], t_comb], writes=[t_acc])
                            else:
                                S.op("dve", lambda e: e.scalar_tensor_tensor(out=al, in0=pyv, scalar=comb[:, tl, ex:ex + 1], in1=al,
                                                                             op0=ALU.mult, op1=ALU.add), reads=[t_py[yb], t_comb, t_acc], writes=[t_acc])
                for tl in tiles:
                    b = tl % 2
                    j = 1 if tl < 2 else 0
                    S.dma(xt[:, b, :], G.xres[tl * 128:(tl + 1) * 128, :], reads=[G.t_xres[tl]], writes=[t_x[b]])
                    al = acc[:, tl - s0, :]
                    S.op("dve", lambda e: e.tensor_tensor(out=al, in0=al, in1=G.gtb[:, 1, j, :], op=ALU.mult), reads=[t_acc, G.t_gtb], writes=[t_acc])
                    S.op("dve", lambda e: e.tensor_tensor(out=xt[:, b, :], in0=al, in1=xt[:, b, :], op=ALU.add), reads=[t_acc, t_x[b]], writes=[t_x[b]])
                    if not last:
                        S.dma(G.xres[tl * 128:(tl + 1) * 128, :], xt[:, b, :], reads=[t_x[b]], writes=[G.t_xres[tl]])
                    else:
                        xv = xt[:, b, :]
                        S.op("act", lambda e: e.activation(out=sq[:], in_=xv, func=AF.Square, accum_out=ss[:, 0:1]), reads=[t_x[b]], writes=[t_sq, t_ss])
                        S.op("act", lambda e: e.activation(out=ss[:, 1:2], in_=ss[:, 0:1], func=AF.Sqrt, bias=G.epsc[:], scale=1.0 / D),
                             reads=[t_ss, G.t_c], writes=[t_ss])
                        S.op("dve", lambda e: e.reciprocal(out=ss[:, 2:3], in_=ss[:, 1:2]), reads=[t_ss], writes=[t_ss])
                        S.op("dve", lambda e: e.scalar_tensor_tensor(out=xv, in0=xv, scalar=ss[:, 2:3], in1=gfb[:], op0=ALU.mult, op1=ALU.mult),
                             reads=[t_x[b], t_ss, t_gf], writes=[t_x[b]])
                        S.dma(G.out[(tl - 2) * 128:(tl - 1) * 128, :], xv, reads=[t_x[b]], writes=[])


def ssd(G, li):
    nc, S, I = G.nc, G.S, G.I
    hT = G.hT
    need_ctx = li < DEPTH - 1
    wv32 = I["w_in"][li].rearrange("(k p) c -> p k c", p=128)
    wvb = G.wb_in[li].rearrange("(k p) c -> p k c", p=128)
    XB0 = 1024
    with ExitStack() as _es:
        xbcT = _es.enter_context(SB(nc, "xbcT", [128, 5, NTOK], BF16))
        xs_tok = _es.enter_context(SB(nc, "xs_tok", [128, NT, 384], BF16))
        B_tok = _es.enter_context(SB(nc, "B_tok", [128, NT, 128], BF16))
        lndt = _es.enter_context(SB(nc, "lndt", [128, NT, 12], F32))
        dta = _es.enter_context(SB(nc, "dta", [128, NT, 12], F32))
        ea = _es.enter_context(SB(nc, "ea", [128, NT, 12], F32))
        ww = _es.enter_context(SB(nc, "ww", [128, NT, 12], F32))
        eT = _es.enter_context(SB(nc, "eT", [128, NT, 12], F32))
        t_xbc, t_xs, t_dt = Tok(), Tok(), Tok()
        with ExitStack() as _es2:
            dts = _es2.enter_context(SB(nc, "dts", [128, NT, 12], F32))
            wst = _es2.enter_context(SB(nc, "wst", [128, 8, 128], F32))
            cwb = _es2.enter_context(SB(nc, "cwb", [128, 3, 128], F32))
            wj = _es2.enter_context(SB(nc, "wj", [128, 3, 8, 128], BF16))
            wdt = _es2.enter_context(SB(nc, "wdt", [128, 8, 12], BF16))
            dtb = _es2.enter_context(SB(nc, "dtb", [128, 2, 12], F32))
            tot = _es2.enter_context(SB(nc, "tot", [128, NT, 12], F32))
            wcol = _es2.enter_context(SB(nc, "wcol", [128, NT, 12], F32))
            pp = _es2.enter_context(PS(nc, "pp", [128, 512], F32))
            pdt = _es2.enter_context(PS(nc, "pdt", [128, NT, 12], F32))
            ptb = _es2.enter_context(PS(nc, "ptb", [128, 4, 128], BF16))
            pc = _es2.enter_context(PS(nc, "pc", [128, NT, 12], F32))
            t_wst, t_cwb, t_wj, t_pp, t_wdt, t_pdt, t_ptb, t_a, t_pc = Tok(), Tok(), Tok(), Tok(), Tok(), Tok(), Tok(), Tok(), Tok()
            ocb = COLS["ssd_conv_b"][0]
            for ch in range(5):
                S.dma(wst[:], wv32[:, :, XB0 + ch * 128:XB0 + (ch + 1) * 128], writes=[t_wst])
                for j in range(3):
                    S.dma(cwb[:, j, :], I["ssd_conv_w"][li, j, ch * 128:(ch + 1) * 128].partition_broadcast(128), writes=[t_cwb])
                for j in range(3):
                    S.op("dve", lambda e: e.tensor_tensor(out=wj[:, j], in0=wst[:], in1=cwb[:, j:j + 1, :].to_broadcast([128, 8, 128]), op=ALU.mult),
                         reads=[t_wst, t_cwb], writes=[t_wj])
                for (c0, t0, n, tile0, ntile) in BLOCKS:
                    for j in range(3):
                        for k in range(8):
                            S.op("pe", lambda e: e.matmul(pp[:, 0:n], lhsT=wj[:, j, k, :], rhs=hT[:, k, c0 + j - 1:c0 + j - 1 + n],
                                                          start=(j == 0 and k == 0), stop=(j == 2 and k == 7)), reads=[t_wj, G.t_hT], writes=[t_pp])
                    S.op("act", lambda e: e.activation(out=xbcT[:, ch, t0:t0 + n], in_=pp[:, 0:n], func=AF.Silu, bias=G.cols[:, ocb + ch:ocb + ch + 1]),
                         reads=[t_pp, G.t_cols], writes=[t_xbc])
            S.dma(wdt[:], wvb[:, :, 1664:1676], reads=[G.t_wb], writes=[t_wdt])
            S.dma(dtb[:, 0, :], I["ssd_dt_bias"][li].rearrange("a h -> (a h)").partition_broadcast(128), writes=[t_wdt])
            S.dma(dtb[:, 1, :], I["ssd_a_log"][li].rearrange("a h -> (a h)").partition_broadcast(128), writes=[t_wdt])
            for tl in range(NT):
                cc = colof(tl)
                for k in range(8):
                    S.op("pe", lambda e: e.matmul(pdt[:, tl, :], lhsT=hT[:, k, cc:cc + 128], rhs=wdt[:, k, :], start=(k == 0), stop=(k == 7)),
                         reads=[t_wdt, G.t_hT], writes=[t_pdt])
            S.op("dve", lambda e: e.tensor_tensor(out=dts[:], in0=pdt[:], in1=dtb[:, 0:1, :].to_broadcast([128, NT, 12]), op=ALU.add),
                 reads=[t_pdt, t_wdt], writes=[t_dt])
            S.op("act", lambda e: e.activation(out=dts[:], in_=dts[:], func=AF.Exp), reads=[t_dt], writes=[t_dt])
            S.op("act", lambda e: e.activation(out=dts[:], in_=dts[:], func=AF.Ln, bias=1.0), reads=[t_dt], writes=[t_dt])
            S.op("act", lambda e: e.activation(out=lndt[:], in_=dts[:], func=AF.Ln), reads=[t_dt], writes=[t_dt])
            S.op("act", lambda e: e.activation(out=dtb[:, 1, :], in_=dtb[:, 1, :], func=AF.Exp), reads=[t_wdt], writes=[t_wdt])
            S.op("dve", lambda e: e.scalar_tensor_tensor(out=dta[:], in0=dts[:], scalar=-1.0, in1=dtb[:, 1:2, :].to_broadcast([128, NT, 12]),
                                                         op0=ALU.mult, op1=ALU.mult), reads=[t_dt, t_wdt], writes=[t_a])
            for tl in range(NT):
                for c in range(4):
                    S.op("pe", lambda e: e.transpose(out=ptb[:, c, :], in_=xbcT[:, c, tl * 128:(tl + 1) * 128], identity=G.identB),
                         reads=[t_xbc, G.t_c], writes=[t_ptb])
                S.op("dve", lambda e: e.tensor_copy(out=xs_tok[:, tl, :], in_=ptb[:, 0:3, :].rearrange("p c t -> p (c t)")), reads=[t_ptb], writes=[t_xs])
                S.op("dve", lambda e: e.tensor_copy(out=B_tok[:, tl, :], in_=ptb[:, 3, :]), reads=[t_ptb], writes=[t_xs])
            for dr in range(2):
                S.op("pe", lambda e: e.matmul(pc[:].rearrange("p n h -> p (n h)"), lhsT=G.cm[:, 1 + dr, :], rhs=dta[:].rearrange("p n h -> p (n h)"),
                                              start=True, stop=True), reads=[t_a, G.t_c, t_dt], writes=[t_pc])
                S.op("dve", lambda e: e.tensor_copy(out=wcol[:, :, dr * 6:(dr + 1) * 6], in_=pc[:, :, dr * 6:(dr + 1) * 6]), reads=[t_pc], writes=[t_dt])
            S.op("pe", lambda e: e.matmul(pc[:].rearrange("p n h -> p (n h)"), lhsT=G.ones[:], rhs=dta[:].rearrange("p n h -> p (n h)"), start=True, stop=True),
                 reads=[t_a, G.t_c, t_dt], writes=[t_pc])
            S.op("dve", lambda e: e.tensor_copy(out=tot[:], in_=pc[:]), reads=[t_pc], writes=[t_dt])
            S.op("act", lambda e: e.activation(out=ea[:], in_=wcol[:], func=AF.Exp), reads=[t_dt], writes=[t_dt])
            S.op("act", lambda e: e.activation(out=eT[:], in_=tot[:], func=AF.Exp), reads=[t_dt], writes=[t_dt])
            S.op("dve", lambda e: e.tensor_tensor(out=ww[:], in0=tot[:], in1=wcol[:], op=ALU.subtract), reads=[t_dt], writes=[t_dt])
            S.op("dve", lambda e: e.tensor_tensor(out=ww[:], in0=ww[:], in1=lndt[:], op=ALU.add), reads=[t_dt], writes=[t_dt])
            S.op("act", lambda e: e.activation(out=ww[:], in_=ww[:], func=AF.Exp), reads=[t_dt], writes=[t_dt])
            S.barrier()
        with ExitStack() as _es2:
            wz = _es2.enter_context(SB(nc, "wz", [128, 8, 384], BF16))
            dbc = _es2.enter_context(SB(nc, "dbc", [128, 6, 64], F32))
            d6 = _es2.enter_context(SB(nc, "d6", [128, 6], F32))
            nwb = _es2.enter_context(SB(nc, "nwb", [128, 384], F32))
            hst = _es2.enter_context(SB(nc, "hst", [128, 192], F32))
            hsb = _es2.enter_context(SB(nc, "hsb", [128, 192], BF16))
            xw = _es2.enter_context(SB(nc, "xw", [128, 2, 192], BF16))
            ybt = _es2.enter_context(SB(nc, "ybt", [128, 2, 384], F32))
            Sm = _es2.enter_context(SB(nc, "Sm", [128, 2, 2, 128], F32))
            rA = _es2.enter_context(SB(nc, "rA", [128, 4, 128], F32))
            Dd = _es2.enter_context(SB(nc, "Dd", [128, 4, 128], F32))
            Mt = _es2.enter_context(SB(nc, "Mt", [128, 4, 128], BF16))
            acc = _es2.enter_context(SB(nc, "acc", [128, 384], F32))
            tmp = _es2.enter_context(SB(nc, "tmp", [128, 384], F32))
            zs = _es2.enter_context(SB(nc, "zs", [128, 384], F32))
            ssq = _es2.enter_context(SB(nc, "ssq", [128, 4], F32))
            ob = _es2.enter_context(SB(nc, "ob", [128, 384], BF16))
            oT = _es2.enter_context(SB(nc, "oT", [128, 2, 3, 128], BF16))
            pis = _es2.enter_context(PS(nc, "pis", [128, 2, 192], F32))
            ps_st = _es2.enter_context(PS(nc, "ps_st", [128, 2, 512], F32))
            pseg = _es2.enter_context(PS(nc, "pseg", [128, 3, 512], F32))
            py = _es2.enter_context(PS(nc, "py", [128, 384], F32))
            pz = py
            ptr = _es2.enter_context(PS(nc, "ptr", [128, 3, 128], BF16))
            t_wz, t_db, t_h, t_xw, t_yb, t_Sm, t_acc, t_tmp, t_zs, t_ssq, t_ob, t_oT = Tok(), Tok(), Tok(), Tok(), [Tok(), Tok()], Tok(), Tok(), Tok(), Tok(), Tok(), Tok(), [Tok(), Tok()]
            t_rA, t_Dd, t_Mt, t_pseg = [Tok() for _ in range(4)], [Tok() for _ in range(4)], [Tok() for _ in range(4)], [Tok() for _ in range(3)]
            t_pis, t_pst, t_py, t_ptr = Tok(), Tok(), Tok(), Tok()
            t_pz = t_py
            S.dma(wz[:], wvb[:, :, 640:1024], reads=[G.t_wb], writes=[t_wz])
            S.dma(d6[:], I["ssd_d"][li].partition_broadcast(128), writes=[t_db])
            S.dma(nwb[:], I["ssd_norm"][li].partition_broadcast(128), writes=[t_db])
            S.op("dve", lambda e: e.tensor_copy(out=dbc[:], in_=d6[:].unsqueeze(2).to_broadcast([128, 6, 64])), reads=[t_db], writes=[t_db])
            yb_d = G.ssd_yb

            def carry_step(c, dr, want_out, dst):
                for g in range(2):
                    hd0 = dr * 6 + 3 * g
                    if want_out:
                        S.op("pe", lambda e: e.matmul(pis[:, 0, :], lhsT=xbcT[g * 64:(g + 1) * 64, 4, c * 128:(c + 1) * 128], rhs=hsb[g * 64:(g + 1) * 64, :],
                                                      start=True, stop=True), reads=[t_xbc, t_h], writes=[t_pis])
                        S.op("dve", lambda e: e.tensor_tensor(out=dst[:, g * 192:(g + 1) * 192].rearrange("p (a d) -> p a d", a=3),
                                                              in0=pis[:, 0, :].rearrange("p (a d) -> p a d", a=3),
                                                              in1=ea[:, c, hd0:hd0 + 3].unsqueeze(2).to_broadcast([128, 3, 64]), op=ALU.mult),
                             reads=[t_pis, t_dt], writes=[dst_tok[0]])
                    S.op("dve", lambda e: e.tensor_tensor(out=xw[:, g, :].rearrange("p (a d) -> p a d", a=3),
                                                          in0=xs_tok[:, c, g * 192:(g + 1) * 192].rearrange("p (a d) -> p a d", a=3),
                                                          in1=ww[:, c, hd0:hd0 + 3].unsqueeze(2).to_broadcast([128, 3, 64]), op=ALU.mult),
                         reads=[t_xs, t_dt], writes=[t_xw])
                    gs = slice(g * 64, (g + 1) * 64)
                    S.op("pe", lambda e: e.matmul(pis[:, 1, :], lhsT=B_tok[:, c, :], rhs=xw[:, g, :], start=True, stop=True),
                         reads=[t_xs, t_xw], writes=[t_pis])
                    S.op("dve", lambda e: e.tensor_tensor(out=hst[gs, :].rearrange("p (a d) -> p a d", a=3),
                                                          in0=hst[gs, :].rearrange("p (a d) -> p a d", a=3),
                                                          in1=eT[gs, c, hd0:hd0 + 3].unsqueeze(2).to_broadcast([64, 3, 64]), op=ALU.mult),
                         reads=[t_h, t_dt], writes=[t_h])
                    S.op("dve", lambda e: e.tensor_tensor(out=hst[gs, :], in0=hst[gs, :], in1=pis[gs, 1, :], op=ALU.add),
                         reads=[t_h, t_pis], writes=[t_h])
                    S.op("act", lambda e: e.copy(out=hsb[gs, :], in_=hst[gs, :]), reads=[t_h], writes=[t_h])

            S.op("dve", lambda e: e.memset(hst[:], 0.0), writes=[t_h])
            S.op("dve", lambda e: e.memset(hsb[:], 0.0), writes=[t_h])
            order_b = [1, 0] + list(range(NT - 1, 1, -1))
            for ci, c in enumerate(order_b):
                want = need_ctx or c >= 2
                b = ci % 2
                dst_tok = [t_yb[b]]
                carry_step(c, 1, want, ybt[:, b, :])
                if want:
                    S.dma(yb_d[c * 128:(c + 1) * 128, :], ybt[:, b, :], reads=[t_yb[b]], writes=[G.t_ssdyb[c]])
            S.op("dve", lambda e: e.memset(hst[:], 0.0), reads=[t_h], writes=[t_h])
            S.op("dve", lambda e: e.memset(hsb[:], 0.0), reads=[t_h], writes=[t_h])
            on = COLS["ssd_norm"][0]
            it = 0
            for c in range(NT):
                want = need_ctx or c >= 2
                dst_tok = [t_acc]
                carry_step(c, 0, want, acc[:])
                if not want:
                    continue
                b = c % 2
                tc0 = c * 128
                S.dma(ybt[:, b, :], yb_d[c * 128:(c + 1) * 128, :], reads=[G.t_ssdyb[c]], writes=[t_yb[b]])
                cc = colof(c)
                for k in range(8):
                    S.op("pe", lambda e: e.matmul(pz[:], lhsT=hT[:, k, cc:cc + 128], rhs=wz[:, k, :], start=(k == 0), stop=(k == 7)),
                         reads=[t_wz, G.t_hT], writes=[t_pz])
                S.op("act", lambda e: e.activation(out=zs[:], in_=pz[:], func=AF.Silu), reads=[t_pz], writes=[t_zs])
                for g in range(2):
                    S.op("pe", lambda e: e.matmul(ps_st[:, g, 0:128], lhsT=xbcT[g * 64:(g + 1) * 64, 3, tc0:tc0 + 128], rhs=xbcT[g * 64:(g + 1) * 64, 4, tc0:tc0 + 128],
                                                  start=True, stop=True), reads=[t_xbc], writes=[t_pst])
                for g in range(2):
                    for dr in range(2):
                        S.op("dve", lambda e: e.tensor_tensor(out=Sm[:, g, dr, :], in0=ps_st[:, g, 0:128], in1=G.cm[:, 1 + dr, :], op=ALU.mult),
                             reads=[t_pst, G.t_c], writes=[t_Sm])
                steps = [(h, dr) for h in range(6) for dr in range(2)]

                def st_a(i):
                    h, dr = steps[i]
                    hd = dr * 6 + h
                    r4, p3 = (it + i) % 4, (it + i) % 3
                    S.op("dve", lambda e: e.tensor_scalar(out=rA[:, r4, :], in0=G.cm[:, 1 + dr, :], scalar1=dta[:, c, hd:hd + 1], scalar2=None, op0=ALU.mult),
                         reads=[G.t_c, t_dt], writes=[t_rA[r4]])
                    S.op("pe", lambda e: e.matmul(pseg[:, p3, 0:128], lhsT=G.cm[:, 8 - dr, :], rhs=rA[:, r4, :], start=True, stop=True),
                         reads=[t_rA[r4], G.t_c], writes=[t_pseg[p3]])
                    S.op("act", lambda e: e.activation(out=Dd[:, r4, :], in_=pseg[:, p3, 0:128], func=AF.Exp, bias=lndt[:, c, hd:hd + 1]),
                         reads=[t_pseg[p3], t_dt], writes=[t_Dd[r4]])

                st_a(0)
                st_a(1)
                for i, (h, dr) in enumerate(steps):
                    r4 = (it + i) % 4
                    if i + 2 < len(steps):
                        st_a(i + 2)
                    S.op("dve", lambda e: e.tensor_tensor(out=Mt[:, r4, :], in0=Dd[:, r4, :], in1=Sm[:, h // 3, dr, :], op=ALU.mult),
                         reads=[t_Dd[r4], t_Sm], writes=[t_Mt[r4]])
                    S.op("pe", lambda e: e.matmul(py[:, h * 64:(h + 1) * 64], lhsT=Mt[:, r4, :], rhs=xs_tok[:, c, h * 64:(h + 1) * 64],
                                                  start=(dr == 0), stop=(dr == 1)), reads=[t_Mt[r4], t_xs], writes=[t_py])
                it += len(steps)
                S.op("dve", lambda e: e.tensor_tensor(out=acc[:], in0=acc[:], in1=py[:], op=ALU.add), reads=[t_acc, t_py], writes=[t_acc])
                S.op("dve", lambda e: e.tensor_tensor(out=acc[:], in0=acc[:], in1=ybt[:, b, :], op=ALU.add), reads=[t_acc, t_yb[b]], writes=[t_acc])
                S.op("dve", lambda e: e.tensor_tensor(out=tmp[:], in0=xs_tok[:, c, :], in1=dbc[:].rearrange("p a d -> p (a d)"), op=ALU.mult),
                     reads=[t_xs, t_db], writes=[t_tmp])
                S.op("dve", lambda e: e.tensor_tensor(out=acc[:], in0=acc[:], in1=tmp[:], op=ALU.add), reads=[t_acc, t_tmp], writes=[t_acc])
                S.op("dve", lambda e: e.tensor_tensor(out=acc[:], in0=acc[:], in1=zs[:], op=ALU.mult), reads=[t_acc, t_zs], writes=[t_acc])
                for g in range(2):
                    S.op("act", lambda e: e.activation(out=tmp[:, g * 192:(g + 1) * 192], in_=acc[:, g * 192:(g + 1) * 192], func=AF.Square, accum_out=ssq[:, g:g + 1]),
                         reads=[t_acc], writes=[t_tmp, t_ssq])
                S.op("act", lambda e: e.activation(out=ssq[:, 2:4], in_=ssq[:, 0:2], func=AF.Sqrt, bias=G.epsc[:], scale=1.0 / 192), reads=[t_ssq, G.t_c], writes=[t_ssq])
                S.op("dve", lambda e: e.reciprocal(out=ssq[:, 2:4], in_=ssq[:, 2:4]), reads=[t_ssq], writes=[t_ssq])
                for g in range(2):
                    S.op("dve", lambda e: e.scalar_tensor_tensor(out=ob[:, g * 192:(g + 1) * 192], in0=acc[:, g * 192:(g + 1) * 192], scalar=ssq[:, 2 + g:3 + g],
                                                                 in1=nwb[:, g * 192:(g + 1) * 192], op0=ALU.mult, op1=ALU.mult), reads=[t_acc, t_ssq, t_db], writes=[t_ob])
                for c3 in range(3):
                    S.op("pe", lambda e: e.transpose(out=ptr[:, c3, :], in_=ob[:, c3 * 128:(c3 + 1) * 128], identity=G.identB), reads=[t_ob, G.t_c], writes=[t_ptr])
                S.op("act", lambda e: e.copy(out=oT[:, b], in_=ptr[:]), reads=[t_ptr], writes=[t_oT[b]])
                S.dma(G.mixT[384:768, tc0:tc0 + 128].rearrange("(c p) t -> p c t", p=128), oT[:, b], reads=[t_oT[b]], writes=[G.t_mix])
        if "ssd" in G.dbg and li == 0:
            dump_bf16(G, G.mixT[384:512, 256:768], G.dbg["ssd"], [G.t_mix])


HY0 = 1676
SEGS = {"lat": dict(L=4096, NC=32, KC=17, off=NCTX), "ctx": dict(L=256, NC=2, KC=2, off=0)}


def hyena_inproj(G, li):
    nc, S, I = G.nc, G.S, G.I
    hT = G.hT
    need_ctx = li < DEPTH - 1
    wv32 = I["w_in"][li].rearrange("(k p) c -> p k c", p=128)
    with ExitStack() as _es:
        wst = _es.enter_context(SB(nc, "wst", [128, 8, 128], F32))
        cwb = _es.enter_context(SB(nc, "cwb", [128, 3, 128], F32))
        wj = _es.enter_context(SB(nc, "wjh", [128, 3, 8, 768], BF16))
        hb = _es.enter_context(SB(nc, "hb", [128, 768], F32))
        hv = _es.enter_context(SB(nc, "hv", [128, 2, 768], BF16))
        ph = _es.enter_context(PS(nc, "ph", [128, 2, 512], F32))
        t_wst, t_cwb, t_wj, t_hb, t_hv, t_ph = Tok(), Tok(), Tok(), Tok(), [Tok(), Tok()], Tok()
        S.dma(hb[:], I["hy_conv_b"][li].partition_broadcast(128), writes=[t_hb])
        for cc in range(6):
            S.dma(wst[:], wv32[:, :, HY0 + cc * 128:HY0 + (cc + 1) * 128], writes=[t_wst])
            for j in range(3):
                S.dma(cwb[:, j, :], I["hy_conv_w"][li, j, cc * 128:(cc + 1) * 128].partition_broadcast(128), writes=[t_cwb])
            for j in range(3):
                S.op("dve", lambda e: e.tensor_tensor(out=wj[:, j, :, cc * 128:(cc + 1) * 128], in0=wst[:],
                                                      in1=cwb[:, j:j + 1, :].to_broadcast([128, 8, 128]), op=ALU.mult),
                     reads=[t_wst, t_cwb], writes=[t_wj])
        dv = G.hyv.rearrange("m t c -> t m c")
        for tl in range(NT):
            if tl < 2 and not need_ctx:
                continue
            col = colof(tl)
            b = tl % 2
            for half in range(2):
                for j in range(3):
                    for k in range(8):
                        S.op("pe", lambda e: e.matmul(ph[:, half, 0:384], lhsT=hT[:, k, col + j - 1:col + j - 1 + 128],
                                                      rhs=wj[:, j, k, half * 384:(half + 1) * 384], start=(j == 0 and k == 0), stop=(j == 2 and k == 7)),
                             reads=[t_wj, G.t_hT], writes=[t_ph])
            S.op("dve", lambda e: e.tensor_tensor(out=hv[:, b, :].rearrange("p (a c) -> p a c", a=2), in0=ph[:, :, 0:384],
                                                  in1=hb[:].rearrange("p (a c) -> p a c", a=2), op=ALU.add), reads=[t_ph, t_hb], writes=[t_hv[b]])
            S.dma(dv[tl * 128:(tl + 1) * 128], hv[:, b, :].rearrange("p (m c) -> p m c", m=3), reads=[t_hv[b]], writes=[G.t_hyv])


def hyena_fft(G, li):
    nc, S, I = G.nc, G.S, G.I
    need_ctx = li < DEPTH - 1
    for sname in (("lat", "ctx") if need_ctx else ("lat",)):
        P = SEGS[sname]
        L, NCk, KC, off = P["L"], P["NC"], P["KC"], P["off"]
        NH = NCk // 2
        FT, IT, FE, WIN, MH = I["ft_" + sname], I["it_" + sname], I["fe_" + sname], I["win_" + sname], I["mh_" + sname]
        Kf = G.Kf[sname]
        t_kf = Tok()
        with ExitStack() as _es:
            hk = _es.enter_context(SB(nc, "hk", [128, NCk, 1024], BF16))
            fe = _es.enter_context(SB(nc, "fe", [33, L], F32))
            h1 = _es.enter_context(SB(nc, "h1", [64, L], F32))
            h2 = _es.enter_context(SB(nc, "h2", [64, L], F32))
            w1 = _es.enter_context(SB(nc, "w1", [33, 64], F32))
            w2 = _es.enter_context(SB(nc, "w2", [64, 64], F32))
            w3 = _es.enter_context(SB(nc, "w3", [64, 1024], F32))
            pre = _es.enter_context(SB(nc, "pre", [64, 512], F32))
            pr2 = _es.enter_context(SB(nc, "pr2", [64, 512], F32))
            t_pr2 = Tok()
            MAGIC = 1.5 * 2 ** 23
            wint = _es.enter_context(SB(nc, "wint", [128, 2, 2, 256], F32))
            hbias = _es.enter_context(SB(nc, "hbias", [128, 512], F32))
            mh = _es.enter_context(SB(nc, "mh", [128, KC], F32))
            slab = _es.enter_context(SB(nc, "slab", [128, 2, NCk, 2, 128], BF16))
            xo = _es.enter_context(SB(nc, "xo", [128, 2, 512], F32))
            sd = _es.enter_context(SB(nc, "sd", [128, 2, 2, 2, 512], F32))
            kf = _es.enter_context(SB(nc, "kf", [128, 2, 2, 2, 256], BF16))
            pm = _es.enter_context(PS(nc, "pm", [64, 512], F32))
            phh = _es.enter_context(PS(nc, "phh", [128, 2, 512], F32))
            psk = _es.enter_context(PS(nc, "psk", [128, 2, 2, 512], F32))
            t_hk, t_fe, t_h1, t_h2, t_w, t_pre, t_win, t_slab, t_eo, t_sd, t_kft, t_pm, t_phh, t_psk = \
                Tok(), Tok(), Tok(), Tok(), Tok(), Tok(), [Tok(), Tok()], [Tok(), Tok()], Tok(), Tok(), Tok(), Tok(), Tok(), Tok()
            S.dma(fe[:], FE[:, :], writes=[t_fe])
            S.dma(w1[:], I["hy_w1"][li], writes=[t_w])
            S.dma(w2[:], I["hy_w2"][li], writes=[t_w])
            S.dma(w3[:], I["hy_w3"][li], writes=[t_w])
            S.dma(hbias[:], I["hy_bias"][li].rearrange("o c -> (o c)").partition_broadcast(128), writes=[t_w])
            S.dma(mh[:], MH[:, :], writes=[t_w])
            ob1, ofr, ob2 = COLS["hy_b1"][0], COLS["hy_freq"][0], COLS["hy_b2"][0]
            for (src, t_src, wt, kk, bcol, dst, t_dst) in ((fe, t_fe, w1, 33, ob1, h1, t_h1), (h1, t_h1, w2, 64, ob2, h2, t_h2)):
                for c0 in range(0, L, 512):
                    n = min(512, L - c0)
                    S.op("pe", lambda e: e.matmul(pm[:, 0:n], lhsT=wt[0:kk, :], rhs=src[0:kk, c0:c0 + n], start=True, stop=True),
                         reads=[t_w, t_src], writes=[t_pm])
                    S.op("dve", lambda e: e.tensor_scalar(out=pre[:, 0:n], in0=pm[:, 0:n], scalar1=G.cols[0:64, bcol:bcol + 1],
                                                          scalar2=G.cols[0:64, ofr:ofr + 1], op0=ALU.add, op1=ALU.mult),
                         reads=[t_pm, G.t_cols], writes=[t_pre])
                    S.op("dve", lambda e: e.tensor_scalar(out=pr2[:, 0:n], in0=pre[:, 0:n], scalar1=1.0 / (2.0 * math.pi), scalar2=MAGIC, op0=ALU.mult, op1=ALU.add),
                         reads=[t_pre], writes=[t_pr2])
                    S.op("dve", lambda e: e.tensor_scalar(out=pr2[:, 0:n], in0=pr2[:, 0:n], scalar1=-MAGIC, scalar2=None, op0=ALU.add),
                         reads=[t_pr2], writes=[t_pr2])
                    S.op("dve", lambda e: e.scalar_tensor_tensor(out=pre[:, 0:n], in0=pr2[:, 0:n], scalar=-2.0 * math.pi, in1=pre[:, 0:n], op0=ALU.mult, op1=ALU.add),
                         reads=[t_pr2, t_pre], writes=[t_pre])
                    S.op("act", lambda e: e.activation(out=dst[:, c0:c0 + n], in_=pre[:, 0:n], func=AF.Sin),
                         reads=[t_pre], writes=[t_dst])
            for c in range(NCk):
                b = c % 2
                S.dma(wint[:, b], WIN[c], writes=[t_win[b]])
                for half in range(2):
                    S.op("pe", lambda e: e.matmul(phh[:, half, :], lhsT=h2[:, c * 128:(c + 1) * 128], rhs=w3[:, half * 512:(half + 1) * 512], start=True, stop=True),
                         reads=[t_h2, t_w], writes=[t_phh])
                for dr in range(2):
                    S.op("dve", lambda e: e.tensor_tensor(out=hk[:, c, dr * 512:(dr + 1) * 512].rearrange("p (o c) -> p o c", o=2),
                                                          in0=phh[:, dr, :].rearrange("p (o c) -> p o c", o=2),
                                                          in1=wint[:, b, dr:dr + 1, :].to_broadcast([128, 2, 256]), op=ALU.mult),
                         reads=[t_phh, t_win[b]], writes=[t_hk])
            for kc in range(KC):
                b = kc % 2
                S.dma(slab[:, b], FT[kc], writes=[t_slab[b]])
                for dr in range(2):
                    for eo_ in range(2):
                        for ri in range(2):
                            for c in range(NH):
                                cc = eo_ * NH + c
                                S.op("pe", lambda e: e.matmul(psk[:, eo_, ri, :], lhsT=slab[:, b, cc, ri, :], rhs=hk[:, cc, dr * 512:(dr + 1) * 512],
                                                              start=(c == 0), stop=(c == NH - 1)), reads=[t_slab[b], t_hk], writes=[t_psk])
                    S.op("act", lambda e: e.copy(out=xo[:], in_=psk[:, 1]), reads=[t_psk], writes=[t_eo])
                    S.op("dve", lambda e: e.tensor_tensor(out=sd[:, dr, 0], in0=psk[:, 0], in1=xo[:], op=ALU.add), reads=[t_psk, t_eo], writes=[t_sd])
                    S.op("dve", lambda e: e.tensor_tensor(out=sd[:, dr, 1], in0=psk[:, 0], in1=xo[:], op=ALU.subtract), reads=[t_psk, t_eo], writes=[t_sd])
                v = lambda ap: ap.rearrange("p (o c) -> p o c", o=2)
                S.op("dve", lambda e: e.tensor_tensor(out=sd[:, 0, 0, 0, :], in0=sd[:, 0, 0, 0, :], in1=hbias[:], op=ALU.add), reads=[t_sd, t_w], writes=[t_sd])
                S.op("dve", lambda e: e.tensor_tensor(out=sd[:, 0, 1, 0, :], in0=sd[:, 0, 1, 0, :], in1=hbias[:], op=ALU.add), reads=[t_sd, t_w], writes=[t_sd])
                S.op("dve", lambda e: e.tensor_tensor(out=kf[:, :, 0, 0, :], in0=v(sd[:, 0, 0, 0, :]), in1=v(sd[:, 1, 0, 0, :]), op=ALU.add), reads=[t_sd], writes=[t_kft])
                S.op("dve", lambda e: e.tensor_tensor(out=kf[:, :, 0, 1, :], in0=v(sd[:, 0, 0, 1, :]), in1=v(sd[:, 1, 0, 1, :]), op=ALU.subtract), reads=[t_sd], writes=[t_kft])
                S.op("dve", lambda e: e.tensor_tensor(out=v(xo[:, 0, :]), in0=v(sd[:, 0, 1, 0, :]), in1=v(sd[:, 1, 1, 0, :]), op=ALU.add), reads=[t_sd], writes=[t_eo])
                S.op("dve", lambda e: e.tensor_tensor(out=v(xo[:, 1, :]), in0=v(sd[:, 1, 1, 1, :]), in1=v(sd[:, 0, 1, 1, :]), op=ALU.subtract), reads=[t_sd], writes=[t_eo])
                S.op("dve", lambda e: e.tensor_scalar(out=kf[:, :, 1, 0, :], in0=v(xo[:, 0, :]), scalar1=mh[:, kc:kc + 1], scalar2=None, op0=ALU.mult),
                     reads=[t_eo, t_w], writes=[t_kft])
                S.op("dve", lambda e: e.tensor_scalar(out=kf[:, :, 1, 1, :], in0=v(xo[:, 1, :]), scalar1=mh[:, kc:kc + 1], scalar2=None, op0=ALU.mult),
                     reads=[t_eo, t_w], writes=[t_kft])
                S.dma(Kf[:, kc].rearrange("o p l r c -> p o l r c"), kf[:], reads=[t_kft], writes=[t_kf])
            S.barrier()
        with ExitStack() as _es:
            vt = _es.enter_context(SB(nc, "vt", [128, NCk, 256], BF16))
            zz1 = _es.enter_context(SB(nc, "zz1", [128, NCk, 256], BF16))
            Y = _es.enter_context(SB(nc, "Y", [128, 2, KC, 2, 256], BF16))
            fsl = _es.enter_context(SB(nc, "fsl", [128, 2, NCk, 2, 128], BF16))
            isl = _es.enter_context(SB(nc, "isl", [128, 2, KC, 2, 128], BF16))
            kft = _es.enter_context(SB(nc, "kft", [128, 2, 2, 2, 256], BF16))
            xo = _es.enter_context(SB(nc, "xo", [128, 2, 256], F32))
            xs_ = _es.enter_context(SB(nc, "xs_", [128, 2, 2, 256], F32))
            ta = _es.enter_context(SB(nc, "ta", [128, 4, 256], F32))
            yl = _es.enter_context(SB(nc, "yl", [128, 2, 2, 256], F32))
            xg = _es.enter_context(SB(nc, "xg", [128, 2, 256], BF16))
            zt = _es.enter_context(SB(nc, "zt", [128, 256], BF16))
            zT = _es.enter_context(SB(nc, "zT", [128, 2, 2, 256], BF16))
            psx = _es.enter_context(PS(nc, "psx", [128, 2, 4, 256], F32))
            psy = _es.enter_context(PS(nc, "psy", [128, 2, 512], F32))
            ptr = _es.enter_context(PS(nc, "ptr", [128, 2, 128], BF16))
            t_vt, t_zz1, t_Y, t_fsl, t_isl, t_kft2, t_ta, t_xg, t_zt, t_zT, t_psx, t_psy, t_ptr, t_xo, t_xs, t_yl = \
                Tok(), Tok(), Tok(), [Tok(), Tok()], [Tok(), Tok()], [Tok(), Tok()], Tok(), [Tok(), Tok()], Tok(), [Tok(), Tok()], [Tok(), Tok()], [Tok(), Tok()], Tok(), Tok(), Tok(), Tok()
            hsrc = lambda m: G.hyv[m, off:off + L, :].rearrange("(c p two) ch -> two p c ch", p=128, two=2)
            for par in range(2):
                S.dma(vt[:, par * NH:(par + 1) * NH, :], hsrc(0)[par], reads=[G.t_hyv], writes=[t_vt])
            mo = G.mixT[768:1024, :].rearrange("(c p) t -> p c t", p=128)
            for order in range(2):
                src, t_src = (vt, t_vt) if order == 0 else (zz1, t_zz1)
                for kc in range(KC):
                    b = kc % 2
                    S.dma(fsl[:, b], FT[kc], writes=[t_fsl[b]])
                    S.dma(kft[:, b], Kf[order, kc], reads=[t_kf], writes=[t_kft2[b]])
                    for eo_ in range(2):
                        for ri in range(2):
                            for c in range(NH):
                                cc = eo_ * NH + c
                                S.op("pe", lambda e: e.matmul(psx[:, b, eo_ * 2 + ri, :], lhsT=fsl[:, b, cc, ri, :], rhs=src[:, cc, :], start=(c == 0), stop=(c == NH - 1)),
                                     reads=[t_fsl[b], t_src], writes=[t_psx[b]])
                    S.op("act", lambda e: e.copy(out=xo[:], in_=psx[:, b, 2:4, :]), reads=[t_psx[b]], writes=[t_xo])
                    S.op("dve", lambda e: e.tensor_tensor(out=xs_[:, 0], in0=psx[:, b, 0:2, :], in1=xo[:], op=ALU.add), reads=[t_psx[b], t_xo], writes=[t_xs])
                    S.op("dve", lambda e: e.tensor_tensor(out=xs_[:, 1, 0, :], in0=psx[:, b, 0, :], in1=xo[:, 0, :], op=ALU.subtract), reads=[t_psx[b], t_xo], writes=[t_xs])
                    S.op("dve", lambda e: e.scalar_tensor_tensor(out=xs_[:, 1, 1, :], in0=psx[:, b, 1, :], scalar=-1.0, in1=xo[:, 1, :], op0=ALU.mult, op1=ALU.add),
                         reads=[t_psx[b], t_xo], writes=[t_xs])
                    S.op("dve", lambda e: e.tensor_tensor(out=ta[:, 0:2, :], in0=xs_[:, :, 0, :], in1=kft[:, b, :, 0, :], op=ALU.mult), reads=[t_xs, t_kft2[b]], writes=[t_ta])
                    S.op("pool", lambda e: e.tensor_tensor(out=ta[:, 2:4, :], in0=xs_[:, :, 1, :], in1=kft[:, b, :, 1, :], op=ALU.mult), reads=[t_xs, t_kft2[b]], writes=[t_ta])
                    S.op("dve", lambda e: e.tensor_tensor(out=yl[:, :, 0, :], in0=ta[:, 0:2, :], in1=ta[:, 2:4, :], op=ALU.subtract), reads=[t_ta], writes=[t_yl])
                    S.op("dve", lambda e: e.tensor_tensor(out=ta[:, 0:2, :], in0=xs_[:, :, 0, :], in1=kft[:, b, :, 1, :], op=ALU.mult), reads=[t_xs, t_kft2[b], t_yl], writes=[t_ta])
                    S.op("pool", lambda e: e.tensor_tensor(out=ta[:, 2:4, :], in0=xs_[:, :, 1, :], in1=kft[:, b, :, 0, :], op=ALU.mult), reads=[t_xs, t_kft2[b], t_yl], writes=[t_ta])
                    S.op("dve", lambda e: e.tensor_tensor(out=yl[:, :, 1, :], in0=ta[:, 0:2, :], in1=ta[:, 2:4, :], op=ALU.add), reads=[t_ta], writes=[t_yl])
                    S.op("dve", lambda e: e.tensor_tensor(out=Y[:, 0, kc, 0, :], in0=yl[:, 0, 0, :], in1=yl[:, 1, 0, :], op=ALU.add), reads=[t_yl], writes=[t_Y])
                    S.op("pool", lambda e: e.tensor_tensor(out=Y[:, 0, kc, 1, :], in0=yl[:, 0, 1, :], in1=yl[:, 1, 1, :], op=ALU.subtract), reads=[t_yl], writes=[t_Y])
                    S.op("dve", lambda e: e.tensor_tensor(out=Y[:, 1, kc, 0, :], in0=yl[:, 0, 0, :], in1=yl[:, 1, 0, :], op=ALU.subtract), reads=[t_yl], writes=[t_Y])
                    S.op("pool", lambda e: e.tensor_tensor(out=Y[:, 1, kc, 1, :], in0=yl[:, 0, 1, :], in1=yl[:, 1, 1, :], op=ALU.add), reads=[t_yl], writes=[t_Y])
                oi = 0
                for c2 in range(NH):
                    for par in range(2):
                        cc = par * NH + c2
                        b = oi % 2
                        oi += 1
                        zb = c2 % 2
                        S.dma(isl[:, b], IT[cc], writes=[t_isl[b]])
                        S.dma(xg[:, b], hsrc(1 + order)[par, :, c2, :], reads=[G.t_hyv], writes=[t_xg[b]])
                        for kc in range(KC):
                            for ri in range(2):
                                S.op("pe", lambda e: e.matmul(psy[:, b, 0:256], lhsT=isl[:, b, kc, ri, :], rhs=Y[:, par, kc, ri, :],
                                                              start=(kc == 0 and ri == 0), stop=(kc == KC - 1 and ri == 1)), reads=[t_isl[b], t_Y], writes=[t_psy[b]])
                        if order == 0:
                            S.op("dve", lambda e: e.tensor_tensor(out=zz1[:, cc, :], in0=psy[:, b, 0:256], in1=xg[:, b, :], op=ALU.mult),
                                 reads=[t_psy[b], t_xg[b]], writes=[t_zz1])
                        else:
                            S.op("dve", lambda e: e.tensor_tensor(out=zt[:], in0=psy[:, b, 0:256], in1=xg[:, b, :], op=ALU.mult),
                                 reads=[t_psy[b], t_xg[b]], writes=[t_zt])
                            for hh in range(2):
                                S.op("pe", lambda e: e.transpose(out=ptr[:, hh, :], in_=zt[:, hh * 128:(hh + 1) * 128], identity=G.identB),
                                     reads=[t_zt, G.t_c], writes=[t_ptr])
                            S.op("act", lambda e: e.copy(out=zT[:, zb].rearrange("p h (t two) -> p h t two", two=2)[:, :, :, par], in_=ptr[:]),
                                 reads=[t_ptr], writes=[t_zT[zb]])
                            if par == 1:
                                S.dma(mo[:, :, off + c2 * 256:off + (c2 + 1) * 256], zT[:, zb], reads=[t_zT[zb]], writes=[G.t_mix])
            S.barrier()
    if "hy" in G.dbg and li == 0:
        dump_bf16(G, G.mixT[768:896, 256:768], G.dbg["hy"], [G.t_mix])


def _hy_tables(L):
    N = 2 * L
    NCk = L // 128
    KC = (L // 2 + 1 + 127) // 128
    perm = np.concatenate([np.arange(0, L, 2), np.arange(1, L, 2)])
    n = perm.astype(np.int64)
    k = np.arange(KC * 128, dtype=np.int64)
    ang = ((n[:, None] * k[None, :]) % N).astype(np.float64) * (2 * np.pi / N)
    valid = (k <= L // 2).astype(np.float64)
    w = np.where(k == 0, 1.0, 2.0) / N * valid
    c, s_ = np.cos(ang), np.sin(ang)
    ft = np.stack([c * valid, -s_ * valid], axis=0)
    ft = ft.reshape(2, NCk, 128, KC, 128).transpose(3, 2, 1, 0, 4)
    it = np.stack([c * w, -s_ * w], axis=0)
    it = it.reshape(2, NCk, 128, KC, 128).transpose(1, 4, 3, 0, 2)
    f = np.float32
    nn = np.arange(L, dtype=f)
    t = nn / f(max(L - 1, 1))
    bands = np.linspace(1e-4, 15, 16, dtype=f)
    wpos = (f(2 * math.pi / L) * nn).astype(f)
    feats = np.concatenate([t[:, None], np.cos(wpos[:, None] * bands), -np.sin(wpos[:, None] * bands)], axis=-1).astype(f)
    deltas = np.abs(np.linspace(math.log(1e-2) / 1.5, math.log(1e-2) / 0.3, 256, dtype=f))
    win = np.exp(-t[:, None] * deltas).astype(f)
    winb = win.copy()
    winb[0] = 0.0
    feats = feats[perm]
    wn = np.stack([win, winb], axis=1)[perm].reshape(NCk, 128, 2, 256)
    mh = ((k != L // 2) & (k <= L // 2)).astype(f).reshape(KC, 128).T
    bf = ml_dtypes.bfloat16
    return (np.ascontiguousarray(ft).astype(bf), np.ascontiguousarray(it).astype(bf),
            np.ascontiguousarray(feats.T), np.ascontiguousarray(wn), np.ascontiguousarray(mh))
```

```python
import math
from contextlib import ExitStack
import numpy as np
import ml_dtypes
import concourse.bass as bass
import concourse.mybir as mybir
from concourse.bass_utils import run_bass_kernel_spmd

F32 = mybir.dt.float32
BF16 = mybir.dt.bfloat16
AF = mybir.ActivationFunctionType
ALU = mybir.AluOpType
AX = mybir.AxisListType

D = 1024
NCTX = 256
NLAT = 4096
NTOK = NCTX + NLAT
NT = NTOK // 128
DEPTH = 2
D_IN = 2444
EPS = 1e-6
HC = NTOK + 3
NE = 16
DFF = 256
DEBUG = False


def colof(tile):
    return 1 + 128 * tile if tile < 2 else 258 + 128 * (tile - 2)


BLOCKS = [(1, 0, 256, 0, 2)] + [(258 + 512 * j, 256 + 512 * j, 512, 2 + 4 * j, 4) for j in range(8)]


class Tok:
    __slots__ = ("w", "r")

    def __init__(self):
        self.w = None
        self.r = {}


class Sched:
    def __init__(self, nc, ndma=8, same_engine_sync=True):
        self.nc = nc
        self.eng = {"pe": nc.tensor, "act": nc.scalar, "dve": nc.vector, "pool": nc.gpsimd, "sp": nc.sync}
        self.semh = {}
        self.cnt = {}
        self.seen = {k: {} for k in self.eng}
        self.same = same_engine_sync
        for k in self.eng:
            self.semh[k] = nc.alloc_semaphore("s_" + k)
            self.cnt[k] = 0
        self.ndma = ndma
        self.dslot = {}
        self.dval = {}
        for q in ("sp", "pool"):
            self.dslot[q] = 0
            for i in range(ndma):
                key = ("dma", q, i)
                self.semh[key] = nc.alloc_semaphore("d_%s_%d" % (q, i))
                self.dval[key] = 0
        self.ninst = 0

    def _wait(self, e, deps):
        for (k, v) in sorted(deps, key=str):
            if k == e and (e == "pe" or not self.same):
                continue
            if self.seen[e].get(k, 0) >= v:
                continue
            self.eng[e].wait_ge(self.semh[k], v)
            self.seen[e][k] = v

    @staticmethod
    def _deps(reads, writes):
        deps = set()
        for t in reads:
            if t.w is not None:
                deps.add(t.w)
        for t in writes:
            if t.w is not None:
                deps.add(t.w)
            for kv in t.r.items():
                deps.add(kv)
        return deps

    @staticmethod
    def _mark(ev, reads, writes):
        k, v = ev
        for t in reads:
            if t.r.get(k, 0) < v:
                t.r[k] = v
        for t in writes:
            t.w = ev
            t.r = {}

    def op(self, e, fn, reads=(), writes=()):
        self._wait(e, self._deps(reads, writes))
        ins = fn(self.eng[e])
        self.cnt[e] += 1
        ins.then_inc(self.semh[e], 1)
        self._mark((e, self.cnt[e]), reads, writes)
        self.ninst += 1
        return ins

    def dma(self, out, in_, reads=(), writes=(), q="sp", **kw):
        i = self.dslot[q]
        self.dslot[q] = (i + 1) % self.ndma
        key = ("dma", q, i)
        deps = self._deps(reads, writes)
        if self.dval[key] > 0:
            deps.add((key, self.dval[key]))
        self._wait(q, deps)
        ins = self.eng[q].dma_start(out=out, in_=in_, **kw)
        self.dval[key] += 16
        ins.then_inc(self.semh[key], 16)
        self._mark((key, self.dval[key]), reads, writes)
        self.ninst += 1
        return ins

    def barrier(self):
        deps = set()
        for key, v in self.dval.items():
            if v > 0:
                deps.add((key, v))
        for k in self.eng:
            if self.cnt[k] > 0:
                deps.add((k, self.cnt[k]))
        for e in self.eng:
            self._wait(e, deps)


class Ctx:
    pass


_UID = [0]


def SB(nc, name, shape, dt):
    _UID[0] += 1
    return nc.sbuf_tensor("%s_%d" % (name, _UID[0]), shape, dt)


def PS(nc, name, shape, dt):
    _UID[0] += 1
    return nc.psum_tensor("%s_%d" % (name, _UID[0]), shape, dt)


def build(dbg=None):
    nc = bass.Bass("TRN2", target_bir_lowering=False)
    S = Sched(nc)
    G = Ctx()
    G.nc, G.S = nc, S

    def din(name, shape, dt=F32):
        return nc.dram_tensor(name, list(shape), dt, kind="ExternalInput").ap()

    def dscr(name, shape, dt):
        return nc.dram_tensor(name, list(shape), dt, kind="Internal").ap()

    I = {}
    I["x"] = din("x", [NLAT, D])
    I["ctx"] = din("ctx", [NCTX, D])
    I["w_mod"] = din("w_mod", [DEPTH, D, 6 * D])
    I["b_mod"] = din("b_mod", [DEPTH, 6 * D])
    I["w_in"] = din("w_in", [DEPTH, D, D_IN])
    I["w_out"] = din("w_out", [DEPTH, D, D])
    I["w_router"] = din("w_router", [D, NE])
    I["router_bias"] = din("router_bias", [NE])
    I["w_gate"] = din("w_gate", [DEPTH, NE, D, DFF])
    I["w_up"] = din("w_up", [DEPTH, NE, D, DFF])
    I["w_down"] = din("w_down", [DEPTH, NE, DFF, D])
    I["g_final"] = din("g_final", [D])
    I["cols"] = din("cols", [DEPTH, 128, NCOLS])
    I["cmat"] = din("cmat", [9, 128, 128])
    I["rope"] = din("rope", [2, 128, NLAT])
    for sname, P in SEGS.items():
        I["ft_" + sname] = din("ft_" + sname, [P["KC"], 128, P["NC"], 2, 128], BF16)
        I["it_" + sname] = din("it_" + sname, [P["NC"], 128, P["KC"], 2, 128], BF16)
        I["fe_" + sname] = din("fe_" + sname, [33, P["L"]])
        I["win_" + sname] = din("win_" + sname, [P["NC"], 128, 2, 256])
        I["mh_" + sname] = din("mh_" + sname, [128, P["KC"]])
    for nm, shp in (("hy_conv_w", [DEPTH, 3, 768]), ("hy_conv_b", [DEPTH, 768]), ("hy_w1", [DEPTH, 33, 64]), ("hy_w2", [DEPTH, 64, 64]),
                    ("hy_w3", [DEPTH, 64, 1024]), ("hy_bias", [DEPTH, 2, 256])):
        I[nm] = din(nm, shp)
    I["ssdmask"] = din("ssdmask", [2, 4, 128, 512], BF16)
    for nm, shp in (("ssd_conv_w", [DEPTH, 3, 640]), ("ssd_dt_bias", [DEPTH, 2, 6]), ("ssd_a_log", [DEPTH, 2, 6]),
                    ("ssd_d", [DEPTH, 6]), ("ssd_norm", [DEPTH, 384])):
        I[nm] = din(nm, shp)
    out = nc.dram_tensor("out", [NLAT, D], F32, kind="ExternalOutput").ap()
    G.I, G.out = I, out
    G.dbg = {}
    if dbg:
        for name, shape in dbg.items():
            G.dbg[name] = nc.dram_tensor("dbg_" + name, list(shape), F32, kind="ExternalOutput").ap()

    G.xres = dscr("xres", [NTOK, D], F32)
    G.t_xres = [Tok() for _ in range(NT)]
    G.wb_in = [dscr("wb_in%d" % i, [D, D_IN], BF16) for i in range(DEPTH)]
    G.wb_out = [dscr("wb_out%d" % i, [D, D], BF16) for i in range(DEPTH)]
    G.wb_gate = [dscr("wb_gate%d" % i, [NE, D, DFF], BF16) for i in range(DEPTH)]
    G.wb_up = [dscr("wb_up%d" % i, [NE, D, DFF], BF16) for i in range(DEPTH)]
    G.wb_down = [dscr("wb_down%d" % i, [NE, DFF, D], BF16) for i in range(DEPTH)]
    G.t_wb = Tok()
    G.mixT = dscr("mixT", [D, NTOK], BF16)
    G.hyv = dscr("hyv", [3, NTOK, 256], BF16)
    G.ssd_yb = dscr("ssd_yb", [NTOK, 384], F32)
    G.t_ssdyb = [Tok() for _ in range(NT)]
    G.t_hyv = Tok()
    G.Kf = {sn: dscr("Kf_" + sn, [2, P["KC"], 128, 2, 2, 256], BF16) for sn, P in SEGS.items()}
    G.t_mix = Tok()

    cm = nc.alloc_sbuf_tensor("cm", [128, 9, 128], F32)
    cmb = nc.alloc_sbuf_tensor("cmb", [128, 9, 128], BF16)
    ones = nc.alloc_sbuf_tensor("ones", [128, 128], F32)
    epsc = nc.alloc_sbuf_tensor("epsc", [128, 1], F32)
    G.t_c = Tok()
    S.dma(cm[:], I["cmat"].rearrange("a p c -> p a c"), writes=[G.t_c])
    S.op("dve", lambda e: e.tensor_copy(out=cmb[:], in_=cm[:]), reads=[G.t_c], writes=[G.t_c])
    S.op("dve", lambda e: e.memset(ones[:], 1.0), writes=[G.t_c])
    S.op("dve", lambda e: e.memset(epsc[:], EPS), writes=[G.t_c])
    G.cm, G.cmb, G.ones, G.epsc = cm, cmb, ones, epsc
    G.negpi = nc.alloc_sbuf_tensor("negpi", [128, 1], F32)
    S.op("dve", lambda e: e.memset(G.negpi[:], -math.pi), writes=[G.t_c])
    G.identF, G.identB = cm[:, 0, :], cmb[:, 0, :]

    S.dma(G.xres[0:NCTX, :], I["ctx"][:, :], writes=G.t_xres[0:2])
    for j in range(4):
        S.dma(G.xres[NCTX + 1024 * j:NCTX + 1024 * (j + 1), :], I["x"][1024 * j:1024 * (j + 1), :],
              writes=G.t_xres[2 + 8 * j:2 + 8 * (j + 1)])

    convert_weights(G)
    S.barrier()
    for li in range(1 if DEBUG else DEPTH):
        layer(G, li)
    S.barrier()
    return nc


def convert_weights(G):
    nc, S, I = G.nc, G.S, G.I
    CH = 4096
    with ExitStack() as _es:
        cf = _es.enter_context(SB(nc, "cv_f", [128, 2, CH], F32))
        cb = _es.enter_context(SB(nc, "cv_b", [128, 2, CH], BF16))
        tf = [Tok(), Tok()]
        tb = [Tok(), Tok()]
        n = 0
        engs = ["dve", "pool", "act"]
        for li in range(DEPTH):
            pairs = [(I["w_in"][li], G.wb_in[li], "a b -> (a b)"), (I["w_out"][li], G.wb_out[li], "a b -> (a b)"),
                     (I["w_gate"][li], G.wb_gate[li], "e a b -> (e a b)"), (I["w_up"][li], G.wb_up[li], "e a b -> (e a b)"),
                     (I["w_down"][li], G.wb_down[li], "e a b -> (e a b)")]
            for src, dst, pat in pairs:
                s1 = src.rearrange(pat).rearrange("(p m) -> p m", p=128)
                d1 = dst.rearrange(pat).rearrange("(p m) -> p m", p=128)
                M = s1.shape[1]
                for c0 in range(0, M, CH):
                    w = min(CH, M - c0)
                    k = n % 2
                    S.dma(cf[:, k, 0:w], s1[:, c0:c0 + w], writes=[tf[k]])
                    en = engs[n % 3]
                    if en == "act":
                        S.op("act", lambda e: e.copy(out=cb[:, k, 0:w], in_=cf[:, k, 0:w]), reads=[tf[k]], writes=[tb[k]])
                    else:
                        S.op(en, lambda e: e.tensor_copy(out=cb[:, k, 0:w], in_=cf[:, k, 0:w]), reads=[tf[k]], writes=[tb[k]])
                    S.dma(d1[:, c0:c0 + w], cb[:, k, 0:w], reads=[tb[k]], writes=[G.t_wb], q="pool")
                    n += 1


COLS = {}
_o = 0
for _name, _n in [("cc", 16), ("bmod", 32), ("g_mix", 8), ("g_ffn", 8), ("ssd_conv_b", 5), ("qg", 1), ("kg", 1),
                  ("ssd_d", 3), ("ssd_norm", 3), ("hy_b1", 1), ("hy_freq", 1), ("hy_b2", 1)]:
    COLS[_name] = (_o, _n)
    _o += _n
NCOLS = _o


def layer(G, li):
    nc, S, I = G.nc, G.S, G.I
    with ExitStack() as _es:
        cols = _es.enter_context(SB(nc, "cols", [128, NCOLS], F32))
        modc = _es.enter_context(SB(nc, "modc", [128, 4, 8, 2], F32))
        gtb = _es.enter_context(SB(nc, "gtb", [128, 2, 2, D], F32))
        G.cols, G.modc, G.gtb = cols, modc, gtb
        G.t_cols, G.t_modc, G.t_gtb = Tok(), Tok(), Tok()
        S.dma(cols[:], I["cols"][li], writes=[G.t_cols])
        adaln(G, li)
        S.barrier()
        with ExitStack() as _es:
            hT = _es.enter_context(SB(nc, "hT", [128, 8, HC], BF16))
            G.hT, G.t_hT = hT, Tok()
            norm_in(G, li)
            S.barrier()
            if "hy" in STAGES:
                hyena_inproj(G, li)
                S.barrier()
            if "att" in STAGES:
                attention(G, li)
                S.barrier()
            if "ssd" in STAGES:
                ssd(G, li)
                S.barrier()
        if "hy" in STAGES:
            hyena_fft(G, li)
            S.barrier()
        if "moe" in STAGES:
            with ExitStack() as _es:
                h2T = _es.enter_context(SB(nc, "h2T", [128, 8, NTOK], BF16))
                rl = _es.enter_context(SB(nc, "rl", [128, NT, NE], F32))
                G.h2T, G.t_h2T, G.rl, G.t_rl = h2T, Tok(), rl, Tok()
                outproj(G, li)
                S.barrier()
                moe(G, li)
                S.barrier()


STAGES = ("att", "ssd", "hy", "moe")


def colap(G, name, j=0, n=1, p0=0, p1=128):
    o, _ = COLS[name]
    return G.cols[p0:p1, o + j:o + j + n]


def adaln(G, li):
    nc, S, I = G.nc, G.S, G.I
    cols, modc, gtb = G.cols, G.modc, G.gtb
    with ExitStack() as _es:
        sc = _es.enter_context(SB(nc, "sc", [128, 8, 2], F32))
        screp = _es.enter_context(SB(nc, "screp", [128, 8, 2, 128], F32))
        wm = _es.enter_context(SB(nc, "wm", [128, 2, 8, 512], F32))
        brow = _es.enter_context(SB(nc, "brow", [128, 2, D], F32))
        ps_a = _es.enter_context(PS(nc, "ps_a", [128, 4, 2], F32))
        ps_g = _es.enter_context(PS(nc, "ps_g", [128, 2, 512], F32))
        t_sc, t_wm, t_pa, t_pg, t_brow = Tok(), [Tok(), Tok()], Tok(), Tok(), Tok()
        o = COLS["cc"][0]
        S.op("act", lambda e: e.activation(out=sc[:].rearrange("p k j -> p (k j)"), in_=cols[:, o:o + 16], func=AF.Silu),
             reads=[G.t_cols], writes=[t_sc])
        S.op("dve", lambda e: e.tensor_copy(out=screp[:].rearrange("p k j c -> p (k j) c"),
                                            in_=sc[:].rearrange("p k j -> p (k j)").unsqueeze(2).to_broadcast([128, 16, 128])),
             reads=[t_sc], writes=[t_sc])
        for g in range(2):
            S.dma(brow[:, g, :], I["b_mod"][li, (2 + 3 * g) * D:(3 + 3 * g) * D].partition_broadcast(128), writes=[t_brow])
        wv = I["w_mod"][li].rearrange("(k p) c -> p k c", p=128)
        ob = COLS["bmod"][0]
        for cj in range(12):
            b = cj % 2
            S.dma(wm[:, b], wv[:, :, cj * 512:(cj + 1) * 512], writes=[t_wm[b]])
            vec = cj // 2
            half = cj % 2
            if vec in (2, 5):
                g = 0 if vec == 2 else 1
                for j in range(2):
                    for kd in range(8):
                        S.op("pe", lambda e: e.matmul(ps_g[:, j, :], lhsT=screp[:, kd, j, :], rhs=wm[:, b, kd, :],
                                                      start=(kd == 0), stop=(kd == 7)), reads=[t_sc, t_wm[b]], writes=[t_pg])
                    S.op("dve", lambda e: e.tensor_tensor(out=gtb[:, g, j, half * 512:(half + 1) * 512], in0=ps_g[:, j, :],
                                                          in1=brow[:, g, half * 512:(half + 1) * 512], op=ALU.add),
                         reads=[t_pg, t_brow], writes=[G.t_gtb])
            else:
                v = {0: 0, 1: 1, 3: 2, 4: 3}[vec]
                for fc in range(4):
                    for kd in range(8):
                        S.op("pe", lambda e: e.matmul(ps_a[:, fc, :], lhsT=wm[:, b, kd, fc * 128:(fc + 1) * 128], rhs=sc[:, kd, :],
                                                      start=(kd == 0), stop=(kd == 7)), reads=[t_sc, t_wm[b]], writes=[t_pa])
                k0 = half * 4
                S.op("dve", lambda e: e.tensor_tensor(out=modc[:, v, k0:k0 + 4, :], in0=ps_a[:],
                                                      in1=cols[:, ob + v * 8 + k0:ob + v * 8 + k0 + 4].unsqueeze(2).to_broadcast([128, 4, 2]),
                                                      op=ALU.add), reads=[t_pa, G.t_cols], writes=[G.t_modc])
        for v, gname in ((1, "g_mix"), (3, "g_ffn")):
            og = COLS[gname][0]
            S.op("dve", lambda e: e.scalar_tensor_tensor(out=modc[:, v], in0=modc[:, v], scalar=1.0,
                                                         in1=cols[:, og:og + 8].unsqueeze(2).to_broadcast([128, 8, 2]),
                                                         op0=ALU.add, op1=ALU.mult), reads=[G.t_modc, G.t_cols], writes=[G.t_modc])


def rms_to_T(G, xt, t_x, tile, vA, vB, dstT, t_dst, dcol, pool):
    nc, S = G.nc, G.S
    sq, ss, xn, ps_t, toks = pool
    t_sq, t_ss, t_xn, t_ps = toks
    j = 1 if tile < 2 else 0
    S.op("act", lambda e: e.activation(out=sq[:], in_=xt, func=AF.Square, accum_out=ss[:, 0:1]), reads=[t_x], writes=[t_sq, t_ss])
    S.op("act", lambda e: e.activation(out=ss[:, 1:2], in_=ss[:, 0:1], func=AF.Sqrt, bias=G.epsc[:], scale=1.0 / D),
         reads=[t_ss, G.t_c], writes=[t_ss])
    S.op("dve", lambda e: e.reciprocal(out=ss[:, 2:3], in_=ss[:, 1:2]), reads=[t_ss], writes=[t_ss])
    S.op("dve", lambda e: e.tensor_scalar(out=xn[:], in0=xt, scalar1=ss[:, 2:3], scalar2=None, op0=ALU.mult),
         reads=[t_x, t_ss], writes=[t_xn])
    for k in range(8):
        S.op("pe", lambda e: e.transpose(out=ps_t[:, k, :], in_=xn[:, k * 128:(k + 1) * 128], identity=G.identB),
             reads=[t_xn, G.t_c], writes=[t_ps])
    S.op("dve", lambda e: e.tensor_tensor(out=sq[:].rearrange("p (k c) -> p k c", k=8), in0=ps_t[:],
                                          in1=G.modc[:, vA, :, j:j + 1].to_broadcast([128, 8, 128]), op=ALU.mult),
         reads=[t_ps, G.t_modc], writes=[t_sq])
    S.op("dve", lambda e: e.tensor_tensor(out=dstT[:, :, dcol:dcol + 128], in0=sq[:].rearrange("p (k c) -> p k c", k=8),
                                          in1=G.modc[:, vB, :, j:j + 1].to_broadcast([128, 8, 128]), op=ALU.add),
         reads=[t_sq, G.t_modc], writes=[t_dst])


def norm_in(G, li):
    nc, S = G.nc, G.S
    hT = G.hT
    with ExitStack() as _es:
        xt = _es.enter_context(SB(nc, "xt", [128, 2, D], F32))
        sq = _es.enter_context(SB(nc, "sq", [128, D], F32))
        ss = _es.enter_context(SB(nc, "ss", [128, 4], F32))
        xn = _es.enter_context(SB(nc, "xn", [128, D], BF16))
        ps_t = _es.enter_context(PS(nc, "ps_t", [128, 8, 128], BF16))
        t_x = [Tok(), Tok()]
        pool = (sq, ss, xn, ps_t, (Tok(), Tok(), Tok(), Tok()))
        for c in (0, 257, HC - 1):
            S.op("pool", lambda e: e.memset(hT[:, :, c:c + 1], 0.0), writes=[G.t_hT])
        for tile in range(NT):
            b = tile % 2
            S.dma(xt[:, b, :], G.xres[tile * 128:(tile + 1) * 128, :], reads=[G.t_xres[tile]], writes=[t_x[b]])
            rms_to_T(G, xt[:, b, :], t_x[b], tile, 1, 0, hT, G.t_hT, colof(tile), pool)
        if "hT" in G.dbg and li == 0:
            with ExitStack() as _es:
                dh = _es.enter_context(SB(nc, "dbgh", [128, 8, 512], F32))
                t = Tok()
                S.op("dve", lambda e: e.tensor_copy(out=dh[:], in_=hT[:, :, 0:512]), reads=[G.t_hT], writes=[t])
                S.dma(G.dbg["hT"].rearrange("(k p) c -> p k c", p=128), dh[:], reads=[t])


def attention(G, li):
    nc, S, I = G.nc, G.S, G.I
    hT = G.hT
    need_ctx = li < DEPTH - 1
    wv = G.wb_in[li].rearrange("(k p) c -> p k c", p=128)
    scale = 64 ** -0.5
    with ExitStack() as _es:
        wq = _es.enter_context(SB(nc, "wqkv", [128, 8, 640], BF16))
        qT = _es.enter_context(SB(nc, "qT", [128, 6, NTOK], BF16))
        kT = _es.enter_context(SB(nc, "kT", [128, NTOK], BF16))
        vp = _es.enter_context(SB(nc, "vp", [128, NT, 2, 128], BF16))
        rp = _es.enter_context(SB(nc, "rp", [128, 2, 2, 512], F32))
        qs = _es.enter_context(SB(nc, "qs", [128, 512], F32))
        q2 = _es.enter_context(SB(nc, "q2", [128, 512], F32))
        qn = _es.enter_context(SB(nc, "qn", [128, 512], F32))
        qnb = _es.enter_context(SB(nc, "qnb", [128, 512], BF16))
        pT = _es.enter_context(SB(nc, "pT", [128, 2, 2, 512], BF16))
        rd = _es.enter_context(SB(nc, "rd", [128, 2, 512], F32))
        ao = _es.enter_context(SB(nc, "ao", [128, 2, 512], BF16))
        ps_q = _es.enter_context(PS(nc, "ps_q", [128, 512], F32))
        ps_r = _es.enter_context(PS(nc, "ps_r", [128, 512], F32))
        ps_s = _es.enter_context(PS(nc, "ps_s", [128, 2, 2, 512], F32))
        ps_o = _es.enter_context(PS(nc, "ps_o", [128, 2, 512], F32))
        t_w, t_q, t_k, t_v, t_rp = Tok(), Tok(), Tok(), Tok(), [Tok(), Tok()]
        t_qs, t_q2, t_qn, t_qnb, t_psq, t_psr = Tok(), Tok(), Tok(), Tok(), Tok(), Tok()
        t_pT, t_pss, t_pso, t_rd, t_ao = [Tok(), Tok()], [Tok(), Tok()], [Tok(), Tok()], [Tok(), Tok()], [Tok(), Tok()]
        for j in range(3):
            S.dma(wq[:, :, j * 128:j * 128 + 64], wv[:, :, j * 64:(j + 1) * 64], reads=[G.t_wb], writes=[t_w])
            S.dma(wq[:, :, j * 128 + 64:(j + 1) * 128], wv[:, :, (3 + j) * 64:(4 + j) * 64], reads=[G.t_wb], writes=[t_w])
        S.dma(wq[:, :, 384:640], wv[:, :, 384:640], reads=[G.t_wb], writes=[t_w])
        S.op("pool", lambda e: e.memset(vp[:, :, :, 64:128], 1.0), writes=[t_v])
        S.op("pool", lambda e: e.memset(qT[64:128, 0:3, :], 0.0), writes=[t_q])
        S.op("pool", lambda e: e.memset(qT[0:64, 3:6, :], 0.0), writes=[t_q])
        og = {0: COLS["qg"][0], 1: COLS["qg"][0], 2: COLS["qg"][0], 3: COLS["kg"][0]}
        for bi, (c0, t0, n, tile0, ntile) in enumerate(BLOCKS):
            if bi > 0:
                b = bi % 2
                S.dma(rp[:, b, :, :], I["rope"][:, :, t0 - NCTX:t0 - NCTX + 512].rearrange("a p c -> p a c"), writes=[t_rp[b]])
            for ch in range(4):
                for k in range(8):
                    S.op("pe", lambda e: e.matmul(ps_q[:, 0:n], lhsT=wq[:, k, ch * 128:(ch + 1) * 128], rhs=hT[:, k, c0:c0 + n],
                                                  start=(k == 0), stop=(k == 7)), reads=[t_w, G.t_hT], writes=[t_psq])
                S.op("act", lambda e: e.copy(out=qs[:, 0:n], in_=ps_q[:, 0:n]), reads=[t_psq], writes=[t_qs])
                S.op("act", lambda e: e.activation(out=q2[:, 0:n], in_=qs[:, 0:n], func=AF.Square), reads=[t_qs], writes=[t_q2])
                S.op("pe", lambda e: e.matmul(ps_r[:, 0:n], lhsT=G.cm[:, 3, :], rhs=q2[:, 0:n], start=True, stop=True),
                     reads=[t_q2, G.t_c], writes=[t_psr])
                S.op("act", lambda e: e.activation(out=q2[:, 0:n], in_=ps_r[:, 0:n], func=AF.Sqrt, bias=G.epsc[:], scale=1.0 / 64),
                     reads=[t_psr, G.t_c], writes=[t_q2])
                S.op("dve", lambda e: e.reciprocal(out=q2[:, 0:n], in_=q2[:, 0:n]), reads=[t_q2], writes=[t_q2])
                S.op("dve", lambda e: e.scalar_tensor_tensor(out=qn[:, 0:n], in0=qs[:, 0:n], scalar=G.cols[:, og[ch]:og[ch] + 1],
                                                             in1=q2[:, 0:n], op0=ALU.mult, op1=ALU.mult),
                     reads=[t_qs, t_q2, G.t_cols], writes=[t_qn])
                t_dst = t_q if ch < 3 else t_k
                halves = [(0, 64, qT[0:64, ch, t0:t0 + n]), (64, 128, qT[64:128, 3 + ch, t0:t0 + n])] if ch < 3 else [(0, 128, kT[:, t0:t0 + n])]
                if bi == 0:
                    for (p0, p1, dst) in halves:
                        S.op("dve", lambda e: e.tensor_copy(out=dst, in_=qn[p0:p1, 0:n]), reads=[t_qn], writes=[t_dst])
                else:
                    b = bi % 2
                    S.op("dve", lambda e: e.tensor_copy(out=qnb[:, 0:n], in_=qn[:, 0:n]), reads=[t_qn], writes=[t_qnb])
                    S.op("pe", lambda e: e.matmul(ps_r[:, 0:n], lhsT=G.cmb[:, 4, :], rhs=qnb[:, 0:n], start=True, stop=True),
                         reads=[t_qnb, G.t_c], writes=[t_psr])
                    S.op("dve", lambda e: e.tensor_tensor(out=qs[:, 0:n], in0=ps_r[:, 0:n], in1=rp[:, b, 1, 0:n], op=ALU.mult),
                         reads=[t_psr, t_rp[b]], writes=[t_qs])
                    S.op("dve", lambda e: e.tensor_tensor(out=qn[:, 0:n], in0=qn[:, 0:n], in1=rp[:, b, 0, 0:n], op=ALU.mult),
                         reads=[t_qn, t_rp[b]], writes=[t_qn])
                    for (p0, p1, dst) in halves:
                        S.op("dve", lambda e: e.tensor_tensor(out=dst, in0=qn[p0:p1, 0:n], in1=qs[p0:p1, 0:n], op=ALU.add),
                             reads=[t_qn, t_qs], writes=[t_dst])
            for tl in range(tile0, tile0 + ntile):
                cc = colof(tl)
                for k in range(8):
                    S.op("pe", lambda e: e.matmul(ps_q[:, 0:128], lhsT=hT[:, k, cc:cc + 128], rhs=wq[:, k, 512:640],
                                                  start=(k == 0), stop=(k == 7)), reads=[t_w, G.t_hT], writes=[t_psq])
                S.op("act", lambda e: e.copy(out=vp[:, tl, :, 0:64], in_=ps_q[:, 0:128].rearrange("p (a d) -> p a d", a=2)),
                     reads=[t_psq], writes=[t_v])
        pairs = []
        oi = 0
        for h in range(6):
            for bi, (c0, t0, n, tile0, ntile) in enumerate(BLOCKS):
                if bi == 0 and not need_ctx:
                    continue
                kcs = list(range(2)) if bi == 0 else list(range(NT))
                for ki in range(0, len(kcs), 2):
                    pairs.append((h, t0, n, kcs[ki], ki == 0, ki + 2 >= len(kcs), oi % 2))
                oi += 1

        def qk(j):
            h, t0, n, kc, first, last, ob = pairs[j]
            pb = (h // 3) * 64
            sb = j % 2
            for u in range(2):
                S.op("pe", lambda e: e.matmul(ps_s[:, sb, u, 0:n], lhsT=kT[:, (kc + u) * 128:(kc + u + 1) * 128],
                                              rhs=qT[:, h, t0:t0 + n], start=True, stop=True),
                     reads=[t_q, t_k], writes=[t_pss[sb]])

        qk(0)
        for j, (h, t0, n, kc, first, last, ob) in enumerate(pairs):
            sb = j % 2
            kv = h // 3
            if j + 1 < len(pairs):
                qk(j + 1)
            S.op("act", lambda e: e.activation(out=pT[:, sb, :, 0:n], in_=ps_s[:, sb, :, 0:n], func=AF.Exp, scale=scale),
                 reads=[t_pss[sb]], writes=[t_pT[sb]])
            for u in range(2):
                S.op("pe", lambda e: e.matmul(ps_o[:, ob, 0:n], lhsT=vp[:, kc + u, kv, :], rhs=pT[:, sb, u, 0:n],
                                              start=(first and u == 0), stop=(last and u == 1)),
                     reads=[t_v, t_pT[sb]], writes=[t_pso[ob]])
            if last:
                S.op("dve", lambda e: e.reciprocal(out=rd[0:64, ob, 0:n], in_=ps_o[64:128, ob, 0:n]), reads=[t_pso[ob]], writes=[t_rd[ob]])
                S.op("dve", lambda e: e.tensor_tensor(out=ao[0:64, ob, 0:n], in0=ps_o[0:64, ob, 0:n], in1=rd[0:64, ob, 0:n], op=ALU.mult),
                     reads=[t_pso[ob], t_rd[ob]], writes=[t_ao[ob]])
                S.dma(G.mixT[h * 64:(h + 1) * 64, t0:t0 + n], ao[0:64, ob, 0:n], reads=[t_ao[ob]], writes=[G.t_mix])
        if "att" in G.dbg and li == 0:
            dump_bf16(G, G.mixT[0:128, 256:768], G.dbg["att"], [G.t_mix])


def dump_bf16(G, src, dst, reads):
    nc, S = G.nc, G.S
    p, n = src.shape
    with ExitStack() as _es:
        a = _es.enter_context(SB(nc, "dmpb", [p, n], BF16))
        b = _es.enter_context(SB(nc, "dmpf", [p, n], F32))
        t = Tok()
        S.dma(a[:], src, reads=reads, writes=[t])
        S.op("dve", lambda e: e.tensor_copy(out=b[:], in_=a[:]), reads=[t], writes=[t])
        S.dma(dst, b[:], reads=[t])
        S.barrier()


def _cols_pack(inp, li, b):
    def colform(v, n):
        return np.ascontiguousarray(v.reshape(n, 128).T)
    parts = {}
    cc = np.zeros((128, 8, 2), np.float32)
    cc[:, :, 0] = colform(inp["c"][b], 8)
    cc[:, :, 1] = colform(inp["c_ctx"], 8)
    parts["cc"] = cc.reshape(128, 16)
    bm = inp["b_mod"][li].reshape(6, 8, 128)
    parts["bmod"] = np.concatenate([bm[v].T for v in (0, 1, 3, 4)], axis=1)
    parts["g_mix"] = colform(inp["g_mix"][li], 8)
    parts["g_ffn"] = colform(inp["g_ffn"][li], 8)
    parts["ssd_conv_b"] = colform(inp["ssd_conv_b"][li], 5)
    parts["qg"] = np.tile(inp["q_norm"][li], 2)[:, None]
    parts["kg"] = np.tile(inp["k_norm"][li], 2)[:, None]
    parts["ssd_d"] = colform(np.repeat(inp["ssd_d"][li], 64), 3)
    parts["ssd_norm"] = colform(inp["ssd_norm"][li], 3)
    for nm in ("hy_b1", "hy_freq", "hy_b2"):
        parts[nm] = np.tile(inp[nm][li], 2)[:, None]
    out = np.zeros((128, NCOLS), np.float32)
    for nm, (o, n) in COLS.items():
        out[:, o:o + n] = parts[nm]
    return out


def _consts():
    ident = np.eye(128, dtype=np.float32)
    s = np.arange(128)
    U = (s[:, None] <= s[None, :]).astype(np.float32)
    Lo = (s[:, None] >= s[None, :]).astype(np.float32)
    bo = np.kron(np.eye(2, dtype=np.float32), np.ones((64, 64), np.float32))
    rot = np.zeros((128, 128), np.float32)
    for hb in (0, 64):
        for d in range(32):
            rot[hb + d + 32, hb + d] = -1.0
            rot[hb + d, hb + d + 32] = 1.0
    top = np.zeros((128, 128), np.float32); top[:64] = 1.0
    bot = np.zeros((128, 128), np.float32); bot[64:] = 1.0
    cmat = np.stack([ident, U, Lo, bo, rot, top, bot, U - ident, Lo - ident])
    rows = NLAT // 64
    row = np.repeat(np.arange(rows), 64).astype(np.float32)
    col = np.tile(np.arange(64), rows).astype(np.float32)
    inv = (10000.0 ** (-np.arange(0, 32, 2, dtype=np.float32) / 32)).astype(np.float32)
    ang = np.concatenate([row[:, None] * inv, col[:, None] * inv], axis=-1).astype(np.float32)
    cs = np.cos(ang).astype(np.float32).T
    sn = np.sin(ang).astype(np.float32).T
    rope = np.stack([np.tile(cs, (4, 1)), np.tile(sn, (4, 1))]).astype(np.float32)
    tt = np.arange(512)[None, None, :]
    ss_ = np.arange(128)[None, :, None]
    jj = np.arange(4)[:, None, None]
    mf = (tt >= 128 * jj + ss_).astype(np.float32)
    mb = (tt <= 128 * jj + ss_).astype(np.float32)
    hyt = {}
    for sname, P in SEGS.items():
        ft, it_, fe, wn, mh = _hy_tables(P["L"])
        hyt["ft_" + sname], hyt["it_" + sname], hyt["fe_" + sname], hyt["win_" + sname], hyt["mh_" + sname] = ft, it_, fe, wn, mh
    return {**hyt, "cmat": cmat, "rope": rope, "ssdmask": np.stack([mf, mb]).astype(ml_dtypes.bfloat16)}


_CONSTS = None


def kernel(**inp):
    global _CONSTS
    inp = {k: np.asarray(v) for k, v in inp.items()}
    if _CONSTS is None:
        _CONSTS = _consts()
    dbg = kernel.dbg if hasattr(kernel, "dbg") else None
    nc = build(dbg)
    ncores = 8
    in_maps = []
    for core in range(ncores):
        b = core % 4
        m = {"x": np.ascontiguousarray(inp["x"][b]), "ctx": np.ascontiguousarray(inp["ctx"][b])}
        for k in ("w_mod", "b_mod", "w_in", "w_out", "w_router", "router_bias", "w_gate", "w_up", "w_down", "g_final",
                  "ssd_conv_w", "ssd_dt_bias", "ssd_a_log", "ssd_d", "ssd_norm", "hy_conv_w", "hy_conv_b", "hy_w1", "hy_w2", "hy_w3", "hy_bias"):
            m[k] = inp[k]
        m["cols"] = np.stack([_cols_pack(inp, li, b) for li in range(DEPTH)])
        m.update(_CONSTS)
        in_maps.append(m)
    res = run_bass_kernel_spmd(nc, in_maps, core_ids=list(range(ncores)))
    kernel.last = res
    return np.stack([res.results[b]["out"] for b in range(4)]).astype(np.float32)


def outproj(G, li):
    nc, S, I = G.nc, G.S, G.I
    need_ctx = li < DEPTH - 1
    with ExitStack() as _es:
        wo = _es.enter_context(SB(nc, "wo", [128, 8, D], BF16))
        mx = _es.enter_context(SB(nc, "mx", [128, 2, 8, 128], BF16))
        xt = _es.enter_context(SB(nc, "xt", [128, 2, D], F32))
        tmp = _es.enter_context(SB(nc, "tmp", [128, D], F32))
        sq = _es.enter_context(SB(nc, "sq", [128, D], F32))
        ss = _es.enter_context(SB(nc, "ss", [128, 4], F32))
        xn = _es.enter_context(SB(nc, "xn", [128, D], F32))
        h2f = _es.enter_context(SB(nc, "h2f", [128, 8, 128], F32))
        wr = _es.enter_context(SB(nc, "wr", [128, 8, NE], F32))
        po = _es.enter_context(PS(nc, "po", [128, 2, 512], F32))
        pt = _es.enter_context(PS(nc, "pt", [128, 8, 128], F32))
        pr = _es.enter_context(PS(nc, "pr", [128, NE], F32))
        t_wo, t_mx, t_x, t_tmp, t_po = Tok(), [Tok(), Tok()], [Tok(), Tok()], Tok(), Tok()
        t_sq, t_ss, t_xn, t_pt, t_h2f, t_wr, t_pr = Tok(), Tok(), Tok(), Tok(), Tok(), Tok(), Tok()
        S.dma(wo[:], G.wb_out[li].rearrange("(k p) c -> p k c", p=128), reads=[G.t_wb], writes=[t_wo])
        S.dma(wr[:], I["w_router"].rearrange("(k p) c -> p k c", p=128), writes=[t_wr])
        mv = G.mixT.rearrange("(k p) t -> p k t", p=128)
        tiles = [t for t in range(NT) if need_ctx or t >= 2]

        def mm(tile):
            b = tile % 2
            S.dma(mx[:, b], mv[:, :, tile * 128:(tile + 1) * 128], reads=[G.t_mix], writes=[t_mx[b]])
            S.dma(xt[:, b, :], G.xres[tile * 128:(tile + 1) * 128, :], reads=[G.t_xres[tile]], writes=[t_x[b]])
            for half in range(2):
                for k in range(8):
                    S.op("pe", lambda e: e.matmul(po[:, half, :], lhsT=mx[:, b, k, :], rhs=wo[:, k, half * 512:(half + 1) * 512],
                                                  start=(k == 0), stop=(k == 7)), reads=[t_mx[b], t_wo], writes=[t_po])

        mm(tiles[0])
        for ti, tile in enumerate(tiles):
            b = tile % 2
            j = 1 if tile < 2 else 0
            S.op("dve", lambda e: e.tensor_tensor(out=tmp[:], in0=po[:].rearrange("p a c -> p (a c)"), in1=G.gtb[:, 0, j, :], op=ALU.mult),
                 reads=[t_po, G.t_gtb], writes=[t_tmp])
            if ti + 1 < len(tiles):
                mm(tiles[ti + 1])
            S.op("dve", lambda e: e.tensor_tensor(out=xt[:, b, :], in0=tmp[:], in1=xt[:, b, :], op=ALU.add),
                 reads=[t_tmp, t_x[b]], writes=[t_x[b]])
            S.dma(G.xres[tile * 128:(tile + 1) * 128, :], xt[:, b, :], reads=[t_x[b]], writes=[G.t_xres[tile]])
            xv = xt[:, b, :]
            S.op("act", lambda e: e.activation(out=sq[:], in_=xv, func=AF.Square, accum_out=ss[:, 0:1]), reads=[t_x[b]], writes=[t_sq, t_ss])
            S.op("act", lambda e: e.activation(out=ss[:, 1:2], in_=ss[:, 0:1], func=AF.Sqrt, bias=G.epsc[:], scale=1.0 / D),
                 reads=[t_ss, G.t_c], writes=[t_ss])
            S.op("dve", lambda e: e.reciprocal(out=ss[:, 2:3], in_=ss[:, 1:2]), reads=[t_ss], writes=[t_ss])
            S.op("dve", lambda e: e.tensor_scalar(out=xn[:], in0=xv, scalar1=ss[:, 2:3], scalar2=None, op0=ALU.mult),
                 reads=[t_x[b], t_ss], writes=[t_xn])
            for k in range(8):
                S.op("pe", lambda e: e.transpose(out=pt[:, k, :], in_=xn[:, k * 128:(k + 1) * 128], identity=G.identF),
                     reads=[t_xn, G.t_c], writes=[t_pt])
            S.op("dve", lambda e: e.tensor_tensor(out=h2f[:], in0=pt[:], in1=G.modc[:, 3, :, j:j + 1].to_broadcast([128, 8, 128]), op=ALU.mult),
                 reads=[t_pt, G.t_modc], writes=[t_h2f])
            S.op("dve", lambda e: e.tensor_tensor(out=h2f[:], in0=h2f[:], in1=G.modc[:, 2, :, j:j + 1].to_broadcast([128, 8, 128]), op=ALU.add),
                 reads=[t_h2f, G.t_modc], writes=[t_h2f])
            S.op("act", lambda e: e.copy(out=G.h2T[:, :, tile * 128:(tile + 1) * 128], in_=h2f[:]), reads=[t_h2f], writes=[G.t_h2T])
            for k in range(8):
                S.op("pe", lambda e: e.matmul(pr[:], lhsT=h2f[:, k, :], rhs=wr[:, k, :], start=(k == 0), stop=(k == 7)),
                     reads=[t_h2f, t_wr], writes=[t_pr])
            S.op("dve", lambda e: e.tensor_copy(out=G.rl[:, tile, :], in_=pr[:]), reads=[t_pr], writes=[G.t_rl])


def moe(G, li):
    nc, S, I = G.nc, G.S, G.I
    need_ctx = li < DEPTH - 1
    last = li == DEPTH - 1
    h2T, rl = G.h2T, G.rl
    T0 = 0 if need_ctx else 2
    NTl = NT - T0
    BIG = 1.0e9
    with ExitStack() as _es:
        comb = _es.enter_context(SB(nc, "comb", [128, NT, NE], F32))
        t_comb = Tok()
        with ExitStack() as _es:
            sc = _es.enter_context(SB(nc, "r_sc", [128, NT, NE], F32))
            sel = _es.enter_context(SB(nc, "r_sel", [128, NT, NE], F32))
            ra = _es.enter_context(SB(nc, "r_a", [128, NT, NE], F32))
            rb = _es.enter_context(SB(nc, "r_b", [128, NT, NE], F32))
            rm = _es.enter_context(SB(nc, "r_m", [128, NT * 4], F32))
            rm2 = _es.enter_context(SB(nc, "r_m2", [128, NT * 4], F32))
            rg = _es.enter_context(SB(nc, "r_g", [128, NT], F32))
            rbias = _es.enter_context(SB(nc, "rbias", [128, NE], F32))
            t = Tok()
            if T0 > 0:
                S.op("dve", lambda e: e.memset(rl[:, 0:T0, :], 0.0), reads=[G.t_rl], writes=[G.t_rl])
            S.dma(rbias[:], I["router_bias"].partition_broadcast(128), writes=[t])
            v3 = lambda a: a[:].rearrange("p n (g x) -> p (n g) x", x=4)
            S.op("act", lambda e: e.activation(out=sc[:], in_=rl[:], func=AF.Sigmoid), reads=[G.t_rl], writes=[t])
            S.op("dve", lambda e: e.tensor_tensor(out=sel[:], in0=sc[:], in1=rbias[:].unsqueeze(1).to_broadcast([128, NT, NE]), op=ALU.add),
                 reads=[t], writes=[t])
            S.op("dve", lambda e: e.tensor_reduce(out=rm[:], in_=v3(sel), axis=AX.X, op=ALU.max), reads=[t], writes=[t])
            S.op("dve", lambda e: e.tensor_tensor(out=v3(ra), in0=v3(sel), in1=rm[:].unsqueeze(2).to_broadcast([128, NT * 4, 4]), op=ALU.is_equal),
                 reads=[t], writes=[t])
            S.op("dve", lambda e: e.scalar_tensor_tensor(out=rb[:], in0=ra[:], scalar=-BIG, in1=sel[:], op0=ALU.mult, op1=ALU.add),
                 reads=[t], writes=[t])
            S.op("dve", lambda e: e.tensor_reduce(out=rm2[:], in_=v3(rb), axis=AX.X, op=ALU.max), reads=[t], writes=[t])
            S.op("dve", lambda e: e.tensor_tensor(out=rm[:], in0=rm[:], in1=rm2[:], op=ALU.add), reads=[t], writes=[t])
            S.op("dve", lambda e: e.tensor_reduce(out=rg[:], in_=rm[:].rearrange("p (n g) -> p n g", g=4), axis=AX.X, op=ALU.max),
                 reads=[t], writes=[t])
            S.op("dve", lambda e: e.tensor_tensor(out=rm2[:].rearrange("p (n g) -> p n g", g=4), in0=rm[:].rearrange("p (n g) -> p n g", g=4),
                                                  in1=rg[:].unsqueeze(2).to_broadcast([128, NT, 4]), op=ALU.is_equal), reads=[t], writes=[t])
            S.op("dve", lambda e: e.tensor_scalar(out=rm2[:], in0=rm2[:], scalar1=1.0, scalar2=BIG, op0=ALU.subtract, op1=ALU.mult),
                 reads=[t], writes=[t])
            S.op("dve", lambda e: e.tensor_tensor(out=v3(sel), in0=v3(sel), in1=rm2[:].unsqueeze(2).to_broadcast([128, NT * 4, 4]), op=ALU.add),
                 reads=[t], writes=[t])
            S.op("dve", lambda e: e.tensor_reduce(out=rg[:], in_=sel[:], axis=AX.X, op=ALU.max), reads=[t], writes=[t])
            S.op("dve", lambda e: e.tensor_tensor(out=ra[:], in0=sel[:], in1=rg[:].unsqueeze(2).to_broadcast([128, NT, NE]), op=ALU.is_equal),
                 reads=[t], writes=[t])
            S.op("dve", lambda e: e.scalar_tensor_tensor(out=sel[:], in0=ra[:], scalar=-BIG, in1=sel[:], op0=ALU.mult, op1=ALU.add),
                 reads=[t], writes=[t])
            S.op("dve", lambda e: e.tensor_reduce(out=rg[:], in_=sel[:], axis=AX.X, op=ALU.max), reads=[t], writes=[t])
            S.op("dve", lambda e: e.tensor_tensor(out=rb[:], in0=sel[:], in1=rg[:].unsqueeze(2).to_broadcast([128, NT, NE]), op=ALU.is_equal),
                 reads=[t], writes=[t])
            S.op("dve", lambda e: e.tensor_tensor(out=ra[:], in0=ra[:], in1=rb[:], op=ALU.add), reads=[t], writes=[t])
            S.op("dve", lambda e: e.tensor_tensor(out=ra[:], in0=ra[:], in1=sc[:], op=ALU.mult), reads=[t], writes=[t])
            S.op("dve", lambda e: e.tensor_reduce(out=rg[:], in_=ra[:], axis=AX.X, op=ALU.add), reads=[t], writes=[t])
            S.op("dve", lambda e: e.reciprocal(out=rg[:], in_=rg[:]), reads=[t], writes=[t])
            S.op("dve", lambda e: e.tensor_tensor(out=comb[:], in0=ra[:], in1=rg[:].unsqueeze(2).to_broadcast([128, NT, NE]), op=ALU.mult),
                 reads=[t], writes=[t_comb])
            S.barrier()
        SGT = 12
        with ExitStack() as _es:
            acc = _es.enter_context(SB(nc, "acc", [128, SGT, D], F32))
            wg = _es.enter_context(SB(nc, "wg", [128, 2, 8, DFF], BF16))
            wu = _es.enter_context(SB(nc, "wu", [128, 2, 8, DFF], BF16))
            wd = _es.enter_context(SB(nc, "wd", [128, 2, 2, D], BF16))
            sgl = _es.enter_context(SB(nc, "sgl", [128, 2, 512], F32))
            aa = _es.enter_context(SB(nc, "aa", [128, 2, 512], BF16))
            xt = _es.enter_context(SB(nc, "xt", [128, 2, D], F32))
            gfb = _es.enter_context(SB(nc, "gfb", [128, D], F32))
            ss = _es.enter_context(SB(nc, "ss", [128, 4], F32))
            sq = _es.enter_context(SB(nc, "sq", [128, D], F32))
            pgu = _es.enter_context(PS(nc, "pgu", [128, 4, 512], F32))
            py = _es.enter_context(PS(nc, "py", [128, 2, 2, 512], F32))
            t_acc, t_w, t_sgl, t_aa, t_pgu, t_py, t_x, t_gf, t_ss, t_sq = Tok(), [Tok(), Tok()], Tok(), Tok(), Tok(), [Tok(), Tok()], [Tok(), Tok()], Tok(), Tok(), Tok()
            if last:
                S.dma(gfb[:], I["g_final"].partition_broadcast(128), writes=[t_gf])
            yi = 0
            for s0 in range(T0, NT, SGT):
                tiles = list(range(s0, min(NT, s0 + SGT)))
                for ex in range(NE):
                    wb_ = ex % 2
                    S.dma(wg[:, wb_], G.wb_gate[li][ex].rearrange("(k p) f -> p k f", p=128), reads=[G.t_wb], writes=[t_w[wb_]])
                    S.dma(wu[:, wb_], G.wb_up[li][ex].rearrange("(k p) f -> p k f", p=128), reads=[G.t_wb], writes=[t_w[wb_]])
                    S.dma(wd[:, wb_], G.wb_down[li][ex].rearrange("(j p) c -> p j c", p=128), reads=[G.t_wb], writes=[t_w[wb_]])
                    for b0 in range(0, len(tiles), 4):
                        bt = tiles[b0:b0 + 4]
                        n = len(bt) * 128
                        c0 = bt[0] * 128
                        for wi, wt in enumerate((wg, wu)):
                            for jj in range(2):
                                for k in range(8):
                                    S.op("pe", lambda e: e.matmul(pgu[:, wi * 2 + jj, 0:n], lhsT=wt[:, wb_, k, jj * 128:(jj + 1) * 128],
                                                                  rhs=h2T[:, k, c0:c0 + n], start=(k == 0), stop=(k == 7)),
                                         reads=[t_w[wb_], G.t_h2T], writes=[t_pgu])
                        S.op("act", lambda e: e.activation(out=sgl[:, :, 0:n], in_=pgu[:, 0:2, 0:n], func=AF.Silu), reads=[t_pgu], writes=[t_sgl])
                        S.op("dve", lambda e: e.tensor_tensor(out=aa[:, :, 0:n], in0=sgl[:, :, 0:n], in1=pgu[:, 2:4, 0:n], op=ALU.mult),
                             reads=[t_sgl, t_pgu], writes=[t_aa])
                        for ti, tl in enumerate(bt):
                            yb = yi % 2
                            yi += 1
                            for half in range(2):
                                for jj in range(2):
                                    S.op("pe", lambda e: e.matmul(py[:, yb, half, :], lhsT=aa[:, jj, ti * 128:(ti + 1) * 128],
                                                                  rhs=wd[:, wb_, jj, half * 512:(half + 1) * 512], start=(jj == 0), stop=(jj == 1)),
                                         reads=[t_aa, t_w[wb_]], writes=[t_py[yb]])
                            al = acc[:, tl - s0, :]
                            pyv = py[:, yb].rearrange("p a c -> p (a c)")
                            if ex == 0:
                                S.op("dve", lambda e: e.tensor_scalar(out=al, in0=pyv, scalar1=comb[:, tl, ex:ex + 1], scalar2=None, op0=ALU.mult),
                                     reads=[t_py[yb], t_comb], writes=[t_acc])
                            else:
                                S.op("dve", lambda e: e.scalar_tensor_tensor(out=al, in0=pyv, scalar=comb[:, tl, ex:ex + 1], in1=al,
                                                                             op0=ALU.mult, op1=ALU.add), reads=[t_py[yb], t_comb, t_acc], writes=[t_acc])
                for tl in tiles:
                    b = tl % 2
                    j = 1 if tl < 2 else 0
                    S.dma(xt[:, b, :], G.xres[tl * 128:(tl + 1) * 128, :], reads=[G.t_xres[tl]], writes=[t_x[b]])
                    al = acc[:, tl - s0, :]
                    S.op("dve", lambda e: e.tensor_tensor(out=al, in0=al, in1=G.gtb[:, 1, j, :], op=ALU.mult), reads=[t_acc, G.t_gtb], writes=[t_acc])
                    S.op("dve", lambda e: e.tensor_tensor(out=xt[:, b, :], in0=al, in1=xt[:, b, :], op=ALU.add), reads=[t_acc, t_x[b]], writes=[t_x[b]])
                    if not last:
                        S.dma(G.xres[tl * 128:(tl + 1) * 128, :], xt[:, b, :], reads=[t_x[b]], writes=[G.t_xres[tl]])
                    else:
                        xv = xt[:, b, :]
                        S.op("act", lambda e: e.activation(out=sq[:], in_=xv, func=AF.Square, accum_out=ss[:, 0:1]), reads=[t_x[b]], writes=[t_sq, t_ss])
                        S.op("act", lambda e: e.activation(out=ss[:, 1:2], in_=ss[:, 0:1], func=AF.Sqrt, bias=G.epsc[:], scale=1.0 / D),
                             reads=[t_ss, G.t_c], writes=[t_ss])
                        S.op("dve", lambda e: e.reciprocal(out=ss[:, 2:3], in_=ss[:, 1:2]), reads=[t_ss], writes=[t_ss])
                        S.op("dve", lambda e: e.scalar_tensor_tensor(out=xv, in0=xv, scalar=ss[:, 2:3], in1=gfb[:], op0=ALU.mult, op1=ALU.mult),
                             reads=[t_x[b], t_ss, t_gf], writes=[t_x[b]])
                        S.dma(G.out[(tl - 2) * 128:(tl - 1) * 128, :], xv, reads=[t_x[b]], writes=[])


def ssd(G, li):
    nc, S, I = G.nc, G.S, G.I
    hT = G.hT
    need_ctx = li < DEPTH - 1
    wv32 = I["w_in"][li].rearrange("(k p) c -> p k c", p=128)
    wvb = G.wb_in[li].rearrange("(k p) c -> p k c", p=128)
    XB0 = 1024
    with ExitStack() as _es:
        xbcT = _es.enter_context(SB(nc, "xbcT", [128, 5, NTOK], BF16))
        xs_tok = _es.enter_context(SB(nc, "xs_tok", [128, NT, 384], BF16))
        B_tok = _es.enter_context(SB(nc, "B_tok", [128, NT, 128], BF16))
        lndt = _es.enter_context(SB(nc, "lndt", [128, NT, 12], F32))
        dta = _es.enter_context(SB(nc, "dta", [128, NT, 12], F32))
        ea = _es.enter_context(SB(nc, "ea", [128, NT, 12], F32))
        ww = _es.enter_context(SB(nc, "ww", [128, NT, 12], F32))
        eT = _es.enter_context(SB(nc, "eT", [128, NT, 12], F32))
        t_xbc, t_xs, t_dt = Tok(), Tok(), Tok()
        with ExitStack() as _es2:
            dts = _es2.enter_context(SB(nc, "dts", [128, NT, 12], F32))
            wst = _es2.enter_context(SB(nc, "wst", [128, 8, 128], F32))
            cwb = _es2.enter_context(SB(nc, "cwb", [128, 3, 128], F32))
            wj = _es2.enter_context(SB(nc, "wj", [128, 3, 8, 128], BF16))
            wdt = _es2.enter_context(SB(nc, "wdt", [128, 8, 12], BF16))
            dtb = _es2.enter_context(SB(nc, "dtb", [128, 2, 12], F32))
            tot = _es2.enter_context(SB(nc, "tot", [128, NT, 12], F32))
            wcol = _es2.enter_context(SB(nc, "wcol", [128, NT, 12], F32))
            pp = _es2.enter_context(PS(nc, "pp", [128, 512], F32))
            pdt = _es2.enter_context(PS(nc, "pdt", [128, NT, 12], F32))
            ptb = _es2.enter_context(PS(nc, "ptb", [128, 4, 128], BF16))
            pc = _es2.enter_context(PS(nc, "pc", [128, NT, 12], F32))
            t_wst, t_cwb, t_wj, t_pp, t_wdt, t_pdt, t_ptb, t_a, t_pc = Tok(), Tok(), Tok(), Tok(), Tok(), Tok(), Tok(), Tok(), Tok()
            ocb = COLS["ssd_conv_b"][0]
            for ch in range(5):
                S.dma(wst[:], wv32[:, :, XB0 + ch * 128:XB0 + (ch + 1) * 128], writes=[t_wst])
                for j in range(3):
                    S.dma(cwb[:, j, :], I["ssd_conv_w"][li, j, ch * 128:(ch + 1) * 128].partition_broadcast(128), writes=[t_cwb])
                for j in range(3):
                    S.op("dve", lambda e: e.tensor_tensor(out=wj[:, j], in0=wst[:], in1=cwb[:, j:j + 1, :].to_broadcast([128, 8, 128]), op=ALU.mult),
                         reads=[t_wst, t_cwb], writes=[t_wj])
                for (c0, t0, n, tile0, ntile) in BLOCKS:
                    for j in range(3):
                        for k in range(8):
                            S.op("pe", lambda e: e.matmul(pp[:, 0:n], lhsT=wj[:, j, k, :], rhs=hT[:, k, c0 + j - 1:c0 + j - 1 + n],
                                                          start=(j == 0 and k == 0), stop=(j == 2 and k == 7)), reads=[t_wj, G.t_hT], writes=[t_pp])
                    S.op("act", lambda e: e.activation(out=xbcT[:, ch, t0:t0 + n], in_=pp[:, 0:n], func=AF.Silu, bias=G.cols[:, ocb + ch:ocb + ch + 1]),
                         reads=[t_pp, G.t_cols], writes=[t_xbc])
            S.dma(wdt[:], wvb[:, :, 1664:1676], reads=[G.t_wb], writes=[t_wdt])
            S.dma(dtb[:, 0, :], I["ssd_dt_bias"][li].rearrange("a h -> (a h)").partition_broadcast(128), writes=[t_wdt])
            S.dma(dtb[:, 1, :], I["ssd_a_log"][li].rearrange("a h -> (a h)").partition_broadcast(128), writes=[t_wdt])
            for tl in range(NT):
                cc = colof(tl)
                for k in range(8):
                    S.op("pe", lambda e: e.matmul(pdt[:, tl, :], lhsT=hT[:, k, cc:cc + 128], rhs=wdt[:, k, :], start=(k == 0), stop=(k == 7)),
                         reads=[t_wdt, G.t_hT], writes=[t_pdt])
            S.op("dve", lambda e: e.tensor_tensor(out=dts[:], in0=pdt[:], in1=dtb[:, 0:1, :].to_broadcast([128, NT, 12]), op=ALU.add),
                 reads=[t_pdt, t_wdt], writes=[t_dt])
            S.op("act", lambda e: e.activation(out=dts[:], in_=dts[:], func=AF.Exp), reads=[t_dt], writes=[t_dt])
            S.op("act", lambda e: e.activation(out=dts[:], in_=dts[:], func=AF.Ln, bias=1.0), reads=[t_dt], writes=[t_dt])
            S.op("act", lambda e: e.activation(out=lndt[:], in_=dts[:], func=AF.Ln), reads=[t_dt], writes=[t_dt])
            S.op("act", lambda e: e.activation(out=dtb[:, 1, :], in_=dtb[:, 1, :], func=AF.Exp), reads=[t_wdt], writes=[t_wdt])
            S.op("dve", lambda e: e.scalar_tensor_tensor(out=dta[:], in0=dts[:], scalar=-1.0, in1=dtb[:, 1:2, :].to_broadcast([128, NT, 12]),
                                                         op0=ALU.mult, op1=ALU.mult), reads=[t_dt, t_wdt], writes=[t_a])
            for tl in range(NT):
                for c in range(4):
                    S.op("pe", lambda e: e.transpose(out=ptb[:, c, :], in_=xbcT[:, c, tl * 128:(tl + 1) * 128], identity=G.identB),
                         reads=[t_xbc, G.t_c], writes=[t_ptb])
                S.op("dve", lambda e: e.tensor_copy(out=xs_tok[:, tl, :], in_=ptb[:, 0:3, :].rearrange("p c t -> p (c t)")), reads=[t_ptb], writes=[t_xs])
                S.op("dve", lambda e: e.tensor_copy(out=B_tok[:, tl, :], in_=ptb[:, 3, :]), reads=[t_ptb], writes=[t_xs])
            for dr in range(2):
                S.op("pe", lambda e: e.matmul(pc[:].rearrange("p n h -> p (n h)"), lhsT=G.cm[:, 1 + dr, :], rhs=dta[:].rearrange("p n h -> p (n h)"),
                                              start=True, stop=True), reads=[t_a, G.t_c, t_dt], writes=[t_pc])
                S.op("dve", lambda e: e.tensor_copy(out=wcol[:, :, dr * 6:(dr + 1) * 6], in_=pc[:, :, dr * 6:(dr + 1) * 6]), reads=[t_pc], writes=[t_dt])
            S.op("pe", lambda e: e.matmul(pc[:].rearrange("p n h -> p (n h)"), lhsT=G.ones[:], rhs=dta[:].rearrange("p n h -> p (n h)"), start=True, stop=True),
                 reads=[t_a, G.t_c, t_dt], writes=[t_pc])
            S.op("dve", lambda e: e.tensor_copy(out=tot[:], in_=pc[:]), reads=[t_pc], writes=[t_dt])
            S.op("act", lambda e: e.activation(out=ea[:], in_=wcol[:], func=AF.Exp), reads=[t_dt], writes=[t_dt])
            S.op("act", lambda e: e.activation(out=eT[:], in_=tot[:], func=AF.Exp), reads=[t_dt], writes=[t_dt])
            S.op("dve", lambda e: e.tensor_tensor(out=ww[:], in0=tot[:], in1=wcol[:], op=ALU.subtract), reads=[t_dt], writes=[t_dt])
            S.op("dve", lambda e: e.tensor_tensor(out=ww[:], in0=ww[:], in1=lndt[:], op=ALU.add), reads=[t_dt], writes=[t_dt])
            S.op("act", lambda e: e.activation(out=ww[:], in_=ww[:], func=AF.Exp), reads=[t_dt], writes=[t_dt])
            S.barrier()
        with ExitStack() as _es2:
            wz = _es2.enter_context(SB(nc, "wz", [128, 8, 384], BF16))
            dbc = _es2.enter_context(SB(nc, "dbc", [128, 6, 64], F32))
            d6 = _es2.enter_context(SB(nc, "d6", [128, 6], F32))
            nwb = _es2.enter_context(SB(nc, "nwb", [128, 384], F32))
            hst = _es2.enter_context(SB(nc, "hst", [128, 192], F32))
            hsb = _es2.enter_context(SB(nc, "hsb", [128, 192], BF16))
            xw = _es2.enter_context(SB(nc, "xw", [128, 2, 192], BF16))
            ybt = _es2.enter_context(SB(nc, "ybt", [128, 2, 384], F32))
            Sm = _es2.enter_context(SB(nc, "Sm", [128, 2, 2, 128], F32))
            rA = _es2.enter_context(SB(nc, "rA", [128, 4, 128], F32))
            Dd = _es2.enter_context(SB(nc, "Dd", [128, 4, 128], F32))
            Mt = _es2.enter_context(SB(nc, "Mt", [128, 4, 128], BF16))
            acc = _es2.enter_context(SB(nc, "acc", [128, 384], F32))
            tmp = _es2.enter_context(SB(nc, "tmp", [128, 384], F32))
            zs = _es2.enter_context(SB(nc, "zs", [128, 384], F32))
            ssq = _es2.enter_context(SB(nc, "ssq", [128, 4], F32))
            ob = _es2.enter_context(SB(nc, "ob", [128, 384], BF16))
            oT = _es2.enter_context(SB(nc, "oT", [128, 2, 3, 128], BF16))
            pis = _es2.enter_context(PS(nc, "pis", [128, 2, 192], F32))
            ps_st = _es2.enter_context(PS(nc, "ps_st", [128, 2, 512], F32))
            pseg = _es2.enter_context(PS(nc, "pseg", [128, 3, 512], F32))
            py = _es2.enter_context(PS(nc, "py", [128, 384], F32))
            pz = py
            ptr = _es2.enter_context(PS(nc, "ptr", [128, 3, 128], BF16))
            t_wz, t_db, t_h, t_xw, t_yb, t_Sm, t_acc, t_tmp, t_zs, t_ssq, t_ob, t_oT = Tok(), Tok(), Tok(), Tok(), [Tok(), Tok()], Tok(), Tok(), Tok(), Tok(), Tok(), Tok(), [Tok(), Tok()]
            t_rA, t_Dd, t_Mt, t_pseg = [Tok() for _ in range(4)], [Tok() for _ in range(4)], [Tok() for _ in range(4)], [Tok() for _ in range(3)]
            t_pis, t_pst, t_py, t_ptr = Tok(), Tok(), Tok(), Tok()
            t_pz = t_py
            S.dma(wz[:], wvb[:, :, 640:1024], reads=[G.t_wb], writes=[t_wz])
            S.dma(d6[:], I["ssd_d"][li].partition_broadcast(128), writes=[t_db])
            S.dma(nwb[:], I["ssd_norm"][li].partition_broadcast(128), writes=[t_db])
            S.op("dve", lambda e: e.tensor_copy(out=dbc[:], in_=d6[:].unsqueeze(2).to_broadcast([128, 6, 64])), reads=[t_db], writes=[t_db])
            yb_d = G.ssd_yb

            def carry_step(c, dr, want_out, dst):
                for g in range(2):
                    hd0 = dr * 6 + 3 * g
                    if want_out:
                        S.op("pe", lambda e: e.matmul(pis[:, 0, :], lhsT=xbcT[g * 64:(g + 1) * 64, 4, c * 128:(c + 1) * 128], rhs=hsb[g * 64:(g + 1) * 64, :],
                                                      start=True, stop=True), reads=[t_xbc, t_h], writes=[t_pis])
                        S.op("dve", lambda e: e.tensor_tensor(out=dst[:, g * 192:(g + 1) * 192].rearrange("p (a d) -> p a d", a=3),
                                                              in0=pis[:, 0, :].rearrange("p (a d) -> p a d", a=3),
                                                              in1=ea[:, c, hd0:hd0 + 3].unsqueeze(2).to_broadcast([128, 3, 64]), op=ALU.mult),
                             reads=[t_pis, t_dt], writes=[dst_tok[0]])
                    S.op("dve", lambda e: e.tensor_tensor(out=xw[:, g, :].rearrange("p (a d) -> p a d", a=3),
                                                          in0=xs_tok[:, c, g * 192:(g + 1) * 192].rearrange("p (a d) -> p a d", a=3),
                                                          in1=ww[:, c, hd0:hd0 + 3].unsqueeze(2).to_broadcast([128, 3, 64]), op=ALU.mult),
                         reads=[t_xs, t_dt], writes=[t_xw])
                    gs = slice(g * 64, (g + 1) * 64)
                    S.op("pe", lambda e: e.matmul(pis[:, 1, :], lhsT=B_tok[:, c, :], rhs=xw[:, g, :], start=True, stop=True),
                         reads=[t_xs, t_xw], writes=[t_pis])
                    S.op("dve", lambda e: e.tensor_tensor(out=hst[gs, :].rearrange("p (a d) -> p a d", a=3),
                                                          in0=hst[gs, :].rearrange("p (a d) -> p a d", a=3),
                                                          in1=eT[gs, c, hd0:hd0 + 3].unsqueeze(2).to_broadcast([64, 3, 64]), op=ALU.mult),
                         reads=[t_h, t_dt], writes=[t_h])
                    S.op("dve", lambda e: e.tensor_tensor(out=hst[gs, :], in0=hst[gs, :], in1=pis[gs, 1, :], op=ALU.add),
                         reads=[t_h, t_pis], writes=[t_h])
                    S.op("act", lambda e: e.copy(out=hsb[gs, :], in_=hst[gs, :]), reads=[t_h], writes=[t_h])

            S.op("dve", lambda e: e.memset(hst[:], 0.0), writes=[t_h])
            S.op("dve", lambda e: e.memset(hsb[:], 0.0), writes=[t_h])
            order_b = [1, 0] + list(range(NT - 1, 1, -1))
            for ci, c in enumerate(order_b):
                want = need_ctx or c >= 2
                b = ci % 2
                dst_tok = [t_yb[b]]
                carry_step(c, 1, want, ybt[:, b, :])
                if want:
                    S.dma(yb_d[c * 128:(c + 1) * 128, :], ybt[:, b, :], reads=[t_yb[b]], writes=[G.t_ssdyb[c]])
            S.op("dve", lambda e: e.memset(hst[:], 0.0), reads=[t_h], writes=[t_h])
            S.op("dve", lambda e: e.memset(hsb[:], 0.0), reads=[t_h], writes=[t_h])
            on = COLS["ssd_norm"][0]
            it = 0
            for c in range(NT):
                want = need_ctx or c >= 2
                dst_tok = [t_acc]
                carry_step(c, 0, want, acc[:])
                if not want:
                    continue
                b = c % 2
                tc0 = c * 128
                S.dma(ybt[:, b, :], yb_d[c * 128:(c + 1) * 128, :], reads=[G.t_ssdyb[c]], writes=[t_yb[b]])
                cc = colof(c)
                for k in range(8):
                    S.op("pe", lambda e: e.matmul(pz[:], lhsT=hT[:, k, cc:cc + 128], rhs=wz[:, k, :], start=(k == 0), stop=(k == 7)),
                         reads=[t_wz, G.t_hT], writes=[t_pz])
                S.op("act", lambda e: e.activation(out=zs[:], in_=pz[:], func=AF.Silu), reads=[t_pz], writes=[t_zs])
                for g in range(2):
                    S.op("pe", lambda e: e.matmul(ps_st[:, g, 0:128], lhsT=xbcT[g * 64:(g + 1) * 64, 3, tc0:tc0 + 128], rhs=xbcT[g * 64:(g + 1) * 64, 4, tc0:tc0 + 128],
                                                  start=True, stop=True), reads=[t_xbc], writes=[t_pst])
                for g in range(2):
                    for dr in range(2):
                        S.op("dve", lambda e: e.tensor_tensor(out=Sm[:, g, dr, :], in0=ps_st[:, g, 0:128], in1=G.cm[:, 1 + dr, :], op=ALU.mult),
                             reads=[t_pst, G.t_c], writes=[t_Sm])
                steps = [(h, dr) for h in range(6) for dr in range(2)]

                def st_a(i):
                    h, dr = steps[i]
                    hd = dr * 6 + h
                    r4, p3 = (it + i) % 4, (it + i) % 3
                    S.op("dve", lambda e: e.tensor_scalar(out=rA[:, r4, :], in0=G.cm[:, 1 + dr, :], scalar1=dta[:, c, hd:hd + 1], scalar2=None, op0=ALU.mult),
                         reads=[G.t_c, t_dt], writes=[t_rA[r4]])
                    S.op("pe", lambda e: e.matmul(pseg[:, p3, 0:128], lhsT=G.cm[:, 8 - dr, :], rhs=rA[:, r4, :], start=True, stop=True),
                         reads=[t_rA[r4], G.t_c], writes=[t_pseg[p3]])
                    S.op("act", lambda e: e.activation(out=Dd[:, r4, :], in_=pseg[:, p3, 0:128], func=AF.Exp, bias=lndt[:, c, hd:hd + 1]),
                         reads=[t_pseg[p3], t_dt], writes=[t_Dd[r4]])

                st_a(0)
                st_a(1)
                for i, (h, dr) in enumerate(steps):
                    r4 = (it + i) % 4
                    if i + 2 < len(steps):
                        st_a(i + 2)
                    S.op("dve", lambda e: e.tensor_tensor(out=Mt[:, r4, :], in0=Dd[:, r4, :], in1=Sm[:, h // 3, dr, :], op=ALU.mult),
                         reads=[t_Dd[r4], t_Sm], writes=[t_Mt[r4]])
                    S.op("pe", lambda e: e.matmul(py[:, h * 64:(h + 1) * 64], lhsT=Mt[:, r4, :], rhs=xs_tok[:, c, h * 64:(h + 1) * 64],
                                                  start=(dr == 0), stop=(dr == 1)), reads=[t_Mt[r4], t_xs], writes=[t_py])
                it += len(steps)
                S.op("dve", lambda e: e.tensor_tensor(out=acc[:], in0=acc[:], in1=py[:], op=ALU.add), reads=[t_acc, t_py], writes=[t_acc])
                S.op("dve", lambda e: e.tensor_tensor(out=acc[:], in0=acc[:], in1=ybt[:, b, :], op=ALU.add), reads=[t_acc, t_yb[b]], writes=[t_acc])
                S.op("dve", lambda e: e.tensor_tensor(out=tmp[:], in0=xs_tok[:, c, :], in1=dbc[:].rearrange("p a d -> p (a d)"), op=ALU.mult),
                     reads=[t_xs, t_db], writes=[t_tmp])
                S.op("dve", lambda e: e.tensor_tensor(out=acc[:], in0=acc[:], in1=tmp[:], op=ALU.add), reads=[t_acc, t_tmp], writes=[t_acc])
                S.op("dve", lambda e: e.tensor_tensor(out=acc[:], in0=acc[:], in1=zs[:], op=ALU.mult), reads=[t_acc, t_zs], writes=[t_acc])
                for g in range(2):
                    S.op("act", lambda e: e.activation(out=tmp[:, g * 192:(g + 1) * 192], in_=acc[:, g * 192:(g + 1) * 192], func=AF.Square, accum_out=ssq[:, g:g + 1]),
                         reads=[t_acc], writes=[t_tmp, t_ssq])
                S.op("act", lambda e: e.activation(out=ssq[:, 2:4], in_=ssq[:, 0:2], func=AF.Sqrt, bias=G.epsc[:], scale=1.0 / 192), reads=[t_ssq, G.t_c], writes=[t_ssq])
                S.op("dve", lambda e: e.reciprocal(out=ssq[:, 2:4], in_=ssq[:, 2:4]), reads=[t_ssq], writes=[t_ssq])
                for g in range(2):
                    S.op("dve", lambda e: e.scalar_tensor_tensor(out=ob[:, g * 192:(g + 1) * 192], in0=acc[:, g * 192:(g + 1) * 192], scalar=ssq[:, 2 + g:3 + g],
                                                                 in1=nwb[:, g * 192:(g + 1) * 192], op0=ALU.mult, op1=ALU.mult), reads=[t_acc, t_ssq, t_db], writes=[t_ob])
                for c3 in range(3):
                    S.op("pe", lambda e: e.transpose(out=ptr[:, c3, :], in_=ob[:, c3 * 128:(c3 + 1) * 128], identity=G.identB), reads=[t_ob, G.t_c], writes=[t_ptr])
                S.op("act", lambda e: e.copy(out=oT[:, b], in_=ptr[:]), reads=[t_ptr], writes=[t_oT[b]])
                S.dma(G.mixT[384:768, tc0:tc0 + 128].rearrange("(c p) t -> p c t", p=128), oT[:, b], reads=[t_oT[b]], writes=[G.t_mix])
        if "ssd" in G.dbg and li == 0:
            dump_bf16(G, G.mixT[384:512, 256:768], G.dbg["ssd"], [G.t_mix])


HY0 = 1676
SEGS = {"lat": dict(L=4096, NC=32, KC=17, off=NCTX), "ctx": dict(L=256, NC=2, KC=2, off=0)}


def hyena_inproj(G, li):
    nc, S, I = G.nc, G.S, G.I
    hT = G.hT
    need_ctx = li < DEPTH - 1
    wv32 = I["w_in"][li].rearrange("(k p) c -> p k c", p=128)
    with ExitStack() as _es:
        wst = _es.enter_context(SB(nc, "wst", [128, 8, 128], F32))
        cwb = _es.enter_context(SB(nc, "cwb", [128, 3, 128], F32))
        wj = _es.enter_context(SB(nc, "wjh", [128, 3, 8, 768], BF16))
        hb = _es.enter_context(SB(nc, "hb", [128, 768], F32))
        hv = _es.enter_context(SB(nc, "hv", [128, 2, 768], BF16))
        ph = _es.enter_context(PS(nc, "ph", [128, 2, 512], F32))
        t_wst, t_cwb, t_wj, t_hb, t_hv, t_ph = Tok(), Tok(), Tok(), Tok(), [Tok(), Tok()], Tok()
        S.dma(hb[:], I["hy_conv_b"][li].partition_broadcast(128), writes=[t_hb])
        for cc in range(6):
            S.dma(wst[:], wv32[:, :, HY0 + cc * 128:HY0 + (cc + 1) * 128], writes=[t_wst])
            for j in range(3):
                S.dma(cwb[:, j, :], I["hy_conv_w"][li, j, cc * 128:(cc + 1) * 128].partition_broadcast(128), writes=[t_cwb])
            for j in range(3):
                S.op("dve", lambda e: e.tensor_tensor(out=wj[:, j, :, cc * 128:(cc + 1) * 128], in0=wst[:],
                                                      in1=cwb[:, j:j + 1, :].to_broadcast([128, 8, 128]), op=ALU.mult),
                     reads=[t_wst, t_cwb], writes=[t_wj])
        dv = G.hyv.rearrange("m t c -> t m c")
        for tl in range(NT):
            if tl < 2 and not need_ctx:
                continue
            col = colof(tl)
            b = tl % 2
            for half in range(2):
                for j in range(3):
                    for k in range(8):
                        S.op("pe", lambda e: e.matmul(ph[:, half, 0:384], lhsT=hT[:, k, col + j - 1:col + j - 1 + 128],
                                                      rhs=wj[:, j, k, half * 384:(half + 1) * 384], start=(j == 0 and k == 0), stop=(j == 2 and k == 7)),
                             reads=[t_wj, G.t_hT], writes=[t_ph])
            S.op("dve", lambda e: e.tensor_tensor(out=hv[:, b, :].rearrange("p (a c) -> p a c", a=2), in0=ph[:, :, 0:384],
                                                  in1=hb[:].rearrange("p (a c) -> p a c", a=2), op=ALU.add), reads=[t_ph, t_hb], writes=[t_hv[b]])
            S.dma(dv[tl * 128:(tl + 1) * 128], hv[:, b, :].rearrange("p (m c) -> p m c", m=3), reads=[t_hv[b]], writes=[G.t_hyv])


def hyena_fft(G, li):
    nc, S, I = G.nc, G.S, G.I
    need_ctx = li < DEPTH - 1
    for sname in (("lat", "ctx") if need_ctx else ("lat",)):
        P = SEGS[sname]
        L, NCk, KC, off = P["L"], P["NC"], P["KC"], P["off"]
        NH = NCk // 2
        FT, IT, FE, WIN, MH = I["ft_" + sname], I["it_" + sname], I["fe_" + sname], I["win_" + sname], I["mh_" + sname]
        Kf = G.Kf[sname]
        t_kf = Tok()
        with ExitStack() as _es:
            hk = _es.enter_context(SB(nc, "hk", [128, NCk, 1024], BF16))
            fe = _es.enter_context(SB(nc, "fe", [33, L], F32))
            h1 = _es.enter_context(SB(nc, "h1", [64, L], F32))
            h2 = _es.enter_context(SB(nc, "h2", [64, L], F32))
            w1 = _es.enter_context(SB(nc, "w1", [33, 64], F32))
            w2 = _es.enter_context(SB(nc, "w2", [64, 64], F32))
            w3 = _es.enter_context(SB(nc, "w3", [64, 1024], F32))
            pre = _es.enter_context(SB(nc, "pre", [64, 512], F32))
            pr2 = _es.enter_context(SB(nc, "pr2", [64, 512], F32))
            t_pr2 = Tok()
            MAGIC = 1.5 * 2 ** 23
            wint = _es.enter_context(SB(nc, "wint", [128, 2, 2, 256], F32))
            hbias = _es.enter_context(SB(nc, "hbias", [128, 512], F32))
            mh = _es.enter_context(SB(nc, "mh", [128, KC], F32))
            slab = _es.enter_context(SB(nc, "slab", [128, 2, NCk, 2, 128], BF16))
            xo = _es.enter_context(SB(nc, "xo", [128, 2, 512], F32))
            sd = _es.enter_context(SB(nc, "sd", [128, 2, 2, 2, 512], F32))
            kf = _es.enter_context(SB(nc, "kf", [128, 2, 2, 2, 256], BF16))
            pm = _es.enter_context(PS(nc, "pm", [64, 512], F32))
            phh = _es.enter_context(PS(nc, "phh", [128, 2, 512], F32))
            psk = _es.enter_context(PS(nc, "psk", [128, 2, 2, 512], F32))
            t_hk, t_fe, t_h1, t_h2, t_w, t_pre, t_win, t_slab, t_eo, t_sd, t_kft, t_pm, t_phh, t_psk = \
                Tok(), Tok(), Tok(), Tok(), Tok(), Tok(), [Tok(), Tok()], [Tok(), Tok()], Tok(), Tok(), Tok(), Tok(), Tok(), Tok()
            S.dma(fe[:], FE[:, :], writes=[t_fe])
            S.dma(w1[:], I["hy_w1"][li], writes=[t_w])
            S.dma(w2[:], I["hy_w2"][li], writes=[t_w])
            S.dma(w3[:], I["hy_w3"][li], writes=[t_w])
            S.dma(hbias[:], I["hy_bias"][li].rearrange("o c -> (o c)").partition_broadcast(128), writes=[t_w])
            S.dma(mh[:], MH[:, :], writes=[t_w])
            ob1, ofr, ob2 = COLS["hy_b1"][0], COLS["hy_freq"][0], COLS["hy_b2"][0]
            for (src, t_src, wt, kk, bcol, dst, t_dst) in ((fe, t_fe, w1, 33, ob1, h1, t_h1), (h1, t_h1, w2, 64, ob2, h2, t_h2)):
                for c0 in range(0, L, 512):
                    n = min(512, L - c0)
                    S.op("pe", lambda e: e.matmul(pm[:, 0:n], lhsT=wt[0:kk, :], rhs=src[0:kk, c0:c0 + n], start=True, stop=True),
                         reads=[t_w, t_src], writes=[t_pm])
                    S.op("dve", lambda e: e.tensor_scalar(out=pre[:, 0:n], in0=pm[:, 0:n], scalar1=G.cols[0:64, bcol:bcol + 1],
                                                          scalar2=G.cols[0:64, ofr:ofr + 1], op0=ALU.add, op1=ALU.mult),
                         reads=[t_pm, G.t_cols], writes=[t_pre])
                    S.op("dve", lambda e: e.tensor_scalar(out=pr2[:, 0:n], in0=pre[:, 0:n], scalar1=1.0 / (2.0 * math.pi), scalar2=MAGIC, op0=ALU.mult, op1=ALU.add),
                         reads=[t_pre], writes=[t_pr2])
                    S.op("dve", lambda e: e.tensor_scalar(out=pr2[:, 0:n], in0=pr2[:, 0:n], scalar1=-MAGIC, scalar2=None, op0=ALU.add),
                         reads=[t_pr2], writes=[t_pr2])
                    S.op("dve", lambda e: e.scalar_tensor_tensor(out=pre[:, 0:n], in0=pr2[:, 0:n], scalar=-2.0 * math.pi, in1=pre[:, 0:n], op0=ALU.mult, op1=ALU.add),
                         reads=[t_pr2, t_pre], writes=[t_pre])
                    S.op("act", lambda e: e.activation(out=dst[:, c0:c0 + n], in_=pre[:, 0:n], func=AF.Sin),
                         reads=[t_pre], writes=[t_dst])
            for c in range(NCk):
                b = c % 2
                S.dma(wint[:, b], WIN[c], writes=[t_win[b]])
                for half in range(2):
                    S.op("pe", lambda e: e.matmul(phh[:, half, :], lhsT=h2[:, c * 128:(c + 1) * 128], rhs=w3[:, half * 512:(half + 1) * 512], start=True, stop=True),
                         reads=[t_h2, t_w], writes=[t_phh])
                for dr in range(2):
                    S.op("dve", lambda e: e.tensor_tensor(out=hk[:, c, dr * 512:(dr + 1) * 512].rearrange("p (o c) -> p o c", o=2),
                                                          in0=phh[:, dr, :].rearrange("p (o c) -> p o c", o=2),
                                                          in1=wint[:, b, dr:dr + 1, :].to_broadcast([128, 2, 256]), op=ALU.mult),
                         reads=[t_phh, t_win[b]], writes=[t_hk])
            for kc in range(KC):
                b = kc % 2
                S.dma(slab[:, b], FT[kc], writes=[t_slab[b]])
                for dr in range(2):
                    for eo_ in range(2):
                        for ri in range(2):
                            for c in range(NH):
                                cc = eo_ * NH + c
                                S.op("pe", lambda e: e.matmul(psk[:, eo_, ri, :], lhsT=slab[:, b, cc, ri, :], rhs=hk[:, cc, dr * 512:(dr + 1) * 512],
                                                              start=(c == 0), stop=(c == NH - 1)), reads=[t_slab[b], t_hk], writes=[t_psk])
                    S.op("act", lambda e: e.copy(out=xo[:], in_=psk[:, 1]), reads=[t_psk], writes=[t_eo])
                    S.op("dve", lambda e: e.tensor_tensor(out=sd[:, dr, 0], in0=psk[:, 0], in1=xo[:], op=ALU.add), reads=[t_psk, t_eo], writes=[t_sd])
                    S.op("dve", lambda e: e.tensor_tensor(out=sd[:, dr, 1], in0=psk[:, 0], in1=xo[:], op=ALU.subtract), reads=[t_psk, t_eo], writes=[t_sd])
                v = lambda ap: ap.rearrange("p (o c) -> p o c", o=2)
                S.op("dve", lambda e: e.tensor_tensor(out=sd[:, 0, 0, 0, :], in0=sd[:, 0, 0, 0, :], in1=hbias[:], op=ALU.add), reads=[t_sd, t_w], writes=[t_sd])
                S.op("dve", lambda e: e.tensor_tensor(out=sd[:, 0, 1, 0, :], in0=sd[:, 0, 1, 0, :], in1=hbias[:], op=ALU.add), reads=[t_sd, t_w], writes=[t_sd])
                S.op("dve", lambda e: e.tensor_tensor(out=kf[:, :, 0, 0, :], in0=v(sd[:, 0, 0, 0, :]), in1=v(sd[:, 1, 0, 0, :]), op=ALU.add), reads=[t_sd], writes=[t_kft])
                S.op("dve", lambda e: e.tensor_tensor(out=kf[:, :, 0, 1, :], in0=v(sd[:, 0, 0, 1, :]), in1=v(sd[:, 1, 0, 1, :]), op=ALU.subtract), reads=[t_sd], writes=[t_kft])
                S.op("dve", lambda e: e.tensor_tensor(out=v(xo[:, 0, :]), in0=v(sd[:, 0, 1, 0, :]), in1=v(sd[:, 1, 1, 0, :]), op=ALU.add), reads=[t_sd], writes=[t_eo])
                S.op("dve", lambda e: e.tensor_tensor(out=v(xo[:, 1, :]), in0=v(sd[:, 1, 1, 1, :]), in1=v(sd[:, 0, 1, 1, :]), op=ALU.subtract), reads=[t_sd], writes=[t_eo])
                S.op("dve", lambda e: e.tensor_scalar(out=kf[:, :, 1, 0, :], in0=v(xo[:, 0, :]), scalar1=mh[:, kc:kc + 1], scalar2=None, op0=ALU.mult),
                     reads=[t_eo, t_w], writes=[t_kft])
                S.op("dve", lambda e: e.tensor_scalar(out=kf[:, :, 1, 1, :], in0=v(xo[:, 1, :]), scalar1=mh[:, kc:kc + 1], scalar2=None, op0=ALU.mult),
                     reads=[t_eo, t_w], writes=[t_kft])
                S.dma(Kf[:, kc].rearrange("o p l r c -> p o l r c"), kf[:], reads=[t_kft], writes=[t_kf])
            S.barrier()
        with ExitStack() as _es:
            vt = _es.enter_context(SB(nc, "vt", [128, NCk, 256], BF16))
            zz1 = _es.enter_context(SB(nc, "zz1", [128, NCk, 256], BF16))
            Y = _es.enter_context(SB(nc, "Y", [128, 2, KC, 2, 256], BF16))
            fsl = _es.enter_context(SB(nc, "fsl", [128, 2, NCk, 2, 128], BF16))
            isl = _es.enter_context(SB(nc, "isl", [128, 2, KC, 2, 128], BF16))
            kft = _es.enter_context(SB(nc, "kft", [128, 2, 2, 2, 256], BF16))
            xo = _es.enter_context(SB(nc, "xo", [128, 2, 256], F32))
            xs_ = _es.enter_context(SB(nc, "xs_", [128, 2, 2, 256], F32))
            ta = _es.enter_context(SB(nc, "ta", [128, 4, 256], F32))
            yl = _es.enter_context(SB(nc, "yl", [128, 2, 2, 256], F32))
            xg = _es.enter_context(SB(nc, "xg", [128, 2, 256], BF16))
            zt = _es.enter_context(SB(nc, "zt", [128, 256], BF16))
            zT = _es.enter_context(SB(nc, "zT", [128, 2, 2, 256], BF16))
            psx = _es.enter_context(PS(nc, "psx", [128, 2, 4, 256], F32))
            psy = _es.enter_context(PS(nc, "psy", [128, 2, 512], F32))
            ptr = _es.enter_context(PS(nc, "ptr", [128, 2, 128], BF16))
            t_vt, t_zz1, t_Y, t_fsl, t_isl, t_kft2, t_ta, t_xg, t_zt, t_zT, t_psx, t_psy, t_ptr, t_xo, t_xs, t_yl = \
                Tok(), Tok(), Tok(), [Tok(), Tok()], [Tok(), Tok()], [Tok(), Tok()], Tok(), [Tok(), Tok()], Tok(), [Tok(), Tok()], [Tok(), Tok()], [Tok(), Tok()], Tok(), Tok(), Tok(), Tok()
            hsrc = lambda m: G.hyv[m, off:off + L, :].rearrange("(c p two) ch -> two p c ch", p=128, two=2)
            for par in range(2):
                S.dma(vt[:, par * NH:(par + 1) * NH, :], hsrc(0)[par], reads=[G.t_hyv], writes=[t_vt])
            mo = G.mixT[768:1024, :].rearrange("(c p) t -> p c t", p=128)
            for order in range(2):
                src, t_src = (vt, t_vt) if order == 0 else (zz1, t_zz1)
                for kc in range(KC):
                    b = kc % 2
                    S.dma(fsl[:, b], FT[kc], writes=[t_fsl[b]])
                    S.dma(kft[:, b], Kf[order, kc], reads=[t_kf], writes=[t_kft2[b]])
                    for eo_ in range(2):
                        for ri in range(2):
                            for c in range(NH):
                                cc = eo_ * NH + c
                                S.op("pe", lambda e: e.matmul(psx[:, b, eo_ * 2 + ri, :], lhsT=fsl[:, b, cc, ri, :], rhs=src[:, cc, :], start=(c == 0), stop=(c == NH - 1)),
                                     reads=[t_fsl[b], t_src], writes=[t_psx[b]])
                    S.op("act", lambda e: e.copy(out=xo[:], in_=psx[:, b, 2:4, :]), reads=[t_psx[b]], writes=[t_xo])
                    S.op("dve", lambda e: e.tensor_tensor(out=xs_[:, 0], in0=psx[:, b, 0:2, :], in1=xo[:], op=ALU.add), reads=[t_psx[b], t_xo], writes=[t_xs])
                    S.op("dve", lambda e: e.tensor_tensor(out=xs_[:, 1, 0, :], in0=psx[:, b, 0, :], in1=xo[:, 0, :], op=ALU.subtract), reads=[t_psx[b], t_xo], writes=[t_xs])
                    S.op("dve", lambda e: e.scalar_tensor_tensor(out=xs_[:, 1, 1, :], in0=psx[:, b, 1, :], scalar=-1.0, in1=xo[:, 1, :], op0=ALU.mult, op1=ALU.add),
                         reads=[t_psx[b], t_xo], writes=[t_xs])
                    S.op("dve", lambda e: e.tensor_tensor(out=ta[:, 0:2, :], in0=xs_[:, :, 0, :], in1=kft[:, b, :, 0, :], op=ALU.mult), reads=[t_xs, t_kft2[b]], writes=[t_ta])
                    S.op("pool", lambda e: e.tensor_tensor(out=ta[:, 2:4, :], in0=xs_[:, :, 1, :], in1=kft[:, b, :, 1, :], op=ALU.mult), reads=[t_xs, t_kft2[b]], writes=[t_ta])
                    S.op("dve", lambda e: e.tensor_tensor(out=yl[:, :, 0, :], in0=ta[:, 0:2, :], in1=ta[:, 2:4, :], op=ALU.subtract), reads=[t_ta], writes=[t_yl])
                    S.op("dve", lambda e: e.tensor_tensor(out=ta[:, 0:2, :], in0=xs_[:, :, 0, :], in1=kft[:, b, :, 1, :], op=ALU.mult), reads=[t_xs, t_kft2[b], t_yl], writes=[t_ta])
                    S.op("pool", lambda e: e.tensor_tensor(out=ta[:, 2:4, :], in0=xs_[:, :, 1, :], in1=kft[:, b, :, 0, :], op=ALU.mult), reads=[t_xs, t_kft2[b], t_yl], writes=[t_ta])
                    S.op("dve", lambda e: e.tensor_tensor(out=yl[:, :, 1, :], in0=ta[:, 0:2, :], in1=ta[:, 2:4, :], op=ALU.add), reads=[t_ta], writes=[t_yl])
                    S.op("dve", lambda e: e.tensor_tensor(out=Y[:, 0, kc, 0, :], in0=yl[:, 0, 0, :], in1=yl[:, 1, 0, :], op=ALU.add), reads=[t_yl], writes=[t_Y])
                    S.op("pool", lambda e: e.tensor_tensor(out=Y[:, 0, kc, 1, :], in0=yl[:, 0, 1, :], in1=yl[:, 1, 1, :], op=ALU.subtract), reads=[t_yl], writes=[t_Y])
                    S.op("dve", lambda e: e.tensor_tensor(out=Y[:, 1, kc, 0, :], in0=yl[:, 0, 0, :], in1=yl[:, 1, 0, :], op=ALU.subtract), reads=[t_yl], writes=[t_Y])
                    S.op("pool", lambda e: e.tensor_tensor(out=Y[:, 1, kc, 1, :], in0=yl[:, 0, 1, :], in1=yl[:, 1, 1, :], op=ALU.add), reads=[t_yl], writes=[t_Y])
                oi = 0
                for c2 in range(NH):
                    for par in range(2):
                        cc = par * NH + c2
                        b = oi % 2
                        oi += 1
                        zb = c2 % 2
                        S.dma(isl[:, b], IT[cc], writes=[t_isl[b]])
                        S.dma(xg[:, b], hsrc(1 + order)[par, :, c2, :], reads=[G.t_hyv], writes=[t_xg[b]])
                        for kc in range(KC):
                            for ri in range(2):
                                S.op("pe", lambda e: e.matmul(psy[:, b, 0:256], lhsT=isl[:, b, kc, ri, :], rhs=Y[:, par, kc, ri, :],
                                                              start=(kc == 0 and ri == 0), stop=(kc == KC - 1 and ri == 1)), reads=[t_isl[b], t_Y], writes=[t_psy[b]])
                        if order == 0:
                            S.op("dve", lambda e: e.tensor_tensor(out=zz1[:, cc, :], in0=psy[:, b, 0:256], in1=xg[:, b, :], op=ALU.mult),
                                 reads=[t_psy[b], t_xg[b]], writes=[t_zz1])
                        else:
                            S.op("dve", lambda e: e.tensor_tensor(out=zt[:], in0=psy[:, b, 0:256], in1=xg[:, b, :], op=ALU.mult),
                                 reads=[t_psy[b], t_xg[b]], writes=[t_zt])
                            for hh in range(2):
                                S.op("pe", lambda e: e.transpose(out=ptr[:, hh, :], in_=zt[:, hh * 128:(hh + 1) * 128], identity=G.identB),
                                     reads=[t_zt, G.t_c], writes=[t_ptr])
                            S.op("act", lambda e: e.copy(out=zT[:, zb].rearrange("p h (t two) -> p h t two", two=2)[:, :, :, par], in_=ptr[:]),
                                 reads=[t_ptr], writes=[t_zT[zb]])
                            if par == 1:
                                S.dma(mo[:, :, off + c2 * 256:off + (c2 + 1) * 256], zT[:, zb], reads=[t_zT[zb]], writes=[G.t_mix])
            S.barrier()
    if "hy" in G.dbg and li == 0:
        dump_bf16(G, G.mixT[768:896, 256:768], G.dbg["hy"], [G.t_mix])


def _hy_tables(L):
    N = 2 * L
    NCk = L // 128
    KC = (L // 2 + 1 + 127) // 128
    perm = np.concatenate([np.arange(0, L, 2), np.arange(1, L, 2)])
    n = perm.astype(np.int64)
    k = np.arange(KC * 128, dtype=np.int64)
    ang = ((n[:, None] * k[None, :]) % N).astype(np.float64) * (2 * np.pi / N)
    valid = (k <= L // 2).astype(np.float64)
    w = np.where(k == 0, 1.0, 2.0) / N * valid
    c, s_ = np.cos(ang), np.sin(ang)
    ft = np.stack([c * valid, -s_ * valid], axis=0)
    ft = ft.reshape(2, NCk, 128, KC, 128).transpose(3, 2, 1, 0, 4)
    it = np.stack([c * w, -s_ * w], axis=0)
    it = it.reshape(2, NCk, 128, KC, 128).transpose(1, 4, 3, 0, 2)
    f = np.float32
    nn = np.arange(L, dtype=f)
    t = nn / f(max(L - 1, 1))
    bands = np.linspace(1e-4, 15, 16, dtype=f)
    wpos = (f(2 * math.pi / L) * nn).astype(f)
    feats = np.concatenate([t[:, None], np.cos(wpos[:, None] * bands), -np.sin(wpos[:, None] * bands)], axis=-1).astype(f)
    deltas = np.abs(np.linspace(math.log(1e-2) / 1.5, math.log(1e-2) / 0.3, 256, dtype=f))
    win = np.exp(-t[:, None] * deltas).astype(f)
    winb = win.copy()
    winb[0] = 0.0
    feats = feats[perm]
    wn = np.stack([win, winb], axis=1)[perm].reshape(NCk, 128, 2, 256)
    mh = ((k != L // 2) & (k <= L // 2)).astype(f).reshape(KC, 128).T
    bf = ml_dtypes.bfloat16
    return (np.ascontiguousarray(ft).astype(bf), np.ascontiguousarray(it).astype(bf),
            np.ascontiguousarray(feats.T), np.ascontiguousarray(wn), np.ascontiguousarray(mh))
```

```python
import math
from contextlib import ExitStack
import numpy as np
import ml_dtypes
import concourse.bass as bass
import concourse.mybir as mybir
from concourse.bass_utils import run_bass_kernel_spmd

F32 = mybir.dt.float32
BF16 = mybir.dt.bfloat16
AF = mybir.ActivationFunctionType
ALU = mybir.AluOpType
AX = mybir.AxisListType

D = 1024
NCTX = 256
NLAT = 4096
NTOK = NCTX + NLAT
NT = NTOK // 128
DEPTH = 2
D_IN = 2444
EPS = 1e-6
HC = NTOK + 3
NE = 16
DFF = 256
DEBUG = False


def colof(tile):
    return 1 + 128 * tile if tile < 2 else 258 + 128 * (tile - 2)


BLOCKS = [(1, 0, 256, 0, 2)] + [(258 + 512 * j, 256 + 512 * j, 512, 2 + 4 * j, 4) for j in range(8)]


class Tok:
    __slots__ = ("w", "r")

    def __init__(self):
        self.w = None
        self.r = {}


class Sched:
    def __init__(self, nc, ndma=8, same_engine_sync=True):
        self.nc = nc
        self.eng = {"pe": nc.tensor, "act": nc.scalar, "dve": nc.vector, "pool": nc.gpsimd, "sp": nc.sync}
        self.semh = {}
        self.cnt = {}
        self.seen = {k: {} for k in self.eng}
        self.same = same_engine_sync
        for k in self.eng:
            self.semh[k] = nc.alloc_semaphore("s_" + k)
            self.cnt[k] = 0
        self.ndma = ndma
        self.dslot = {}
        self.dval = {}
        for q in ("sp", "pool"):
            self.dslot[q] = 0
            for i in range(ndma):
                key = ("dma", q, i)
                self.semh[key] = nc.alloc_semaphore("d_%s_%d" % (q, i))
                self.dval[key] = 0
        self.ninst = 0

    def _wait(self, e, deps):
        for (k, v) in sorted(deps, key=str):
            if k == e and (e == "pe" or not self.same):
                continue
            if self.seen[e].get(k, 0) >= v:
                continue
            self.eng[e].wait_ge(self.semh[k], v)
            self.seen[e][k] = v

    @staticmethod
    def _deps(reads, writes):
        deps = set()
        for t in reads:
            if t.w is not None:
                deps.add(t.w)
        for t in writes:
            if t.w is not None:
                deps.add(t.w)
            for kv in t.r.items():
                deps.add(kv)
        return deps

    @staticmethod
    def _mark(ev, reads, writes):
        k, v = ev
        for t in reads:
            if t.r.get(k, 0) < v:
                t.r[k] = v
        for t in writes:
            t.w = ev
            t.r = {}

    def op(self, e, fn, reads=(), writes=()):
        self._wait(e, self._deps(reads, writes))
        ins = fn(self.eng[e])
        self.cnt[e] += 1
        ins.then_inc(self.semh[e], 1)
        self._mark((e, self.cnt[e]), reads, writes)
        self.ninst += 1
        return ins

    def dma(self, out, in_, reads=(), writes=(), q="sp", **kw):
        i = self.dslot[q]
        self.dslot[q] = (i + 1) % self.ndma
        key = ("dma", q, i)
        deps = self._deps(reads, writes)
        if self.dval[key] > 0:
            deps.add((key, self.dval[key]))
        self._wait(q, deps)
        ins = self.eng[q].dma_start(out=out, in_=in_, **kw)
        self.dval[key] += 16
        ins.then_inc(self.semh[key], 16)
        self._mark((key, self.dval[key]), reads, writes)
        self.ninst += 1
        return ins

    def barrier(self):
        deps = set()
        for key, v in self.dval.items():
            if v > 0:
                deps.add((key, v))
        for k in self.eng:
            if self.cnt[k] > 0:
                deps.add((k, self.cnt[k]))
        for e in self.eng:
            self._wait(e, deps)


class Ctx:
    pass


_UID = [0]


def SB(nc, name, shape, dt):
    _UID[0] += 1
    return nc.sbuf_tensor("%s_%d" % (name, _UID[0]), shape, dt)


def PS(nc, name, shape, dt):
    _UID[0] += 1
    return nc.psum_tensor("%s_%d" % (name, _UID[0]), shape, dt)


def build(dbg=None):
    nc = bass.Bass("TRN2", target_bir_lowering=False)
    S = Sched(nc)
    G = Ctx()
    G.nc, G.S = nc, S

    def din(name, shape, dt=F32):
        return nc.dram_tensor(name, list(shape), dt, kind="ExternalInput").ap()

    def dscr(name, shape, dt):
        return nc.dram_tensor(name, list(shape), dt, kind="Internal").ap()

    I = {}
    I["x"] = din("x", [NLAT, D])
    I["ctx"] = din("ctx", [NCTX, D])
    I["w_mod"] = din("w_mod", [DEPTH, D, 6 * D])
    I["b_mod"] = din("b_mod", [DEPTH, 6 * D])
    I["w_in"] = din("w_in", [DEPTH, D, D_IN])
    I["w_out"] = din("w_out", [DEPTH, D, D])
    I["w_router"] = din("w_router", [D, NE])
    I["router_bias"] = din("router_bias", [NE])
    I["w_gate"] = din("w_gate", [DEPTH, NE, D, DFF])
    I["w_up"] = din("w_up", [DEPTH, NE, D, DFF])
    I["w_down"] = din("w_down", [DEPTH, NE, DFF, D])
    I["g_final"] = din("g_final", [D])
    I["cols"] = din("cols", [DEPTH, 128, NCOLS])
    I["cmat"] = din("cmat", [9, 128, 128])
    I["rope"] = din("rope", [2, 128, NLAT])
    for sname, P in SEGS.items():
        I["ft_" + sname] = din("ft_" + sname, [P["KC"], 128, P["NC"], 2, 128], BF16)
        I["it_" + sname] = din("it_" + sname, [P["NC"], 128, P["KC"], 2, 128], BF16)
        I["fe_" + sname] = din("fe_" + sname, [33, P["L"]])
        I["win_" + sname] = din("win_" + sname, [P["NC"], 128, 2, 256])
        I["mh_" + sname] = din("mh_" + sname, [128, P["KC"]])
    for nm, shp in (("hy_conv_w", [DEPTH, 3, 768]), ("hy_conv_b", [DEPTH, 768]), ("hy_w1", [DEPTH, 33, 64]), ("hy_w2", [DEPTH, 64, 64]),
                    ("hy_w3", [DEPTH, 64, 1024]), ("hy_bias", [DEPTH, 2, 256])):
        I[nm] = din(nm, shp)
    I["ssdmask"] = din("ssdmask", [2, 4, 128, 512], BF16)
    for nm, shp in (("ssd_conv_w", [DEPTH, 3, 640]), ("ssd_dt_bias", [DEPTH, 2, 6]), ("ssd_a_log", [DEPTH, 2, 6]),
                    ("ssd_d", [DEPTH, 6]), ("ssd_norm", [DEPTH, 384])):
        I[nm] = din(nm, shp)
    out = nc.dram_tensor("out", [NLAT, D], F32, kind="ExternalOutput").ap()
    G.I, G.out = I, out
    G.dbg = {}
    if dbg:
        for name, shape in dbg.items():
            G.dbg[name] = nc.dram_tensor("dbg_" + name, list(shape), F32, kind="ExternalOutput").ap()

    G.xres = dscr("xres", [NTOK, D], F32)
    G.t_xres = [Tok() for _ in range(NT)]
    G.wb_in = [dscr("wb_in%d" % i, [D, D_IN], BF16) for i in range(DEPTH)]
    G.wb_out = [dscr("wb_out%d" % i, [D, D], BF16) for i in range(DEPTH)]
    G.wb_gate = [dscr("wb_gate%d" % i, [NE, D, DFF], BF16) for i in range(DEPTH)]
    G.wb_up = [dscr("wb_up%d" % i, [NE, D, DFF], BF16) for i in range(DEPTH)]
    G.wb_down = [dscr("wb_down%d" % i, [NE, DFF, D], BF16) for i in range(DEPTH)]
    G.t_wb = Tok()
    G.mixT = dscr("mixT", [D, NTOK], BF16)
    G.hyv = dscr("hyv", [3, NTOK, 256], BF16)
    G.ssd_yb = dscr("ssd_yb", [NTOK, 384], F32)
    G.t_ssdyb = [Tok() for _ in range(NT)]
    G.t_hyv = Tok()
    G.Kf = {sn: dscr("Kf_" + sn, [2, P["KC"], 128, 2, 2, 256], BF16) for sn, P in SEGS.items()}
    G.t_mix = Tok()

    cm = nc.alloc_sbuf_tensor("cm", [128, 9, 128], F32)
    cmb = nc.alloc_sbuf_tensor("cmb", [128, 9, 128], BF16)
    ones = nc.alloc_sbuf_tensor("ones", [128, 128], F32)
    epsc = nc.alloc_sbuf_tensor("epsc", [128, 1], F32)
    G.t_c = Tok()
    S.dma(cm[:], I["cmat"].rearrange("a p c -> p a c"), writes=[G.t_c])
    S.op("dve", lambda e: e.tensor_copy(out=cmb[:], in_=cm[:]), reads=[G.t_c], writes=[G.t_c])
    S.op("dve", lambda e: e.memset(ones[:], 1.0), writes=[G.t_c])
    S.op("dve", lambda e: e.memset(epsc[:], EPS), writes=[G.t_c])
    G.cm, G.cmb, G.ones, G.epsc = cm, cmb, ones, epsc
    G.negpi = nc.alloc_sbuf_tensor("negpi", [128, 1], F32)
    S.op("dve", lambda e: e.memset(G.negpi[:], -math.pi), writes=[G.t_c])
    G.identF, G.identB = cm[:, 0, :], cmb[:, 0, :]

    S.dma(G.xres[0:NCTX, :], I["ctx"][:, :], writes=G.t_xres[0:2])
    for j in range(4):
        S.dma(G.xres[NCTX + 1024 * j:NCTX + 1024 * (j + 1), :], I["x"][1024 * j:1024 * (j + 1), :],
              writes=G.t_xres[2 + 8 * j:2 + 8 * (j + 1)])

    convert_weights(G)
    S.barrier()
    for li in range(1 if DEBUG else DEPTH):
        layer(G, li)
    S.barrier()
    return nc


def convert_weights(G):
    nc, S, I = G.nc, G.S, G.I
    CH = 4096
    with ExitStack() as _es:
        cf = _es.enter_context(SB(nc, "cv_f", [128, 2, CH], F32))
        cb = _es.enter_context(SB(nc, "cv_b", [128, 2, CH], BF16))
        tf = [Tok(), Tok()]
        tb = [Tok(), Tok()]
        n = 0
        engs = ["dve", "pool", "act"]
        for li in range(DEPTH):
            pairs = [(I["w_in"][li], G.wb_in[li], "a b -> (a b)"), (I["w_out"][li], G.wb_out[li], "a b -> (a b)"),
                     (I["w_gate"][li], G.wb_gate[li], "e a b -> (e a b)"), (I["w_up"][li], G.wb_up[li], "e a b -> (e a b)"),
                     (I["w_down"][li], G.wb_down[li], "e a b -> (e a b)")]
            for src, dst, pat in pairs:
                s1 = src.rearrange(pat).rearrange("(p m) -> p m", p=128)
                d1 = dst.rearrange(pat).rearrange("(p m) -> p m", p=128)
                M = s1.shape[1]
                for c0 in range(0, M, CH):
                    w = min(CH, M - c0)
                    k = n % 2
                    S.dma(cf[:, k, 0:w], s1[:, c0:c0 + w], writes=[tf[k]])
                    en = engs[n % 3]
                    if en == "act":
                        S.op("act", lambda e: e.copy(out=cb[:, k, 0:w], in_=cf[:, k, 0:w]), reads=[tf[k]], writes=[tb[k]])
                    else:
                        S.op(en, lambda e: e.tensor_copy(out=cb[:, k, 0:w], in_=cf[:, k, 0:w]), reads=[tf[k]], writes=[tb[k]])
                    S.dma(d1[:, c0:c0 + w], cb[:, k, 0:w], reads=[tb[k]], writes=[G.t_wb], q="pool")
                    n += 1


COLS = {}
_o = 0
for _name, _n in [("cc", 16), ("bmod", 32), ("g_mix", 8), ("g_ffn", 8), ("ssd_conv_b", 5), ("qg", 1), ("kg", 1),
                  ("ssd_d", 3), ("ssd_norm", 3), ("hy_b1", 1), ("hy_freq", 1), ("hy_b2", 1)]:
    COLS[_name] = (_o, _n)
    _o += _n
NCOLS = _o


def layer(G, li):
    nc, S, I = G.nc, G.S, G.I
    with ExitStack() as _es:
        cols = _es.enter_context(SB(nc, "cols", [128, NCOLS], F32))
        modc = _es.enter_context(SB(nc, "modc", [128, 4, 8, 2], F32))
        gtb = _es.enter_context(SB(nc, "gtb", [128, 2, 2, D], F32))
        G.cols, G.modc, G.gtb = cols, modc, gtb
        G.t_cols, G.t_modc, G.t_gtb = Tok(), Tok(), Tok()
        S.dma(cols[:], I["cols"][li], writes=[G.t_cols])
        adaln(G, li)
        S.barrier()
        with ExitStack() as _es:
            hT = _es.enter_context(SB(nc, "hT", [128, 8, HC], BF16))
            G.hT, G.t_hT = hT, Tok()
            norm_in(G, li)
            S.barrier()
            if "hy" in STAGES:
                hyena_inproj(G, li)
                S.barrier()
            if "att" in STAGES:
                attention(G, li)
                S.barrier()
            if "ssd" in STAGES:
                ssd(G, li)
                S.barrier()
        if "hy" in STAGES:
            hyena_fft(G, li)
            S.barrier()
        if "moe" in STAGES:
            with ExitStack() as _es:
                h2T = _es.enter_context(SB(nc, "h2T", [128, 8, NTOK], BF16))
                rl = _es.enter_context(SB(nc, "rl", [128, NT, NE], F32))
                G.h2T, G.t_h2T, G.rl, G.t_rl = h2T, Tok(), rl, Tok()
                outproj(G, li)
                S.barrier()
                moe(G, li)
                S.barrier()


STAGES = ("att", "ssd", "hy", "moe")


def colap(G, name, j=0, n=1, p0=0, p1=128):
    o, _ = COLS[name]
    return G.cols[p0:p1, o + j:o + j + n]


def adaln(G, li):
    nc, S, I = G.nc, G.S, G.I
    cols, modc, gtb = G.cols, G.modc, G.gtb
    with ExitStack() as _es:
        sc = _es.enter_context(SB(nc, "sc", [128, 8, 2], F32))
        screp = _es.enter_context(SB(nc, "screp", [128, 8, 2, 128], F32))
        wm = _es.enter_context(SB(nc, "wm", [128, 2, 8, 512], F32))
        brow = _es.enter_context(SB(nc, "brow", [128, 2, D], F32))
        ps_a = _es.enter_context(PS(nc, "ps_a", [128, 4, 2], F32))
        ps_g = _es.enter_context(PS(nc, "ps_g", [128, 2, 512], F32))
        t_sc, t_wm, t_pa, t_pg, t_brow = Tok(), [Tok(), Tok()], Tok(), Tok(), Tok()
        o = COLS["cc"][0]
        S.op("act", lambda e: e.activation(out=sc[:].rearrange("p k j -> p (k j)"), in_=cols[:, o:o + 16], func=AF.Silu),
             reads=[G.t_cols], writes=[t_sc])
        S.op("dve", lambda e: e.tensor_copy(out=screp[:].rearrange("p k j c -> p (k j) c"),
                                            in_=sc[:].rearrange("p k j -> p (k j)").unsqueeze(2).to_broadcast([128, 16, 128])),
             reads=[t_sc], writes=[t_sc])
        for g in range(2):
            S.dma(brow[:, g, :], I["b_mod"][li, (2 + 3 * g) * D:(3 + 3 * g) * D].partition_broadcast(128), writes=[t_brow])
        wv = I["w_mod"][li].rearrange("(k p) c -> p k c", p=128)
        ob = COLS["bmod"][0]
        for cj in range(12):
            b = cj % 2
            S.dma(wm[:, b], wv[:, :, cj * 512:(cj + 1) * 512], writes=[t_wm[b]])
            vec = cj // 2
            half = cj % 2
            if vec in (2, 5):
                g = 0 if vec == 2 else 1
                for j in range(2):
                    for kd in range(8):
                        S.op("pe", lambda e: e.matmul(ps_g[:, j, :], lhsT=screp[:, kd, j, :], rhs=wm[:, b, kd, :],
                                                      start=(kd == 0), stop=(kd == 7)), reads=[t_sc, t_wm[b]], writes=[t_pg])
                    S.op("dve", lambda e: e.tensor_tensor(out=gtb[:, g, j, half * 512:(half + 1) * 512], in0=ps_g[:, j, :],
                                                          in1=brow[:, g, half * 512:(half + 1) * 512], op=ALU.add),
                         reads=[t_pg, t_brow], writes=[G.t_gtb])
            else:
                v = {0: 0, 1: 1, 3: 2, 4: 3}[vec]
                for fc in range(4):
                    for kd in range(8):
                        S.op("pe", lambda e: e.matmul(ps_a[:, fc, :], lhsT=wm[:, b, kd, fc * 128:(fc + 1) * 128], rhs=sc[:, kd, :],
                                                      start=(kd == 0), stop=(kd == 7)), reads=[t_sc, t_wm[b]], writes=[t_pa])
                k0 = half * 4
                S.op("dve", lambda e: e.tensor_tensor(out=modc[:, v, k0:k0 + 4, :], in0=ps_a[:],
                                                      in1=cols[:, ob + v * 8 + k0:ob + v * 8 + k0 + 4].unsqueeze(2).to_broadcast([128, 4, 2]),
                                                      op=ALU.add), reads=[t_pa, G.t_cols], writes=[G.t_modc])
        for v, gname in ((1, "g_mix"), (3, "g_ffn")):
            og = COLS[gname][0]
            S.op("dve", lambda e: e.scalar_tensor_tensor(out=modc[:, v], in0=modc[:, v], scalar=1.0,
                                                         in1=cols[:, og:og + 8].unsqueeze(2).to_broadcast([128, 8, 2]),
                                                         op0=ALU.add, op1=ALU.mult), reads=[G.t_modc, G.t_cols], writes=[G.t_modc])


def rms_to_T(G, xt, t_x, tile, vA, vB, dstT, t_dst, dcol, pool):
    nc, S = G.nc, G.S
    sq, ss, xn, ps_t, toks = pool
    t_sq, t_ss, t_xn, t_ps = toks
    j = 1 if tile < 2 else 0
    S.op("act", lambda e: e.activation(out=sq[:], in_=xt, func=AF.Square, accum_out=ss[:, 0:1]), reads=[t_x], writes=[t_sq, t_ss])
    S.op("act", lambda e: e.activation(out=ss[:, 1:2], in_=ss[:, 0:1], func=AF.Sqrt, bias=G.epsc[:], scale=1.0 / D),
         reads=[t_ss, G.t_c], writes=[t_ss])
    S.op("dve", lambda e: e.reciprocal(out=ss[:, 2:3], in_=ss[:, 1:2]), reads=[t_ss], writes=[t_ss])
    S.op("dve", lambda e: e.tensor_scalar(out=xn[:], in0=xt, scalar1=ss[:, 2:3], scalar2=None, op0=ALU.mult),
         reads=[t_x, t_ss], writes=[t_xn])
    for k in range(8):
        S.op("pe", lambda e: e.transpose(out=ps_t[:, k, :], in_=xn[:, k * 128:(k + 1) * 128], identity=G.identB),
             reads=[t_xn, G.t_c], writes=[t_ps])
    S.op("dve", lambda e: e.tensor_tensor(out=sq[:].rearrange("p (k c) -> p k c", k=8), in0=ps_t[:],
                                          in1=G.modc[:, vA, :, j:j + 1].to_broadcast([128, 8, 128]), op=ALU.mult),
         reads=[t_ps, G.t_modc], writes=[t_sq])
    S.op("dve", lambda e: e.tensor_tensor(out=dstT[:, :, dcol:dcol + 128], in0=sq[:].rearrange("p (k c) -> p k c", k=8),
                                          in1=G.modc[:, vB, :, j:j + 1].to_broadcast([128, 8, 128]), op=ALU.add),
         reads=[t_sq, G.t_modc], writes=[t_dst])


def norm_in(G, li):
    nc, S = G.nc, G.S
    hT = G.hT
    with ExitStack() as _es:
        xt = _es.enter_context(SB(nc, "xt", [128, 2, D], F32))
        sq = _es.enter_context(SB(nc, "sq", [128, 2, D], F32))
        ss = _es.enter_context(SB(nc, "ss", [128, 2, 4], F32))
        xn = _es.enter_context(SB(nc, "xn", [128, 2, D], BF16))
        ps_t = _es.enter_context(PS(nc, "ps_t", [128, 2, 8, 128], BF16))
        t_x = [Tok(), Tok()]
        pools = [(sq[:, i, :], ss[:, i, :], xn[:, i, :], ps_t[:, i], (Tok(), Tok(), Tok(), Tok())) for i in range(2)]
        for c in (0, 257, HC - 1):
            S.op("pool", lambda e: e.memset(hT[:, :, c:c + 1], 0.0), writes=[G.t_hT])
        for tile in range(NT):
            b = tile % 2
            S.dma(xt[:, b, :], G.xres[tile * 128:(tile + 1) * 128, :], reads=[G.t_xres[tile]], writes=[t_x[b]])
            rms_to_T(G, xt[:, b, :], t_x[b], tile, 1, 0, hT, G.t_hT, colof(tile), pools[b])
        if "hT" in G.dbg and li == 0:
            with ExitStack() as _es:
                dh = _es.enter_context(SB(nc, "dbgh", [128, 8, 512], F32))
                t = Tok()
                S.op("dve", lambda e: e.tensor_copy(out=dh[:], in_=hT[:, :, 0:512]), reads=[G.t_hT], writes=[t])
                S.dma(G.dbg["hT"].rearrange("(k p) c -> p k c", p=128), dh[:], reads=[t])


def attention(G, li):
    nc, S, I = G.nc, G.S, G.I
    hT = G.hT
    need_ctx = li < DEPTH - 1
    wv = G.wb_in[li].rearrange("(k p) c -> p k c", p=128)
    scale = 64 ** -0.5
    with ExitStack() as _es:
        wq = _es.enter_context(SB(nc, "wqkv", [128, 8, 640], BF16))
        qT = _es.enter_context(SB(nc, "qT", [128, 6, NTOK], BF16))
        kT = _es.enter_context(SB(nc, "kT", [128, NTOK], BF16))
        vp = _es.enter_context(SB(nc, "vp", [128, NT, 2, 128], BF16))
        rp = _es.enter_context(SB(nc, "rp", [128, 2, 2, 512], F32))
        qs = _es.enter_context(SB(nc, "qs", [128, 512], F32))
        q2 = _es.enter_context(SB(nc, "q2", [128, 512], F32))
        qn = _es.enter_context(SB(nc, "qn", [128, 512], F32))
        qnb = _es.enter_context(SB(nc, "qnb", [128, 512], BF16))
        pT = _es.enter_context(SB(nc, "pT", [128, 2, 2, 512], BF16))
        rd = _es.enter_context(SB(nc, "rd", [128, 2, 512], F32))
        ao = _es.enter_context(SB(nc, "ao", [128, 2, 512], BF16))
        ps_q = _es.enter_context(PS(nc, "ps_q", [128, 512], F32))
        ps_r = _es.enter_context(PS(nc, "ps_r", [128, 512], F32))
        ps_s = _es.enter_context(PS(nc, "ps_s", [128, 2, 2, 512], F32))
        ps_o = _es.enter_context(PS(nc, "ps_o", [128, 2, 512], F32))
        t_w, t_q, t_k, t_v, t_rp = Tok(), Tok(), Tok(), Tok(), [Tok(), Tok()]
        t_qs, t_q2, t_qn, t_qnb, t_psq, t_psr = Tok(), Tok(), Tok(), Tok(), Tok(), Tok()
        t_pT, t_pss, t_pso, t_rd, t_ao = [Tok(), Tok()], [Tok(), Tok()], [Tok(), Tok()], [Tok(), Tok()], [Tok(), Tok()]
        for j in range(3):
            S.dma(wq[:, :, j * 128:j * 128 + 64], wv[:, :, j * 64:(j + 1) * 64], reads=[G.t_wb], writes=[t_w])
            S.dma(wq[:, :, j * 128 + 64:(j + 1) * 128], wv[:, :, (3 + j) * 64:(4 + j) * 64], reads=[G.t_wb], writes=[t_w])
        S.dma(wq[:, :, 384:640], wv[:, :, 384:640], reads=[G.t_wb], writes=[t_w])
        S.op("pool", lambda e: e.memset(vp[:, :, :, 64:128], 1.0), writes=[t_v])
        S.op("pool", lambda e: e.memset(qT[64:128, 0:3, :], 0.0), writes=[t_q])
        S.op("pool", lambda e: e.memset(qT[0:64, 3:6, :], 0.0), writes=[t_q])
        og = {0: COLS["qg"][0], 1: COLS["qg"][0], 2: COLS["qg"][0], 3: COLS["kg"][0]}
        for bi, (c0, t0, n, tile0, ntile) in enumerate(BLOCKS):
            if bi > 0:
                b = bi % 2
                S.dma(rp[:, b, :, :], I["rope"][:, :, t0 - NCTX:t0 - NCTX + 512].rearrange("a p c -> p a c"), writes=[t_rp[b]])
            for ch in range(4):
                for k in range(8):
                    S.op("pe", lambda e: e.matmul(ps_q[:, 0:n], lhsT=wq[:, k, ch * 128:(ch + 1) * 128], rhs=hT[:, k, c0:c0 + n],
                                                  start=(k == 0), stop=(k == 7)), reads=[t_w, G.t_hT], writes=[t_psq])
                S.op("act", lambda e: e.copy(out=qs[:, 0:n], in_=ps_q[:, 0:n]), reads=[t_psq], writes=[t_qs])
                S.op("act", lambda e: e.activation(out=q2[:, 0:n], in_=qs[:, 0:n], func=AF.Square), reads=[t_qs], writes=[t_q2])
                S.op("pe", lambda e: e.matmul(ps_r[:, 0:n], lhsT=G.cm[:, 3, :], rhs=q2[:, 0:n], start=True, stop=True),
                     reads=[t_q2, G.t_c], writes=[t_psr])
                S.op("act", lambda e: e.activation(out=q2[:, 0:n], in_=ps_r[:, 0:n], func=AF.Sqrt, bias=G.epsc[:], scale=1.0 / 64),
                     reads=[t_psr, G.t_c], writes=[t_q2])
                S.op("dve", lambda e: e.reciprocal(out=q2[:, 0:n], in_=q2[:, 0:n]), reads=[t_q2], writes=[t_q2])
                S.op("dve", lambda e: e.scalar_tensor_tensor(out=qn[:, 0:n], in0=qs[:, 0:n], scalar=G.cols[:, og[ch]:og[ch] + 1],
                                                             in1=q2[:, 0:n], op0=ALU.mult, op1=ALU.mult),
                     reads=[t_qs, t_q2, G.t_cols], writes=[t_qn])
                t_dst = t_q if ch < 3 else t_k
                halves = [(0, 64, qT[0:64, ch, t0:t0 + n]), (64, 128, qT[64:128, 3 + ch, t0:t0 + n])] if ch < 3 else [(0, 128, kT[:, t0:t0 + n])]
                if bi == 0:
                    for (p0, p1, dst) in halves:
                        S.op("dve", lambda e: e.tensor_copy(out=dst, in_=qn[p0:p1, 0:n]), reads=[t_qn], writes=[t_dst])
                else:
                    b = bi % 2
                    S.op("dve", lambda e: e.tensor_copy(out=qnb[:, 0:n], in_=qn[:, 0:n]), reads=[t_qn], writes=[t_qnb])
                    S.op("pe", lambda e: e.matmul(ps_r[:, 0:n], lhsT=G.cmb[:, 4, :], rhs=qnb[:, 0:n], start=True, stop=True),
                         reads=[t_qnb, G.t_c], writes=[t_psr])
                    S.op("dve", lambda e: e.tensor_tensor(out=qs[:, 0:n], in0=ps_r[:, 0:n], in1=rp[:, b, 1, 0:n], op=ALU.mult),
                         reads=[t_psr, t_rp[b]], writes=[t_qs])
                    S.op("dve", lambda e: e.tensor_tensor(out=qn[:, 0:n], in0=qn[:, 0:n], in1=rp[:, b, 0, 0:n], op=ALU.mult),
                         reads=[t_qn, t_rp[b]], writes=[t_qn])
                    for (p0, p1, dst) in halves:
                        S.op("dve", lambda e: e.tensor_tensor(out=dst, in0=qn[p0:p1, 0:n], in1=qs[p0:p1, 0:n], op=ALU.add),
                             reads=[t_qn, t_qs], writes=[t_dst])
            for tl in range(tile0, tile0 + ntile):
                cc = colof(tl)
                for k in range(8):
                    S.op("pe", lambda e: e.matmul(ps_q[:, 0:128], lhsT=hT[:, k, cc:cc + 128], rhs=wq[:, k, 512:640],
                                                  start=(k == 0), stop=(k == 7)), reads=[t_w, G.t_hT], writes=[t_psq])
                S.op("act", lambda e: e.copy(out=vp[:, tl, :, 0:64], in_=ps_q[:, 0:128].rearrange("p (a d) -> p a d", a=2)),
                     reads=[t_psq], writes=[t_v])
        pairs = []
        oi = 0
        for h in range(6):
            for bi, (c0, t0, n, tile0, ntile) in enumerate(BLOCKS):
                if bi == 0 and not need_ctx:
                    continue
                kcs = list(range(2)) if bi == 0 else list(range(NT))
                for ki in range(0, len(kcs), 2):
                    pairs.append((h, t0, n, kcs[ki], ki == 0, ki + 2 >= len(kcs), oi % 2))
                oi += 1

        def qk(j):
            h, t0, n, kc, first, last, ob = pairs[j]
            pb = (h // 3) * 64
            sb = j % 2
            for u in range(2):
                S.op("pe", lambda e: e.matmul(ps_s[:, sb, u, 0:n], lhsT=kT[:, (kc + u) * 128:(kc + u + 1) * 128],
                                              rhs=qT[:, h, t0:t0 + n], start=True, stop=True),
                     reads=[t_q, t_k], writes=[t_pss[sb]])

        qk(0)
        for j, (h, t0, n, kc, first, last, ob) in enumerate(pairs):
            sb = j % 2
            kv = h // 3
            if j + 1 < len(pairs):
                qk(j + 1)
            S.op("act", lambda e: e.activation(out=pT[:, sb, :, 0:n], in_=ps_s[:, sb, :, 0:n], func=AF.Exp, scale=scale),
                 reads=[t_pss[sb]], writes=[t_pT[sb]])
            for u in range(2):
                S.op("pe", lambda e: e.matmul(ps_o[:, ob, 0:n], lhsT=vp[:, kc + u, kv, :], rhs=pT[:, sb, u, 0:n],
                                              start=(first and u == 0), stop=(last and u == 1)),
                     reads=[t_v, t_pT[sb]], writes=[t_pso[ob]])
            if last:
                S.op("dve", lambda e: e.reciprocal(out=rd[0:64, ob, 0:n], in_=ps_o[64:128, ob, 0:n]), reads=[t_pso[ob]], writes=[t_rd[ob]])
                S.op("dve", lambda e: e.tensor_tensor(out=ao[0:64, ob, 0:n], in0=ps_o[0:64, ob, 0:n], in1=rd[0:64, ob, 0:n], op=ALU.mult),
                     reads=[t_pso[ob], t_rd[ob]], writes=[t_ao[ob]])
                S.dma(G.mixT[h * 64:(h + 1) * 64, t0:t0 + n], ao[0:64, ob, 0:n], reads=[t_ao[ob]], writes=[G.t_mix])
        if "att" in G.dbg and li == 0:
            dump_bf16(G, G.mixT[0:128, 256:768], G.dbg["att"], [G.t_mix])


def dump_bf16(G, src, dst, reads):
    nc, S = G.nc, G.S
    p, n = src.shape
    with ExitStack() as _es:
        a = _es.enter_context(SB(nc, "dmpb", [p, n], BF16))
        b = _es.enter_context(SB(nc, "dmpf", [p, n], F32))
        t = Tok()
        S.dma(a[:], src, reads=reads, writes=[t])
        S.op("dve", lambda e: e.tensor_copy(out=b[:], in_=a[:]), reads=[t], writes=[t])
        S.dma(dst, b[:], reads=[t])
        S.barrier()


def _cols_pack(inp, li, b):
    def colform(v, n):
        return np.ascontiguousarray(v.reshape(n, 128).T)
    parts = {}
    cc = np.zeros((128, 8, 2), np.float32)
    cc[:, :, 0] = colform(inp["c"][b], 8)
    cc[:, :, 1] = colform(inp["c_ctx"], 8)
    parts["cc"] = cc.reshape(128, 16)
    bm = inp["b_mod"][li].reshape(6, 8, 128)
    parts["bmod"] = np.concatenate([bm[v].T for v in (0, 1, 3, 4)], axis=1)
    parts["g_mix"] = colform(inp["g_mix"][li], 8)
    parts["g_ffn"] = colform(inp["g_ffn"][li], 8)
    parts["ssd_conv_b"] = colform(inp["ssd_conv_b"][li], 5)
    parts["qg"] = np.tile(inp["q_norm"][li], 2)[:, None]
    parts["kg"] = np.tile(inp["k_norm"][li], 2)[:, None]
    parts["ssd_d"] = colform(np.repeat(inp["ssd_d"][li], 64), 3)
    parts["ssd_norm"] = colform(inp["ssd_norm"][li], 3)
    for nm in ("hy_b1", "hy_freq", "hy_b2"):
        parts[nm] = np.tile(inp[nm][li], 2)[:, None]
    out = np.zeros((128, NCOLS), np.float32)
    for nm, (o, n) in COLS.items():
        out[:, o:o + n] = parts[nm]
    return out


def _consts():
    ident = np.eye(128, dtype=np.float32)
    s = np.arange(128)
    U = (s[:, None] <= s[None, :]).astype(np.float32)
    Lo = (s[:, None] >= s[None, :]).astype(np.float32)
    bo = np.kron(np.eye(2, dtype=np.float32), np.ones((64, 64), np.float32))
    rot = np.zeros((128, 128), np.float32)
    for hb in (0, 64):
        for d in range(32):
            rot[hb + d + 32, hb + d] = -1.0
            rot[hb + d, hb + d + 32] = 1.0
    top = np.zeros((128, 128), np.float32); top[:64] = 1.0
    bot = np.zeros((128, 128), np.float32); bot[64:] = 1.0
    cmat = np.stack([ident, U, Lo, bo, rot, top, bot, U - ident, Lo - ident])
    rows = NLAT // 64
    row = np.repeat(np.arange(rows), 64).astype(np.float32)
    col = np.tile(np.arange(64), rows).astype(np.float32)
    inv = (10000.0 ** (-np.arange(0, 32, 2, dtype=np.float32) / 32)).astype(np.float32)
    ang = np.concatenate([row[:, None] * inv, col[:, None] * inv], axis=-1).astype(np.float32)
    cs = np.cos(ang).astype(np.float32).T
    sn = np.sin(ang).astype(np.float32).T
    rope = np.stack([np.tile(cs, (4, 1)), np.tile(sn, (4, 1))]).astype(np.float32)
    tt = np.arange(512)[None, None, :]
    ss_ = np.arange(128)[None, :, None]
    jj = np.arange(4)[:, None, None]
    mf = (tt >= 128 * jj + ss_).astype(np.float32)
    mb = (tt <= 128 * jj + ss_).astype(np.float32)
    hyt = {}
    for sname, P in SEGS.items():
        ft, it_, fe, wn, mh = _hy_tables(P["L"])
        hyt["ft_" + sname], hyt["it_" + sname], hyt["fe_" + sname], hyt["win_" + sname], hyt["mh_" + sname] = ft, it_, fe, wn, mh
    return {**hyt, "cmat": cmat, "rope": rope, "ssdmask": np.stack([mf, mb]).astype(ml_dtypes.bfloat16)}


_CONSTS = None


def kernel(**inp):
    global _CONSTS
    inp = {k: np.asarray(v) for k, v in inp.items()}
    if _CONSTS is None:
        _CONSTS = _consts()
    dbg = kernel.dbg if hasattr(kernel, "dbg") else None
    nc = build(dbg)
    ncores = 8
    in_maps = []
    for core in range(ncores):
        b = core % 4
        m = {"x": np.ascontiguousarray(inp["x"][b]), "ctx": np.ascontiguousarray(inp["ctx"][b])}
        for k in ("w_mod", "b_mod", "w_in", "w_out", "w_router", "router_bias", "w_gate", "w_up", "w_down", "g_final",
                  "ssd_conv_w", "ssd_dt_bias", "ssd_a_log", "ssd_d", "ssd_norm", "hy_conv_w", "hy_conv_b", "hy_w1", "hy_w2", "hy_w3", "hy_bias"):
            m[k] = inp[k]
        m["cols"] = np.stack([_cols_pack(inp, li, b) for li in range(DEPTH)])
        m.update(_CONSTS)
        in_maps.append(m)
    res = run_bass_kernel_spmd(nc, in_maps, core_ids=list(range(ncores)))
    kernel.last = res
    return np.stack([res.results[b]["out"] for b in range(4)]).astype(np.float32)


def outproj(G, li):
    nc, S, I = G.nc, G.S, G.I
    need_ctx = li < DEPTH - 1
    with ExitStack() as _es:
        wo = _es.enter_context(SB(nc, "wo", [128, 8, D], BF16))
        mx = _es.enter_context(SB(nc, "mx", [128, 2, 8, 128], BF16))
        xt = _es.enter_context(SB(nc, "xt", [128, 2, D], F32))
        tmp = _es.enter_context(SB(nc, "tmp", [128, D], F32))
        sq = _es.enter_context(SB(nc, "sq", [128, D], F32))
        ss = _es.enter_context(SB(nc, "ss", [128, 4], F32))
        xn = _es.enter_context(SB(nc, "xn", [128, D], F32))
        h2f = _es.enter_context(SB(nc, "h2f", [128, 8, 128], F32))
        wr = _es.enter_context(SB(nc, "wr", [128, 8, NE], F32))
        po = _es.enter_context(PS(nc, "po", [128, 2, 512], F32))
        pt = _es.enter_context(PS(nc, "pt", [128, 8, 128], F32))
        pr = _es.enter_context(PS(nc, "pr", [128, NE], F32))
        t_wo, t_mx, t_x, t_tmp, t_po = Tok(), [Tok(), Tok()], [Tok(), Tok()], Tok(), Tok()
        t_sq, t_ss, t_xn, t_pt, t_h2f, t_wr, t_pr = Tok(), Tok(), Tok(), Tok(), Tok(), Tok(), Tok()
        S.dma(wo[:], G.wb_out[li].rearrange("(k p) c -> p k c", p=128), reads=[G.t_wb], writes=[t_wo])
        S.dma(wr[:], I["w_router"].rearrange("(k p) c -> p k c", p=128), writes=[t_wr])
        mv = G.mixT.rearrange("(k p) t -> p k t", p=128)
        tiles = [t for t in range(NT) if need_ctx or t >= 2]

        def mm(tile):
            b = tile % 2
            S.dma(mx[:, b], mv[:, :, tile * 128:(tile + 1) * 128], reads=[G.t_mix], writes=[t_mx[b]])
            S.dma(xt[:, b, :], G.xres[tile * 128:(tile + 1) * 128, :], reads=[G.t_xres[tile]], writes=[t_x[b]])
            for half in range(2):
                for k in range(8):
                    S.op("pe", lambda e: e.matmul(po[:, half, :], lhsT=mx[:, b, k, :], rhs=wo[:, k, half * 512:(half + 1) * 512],
                                                  start=(k == 0), stop=(k == 7)), reads=[t_mx[b], t_wo], writes=[t_po])

        mm(tiles[0])
        for ti, tile in enumerate(tiles):
            b = tile % 2
            j = 1 if tile < 2 else 0
            S.op("dve", lambda e: e.tensor_tensor(out=tmp[:], in0=po[:].rearrange("p a c -> p (a c)"), in1=G.gtb[:, 0, j, :], op=ALU.mult),
                 reads=[t_po, G.t_gtb], writes=[t_tmp])
            if ti + 1 < len(tiles):
                mm(tiles[ti + 1])
            S.op("dve", lambda e: e.tensor_tensor(out=xt[:, b, :], in0=tmp[:], in1=xt[:, b, :], op=ALU.add),
                 reads=[t_tmp, t_x[b]], writes=[t_x[b]])
            S.dma(G.xres[tile * 128:(tile + 1) * 128, :], xt[:, b, :], reads=[t_x[b]], writes=[G.t_xres[tile]])
            xv = xt[:, b, :]
            S.op("act", lambda e: e.activation(out=sq[:], in_=xv, func=AF.Square, accum_out=ss[:, 0:1]), reads=[t_x[b]], writes=[t_sq, t_ss])
            S.op("act", lambda e: e.activation(out=ss[:, 1:2], in_=ss[:, 0:1], func=AF.Sqrt, bias=G.epsc[:], scale=1.0 / D),
                 reads=[t_ss, G.t_c], writes=[t_ss])
            S.op("dve", lambda e: e.reciprocal(out=ss[:, 2:3], in_=ss[:, 1:2]), reads=[t_ss], writes=[t_ss])
            S.op("dve", lambda e: e.tensor_scalar(out=xn[:], in0=xv, scalar1=ss[:, 2:3], scalar2=None, op0=ALU.mult),
                 reads=[t_x[b], t_ss], writes=[t_xn])
            for k in range(8):
                S.op("pe", lambda e: e.transpose(out=pt[:, k, :], in_=xn[:, k * 128:(k + 1) * 128], identity=G.identF),
                     reads=[t_xn, G.t_c], writes=[t_pt])
            S.op("dve", lambda e: e.tensor_tensor(out=h2f[:], in0=pt[:], in1=G.modc[:, 3, :, j:j + 1].to_broadcast([128, 8, 128]), op=ALU.mult),
                 reads=[t_pt, G.t_modc], writes=[t_h2f])
            S.op("dve", lambda e: e.tensor_tensor(out=h2f[:], in0=h2f[:], in1=G.modc[:, 2, :, j:j + 1].to_broadcast([128, 8, 128]), op=ALU.add),
                 reads=[t_h2f, G.t_modc], writes=[t_h2f])
            S.op("act", lambda e: e.copy(out=G.h2T[:, :, tile * 128:(tile + 1) * 128], in_=h2f[:]), reads=[t_h2f], writes=[G.t_h2T])
            for k in range(8):
                S.op("pe", lambda e: e.matmul(pr[:], lhsT=h2f[:, k, :], rhs=wr[:, k, :], start=(k == 0), stop=(k == 7)),
                     reads=[t_h2f, t_wr], writes=[t_pr])
            S.op("dve", lambda e: e.tensor_copy(out=G.rl[:, tile, :], in_=pr[:]), reads=[t_pr], writes=[G.t_rl])


def moe(G, li):
    nc, S, I = G.nc, G.S, G.I
    need_ctx = li < DEPTH - 1
    last = li == DEPTH - 1
    h2T, rl = G.h2T, G.rl
    T0 = 0 if need_ctx else 2
    NTl = NT - T0
    BIG = 1.0e9
    with ExitStack() as _es:
        comb = _es.enter_context(SB(nc, "comb", [128, NT, NE], F32))
        t_comb = Tok()
        with ExitStack() as _es:
            sc = _es.enter_context(SB(nc, "r_sc", [128, NT, NE], F32))
            sel = _es.enter_context(SB(nc, "r_sel", [128, NT, NE], F32))
            ra = _es.enter_context(SB(nc, "r_a", [128, NT, NE], F32))
            rb = _es.enter_context(SB(nc, "r_b", [128, NT, NE], F32))
            rm = _es.enter_context(SB(nc, "r_m", [128, NT * 4], F32))
            rm2 = _es.enter_context(SB(nc, "r_m2", [128, NT * 4], F32))
            rg = _es.enter_context(SB(nc, "r_g", [128, NT], F32))
            rbias = _es.enter_context(SB(nc, "rbias", [128, NE], F32))
            t = Tok()
            if T0 > 0:
                S.op("dve", lambda e: e.memset(rl[:, 0:T0, :], 0.0), reads=[G.t_rl], writes=[G.t_rl])
            S.dma(rbias[:], I["router_bias"].partition_broadcast(128), writes=[t])
            v3 = lambda a: a[:].rearrange("p n (g x) -> p (n g) x", x=4)
            S.op("act", lambda e: e.activation(out=sc[:], in_=rl[:], func=AF.Sigmoid), reads=[G.t_rl], writes=[t])
            S.op("dve", lambda e: e.tensor_tensor(out=sel[:], in0=sc[:], in1=rbias[:].unsqueeze(1).to_broadcast([128, NT, NE]), op=ALU.add),
                 reads=[t], writes=[t])
            S.op("dve", lambda e: e.tensor_reduce(out=rm[:], in_=v3(sel), axis=AX.X, op=ALU.max), reads=[t], writes=[t])
            S.op("dve", lambda e: e.tensor_tensor(out=v3(ra), in0=v3(sel), in1=rm[:].unsqueeze(2).to_broadcast([128, NT * 4, 4]), op=ALU.is_equal),
                 reads=[t], writes=[t])
            S.op("dve", lambda e: e.scalar_tensor_tensor(out=rb[:], in0=ra[:], scalar=-BIG, in1=sel[:], op0=ALU.mult, op1=ALU.add),
                 reads=[t], writes=[t])
            S.op("dve", lambda e: e.tensor_reduce(out=rm2[:], in_=v3(rb), axis=AX.X, op=ALU.max), reads=[t], writes=[t])
            S.op("dve", lambda e: e.tensor_tensor(out=rm[:], in0=rm[:], in1=rm2[:], op=ALU.add), reads=[t], writes=[t])
            S.op("dve", lambda e: e.tensor_reduce(out=rg[:], in_=rm[:].rearrange("p (n g) -> p n g", g=4), axis=AX.X, op=ALU.max),
                 reads=[t], writes=[t])
            S.op("dve", lambda e: e.tensor_tensor(out=rm2[:].rearrange("p (n g) -> p n g", g=4), in0=rm[:].rearrange("p (n g) -> p n g", g=4),
                                                  in1=rg[:].unsqueeze(2).to_broadcast([128, NT, 4]), op=ALU.is_equal), reads=[t], writes=[t])
            S.op("dve", lambda e: e.tensor_scalar(out=rm2[:], in0=rm2[:], scalar1=1.0, scalar2=BIG, op0=ALU.subtract, op1=ALU.mult),
                 reads=[t], writes=[t])
            S.op("dve", lambda e: e.tensor_tensor(out=v3(sel), in0=v3(sel), in1=rm2[:].unsqueeze(2).to_broadcast([128, NT * 4, 4]), op=ALU.add),
                 reads=[t], writes=[t])
            S.op("dve", lambda e: e.tensor_reduce(out=rg[:], in_=sel[:], axis=AX.X, op=ALU.max), reads=[t], writes=[t])
            S.op("dve", lambda e: e.tensor_tensor(out=ra[:], in0=sel[:], in1=rg[:].unsqueeze(2).to_broadcast([128, NT, NE]), op=ALU.is_equal),
                 reads=[t], writes=[t])
            S.op("dve", lambda e: e.scalar_tensor_tensor(out=sel[:], in0=ra[:], scalar=-BIG, in1=sel[:], op0=ALU.mult, op1=ALU.add),
                 reads=[t], writes=[t])
            S.op("dve", lambda e: e.tensor_reduce(out=rg[:], in_=sel[:], axis=AX.X, op=ALU.max), reads=[t], writes=[t])
            S.op("dve", lambda e: e.tensor_tensor(out=rb[:], in0=sel[:], in1=rg[:].unsqueeze(2).to_broadcast([128, NT, NE]), op=ALU.is_equal),
                 reads=[t], writes=[t])
            S.op("dve", lambda e: e.tensor_tensor(out=ra[:], in0=ra[:], in1=rb[:], op=ALU.add), reads=[t], writes=[t])
            S.op("dve", lambda e: e.tensor_tensor(out=ra[:], in0=ra[:], in1=sc[:], op=ALU.mult), reads=[t], writes=[t])
            S.op("dve", lambda e: e.tensor_reduce(out=rg[:], in_=ra[:], axis=AX.X, op=ALU.add), reads=[t], writes=[t])
            S.op("dve", lambda e: e.reciprocal(out=rg[:], in_=rg[:]), reads=[t], writes=[t])
            S.op("dve", lambda e: e.tensor_tensor(out=comb[:], in0=ra[:], in1=rg[:].unsqueeze(2).to_broadcast([128, NT, NE]), op=ALU.mult),
                 reads=[t], writes=[t_comb])
            S.barrier()
        SGT = 12
        with ExitStack() as _es:
            acc = _es.enter_context(SB(nc, "acc", [128, SGT, D], F32))
            wg = _es.enter_context(SB(nc, "wg", [128, 2, 8, DFF], BF16))
            wu = _es.enter_context(SB(nc, "wu", [128, 2, 8, DFF], BF16))
            wd = _es.enter_context(SB(nc, "wd", [128, 2, 2, D], BF16))
            sgl = _es.enter_context(SB(nc, "sgl", [128, 2, 512], F32))
            aa = _es.enter_context(SB(nc, "aa", [128, 2, 512], BF16))
            xt = _es.enter_context(SB(nc, "xt", [128, 2, D], F32))
            gfb = _es.enter_context(SB(nc, "gfb", [128, D], F32))
            ss = _es.enter_context(SB(nc, "ss", [128, 4], F32))
            sq = _es.enter_context(SB(nc, "sq", [128, D], F32))
            pgu = _es.enter_context(PS(nc, "pgu", [128, 4, 512], F32))
            py = _es.enter_context(PS(nc, "py", [128, 2, 2, 512], F32))
            t_acc, t_w, t_sgl, t_aa, t_pgu, t_py, t_x, t_gf, t_ss, t_sq = Tok(), [Tok(), Tok()], Tok(), Tok(), Tok(), [Tok(), Tok()], [Tok(), Tok()], Tok(), Tok(), Tok()
            if last:
                S.dma(gfb[:], I["g_final"].partition_broadcast(128), writes=[t_gf])
            yi = 0
            for s0 in range(T0, NT, SGT):
                tiles = list(range(s0, min(NT, s0 + SGT)))
                for ex in range(NE):
                    wb_ = ex % 2
                    S.dma(wg[:, wb_], G.wb_gate[li][ex].rearrange("(k p) f -> p k f", p=128), reads=[G.t_wb], writes=[t_w[wb_]])
                    S.dma(wu[:, wb_], G.wb_up[li][ex].rearrange("(k p) f -> p k f", p=128), reads=[G.t_wb], writes=[t_w[wb_]])
                    S.dma(wd[:, wb_], G.wb_down[li][ex].rearrange("(j p) c -> p j c", p=128), reads=[G.t_wb], writes=[t_w[wb_]])
                    for b0 in range(0, len(tiles), 4):
                        bt = tiles[b0:b0 + 4]
                        n = len(bt) * 128
                        c0 = bt[0] * 128
                        for wi, wt in enumerate((wg, wu)):
                            for jj in range(2):
                                for k in range(8):
                                    S.op("pe", lambda e: e.matmul(pgu[:, wi * 2 + jj, 0:n], lhsT=wt[:, wb_, k, jj * 128:(jj + 1) * 128],
                                                                  rhs=h2T[:, k, c0:c0 + n], start=(k == 0), stop=(k == 7)),
                                         reads=[t_w[wb_], G.t_h2T], writes=[t_pgu])
                        S.op("act", lambda e: e.activation(out=sgl[:, :, 0:n], in_=pgu[:, 0:2, 0:n], func=AF.Silu), reads=[t_pgu], writes=[t_sgl])
                        S.op("dve", lambda e: e.tensor_tensor(out=aa[:, :, 0:n], in0=sgl[:, :, 0:n], in1=pgu[:, 2:4, 0:n], op=ALU.mult),
                             reads=[t_sgl, t_pgu], writes=[t_aa])
                        for ti, tl in enumerate(bt):
                            yb = yi % 2
                            yi += 1
                            for half in range(2):
                                for jj in range(2):
                                    S.op("pe", lambda e: e.matmul(py[:, yb, half, :], lhsT=aa[:, jj, ti * 128:(ti + 1) * 128],
                                                                  rhs=wd[:, wb_, jj, half * 512:(half + 1) * 512], start=(jj == 0), stop=(jj == 1)),
                                         reads=[t_aa, t_w[wb_]], writes=[t_py[yb]])
                            al = acc[:, tl - s0, :]
                            pyv = py[:, yb].rearrange("p a c -> p (a c)")
                            if ex == 0:
                                S.op("dve", lambda e: e.tensor_scalar(out=al, in0=pyv, scalar1=comb[:, tl, ex:ex + 1], scalar2=None, op0=ALU.mult),
                                     reads=[t_py[yb], t_comb], writes=[t_acc])
                            else:
                                S.op("dve", lambda e: e.scalar_tensor_tensor(out=al, in0=pyv, scalar=comb[:, tl, ex:ex + 1], in1=al,
                                                                             op0=ALU.mult, op1=ALU.add), reads=[t_py[yb], t_comb, t_acc], writes=[t_acc])
                for tl in tiles:
                    b = tl % 2
                    j = 1 if tl < 2 else 0
                    S.dma(xt[:, b, :], G.xres[tl * 128:(tl + 1) * 128, :], reads=[G.t_xres[tl]], writes=[t_x[b]])
                    al = acc[:, tl - s0, :]
                    S.op("dve", lambda e: e.tensor_tensor(out=al, in0=al, in1=G.gtb[:, 1, j, :], op=ALU.mult), reads=[t_acc, G.t_gtb], writes=[t_acc])
                    S.op("dve", lambda e: e.tensor_tensor(out=xt[:, b, :], in0=al, in1=xt[:, b, :], op=ALU.add), reads=[t_acc, t_x[b]], writes=[t_x[b]])
                    if not last:
                        S.dma(G.xres[tl * 128:(tl + 1) * 128, :], xt[:, b, :], reads=[t_x[b]], writes=[G.t_xres[tl]])
                    else:
                        xv = xt[:, b, :]
                        S.op("act", lambda e: e.activation(out=sq[:], in_=xv, func=AF.Square, accum_out=ss[:, 0:1]), reads=[t_x[b]], writes=[t_sq, t_ss])
                        S.op("act", lambda e: e.activation(out=ss[:, 1:2], in_=ss[:, 0:1], func=AF.Sqrt, bias=G.epsc[:], scale=1.0 / D),
                             reads=[t_ss, G.t_c], writes=[t_ss])
                        S.op("dve", lambda e: e.reciprocal(out=ss[:, 2:3], in_=ss[:, 1:2]), reads=[t_ss], writes=[t_ss])
                        S.op("dve", lambda e: e.scalar_tensor_tensor(out=xv, in0=xv, scalar=ss[:, 2:3], in1=gfb[:], op0=ALU.mult, op1=ALU.mult),
                             reads=[t_x[b], t_ss, t_gf], writes=[t_x[b]])
                        S.dma(G.out[(tl - 2) * 128:(tl - 1) * 128, :], xv, reads=[t_x[b]], writes=[])


def ssd(G, li):
    nc, S, I = G.nc, G.S, G.I
    hT = G.hT
    need_ctx = li < DEPTH - 1
    wv32 = I["w_in"][li].rearrange("(k p) c -> p k c", p=128)
    wvb = G.wb_in[li].rearrange("(k p) c -> p k c", p=128)
    XB0 = 1024
    with ExitStack() as _es:
        xbcT = _es.enter_context(SB(nc, "xbcT", [128, 5, NTOK], BF16))
        xs_tok = _es.enter_context(SB(nc, "xs_tok", [128, NT, 384], BF16))
        B_tok = _es.enter_context(SB(nc, "B_tok", [128, NT, 128], BF16))
        lndt = _es.enter_context(SB(nc, "lndt", [128, NT, 12], F32))
        dta = _es.enter_context(SB(nc, "dta", [128, NT, 12], F32))
        ea = _es.enter_context(SB(nc, "ea", [128, NT, 12], F32))
        ww = _es.enter_context(SB(nc, "ww", [128, NT, 12], F32))
        eT = _es.enter_context(SB(nc, "eT", [128, NT, 12], F32))
        t_xbc, t_xs, t_dt = Tok(), Tok(), Tok()
        with ExitStack() as _es2:
            dts = _es2.enter_context(SB(nc, "dts", [128, NT, 12], F32))
            wst = _es2.enter_context(SB(nc, "wst", [128, 8, 128], F32))
            cwb = _es2.enter_context(SB(nc, "cwb", [128, 3, 128], F32))
            wj = _es2.enter_context(SB(nc, "wj", [128, 3, 8, 128], BF16))
            wdt = _es2.enter_context(SB(nc, "wdt", [128, 8, 12], BF16))
            dtb = _es2.enter_context(SB(nc, "dtb", [128, 2, 12], F32))
            tot = _es2.enter_context(SB(nc, "tot", [128, NT, 12], F32))
            wcol = _es2.enter_context(SB(nc, "wcol", [128, NT, 12], F32))
            pp2 = _es2.enter_context(PS(nc, "pp", [128, 2, 512], F32))
            pdt = _es2.enter_context(PS(nc, "pdt", [128, NT, 12], F32))
            ptb = _es2.enter_context(PS(nc, "ptb", [128, 4, 128], BF16))
            pc = _es2.enter_context(PS(nc, "pc", [128, NT, 12], F32))
            t_wst, t_cwb, t_wj, t_pp, t_wdt, t_pdt, t_ptb, t_a, t_pc = Tok(), Tok(), Tok(), Tok(), Tok(), Tok(), Tok(), Tok(), Tok()
            ocb = COLS["ssd_conv_b"][0]
            t_pp2 = [Tok(), Tok()]
            for ch in range(5):
                S.dma(wst[:], wv32[:, :, XB0 + ch * 128:XB0 + (ch + 1) * 128], writes=[t_wst])
                for j in range(3):
                    S.dma(cwb[:, j, :], I["ssd_conv_w"][li, j, ch * 128:(ch + 1) * 128].partition_broadcast(128), writes=[t_cwb])
                for j in range(3):
                    S.op("dve", lambda e: e.tensor_tensor(out=wj[:, j], in0=wst[:], in1=cwb[:, j:j + 1, :].to_broadcast([128, 8, 128]), op=ALU.mult),
                         reads=[t_wst, t_cwb], writes=[t_wj])
                for bi_, (c0, t0, n, tile0, ntile) in enumerate(BLOCKS):
                    pb_ = (ch * len(BLOCKS) + bi_) % 2
                    pp = pp2[:, pb_, :]
                    for j in range(3):
                        for k in range(8):
                            S.op("pe", lambda e: e.matmul(pp[:, 0:n], lhsT=wj[:, j, k, :], rhs=hT[:, k, c0 + j - 1:c0 + j - 1 + n],
                                                          start=(j == 0 and k == 0), stop=(j == 2 and k == 7)), reads=[t_wj, G.t_hT], writes=[t_pp2[pb_]])
                    S.op("act", lambda e: e.activation(out=xbcT[:, ch, t0:t0 + n], in_=pp[:, 0:n], func=AF.Silu, bias=G.cols[:, ocb + ch:ocb + ch + 1]),
                         reads=[t_pp2[pb_], G.t_cols], writes=[t_xbc])
            S.dma(wdt[:], wvb[:, :, 1664:1676], reads=[G.t_wb], writes=[t_wdt])
            S.dma(dtb[:, 0, :], I["ssd_dt_bias"][li].rearrange("a h -> (a h)").partition_broadcast(128), writes=[t_wdt])
            S.dma(dtb[:, 1, :], I["ssd_a_log"][li].rearrange("a h -> (a h)").partition_broadcast(128), writes=[t_wdt])
            for tl in range(NT):
                cc = colof(tl)
                for k in range(8):
                    S.op("pe", lambda e: e.matmul(pdt[:, tl, :], lhsT=hT[:, k, cc:cc + 128], rhs=wdt[:, k, :], start=(k == 0), stop=(k == 7)),
                         reads=[t_wdt, G.t_hT], writes=[t_pdt])
            S.op("dve", lambda e: e.tensor_tensor(out=dts[:], in0=pdt[:], in1=dtb[:, 0:1, :].to_broadcast([128, NT, 12]), op=ALU.add),
                 reads=[t_pdt, t_wdt], writes=[t_dt])
            S.op("act", lambda e: e.activation(out=dts[:], in_=dts[:], func=AF.Exp), reads=[t_dt], writes=[t_dt])
            S.op("act", lambda e: e.activation(out=dts[:], in_=dts[:], func=AF.Ln, bias=1.0), reads=[t_dt], writes=[t_dt])
            S.op("act", lambda e: e.activation(out=lndt[:], in_=dts[:], func=AF.Ln), reads=[t_dt], writes=[t_dt])
            S.op("act", lambda e: e.activation(out=dtb[:, 1, :], in_=dtb[:, 1, :], func=AF.Exp), reads=[t_wdt], writes=[t_wdt])
            S.op("dve", lambda e: e.scalar_tensor_tensor(out=dta[:], in0=dts[:], scalar=-1.0, in1=dtb[:, 1:2, :].to_broadcast([128, NT, 12]),
                                                         op0=ALU.mult, op1=ALU.mult), reads=[t_dt, t_wdt], writes=[t_a])
            for tl in range(NT):
                for c in range(4):
                    S.op("pe", lambda e: e.transpose(out=ptb[:, c, :], in_=xbcT[:, c, tl * 128:(tl + 1) * 128], identity=G.identB),
                         reads=[t_xbc, G.t_c], writes=[t_ptb])
                S.op("dve", lambda e: e.tensor_copy(out=xs_tok[:, tl, :], in_=ptb[:, 0:3, :].rearrange("p c t -> p (c t)")), reads=[t_ptb], writes=[t_xs])
                S.op("dve", lambda e: e.tensor_copy(out=B_tok[:, tl, :], in_=ptb[:, 3, :]), reads=[t_ptb], writes=[t_xs])
            for dr in range(2):
                S.op("pe", lambda e: e.matmul(pc[:].rearrange("p n h -> p (n h)"), lhsT=G.cm[:, 1 + dr, :], rhs=dta[:].rearrange("p n h -> p (n h)"),
                                              start=True, stop=True), reads=[t_a, G.t_c, t_dt], writes=[t_pc])
                S.op("dve", lambda e: e.tensor_copy(out=wcol[:, :, dr * 6:(dr + 1) * 6], in_=pc[:, :, dr * 6:(dr + 1) * 6]), reads=[t_pc], writes=[t_dt])
            S.op("pe", lambda e: e.matmul(pc[:].rearrange("p n h -> p (n h)"), lhsT=G.ones[:], rhs=dta[:].rearrange("p n h -> p (n h)"), start=True, stop=True),
                 reads=[t_a, G.t_c, t_dt], writes=[t_pc])
            S.op("dve", lambda e: e.tensor_copy(out=tot[:], in_=pc[:]), reads=[t_pc], writes=[t_dt])
            S.op("act", lambda e: e.activation(out=ea[:], in_=wcol[:], func=AF.Exp), reads=[t_dt], writes=[t_dt])
            S.op("act", lambda e: e.activation(out=eT[:], in_=tot[:], func=AF.Exp), reads=[t_dt], writes=[t_dt])
            S.op("dve", lambda e: e.tensor_tensor(out=ww[:], in0=tot[:], in1=wcol[:], op=ALU.subtract), reads=[t_dt], writes=[t_dt])
            S.op("dve", lambda e: e.tensor_tensor(out=ww[:], in0=ww[:], in1=lndt[:], op=ALU.add), reads=[t_dt], writes=[t_dt])
            S.op("act", lambda e: e.activation(out=ww[:], in_=ww[:], func=AF.Exp), reads=[t_dt], writes=[t_dt])
            S.barrier()
        with ExitStack() as _es2:
            wz = _es2.enter_context(SB(nc, "wz", [128, 8, 384], BF16))
            dbc = _es2.enter_context(SB(nc, "dbc", [128, 6, 64], F32))
            d6 = _es2.enter_context(SB(nc, "d6", [128, 6], F32))
            nwb = _es2.enter_context(SB(nc, "nwb", [128, 384], F32))
            hst = _es2.enter_context(SB(nc, "hst", [128, 192], F32))
            hsb = _es2.enter_context(SB(nc, "hsb", [128, 192], BF16))
            xw = _es2.enter_context(SB(nc, "xw", [128, 2, 192], BF16))
            ybt = _es2.enter_context(SB(nc, "ybt", [128, 2, 384], F32))
            Sm = _es2.enter_context(SB(nc, "Sm", [128, 2, 2, 128], F32))
            rA = _es2.enter_context(SB(nc, "rA", [128, 4, 128], F32))
            Dd = _es2.enter_context(SB(nc, "Dd", [128, 4, 128], F32))
            Mt = _es2.enter_context(SB(nc, "Mt", [128, 4, 128], BF16))
            acc = _es2.enter_context(SB(nc, "acc", [128, 384], F32))
            tmp = _es2.enter_context(SB(nc, "tmp", [128, 384], F32))
            zs = _es2.enter_context(SB(nc, "zs", [128, 384], F32))
            ssq = _es2.enter_context(SB(nc, "ssq", [128, 4], F32))
            ob = _es2.enter_context(SB(nc, "ob", [128, 384], BF16))
            oT = _es2.enter_context(SB(nc, "oT", [128, 2, 3, 128], BF16))
            pis = _es2.enter_context(PS(nc, "pis", [128, 2, 192], F32))
            ps_st = _es2.enter_context(PS(nc, "ps_st", [128, 2, 512], F32))
            pseg = _es2.enter_context(PS(nc, "pseg", [128, 3, 512], F32))
            py = _es2.enter_context(PS(nc, "py", [128, 384], F32))
            pz = py
            ptr = _es2.enter_context(PS(nc, "ptr", [128, 3, 128], BF16))
            t_wz, t_db, t_h, t_xw, t_yb, t_Sm, t_acc, t_tmp, t_zs, t_ssq, t_ob, t_oT = Tok(), Tok(), Tok(), Tok(), [Tok(), Tok()], Tok(), Tok(), Tok(), Tok(), Tok(), Tok(), [Tok(), Tok()]
            t_rA, t_Dd, t_Mt, t_pseg = [Tok() for _ in range(4)], [Tok() for _ in range(4)], [Tok() for _ in range(4)], [Tok() for _ in range(3)]
            t_pis, t_pst, t_py, t_ptr = Tok(), Tok(), Tok(), Tok()
            t_pz = t_py
            S.dma(wz[:], wvb[:, :, 640:1024], reads=[G.t_wb], writes=[t_wz])
            S.dma(d6[:], I["ssd_d"][li].partition_broadcast(128), writes=[t_db])
            S.dma(nwb[:], I["ssd_norm"][li].partition_broadcast(128), writes=[t_db])
            S.op("dve", lambda e: e.tensor_copy(out=dbc[:], in_=d6[:].unsqueeze(2).to_broadcast([128, 6, 64])), reads=[t_db], writes=[t_db])
            yb_d = G.ssd_yb

            def carry_step(c, dr, want_out, dst):
                for g in range(2):
                    hd0 = dr * 6 + 3 * g
                    if want_out:
                        S.op("pe", lambda e: e.matmul(pis[:, 0, :], lhsT=xbcT[g * 64:(g + 1) * 64, 4, c * 128:(c + 1) * 128], rhs=hsb[g * 64:(g + 1) * 64, :],
                                                      start=True, stop=True), reads=[t_xbc, t_h], writes=[t_pis])
                        S.op("dve", lambda e: e.tensor_tensor(out=dst[:, g * 192:(g + 1) * 192].rearrange("p (a d) -> p a d", a=3),
                                                              in0=pis[:, 0, :].rearrange("p (a d) -> p a d", a=3),
                                                              in1=ea[:, c, hd0:hd0 + 3].unsqueeze(2).to_broadcast([128, 3, 64]), op=ALU.mult),
                             reads=[t_pis, t_dt], writes=[dst_tok[0]])
                    S.op("dve", lambda e: e.tensor_tensor(out=xw[:, g, :].rearrange("p (a d) -> p a d", a=3),
                                                          in0=xs_tok[:, c, g * 192:(g + 1) * 192].rearrange("p (a d) -> p a d", a=3),
                                                          in1=ww[:, c, hd0:hd0 + 3].unsqueeze(2).to_broadcast([128, 3, 64]), op=ALU.mult),
                         reads=[t_xs, t_dt], writes=[t_xw])
                    gs = slice(g * 64, (g + 1) * 64)
                    S.op("pe", lambda e: e.matmul(pis[:, 1, :], lhsT=B_tok[:, c, :], rhs=xw[:, g, :], start=True, stop=True),
                         reads=[t_xs, t_xw], writes=[t_pis])
                    S.op("dve", lambda e: e.tensor_tensor(out=hst[gs, :].rearrange("p (a d) -> p a d", a=3),
                                                          in0=hst[gs, :].rearrange("p (a d) -> p a d", a=3),
                                                          in1=eT[gs, c, hd0:hd0 + 3].unsqueeze(2).to_broadcast([64, 3, 64]), op=ALU.mult),
                         reads=[t_h, t_dt], writes=[t_h])
                    S.op("dve", lambda e: e.tensor_tensor(out=hst[gs, :], in0=hst[gs, :], in1=pis[gs, 1, :], op=ALU.add),
                         reads=[t_h, t_pis], writes=[t_h])
                    S.op("act", lambda e: e.copy(out=hsb[gs, :], in_=hst[gs, :]), reads=[t_h], writes=[t_h])

            S.op("dve", lambda e: e.memset(hst[:], 0.0), writes=[t_h])
            S.op("dve", lambda e: e.memset(hsb[:], 0.0), writes=[t_h])
            order_b = [1, 0] + list(range(NT - 1, 1, -1))
            for ci, c in enumerate(order_b):
                want = need_ctx or c >= 2
                b = ci % 2
                dst_tok = [t_yb[b]]
                carry_step(c, 1, want, ybt[:, b, :])
                if want:
                    S.dma(yb_d[c * 128:(c + 1) * 128, :], ybt[:, b, :], reads=[t_yb[b]], writes=[G.t_ssdyb[c]])
            S.op("dve", lambda e: e.memset(hst[:], 0.0), reads=[t_h], writes=[t_h])
            S.op("dve", lambda e: e.memset(hsb[:], 0.0), reads=[t_h], writes=[t_h])
            on = COLS["ssd_norm"][0]
            it = 0
            for c in range(NT):
                want = need_ctx or c >= 2
                dst_tok = [t_acc]
                carry_step(c, 0, want, acc[:])
                if not want:
                    continue
                b = c % 2
                tc0 = c * 128
                S.dma(ybt[:, b, :], yb_d[c * 128:(c + 1) * 128, :], reads=[G.t_ssdyb[c]], writes=[t_yb[b]])
                cc = colof(c)
                for k in range(8):
                    S.op("pe", lambda e: e.matmul(pz[:], lhsT=hT[:, k, cc:cc + 128], rhs=wz[:, k, :], start=(k == 0), stop=(k == 7)),
                         reads=[t_wz, G.t_hT], writes=[t_pz])
                S.op("act", lambda e: e.activation(out=zs[:], in_=pz[:], func=AF.Silu), reads=[t_pz], writes=[t_zs])
                for g in range(2):
                    S.op("pe", lambda e: e.matmul(ps_st[:, g, 0:128], lhsT=xbcT[g * 64:(g + 1) * 64, 3, tc0:tc0 + 128], rhs=xbcT[g * 64:(g + 1) * 64, 4, tc0:tc0 + 128],
                                                  start=True, stop=True), reads=[t_xbc], writes=[t_pst])
                for g in range(2):
                    for dr in range(2):
                        S.op("dve", lambda e: e.tensor_tensor(out=Sm[:, g, dr, :], in0=ps_st[:, g, 0:128], in1=G.cm[:, 1 + dr, :], op=ALU.mult),
                             reads=[t_pst, G.t_c], writes=[t_Sm])
                steps = [(h, dr) for h in range(6) for dr in range(2)]

                def st_a(i):
                    h, dr = steps[i]
                    hd = dr * 6 + h
                    r4, p3 = (it + i) % 4, (it + i) % 3
                    S.op("dve", lambda e: e.tensor_scalar(out=rA[:, r4, :], in0=G.cm[:, 1 + dr, :], scalar1=dta[:, c, hd:hd + 1], scalar2=None, op0=ALU.mult),
                         reads=[G.t_c, t_dt], writes=[t_rA[r4]])
                    S.op("pe", lambda e: e.matmul(pseg[:, p3, 0:128], lhsT=G.cm[:, 8 - dr, :], rhs=rA[:, r4, :], start=True, stop=True),
                         reads=[t_rA[r4], G.t_c], writes=[t_pseg[p3]])
                    S.op("act", lambda e: e.activation(out=Dd[:, r4, :], in_=pseg[:, p3, 0:128], func=AF.Exp, bias=lndt[:, c, hd:hd + 1]),
                         reads=[t_pseg[p3], t_dt], writes=[t_Dd[r4]])

                st_a(0)
                st_a(1)
                for i, (h, dr) in enumerate(steps):
                    r4 = (it + i) % 4
                    if i + 2 < len(steps):
                        st_a(i + 2)
                    S.op("dve", lambda e: e.tensor_tensor(out=Mt[:, r4, :], in0=Dd[:, r4, :], in1=Sm[:, h // 3, dr, :], op=ALU.mult),
                         reads=[t_Dd[r4], t_Sm], writes=[t_Mt[r4]])
                    S.op("pe", lambda e: e.matmul(py[:, h * 64:(h + 1) * 64], lhsT=Mt[:, r4, :], rhs=xs_tok[:, c, h * 64:(h + 1) * 64],
                                                  start=(dr == 0), stop=(dr == 1)), reads=[t_Mt[r4], t_xs], writes=[t_py])
                it += len(steps)
                S.op("dve", lambda e: e.tensor_tensor(out=acc[:], in0=acc[:], in1=py[:], op=ALU.add), reads=[t_acc, t_py], writes=[t_acc])
                S.op("dve", lambda e: e.tensor_tensor(out=acc[:], in0=acc[:], in1=ybt[:, b, :], op=ALU.add), reads=[t_acc, t_yb[b]], writes=[t_acc])
                S.op("dve", lambda e: e.tensor_tensor(out=tmp[:], in0=xs_tok[:, c, :], in1=dbc[:].rearrange("p a d -> p (a d)"), op=ALU.mult),
                     reads=[t_xs, t_db], writes=[t_tmp])
                S.op("dve", lambda e: e.tensor_tensor(out=acc[:], in0=acc[:], in1=tmp[:], op=ALU.add), reads=[t_acc, t_tmp], writes=[t_acc])
                S.op("dve", lambda e: e.tensor_tensor(out=acc[:], in0=acc[:], in1=zs[:], op=ALU.mult), reads=[t_acc, t_zs], writes=[t_acc])
                for g in range(2):
                    S.op("act", lambda e: e.activation(out=tmp[:, g * 192:(g + 1) * 192], in_=acc[:, g * 192:(g + 1) * 192], func=AF.Square, accum_out=ssq[:, g:g + 1]),
                         reads=[t_acc], writes=[t_tmp, t_ssq])
                S.op("act", lambda e: e.activation(out=ssq[:, 2:4], in_=ssq[:, 0:2], func=AF.Sqrt, bias=G.epsc[:], scale=1.0 / 192), reads=[t_ssq, G.t_c], writes=[t_ssq])
                S.op("dve", lambda e: e.reciprocal(out=ssq[:, 2:4], in_=ssq[:, 2:4]), reads=[t_ssq], writes=[t_ssq])
                for g in range(2):
                    S.op("dve", lambda e: e.scalar_tensor_tensor(out=ob[:, g * 192:(g + 1) * 192], in0=acc[:, g * 192:(g + 1) * 192], scalar=ssq[:, 2 + g:3 + g],
                                                                 in1=nwb[:, g * 192:(g + 1) * 192], op0=ALU.mult, op1=ALU.mult), reads=[t_acc, t_ssq, t_db], writes=[t_ob])
                for c3 in range(3):
                    S.op("pe", lambda e: e.transpose(out=ptr[:, c3, :], in_=ob[:, c3 * 128:(c3 + 1) * 128], identity=G.identB), reads=[t_ob, G.t_c], writes=[t_ptr])
                S.op("act", lambda e: e.copy(out=oT[:, b], in_=ptr[:]), reads=[t_ptr], writes=[t_oT[b]])
                S.dma(G.mixT[384:768, tc0:tc0 + 128].rearrange("(c p) t -> p c t", p=128), oT[:, b], reads=[t_oT[b]], writes=[G.t_mix])
        if "ssd" in G.dbg and li == 0:
            dump_bf16(G, G.mixT[384:512, 256:768], G.dbg["ssd"], [G.t_mix])


HY0 = 1676
SEGS = {"lat": dict(L=4096, NC=32, KC=17, off=NCTX), "ctx": dict(L=256, NC=2, KC=2, off=0)}


def hyena_inproj(G, li):
    nc, S, I = G.nc, G.S, G.I
    hT = G.hT
    need_ctx = li < DEPTH - 1
    wv32 = I["w_in"][li].rearrange("(k p) c -> p k c", p=128)
    with ExitStack() as _es:
        wst = _es.enter_context(SB(nc, "wst", [128, 8, 128], F32))
        cwb = _es.enter_context(SB(nc, "cwb", [128, 3, 128], F32))
        wj = _es.enter_context(SB(nc, "wjh", [128, 3, 8, 768], BF16))
        hb = _es.enter_context(SB(nc, "hb", [128, 768], F32))
        hv = _es.enter_context(SB(nc, "hv", [128, 2, 768], BF16))
        ph4 = _es.enter_context(PS(nc, "ph", [128, 2, 2, 512], F32))
        t_wst, t_cwb, t_wj, t_hb, t_hv, t_ph2 = Tok(), Tok(), Tok(), Tok(), [Tok(), Tok()], [Tok(), Tok()]
        S.dma(hb[:], I["hy_conv_b"][li].partition_broadcast(128), writes=[t_hb])
        for cc in range(6):
            S.dma(wst[:], wv32[:, :, HY0 + cc * 128:HY0 + (cc + 1) * 128], writes=[t_wst])
            for j in range(3):
                S.dma(cwb[:, j, :], I["hy_conv_w"][li, j, cc * 128:(cc + 1) * 128].partition_broadcast(128), writes=[t_cwb])
            for j in range(3):
                S.op("dve", lambda e: e.tensor_tensor(out=wj[:, j, :, cc * 128:(cc + 1) * 128], in0=wst[:],
                                                      in1=cwb[:, j:j + 1, :].to_broadcast([128, 8, 128]), op=ALU.mult),
                     reads=[t_wst, t_cwb], writes=[t_wj])
        dv = G.hyv.rearrange("m t c -> t m c")
        for tl in range(NT):
            if tl < 2 and not need_ctx:
                continue
            col = colof(tl)
            b = tl % 2
            ph = ph4[:, b]
            t_ph = t_ph2[b]
            for half in range(2):
                for j in range(3):
                    for k in range(8):
                        S.op("pe", lambda e: e.matmul(ph[:, half, 0:384], lhsT=hT[:, k, col + j - 1:col + j - 1 + 128],
                                                      rhs=wj[:, j, k, half * 384:(half + 1) * 384], start=(j == 0 and k == 0), stop=(j == 2 and k == 7)),
                             reads=[t_wj, G.t_hT], writes=[t_ph])
            S.op("dve", lambda e: e.tensor_tensor(out=hv[:, b, :].rearrange("p (a c) -> p a c", a=2), in0=ph[:, :, 0:384],
                                                  in1=hb[:].rearrange("p (a c) -> p a c", a=2), op=ALU.add), reads=[t_ph, t_hb], writes=[t_hv[b]])
            S.dma(dv[tl * 128:(tl + 1) * 128], hv[:, b, :].rearrange("p (m c) -> p m c", m=3), reads=[t_hv[b]], writes=[G.t_hyv])


def hyena_fft(G, li):
    nc, S, I = G.nc, G.S, G.I
    need_ctx = li < DEPTH - 1
    for sname in (("lat", "ctx") if need_ctx else ("lat",)):
        P = SEGS[sname]
        L, NCk, KC, off = P["L"], P["NC"], P["KC"], P["off"]
        NH = NCk // 2
        FT, IT, FE, WIN, MH = I["ft_" + sname], I["it_" + sname], I["fe_" + sname], I["win_" + sname], I["mh_" + sname]
        Kf = G.Kf[sname]
        t_kf = Tok()
        with ExitStack() as _es:
            hk = _es.enter_context(SB(nc, "hk", [128, NCk, 1024], BF16))
            fe = _es.enter_context(SB(nc, "fe", [33, L], F32))
            h1 = _es.enter_context(SB(nc, "h1", [64, L], F32))
            h2 = _es.enter_context(SB(nc, "h2", [64, L], F32))
            w1 = _es.enter_context(SB(nc, "w1", [33, 64], F32))
            w2 = _es.enter_context(SB(nc, "w2", [64, 64], F32))
            w3 = _es.enter_context(SB(nc, "w3", [64, 1024], F32))
            pre = _es.enter_context(SB(nc, "pre", [64, 512], F32))
            pr2 = _es.enter_context(SB(nc, "pr2", [64, 512], F32))
            t_pr2 = Tok()
            MAGIC = 1.5 * 2 ** 23
            wint = _es.enter_context(SB(nc, "wint", [128, 2, 2, 256], F32))
            hbias = _es.enter_context(SB(nc, "hbias", [128, 512], F32))
            mh = _es.enter_context(SB(nc, "mh", [128, KC], F32))
            slab = _es.enter_context(SB(nc, "slab", [128, 2, NCk, 2, 128], BF16))
            xo = _es.enter_context(SB(nc, "xo", [128, 2, 512], F32))
            sd = _es.enter_context(SB(nc, "sd", [128, 2, 2, 2, 512], F32))
            kf = _es.enter_context(SB(nc, "kf", [128, 2, 2, 2, 256], BF16))
            pm = _es.enter_context(PS(nc, "pm", [64, 512], F32))
            phh = _es.enter_context(PS(nc, "phh", [128, 2, 512], F32))
            psk = _es.enter_context(PS(nc, "psk", [128, 2, 2, 512], F32))
            t_hk, t_fe, t_h1, t_h2, t_w, t_pre, t_win, t_slab, t_eo, t_sd, t_kft, t_pm, t_phh, t_psk = \
                Tok(), Tok(), Tok(), Tok(), Tok(), Tok(), [Tok(), Tok()], [Tok(), Tok()], Tok(), Tok(), Tok(), Tok(), Tok(), Tok()
            S.dma(fe[:], FE[:, :], writes=[t_fe])
            S.dma(w1[:], I["hy_w1"][li], writes=[t_w])
            S.dma(w2[:], I["hy_w2"][li], writes=[t_w])
            S.dma(w3[:], I["hy_w3"][li], writes=[t_w])
            S.dma(hbias[:], I["hy_bias"][li].rearrange("o c -> (o c)").partition_broadcast(128), writes=[t_w])
            S.dma(mh[:], MH[:, :], writes=[t_w])
            ob1, ofr, ob2 = COLS["hy_b1"][0], COLS["hy_freq"][0], COLS["hy_b2"][0]
            for (src, t_src, wt, kk, bcol, dst, t_dst) in ((fe, t_fe, w1, 33, ob1, h1, t_h1), (h1, t_h1, w2, 64, ob2, h2, t_h2)):
                for c0 in range(0, L, 512):
                    n = min(512, L - c0)
                    S.op("pe", lambda e: e.matmul(pm[:, 0:n], lhsT=wt[0:kk, :], rhs=src[0:kk, c0:c0 + n], start=True, stop=True),
                         reads=[t_w, t_src], writes=[t_pm])
                    S.op("dve", lambda e: e.tensor_scalar(out=pre[:, 0:n], in0=pm[:, 0:n], scalar1=G.cols[0:64, bcol:bcol + 1],
                                                          scalar2=G.cols[0:64, ofr:ofr + 1], op0=ALU.add, op1=ALU.mult),
                         reads=[t_pm, G.t_cols], writes=[t_pre])
                    S.op("dve", lambda e: e.tensor_scalar(out=pr2[:, 0:n], in0=pre[:, 0:n], scalar1=1.0 / (2.0 * math.pi), scalar2=MAGIC, op0=ALU.mult, op1=ALU.add),
                         reads=[t_pre], writes=[t_pr2])
                    S.op("dve", lambda e: e.tensor_scalar(out=pr2[:, 0:n], in0=pr2[:, 0:n], scalar1=-MAGIC, scalar2=None, op0=ALU.add),
                         reads=[t_pr2], writes=[t_pr2])
                    S.op("dve", lambda e: e.scalar_tensor_tensor(out=pre[:, 0:n], in0=pr2[:, 0:n], scalar=-2.0 * math.pi, in1=pre[:, 0:n], op0=ALU.mult, op1=ALU.add),
                         reads=[t_pr2, t_pre], writes=[t_pre])
                    S.op("act", lambda e: e.activation(out=dst[:, c0:c0 + n], in_=pre[:, 0:n], func=AF.Sin),
                         reads=[t_pre], writes=[t_dst])
            for c in range(NCk):
                b = c % 2
                S.dma(wint[:, b], WIN[c], writes=[t_win[b]])
                for half in range(2):
                    S.op("pe", lambda e: e.matmul(phh[:, half, :], lhsT=h2[:, c * 128:(c + 1) * 128], rhs=w3[:, half * 512:(half + 1) * 512], start=True, stop=True),
                         reads=[t_h2, t_w], writes=[t_phh])
                for dr in range(2):
                    S.op("dve", lambda e: e.tensor_tensor(out=hk[:, c, dr * 512:(dr + 1) * 512].rearrange("p (o c) -> p o c", o=2),
                                                          in0=phh[:, dr, :].rearrange("p (o c) -> p o c", o=2),
                                                          in1=wint[:, b, dr:dr + 1, :].to_broadcast([128, 2, 256]), op=ALU.mult),
                         reads=[t_phh, t_win[b]], writes=[t_hk])
            for kc in range(KC):
                b = kc % 2
                S.dma(slab[:, b], FT[kc], writes=[t_slab[b]])
                for dr in range(2):
                    for eo_ in range(2):
                        for ri in range(2):
                            for c in range(NH):
                                cc = eo_ * NH + c
                                S.op("pe", lambda e: e.matmul(psk[:, eo_, ri, :], lhsT=slab[:, b, cc, ri, :], rhs=hk[:, cc, dr * 512:(dr + 1) * 512],
                                                              start=(c == 0), stop=(c == NH - 1)), reads=[t_slab[b], t_hk], writes=[t_psk])
                    S.op("act", lambda e: e.copy(out=xo[:], in_=psk[:, 1]), reads=[t_psk], writes=[t_eo])
                    S.op("dve", lambda e: e.tensor_tensor(out=sd[:, dr, 0], in0=psk[:, 0], in1=xo[:], op=ALU.add), reads=[t_psk, t_eo], writes=[t_sd])
                    S.op("dve", lambda e: e.tensor_tensor(out=sd[:, dr, 1], in0=psk[:, 0], in1=xo[:], op=ALU.subtract), reads=[t_psk, t_eo], writes=[t_sd])
                v = lambda ap: ap.rearrange("p (o c) -> p o c", o=2)
                S.op("dve", lambda e: e.tensor_tensor(out=sd[:, 0, 0, 0, :], in0=sd[:, 0, 0, 0, :], in1=hbias[:], op=ALU.add), reads=[t_sd, t_w], writes=[t_sd])
                S.op("dve", lambda e: e.tensor_tensor(out=sd[:, 0, 1, 0, :], in0=sd[:, 0, 1, 0, :], in1=hbias[:], op=ALU.add), reads=[t_sd, t_w], writes=[t_sd])
                S.op("dve", lambda e: e.tensor_tensor(out=kf[:, :, 0, 0, :], in0=v(sd[:, 0, 0, 0, :]), in1=v(sd[:, 1, 0, 0, :]), op=ALU.add), reads=[t_sd], writes=[t_kft])
                S.op("dve", lambda e: e.tensor_tensor(out=kf[:, :, 0, 1, :], in0=v(sd[:, 0, 0, 1, :]), in1=v(sd[:, 1, 0, 1, :]), op=ALU.subtract), reads=[t_sd], writes=[t_kft])
                S.op("dve", lambda e: e.tensor_tensor(out=v(xo[:, 0, :]), in0=v(sd[:, 0, 1, 0, :]), in1=v(sd[:, 1, 1, 0, :]), op=ALU.add), reads=[t_sd], writes=[t_eo])
                S.op("dve", lambda e: e.tensor_tensor(out=v(xo[:, 1, :]), in0=v(sd[:, 1, 1, 1, :]), in1=v(sd[:, 0, 1, 1, :]), op=ALU.subtract), reads=[t_sd], writes=[t_eo])
                S.op("dve", lambda e: e.tensor_scalar(out=kf[:, :, 1, 0, :], in0=v(xo[:, 0, :]), scalar1=mh[:, kc:kc + 1], scalar2=None, op0=ALU.mult),
                     reads=[t_eo, t_w], writes=[t_kft])
                S.op("dve", lambda e: e.tensor_scalar(out=kf[:, :, 1, 1, :], in0=v(xo[:, 1, :]), scalar1=mh[:, kc:kc + 1], scalar2=None, op0=ALU.mult),
                     reads=[t_eo, t_w], writes=[t_kft])
                S.dma(Kf[:, kc].rearrange("o p l r c -> p o l r c"), kf[:], reads=[t_kft], writes=[t_kf])
            S.barrier()
        with ExitStack() as _es:
            vt = _es.enter_context(SB(nc, "vt", [128, NCk, 256], BF16))
            zz1 = _es.enter_context(SB(nc, "zz1", [128, NCk, 256], BF16))
            Y = _es.enter_context(SB(nc, "Y", [128, 2, KC, 2, 256], BF16))
            fsl = _es.enter_context(SB(nc, "fsl", [128, 2, NCk, 2, 128], BF16))
            isl = _es.enter_context(SB(nc, "isl", [128, 2, KC, 2, 128], BF16))
            kft = _es.enter_context(SB(nc, "kft", [128, 2, 2, 2, 256], BF16))
            xo = _es.enter_context(SB(nc, "xo", [128, 2, 256], F32))
            xs_ = _es.enter_context(SB(nc, "xs_", [128, 2, 2, 256], F32))
            ta = _es.enter_context(SB(nc, "ta", [128, 4, 256], F32))
            yl = _es.enter_context(SB(nc, "yl", [128, 2, 2, 256], F32))
            xg = _es.enter_context(SB(nc, "xg", [128, 2, 256], BF16))
            zt = _es.enter_context(SB(nc, "zt", [128, 256], BF16))
            zT = _es.enter_context(SB(nc, "zT", [128, 2, 2, 256], BF16))
            psx = _es.enter_context(PS(nc, "psx", [128, 2, 4, 256], F32))
            psy = _es.enter_context(PS(nc, "psy", [128, 2, 512], F32))
            ptr = _es.enter_context(PS(nc, "ptr", [128, 2, 128], BF16))
            t_vt, t_zz1, t_Y, t_fsl, t_isl, t_kft2, t_ta, t_xg, t_zt, t_zT, t_psx, t_psy, t_ptr, t_xo, t_xs, t_yl = \
                Tok(), Tok(), Tok(), [Tok(), Tok()], [Tok(), Tok()], [Tok(), Tok()], Tok(), [Tok(), Tok()], Tok(), [Tok(), Tok()], [Tok(), Tok()], [Tok(), Tok()], Tok(), Tok(), Tok(), Tok()
            hsrc = lambda m: G.hyv[m, off:off + L, :].rearrange("(c p two) ch -> two p c ch", p=128, two=2)
            for par in range(2):
                S.dma(vt[:, par * NH:(par + 1) * NH, :], hsrc(0)[par], reads=[G.t_hyv], writes=[t_vt])
            mo = G.mixT[768:1024, :].rearrange("(c p) t -> p c t", p=128)
            for order in range(2):
                src, t_src = (vt, t_vt) if order == 0 else (zz1, t_zz1)
                for kc in range(KC):
                    b = kc % 2
                    S.dma(fsl[:, b], FT[kc], writes=[t_fsl[b]])
                    S.dma(kft[:, b], Kf[order, kc], reads=[t_kf], writes=[t_kft2[b]])
                    for eo_ in range(2):
                        for ri in range(2):
                            for c in range(NH):
                                cc = eo_ * NH + c
                                S.op("pe", lambda e: e.matmul(psx[:, b, eo_ * 2 + ri, :], lhsT=fsl[:, b, cc, ri, :], rhs=src[:, cc, :], start=(c == 0), stop=(c == NH - 1)),
                                     reads=[t_fsl[b], t_src], writes=[t_psx[b]])
                    S.op("act", lambda e: e.copy(out=xo[:], in_=psx[:, b, 2:4, :]), reads=[t_psx[b]], writes=[t_xo])
                    S.op("dve", lambda e: e.tensor_tensor(out=xs_[:, 0], in0=psx[:, b, 0:2, :], in1=xo[:], op=ALU.add), reads=[t_psx[b], t_xo], writes=[t_xs])
                    S.op("dve", lambda e: e.tensor_tensor(out=xs_[:, 1, 0, :], in0=psx[:, b, 0, :], in1=xo[:, 0, :], op=ALU.subtract), reads=[t_psx[b], t_xo], writes=[t_xs])
                    S.op("dve", lambda e: e.scalar_tensor_tensor(out=xs_[:, 1, 1, :], in0=psx[:, b, 1, :], scalar=-1.0, in1=xo[:, 1, :], op0=ALU.mult, op1=ALU.add),
                         reads=[t_psx[b], t_xo], writes=[t_xs])
                    S.op("dve", lambda e: e.tensor_tensor(out=ta[:, 0:2, :], in0=xs_[:, :, 0, :], in1=kft[:, b, :, 0, :], op=ALU.mult), reads=[t_xs, t_kft2[b]], writes=[t_ta])
                    S.op("pool", lambda e: e.tensor_tensor(out=ta[:, 2:4, :], in0=xs_[:, :, 1, :], in1=kft[:, b, :, 1, :], op=ALU.mult), reads=[t_xs, t_kft2[b]], writes=[t_ta])
                    S.op("dve", lambda e: e.tensor_tensor(out=yl[:, :, 0, :], in0=ta[:, 0:2, :], in1=ta[:, 2:4, :], op=ALU.subtract), reads=[t_ta], writes=[t_yl])
                    S.op("dve", lambda e: e.tensor_tensor(out=ta[:, 0:2, :], in0=xs_[:, :, 0, :], in1=kft[:, b, :, 1, :], op=ALU.mult), reads=[t_xs, t_kft2[b], t_yl], writes=[t_ta])
                    S.op("pool", lambda e: e.tensor_tensor(out=ta[:, 2:4, :], in0=xs_[:, :, 1, :], in1=kft[:, b, :, 0, :], op=ALU.mult), reads=[t_xs, t_kft2[b], t_yl], writes=[t_ta])
                    S.op("dve", lambda e: e.tensor_tensor(out=yl[:, :, 1, :], in0=ta[:, 0:2, :], in1=ta[:, 2:4, :], op=ALU.add), reads=[t_ta], writes=[t_yl])
                    S.op("dve", lambda e: e.tensor_tensor(out=Y[:, 0, kc, 0, :], in0=yl[:, 0, 0, :], in1=yl[:, 1, 0, :], op=ALU.add), reads=[t_yl], writes=[t_Y])
                    S.op("pool", lambda e: e.tensor_tensor(out=Y[:, 0, kc, 1, :], in0=yl[:, 0, 1, :], in1=yl[:, 1, 1, :], op=ALU.subtract), reads=[t_yl], writes=[t_Y])
                    S.op("dve", lambda e: e.tensor_tensor(out=Y[:, 1, kc, 0, :], in0=yl[:, 0, 0, :], in1=yl[:, 1, 0, :], op=ALU.subtract), reads=[t_yl], writes=[t_Y])
                    S.op("pool", lambda e: e.tensor_tensor(out=Y[:, 1, kc, 1, :], in0=yl[:, 0, 1, :], in1=yl[:, 1, 1, :], op=ALU.add), reads=[t_yl], writes=[t_Y])
                oi = 0
                for c2 in range(NH):
                    for par in range(2):
                        cc = par * NH + c2
                        b = oi % 2
                        oi += 1
                        zb = c2 % 2
                        S.dma(isl[:, b], IT[cc], writes=[t_isl[b]])
                        S.dma(xg[:, b], hsrc(1 + order)[par, :, c2, :], reads=[G.t_hyv], writes=[t_xg[b]])
                        for kc in range(KC):
                            for ri in range(2):
                                S.op("pe", lambda e: e.matmul(psy[:, b, 0:256], lhsT=isl[:, b, kc, ri, :], rhs=Y[:, par, kc, ri, :],
                                                              start=(kc == 0 and ri == 0), stop=(kc == KC - 1 and ri == 1)), reads=[t_isl[b], t_Y], writes=[t_psy[b]])
                        if order == 0:
                            S.op("dve", lambda e: e.tensor_tensor(out=zz1[:, cc, :], in0=psy[:, b, 0:256], in1=xg[:, b, :], op=ALU.mult),
                                 reads=[t_psy[b], t_xg[b]], writes=[t_zz1])
                        else:
                            S.op("dve", lambda e: e.tensor_tensor(out=zt[:], in0=psy[:, b, 0:256], in1=xg[:, b, :], op=ALU.mult),
                                 reads=[t_psy[b], t_xg[b]], writes=[t_zt])
                            for hh in range(2):
                                S.op("pe", lambda e: e.transpose(out=ptr[:, hh, :], in_=zt[:, hh * 128:(hh + 1) * 128], identity=G.identB),
                                     reads=[t_zt, G.t_c], writes=[t_ptr])
                            S.op("act", lambda e: e.copy(out=zT[:, zb].rearrange("p h (t two) -> p h t two", two=2)[:, :, :, par], in_=ptr[:]),
                                 reads=[t_ptr], writes=[t_zT[zb]])
                            if par == 1:
                                S.dma(mo[:, :, off + c2 * 256:off + (c2 + 1) * 256], zT[:, zb], reads=[t_zT[zb]], writes=[G.t_mix])
            S.barrier()
    if "hy" in G.dbg and li == 0:
        dump_bf16(G, G.mixT[768:896, 256:768], G.dbg["hy"], [G.t_mix])


def _hy_tables(L):
    N = 2 * L
    NCk = L // 128
    KC = (L // 2 + 1 + 127) // 128
    perm = np.concatenate([np.arange(0, L, 2), np.arange(1, L, 2)])
    n = perm.astype(np.int64)
    k = np.arange(KC * 128, dtype=np.int64)
    ang = ((n[:, None] * k[None, :]) % N).astype(np.float64) * (2 * np.pi / N)
    valid = (k <= L // 2).astype(np.float64)
    w = np.where(k == 0, 1.0, 2.0) / N * valid
    c, s_ = np.cos(ang), np.sin(ang)
    ft = np.stack([c * valid, -s_ * valid], axis=0)
    ft = ft.reshape(2, NCk, 128, KC, 128).transpose(3, 2, 1, 0, 4)
    it = np.stack([c * w, -s_ * w], axis=0)
    it = it.reshape(2, NCk, 128, KC, 128).transpose(1, 4, 3, 0, 2)
    f = np.float32
    nn = np.arange(L, dtype=f)
    t = nn / f(max(L - 1, 1))
    bands = np.linspace(1e-4, 15, 16, dtype=f)
    wpos = (f(2 * math.pi / L) * nn).astype(f)
    feats = np.concatenate([t[:, None], np.cos(wpos[:, None] * bands), -np.sin(wpos[:, None] * bands)], axis=-1).astype(f)
    deltas = np.abs(np.linspace(math.log(1e-2) / 1.5, math.log(1e-2) / 0.3, 256, dtype=f))
    win = np.exp(-t[:, None] * deltas).astype(f)
    winb = win.copy()
    winb[0] = 0.0
    feats = feats[perm]
    wn = np.stack([win, winb], axis=1)[perm].reshape(NCk, 128, 2, 256)
    mh = ((k != L // 2) & (k <= L // 2)).astype(f).reshape(KC, 128).T
    bf = ml_dtypes.bfloat16
    return (np.ascontiguousarray(ft).astype(bf), np.ascontiguousarray(it).astype(bf),
            np.ascontiguousarray(feats.T), np.ascontiguousarray(wn), np.ascontiguousarray(mh))
```

```python
import math
from contextlib import ExitStack
import numpy as np
import ml_dtypes
import concourse.bass as bass
import concourse.mybir as mybir
from concourse.bass_utils import run_bass_kernel_spmd

F32 = mybir.dt.float32
BF16 = mybir.dt.bfloat16
AF = mybir.ActivationFunctionType
ALU = mybir.AluOpType
AX = mybir.AxisListType

D = 1024
NCTX = 256
NLAT = 4096
NTOK = NCTX + NLAT
NT = NTOK // 128
DEPTH = 2
D_IN = 2444
EPS = 1e-6
HC = NTOK + 3
NE = 16
DFF = 256
DEBUG = False


def colof(tile):
    return 1 + 128 * tile if tile < 2 else 258 + 128 * (tile - 2)


BLOCKS = [(1, 0, 256, 0, 2)] + [(258 + 512 * j, 256 + 512 * j, 512, 2 + 4 * j, 4) for j in range(8)]


class Tok:
    __slots__ = ("w", "r")

    def __init__(self):
        self.w = None
        self.r = {}


class Sched:
    def __init__(self, nc, ndma=8, same_engine_sync=True):
        self.nc = nc
        self.eng = {"pe": nc.tensor, "act": nc.scalar, "dve": nc.vector, "pool": nc.gpsimd, "sp": nc.sync}
        self.semh = {}
        self.cnt = {}
        self.seen = {k: {} for k in self.eng}
        self.same = same_engine_sync
        for k in self.eng:
            self.semh[k] = nc.alloc_semaphore("s_" + k)
            self.cnt[k] = 0
        self.ndma = ndma
        self.dslot = {}
        self.dval = {}
        for q in ("sp", "pool"):
            self.dslot[q] = 0
            for i in range(ndma):
                key = ("dma", q, i)
                self.semh[key] = nc.alloc_semaphore("d_%s_%d" % (q, i))
                self.dval[key] = 0
        self.ninst = 0

    def _wait(self, e, deps):
        for (k, v) in sorted(deps, key=str):
            if k == e and (e == "pe" or not self.same):
                continue
            if self.seen[e].get(k, 0) >= v:
                continue
            self.eng[e].wait_ge(self.semh[k], v)
            self.seen[e][k] = v

    @staticmethod
    def _deps(reads, writes):
        deps = set()
        for t in reads:
            if t.w is not None:
                deps.add(t.w)
        for t in writes:
            if t.w is not None:
                deps.add(t.w)
            for kv in t.r.items():
                deps.add(kv)
        return deps

    @staticmethod
    def _mark(ev, reads, writes):
        k, v = ev
        for t in reads:
            if t.r.get(k, 0) < v:
                t.r[k] = v
        for t in writes:
            t.w = ev
            t.r = {}

    def op(self, e, fn, reads=(), writes=()):
        self._wait(e, self._deps(reads, writes))
        ins = fn(self.eng[e])
        self.cnt[e] += 1
        ins.then_inc(self.semh[e], 1)
        self._mark((e, self.cnt[e]), reads, writes)
        self.ninst += 1
        return ins

    def dma(self, out, in_, reads=(), writes=(), q="sp", **kw):
        i = self.dslot[q]
        self.dslot[q] = (i + 1) % self.ndma
        key = ("dma", q, i)
        deps = self._deps(reads, writes)
        if self.dval[key] > 0:
            deps.add((key, self.dval[key]))
        self._wait(q, deps)
        ins = self.eng[q].dma_start(out=out, in_=in_, **kw)
        self.dval[key] += 16
        ins.then_inc(self.semh[key], 16)
        self._mark((key, self.dval[key]), reads, writes)
        self.ninst += 1
        return ins

    def barrier(self):
        deps = set()
        for key, v in self.dval.items():
            if v > 0:
                deps.add((key, v))
        for k in self.eng:
            if self.cnt[k] > 0:
                deps.add((k, self.cnt[k]))
        for e in self.eng:
            self._wait(e, deps)


class Ctx:
    pass


_UID = [0]


def SB(nc, name, shape, dt):
    _UID[0] += 1
    return nc.sbuf_tensor("%s_%d" % (name, _UID[0]), shape, dt)


def PS(nc, name, shape, dt):
    _UID[0] += 1
    return nc.psum_tensor("%s_%d" % (name, _UID[0]), shape, dt)


def build(dbg=None):
    nc = bass.Bass("TRN2", target_bir_lowering=False)
    S = Sched(nc)
    G = Ctx()
    G.nc, G.S = nc, S

    def din(name, shape, dt=F32):
        return nc.dram_tensor(name, list(shape), dt, kind="ExternalInput").ap()

    def dscr(name, shape, dt):
        return nc.dram_tensor(name, list(shape), dt, kind="Internal").ap()

    I = {}
    I["x"] = din("x", [NLAT, D])
    I["ctx"] = din("ctx", [NCTX, D])
    I["w_mod"] = din("w_mod", [DEPTH, D, 6 * D])
    I["b_mod"] = din("b_mod", [DEPTH, 6 * D])
    I["w_in"] = din("w_in", [DEPTH, D, D_IN])
    I["w_out"] = din("w_out", [DEPTH, D, D])
    I["w_router"] = din("w_router", [D, NE])
    I["router_bias"] = din("router_bias", [NE])
    I["w_gate"] = din("w_gate", [DEPTH, NE, D, DFF])
    I["w_up"] = din("w_up", [DEPTH, NE, D, DFF])
    I["w_down"] = din("w_down", [DEPTH, NE, DFF, D])
    I["g_final"] = din("g_final", [D])
    I["cols"] = din("cols", [DEPTH, 128, NCOLS])
    I["cmat"] = din("cmat", [9, 128, 128])
    I["rope"] = din("rope", [2, 128, NLAT])
    for sname, P in SEGS.items():
        I["ft_" + sname] = din("ft_" + sname, [P["KC"], 128, P["NC"], 2, 128], BF16)
        I["it_" + sname] = din("it_" + sname, [P["NC"], 128, P["KC"], 2, 128], BF16)
        I["fe_" + sname] = din("fe_" + sname, [33, P["L"]])
        I["win_" + sname] = din("win_" + sname, [P["NC"], 128, 2, 256])
        I["mh_" + sname] = din("mh_" + sname, [128, P["KC"]])
    for nm, shp in (("hy_conv_w", [DEPTH, 3, 768]), ("hy_conv_b", [DEPTH, 768]), ("hy_w1", [DEPTH, 33, 64]), ("hy_w2", [DEPTH, 64, 64]),
                    ("hy_w3", [DEPTH, 64, 1024]), ("hy_bias", [DEPTH, 2, 256])):
        I[nm] = din(nm, shp)
    I["ssdmask"] = din("ssdmask", [2, 4, 128, 512], BF16)
    for nm, shp in (("ssd_conv_w", [DEPTH, 3, 640]), ("ssd_dt_bias", [DEPTH, 2, 6]), ("ssd_a_log", [DEPTH, 2, 6]),
                    ("ssd_d", [DEPTH, 6]), ("ssd_norm", [DEPTH, 384])):
        I[nm] = din(nm, shp)
    out = nc.dram_tensor("out", [NLAT, D], F32, kind="ExternalOutput").ap()
    G.I, G.out = I, out
    G.dbg = {}
    if dbg:
        for name, shape in dbg.items():
            G.dbg[name] = nc.dram_tensor("dbg_" + name, list(shape), F32, kind="ExternalOutput").ap()

    G.xres = dscr("xres", [NTOK, D], F32)
    G.t_xres = [Tok() for _ in range(NT)]
    G.wb_in = [dscr("wb_in%d" % i, [D, D_IN], BF16) for i in range(DEPTH)]
    G.wb_out = [dscr("wb_out%d" % i, [D, D], BF16) for i in range(DEPTH)]
    G.wb_gate = [dscr("wb_gate%d" % i, [NE, D, DFF], BF16) for i in range(DEPTH)]
    G.wb_up = [dscr("wb_up%d" % i, [NE, D, DFF], BF16) for i in range(DEPTH)]
    G.wb_down = [dscr("wb_down%d" % i, [NE, DFF, D], BF16) for i in range(DEPTH)]
    G.t_wb = Tok()
    G.mixT = dscr("mixT", [D, NTOK], BF16)
    G.hyv = dscr("hyv", [3, NTOK, 256], BF16)
    G.ssd_yb = dscr("ssd_yb", [NTOK, 384], F32)
    G.t_ssdyb = [Tok() for _ in range(NT)]
    G.t_hyv = Tok()
    G.Kf = {sn: dscr("Kf_" + sn, [2, P["KC"], 128, 2, 2, 256], BF16) for sn, P in SEGS.items()}
    G.t_mix = Tok()

    cm = nc.alloc_sbuf_tensor("cm", [128, 9, 128], F32)
    cmb = nc.alloc_sbuf_tensor("cmb", [128, 9, 128], BF16)
    ones = nc.alloc_sbuf_tensor("ones", [128, 128], F32)
    epsc = nc.alloc_sbuf_tensor("epsc", [128, 1], F32)
    G.t_c = Tok()
    S.dma(cm[:], I["cmat"].rearrange("a p c -> p a c"), writes=[G.t_c])
    S.op("dve", lambda e: e.tensor_copy(out=cmb[:], in_=cm[:]), reads=[G.t_c], writes=[G.t_c])
    S.op("dve", lambda e: e.memset(ones[:], 1.0), writes=[G.t_c])
    S.op("dve", lambda e: e.memset(epsc[:], EPS), writes=[G.t_c])
    G.cm, G.cmb, G.ones, G.epsc = cm, cmb, ones, epsc
    G.negpi = nc.alloc_sbuf_tensor("negpi", [128, 1], F32)
    S.op("dve", lambda e: e.memset(G.negpi[:], -math.pi), writes=[G.t_c])
    G.identF, G.identB = cm[:, 0, :], cmb[:, 0, :]

    S.dma(G.xres[0:NCTX, :], I["ctx"][:, :], writes=G.t_xres[0:2])
    for j in range(4):
        S.dma(G.xres[NCTX + 1024 * j:NCTX + 1024 * (j + 1), :], I["x"][1024 * j:1024 * (j + 1), :],
              writes=G.t_xres[2 + 8 * j:2 + 8 * (j + 1)])

    convert_weights(G)
    S.barrier()
    for li in range(1 if DEBUG else DEPTH):
        layer(G, li)
    S.barrier()
    return nc


def convert_weights(G):
    nc, S, I = G.nc, G.S, G.I
    CH = 4096
    with ExitStack() as _es:
        cf = _es.enter_context(SB(nc, "cv_f", [128, 2, CH], F32))
        cb = _es.enter_context(SB(nc, "cv_b", [128, 2, CH], BF16))
        tf = [Tok(), Tok()]
        tb = [Tok(), Tok()]
        n = 0
        engs = ["dve", "pool", "act"]
        for li in range(DEPTH):
            pairs = [(I["w_in"][li], G.wb_in[li], "a b -> (a b)"), (I["w_out"][li], G.wb_out[li], "a b -> (a b)"),
                     (I["w_gate"][li], G.wb_gate[li], "e a b -> (e a b)"), (I["w_up"][li], G.wb_up[li], "e a b -> (e a b)"),
                     (I["w_down"][li], G.wb_down[li], "e a b -> (e a b)")]
            for src, dst, pat in pairs:
                s1 = src.rearrange(pat).rearrange("(p m) -> p m", p=128)
                d1 = dst.rearrange(pat).rearrange("(p m) -> p m", p=128)
                M = s1.shape[1]
                for c0 in range(0, M, CH):
                    w = min(CH, M - c0)
                    k = n % 2
                    S.dma(cf[:, k, 0:w], s1[:, c0:c0 + w], writes=[tf[k]])
                    en = engs[n % 3]
                    if en == "act":
                        S.op("act", lambda e: e.copy(out=cb[:, k, 0:w], in_=cf[:, k, 0:w]), reads=[tf[k]], writes=[tb[k]])
                    else:
                        S.op(en, lambda e: e.tensor_copy(out=cb[:, k, 0:w], in_=cf[:, k, 0:w]), reads=[tf[k]], writes=[tb[k]])
                    S.dma(d1[:, c0:c0 + w], cb[:, k, 0:w], reads=[tb[k]], writes=[G.t_wb], q="pool")
                    n += 1


COLS = {}
_o = 0
for _name, _n in [("cc", 16), ("bmod", 32), ("g_mix", 8), ("g_ffn", 8), ("ssd_conv_b", 5), ("qg", 1), ("kg", 1),
                  ("ssd_d", 3), ("ssd_norm", 3), ("hy_b1", 1), ("hy_freq", 1), ("hy_b2", 1)]:
    COLS[_name] = (_o, _n)
    _o += _n
NCOLS = _o


def layer(G, li):
    nc, S, I = G.nc, G.S, G.I
    with ExitStack() as _es:
        cols = _es.enter_context(SB(nc, "cols", [128, NCOLS], F32))
        modc = _es.enter_context(SB(nc, "modc", [128, 4, 8, 2], F32))
        gtb = _es.enter_context(SB(nc, "gtb", [128, 2, 2, D], F32))
        G.cols, G.modc, G.gtb = cols, modc, gtb
        G.t_cols, G.t_modc, G.t_gtb = Tok(), Tok(), Tok()
        S.dma(cols[:], I["cols"][li], writes=[G.t_cols])
        adaln(G, li)
        S.barrier()
        with ExitStack() as _es:
            hT = _es.enter_context(SB(nc, "hT", [128, 8, HC], BF16))
            G.hT, G.t_hT = hT, Tok()
            norm_in(G, li)
            S.barrier()
            if "hy" in STAGES:
                hyena_inproj(G, li)
                S.barrier()
            if "att" in STAGES:
                attention(G, li)
                S.barrier()
            if "ssd" in STAGES:
                ssd(G, li)
                S.barrier()
        if "hy" in STAGES:
            hyena_fft(G, li)
            S.barrier()
        if "moe" in STAGES:
            with ExitStack() as _es:
                h2T = _es.enter_context(SB(nc, "h2T", [128, 8, NTOK], BF16))
                rl = _es.enter_context(SB(nc, "rl", [128, NT, NE], F32))
                G.h2T, G.t_h2T, G.rl, G.t_rl = h2T, Tok(), rl, Tok()
                outproj(G, li)
                S.barrier()
                moe(G, li)
                S.barrier()


STAGES = ("att", "ssd", "hy", "moe")


def colap(G, name, j=0, n=1, p0=0, p1=128):
    o, _ = COLS[name]
    return G.cols[p0:p1, o + j:o + j + n]


def adaln(G, li):
    nc, S, I = G.nc, G.S, G.I
    cols, modc, gtb = G.cols, G.modc, G.gtb
    with ExitStack() as _es:
        sc = _es.enter_context(SB(nc, "sc", [128, 8, 2], F32))
        screp = _es.enter_context(SB(nc, "screp", [128, 8, 2, 128], F32))
        wm = _es.enter_context(SB(nc, "wm", [128, 2, 8, 512], F32))
        brow = _es.enter_context(SB(nc, "brow", [128, 2, D], F32))
        ps_a = _es.enter_context(PS(nc, "ps_a", [128, 4, 2], F32))
        ps_g = _es.enter_context(PS(nc, "ps_g", [128, 2, 512], F32))
        t_sc, t_wm, t_pa, t_pg, t_brow = Tok(), [Tok(), Tok()], Tok(), Tok(), Tok()
        o = COLS["cc"][0]
        S.op("act", lambda e: e.activation(out=sc[:].rearrange("p k j -> p (k j)"), in_=cols[:, o:o + 16], func=AF.Silu),
             reads=[G.t_cols], writes=[t_sc])
        S.op("dve", lambda e: e.tensor_copy(out=screp[:].rearrange("p k j c -> p (k j) c"),
                                            in_=sc[:].rearrange("p k j -> p (k j)").unsqueeze(2).to_broadcast([128, 16, 128])),
             reads=[t_sc], writes=[t_sc])
        for g in range(2):
            S.dma(brow[:, g, :], I["b_mod"][li, (2 + 3 * g) * D:(3 + 3 * g) * D].partition_broadcast(128), writes=[t_brow])
        wv = I["w_mod"][li].rearrange("(k p) c -> p k c", p=128)
        ob = COLS["bmod"][0]
        for cj in range(12):
            b = cj % 2
            S.dma(wm[:, b], wv[:, :, cj * 512:(cj + 1) * 512], writes=[t_wm[b]])
            vec = cj // 2
            half = cj % 2
            if vec in (2, 5):
                g = 0 if vec == 2 else 1
                for j in range(2):
                    for kd in range(8):
                        S.op("pe", lambda e: e.matmul(ps_g[:, j, :], lhsT=screp[:, kd, j, :], rhs=wm[:, b, kd, :],
                                                      start=(kd == 0), stop=(kd == 7)), reads=[t_sc, t_wm[b]], writes=[t_pg])
                    S.op("dve", lambda e: e.tensor_tensor(out=gtb[:, g, j, half * 512:(half + 1) * 512], in0=ps_g[:, j, :],
                                                          in1=brow[:, g, half * 512:(half + 1) * 512], op=ALU.add),
                         reads=[t_pg, t_brow], writes=[G.t_gtb])
            else:
                v = {0: 0, 1: 1, 3: 2, 4: 3}[vec]
                for fc in range(4):
                    for kd in range(8):
                        S.op("pe", lambda e: e.matmul(ps_a[:, fc, :], lhsT=wm[:, b, kd, fc * 128:(fc + 1) * 128], rhs=sc[:, kd, :],
                                                      start=(kd == 0), stop=(kd == 7)), reads=[t_sc, t_wm[b]], writes=[t_pa])
                k0 = half * 4
                S.op("dve", lambda e: e.tensor_tensor(out=modc[:, v, k0:k0 + 4, :], in0=ps_a[:],
                                                      in1=cols[:, ob + v * 8 + k0:ob + v * 8 + k0 + 4].unsqueeze(2).to_broadcast([128, 4, 2]),
                                                      op=ALU.add), reads=[t_pa, G.t_cols], writes=[G.t_modc])
        for v, gname in ((1, "g_mix"), (3, "g_ffn")):
            og = COLS[gname][0]
            S.op("dve", lambda e: e.scalar_tensor_tensor(out=modc[:, v], in0=modc[:, v], scalar=1.0,
                                                         in1=cols[:, og:og + 8].unsqueeze(2).to_broadcast([128, 8, 2]),
                                                         op0=ALU.add, op1=ALU.mult), reads=[G.t_modc, G.t_cols], writes=[G.t_modc])


def rms_to_T(G, xt, t_x, tile, vA, vB, dstT, t_dst, dcol, pool):
    nc, S = G.nc, G.S
    sq, ss, xn, ps_t, toks = pool
    t_sq, t_ss, t_xn, t_ps = toks
    j = 1 if tile < 2 else 0
    S.op("act", lambda e: e.activation(out=sq[:], in_=xt, func=AF.Square, accum_out=ss[:, 0:1]), reads=[t_x], writes=[t_sq, t_ss])
    S.op("act", lambda e: e.activation(out=ss[:, 1:2], in_=ss[:, 0:1], func=AF.Sqrt, bias=G.epsc[:], scale=1.0 / D),
         reads=[t_ss, G.t_c], writes=[t_ss])
    S.op("dve", lambda e: e.reciprocal(out=ss[:, 2:3], in_=ss[:, 1:2]), reads=[t_ss], writes=[t_ss])
    S.op("dve", lambda e: e.tensor_scalar(out=xn[:], in0=xt, scalar1=ss[:, 2:3], scalar2=None, op0=ALU.mult),
         reads=[t_x, t_ss], writes=[t_xn])
    for k in range(8):
        S.op("pe", lambda e: e.transpose(out=ps_t[:, k, :], in_=xn[:, k * 128:(k + 1) * 128], identity=G.identB),
             reads=[t_xn, G.t_c], writes=[t_ps])
    S.op("dve", lambda e: e.tensor_tensor(out=sq[:].rearrange("p (k c) -> p k c", k=8), in0=ps_t[:],
                                          in1=G.modc[:, vA, :, j:j + 1].to_broadcast([128, 8, 128]), op=ALU.mult),
         reads=[t_ps, G.t_modc], writes=[t_sq])
    S.op("dve", lambda e: e.tensor_tensor(out=dstT[:, :, dcol:dcol + 128], in0=sq[:].rearrange("p (k c) -> p k c", k=8),
                                          in1=G.modc[:, vB, :, j:j + 1].to_broadcast([128, 8, 128]), op=ALU.add),
         reads=[t_sq, G.t_modc], writes=[t_dst])


def norm_in(G, li):
    nc, S = G.nc, G.S
    hT = G.hT
    with ExitStack() as _es:
        xt = _es.enter_context(SB(nc, "xt", [128, 2, D], F32))
        sq = _es.enter_context(SB(nc, "sq", [128, 2, D], F32))
        ss = _es.enter_context(SB(nc, "ss", [128, 2, 4], F32))
        xn = _es.enter_context(SB(nc, "xn", [128, 2, D], BF16))
        ps_t = _es.enter_context(PS(nc, "ps_t", [128, 2, 8, 128], BF16))
        t_x = [Tok(), Tok()]
        pools = [(sq[:, i, :], ss[:, i, :], xn[:, i, :], ps_t[:, i], (Tok(), Tok(), Tok(), Tok())) for i in range(2)]
        for c in (0, 257, HC - 1):
            S.op("pool", lambda e: e.memset(hT[:, :, c:c + 1], 0.0), writes=[G.t_hT])
        for tile in range(NT):
            b = tile % 2
            S.dma(xt[:, b, :], G.xres[tile * 128:(tile + 1) * 128, :], reads=[G.t_xres[tile]], writes=[t_x[b]])
            rms_to_T(G, xt[:, b, :], t_x[b], tile, 1, 0, hT, G.t_hT, colof(tile), pools[b])
        if "hT" in G.dbg and li == 0:
            with ExitStack() as _es:
                dh = _es.enter_context(SB(nc, "dbgh", [128, 8, 512], F32))
                t = Tok()
                S.op("dve", lambda e: e.tensor_copy(out=dh[:], in_=hT[:, :, 0:512]), reads=[G.t_hT], writes=[t])
                S.dma(G.dbg["hT"].rearrange("(k p) c -> p k c", p=128), dh[:], reads=[t])


def attention(G, li):
    nc, S, I = G.nc, G.S, G.I
    hT = G.hT
    need_ctx = li < DEPTH - 1
    wv = G.wb_in[li].rearrange("(k p) c -> p k c", p=128)
    scale = 64 ** -0.5
    with ExitStack() as _es:
        wq = _es.enter_context(SB(nc, "wqkv", [128, 8, 640], BF16))
        qT = _es.enter_context(SB(nc, "qT", [128, 6, NTOK], BF16))
        kT = _es.enter_context(SB(nc, "kT", [128, NTOK], BF16))
        vp = _es.enter_context(SB(nc, "vp", [128, NT, 2, 128], BF16))
        rp = _es.enter_context(SB(nc, "rp", [128, 2, 2, 512], F32))
        qs = _es.enter_context(SB(nc, "qs", [128, 512], F32))
        q2 = _es.enter_context(SB(nc, "q2", [128, 512], F32))
        qn = _es.enter_context(SB(nc, "qn", [128, 512], F32))
        qnb = _es.enter_context(SB(nc, "qnb", [128, 512], BF16))
        pT = _es.enter_context(SB(nc, "pT", [128, 2, 2, 512], BF16))
        rd = _es.enter_context(SB(nc, "rd", [128, 2, 512], F32))
        ao = _es.enter_context(SB(nc, "ao", [128, 2, 512], BF16))
        ps_q = _es.enter_context(PS(nc, "ps_q", [128, 512], F32))
        ps_r = _es.enter_context(PS(nc, "ps_r", [128, 512], F32))
        ps_s = _es.enter_context(PS(nc, "ps_s", [128, 2, 2, 512], F32))
        ps_o = _es.enter_context(PS(nc, "ps_o", [128, 2, 512], F32))
        t_w, t_q, t_k, t_v, t_rp = Tok(), Tok(), Tok(), Tok(), [Tok(), Tok()]
        t_qs, t_q2, t_qn, t_qnb, t_psq, t_psr = Tok(), Tok(), Tok(), Tok(), Tok(), Tok()
        t_pT, t_pss, t_pso, t_rd, t_ao = [Tok(), Tok()], [Tok(), Tok()], [Tok(), Tok()], [Tok(), Tok()], [Tok(), Tok()]
        for j in range(3):
            S.dma(wq[:, :, j * 128:j * 128 + 64], wv[:, :, j * 64:(j + 1) * 64], reads=[G.t_wb], writes=[t_w])
            S.dma(wq[:, :, j * 128 + 64:(j + 1) * 128], wv[:, :, (3 + j) * 64:(4 + j) * 64], reads=[G.t_wb], writes=[t_w])
        S.dma(wq[:, :, 384:640], wv[:, :, 384:640], reads=[G.t_wb], writes=[t_w])
        S.op("pool", lambda e: e.memset(vp[:, :, :, 64:128], 1.0), writes=[t_v])
        S.op("pool", lambda e: e.memset(qT[64:128, 0:3, :], 0.0), writes=[t_q])
        S.op("pool", lambda e: e.memset(qT[0:64, 3:6, :], 0.0), writes=[t_q])
        og = {0: COLS["qg"][0], 1: COLS["qg"][0], 2: COLS["qg"][0], 3: COLS["kg"][0]}
        for bi, (c0, t0, n, tile0, ntile) in enumerate(BLOCKS):
            if bi > 0:
                b = bi % 2
                S.dma(rp[:, b, :, :], I["rope"][:, :, t0 - NCTX:t0 - NCTX + 512].rearrange("a p c -> p a c"), writes=[t_rp[b]])
            for ch in range(4):
                for k in range(8):
                    S.op("pe", lambda e: e.matmul(ps_q[:, 0:n], lhsT=wq[:, k, ch * 128:(ch + 1) * 128], rhs=hT[:, k, c0:c0 + n],
                                                  start=(k == 0), stop=(k == 7)), reads=[t_w, G.t_hT], writes=[t_psq])
                S.op("act", lambda e: e.copy(out=qs[:, 0:n], in_=ps_q[:, 0:n]), reads=[t_psq], writes=[t_qs])
                S.op("act", lambda e: e.activation(out=q2[:, 0:n], in_=qs[:, 0:n], func=AF.Square), reads=[t_qs], writes=[t_q2])
                S.op("pe", lambda e: e.matmul(ps_r[:, 0:n], lhsT=G.cm[:, 3, :], rhs=q2[:, 0:n], start=True, stop=True),
                     reads=[t_q2, G.t_c], writes=[t_psr])
                S.op("act", lambda e: e.activation(out=q2[:, 0:n], in_=ps_r[:, 0:n], func=AF.Sqrt, bias=G.epsc[:], scale=1.0 / 64),
                     reads=[t_psr, G.t_c], writes=[t_q2])
                S.op("dve", lambda e: e.reciprocal(out=q2[:, 0:n], in_=q2[:, 0:n]), reads=[t_q2], writes=[t_q2])
                S.op("dve", lambda e: e.scalar_tensor_tensor(out=qn[:, 0:n], in0=qs[:, 0:n], scalar=G.cols[:, og[ch]:og[ch] + 1],
                                                             in1=q2[:, 0:n], op0=ALU.mult, op1=ALU.mult),
                     reads=[t_qs, t_q2, G.t_cols], writes=[t_qn])
                t_dst = t_q if ch < 3 else t_k
                halves = [(0, 64, qT[0:64, ch, t0:t0 + n]), (64, 128, qT[64:128, 3 + ch, t0:t0 + n])] if ch < 3 else [(0, 128, kT[:, t0:t0 + n])]
                if bi == 0:
                    for (p0, p1, dst) in halves:
                        S.op("dve", lambda e: e.tensor_copy(out=dst, in_=qn[p0:p1, 0:n]), reads=[t_qn], writes=[t_dst])
                else:
                    b = bi % 2
                    S.op("dve", lambda e: e.tensor_copy(out=qnb[:, 0:n], in_=qn[:, 0:n]), reads=[t_qn], writes=[t_qnb])
                    S.op("pe", lambda e: e.matmul(ps_r[:, 0:n], lhsT=G.cmb[:, 4, :], rhs=qnb[:, 0:n], start=True, stop=True),
                         reads=[t_qnb, G.t_c], writes=[t_psr])
                    S.op("dve", lambda e: e.tensor_tensor(out=qs[:, 0:n], in0=ps_r[:, 0:n], in1=rp[:, b, 1, 0:n], op=ALU.mult),
                         reads=[t_psr, t_rp[b]], writes=[t_qs])
                    S.op("dve", lambda e: e.tensor_tensor(out=qn[:, 0:n], in0=qn[:, 0:n], in1=rp[:, b, 0, 0:n], op=ALU.mult),
                         reads=[t_qn, t_rp[b]], writes=[t_qn])
                    for (p0, p1, dst) in halves:
                        S.op("dve", lambda e: e.tensor_tensor(out=dst, in0=qn[p0:p1, 0:n], in1=qs[p0:p1, 0:n], op=ALU.add),
                             reads=[t_qn, t_qs], writes=[t_dst])
            for tl in range(tile0, tile0 + ntile):
                cc = colof(tl)
                for k in range(8):
                    S.op("pe", lambda e: e.matmul(ps_q[:, 0:128], lhsT=hT[:, k, cc:cc + 128], rhs=wq[:, k, 512:640],
                                                  start=(k == 0), stop=(k == 7)), reads=[t_w, G.t_hT], writes=[t_psq])
                S.op("act", lambda e: e.copy(out=vp[:, tl, :, 0:64], in_=ps_q[:, 0:128].rearrange("p (a d) -> p a d", a=2)),
                     reads=[t_psq], writes=[t_v])
        pairs = []
        oi = 0
        for h in range(6):
            for bi, (c0, t0, n, tile0, ntile) in enumerate(BLOCKS):
                if bi == 0 and not need_ctx:
                    continue
                kcs = list(range(2)) if bi == 0 else list(range(NT))
                for ki in range(0, len(kcs), 2):
                    pairs.append((h, t0, n, kcs[ki], ki == 0, ki + 2 >= len(kcs), oi % 2))
                oi += 1

        def qk(j):
            h, t0, n, kc, first, last, ob = pairs[j]
            pb = (h // 3) * 64
            sb = j % 2
            for u in range(2):
                S.op("pe", lambda e: e.matmul(ps_s[:, sb, u, 0:n], lhsT=kT[:, (kc + u) * 128:(kc + u + 1) * 128],
                                              rhs=qT[:, h, t0:t0 + n], start=True, stop=True),
                     reads=[t_q, t_k], writes=[t_pss[sb]])

        qk(0)
        for j, (h, t0, n, kc, first, last, ob) in enumerate(pairs):
            sb = j % 2
            kv = h // 3
            if j + 1 < len(pairs):
                qk(j + 1)
            S.op("act", lambda e: e.activation(out=pT[:, sb, :, 0:n], in_=ps_s[:, sb, :, 0:n], func=AF.Exp, scale=scale),
                 reads=[t_pss[sb]], writes=[t_pT[sb]])
            for u in range(2):
                S.op("pe", lambda e: e.matmul(ps_o[:, ob, 0:n], lhsT=vp[:, kc + u, kv, :], rhs=pT[:, sb, u, 0:n],
                                              start=(first and u == 0), stop=(last and u == 1)),
                     reads=[t_v, t_pT[sb]], writes=[t_pso[ob]])
            if last:
                S.op("dve", lambda e: e.reciprocal(out=rd[0:64, ob, 0:n], in_=ps_o[64:128, ob, 0:n]), reads=[t_pso[ob]], writes=[t_rd[ob]])
                S.op("dve", lambda e: e.tensor_tensor(out=ao[0:64, ob, 0:n], in0=ps_o[0:64, ob, 0:n], in1=rd[0:64, ob, 0:n], op=ALU.mult),
                     reads=[t_pso[ob], t_rd[ob]], writes=[t_ao[ob]])
                S.dma(G.mixT[h * 64:(h + 1) * 64, t0:t0 + n], ao[0:64, ob, 0:n], reads=[t_ao[ob]], writes=[G.t_mix])
        if "att" in G.dbg and li == 0:
            dump_bf16(G, G.mixT[0:128, 256:768], G.dbg["att"], [G.t_mix])


def dump_bf16(G, src, dst, reads):
    nc, S = G.nc, G.S
    p, n = src.shape
    with ExitStack() as _es:
        a = _es.enter_context(SB(nc, "dmpb", [p, n], BF16))
        b = _es.enter_context(SB(nc, "dmpf", [p, n], F32))
        t = Tok()
        S.dma(a[:], src, reads=reads, writes=[t])
        S.op("dve", lambda e: e.tensor_copy(out=b[:], in_=a[:]), reads=[t], writes=[t])
        S.dma(dst, b[:], reads=[t])
        S.barrier()


def _cols_pack(inp, li, b):
    def colform(v, n):
        return np.ascontiguousarray(v.reshape(n, 128).T)
    parts = {}
    cc = np.zeros((128, 8, 2), np.float32)
    cc[:, :, 0] = colform(inp["c"][b], 8)
    cc[:, :, 1] = colform(inp["c_ctx"], 8)
    parts["cc"] = cc.reshape(128, 16)
    bm = inp["b_mod"][li].reshape(6, 8, 128)
    parts["bmod"] = np.concatenate([bm[v].T for v in (0, 1, 3, 4)], axis=1)
    parts["g_mix"] = colform(inp["g_mix"][li], 8)
    parts["g_ffn"] = colform(inp["g_ffn"][li], 8)
    parts["ssd_conv_b"] = colform(inp["ssd_conv_b"][li], 5)
    parts["qg"] = np.tile(inp["q_norm"][li], 2)[:, None]
    parts["kg"] = np.tile(inp["k_norm"][li], 2)[:, None]
    parts["ssd_d"] = colform(np.repeat(inp["ssd_d"][li], 64), 3)
    parts["ssd_norm"] = colform(inp["ssd_norm"][li], 3)
    for nm in ("hy_b1", "hy_freq", "hy_b2"):
        parts[nm] = np.tile(inp[nm][li], 2)[:, None]
    out = np.zeros((128, NCOLS), np.float32)
    for nm, (o, n) in COLS.items():
        out[:, o:o + n] = parts[nm]
    return out


def _consts():
    ident = np.eye(128, dtype=np.float32)
    s = np.arange(128)
    U = (s[:, None] <= s[None, :]).astype(np.float32)
    Lo = (s[:, None] >= s[None, :]).astype(np.float32)
    bo = np.kron(np.eye(2, dtype=np.float32), np.ones((64, 64), np.float32))
    rot = np.zeros((128, 128), np.float32)
    for hb in (0, 64):
        for d in range(32):
            rot[hb + d + 32, hb + d] = -1.0
            rot[hb + d, hb + d + 32] = 1.0
    top = np.zeros((128, 128), np.float32); top[:64] = 1.0
    bot = np.zeros((128, 128), np.float32); bot[64:] = 1.0
    cmat = np.stack([ident, U, Lo, bo, rot, top, bot, U - ident, Lo - ident])
    rows = NLAT // 64
    row = np.repeat(np.arange(rows), 64).astype(np.float32)
    col = np.tile(np.arange(64), rows).astype(np.float32)
    inv = (10000.0 ** (-np.arange(0, 32, 2, dtype=np.float32) / 32)).astype(np.float32)
    ang = np.concatenate([row[:, None] * inv, col[:, None] * inv], axis=-1).astype(np.float32)
    cs = np.cos(ang).astype(np.float32).T
    sn = np.sin(ang).astype(np.float32).T
    rope = np.stack([np.tile(cs, (4, 1)), np.tile(sn, (4, 1))]).astype(np.float32)
    tt = np.arange(512)[None, None, :]
    ss_ = np.arange(128)[None, :, None]
    jj = np.arange(4)[:, None, None]
    mf = (tt >= 128 * jj + ss_).astype(np.float32)
    mb = (tt <= 128 * jj + ss_).astype(np.float32)
    hyt = {}
    for sname, P in SEGS.items():
        ft, it_, fe, wn, mh = _hy_tables(P["L"])
        hyt["ft_" + sname], hyt["it_" + sname], hyt["fe_" + sname], hyt["win_" + sname], hyt["mh_" + sname] = ft, it_, fe, wn, mh
    return {**hyt, "cmat": cmat, "rope": rope, "ssdmask": np.stack([mf, mb]).astype(ml_dtypes.bfloat16)}


_CONSTS = None


def kernel(**inp):
    global _CONSTS
    inp = {k: np.asarray(v) for k, v in inp.items()}
    if _CONSTS is None:
        _CONSTS = _consts()
    dbg = kernel.dbg if hasattr(kernel, "dbg") else None
    nc = build(dbg)
    ncores = 8
    in_maps = []
    for core in range(ncores):
        b = core % 4
        m = {"x": np.ascontiguousarray(inp["x"][b]), "ctx": np.ascontiguousarray(inp["ctx"][b])}
        for k in ("w_mod", "b_mod", "w_in", "w_out", "w_router", "router_bias", "w_gate", "w_up", "w_down", "g_final",
                  "ssd_conv_w", "ssd_dt_bias", "ssd_a_log", "ssd_d", "ssd_norm", "hy_conv_w", "hy_conv_b", "hy_w1", "hy_w2", "hy_w3", "hy_bias"):
            m[k] = inp[k]
        m["cols"] = np.stack([_cols_pack(inp, li, b) for li in range(DEPTH)])
        m.update(_CONSTS)
        in_maps.append(m)
    res = run_bass_kernel_spmd(nc, in_maps, core_ids=list(range(ncores)))
    kernel.last = res
    return np.stack([res.results[b]["out"] for b in range(4)]).astype(np.float32)


def outproj(G, li):
    nc, S, I = G.nc, G.S, G.I
    need_ctx = li < DEPTH - 1
    with ExitStack() as _es:
        wo = _es.enter_context(SB(nc, "wo", [128, 8, D], BF16))
        mx = _es.enter_context(SB(nc, "mx", [128, 2, 8, 128], BF16))
        xt = _es.enter_context(SB(nc, "xt", [128, 2, D], F32))
        tmp = _es.enter_context(SB(nc, "tmp", [128, D], F32))
        sq = _es.enter_context(SB(nc, "sq", [128, D], F32))
        ss = _es.enter_context(SB(nc, "ss", [128, 4], F32))
        xn = _es.enter_context(SB(nc, "xn", [128, D], F32))
        h2f = _es.enter_context(SB(nc, "h2f", [128, 8, 128], F32))
        wr = _es.enter_context(SB(nc, "wr", [128, 8, NE], F32))
        po = _es.enter_context(PS(nc, "po", [128, 2, 512], F32))
        pt = _es.enter_context(PS(nc, "pt", [128, 8, 128], F32))
        pr = _es.enter_context(PS(nc, "pr", [128, NE], F32))
        t_wo, t_mx, t_x, t_tmp, t_po = Tok(), [Tok(), Tok()], [Tok(), Tok()], Tok(), Tok()
        t_sq, t_ss, t_xn, t_pt, t_h2f, t_wr, t_pr = Tok(), Tok(), Tok(), Tok(), Tok(), Tok(), Tok()
        S.dma(wo[:], G.wb_out[li].rearrange("(k p) c -> p k c", p=128), reads=[G.t_wb], writes=[t_wo])
        S.dma(wr[:], I["w_router"].rearrange("(k p) c -> p k c", p=128), writes=[t_wr])
        mv = G.mixT.rearrange("(k p) t -> p k t", p=128)
        tiles = [t for t in range(NT) if need_ctx or t >= 2]

        def mm(tile):
            b = tile % 2
            S.dma(mx[:, b], mv[:, :, tile * 128:(tile + 1) * 128], reads=[G.t_mix], writes=[t_mx[b]])
            S.dma(xt[:, b, :], G.xres[tile * 128:(tile + 1) * 128, :], reads=[G.t_xres[tile]], writes=[t_x[b]])
            for half in range(2):
                for k in range(8):
                    S.op("pe", lambda e: e.matmul(po[:, half, :], lhsT=mx[:, b, k, :], rhs=wo[:, k, half * 512:(half + 1) * 512],
                                                  start=(k == 0), stop=(k == 7)), reads=[t_mx[b], t_wo], writes=[t_po])

        mm(tiles[0])
        for ti, tile in enumerate(tiles):
            b = tile % 2
            j = 1 if tile < 2 else 0
            S.op("dve", lambda e: e.tensor_tensor(out=tmp[:], in0=po[:].rearrange("p a c -> p (a c)"), in1=G.gtb[:, 0, j, :], op=ALU.mult),
                 reads=[t_po, G.t_gtb], writes=[t_tmp])
            if ti + 1 < len(tiles):
                mm(tiles[ti + 1])
            S.op("dve", lambda e: e.tensor_tensor(out=xt[:, b, :], in0=tmp[:], in1=xt[:, b, :], op=ALU.add),
                 reads=[t_tmp, t_x[b]], writes=[t_x[b]])
            S.dma(G.xres[tile * 128:(tile + 1) * 128, :], xt[:, b, :], reads=[t_x[b]], writes=[G.t_xres[tile]])
            xv = xt[:, b, :]
            S.op("act", lambda e: e.activation(out=sq[:], in_=xv, func=AF.Square, accum_out=ss[:, 0:1]), reads=[t_x[b]], writes=[t_sq, t_ss])
            S.op("act", lambda e: e.activation(out=ss[:, 1:2], in_=ss[:, 0:1], func=AF.Sqrt, bias=G.epsc[:], scale=1.0 / D),
                 reads=[t_ss, G.t_c], writes=[t_ss])
            S.op("dve", lambda e: e.reciprocal(out=ss[:, 2:3], in_=ss[:, 1:2]), reads=[t_ss], writes=[t_ss])
            S.op("dve", lambda e: e.tensor_scalar(out=xn[:], in0=xv, scalar1=ss[:, 2:3], scalar2=None, op0=ALU.mult),
                 reads=[t_x[b], t_ss], writes=[t_xn])
            for k in range(8):
                S.op("pe", lambda e: e.transpose(out=pt[:, k, :], in_=xn[:, k * 128:(k + 1) * 128], identity=G.identF),
                     reads=[t_xn, G.t_c], writes=[t_pt])
            S.op("dve", lambda e: e.tensor_tensor(out=h2f[:], in0=pt[:], in1=G.modc[:, 3, :, j:j + 1].to_broadcast([128, 8, 128]), op=ALU.mult),
                 reads=[t_pt, G.t_modc], writes=[t_h2f])
            S.op("dve", lambda e: e.tensor_tensor(out=h2f[:], in0=h2f[:], in1=G.modc[:, 2, :, j:j + 1].to_broadcast([128, 8, 128]), op=ALU.add),
                 reads=[t_h2f, G.t_modc], writes=[t_h2f])
            S.op("act", lambda e: e.copy(out=G.h2T[:, :, tile * 128:(tile + 1) * 128], in_=h2f[:]), reads=[t_h2f], writes=[G.t_h2T])
            for k in range(8):
                S.op("pe", lambda e: e.matmul(pr[:], lhsT=h2f[:, k, :], rhs=wr[:, k, :], start=(k == 0), stop=(k == 7)),
                     reads=[t_h2f, t_wr], writes=[t_pr])
            S.op("dve", lambda e: e.tensor_copy(out=G.rl[:, tile, :], in_=pr[:]), reads=[t_pr], writes=[G.t_rl])


def moe(G, li):
    nc, S, I = G.nc, G.S, G.I
    need_ctx = li < DEPTH - 1
    last = li == DEPTH - 1
    h2T, rl = G.h2T, G.rl
    T0 = 0 if need_ctx else 2
    NTl = NT - T0
    BIG = 1.0e9
    with ExitStack() as _es:
        comb = _es.enter_context(SB(nc, "comb", [128, NT, NE], F32))
        t_comb = Tok()
        with ExitStack() as _es:
            sc = _es.enter_context(SB(nc, "r_sc", [128, NT, NE], F32))
            sel = _es.enter_context(SB(nc, "r_sel", [128, NT, NE], F32))
            ra = _es.enter_context(SB(nc, "r_a", [128, NT, NE], F32))
            rb = _es.enter_context(SB(nc, "r_b", [128, NT, NE], F32))
            rm = _es.enter_context(SB(nc, "r_m", [128, NT * 4], F32))
            rm2 = _es.enter_context(SB(nc, "r_m2", [128, NT * 4], F32))
            rg = _es.enter_context(SB(nc, "r_g", [128, NT], F32))
            rbias = _es.enter_context(SB(nc, "rbias", [128, NE], F32))
            t = Tok()
            if T0 > 0:
                S.op("dve", lambda e: e.memset(rl[:, 0:T0, :], 0.0), reads=[G.t_rl], writes=[G.t_rl])
            S.dma(rbias[:], I["router_bias"].partition_broadcast(128), writes=[t])
            v3 = lambda a: a[:].rearrange("p n (g x) -> p (n g) x", x=4)
            S.op("act", lambda e: e.activation(out=sc[:], in_=rl[:], func=AF.Sigmoid), reads=[G.t_rl], writes=[t])
            S.op("dve", lambda e: e.tensor_tensor(out=sel[:], in0=sc[:], in1=rbias[:].unsqueeze(1).to_broadcast([128, NT, NE]), op=ALU.add),
                 reads=[t], writes=[t])
            S.op("dve", lambda e: e.tensor_reduce(out=rm[:], in_=v3(sel), axis=AX.X, op=ALU.max), reads=[t], writes=[t])
            S.op("dve", lambda e: e.tensor_tensor(out=v3(ra), in0=v3(sel), in1=rm[:].unsqueeze(2).to_broadcast([128, NT * 4, 4]), op=ALU.is_equal),
                 reads=[t], writes=[t])
            S.op("dve", lambda e: e.scalar_tensor_tensor(out=rb[:], in0=ra[:], scalar=-BIG, in1=sel[:], op0=ALU.mult, op1=ALU.add),
                 reads=[t], writes=[t])
            S.op("dve", lambda e: e.tensor_reduce(out=rm2[:], in_=v3(rb), axis=AX.X, op=ALU.max), reads=[t], writes=[t])
            S.op("dve", lambda e: e.tensor_tensor(out=rm[:], in0=rm[:], in1=rm2[:], op=ALU.add), reads=[t], writes=[t])
            S.op("dve", lambda e: e.tensor_reduce(out=rg[:], in_=rm[:].rearrange("p (n g) -> p n g", g=4), axis=AX.X, op=ALU.max),
                 reads=[t], writes=[t])
            S.op("dve", lambda e: e.tensor_tensor(out=rm2[:].rearrange("p (n g) -> p n g", g=4), in0=rm[:].rearrange("p (n g) -> p n g", g=4),
                                                  in1=rg[:].unsqueeze(2).to_broadcast([128, NT, 4]), op=ALU.is_equal), reads=[t], writes=[t])
            S.op("dve", lambda e: e.tensor_scalar(out=rm2[:], in0=rm2[:], scalar1=1.0, scalar2=BIG, op0=ALU.subtract, op1=ALU.mult),
                 reads=[t], writes=[t])
            S.op("dve", lambda e: e.tensor_tensor(out=v3(sel), in0=v3(sel), in1=rm2[:].unsqueeze(2).to_broadcast([128, NT * 4, 4]), op=ALU.add),
                 reads=[t], writes=[t])
            S.op("dve", lambda e: e.tensor_reduce(out=rg[:], in_=sel[:], axis=AX.X, op=ALU.max), reads=[t], writes=[t])
            S.op("dve", lambda e: e.tensor_tensor(out=ra[:], in0=sel[:], in1=rg[:].unsqueeze(2).to_broadcast([128, NT, NE]), op=ALU.is_equal),
                 reads=[t], writes=[t])
            S.op("dve", lambda e: e.scalar_tensor_tensor(out=sel[:], in0=ra[:], scalar=-BIG, in1=sel[:], op0=ALU.mult, op1=ALU.add),
                 reads=[t], writes=[t])
            S.op("dve", lambda e: e.tensor_reduce(out=rg[:], in_=sel[:], axis=AX.X, op=ALU.max), reads=[t], writes=[t])
            S.op("dve", lambda e: e.tensor_tensor(out=rb[:], in0=sel[:], in1=rg[:].unsqueeze(2).to_broadcast([128, NT, NE]), op=ALU.is_equal),
                 reads=[t], writes=[t])
            S.op("dve", lambda e: e.tensor_tensor(out=ra[:], in0=ra[:], in1=rb[:], op=ALU.add), reads=[t], writes=[t])
            S.op("dve", lambda e: e.tensor_tensor(out=ra[:], in0=ra[:], in1=sc[:], op=ALU.mult), reads=[t], writes=[t])
            S.op("dve", lambda e: e.tensor_reduce(out=rg[:], in_=ra[:], axis=AX.X, op=ALU.add), reads=[t], writes=[t])
            S.op("dve", lambda e: e.reciprocal(out=rg[:], in_=rg[:]), reads=[t], writes=[t])
            S.op("dve", lambda e: e.tensor_tensor(out=comb[:], in0=ra[:], in1=rg[:].unsqueeze(2).to_broadcast([128, NT, NE]), op=ALU.mult),
                 reads=[t], writes=[t_comb])
            S.barrier()
        SGT = 12
        with ExitStack() as _es:
            acc = _es.enter_context(SB(nc, "acc", [128, SGT, D], F32))
            wg = _es.enter_context(SB(nc, "wg", [128, 2, 8, DFF], BF16))
            wu = _es.enter_context(SB(nc, "wu", [128, 2, 8, DFF], BF16))
            wd = _es.enter_context(SB(nc, "wd", [128, 2, 2, D], BF16))
            sgl = _es.enter_context(SB(nc, "sgl", [128, 2, 512], F32))
            aa2 = _es.enter_context(SB(nc, "aa", [128, 2, 2, 512], BF16))
            xt = _es.enter_context(SB(nc, "xt", [128, 2, D], F32))
            gfb = _es.enter_context(SB(nc, "gfb", [128, D], F32))
            ss = _es.enter_context(SB(nc, "ss", [128, 4], F32))
            sq = _es.enter_context(SB(nc, "sq", [128, D], F32))
            pgu = _es.enter_context(PS(nc, "pgu", [128, 4, 512], F32))
            py = _es.enter_context(PS(nc, "py", [128, 2, 2, 512], F32))
            t_pgu2, t_sgl2, t_aa2 = [Tok(), Tok()], [Tok(), Tok()], [Tok(), Tok()]
            t_acc, t_w, t_sgl, t_aa, t_pgu, t_py, t_x, t_gf, t_ss, t_sq = Tok(), [Tok(), Tok()], Tok(), Tok(), Tok(), [Tok(), Tok()], [Tok(), Tok()], Tok(), Tok(), Tok()
            if last:
                S.dma(gfb[:], I["g_final"].partition_broadcast(128), writes=[t_gf])
            yi = 0
            ui = 0
            for s0 in range(T0, NT, SGT):
                tiles = list(range(s0, min(NT, s0 + SGT)))
                units = []
                for ex in range(NE):
                    for b0 in range(0, len(tiles), 4):
                        bt = tiles[b0:b0 + 4]
                        units.append((ex, bt, len(bt) * 128, bt[0] * 128, b0 == 0))

                def gu(u, jj):
                    ex, bt, n, c0, newex = units[u]
                    wb_ = ex % 2
                    ub = (ui + u) % 2
                    if newex and jj == 0:
                        S.dma(wg[:, wb_], G.wb_gate[li][ex].rearrange("(k p) f -> p k f", p=128), reads=[G.t_wb], writes=[t_w[wb_]])
                        S.dma(wu[:, wb_], G.wb_up[li][ex].rearrange("(k p) f -> p k f", p=128), reads=[G.t_wb], writes=[t_w[wb_]])
                        S.dma(wd[:, wb_], G.wb_down[li][ex].rearrange("(j p) c -> p j c", p=128), reads=[G.t_wb], writes=[t_w[wb_]])
                    for wi, wt in enumerate((wg, wu)):
                        for k in range(8):
                            S.op("pe", lambda e: e.matmul(pgu[:, jj * 2 + wi, 0:n], lhsT=wt[:, wb_, k, jj * 128:(jj + 1) * 128],
                                                          rhs=h2T[:, k, c0:c0 + n], start=(k == 0), stop=(k == 7)),
                                 reads=[t_w[wb_], G.t_h2T], writes=[t_pgu2[jj]])
                    S.op("act", lambda e: e.activation(out=sgl[:, jj, 0:n], in_=pgu[:, jj * 2, 0:n], func=AF.Silu), reads=[t_pgu2[jj]], writes=[t_sgl2[jj]])
                    S.op("dve", lambda e: e.tensor_tensor(out=aa2[:, ub, jj, 0:n], in0=sgl[:, jj, 0:n], in1=pgu[:, jj * 2 + 1, 0:n], op=ALU.mult),
                         reads=[t_sgl2[jj], t_pgu2[jj]], writes=[t_aa2[ub]])

                def down(u):
                    nonlocal yi
                    ex, bt, n, c0, newex = units[u]
                    wb_ = ex % 2
                    ub = (ui + u) % 2
                    for ti, tl in enumerate(bt):
                        yb = yi % 2
                        yi += 1
                        for half in range(2):
                            for jj in range(2):
                                S.op("pe", lambda e: e.matmul(py[:, yb, half, :], lhsT=aa2[:, ub, jj, ti * 128:(ti + 1) * 128],
                                                              rhs=wd[:, wb_, jj, half * 512:(half + 1) * 512], start=(jj == 0), stop=(jj == 1)),
                                     reads=[t_aa2[ub], t_w[wb_]], writes=[t_py[yb]])
                        al = acc[:, tl - s0, :]
                        pyv = py[:, yb].rearrange("p a c -> p (a c)")
                        if ex == 0:
                            S.op("dve", lambda e: e.tensor_scalar(out=al, in0=pyv, scalar1=comb[:, tl, ex:ex + 1], scalar2=None, op0=ALU.mult),
                                 reads=[t_py[yb], t_comb], writes=[t_acc])
                        else:
                            S.op("dve", lambda e: e.scalar_tensor_tensor(out=al, in0=pyv, scalar=comb[:, tl, ex:ex + 1], in1=al,
                                                                         op0=ALU.mult, op1=ALU.add), reads=[t_py[yb], t_comb, t_acc], writes=[t_acc])

                gu(0, 0)
                gu(0, 1)
                for u in range(len(units)):
                    if u + 1 < len(units):
                        gu(u + 1, 0)
                    down(u)
                    if u + 1 < len(units):
                        gu(u + 1, 1)
                ui += len(units)
                for tl in tiles:
                    b = tl % 2
                    j = 1 if tl < 2 else 0
                    S.dma(xt[:, b, :], G.xres[tl * 128:(tl + 1) * 128, :], reads=[G.t_xres[tl]], writes=[t_x[b]])
                    al = acc[:, tl - s0, :]
                    S.op("dve", lambda e: e.tensor_tensor(out=al, in0=al, in1=G.gtb[:, 1, j, :], op=ALU.mult), reads=[t_acc, G.t_gtb], writes=[t_acc])
                    S.op("dve", lambda e: e.tensor_tensor(out=xt[:, b, :], in0=al, in1=xt[:, b, :], op=ALU.add), reads=[t_acc, t_x[b]], writes=[t_x[b]])
                    if not last:
                        S.dma(G.xres[tl * 128:(tl + 1) * 128, :], xt[:, b, :], reads=[t_x[b]], writes=[G.t_xres[tl]])
                    else:
                        xv = xt[:, b, :]
                        S.op("act", lambda e: e.activation(out=sq[:], in_=xv, func=AF.Square, accum_out=ss[:, 0:1]), reads=[t_x[b]], writes=[t_sq, t_ss])
                        S.op("act", lambda e: e.activation(out=ss[:, 1:2], in_=ss[:, 0:1], func=AF.Sqrt, bias=G.epsc[:], scale=1.0 / D),
                             reads=[t_ss, G.t_c], writes=[t_ss])
                        S.op("dve", lambda e: e.reciprocal(out=ss[:, 2:3], in_=ss[:, 1:2]), reads=[t_ss], writes=[t_ss])
                        S.op("dve", lambda e: e.scalar_tensor_tensor(out=xv, in0=xv, scalar=ss[:, 2:3], in1=gfb[:], op0=ALU.mult, op1=ALU.mult),
                             reads=[t_x[b], t_ss, t_gf], writes=[t_x[b]])
                        S.dma(G.out[(tl - 2) * 128:(tl - 1) * 128, :], xv, reads=[t_x[b]], writes=[])


def ssd(G, li):
    nc, S, I = G.nc, G.S, G.I
    hT = G.hT
    need_ctx = li < DEPTH - 1
    wv32 = I["w_in"][li].rearrange("(k p) c -> p k c", p=128)
    wvb = G.wb_in[li].rearrange("(k p) c -> p k c", p=128)
    XB0 = 1024
    with ExitStack() as _es:
        xbcT = _es.enter_context(SB(nc, "xbcT", [128, 5, NTOK], BF16))
        xs_tok = _es.enter_context(SB(nc, "xs_tok", [128, NT, 384], BF16))
        B_tok = _es.enter_context(SB(nc, "B_tok", [128, NT, 128], BF16))
        lndt = _es.enter_context(SB(nc, "lndt", [128, NT, 12], F32))
        dta = _es.enter_context(SB(nc, "dta", [128, NT, 12], F32))
        ea = _es.enter_context(SB(nc, "ea", [128, NT, 12], F32))
        ww = _es.enter_context(SB(nc, "ww", [128, NT, 12], F32))
        eT = _es.enter_context(SB(nc, "eT", [128, NT, 12], F32))
        t_xbc, t_xs, t_dt = Tok(), Tok(), Tok()
        with ExitStack() as _es2:
            dts = _es2.enter_context(SB(nc, "dts", [128, NT, 12], F32))
            wst = _es2.enter_context(SB(nc, "wst", [128, 8, 128], F32))
            cwb = _es2.enter_context(SB(nc, "cwb", [128, 3, 128], F32))
            wj = _es2.enter_context(SB(nc, "wj", [128, 3, 8, 128], BF16))
            wdt = _es2.enter_context(SB(nc, "wdt", [128, 8, 12], BF16))
            dtb = _es2.enter_context(SB(nc, "dtb", [128, 2, 12], F32))
            tot = _es2.enter_context(SB(nc, "tot", [128, NT, 12], F32))
            wcol = _es2.enter_context(SB(nc, "wcol", [128, NT, 12], F32))
            pp2 = _es2.enter_context(PS(nc, "pp", [128, 2, 512], F32))
            pdt = _es2.enter_context(PS(nc, "pdt", [128, NT, 12], F32))
            ptb = _es2.enter_context(PS(nc, "ptb", [128, 4, 128], BF16))
            pc = _es2.enter_context(PS(nc, "pc", [128, NT, 12], F32))
            t_wst, t_cwb, t_wj, t_pp, t_wdt, t_pdt, t_ptb, t_a, t_pc = Tok(), Tok(), Tok(), Tok(), Tok(), Tok(), Tok(), Tok(), Tok()
            ocb = COLS["ssd_conv_b"][0]
            t_pp2 = [Tok(), Tok()]
            for ch in range(5):
                S.dma(wst[:], wv32[:, :, XB0 + ch * 128:XB0 + (ch + 1) * 128], writes=[t_wst])
                for j in range(3):
                    S.dma(cwb[:, j, :], I["ssd_conv_w"][li, j, ch * 128:(ch + 1) * 128].partition_broadcast(128), writes=[t_cwb])
                for j in range(3):
                    S.op("dve", lambda e: e.tensor_tensor(out=wj[:, j], in0=wst[:], in1=cwb[:, j:j + 1, :].to_broadcast([128, 8, 128]), op=ALU.mult),
                         reads=[t_wst, t_cwb], writes=[t_wj])
                for bi_, (c0, t0, n, tile0, ntile) in enumerate(BLOCKS):
                    pb_ = (ch * len(BLOCKS) + bi_) % 2
                    pp = pp2[:, pb_, :]
                    for j in range(3):
                        for k in range(8):
                            S.op("pe", lambda e: e.matmul(pp[:, 0:n], lhsT=wj[:, j, k, :], rhs=hT[:, k, c0 + j - 1:c0 + j - 1 + n],
                                                          start=(j == 0 and k == 0), stop=(j == 2 and k == 7)), reads=[t_wj, G.t_hT], writes=[t_pp2[pb_]])
                    S.op("act", lambda e: e.activation(out=xbcT[:, ch, t0:t0 + n], in_=pp[:, 0:n], func=AF.Silu, bias=G.cols[:, ocb + ch:ocb + ch + 1]),
                         reads=[t_pp2[pb_], G.t_cols], writes=[t_xbc])
            S.dma(wdt[:], wvb[:, :, 1664:1676], reads=[G.t_wb], writes=[t_wdt])
            S.dma(dtb[:, 0, :], I["ssd_dt_bias"][li].rearrange("a h -> (a h)").partition_broadcast(128), writes=[t_wdt])
            S.dma(dtb[:, 1, :], I["ssd_a_log"][li].rearrange("a h -> (a h)").partition_broadcast(128), writes=[t_wdt])
            for tl in range(NT):
                cc = colof(tl)
                for k in range(8):
                    S.op("pe", lambda e: e.matmul(pdt[:, tl, :], lhsT=hT[:, k, cc:cc + 128], rhs=wdt[:, k, :], start=(k == 0), stop=(k == 7)),
                         reads=[t_wdt, G.t_hT], writes=[t_pdt])
            S.op("dve", lambda e: e.tensor_tensor(out=dts[:], in0=pdt[:], in1=dtb[:, 0:1, :].to_broadcast([128, NT, 12]), op=ALU.add),
                 reads=[t_pdt, t_wdt], writes=[t_dt])
            S.op("act", lambda e: e.activation(out=dts[:], in_=dts[:], func=AF.Exp), reads=[t_dt], writes=[t_dt])
            S.op("act", lambda e: e.activation(out=dts[:], in_=dts[:], func=AF.Ln, bias=1.0), reads=[t_dt], writes=[t_dt])
            S.op("act", lambda e: e.activation(out=lndt[:], in_=dts[:], func=AF.Ln), reads=[t_dt], writes=[t_dt])
            S.op("act", lambda e: e.activation(out=dtb[:, 1, :], in_=dtb[:, 1, :], func=AF.Exp), reads=[t_wdt], writes=[t_wdt])
            S.op("dve", lambda e: e.scalar_tensor_tensor(out=dta[:], in0=dts[:], scalar=-1.0, in1=dtb[:, 1:2, :].to_broadcast([128, NT, 12]),
                                                         op0=ALU.mult, op1=ALU.mult), reads=[t_dt, t_wdt], writes=[t_a])
            for tl in range(NT):
                for c in range(4):
                    S.op("pe", lambda e: e.transpose(out=ptb[:, c, :], in_=xbcT[:, c, tl * 128:(tl + 1) * 128], identity=G.identB),
                         reads=[t_xbc, G.t_c], writes=[t_ptb])
                S.op("dve", lambda e: e.tensor_copy(out=xs_tok[:, tl, :], in_=ptb[:, 0:3, :].rearrange("p c t -> p (c t)")), reads=[t_ptb], writes=[t_xs])
                S.op("dve", lambda e: e.tensor_copy(out=B_tok[:, tl, :], in_=ptb[:, 3, :]), reads=[t_ptb], writes=[t_xs])
            for dr in range(2):
                S.op("pe", lambda e: e.matmul(pc[:].rearrange("p n h -> p (n h)"), lhsT=G.cm[:, 1 + dr, :], rhs=dta[:].rearrange("p n h -> p (n h)"),
                                              start=True, stop=True), reads=[t_a, G.t_c, t_dt], writes=[t_pc])
                S.op("dve", lambda e: e.tensor_copy(out=wcol[:, :, dr * 6:(dr + 1) * 6], in_=pc[:, :, dr * 6:(dr + 1) * 6]), reads=[t_pc], writes=[t_dt])
            S.op("pe", lambda e: e.matmul(pc[:].rearrange("p n h -> p (n h)"), lhsT=G.ones[:], rhs=dta[:].rearrange("p n h -> p (n h)"), start=True, stop=True),
                 reads=[t_a, G.t_c, t_dt], writes=[t_pc])
            S.op("dve", lambda e: e.tensor_copy(out=tot[:], in_=pc[:]), reads=[t_pc], writes=[t_dt])
            S.op("act", lambda e: e.activation(out=ea[:], in_=wcol[:], func=AF.Exp), reads=[t_dt], writes=[t_dt])
            S.op("act", lambda e: e.activation(out=eT[:], in_=tot[:], func=AF.Exp), reads=[t_dt], writes=[t_dt])
            S.op("dve", lambda e: e.tensor_tensor(out=ww[:], in0=tot[:], in1=wcol[:], op=ALU.subtract), reads=[t_dt], writes=[t_dt])
            S.op("dve", lambda e: e.tensor_tensor(out=ww[:], in0=ww[:], in1=lndt[:], op=ALU.add), reads=[t_dt], writes=[t_dt])
            S.op("act", lambda e: e.activation(out=ww[:], in_=ww[:], func=AF.Exp), reads=[t_dt], writes=[t_dt])
            S.barrier()
        with ExitStack() as _es2:
            wz = _es2.enter_context(SB(nc, "wz", [128, 8, 384], BF16))
            dbc = _es2.enter_context(SB(nc, "dbc", [128, 6, 64], F32))
            d6 = _es2.enter_context(SB(nc, "d6", [128, 6], F32))
            nwb = _es2.enter_context(SB(nc, "nwb", [128, 384], F32))
            hst = _es2.enter_context(SB(nc, "hst", [128, 192], F32))
            hsb = _es2.enter_context(SB(nc, "hsb", [128, 192], BF16))
            xw = _es2.enter_context(SB(nc, "xw", [128, 2, 192], BF16))
            ybt = _es2.enter_context(SB(nc, "ybt", [128, 2, 384], F32))
            Sm = _es2.enter_context(SB(nc, "Sm", [128, 2, 2, 128], F32))
            rA = _es2.enter_context(SB(nc, "rA", [128, 4, 128], F32))
            Dd = _es2.enter_context(SB(nc, "Dd", [128, 4, 128], F32))
            Mt = _es2.enter_context(SB(nc, "Mt", [128, 4, 128], BF16))
            acc = _es2.enter_context(SB(nc, "acc", [128, 384], F32))
            tmp = _es2.enter_context(SB(nc, "tmp", [128, 384], F32))
            zs = _es2.enter_context(SB(nc, "zs", [128, 384], F32))
            ssq = _es2.enter_context(SB(nc, "ssq", [128, 4], F32))
            ob = _es2.enter_context(SB(nc, "ob", [128, 384], BF16))
            oT = _es2.enter_context(SB(nc, "oT", [128, 2, 3, 128], BF16))
            pis = _es2.enter_context(PS(nc, "pis", [128, 2, 192], F32))
            ps_st = _es2.enter_context(PS(nc, "ps_st", [128, 2, 512], F32))
            pseg = _es2.enter_context(PS(nc, "pseg", [128, 3, 512], F32))
            py = _es2.enter_context(PS(nc, "py", [128, 384], F32))
            pz = py
            ptr = _es2.enter_context(PS(nc, "ptr", [128, 3, 128], BF16))
            t_wz, t_db, t_h, t_xw, t_yb, t_Sm, t_acc, t_tmp, t_zs, t_ssq, t_ob, t_oT = Tok(), Tok(), Tok(), Tok(), [Tok(), Tok()], Tok(), Tok(), Tok(), Tok(), Tok(), Tok(), [Tok(), Tok()]
            t_rA, t_Dd, t_Mt, t_pseg = [Tok() for _ in range(4)], [Tok() for _ in range(4)], [Tok() for _ in range(4)], [Tok() for _ in range(3)]
            t_pis, t_pst, t_py, t_ptr = Tok(), Tok(), Tok(), Tok()
            t_pz = t_py
            S.dma(wz[:], wvb[:, :, 640:1024], reads=[G.t_wb], writes=[t_wz])
            S.dma(d6[:], I["ssd_d"][li].partition_broadcast(128), writes=[t_db])
            S.dma(nwb[:], I["ssd_norm"][li].partition_broadcast(128), writes=[t_db])
            S.op("dve", lambda e: e.tensor_copy(out=dbc[:], in_=d6[:].unsqueeze(2).to_broadcast([128, 6, 64])), reads=[t_db], writes=[t_db])
            yb_d = G.ssd_yb

            def carry_step(c, dr, want_out, dst):
                for g in range(2):
                    hd0 = dr * 6 + 3 * g
                    if want_out:
                        S.op("pe", lambda e: e.matmul(pis[:, 0, :], lhsT=xbcT[g * 64:(g + 1) * 64, 4, c * 128:(c + 1) * 128], rhs=hsb[g * 64:(g + 1) * 64, :],
                                                      start=True, stop=True), reads=[t_xbc, t_h], writes=[t_pis])
                        S.op("dve", lambda e: e.tensor_tensor(out=dst[:, g * 192:(g + 1) * 192].rearrange("p (a d) -> p a d", a=3),
                                                              in0=pis[:, 0, :].rearrange("p (a d) -> p a d", a=3),
                                                              in1=ea[:, c, hd0:hd0 + 3].unsqueeze(2).to_broadcast([128, 3, 64]), op=ALU.mult),
                             reads=[t_pis, t_dt], writes=[dst_tok[0]])
                    S.op("dve", lambda e: e.tensor_tensor(out=xw[:, g, :].rearrange("p (a d) -> p a d", a=3),
                                                          in0=xs_tok[:, c, g * 192:(g + 1) * 192].rearrange("p (a d) -> p a d", a=3),
                                                          in1=ww[:, c, hd0:hd0 + 3].unsqueeze(2).to_broadcast([128, 3, 64]), op=ALU.mult),
                         reads=[t_xs, t_dt], writes=[t_xw])
                    gs = slice(g * 64, (g + 1) * 64)
                    S.op("pe", lambda e: e.matmul(pis[:, 1, :], lhsT=B_tok[:, c, :], rhs=xw[:, g, :], start=True, stop=True),
                         reads=[t_xs, t_xw], writes=[t_pis])
                    S.op("dve", lambda e: e.tensor_tensor(out=hst[gs, :].rearrange("p (a d) -> p a d", a=3),
                                                          in0=hst[gs, :].rearrange("p (a d) -> p a d", a=3),
                                                          in1=eT[gs, c, hd0:hd0 + 3].unsqueeze(2).to_broadcast([64, 3, 64]), op=ALU.mult),
                         reads=[t_h, t_dt], writes=[t_h])
                    S.op("dve", lambda e: e.tensor_tensor(out=hst[gs, :], in0=hst[gs, :], in1=pis[gs, 1, :], op=ALU.add),
                         reads=[t_h, t_pis], writes=[t_h])
                    S.op("act", lambda e: e.copy(out=hsb[gs, :], in_=hst[gs, :]), reads=[t_h], writes=[t_h])

            S.op("dve", lambda e: e.memset(hst[:], 0.0), writes=[t_h])
            S.op("dve", lambda e: e.memset(hsb[:], 0.0), writes=[t_h])
            order_b = [1, 0] + list(range(NT - 1, 1, -1))
            for ci, c in enumerate(order_b):
                want = need_ctx or c >= 2
                b = ci % 2
                dst_tok = [t_yb[b]]
                carry_step(c, 1, want, ybt[:, b, :])
                if want:
                    S.dma(yb_d[c * 128:(c + 1) * 128, :], ybt[:, b, :], reads=[t_yb[b]], writes=[G.t_ssdyb[c]])
            S.op("dve", lambda e: e.memset(hst[:], 0.0), reads=[t_h], writes=[t_h])
            S.op("dve", lambda e: e.memset(hsb[:], 0.0), reads=[t_h], writes=[t_h])
            on = COLS["ssd_norm"][0]
            it = 0
            for c in range(NT):
                want = need_ctx or c >= 2
                dst_tok = [t_acc]
                carry_step(c, 0, want, acc[:])
                if not want:
                    continue
                b = c % 2
                tc0 = c * 128
                S.dma(ybt[:, b, :], yb_d[c * 128:(c + 1) * 128, :], reads=[G.t_ssdyb[c]], writes=[t_yb[b]])
                cc = colof(c)
                for k in range(8):
                    S.op("pe", lambda e: e.matmul(pz[:], lhsT=hT[:, k, cc:cc + 128], rhs=wz[:, k, :], start=(k == 0), stop=(k == 7)),
                         reads=[t_wz, G.t_hT], writes=[t_pz])
                S.op("act", lambda e: e.activation(out=zs[:], in_=pz[:], func=AF.Silu), reads=[t_pz], writes=[t_zs])
                for g in range(2):
                    S.op("pe", lambda e: e.matmul(ps_st[:, g, 0:128], lhsT=xbcT[g * 64:(g + 1) * 64, 3, tc0:tc0 + 128], rhs=xbcT[g * 64:(g + 1) * 64, 4, tc0:tc0 + 128],
                                                  start=True, stop=True), reads=[t_xbc], writes=[t_pst])
                for g in range(2):
                    for dr in range(2):
                        S.op("dve", lambda e: e.tensor_tensor(out=Sm[:, g, dr, :], in0=ps_st[:, g, 0:128], in1=G.cm[:, 1 + dr, :], op=ALU.mult),
                             reads=[t_pst, G.t_c], writes=[t_Sm])
                steps = [(h, dr) for h in range(6) for dr in range(2)]

                def st_a(i):
                    h, dr = steps[i]
                    hd = dr * 6 + h
                    r4, p3 = (it + i) % 4, (it + i) % 3
                    S.op("dve", lambda e: e.tensor_scalar(out=rA[:, r4, :], in0=G.cm[:, 1 + dr, :], scalar1=dta[:, c, hd:hd + 1], scalar2=None, op0=ALU.mult),
                         reads=[G.t_c, t_dt], writes=[t_rA[r4]])
                    S.op("pe", lambda e: e.matmul(pseg[:, p3, 0:128], lhsT=G.cm[:, 8 - dr, :], rhs=rA[:, r4, :], start=True, stop=True),
                         reads=[t_rA[r4], G.t_c], writes=[t_pseg[p3]])
                    S.op("act", lambda e: e.activation(out=Dd[:, r4, :], in_=pseg[:, p3, 0:128], func=AF.Exp, bias=lndt[:, c, hd:hd + 1]),
                         reads=[t_pseg[p3], t_dt], writes=[t_Dd[r4]])

                st_a(0)
                st_a(1)
                for i, (h, dr) in enumerate(steps):
                    r4 = (it + i) % 4
                    if i + 2 < len(steps):
                        st_a(i + 2)
                    S.op("dve", lambda e: e.tensor_tensor(out=Mt[:, r4, :], in0=Dd[:, r4, :], in1=Sm[:, h // 3, dr, :], op=ALU.mult),
                         reads=[t_Dd[r4], t_Sm], writes=[t_Mt[r4]])
                    S.op("pe", lambda e: e.matmul(py[:, h * 64:(h + 1) * 64], lhsT=Mt[:, r4, :], rhs=xs_tok[:, c, h * 64:(h + 1) * 64],
                                                  start=(dr == 0), stop=(dr == 1)), reads=[t_Mt[r4], t_xs], writes=[t_py])
                it += len(steps)
                S.op("dve", lambda e: e.tensor_tensor(out=acc[:], in0=acc[:], in1=py[:], op=ALU.add), reads=[t_acc, t_py], writes=[t_acc])
                S.op("dve", lambda e: e.tensor_tensor(out=acc[:], in0=acc[:], in1=ybt[:, b, :], op=ALU.add), reads=[t_acc, t_yb[b]], writes=[t_acc])
                S.op("dve", lambda e: e.tensor_tensor(out=tmp[:], in0=xs_tok[:, c, :], in1=dbc[:].rearrange("p a d -> p (a d)"), op=ALU.mult),
                     reads=[t_xs, t_db], writes=[t_tmp])
                S.op("dve", lambda e: e.tensor_tensor(out=acc[:], in0=acc[:], in1=tmp[:], op=ALU.add), reads=[t_acc, t_tmp], writes=[t_acc])
                S.op("dve", lambda e: e.tensor_tensor(out=acc[:], in0=acc[:], in1=zs[:], op=ALU.mult), reads=[t_acc, t_zs], writes=[t_acc])
                for g in range(2):
                    S.op("act", lambda e: e.activation(out=tmp[:, g * 192:(g + 1) * 192], in_=acc[:, g * 192:(g + 1) * 192], func=AF.Square, accum_out=ssq[:, g:g + 1]),
                         reads=[t_acc], writes=[t_tmp, t_ssq])
                S.op("act", lambda e: e.activation(out=ssq[:, 2:4], in_=ssq[:, 0:2], func=AF.Sqrt, bias=G.epsc[:], scale=1.0 / 192), reads=[t_ssq, G.t_c], writes=[t_ssq])
                S.op("dve", lambda e: e.reciprocal(out=ssq[:, 2:4], in_=ssq[:, 2:4]), reads=[t_ssq], writes=[t_ssq])
                for g in range(2):
                    S.op("dve", lambda e: e.scalar_tensor_tensor(out=ob[:, g * 192:(g + 1) * 192], in0=acc[:, g * 192:(g + 1) * 192], scalar=ssq[:, 2 + g:3 + g],
                                                                 in1=nwb[:, g * 192:(g + 1) * 192], op0=ALU.mult, op1=ALU.mult), reads=[t_acc, t_ssq, t_db], writes=[t_ob])
                for c3 in range(3):
                    S.op("pe", lambda e: e.transpose(out=ptr[:, c3, :], in_=ob[:, c3 * 128:(c3 + 1) * 128], identity=G.identB), reads=[t_ob, G.t_c], writes=[t_ptr])
                S.op("act", lambda e: e.copy(out=oT[:, b], in_=ptr[:]), reads=[t_ptr], writes=[t_oT[b]])
                S.dma(G.mixT[384:768, tc0:tc0 + 128].rearrange("(c p) t -> p c t", p=128), oT[:, b], reads=[t_oT[b]], writes=[G.t_mix])
        if "ssd" in G.dbg and li == 0:
            dump_bf16(G, G.mixT[384:512, 256:768], G.dbg["ssd"], [G.t_mix])


HY0 = 1676
SEGS = {"lat": dict(L=4096, NC=32, KC=17, off=NCTX), "ctx": dict(L=256, NC=2, KC=2, off=0)}


def hyena_inproj(G, li):
    nc, S, I = G.nc, G.S, G.I
    hT = G.hT
    need_ctx = li < DEPTH - 1
    wv32 = I["w_in"][li].rearrange("(k p) c -> p k c", p=128)
    with ExitStack() as _es:
        wst = _es.enter_context(SB(nc, "wst", [128, 8, 128], F32))
        cwb = _es.enter_context(SB(nc, "cwb", [128, 3, 128], F32))
        wj = _es.enter_context(SB(nc, "wjh", [128, 3, 8, 768], BF16))
        hb = _es.enter_context(SB(nc, "hb", [128, 768], F32))
        hv = _es.enter_context(SB(nc, "hv", [128, 2, 768], BF16))
        ph4 = _es.enter_context(PS(nc, "ph", [128, 2, 2, 512], F32))
        t_wst, t_cwb, t_wj, t_hb, t_hv, t_ph2 = Tok(), Tok(), Tok(), Tok(), [Tok(), Tok()], [Tok(), Tok()]
        S.dma(hb[:], I["hy_conv_b"][li].partition_broadcast(128), writes=[t_hb])
        for cc in range(6):
            S.dma(wst[:], wv32[:, :, HY0 + cc * 128:HY0 + (cc + 1) * 128], writes=[t_wst])
            for j in range(3):
                S.dma(cwb[:, j, :], I["hy_conv_w"][li, j, cc * 128:(cc + 1) * 128].partition_broadcast(128), writes=[t_cwb])
            for j in range(3):
                S.op("dve", lambda e: e.tensor_tensor(out=wj[:, j, :, cc * 128:(cc + 1) * 128], in0=wst[:],
                                                      in1=cwb[:, j:j + 1, :].to_broadcast([128, 8, 128]), op=ALU.mult),
                     reads=[t_wst, t_cwb], writes=[t_wj])
        dv = G.hyv.rearrange("m t c -> t m c")
        for tl in range(NT):
            if tl < 2 and not need_ctx:
                continue
            col = colof(tl)
            b = tl % 2
            ph = ph4[:, b]
            t_ph = t_ph2[b]
            for half in range(2):
                for j in range(3):
                    for k in range(8):
                        S.op("pe", lambda e: e.matmul(ph[:, half, 0:384], lhsT=hT[:, k, col + j - 1:col + j - 1 + 128],
                                                      rhs=wj[:, j, k, half * 384:(half + 1) * 384], start=(j == 0 and k == 0), stop=(j == 2 and k == 7)),
                             reads=[t_wj, G.t_hT], writes=[t_ph])
            S.op("dve", lambda e: e.tensor_tensor(out=hv[:, b, :].rearrange("p (a c) -> p a c", a=2), in0=ph[:, :, 0:384],
                                                  in1=hb[:].rearrange("p (a c) -> p a c", a=2), op=ALU.add), reads=[t_ph, t_hb], writes=[t_hv[b]])
            S.dma(dv[tl * 128:(tl + 1) * 128], hv[:, b, :].rearrange("p (m c) -> p m c", m=3), reads=[t_hv[b]], writes=[G.t_hyv])


def hyena_fft(G, li):
    nc, S, I = G.nc, G.S, G.I
    need_ctx = li < DEPTH - 1
    for sname in (("lat", "ctx") if need_ctx else ("lat",)):
        P = SEGS[sname]
        L, NCk, KC, off = P["L"], P["NC"], P["KC"], P["off"]
        NH = NCk // 2
        FT, IT, FE, WIN, MH = I["ft_" + sname], I["it_" + sname], I["fe_" + sname], I["win_" + sname], I["mh_" + sname]
        Kf = G.Kf[sname]
        t_kf = Tok()
        with ExitStack() as _es:
            hk = _es.enter_context(SB(nc, "hk", [128, NCk, 1024], BF16))
            fe = _es.enter_context(SB(nc, "fe", [33, L], F32))
            h1 = _es.enter_context(SB(nc, "h1", [64, L], F32))
            h2 = _es.enter_context(SB(nc, "h2", [64, L], F32))
            w1 = _es.enter_context(SB(nc, "w1", [33, 64], F32))
            w2 = _es.enter_context(SB(nc, "w2", [64, 64], F32))
            w3 = _es.enter_context(SB(nc, "w3", [64, 1024], F32))
            pre = _es.enter_context(SB(nc, "pre", [64, 512], F32))
            pr2 = _es.enter_context(SB(nc, "pr2", [64, 512], F32))
            t_pr2 = Tok()
            MAGIC = 1.5 * 2 ** 23
            wint = _es.enter_context(SB(nc, "wint", [128, 2, 2, 256], F32))
            hbias = _es.enter_context(SB(nc, "hbias", [128, 512], F32))
            mh = _es.enter_context(SB(nc, "mh", [128, KC], F32))
            slab = _es.enter_context(SB(nc, "slab", [128, 2, NCk, 2, 128], BF16))
            xo = _es.enter_context(SB(nc, "xo", [128, 2, 512], F32))
            sd = _es.enter_context(SB(nc, "sd", [128, 2, 2, 2, 512], F32))
            kf = _es.enter_context(SB(nc, "kf", [128, 2, 2, 2, 256], BF16))
            pm = _es.enter_context(PS(nc, "pm", [64, 512], F32))
            phh = _es.enter_context(PS(nc, "phh", [128, 2, 512], F32))
            psk = _es.enter_context(PS(nc, "psk", [128, 2, 2, 512], F32))
            t_hk, t_fe, t_h1, t_h2, t_w, t_pre, t_win, t_slab, t_eo, t_sd, t_kft, t_pm, t_phh, t_psk = \
                Tok(), Tok(), Tok(), Tok(), Tok(), Tok(), [Tok(), Tok()], [Tok(), Tok()], Tok(), Tok(), Tok(), Tok(), Tok(), Tok()
            S.dma(fe[:], FE[:, :], writes=[t_fe])
            S.dma(w1[:], I["hy_w1"][li], writes=[t_w])
            S.dma(w2[:], I["hy_w2"][li], writes=[t_w])
            S.dma(w3[:], I["hy_w3"][li], writes=[t_w])
            S.dma(hbias[:], I["hy_bias"][li].rearrange("o c -> (o c)").partition_broadcast(128), writes=[t_w])
            S.dma(mh[:], MH[:, :], writes=[t_w])
            ob1, ofr, ob2 = COLS["hy_b1"][0], COLS["hy_freq"][0], COLS["hy_b2"][0]
            for (src, t_src, wt, kk, bcol, dst, t_dst) in ((fe, t_fe, w1, 33, ob1, h1, t_h1), (h1, t_h1, w2, 64, ob2, h2, t_h2)):
                for c0 in range(0, L, 512):
                    n = min(512, L - c0)
                    S.op("pe", lambda e: e.matmul(pm[:, 0:n], lhsT=wt[0:kk, :], rhs=src[0:kk, c0:c0 + n], start=True, stop=True),
                         reads=[t_w, t_src], writes=[t_pm])
                    S.op("dve", lambda e: e.tensor_scalar(out=pre[:, 0:n], in0=pm[:, 0:n], scalar1=G.cols[0:64, bcol:bcol + 1],
                                                          scalar2=G.cols[0:64, ofr:ofr + 1], op0=ALU.add, op1=ALU.mult),
                         reads=[t_pm, G.t_cols], writes=[t_pre])
                    S.op("dve", lambda e: e.tensor_scalar(out=pr2[:, 0:n], in0=pre[:, 0:n], scalar1=1.0 / (2.0 * math.pi), scalar2=MAGIC, op0=ALU.mult, op1=ALU.add),
                         reads=[t_pre], writes=[t_pr2])
                    S.op("dve", lambda e: e.tensor_scalar(out=pr2[:, 0:n], in0=pr2[:, 0:n], scalar1=-MAGIC, scalar2=None, op0=ALU.add),
                         reads=[t_pr2], writes=[t_pr2])
                    S.op("dve", lambda e: e.scalar_tensor_tensor(out=pre[:, 0:n], in0=pr2[:, 0:n], scalar=-2.0 * math.pi, in1=pre[:, 0:n], op0=ALU.mult, op1=ALU.add),
                         reads=[t_pr2, t_pre], writes=[t_pre])
                    S.op("act", lambda e: e.activation(out=dst[:, c0:c0 + n], in_=pre[:, 0:n], func=AF.Sin),
                         reads=[t_pre], writes=[t_dst])
            for c in range(NCk):
                b = c % 2
                S.dma(wint[:, b], WIN[c], writes=[t_win[b]])
                for half in range(2):
                    S.op("pe", lambda e: e.matmul(phh[:, half, :], lhsT=h2[:, c * 128:(c + 1) * 128], rhs=w3[:, half * 512:(half + 1) * 512], start=True, stop=True),
                         reads=[t_h2, t_w], writes=[t_phh])
                for dr in range(2):
                    S.op("dve", lambda e: e.tensor_tensor(out=hk[:, c, dr * 512:(dr + 1) * 512].rearrange("p (o c) -> p o c", o=2),
                                                          in0=phh[:, dr, :].rearrange("p (o c) -> p o c", o=2),
                                                          in1=wint[:, b, dr:dr + 1, :].to_broadcast([128, 2, 256]), op=ALU.mult),
                         reads=[t_phh, t_win[b]], writes=[t_hk])
            for kc in range(KC):
                b = kc % 2
                S.dma(slab[:, b], FT[kc], writes=[t_slab[b]])
                for dr in range(2):
                    for eo_ in range(2):
                        for ri in range(2):
                            for c in range(NH):
                                cc = eo_ * NH + c
                                S.op("pe", lambda e: e.matmul(psk[:, eo_, ri, :], lhsT=slab[:, b, cc, ri, :], rhs=hk[:, cc, dr * 512:(dr + 1) * 512],
                                                              start=(c == 0), stop=(c == NH - 1)), reads=[t_slab[b], t_hk], writes=[t_psk])
                    S.op("act", lambda e: e.copy(out=xo[:], in_=psk[:, 1]), reads=[t_psk], writes=[t_eo])
                    S.op("dve", lambda e: e.tensor_tensor(out=sd[:, dr, 0], in0=psk[:, 0], in1=xo[:], op=ALU.add), reads=[t_psk, t_eo], writes=[t_sd])
                    S.op("dve", lambda e: e.tensor_tensor(out=sd[:, dr, 1], in0=psk[:, 0], in1=xo[:], op=ALU.subtract), reads=[t_psk, t_eo], writes=[t_sd])
                v = lambda ap: ap.rearrange("p (o c) -> p o c", o=2)
                S.op("dve", lambda e: e.tensor_tensor(out=sd[:, 0, 0, 0, :], in0=sd[:, 0, 0, 0, :], in1=hbias[:], op=ALU.add), reads=[t_sd, t_w], writes=[t_sd])
                S.op("dve", lambda e: e.tensor_tensor(out=sd[:, 0, 1, 0, :], in0=sd[:, 0, 1, 0, :], in1=hbias[:], op=ALU.add), reads=[t_sd, t_w], writes=[t_sd])
                S.op("dve", lambda e: e.tensor_tensor(out=kf[:, :, 0, 0, :], in0=v(sd[:, 0, 0, 0, :]), in1=v(sd[:, 1, 0, 0, :]), op=ALU.add), reads=[t_sd], writes=[t_kft])
                S.op("dve", lambda e: e.tensor_tensor(out=kf[:, :, 0, 1, :], in0=v(sd[:, 0, 0, 1, :]), in1=v(sd[:, 1, 0, 1, :]), op=ALU.subtract), reads=[t_sd], writes=[t_kft])
                S.op("dve", lambda e: e.tensor_tensor(out=v(xo[:, 0, :]), in0=v(sd[:, 0, 1, 0, :]), in1=v(sd[:, 1, 1, 0, :]), op=ALU.add), reads=[t_sd], writes=[t_eo])
                S.op("dve", lambda e: e.tensor_tensor(out=v(xo[:, 1, :]), in0=v(sd[:, 1, 1, 1, :]), in1=v(sd[:, 0, 1, 1, :]), op=ALU.subtract), reads=[t_sd], writes=[t_eo])
                S.op("dve", lambda e: e.tensor_scalar(out=kf[:, :, 1, 0, :], in0=v(xo[:, 0, :]), scalar1=mh[:, kc:kc + 1], scalar2=None, op0=ALU.mult),
                     reads=[t_eo, t_w], writes=[t_kft])
                S.op("dve", lambda e: e.tensor_scalar(out=kf[:, :, 1, 1, :], in0=v(xo[:, 1, :]), scalar1=mh[:, kc:kc + 1], scalar2=None, op0=ALU.mult),
                     reads=[t_eo, t_w], writes=[t_kft])
                S.dma(Kf[:, kc].rearrange("o p l r c -> p o l r c"), kf[:], reads=[t_kft], writes=[t_kf])
            S.barrier()
        with ExitStack() as _es:
            vt = _es.enter_context(SB(nc, "vt", [128, NCk, 256], BF16))
            zz1 = _es.enter_context(SB(nc, "zz1", [128, NCk, 256], BF16))
            Y = _es.enter_context(SB(nc, "Y", [128, 2, KC, 2, 256], BF16))
            fsl = _es.enter_context(SB(nc, "fsl", [128, 2, NCk, 2, 128], BF16))
            isl = _es.enter_context(SB(nc, "isl", [128, 2, KC, 2, 128], BF16))
            kft = _es.enter_context(SB(nc, "kft", [128, 2, 2, 2, 256], BF16))
            xo = _es.enter_context(SB(nc, "xo", [128, 2, 256], F32))
            xs_ = _es.enter_context(SB(nc, "xs_", [128, 2, 2, 256], F32))
            ta = _es.enter_context(SB(nc, "ta", [128, 4, 256], F32))
            yl = _es.enter_context(SB(nc, "yl", [128, 2, 2, 256], F32))
            xg = _es.enter_context(SB(nc, "xg", [128, 2, 256], BF16))
            zt = _es.enter_context(SB(nc, "zt", [128, 256], BF16))
            zT = _es.enter_context(SB(nc, "zT", [128, 2, 2, 256], BF16))
            psx = _es.enter_context(PS(nc, "psx", [128, 2, 4, 256], F32))
            psy = _es.enter_context(PS(nc, "psy", [128, 2, 512], F32))
            ptr = _es.enter_context(PS(nc, "ptr", [128, 2, 128], BF16))
            t_vt, t_zz1, t_Y, t_fsl, t_isl, t_kft2, t_ta, t_xg, t_zt, t_zT, t_psx, t_psy, t_ptr, t_xo, t_xs, t_yl = \
                Tok(), Tok(), Tok(), [Tok(), Tok()], [Tok(), Tok()], [Tok(), Tok()], Tok(), [Tok(), Tok()], Tok(), [Tok(), Tok()], [Tok(), Tok()], [Tok(), Tok()], Tok(), Tok(), Tok(), Tok()
            hsrc = lambda m: G.hyv[m, off:off + L, :].rearrange("(c p two) ch -> two p c ch", p=128, two=2)
            for par in range(2):
                S.dma(vt[:, par * NH:(par + 1) * NH, :], hsrc(0)[par], reads=[G.t_hyv], writes=[t_vt])
            mo = G.mixT[768:1024, :].rearrange("(c p) t -> p c t", p=128)
            for order in range(2):
                src, t_src = (vt, t_vt) if order == 0 else (zz1, t_zz1)
                for kc in range(KC):
                    b = kc % 2
                    S.dma(fsl[:, b], FT[kc], writes=[t_fsl[b]])
                    S.dma(kft[:, b], Kf[order, kc], reads=[t_kf], writes=[t_kft2[b]])
                    for eo_ in range(2):
                        for ri in range(2):
                            for c in range(NH):
                                cc = eo_ * NH + c
                                S.op("pe", lambda e: e.matmul(psx[:, b, eo_ * 2 + ri, :], lhsT=fsl[:, b, cc, ri, :], rhs=src[:, cc, :], start=(c == 0), stop=(c == NH - 1)),
                                     reads=[t_fsl[b], t_src], writes=[t_psx[b]])
                    S.op("act", lambda e: e.copy(out=xo[:], in_=psx[:, b, 2:4, :]), reads=[t_psx[b]], writes=[t_xo])
                    S.op("dve", lambda e: e.tensor_tensor(out=xs_[:, 0], in0=psx[:, b, 0:2, :], in1=xo[:], op=ALU.add), reads=[t_psx[b], t_xo], writes=[t_xs])
                    S.op("dve", lambda e: e.tensor_tensor(out=xs_[:, 1, 0, :], in0=psx[:, b, 0, :], in1=xo[:, 0, :], op=ALU.subtract), reads=[t_psx[b], t_xo], writes=[t_xs])
                    S.op("dve", lambda e: e.scalar_tensor_tensor(out=xs_[:, 1, 1, :], in0=psx[:, b, 1, :], scalar=-1.0, in1=xo[:, 1, :], op0=ALU.mult, op1=ALU.add),
                         reads=[t_psx[b], t_xo], writes=[t_xs])
                    S.op("dve", lambda e: e.tensor_tensor(out=ta[:, 0:2, :], in0=xs_[:, :, 0, :], in1=kft[:, b, :, 0, :], op=ALU.mult), reads=[t_xs, t_kft2[b]], writes=[t_ta])
                    S.op("pool", lambda e: e.tensor_tensor(out=ta[:, 2:4, :], in0=xs_[:, :, 1, :], in1=kft[:, b, :, 1, :], op=ALU.mult), reads=[t_xs, t_kft2[b]], writes=[t_ta])
                    S.op("dve", lambda e: e.tensor_tensor(out=yl[:, :, 0, :], in0=ta[:, 0:2, :], in1=ta[:, 2:4, :], op=ALU.subtract), reads=[t_ta], writes=[t_yl])
                    S.op("dve", lambda e: e.tensor_tensor(out=ta[:, 0:2, :], in0=xs_[:, :, 0, :], in1=kft[:, b, :, 1, :], op=ALU.mult), reads=[t_xs, t_kft2[b], t_yl], writes=[t_ta])
                    S.op("pool", lambda e: e.tensor_tensor(out=ta[:, 2:4, :], in0=xs_[:, :, 1, :], in1=kft[:, b, :, 0, :], op=ALU.mult), reads=[t_xs, t_kft2[b], t_yl], writes=[t_ta])
                    S.op("dve", lambda e: e.tensor_tensor(out=yl[:, :, 1, :], in0=ta[:, 0:2, :], in1=ta[:, 2:4, :], op=ALU.add), reads=[t_ta], writes=[t_yl])
                    S.op("dve", lambda e: e.tensor_tensor(out=Y[:, 0, kc, 0, :], in0=yl[:, 0, 0, :], in1=yl[:, 1, 0, :], op=ALU.add), reads=[t_yl], writes=[t_Y])
                    S.op("pool", lambda e: e.tensor_tensor(out=Y[:, 0, kc, 1, :], in0=yl[:, 0, 1, :], in1=yl[:, 1, 1, :], op=ALU.subtract), reads=[t_yl], writes=[t_Y])
                    S.op("dve", lambda e: e.tensor_tensor(out=Y[:, 1, kc, 0, :], in0=yl[:, 0, 0, :], in1=yl[:, 1, 0, :], op=ALU.subtract), reads=[t_yl], writes=[t_Y])
                    S.op("pool", lambda e: e.tensor_tensor(out=Y[:, 1, kc, 1, :], in0=yl[:, 0, 1, :], in1=yl[:, 1, 1, :], op=ALU.add), reads=[t_yl], writes=[t_Y])
                oi = 0
                for c2 in range(NH):
                    for par in range(2):
                        cc = par * NH + c2
                        b = oi % 2
                        oi += 1
                        zb = c2 % 2
                        S.dma(isl[:, b], IT[cc], writes=[t_isl[b]])
                        S.dma(xg[:, b], hsrc(1 + order)[par, :, c2, :], reads=[G.t_hyv], writes=[t_xg[b]])
                        for kc in range(KC):
                            for ri in range(2):
                                S.op("pe", lambda e: e.matmul(psy[:, b, 0:256], lhsT=isl[:, b, kc, ri, :], rhs=Y[:, par, kc, ri, :],
                                                              start=(kc == 0 and ri == 0), stop=(kc == KC - 1 and ri == 1)), reads=[t_isl[b], t_Y], writes=[t_psy[b]])
                        if order == 0:
                            S.op("dve", lambda e: e.tensor_tensor(out=zz1[:, cc, :], in0=psy[:, b, 0:256], in1=xg[:, b, :], op=ALU.mult),
                                 reads=[t_psy[b], t_xg[b]], writes=[t_zz1])
                        else:
                            S.op("dve", lambda e: e.tensor_tensor(out=zt[:], in0=psy[:, b, 0:256], in1=xg[:, b, :], op=ALU.mult),
                                 reads=[t_psy[b], t_xg[b]], writes=[t_zt])
                            for hh in range(2):
                                S.op("pe", lambda e: e.transpose(out=ptr[:, hh, :], in_=zt[:, hh * 128:(hh + 1) * 128], identity=G.identB),
                                     reads=[t_zt, G.t_c], writes=[t_ptr])
                            S.op("act", lambda e: e.copy(out=zT[:, zb].rearrange("p h (t two) -> p h t two", two=2)[:, :, :, par], in_=ptr[:]),
                                 reads=[t_ptr], writes=[t_zT[zb]])
                            if par == 1:
                                S.dma(mo[:, :, off + c2 * 256:off + (c2 + 1) * 256], zT[:, zb], reads=[t_zT[zb]], writes=[G.t_mix])
            S.barrier()
    if "hy" in G.dbg and li == 0:
        dump_bf16(G, G.mixT[768:896, 256:768], G.dbg["hy"], [G.t_mix])


def _hy_tables(L):
    N = 2 * L
    NCk = L // 128
    KC = (L // 2 + 1 + 127) // 128
    perm = np.concatenate([np.arange(0, L, 2), np.arange(1, L, 2)])
    n = perm.astype(np.int64)
    k = np.arange(KC * 128, dtype=np.int64)
    ang = ((n[:, None] * k[None, :]) % N).astype(np.float64) * (2 * np.pi / N)
    valid = (k <= L // 2).astype(np.float64)
    w = np.where(k == 0, 1.0, 2.0) / N * valid
    c, s_ = np.cos(ang), np.sin(ang)
    ft = np.stack([c * valid, -s_ * valid], axis=0)
    ft = ft.reshape(2, NCk, 128, KC, 128).transpose(3, 2, 1, 0, 4)
    it = np.stack([c * w, -s_ * w], axis=0)
    it = it.reshape(2, NCk, 128, KC, 128).transpose(1, 4, 3, 0, 2)
    f = np.float32
    nn = np.arange(L, dtype=f)
    t = nn / f(max(L - 1, 1))
    bands = np.linspace(1e-4, 15, 16, dtype=f)
    wpos = (f(2 * math.pi / L) * nn).astype(f)
    feats = np.concatenate([t[:, None], np.cos(wpos[:, None] * bands), -np.sin(wpos[:, None] * bands)], axis=-1).astype(f)
    deltas = np.abs(np.linspace(math.log(1e-2) / 1.5, math.log(1e-2) / 0.3, 256, dtype=f))
    win = np.exp(-t[:, None] * deltas).astype(f)
    winb = win.copy()
    winb[0] = 0.0
    feats = feats[perm]
    wn = np.stack([win, winb], axis=1)[perm].reshape(NCk, 128, 2, 256)
    mh = ((k != L // 2) & (k <= L // 2)).astype(f).reshape(KC, 128).T
    bf = ml_dtypes.bfloat16
    return (np.ascontiguousarray(ft).astype(bf), np.ascontiguousarray(it).astype(bf),
            np.ascontiguousarray(feats.T), np.ascontiguousarray(wn), np.ascontiguousarray(mh))
```

```python
import math
from contextlib import ExitStack
import numpy as np
import ml_dtypes
import concourse.bass as bass
import concourse.mybir as mybir
from concourse.bass_utils import run_bass_kernel_spmd

F32 = mybir.dt.float32
BF16 = mybir.dt.bfloat16
AF = mybir.ActivationFunctionType
ALU = mybir.AluOpType
AX = mybir.AxisListType

D = 1024
NCTX = 256
NLAT = 4096
NTOK = NCTX + NLAT
NT = NTOK // 128
DEPTH = 2
D_IN = 2444
EPS = 1e-6
HC = NTOK + 3
NE = 16
DFF = 256
DEBUG = False


def colof(tile):
    return 1 + 128 * tile if tile < 2 else 258 + 128 * (tile - 2)


BLOCKS = [(1, 0, 256, 0, 2)] + [(258 + 512 * j, 256 + 512 * j, 512, 2 + 4 * j, 4) for j in range(8)]


class Tok:
    __slots__ = ("w", "r")

    def __init__(self):
        self.w = None
        self.r = {}


class Sched:
    def __init__(self, nc, ndma=8, same_engine_sync=True):
        self.nc = nc
        self.eng = {"pe": nc.tensor, "act": nc.scalar, "dve": nc.vector, "pool": nc.gpsimd, "sp": nc.sync}
        self.semh = {}
        self.cnt = {}
        self.seen = {k: {} for k in self.eng}
        self.same = same_engine_sync
        for k in self.eng:
            self.semh[k] = nc.alloc_semaphore("s_" + k)
            self.cnt[k] = 0
        self.ndma = ndma
        self.dslot = {}
        self.dval = {}
        for q in ("sp", "pool"):
            self.dslot[q] = 0
            for i in range(ndma):
                key = ("dma", q, i)
                self.semh[key] = nc.alloc_semaphore("d_%s_%d" % (q, i))
                self.dval[key] = 0
        self.ninst = 0

    def _wait(self, e, deps):
        for (k, v) in sorted(deps, key=str):
            if k == e and (e == "pe" or not self.same):
                continue
            if self.seen[e].get(k, 0) >= v:
                continue
            self.eng[e].wait_ge(self.semh[k], v)
            self.seen[e][k] = v

    @staticmethod
    def _deps(reads, writes):
        deps = set()
        for t in reads:
            if t.w is not None:
                deps.add(t.w)
        for t in writes:
            if t.w is not None:
                deps.add(t.w)
            for kv in t.r.items():
                deps.add(kv)
        return deps

    @staticmethod
    def _mark(ev, reads, writes):
        k, v = ev
        for t in reads:
            if t.r.get(k, 0) < v:
                t.r[k] = v
        for t in writes:
            t.w = ev
            t.r = {}

    def op(self, e, fn, reads=(), writes=()):
        self._wait(e, self._deps(reads, writes))
        ins = fn(self.eng[e])
        self.cnt[e] += 1
        ins.then_inc(self.semh[e], 1)
        self._mark((e, self.cnt[e]), reads, writes)
        self.ninst += 1
        return ins

    def dma(self, out, in_, reads=(), writes=(), q="sp", **kw):
        i = self.dslot[q]
        self.dslot[q] = (i + 1) % self.ndma
        key = ("dma", q, i)
        deps = self._deps(reads, writes)
        if self.dval[key] > 0:
            deps.add((key, self.dval[key]))
        self._wait(q, deps)
        ins = self.eng[q].dma_start(out=out, in_=in_, **kw)
        self.dval[key] += 16
        ins.then_inc(self.semh[key], 16)
        self._mark((key, self.dval[key]), reads, writes)
        self.ninst += 1
        return ins

    def barrier(self):
        deps = set()
        for key, v in self.dval.items():
            if v > 0:
                deps.add((key, v))
        for k in self.eng:
            if self.cnt[k] > 0:
                deps.add((k, self.cnt[k]))
        for e in self.eng:
            self._wait(e, deps)


class Ctx:
    pass


_UID = [0]


def SB(nc, name, shape, dt):
    _UID[0] += 1
    return nc.sbuf_tensor("%s_%d" % (name, _UID[0]), shape, dt)


def PS(nc, name, shape, dt):
    _UID[0] += 1
    return nc.psum_tensor("%s_%d" % (name, _UID[0]), shape, dt)


def build(dbg=None):
    nc = bass.Bass("TRN2", target_bir_lowering=False)
    S = Sched(nc)
    G = Ctx()
    G.nc, G.S = nc, S

    def din(name, shape, dt=F32):
        return nc.dram_tensor(name, list(shape), dt, kind="ExternalInput").ap()

    def dscr(name, shape, dt):
        return nc.dram_tensor(name, list(shape), dt, kind="Internal").ap()

    I = {}
    I["x"] = din("x", [NLAT, D])
    I["ctx"] = din("ctx", [NCTX, D])
    I["w_mod"] = din("w_mod", [DEPTH, D, 6 * D])
    I["b_mod"] = din("b_mod", [DEPTH, 6 * D])
    I["w_in"] = din("w_in", [DEPTH, D, D_IN])
    I["w_out"] = din("w_out", [DEPTH, D, D])
    I["w_router"] = din("w_router", [D, NE])
    I["router_bias"] = din("router_bias", [NE])
    I["w_gate"] = din("w_gate", [DEPTH, NE, D, DFF])
    I["w_up"] = din("w_up", [DEPTH, NE, D, DFF])
    I["w_down"] = din("w_down", [DEPTH, NE, DFF, D])
    I["g_final"] = din("g_final", [D])
    I["cols"] = din("cols", [DEPTH, 128, NCOLS])
    I["cmat"] = din("cmat", [9, 128, 128])
    I["rope"] = din("rope", [2, 128, NLAT])
    for sname, P in SEGS.items():
        I["ft_" + sname] = din("ft_" + sname, [P["KC"], 128, P["NC"], 2, 128], BF16)
        I["it_" + sname] = din("it_" + sname, [P["NC"], 128, P["KC"], 2, 128], BF16)
        I["fe_" + sname] = din("fe_" + sname, [33, P["L"]])
        I["win_" + sname] = din("win_" + sname, [P["NC"], 128, 2, 256])
        I["mh_" + sname] = din("mh_" + sname, [128, P["KC"]])
    for nm, shp in (("hy_conv_w", [DEPTH, 3, 768]), ("hy_conv_b", [DEPTH, 768]), ("hy_w1", [DEPTH, 33, 64]), ("hy_w2", [DEPTH, 64, 64]),
                    ("hy_w3", [DEPTH, 64, 1024]), ("hy_bias", [DEPTH, 2, 256])):
        I[nm] = din(nm, shp)
    I["ssdmask"] = din("ssdmask", [2, 4, 128, 512], BF16)
    for nm, shp in (("ssd_conv_w", [DEPTH, 3, 640]), ("ssd_dt_bias", [DEPTH, 2, 6]), ("ssd_a_log", [DEPTH, 2, 6]),
                    ("ssd_d", [DEPTH, 6]), ("ssd_norm", [DEPTH, 384])):
        I[nm] = din(nm, shp)
    out = nc.dram_tensor("out", [NLAT, D], F32, kind="ExternalOutput").ap()
    G.I, G.out = I, out
    G.dbg = {}
    if dbg:
        for name, shape in dbg.items():
            G.dbg[name] = nc.dram_tensor("dbg_" + name, list(shape), F32, kind="ExternalOutput").ap()

    G.xres = dscr("xres", [NTOK, D], F32)
    G.t_xres = [Tok() for _ in range(NT)]
    G.wb_in = [dscr("wb_in%d" % i, [D, D_IN], BF16) for i in range(DEPTH)]
    G.wb_out = [dscr("wb_out%d" % i, [D, D], BF16) for i in range(DEPTH)]
    G.wb_gate = [dscr("wb_gate%d" % i, [NE, D, DFF], BF16) for i in range(DEPTH)]
    G.wb_up = [dscr("wb_up%d" % i, [NE, D, DFF], BF16) for i in range(DEPTH)]
    G.wb_down = [dscr("wb_down%d" % i, [NE, DFF, D], BF16) for i in range(DEPTH)]
    G.t_wb = Tok()
    G.mixT = dscr("mixT", [D, NTOK], BF16)
    G.hyv = dscr("hyv", [3, NTOK, 256], BF16)
    G.ssd_yb = dscr("ssd_yb", [NTOK, 384], F32)
    G.t_ssdyb = [Tok() for _ in range(NT)]
    G.t_hyv = Tok()
    G.Kf = {sn: dscr("Kf_" + sn, [2, P["KC"], 128, 2, 2, 256], BF16) for sn, P in SEGS.items()}
    G.t_mix = Tok()

    cm = nc.alloc_sbuf_tensor("cm", [128, 9, 128], F32)
    cmb = nc.alloc_sbuf_tensor("cmb", [128, 9, 128], BF16)
    ones = nc.alloc_sbuf_tensor("ones", [128, 128], F32)
    epsc = nc.alloc_sbuf_tensor("epsc", [128, 1], F32)
    G.t_c = Tok()
    S.dma(cm[:], I["cmat"].rearrange("a p c -> p a c"), writes=[G.t_c])
    S.op("dve", lambda e: e.tensor_copy(out=cmb[:], in_=cm[:]), reads=[G.t_c], writes=[G.t_c])
    S.op("dve", lambda e: e.memset(ones[:], 1.0), writes=[G.t_c])
    S.op("dve", lambda e: e.memset(epsc[:], EPS), writes=[G.t_c])
    G.cm, G.cmb, G.ones, G.epsc = cm, cmb, ones, epsc
    G.negpi = nc.alloc_sbuf_tensor("negpi", [128, 1], F32)
    S.op("dve", lambda e: e.memset(G.negpi[:], -math.pi), writes=[G.t_c])
    G.identF, G.identB = cm[:, 0, :], cmb[:, 0, :]

    S.dma(G.xres[0:NCTX, :], I["ctx"][:, :], writes=G.t_xres[0:2])
    for j in range(4):
        S.dma(G.xres[NCTX + 1024 * j:NCTX + 1024 * (j + 1), :], I["x"][1024 * j:1024 * (j + 1), :],
              writes=G.t_xres[2 + 8 * j:2 + 8 * (j + 1)])

    convert_weights(G)
    S.barrier()
    for li in range(1 if DEBUG else DEPTH):
        layer(G, li)
    S.barrier()
    return nc


def convert_weights(G):
    nc, S, I = G.nc, G.S, G.I
    CH = 4096
    with ExitStack() as _es:
        cf = _es.enter_context(SB(nc, "cv_f", [128, 2, CH], F32))
        cb = _es.enter_context(SB(nc, "cv_b", [128, 2, CH], BF16))
        tf = [Tok(), Tok()]
        tb = [Tok(), Tok()]
        n = 0
        engs = ["dve", "pool", "act"]
        for li in range(DEPTH):
            pairs = [(I["w_in"][li], G.wb_in[li], "a b -> (a b)"), (I["w_out"][li], G.wb_out[li], "a b -> (a b)"),
                     (I["w_gate"][li], G.wb_gate[li], "e a b -> (e a b)"), (I["w_up"][li], G.wb_up[li], "e a b -> (e a b)"),
                     (I["w_down"][li], G.wb_down[li], "e a b -> (e a b)")]
            for src, dst, pat in pairs:
                s1 = src.rearrange(pat).rearrange("(p m) -> p m", p=128)
                d1 = dst.rearrange(pat).rearrange("(p m) -> p m", p=128)
                M = s1.shape[1]
                for c0 in range(0, M, CH):
                    w = min(CH, M - c0)
                    k = n % 2
                    S.dma(cf[:, k, 0:w], s1[:, c0:c0 + w], writes=[tf[k]])
                    en = engs[n % 3]
                    if en == "act":
                        S.op("act", lambda e: e.copy(out=cb[:, k, 0:w], in_=cf[:, k, 0:w]), reads=[tf[k]], writes=[tb[k]])
                    else:
                        S.op(en, lambda e: e.tensor_copy(out=cb[:, k, 0:w], in_=cf[:, k, 0:w]), reads=[tf[k]], writes=[tb[k]])
                    S.dma(d1[:, c0:c0 + w], cb[:, k, 0:w], reads=[tb[k]], writes=[G.t_wb], q="pool")
                    n += 1


COLS = {}
_o = 0
for _name, _n in [("cc", 16), ("bmod", 32), ("g_mix", 8), ("g_ffn", 8), ("ssd_conv_b", 5), ("qg", 1), ("kg", 1),
                  ("ssd_d", 3), ("ssd_norm", 3), ("hy_b1", 1), ("hy_freq", 1), ("hy_b2", 1)]:
    COLS[_name] = (_o, _n)
    _o += _n
NCOLS = _o


def layer(G, li):
    nc, S, I = G.nc, G.S, G.I
    with ExitStack() as _es:
        cols = _es.enter_context(SB(nc, "cols", [128, NCOLS], F32))
        modc = _es.enter_context(SB(nc, "modc", [128, 4, 8, 2], F32))
        gtb = _es.enter_context(SB(nc, "gtb", [128, 2, 2, D], F32))
        G.cols, G.modc, G.gtb = cols, modc, gtb
        G.t_cols, G.t_modc, G.t_gtb = Tok(), Tok(), Tok()
        S.dma(cols[:], I["cols"][li], writes=[G.t_cols])
        adaln(G, li)
        S.barrier()
        with ExitStack() as _es:
            hT = _es.enter_context(SB(nc, "hT", [128, 8, HC], BF16))
            G.hT, G.t_hT = hT, Tok()
            norm_in(G, li)
            S.barrier()
            if "hy" in STAGES:
                hyena_inproj(G, li)
                S.barrier()
            if "att" in STAGES:
                attention(G, li)
                S.barrier()
            if "ssd" in STAGES:
                ssd(G, li)
                S.barrier()
        if "hy" in STAGES:
            hyena_fft(G, li)
            S.barrier()
        if "moe" in STAGES:
            with ExitStack() as _es:
                h2T = _es.enter_context(SB(nc, "h2T", [128, 8, NTOK], BF16))
                rl = _es.enter_context(SB(nc, "rl", [128, NT, NE], F32))
                G.h2T, G.t_h2T, G.rl, G.t_rl = h2T, Tok(), rl, Tok()
                outproj(G, li)
                S.barrier()
                moe(G, li)
                S.barrier()


STAGES = ("att", "ssd", "hy", "moe")


def colap(G, name, j=0, n=1, p0=0, p1=128):
    o, _ = COLS[name]
    return G.cols[p0:p1, o + j:o + j + n]


def adaln(G, li):
    nc, S, I = G.nc, G.S, G.I
    cols, modc, gtb = G.cols, G.modc, G.gtb
    with ExitStack() as _es:
        sc = _es.enter_context(SB(nc, "sc", [128, 8, 2], F32))
        screp = _es.enter_context(SB(nc, "screp", [128, 8, 2, 128], F32))
        wm = _es.enter_context(SB(nc, "wm", [128, 2, 8, 512], F32))
        brow = _es.enter_context(SB(nc, "brow", [128, 2, D], F32))
        ps_a = _es.enter_context(PS(nc, "ps_a", [128, 4, 2], F32))
        ps_g = _es.enter_context(PS(nc, "ps_g", [128, 2, 512], F32))
        t_sc, t_wm, t_pa, t_pg, t_brow = Tok(), [Tok(), Tok()], Tok(), Tok(), Tok()
        o = COLS["cc"][0]
        S.op("act", lambda e: e.activation(out=sc[:].rearrange("p k j -> p (k j)"), in_=cols[:, o:o + 16], func=AF.Silu),
             reads=[G.t_cols], writes=[t_sc])
        S.op("dve", lambda e: e.tensor_copy(out=screp[:].rearrange("p k j c -> p (k j) c"),
                                            in_=sc[:].rearrange("p k j -> p (k j)").unsqueeze(2).to_broadcast([128, 16, 128])),
             reads=[t_sc], writes=[t_sc])
        for g in range(2):
            S.dma(brow[:, g, :], I["b_mod"][li, (2 + 3 * g) * D:(3 + 3 * g) * D].partition_broadcast(128), writes=[t_brow])
        wv = I["w_mod"][li].rearrange("(k p) c -> p k c", p=128)
        ob = COLS["bmod"][0]
        for cj in range(12):
            b = cj % 2
            S.dma(wm[:, b], wv[:, :, cj * 512:(cj + 1) * 512], writes=[t_wm[b]])
            vec = cj // 2
            half = cj % 2
            if vec in (2, 5):
                g = 0 if vec == 2 else 1
                for j in range(2):
                    for kd in range(8):
                        S.op("pe", lambda e: e.matmul(ps_g[:, j, :], lhsT=screp[:, kd, j, :], rhs=wm[:, b, kd, :],
                                                      start=(kd == 0), stop=(kd == 7)), reads=[t_sc, t_wm[b]], writes=[t_pg])
                    S.op("dve", lambda e: e.tensor_tensor(out=gtb[:, g, j, half * 512:(half + 1) * 512], in0=ps_g[:, j, :],
                                                          in1=brow[:, g, half * 512:(half + 1) * 512], op=ALU.add),
                         reads=[t_pg, t_brow], writes=[G.t_gtb])
            else:
                v = {0: 0, 1: 1, 3: 2, 4: 3}[vec]
                for fc in range(4):
                    for kd in range(8):
                        S.op("pe", lambda e: e.matmul(ps_a[:, fc, :], lhsT=wm[:, b, kd, fc * 128:(fc + 1) * 128], rhs=sc[:, kd, :],
                                                      start=(kd == 0), stop=(kd == 7)), reads=[t_sc, t_wm[b]], writes=[t_pa])
                k0 = half * 4
                S.op("dve", lambda e: e.tensor_tensor(out=modc[:, v, k0:k0 + 4, :], in0=ps_a[:],
                                                      in1=cols[:, ob + v * 8 + k0:ob + v * 8 + k0 + 4].unsqueeze(2).to_broadcast([128, 4, 2]),
                                                      op=ALU.add), reads=[t_pa, G.t_cols], writes=[G.t_modc])
        for v, gname in ((1, "g_mix"), (3, "g_ffn")):
            og = COLS[gname][0]
            S.op("dve", lambda e: e.scalar_tensor_tensor(out=modc[:, v], in0=modc[:, v], scalar=1.0,
                                                         in1=cols[:, og:og + 8].unsqueeze(2).to_broadcast([128, 8, 2]),
                                                         op0=ALU.add, op1=ALU.mult), reads=[G.t_modc, G.t_cols], writes=[G.t_modc])


def rms_to_T(G, xt, t_x, tile, vA, vB, dstT, t_dst, dcol, pool):
    nc, S = G.nc, G.S
    sq, ss, xn, ps_t, toks = pool
    t_sq, t_ss, t_xn, t_ps = toks
    j = 1 if tile < 2 else 0
    S.op("act", lambda e: e.activation(out=sq[:], in_=xt, func=AF.Square, accum_out=ss[:, 0:1]), reads=[t_x], writes=[t_sq, t_ss])
    S.op("act", lambda e: e.activation(out=ss[:, 1:2], in_=ss[:, 0:1], func=AF.Sqrt, bias=G.epsc[:], scale=1.0 / D),
         reads=[t_ss, G.t_c], writes=[t_ss])
    S.op("dve", lambda e: e.reciprocal(out=ss[:, 2:3], in_=ss[:, 1:2]), reads=[t_ss], writes=[t_ss])
    S.op("dve", lambda e: e.tensor_scalar(out=xn[:], in0=xt, scalar1=ss[:, 2:3], scalar2=None, op0=ALU.mult),
         reads=[t_x, t_ss], writes=[t_xn])
    for k in range(8):
        S.op("pe", lambda e: e.transpose(out=ps_t[:, k, :], in_=xn[:, k * 128:(k + 1) * 128], identity=G.identB),
             reads=[t_xn, G.t_c], writes=[t_ps])
    S.op("dve", lambda e: e.tensor_tensor(out=sq[:].rearrange("p (k c) -> p k c", k=8), in0=ps_t[:],
                                          in1=G.modc[:, vA, :, j:j + 1].to_broadcast([128, 8, 128]), op=ALU.mult),
         reads=[t_ps, G.t_modc], writes=[t_sq])
    S.op("dve", lambda e: e.tensor_tensor(out=dstT[:, :, dcol:dcol + 128], in0=sq[:].rearrange("p (k c) -> p k c", k=8),
                                          in1=G.modc[:, vB, :, j:j + 1].to_broadcast([128, 8, 128]), op=ALU.add),
         reads=[t_sq, G.t_modc], writes=[t_dst])


def norm_in(G, li):
    nc, S = G.nc, G.S
    hT = G.hT
    with ExitStack() as _es:
        xt = _es.enter_context(SB(nc, "xt", [128, 2, D], F32))
        sq = _es.enter_context(SB(nc, "sq", [128, 2, D], F32))
        ss = _es.enter_context(SB(nc, "ss", [128, 2, 4], F32))
        xn = _es.enter_context(SB(nc, "xn", [128, 2, D], BF16))
        ps_t = _es.enter_context(PS(nc, "ps_t", [128, 2, 8, 128], BF16))
        t_x = [Tok(), Tok()]
        pools = [(sq[:, i, :], ss[:, i, :], xn[:, i, :], ps_t[:, i], (Tok(), Tok(), Tok(), Tok())) for i in range(2)]
        for c in (0, 257, HC - 1):
            S.op("pool", lambda e: e.memset(hT[:, :, c:c + 1], 0.0), writes=[G.t_hT])
        for tile in range(NT):
            b = tile % 2
            S.dma(xt[:, b, :], G.xres[tile * 128:(tile + 1) * 128, :], reads=[G.t_xres[tile]], writes=[t_x[b]])
            rms_to_T(G, xt[:, b, :], t_x[b], tile, 1, 0, hT, G.t_hT, colof(tile), pools[b])
        if "hT" in G.dbg and li == 0:
            with ExitStack() as _es:
                dh = _es.enter_context(SB(nc, "dbgh", [128, 8, 512], F32))
                t = Tok()
                S.op("dve", lambda e: e.tensor_copy(out=dh[:], in_=hT[:, :, 0:512]), reads=[G.t_hT], writes=[t])
                S.dma(G.dbg["hT"].rearrange("(k p) c -> p k c", p=128), dh[:], reads=[t])


def attention(G, li):
    nc, S, I = G.nc, G.S, G.I
    hT = G.hT
    need_ctx = li < DEPTH - 1
    wv = G.wb_in[li].rearrange("(k p) c -> p k c", p=128)
    scale = 64 ** -0.5
    with ExitStack() as _es:
        wq = _es.enter_context(SB(nc, "wqkv", [128, 8, 640], BF16))
        qT = _es.enter_context(SB(nc, "qT", [128, 6, NTOK], BF16))
        kT = _es.enter_context(SB(nc, "kT", [128, NTOK], BF16))
        vp = _es.enter_context(SB(nc, "vp", [128, NT, 2, 128], BF16))
        rp = _es.enter_context(SB(nc, "rp", [128, 2, 2, 512], F32))
        qs = _es.enter_context(SB(nc, "qs", [128, 512], F32))
        q2 = _es.enter_context(SB(nc, "q2", [128, 512], F32))
        qn = _es.enter_context(SB(nc, "qn", [128, 512], F32))
        qnb = _es.enter_context(SB(nc, "qnb", [128, 512], BF16))
        pT = _es.enter_context(SB(nc, "pT", [128, 2, 2, 512], BF16))
        rd = _es.enter_context(SB(nc, "rd", [128, 2, 512], F32))
        ao = _es.enter_context(SB(nc, "ao", [128, 2, 512], BF16))
        ps_q = _es.enter_context(PS(nc, "ps_q", [128, 512], F32))
        ps_r = _es.enter_context(PS(nc, "ps_r", [128, 512], F32))
        ps_s = _es.enter_context(PS(nc, "ps_s", [128, 2, 2, 512], F32))
        ps_o = _es.enter_context(PS(nc, "ps_o", [128, 2, 512], F32))
        t_w, t_q, t_k, t_v, t_rp = Tok(), Tok(), Tok(), Tok(), [Tok(), Tok()]
        t_qs, t_q2, t_qn, t_qnb, t_psq, t_psr = Tok(), Tok(), Tok(), Tok(), Tok(), Tok()
        t_pT, t_pss, t_pso, t_rd, t_ao = [Tok(), Tok()], [Tok(), Tok()], [Tok(), Tok()], [Tok(), Tok()], [Tok(), Tok()]
        for j in range(3):
            S.dma(wq[:, :, j * 128:j * 128 + 64], wv[:, :, j * 64:(j + 1) * 64], reads=[G.t_wb], writes=[t_w])
            S.dma(wq[:, :, j * 128 + 64:(j + 1) * 128], wv[:, :, (3 + j) * 64:(4 + j) * 64], reads=[G.t_wb], writes=[t_w])
        S.dma(wq[:, :, 384:640], wv[:, :, 384:640], reads=[G.t_wb], writes=[t_w])
        S.op("pool", lambda e: e.memset(vp[:, :, :, 64:128], 1.0), writes=[t_v])
        S.op("pool", lambda e: e.memset(qT[64:128, 0:3, :], 0.0), writes=[t_q])
        S.op("pool", lambda e: e.memset(qT[0:64, 3:6, :], 0.0), writes=[t_q])
        og = {0: COLS["qg"][0], 1: COLS["qg"][0], 2: COLS["qg"][0], 3: COLS["kg"][0]}
        for bi, (c0, t0, n, tile0, ntile) in enumerate(BLOCKS):
            if bi > 0:
                b = bi % 2
                S.dma(rp[:, b, :, :], I["rope"][:, :, t0 - NCTX:t0 - NCTX + 512].rearrange("a p c -> p a c"), writes=[t_rp[b]])
            for ch in range(4):
                for k in range(8):
                    S.op("pe", lambda e: e.matmul(ps_q[:, 0:n], lhsT=wq[:, k, ch * 128:(ch + 1) * 128], rhs=hT[:, k, c0:c0 + n],
                                                  start=(k == 0), stop=(k == 7)), reads=[t_w, G.t_hT], writes=[t_psq])
                S.op("act", lambda e: e.copy(out=qs[:, 0:n], in_=ps_q[:, 0:n]), reads=[t_psq], writes=[t_qs])
                S.op("act", lambda e: e.activation(out=q2[:, 0:n], in_=qs[:, 0:n], func=AF.Square), reads=[t_qs], writes=[t_q2])
                S.op("pe", lambda e: e.matmul(ps_r[:, 0:n], lhsT=G.cm[:, 3, :], rhs=q2[:, 0:n], start=True, stop=True),
                     reads=[t_q2, G.t_c], writes=[t_psr])
                S.op("act", lambda e: e.activation(out=q2[:, 0:n], in_=ps_r[:, 0:n], func=AF.Sqrt, bias=G.epsc[:], scale=1.0 / 64),
                     reads=[t_psr, G.t_c], writes=[t_q2])
                S.op("dve", lambda e: e.reciprocal(out=q2[:, 0:n], in_=q2[:, 0:n]), reads=[t_q2], writes=[t_q2])
                S.op("dve", lambda e: e.scalar_tensor_tensor(out=qn[:, 0:n], in0=qs[:, 0:n], scalar=G.cols[:, og[ch]:og[ch] + 1],
                                                             in1=q2[:, 0:n], op0=ALU.mult, op1=ALU.mult),
                     reads=[t_qs, t_q2, G.t_cols], writes=[t_qn])
                t_dst = t_q if ch < 3 else t_k
                halves = [(0, 64, qT[0:64, ch, t0:t0 + n]), (64, 128, qT[64:128, 3 + ch, t0:t0 + n])] if ch < 3 else [(0, 128, kT[:, t0:t0 + n])]
                if bi == 0:
                    for (p0, p1, dst) in halves:
                        S.op("dve", lambda e: e.tensor_copy(out=dst, in_=qn[p0:p1, 0:n]), reads=[t_qn], writes=[t_dst])
                else:
                    b = bi % 2
                    S.op("dve", lambda e: e.tensor_copy(out=qnb[:, 0:n], in_=qn[:, 0:n]), reads=[t_qn], writes=[t_qnb])
                    S.op("pe", lambda e: e.matmul(ps_r[:, 0:n], lhsT=G.cmb[:, 4, :], rhs=qnb[:, 0:n], start=True, stop=True),
                         reads=[t_qnb, G.t_c], writes=[t_psr])
                    S.op("dve", lambda e: e.tensor_tensor(out=qs[:, 0:n], in0=ps_r[:, 0:n], in1=rp[:, b, 1, 0:n], op=ALU.mult),
                         reads=[t_psr, t_rp[b]], writes=[t_qs])
                    S.op("dve", lambda e: e.tensor_tensor(out=qn[:, 0:n], in0=qn[:, 0:n], in1=rp[:, b, 0, 0:n], op=ALU.mult),
                         reads=[t_qn, t_rp[b]], writes=[t_qn])
                    for (p0, p1, dst) in halves:
                        S.op("dve", lambda e: e.tensor_tensor(out=dst, in0=qn[p0:p1, 0:n], in1=qs[p0:p1, 0:n], op=ALU.add),
                             reads=[t_qn, t_qs], writes=[t_dst])
            for tl in range(tile0, tile0 + ntile):
                cc = colof(tl)
                for k in range(8):
                    S.op("pe", lambda e: e.matmul(ps_q[:, 0:128], lhsT=hT[:, k, cc:cc + 128], rhs=wq[:, k, 512:640],
                                                  start=(k == 0), stop=(k == 7)), reads=[t_w, G.t_hT], writes=[t_psq])
                S.op("act", lambda e: e.copy(out=vp[:, tl, :, 0:64], in_=ps_q[:, 0:128].rearrange("p (a d) -> p a d", a=2)),
                     reads=[t_psq], writes=[t_v])
        pairs = []
        oi = 0
        for h in range(6):
            for bi, (c0, t0, n, tile0, ntile) in enumerate(BLOCKS):
                if bi == 0 and not need_ctx:
                    continue
                kcs = list(range(2)) if bi == 0 else list(range(NT))
                for ki in range(0, len(kcs), 2):
                    pairs.append((h, t0, n, kcs[ki], ki == 0, ki + 2 >= len(kcs), oi % 2))
                oi += 1

        def qk(j):
            h, t0, n, kc, first, last, ob = pairs[j]
            pb = (h // 3) * 64
            sb = j % 2
            for u in range(2):
                S.op("pe", lambda e: e.matmul(ps_s[:, sb, u, 0:n], lhsT=kT[:, (kc + u) * 128:(kc + u + 1) * 128],
                                              rhs=qT[:, h, t0:t0 + n], start=True, stop=True),
                     reads=[t_q, t_k], writes=[t_pss[sb]])

        qk(0)
        for j, (h, t0, n, kc, first, last, ob) in enumerate(pairs):
            sb = j % 2
            kv = h // 3
            if j + 1 < len(pairs):
                qk(j + 1)
            S.op("act", lambda e: e.activation(out=pT[:, sb, :, 0:n], in_=ps_s[:, sb, :, 0:n], func=AF.Exp, scale=scale),
                 reads=[t_pss[sb]], writes=[t_pT[sb]])
            for u in range(2):
                S.op("pe", lambda e: e.matmul(ps_o[:, ob, 0:n], lhsT=vp[:, kc + u, kv, :], rhs=pT[:, sb, u, 0:n],
                                              start=(first and u == 0), stop=(last and u == 1)),
                     reads=[t_v, t_pT[sb]], writes=[t_pso[ob]])
            if last:
                S.op("dve", lambda e: e.reciprocal(out=rd[0:64, ob, 0:n], in_=ps_o[64:128, ob, 0:n]), reads=[t_pso[ob]], writes=[t_rd[ob]])
                S.op("dve", lambda e: e.tensor_tensor(out=ao[0:64, ob, 0:n], in0=ps_o[0:64, ob, 0:n], in1=rd[0:64, ob, 0:n], op=ALU.mult),
                     reads=[t_pso[ob], t_rd[ob]], writes=[t_ao[ob]])
                S.dma(G.mixT[h * 64:(h + 1) * 64, t0:t0 + n], ao[0:64, ob, 0:n], reads=[t_ao[ob]], writes=[G.t_mix])
        if "att" in G.dbg and li == 0:
            dump_bf16(G, G.mixT[0:128, 256:768], G.dbg["att"], [G.t_mix])


def dump_bf16(G, src, dst, reads):
    nc, S = G.nc, G.S
    p, n = src.shape
    with ExitStack() as _es:
        a = _es.enter_context(SB(nc, "dmpb", [p, n], BF16))
        b = _es.enter_context(SB(nc, "dmpf", [p, n], F32))
        t = Tok()
        S.dma(a[:], src, reads=reads, writes=[t])
        S.op("dve", lambda e: e.tensor_copy(out=b[:], in_=a[:]), reads=[t], writes=[t])
        S.dma(dst, b[:], reads=[t])
        S.barrier()


def _cols_pack(inp, li, b):
    def colform(v, n):
        return np.ascontiguousarray(v.reshape(n, 128).T)
    parts = {}
    cc = np.zeros((128, 8, 2), np.float32)
    cc[:, :, 0] = colform(inp["c"][b], 8)
    cc[:, :, 1] = colform(inp["c_ctx"], 8)
    parts["cc"] = cc.reshape(128, 16)
    bm = inp["b_mod"][li].reshape(6, 8, 128)
    parts["bmod"] = np.concatenate([bm[v].T for v in (0, 1, 3, 4)], axis=1)
    parts["g_mix"] = colform(inp["g_mix"][li], 8)
    parts["g_ffn"] = colform(inp["g_ffn"][li], 8)
    parts["ssd_conv_b"] = colform(inp["ssd_conv_b"][li], 5)
    parts["qg"] = np.tile(inp["q_norm"][li], 2)[:, None]
    parts["kg"] = np.tile(inp["k_norm"][li], 2)[:, None]
    parts["ssd_d"] = colform(np.repeat(inp["ssd_d"][li], 64), 3)
    parts["ssd_norm"] = colform(inp["ssd_norm"][li], 3)
    for nm in ("hy_b1", "hy_freq", "hy_b2"):
        parts[nm] = np.tile(inp[nm][li], 2)[:, None]
    out = np.zeros((128, NCOLS), np.float32)
    for nm, (o, n) in COLS.items():
        out[:, o:o + n] = parts[nm]
    return out


def _consts():
    ident = np.eye(128, dtype=np.float32)
    s = np.arange(128)
    U = (s[:, None] <= s[None, :]).astype(np.float32)
    Lo = (s[:, None] >= s[None, :]).astype(np.float32)
    bo = np.kron(np.eye(2, dtype=np.float32), np.ones((64, 64), np.float32))
    rot = np.zeros((128, 128), np.float32)
    for hb in (0, 64):
        for d in range(32):
            rot[hb + d + 32, hb + d] = -1.0
            rot[hb + d, hb + d + 32] = 1.0
    top = np.zeros((128, 128), np.float32); top[:64] = 1.0
    bot = np.zeros((128, 128), np.float32); bot[64:] = 1.0
    cmat = np.stack([ident, U, Lo, bo, rot, top, bot, U - ident, Lo - ident])
    rows = NLAT // 64
    row = np.repeat(np.arange(rows), 64).astype(np.float32)
    col = np.tile(np.arange(64), rows).astype(np.float32)
    inv = (10000.0 ** (-np.arange(0, 32, 2, dtype=np.float32) / 32)).astype(np.float32)
    ang = np.concatenate([row[:, None] * inv, col[:, None] * inv], axis=-1).astype(np.float32)
    cs = np.cos(ang).astype(np.float32).T
    sn = np.sin(ang).astype(np.float32).T
    rope = np.stack([np.tile(cs, (4, 1)), np.tile(sn, (4, 1))]).astype(np.float32)
    tt = np.arange(512)[None, None, :]
    ss_ = np.arange(128)[None, :, None]
    jj = np.arange(4)[:, None, None]
    mf = (tt >= 128 * jj + ss_).astype(np.float32)
    mb = (tt <= 128 * jj + ss_).astype(np.float32)
    hyt = {}
    for sname, P in SEGS.items():
        ft, it_, fe, wn, mh = _hy_tables(P["L"])
        hyt["ft_" + sname], hyt["it_" + sname], hyt["fe_" + sname], hyt["win_" + sname], hyt["mh_" + sname] = ft, it_, fe, wn, mh
    return {**hyt, "cmat": cmat, "rope": rope, "ssdmask": np.stack([mf, mb]).astype(ml_dtypes.bfloat16)}


_CONSTS = None


def kernel(**inp):
    global _CONSTS
    inp = {k: np.asarray(v) for k, v in inp.items()}
    if _CONSTS is None:
        _CONSTS = _consts()
    dbg = kernel.dbg if hasattr(kernel, "dbg") else None
    nc = build(dbg)
    ncores = 8
    in_maps = []
    for core in range(ncores):
        b = core % 4
        m = {"x": np.ascontiguousarray(inp["x"][b]), "ctx": np.ascontiguousarray(inp["ctx"][b])}
        for k in ("w_mod", "b_mod", "w_in", "w_out", "w_router", "router_bias", "w_gate", "w_up", "w_down", "g_final",
                  "ssd_conv_w", "ssd_dt_bias", "ssd_a_log", "ssd_d", "ssd_norm", "hy_conv_w", "hy_conv_b", "hy_w1", "hy_w2", "hy_w3", "hy_bias"):
            m[k] = inp[k]
        m["cols"] = np.stack([_cols_pack(inp, li, b) for li in range(DEPTH)])
        m.update(_CONSTS)
        in_maps.append(m)
    res = run_bass_kernel_spmd(nc, in_maps, core_ids=list(range(ncores)))
    kernel.last = res
    return np.stack([res.results[b]["out"] for b in range(4)]).astype(np.float32)


def outproj(G, li):
    nc, S, I = G.nc, G.S, G.I
    need_ctx = li < DEPTH - 1
    with ExitStack() as _es:
        wo = _es.enter_context(SB(nc, "wo", [128, 8, D], BF16))
        mx = _es.enter_context(SB(nc, "mx", [128, 2, 8, 128], BF16))
        xt = _es.enter_context(SB(nc, "xt", [128, 2, D], F32))
        tmp = _es.enter_context(SB(nc, "tmp", [128, D], F32))
        sq = _es.enter_context(SB(nc, "sq", [128, D], F32))
        ss = _es.enter_context(SB(nc, "ss", [128, 4], F32))
        xn = _es.enter_context(SB(nc, "xn", [128, D], F32))
        h2f = _es.enter_context(SB(nc, "h2f", [128, 8, 128], F32))
        wr = _es.enter_context(SB(nc, "wr", [128, 8, NE], F32))
        po = _es.enter_context(PS(nc, "po", [128, 2, 512], F32))
        pt = _es.enter_context(PS(nc, "pt", [128, 8, 128], F32))
        pr = _es.enter_context(PS(nc, "pr", [128, NE], F32))
        t_wo, t_mx, t_x, t_tmp, t_po = Tok(), [Tok(), Tok()], [Tok(), Tok()], Tok(), Tok()
        t_sq, t_ss, t_xn, t_pt, t_h2f, t_wr, t_pr = Tok(), Tok(), Tok(), Tok(), Tok(), Tok(), Tok()
        S.dma(wo[:], G.wb_out[li].rearrange("(k p) c -> p k c", p=128), reads=[G.t_wb], writes=[t_wo])
        S.dma(wr[:], I["w_router"].rearrange("(k p) c -> p k c", p=128), writes=[t_wr])
        mv = G.mixT.rearrange("(k p) t -> p k t", p=128)
        tiles = [t for t in range(NT) if need_ctx or t >= 2]

        def mm(tile):
            b = tile % 2
            S.dma(mx[:, b], mv[:, :, tile * 128:(tile + 1) * 128], reads=[G.t_mix], writes=[t_mx[b]])
            S.dma(xt[:, b, :], G.xres[tile * 128:(tile + 1) * 128, :], reads=[G.t_xres[tile]], writes=[t_x[b]])
            for half in range(2):
                for k in range(8):
                    S.op("pe", lambda e: e.matmul(po[:, half, :], lhsT=mx[:, b, k, :], rhs=wo[:, k, half * 512:(half + 1) * 512],
                                                  start=(k == 0), stop=(k == 7)), reads=[t_mx[b], t_wo], writes=[t_po])

        mm(tiles[0])
        for ti, tile in enumerate(tiles):
            b = tile % 2
            j = 1 if tile < 2 else 0
            S.op("dve", lambda e: e.tensor_tensor(out=tmp[:], in0=po[:].rearrange("p a c -> p (a c)"), in1=G.gtb[:, 0, j, :], op=ALU.mult),
                 reads=[t_po, G.t_gtb], writes=[t_tmp])
            if ti + 1 < len(tiles):
                mm(tiles[ti + 1])
            S.op("dve", lambda e: e.tensor_tensor(out=xt[:, b, :], in0=tmp[:], in1=xt[:, b, :], op=ALU.add),
                 reads=[t_tmp, t_x[b]], writes=[t_x[b]])
            S.dma(G.xres[tile * 128:(tile + 1) * 128, :], xt[:, b, :], reads=[t_x[b]], writes=[G.t_xres[tile]])
            xv = xt[:, b, :]
            S.op("act", lambda e: e.activation(out=sq[:], in_=xv, func=AF.Square, accum_out=ss[:, 0:1]), reads=[t_x[b]], writes=[t_sq, t_ss])
            S.op("act", lambda e: e.activation(out=ss[:, 1:2], in_=ss[:, 0:1], func=AF.Sqrt, bias=G.epsc[:], scale=1.0 / D),
                 reads=[t_ss, G.t_c], writes=[t_ss])
            S.op("dve", lambda e: e.reciprocal(out=ss[:, 2:3], in_=ss[:, 1:2]), reads=[t_ss], writes=[t_ss])
            S.op("dve", lambda e: e.tensor_scalar(out=xn[:], in0=xv, scalar1=ss[:, 2:3], scalar2=None, op0=ALU.mult),
                 reads=[t_x[b], t_ss], writes=[t_xn])
            for k in range(8):
                S.op("pe", lambda e: e.transpose(out=pt[:, k, :], in_=xn[:, k * 128:(k + 1) * 128], identity=G.identF),
                     reads=[t_xn, G.t_c], writes=[t_pt])
            S.op("dve", lambda e: e.tensor_tensor(out=h2f[:], in0=pt[:], in1=G.modc[:, 3, :, j:j + 1].to_broadcast([128, 8, 128]), op=ALU.mult),
                 reads=[t_pt, G.t_modc], writes=[t_h2f])
            S.op("dve", lambda e: e.tensor_tensor(out=h2f[:], in0=h2f[:], in1=G.modc[:, 2, :, j:j + 1].to_broadcast([128, 8, 128]), op=ALU.add),
                 reads=[t_h2f, G.t_modc], writes=[t_h2f])
            S.op("act", lambda e: e.copy(out=G.h2T[:, :, tile * 128:(tile + 1) * 128], in_=h2f[:]), reads=[t_h2f], writes=[G.t_h2T])
            for k in range(8):
                S.op("pe", lambda e: e.matmul(pr[:], lhsT=h2f[:, k, :], rhs=wr[:, k, :], start=(k == 0), stop=(k == 7)),
                     reads=[t_h2f, t_wr], writes=[t_pr])
            S.op("dve", lambda e: e.tensor_copy(out=G.rl[:, tile, :], in_=pr[:]), reads=[t_pr], writes=[G.t_rl])


def moe(G, li):
    nc, S, I = G.nc, G.S, G.I
    need_ctx = li < DEPTH - 1
    last = li == DEPTH - 1
    h2T, rl = G.h2T, G.rl
    T0 = 0 if need_ctx else 2
    NTl = NT - T0
    BIG = 1.0e9
    with ExitStack() as _es:
        comb = _es.enter_context(SB(nc, "comb", [128, NT, NE], F32))
        t_comb = Tok()
        with ExitStack() as _es:
            sc = _es.enter_context(SB(nc, "r_sc", [128, NT, NE], F32))
            sel = _es.enter_context(SB(nc, "r_sel", [128, NT, NE], F32))
            ra = _es.enter_context(SB(nc, "r_a", [128, NT, NE], F32))
            rb = _es.enter_context(SB(nc, "r_b", [128, NT, NE], F32))
            rm = _es.enter_context(SB(nc, "r_m", [128, NT * 4], F32))
            rm2 = _es.enter_context(SB(nc, "r_m2", [128, NT * 4], F32))
            rg = _es.enter_context(SB(nc, "r_g", [128, NT], F32))
            rbias = _es.enter_context(SB(nc, "rbias", [128, NE], F32))
            t = Tok()
            if T0 > 0:
                S.op("dve", lambda e: e.memset(rl[:, 0:T0, :], 0.0), reads=[G.t_rl], writes=[G.t_rl])
            S.dma(rbias[:], I["router_bias"].partition_broadcast(128), writes=[t])
            v3 = lambda a: a[:].rearrange("p n (g x) -> p (n g) x", x=4)
            S.op("act", lambda e: e.activation(out=sc[:], in_=rl[:], func=AF.Sigmoid), reads=[G.t_rl], writes=[t])
            S.op("dve", lambda e: e.tensor_tensor(out=sel[:], in0=sc[:], in1=rbias[:].unsqueeze(1).to_broadcast([128, NT, NE]), op=ALU.add),
                 reads=[t], writes=[t])
            S.op("dve", lambda e: e.tensor_reduce(out=rm[:], in_=v3(sel), axis=AX.X, op=ALU.max), reads=[t], writes=[t])
            S.op("dve", lambda e: e.tensor_tensor(out=v3(ra), in0=v3(sel), in1=rm[:].unsqueeze(2).to_broadcast([128, NT * 4, 4]), op=ALU.is_equal),
                 reads=[t], writes=[t])
            S.op("dve", lambda e: e.scalar_tensor_tensor(out=rb[:], in0=ra[:], scalar=-BIG, in1=sel[:], op0=ALU.mult, op1=ALU.add),
                 reads=[t], writes=[t])
            S.op("dve", lambda e: e.tensor_reduce(out=rm2[:], in_=v3(rb), axis=AX.X, op=ALU.max), reads=[t], writes=[t])
            S.op("dve", lambda e: e.tensor_tensor(out=rm[:], in0=rm[:], in1=rm2[:], op=ALU.add), reads=[t], writes=[t])
            S.op("dve", lambda e: e.tensor_reduce(out=rg[:], in_=rm[:].rearrange("p (n g) -> p n g", g=4), axis=AX.X, op=ALU.max),
                 reads=[t], writes=[t])
            S.op("dve", lambda e: e.tensor_tensor(out=rm2[:].rearrange("p (n g) -> p n g", g=4), in0=rm[:].rearrange("p (n g) -> p n g", g=4),
                                                  in1=rg[:].unsqueeze(2).to_broadcast([128, NT, 4]), op=ALU.is_equal), reads=[t], writes=[t])
            S.op("dve", lambda e: e.tensor_scalar(out=rm2[:], in0=rm2[:], scalar1=1.0, scalar2=BIG, op0=ALU.subtract, op1=ALU.mult),
                 reads=[t], writes=[t])
            S.op("dve", lambda e: e.tensor_tensor(out=v3(sel), in0=v3(sel), in1=rm2[:].unsqueeze(2).to_broadcast([128, NT * 4, 4]), op=ALU.add),
                 reads=[t], writes=[t])
            S.op("dve", lambda e: e.tensor_reduce(out=rg[:], in_=sel[:], axis=AX.X, op=ALU.max), reads=[t], writes=[t])
            S.op("dve", lambda e: e.tensor_tensor(out=ra[:], in0=sel[:], in1=rg[:].unsqueeze(2).to_broadcast([128, NT, NE]), op=ALU.is_equal),
                 reads=[t], writes=[t])
            S.op("dve", lambda e: e.scalar_tensor_tensor(out=sel[:], in0=ra[:], scalar=-BIG, in1=sel[:], op0=ALU.mult, op1=ALU.add),
                 reads=[t], writes=[t])
            S.op("dve", lambda e: e.tensor_reduce(out=rg[:], in_=sel[:], axis=AX.X, op=ALU.max), reads=[t], writes=[t])
            S.op("dve", lambda e: e.tensor_tensor(out=rb[:], in0=sel[:], in1=rg[:].unsqueeze(2).to_broadcast([128, NT, NE]), op=ALU.is_equal),
                 reads=[t], writes=[t])
            S.op("dve", lambda e: e.tensor_tensor(out=ra[:], in0=ra[:], in1=rb[:], op=ALU.add), reads=[t], writes=[t])
            S.op("dve", lambda e: e.tensor_tensor(out=ra[:], in0=ra[:], in1=sc[:], op=ALU.mult), reads=[t], writes=[t])
            S.op("dve", lambda e: e.tensor_reduce(out=rg[:], in_=ra[:], axis=AX.X, op=ALU.add), reads=[t], writes=[t])
            S.op("dve", lambda e: e.reciprocal(out=rg[:], in_=rg[:]), reads=[t], writes=[t])
            S.op("dve", lambda e: e.tensor_tensor(out=comb[:], in0=ra[:], in1=rg[:].unsqueeze(2).to_broadcast([128, NT, NE]), op=ALU.mult),
                 reads=[t], writes=[t_comb])
            S.barrier()
        SGT = 12
        with ExitStack() as _es:
            acc = _es.enter_context(SB(nc, "acc", [128, SGT, D], F32))
            wg = _es.enter_context(SB(nc, "wg", [128, 2, 8, DFF], BF16))
            wu = _es.enter_context(SB(nc, "wu", [128, 2, 8, DFF], BF16))
            wd = _es.enter_context(SB(nc, "wd", [128, 2, 2, D], BF16))
            sgl = _es.enter_context(SB(nc, "sgl", [128, 2, 512], F32))
            aa2 = _es.enter_context(SB(nc, "aa", [128, 2, 2, 512], BF16))
            xt = _es.enter_context(SB(nc, "xt", [128, 2, D], F32))
            gfb = _es.enter_context(SB(nc, "gfb", [128, D], F32))
            ss = _es.enter_context(SB(nc, "ss", [128, 4], F32))
            sq = _es.enter_context(SB(nc, "sq", [128, D], F32))
            pgu = _es.enter_context(PS(nc, "pgu", [128, 4, 512], F32))
            py = _es.enter_context(PS(nc, "py", [128, 2, 2, 512], F32))
            t_pgu2, t_sgl2, t_aa2 = [Tok(), Tok()], [Tok(), Tok()], [Tok(), Tok()]
            t_acc, t_w, t_sgl, t_aa, t_pgu, t_py, t_x, t_gf, t_ss, t_sq = Tok(), [Tok(), Tok()], Tok(), Tok(), Tok(), [Tok(), Tok()], [Tok(), Tok()], Tok(), Tok(), Tok()
            if last:
                S.dma(gfb[:], I["g_final"].partition_broadcast(128), writes=[t_gf])
            yi = 0
            ui = 0
            for s0 in range(T0, NT, SGT):
                tiles = list(range(s0, min(NT, s0 + SGT)))
                units = []
                for ex in range(NE):
                    for b0 in range(0, len(tiles), 4):
                        bt = tiles[b0:b0 + 4]
                        units.append((ex, bt, len(bt) * 128, bt[0] * 128, b0 == 0))

                def gu(u, jj):
                    ex, bt, n, c0, newex = units[u]
                    wb_ = ex % 2
                    ub = (ui + u) % 2
                    if newex and jj == 0:
                        S.dma(wg[:, wb_], G.wb_gate[li][ex].rearrange("(k p) f -> p k f", p=128), reads=[G.t_wb], writes=[t_w[wb_]])
                        S.dma(wu[:, wb_], G.wb_up[li][ex].rearrange("(k p) f -> p k f", p=128), reads=[G.t_wb], writes=[t_w[wb_]])
                        S.dma(wd[:, wb_], G.wb_down[li][ex].rearrange("(j p) c -> p j c", p=128), reads=[G.t_wb], writes=[t_w[wb_]])
                    for wi, wt in enumerate((wg, wu)):
                        for k in range(8):
                            S.op("pe", lambda e: e.matmul(pgu[:, jj * 2 + wi, 0:n], lhsT=wt[:, wb_, k, jj * 128:(jj + 1) * 128],
                                                          rhs=h2T[:, k, c0:c0 + n], start=(k == 0), stop=(k == 7)),
                                 reads=[t_w[wb_], G.t_h2T], writes=[t_pgu2[jj]])
                    S.op("act", lambda e: e.activation(out=sgl[:, jj, 0:n], in_=pgu[:, jj * 2, 0:n], func=AF.Silu), reads=[t_pgu2[jj]], writes=[t_sgl2[jj]])
                    S.op("dve", lambda e: e.tensor_tensor(out=aa2[:, ub, jj, 0:n], in0=sgl[:, jj, 0:n], in1=pgu[:, jj * 2 + 1, 0:n], op=ALU.mult),
                         reads=[t_sgl2[jj], t_pgu2[jj]], writes=[t_aa2[ub]])

                def down(u):
                    nonlocal yi
                    ex, bt, n, c0, newex = units[u]
                    wb_ = ex % 2
                    ub = (ui + u) % 2
                    for ti, tl in enumerate(bt):
                        yb = yi % 2
                        yi += 1
                        for half in range(2):
                            for jj in range(2):
                                S.op("pe", lambda e: e.matmul(py[:, yb, half, :], lhsT=aa2[:, ub, jj, ti * 128:(ti + 1) * 128],
                                                              rhs=wd[:, wb_, jj, half * 512:(half + 1) * 512], start=(jj == 0), stop=(jj == 1)),
                                     reads=[t_aa2[ub], t_w[wb_]], writes=[t_py[yb]])
                        al = acc[:, tl - s0, :]
                        pyv = py[:, yb].rearrange("p a c -> p (a c)")
                        if ex == 0:
                            S.op("dve", lambda e: e.tensor_scalar(out=al, in0=pyv, scalar1=comb[:, tl, ex:ex + 1], scalar2=None, op0=ALU.mult),
                                 reads=[t_py[yb], t_comb], writes=[t_acc])
                        else:
                            S.op("dve", lambda e: e.scalar_tensor_tensor(out=al, in0=pyv, scalar=comb[:, tl, ex:ex + 1], in1=al,
                                                                         op0=ALU.mult, op1=ALU.add), reads=[t_py[yb], t_comb, t_acc], writes=[t_acc])

                gu(0, 0)
                gu(0, 1)
                for u in range(len(units)):
                    if u + 1 < len(units):
                        gu(u + 1, 0)
                    down(u)
                    if u + 1 < len(units):
                        gu(u + 1, 1)
                ui += len(units)
                for tl in tiles:
                    b = tl % 2
                    j = 1 if tl < 2 else 0
                    S.dma(xt[:, b, :], G.xres[tl * 128:(tl + 1) * 128, :], reads=[G.t_xres[tl]], writes=[t_x[b]])
                    al = acc[:, tl - s0, :]
                    S.op("dve", lambda e: e.tensor_tensor(out=al, in0=al, in1=G.gtb[:, 1, j, :], op=ALU.mult), reads=[t_acc, G.t_gtb], writes=[t_acc])
                    S.op("dve", lambda e: e.tensor_tensor(out=xt[:, b, :], in0=al, in1=xt[:, b, :], op=ALU.add), reads=[t_acc, t_x[b]], writes=[t_x[b]])
                    if not last:
                        S.dma(G.xres[tl * 128:(tl + 1) * 128, :], xt[:, b, :], reads=[t_x[b]], writes=[G.t_xres[tl]])
                    else:
                        xv = xt[:, b, :]
                        S.op("act", lambda e: e.activation(out=sq[:], in_=xv, func=AF.Square, accum_out=ss[:, 0:1]), reads=[t_x[b]], writes=[t_sq, t_ss])
                        S.op("act", lambda e: e.activation(out=ss[:, 1:2], in_=ss[:, 0:1], func=AF.Sqrt, bias=G.epsc[:], scale=1.0 / D),
                             reads=[t_ss, G.t_c], writes=[t_ss])
                        S.op("dve", lambda e: e.reciprocal(out=ss[:, 2:3], in_=ss[:, 1:2]), reads=[t_ss], writes=[t_ss])
                        S.op("dve", lambda e: e.scalar_tensor_tensor(out=xv, in0=xv, scalar=ss[:, 2:3], in1=gfb[:], op0=ALU.mult, op1=ALU.mult),
                             reads=[t_x[b], t_ss, t_gf], writes=[t_x[b]])
                        S.dma(G.out[(tl - 2) * 128:(tl - 1) * 128, :], xv, reads=[t_x[b]], writes=[])


def ssd(G, li):
    nc, S, I = G.nc, G.S, G.I
    hT = G.hT
    need_ctx = li < DEPTH - 1
    wv32 = I["w_in"][li].rearrange("(k p) c -> p k c", p=128)
    wvb = G.wb_in[li].rearrange("(k p) c -> p k c", p=128)
    XB0 = 1024
    with ExitStack() as _es:
        xbcT = _es.enter_context(SB(nc, "xbcT", [128, 5, NTOK], BF16))
        xs_tok = _es.enter_context(SB(nc, "xs_tok", [128, NT, 384], BF16))
        B_tok = _es.enter_context(SB(nc, "B_tok", [128, NT, 128], BF16))
        lndt = _es.enter_context(SB(nc, "lndt", [128, NT, 12], F32))
        dta = _es.enter_context(SB(nc, "dta", [128, NT, 12], F32))
        ea = _es.enter_context(SB(nc, "ea", [128, NT, 12], F32))
        ww = _es.enter_context(SB(nc, "ww", [128, NT, 12], F32))
        eT = _es.enter_context(SB(nc, "eT", [128, NT, 12], F32))
        t_xbc, t_xs, t_dt = Tok(), Tok(), Tok()
        with ExitStack() as _es2:
            dts = _es2.enter_context(SB(nc, "dts", [128, NT, 12], F32))
            wst = _es2.enter_context(SB(nc, "wst", [128, 8, 128], F32))
            cwb = _es2.enter_context(SB(nc, "cwb", [128, 3, 128], F32))
            wj = _es2.enter_context(SB(nc, "wj", [128, 3, 8, 128], BF16))
            wdt = _es2.enter_context(SB(nc, "wdt", [128, 8, 12], BF16))
            dtb = _es2.enter_context(SB(nc, "dtb", [128, 2, 12], F32))
            tot = _es2.enter_context(SB(nc, "tot", [128, NT, 12], F32))
            wcol = _es2.enter_context(SB(nc, "wcol", [128, NT, 12], F32))
            pp2 = _es2.enter_context(PS(nc, "pp", [128, 2, 512], F32))
            pdt = _es2.enter_context(PS(nc, "pdt", [128, NT, 12], F32))
            ptb = _es2.enter_context(PS(nc, "ptb", [128, 4, 128], BF16))
            pc = _es2.enter_context(PS(nc, "pc", [128, NT, 12], F32))
            t_wst, t_cwb, t_wj, t_pp, t_wdt, t_pdt, t_ptb, t_a, t_pc = Tok(), Tok(), Tok(), Tok(), Tok(), Tok(), Tok(), Tok(), Tok()
            ocb = COLS["ssd_conv_b"][0]
            t_pp2 = [Tok(), Tok()]
            for ch in range(5):
                S.dma(wst[:], wv32[:, :, XB0 + ch * 128:XB0 + (ch + 1) * 128], writes=[t_wst])
                for j in range(3):
                    S.dma(cwb[:, j, :], I["ssd_conv_w"][li, j, ch * 128:(ch + 1) * 128].partition_broadcast(128), writes=[t_cwb])
                for j in range(3):
                    S.op("dve", lambda e: e.tensor_tensor(out=wj[:, j], in0=wst[:], in1=cwb[:, j:j + 1, :].to_broadcast([128, 8, 128]), op=ALU.mult),
                         reads=[t_wst, t_cwb], writes=[t_wj])
                for bi_, (c0, t0, n, tile0, ntile) in enumerate(BLOCKS):
                    pb_ = (ch * len(BLOCKS) + bi_) % 2
                    pp = pp2[:, pb_, :]
                    for j in range(3):
                        for k in range(8):
                            S.op("pe", lambda e: e.matmul(pp[:, 0:n], lhsT=wj[:, j, k, :], rhs=hT[:, k, c0 + j - 1:c0 + j - 1 + n],
                                                          start=(j == 0 and k == 0), stop=(j == 2 and k == 7)), reads=[t_wj, G.t_hT], writes=[t_pp2[pb_]])
                    S.op("act", lambda e: e.activation(out=xbcT[:, ch, t0:t0 + n], in_=pp[:, 0:n], func=AF.Silu, bias=G.cols[:, ocb + ch:ocb + ch + 1]),
                         reads=[t_pp2[pb_], G.t_cols], writes=[t_xbc])
            S.dma(wdt[:], wvb[:, :, 1664:1676], reads=[G.t_wb], writes=[t_wdt])
            S.dma(dtb[:, 0, :], I["ssd_dt_bias"][li].rearrange("a h -> (a h)").partition_broadcast(128), writes=[t_wdt])
            S.dma(dtb[:, 1, :], I["ssd_a_log"][li].rearrange("a h -> (a h)").partition_broadcast(128), writes=[t_wdt])
            for tl in range(NT):
                cc = colof(tl)
                for k in range(8):
                    S.op("pe", lambda e: e.matmul(pdt[:, tl, :], lhsT=hT[:, k, cc:cc + 128], rhs=wdt[:, k, :], start=(k == 0), stop=(k == 7)),
                         reads=[t_wdt, G.t_hT], writes=[t_pdt])
            S.op("dve", lambda e: e.tensor_tensor(out=dts[:], in0=pdt[:], in1=dtb[:, 0:1, :].to_broadcast([128, NT, 12]), op=ALU.add),
                 reads=[t_pdt, t_wdt], writes=[t_dt])
            S.op("act", lambda e: e.activation(out=dts[:], in_=dts[:], func=AF.Exp), reads=[t_dt], writes=[t_dt])
            S.op("act", lambda e: e.activation(out=dts[:], in_=dts[:], func=AF.Ln, bias=1.0), reads=[t_dt], writes=[t_dt])
            S.op("act", lambda e: e.activation(out=lndt[:], in_=dts[:], func=AF.Ln), reads=[t_dt], writes=[t_dt])
            S.op("act", lambda e: e.activation(out=dtb[:, 1, :], in_=dtb[:, 1, :], func=AF.Exp), reads=[t_wdt], writes=[t_wdt])
            S.op("dve", lambda e: e.scalar_tensor_tensor(out=dta[:], in0=dts[:], scalar=-1.0, in1=dtb[:, 1:2, :].to_broadcast([128, NT, 12]),
                                                         op0=ALU.mult, op1=ALU.mult), reads=[t_dt, t_wdt], writes=[t_a])
            for tl in range(NT):
                for c in range(4):
                    S.op("pe", lambda e: e.transpose(out=ptb[:, c, :], in_=xbcT[:, c, tl * 128:(tl + 1) * 128], identity=G.identB),
                         reads=[t_xbc, G.t_c], writes=[t_ptb])
                S.op("dve", lambda e: e.tensor_copy(out=xs_tok[:, tl, :], in_=ptb[:, 0:3, :].rearrange("p c t -> p (c t)")), reads=[t_ptb], writes=[t_xs])
                S.op("dve", lambda e: e.tensor_copy(out=B_tok[:, tl, :], in_=ptb[:, 3, :]), reads=[t_ptb], writes=[t_xs])
            for dr in range(2):
                S.op("pe", lambda e: e.matmul(pc[:].rearrange("p n h -> p (n h)"), lhsT=G.cm[:, 1 + dr, :], rhs=dta[:].rearrange("p n h -> p (n h)"),
                                              start=True, stop=True), reads=[t_a, G.t_c, t_dt], writes=[t_pc])
                S.op("dve", lambda e: e.tensor_copy(out=wcol[:, :, dr * 6:(dr + 1) * 6], in_=pc[:, :, dr * 6:(dr + 1) * 6]), reads=[t_pc], writes=[t_dt])
            S.op("pe", lambda e: e.matmul(pc[:].rearrange("p n h -> p (n h)"), lhsT=G.ones[:], rhs=dta[:].rearrange("p n h -> p (n h)"), start=True, stop=True),
                 reads=[t_a, G.t_c, t_dt], writes=[t_pc])
            S.op("dve", lambda e: e.tensor_copy(out=tot[:], in_=pc[:]), reads=[t_pc], writes=[t_dt])
            S.op("act", lambda e: e.activation(out=ea[:], in_=wcol[:], func=AF.Exp), reads=[t_dt], writes=[t_dt])
            S.op("act", lambda e: e.activation(out=eT[:], in_=tot[:], func=AF.Exp), reads=[t_dt], writes=[t_dt])
            S.op("dve", lambda e: e.tensor_tensor(out=ww[:], in0=tot[:], in1=wcol[:], op=ALU.subtract), reads=[t_dt], writes=[t_dt])
            S.op("dve", lambda e: e.tensor_tensor(out=ww[:], in0=ww[:], in1=lndt[:], op=ALU.add), reads=[t_dt], writes=[t_dt])
            S.op("act", lambda e: e.activation(out=ww[:], in_=ww[:], func=AF.Exp), reads=[t_dt], writes=[t_dt])
            S.barrier()
        with ExitStack() as _es2:
            wz = _es2.enter_context(SB(nc, "wz", [128, 8, 384], BF16))
            dbc = _es2.enter_context(SB(nc, "dbc", [128, 6, 64], F32))
            d6 = _es2.enter_context(SB(nc, "d6", [128, 6], F32))
            nwb = _es2.enter_context(SB(nc, "nwb", [128, 384], F32))
            hst = _es2.enter_context(SB(nc, "hst", [128, 192], F32))
            hsb = _es2.enter_context(SB(nc, "hsb", [128, 192], BF16))
            xw = _es2.enter_context(SB(nc, "xw", [128, 2, 192], BF16))
            ybt = _es2.enter_context(SB(nc, "ybt", [128, 2, 384], F32))
            Sm = _es2.enter_context(SB(nc, "Sm", [128, 2, 2, 128], F32))
            rA = _es2.enter_context(SB(nc, "rA", [128, 4, 128], F32))
            Dd = _es2.enter_context(SB(nc, "Dd", [128, 4, 128], F32))
            Mt = _es2.enter_context(SB(nc, "Mt", [128, 4, 128], BF16))
            acc = _es2.enter_context(SB(nc, "acc", [128, 384], F32))
            tmp = _es2.enter_context(SB(nc, "tmp", [128, 384], F32))
            zs = _es2.enter_context(SB(nc, "zs", [128, 384], F32))
            ssq = _es2.enter_context(SB(nc, "ssq", [128, 4], F32))
            ob = _es2.enter_context(SB(nc, "ob", [128, 384], BF16))
            oT = _es2.enter_context(SB(nc, "oT", [128, 2, 3, 128], BF16))
            pis = _es2.enter_context(PS(nc, "pis", [128, 2, 192], F32))
            ps_st = _es2.enter_context(PS(nc, "ps_st", [128, 2, 512], F32))
            pseg = _es2.enter_context(PS(nc, "pseg", [128, 3, 512], F32))
            py = _es2.enter_context(PS(nc, "py", [128, 384], F32))
            pz = py
            ptr = _es2.enter_context(PS(nc, "ptr", [128, 3, 128], BF16))
            t_wz, t_db, t_h, t_xw, t_yb, t_Sm, t_acc, t_tmp, t_zs, t_ssq, t_ob, t_oT = Tok(), Tok(), Tok(), Tok(), [Tok(), Tok()], Tok(), Tok(), Tok(), Tok(), Tok(), Tok(), [Tok(), Tok()]
            t_rA, t_Dd, t_Mt, t_pseg = [Tok() for _ in range(4)], [Tok() for _ in range(4)], [Tok() for _ in range(4)], [Tok() for _ in range(3)]
            t_pis, t_pst, t_py, t_ptr = Tok(), Tok(), Tok(), Tok()
            t_pz = t_py
            S.dma(wz[:], wvb[:, :, 640:1024], reads=[G.t_wb], writes=[t_wz])
            S.dma(d6[:], I["ssd_d"][li].partition_broadcast(128), writes=[t_db])
            S.dma(nwb[:], I["ssd_norm"][li].partition_broadcast(128), writes=[t_db])
            S.op("dve", lambda e: e.tensor_copy(out=dbc[:], in_=d6[:].unsqueeze(2).to_broadcast([128, 6, 64])), reads=[t_db], writes=[t_db])
            yb_d = G.ssd_yb

            def carry_step(c, dr, want_out, dst):
                for g in range(2):
                    hd0 = dr * 6 + 3 * g
                    if want_out:
                        S.op("pe", lambda e: e.matmul(pis[:, 0, :], lhsT=xbcT[g * 64:(g + 1) * 64, 4, c * 128:(c + 1) * 128], rhs=hsb[g * 64:(g + 1) * 64, :],
                                                      start=True, stop=True), reads=[t_xbc, t_h], writes=[t_pis])
                        S.op("dve", lambda e: e.tensor_tensor(out=dst[:, g * 192:(g + 1) * 192].rearrange("p (a d) -> p a d", a=3),
                                                              in0=pis[:, 0, :].rearrange("p (a d) -> p a d", a=3),
                                                              in1=ea[:, c, hd0:hd0 + 3].unsqueeze(2).to_broadcast([128, 3, 64]), op=ALU.mult),
                             reads=[t_pis, t_dt], writes=[dst_tok[0]])
                    S.op("dve", lambda e: e.tensor_tensor(out=xw[:, g, :].rearrange("p (a d) -> p a d", a=3),
                                                          in0=xs_tok[:, c, g * 192:(g + 1) * 192].rearrange("p (a d) -> p a d", a=3),
                                                          in1=ww[:, c, hd0:hd0 + 3].unsqueeze(2).to_broadcast([128, 3, 64]), op=ALU.mult),
                         reads=[t_xs, t_dt], writes=[t_xw])
                    gs = slice(g * 64, (g + 1) * 64)
                    S.op("pe", lambda e: e.matmul(pis[:, 1, :], lhsT=B_tok[:, c, :], rhs=xw[:, g, :], start=True, stop=True),
                         reads=[t_xs, t_xw], writes=[t_pis])
                    S.op("dve", lambda e: e.tensor_tensor(out=hst[gs, :].rearrange("p (a d) -> p a d", a=3),
                                                          in0=hst[gs, :].rearrange("p (a d) -> p a d", a=3),
                                                          in1=eT[gs, c, hd0:hd0 + 3].unsqueeze(2).to_broadcast([64, 3, 64]), op=ALU.mult),
                         reads=[t_h, t_dt], writes=[t_h])
                    S.op("dve", lambda e: e.tensor_tensor(out=hst[gs, :], in0=hst[gs, :], in1=pis[gs, 1, :], op=ALU.add),
                         reads=[t_h, t_pis], writes=[t_h])
                    S.op("act", lambda e: e.copy(out=hsb[gs, :], in_=hst[gs, :]), reads=[t_h], writes=[t_h])

            S.op("dve", lambda e: e.memset(hst[:], 0.0), writes=[t_h])
            S.op("dve", lambda e: e.memset(hsb[:], 0.0), writes=[t_h])
            order_b = [1, 0] + list(range(NT - 1, 1, -1))
            for ci, c in enumerate(order_b):
                want = need_ctx or c >= 2
                b = ci % 2
                dst_tok = [t_yb[b]]
                carry_step(c, 1, want, ybt[:, b, :])
                if want:
                    S.dma(yb_d[c * 128:(c + 1) * 128, :], ybt[:, b, :], reads=[t_yb[b]], writes=[G.t_ssdyb[c]])
            S.op("dve", lambda e: e.memset(hst[:], 0.0), reads=[t_h], writes=[t_h])
            S.op("dve", lambda e: e.memset(hsb[:], 0.0), reads=[t_h], writes=[t_h])
            on = COLS["ssd_norm"][0]
            it = 0
            for c in range(NT):
                want = need_ctx or c >= 2
                dst_tok = [t_acc]
                carry_step(c, 0, want, acc[:])
                if not want:
                    continue
                b = c % 2
                tc0 = c * 128
                S.dma(ybt[:, b, :], yb_d[c * 128:(c + 1) * 128, :], reads=[G.t_ssdyb[c]], writes=[t_yb[b]])
                cc = colof(c)
                for k in range(8):
                    S.op("pe", lambda e: e.matmul(pz[:], lhsT=hT[:, k, cc:cc + 128], rhs=wz[:, k, :], start=(k == 0), stop=(k == 7)),
                         reads=[t_wz, G.t_hT], writes=[t_pz])
                S.op("act", lambda e: e.activation(out=zs[:], in_=pz[:], func=AF.Silu), reads=[t_pz], writes=[t_zs])
                for g in range(2):
                    S.op("pe", lambda e: e.matmul(ps_st[:, g, 0:128], lhsT=xbcT[g * 64:(g + 1) * 64, 3, tc0:tc0 + 128], rhs=xbcT[g * 64:(g + 1) * 64, 4, tc0:tc0 + 128],
                                                  start=True, stop=True), reads=[t_xbc], writes=[t_pst])
                for g in range(2):
                    for dr in range(2):
                        S.op("dve", lambda e: e.tensor_tensor(out=Sm[:, g, dr, :], in0=ps_st[:, g, 0:128], in1=G.cm[:, 1 + dr, :], op=ALU.mult),
                             reads=[t_pst, G.t_c], writes=[t_Sm])
                steps = [(h, dr) for h in range(6) for dr in range(2)]

                def st_a(i):
                    h, dr = steps[i]
                    hd = dr * 6 + h
                    r4, p3 = (it + i) % 4, (it + i) % 3
                    S.op("dve", lambda e: e.tensor_scalar(out=rA[:, r4, :], in0=G.cm[:, 1 + dr, :], scalar1=dta[:, c, hd:hd + 1], scalar2=None, op0=ALU.mult),
                         reads=[G.t_c, t_dt], writes=[t_rA[r4]])
                    S.op("pe", lambda e: e.matmul(pseg[:, p3, 0:128], lhsT=G.cm[:, 8 - dr, :], rhs=rA[:, r4, :], start=True, stop=True),
                         reads=[t_rA[r4], G.t_c], writes=[t_pseg[p3]])
                    S.op("act", lambda e: e.activation(out=Dd[:, r4, :], in_=pseg[:, p3, 0:128], func=AF.Exp, bias=lndt[:, c, hd:hd + 1]),
                         reads=[t_pseg[p3], t_dt], writes=[t_Dd[r4]])

                st_a(0)
                st_a(1)
                for i, (h, dr) in enumerate(steps):
                    r4 = (it + i) % 4
                    if i + 2 < len(steps):
                        st_a(i + 2)
                    S.op("dve", lambda e: e.tensor_tensor(out=Mt[:, r4, :], in0=Dd[:, r4, :], in1=Sm[:, h // 3, dr, :], op=ALU.mult),
                         reads=[t_Dd[r4], t_Sm], writes=[t_Mt[r4]])
                    S.op("pe", lambda e: e.matmul(py[:, h * 64:(h + 1) * 64], lhsT=Mt[:, r4, :], rhs=xs_tok[:, c, h * 64:(h + 1) * 64],
                                                  start=(dr == 0), stop=(dr == 1)), reads=[t_Mt[r4], t_xs], writes=[t_py])
                it += len(steps)
                S.op("dve", lambda e: e.tensor_tensor(out=acc[:], in0=acc[:], in1=py[:], op=ALU.add), reads=[t_acc, t_py], writes=[t_acc])
                S.op("dve", lambda e: e.tensor_tensor(out=acc[:], in0=acc[:], in1=ybt[:, b, :], op=ALU.add), reads=[t_acc, t_yb[b]], writes=[t_acc])
                S.op("dve", lambda e: e.tensor_tensor(out=tmp[:], in0=xs_tok[:, c, :], in1=dbc[:].rearrange("p a d -> p (a d)"), op=ALU.mult),
                     reads=[t_xs, t_db], writes=[t_tmp])
                S.op("dve", lambda e: e.tensor_tensor(out=acc[:], in0=acc[:], in1=tmp[:], op=ALU.add), reads=[t_acc, t_tmp], writes=[t_acc])
                S.op("dve", lambda e: e.tensor_tensor(out=acc[:], in0=acc[:], in1=zs[:], op=ALU.mult), reads=[t_acc, t_zs], writes=[t_acc])
                for g in range(2):
                    S.op("act", lambda e: e.activation(out=tmp[:, g * 192:(g + 1) * 192], in_=acc[:, g * 192:(g + 1) * 192], func=AF.Square, accum_out=ssq[:, g:g + 1]),
                         reads=[t_acc], writes=[t_tmp, t_ssq])
                S.op("act", lambda e: e.activation(out=ssq[:, 2:4], in_=ssq[:, 0:2], func=AF.Sqrt, bias=G.epsc[:], scale=1.0 / 192), reads=[t_ssq, G.t_c], writes=[t_ssq])
                S.op("dve", lambda e: e.reciprocal(out=ssq[:, 2:4], in_=ssq[:, 2:4]), reads=[t_ssq], writes=[t_ssq])
                for g in range(2):
                    S.op("dve", lambda e: e.scalar_tensor_tensor(out=ob[:, g * 192:(g + 1) * 192], in0=acc[:, g * 192:(g + 1) * 192], scalar=ssq[:, 2 + g:3 + g],
                                                                 in1=nwb[:, g * 192:(g + 1) * 192], op0=ALU.mult, op1=ALU.mult), reads=[t_acc, t_ssq, t_db], writes=[t_ob])
                for c3 in range(3):
                    S.op("pe", lambda e: e.transpose(out=ptr[:, c3, :], in_=ob[:, c3 * 128:(c3 + 1) * 128], identity=G.identB), reads=[t_ob, G.t_c], writes=[t_ptr])
                S.op("act", lambda e: e.copy(out=oT[:, b], in_=ptr[:]), reads=[t_ptr], writes=[t_oT[b]])
                S.dma(G.mixT[384:768, tc0:tc0 + 128].rearrange("(c p) t -> p c t", p=128), oT[:, b], reads=[t_oT[b]], writes=[G.t_mix])
        if "ssd" in G.dbg and li == 0:
            dump_bf16(G, G.mixT[384:512, 256:768], G.dbg["ssd"], [G.t_mix])


HY0 = 1676
SEGS = {"lat": dict(L=4096, NC=32, KC=17, off=NCTX), "ctx": dict(L=256, NC=2, KC=2, off=0)}


def hyena_inproj(G, li):
    nc, S, I = G.nc, G.S, G.I
    hT = G.hT
    need_ctx = li < DEPTH - 1
    wv32 = I["w_in"][li].rearrange("(k p) c -> p k c", p=128)
    with ExitStack() as _es:
        wst = _es.enter_context(SB(nc, "wst", [128, 8, 128], F32))
        cwb = _es.enter_context(SB(nc, "cwb", [128, 3, 128], F32))
        wj = _es.enter_context(SB(nc, "wjh", [128, 3, 8, 768], BF16))
        hb = _es.enter_context(SB(nc, "hb", [128, 768], F32))
        hv = _es.enter_context(SB(nc, "hv", [128, 2, 768], BF16))
        ph4 = _es.enter_context(PS(nc, "ph", [128, 2, 2, 512], F32))
        t_wst, t_cwb, t_wj, t_hb, t_hv, t_ph2 = Tok(), Tok(), Tok(), Tok(), [Tok(), Tok()], [Tok(), Tok()]
        S.dma(hb[:], I["hy_conv_b"][li].partition_broadcast(128), writes=[t_hb])
        for cc in range(6):
            S.dma(wst[:], wv32[:, :, HY0 + cc * 128:HY0 + (cc + 1) * 128], writes=[t_wst])
            for j in range(3):
                S.dma(cwb[:, j, :], I["hy_conv_w"][li, j, cc * 128:(cc + 1) * 128].partition_broadcast(128), writes=[t_cwb])
            for j in range(3):
                S.op("dve", lambda e: e.tensor_tensor(out=wj[:, j, :, cc * 128:(cc + 1) * 128], in0=wst[:],
                                                      in1=cwb[:, j:j + 1, :].to_broadcast([128, 8, 128]), op=ALU.mult),
                     reads=[t_wst, t_cwb], writes=[t_wj])
        dv = G.hyv.rearrange("m t c -> t m c")
        for tl in range(NT):
            if tl < 2 and not need_ctx:
                continue
            col = colof(tl)
            b = tl % 2
            ph = ph4[:, b]
            t_ph = t_ph2[b]
            for half in range(2):
                for j in range(3):
                    for k in range(8):
                        S.op("pe", lambda e: e.matmul(ph[:, half, 0:384], lhsT=hT[:, k, col + j - 1:col + j - 1 + 128],
                                                      rhs=wj[:, j, k, half * 384:(half + 1) * 384], start=(j == 0 and k == 0), stop=(j == 2 and k == 7)),
                             reads=[t_wj, G.t_hT], writes=[t_ph])
            S.op("dve", lambda e: e.tensor_tensor(out=hv[:, b, :].rearrange("p (a c) -> p a c", a=2), in0=ph[:, :, 0:384],
                                                  in1=hb[:].rearrange("p (a c) -> p a c", a=2), op=ALU.add), reads=[t_ph, t_hb], writes=[t_hv[b]])
            S.dma(dv[tl * 128:(tl + 1) * 128], hv[:, b, :].rearrange("p (m c) -> p m c", m=3), reads=[t_hv[b]], writes=[G.t_hyv])


def hyena_fft(G, li):
    nc, S, I = G.nc, G.S, G.I
    need_ctx = li < DEPTH - 1
    for sname in (("lat", "ctx") if need_ctx else ("lat",)):
        P = SEGS[sname]
        L, NCk, KC, off = P["L"], P["NC"], P["KC"], P["off"]
        NH = NCk // 2
        FT, IT, FE, WIN, MH = I["ft_" + sname], I["it_" + sname], I["fe_" + sname], I["win_" + sname], I["mh_" + sname]
        Kf = G.Kf[sname]
        t_kf = Tok()
        with ExitStack() as _es:
            hk = _es.enter_context(SB(nc, "hk", [128, NCk, 1024], BF16))
            fe = _es.enter_context(SB(nc, "fe", [33, L], F32))
            h1 = _es.enter_context(SB(nc, "h1", [64, L], F32))
            h2 = _es.enter_context(SB(nc, "h2", [64, L], F32))
            w1 = _es.enter_context(SB(nc, "w1", [33, 64], F32))
            w2 = _es.enter_context(SB(nc, "w2", [64, 64], F32))
            w3 = _es.enter_context(SB(nc, "w3", [64, 1024], F32))
            pre = _es.enter_context(SB(nc, "pre", [64, 512], F32))
            pr2 = _es.enter_context(SB(nc, "pr2", [64, 512], F32))
            t_pr2 = Tok()
            MAGIC = 1.5 * 2 ** 23
            wint = _es.enter_context(SB(nc, "wint", [128, 2, 2, 256], F32))
            hbias = _es.enter_context(SB(nc, "hbias", [128, 512], F32))
            mh = _es.enter_context(SB(nc, "mh", [128, KC], F32))
            slab = _es.enter_context(SB(nc, "slab", [128, 2, NCk, 2, 128], BF16))
            xo = _es.enter_context(SB(nc, "xo", [128, 2, 512], F32))
            sd = _es.enter_context(SB(nc, "sd", [128, 2, 2, 2, 512], F32))
            kf = _es.enter_context(SB(nc, "kf", [128, 2, 2, 2, 256], BF16))
            _es_mlp = ExitStack()
            pm = _es_mlp.enter_context(PS(nc, "pm", [64, 512], F32))
            phh = _es_mlp.enter_context(PS(nc, "phh", [128, 2, 512], F32))
            t_hk, t_fe, t_h1, t_h2, t_w, t_pre, t_win, t_slab, t_eo, t_sd, t_kft, t_pm, t_phh, t_psk = \
                Tok(), Tok(), Tok(), Tok(), Tok(), Tok(), [Tok(), Tok()], [Tok(), Tok()], Tok(), Tok(), Tok(), Tok(), Tok(), Tok()
            S.dma(fe[:], FE[:, :], writes=[t_fe])
            S.dma(w1[:], I["hy_w1"][li], writes=[t_w])
            S.dma(w2[:], I["hy_w2"][li], writes=[t_w])
            S.dma(w3[:], I["hy_w3"][li], writes=[t_w])
            S.dma(hbias[:], I["hy_bias"][li].rearrange("o c -> (o c)").partition_broadcast(128), writes=[t_w])
            S.dma(mh[:], MH[:, :], writes=[t_w])
            ob1, ofr, ob2 = COLS["hy_b1"][0], COLS["hy_freq"][0], COLS["hy_b2"][0]
            for (src, t_src, wt, kk, bcol, dst, t_dst) in ((fe, t_fe, w1, 33, ob1, h1, t_h1), (h1, t_h1, w2, 64, ob2, h2, t_h2)):
                for c0 in range(0, L, 512):
                    n = min(512, L - c0)
                    S.op("pe", lambda e: e.matmul(pm[:, 0:n], lhsT=wt[0:kk, :], rhs=src[0:kk, c0:c0 + n], start=True, stop=True),
                         reads=[t_w, t_src], writes=[t_pm])
                    S.op("dve", lambda e: e.tensor_scalar(out=pre[:, 0:n], in0=pm[:, 0:n], scalar1=G.cols[0:64, bcol:bcol + 1],
                                                          scalar2=G.cols[0:64, ofr:ofr + 1], op0=ALU.add, op1=ALU.mult),
                         reads=[t_pm, G.t_cols], writes=[t_pre])
                    S.op("dve", lambda e: e.tensor_scalar(out=pr2[:, 0:n], in0=pre[:, 0:n], scalar1=1.0 / (2.0 * math.pi), scalar2=MAGIC, op0=ALU.mult, op1=ALU.add),
                         reads=[t_pre], writes=[t_pr2])
                    S.op("dve", lambda e: e.tensor_scalar(out=pr2[:, 0:n], in0=pr2[:, 0:n], scalar1=-MAGIC, scalar2=None, op0=ALU.add),
                         reads=[t_pr2], writes=[t_pr2])
                    S.op("dve", lambda e: e.scalar_tensor_tensor(out=pre[:, 0:n], in0=pr2[:, 0:n], scalar=-2.0 * math.pi, in1=pre[:, 0:n], op0=ALU.mult, op1=ALU.add),
                         reads=[t_pr2, t_pre], writes=[t_pre])
                    S.op("act", lambda e: e.activation(out=dst[:, c0:c0 + n], in_=pre[:, 0:n], func=AF.Sin),
                         reads=[t_pre], writes=[t_dst])
            for c in range(NCk):
                b = c % 2
                S.dma(wint[:, b], WIN[c], writes=[t_win[b]])
                for half in range(2):
                    S.op("pe", lambda e: e.matmul(phh[:, half, :], lhsT=h2[:, c * 128:(c + 1) * 128], rhs=w3[:, half * 512:(half + 1) * 512], start=True, stop=True),
                         reads=[t_h2, t_w], writes=[t_phh])
                for dr in range(2):
                    S.op("dve", lambda e: e.tensor_tensor(out=hk[:, c, dr * 512:(dr + 1) * 512].rearrange("p (o c) -> p o c", o=2),
                                                          in0=phh[:, dr, :].rearrange("p (o c) -> p o c", o=2),
                                                          in1=wint[:, b, dr:dr + 1, :].to_broadcast([128, 2, 256]), op=ALU.mult),
                         reads=[t_phh, t_win[b]], writes=[t_hk])
            S.barrier()
            _es_mlp.close()
            psk2 = _es.enter_context(PS(nc, "psk", [128, 2, 2, 2, 512], F32))
            t_psk2 = [Tok(), Tok()]
            for kc in range(KC):
                b = kc % 2
                S.dma(slab[:, b], FT[kc], writes=[t_slab[b]])
                for dr in range(2):
                    psk = psk2[:, dr]
                    t_psk = t_psk2[dr]
                    for eo_ in range(2):
                        for ri in range(2):
                            for c in range(NH):
                                cc = eo_ * NH + c
                                S.op("pe", lambda e: e.matmul(psk[:, eo_, ri, :], lhsT=slab[:, b, cc, ri, :], rhs=hk[:, cc, dr * 512:(dr + 1) * 512],
                                                              start=(c == 0), stop=(c == NH - 1)), reads=[t_slab[b], t_hk], writes=[t_psk])
                    S.op("act", lambda e: e.copy(out=xo[:], in_=psk[:, 1]), reads=[t_psk], writes=[t_eo])
                    S.op("dve", lambda e: e.tensor_tensor(out=sd[:, dr, 0], in0=psk[:, 0], in1=xo[:], op=ALU.add), reads=[t_psk, t_eo], writes=[t_sd])
                    S.op("dve", lambda e: e.tensor_tensor(out=sd[:, dr, 1], in0=psk[:, 0], in1=xo[:], op=ALU.subtract), reads=[t_psk, t_eo], writes=[t_sd])
                v = lambda ap: ap.rearrange("p (o c) -> p o c", o=2)
                S.op("dve", lambda e: e.tensor_tensor(out=sd[:, 0, 0, 0, :], in0=sd[:, 0, 0, 0, :], in1=hbias[:], op=ALU.add), reads=[t_sd, t_w], writes=[t_sd])
                S.op("dve", lambda e: e.tensor_tensor(out=sd[:, 0, 1, 0, :], in0=sd[:, 0, 1, 0, :], in1=hbias[:], op=ALU.add), reads=[t_sd, t_w], writes=[t_sd])
                S.op("dve", lambda e: e.tensor_tensor(out=kf[:, :, 0, 0, :], in0=v(sd[:, 0, 0, 0, :]), in1=v(sd[:, 1, 0, 0, :]), op=ALU.add), reads=[t_sd], writes=[t_kft])
                S.op("dve", lambda e: e.tensor_tensor(out=kf[:, :, 0, 1, :], in0=v(sd[:, 0, 0, 1, :]), in1=v(sd[:, 1, 0, 1, :]), op=ALU.subtract), reads=[t_sd], writes=[t_kft])
                S.op("dve", lambda e: e.tensor_tensor(out=v(xo[:, 0, :]), in0=v(sd[:, 0, 1, 0, :]), in1=v(sd[:, 1, 1, 0, :]), op=ALU.add), reads=[t_sd], writes=[t_eo])
                S.op("dve", lambda e: e.tensor_tensor(out=v(xo[:, 1, :]), in0=v(sd[:, 1, 1, 1, :]), in1=v(sd[:, 0, 1, 1, :]), op=ALU.subtract), reads=[t_sd], writes=[t_eo])
                S.op("dve", lambda e: e.tensor_scalar(out=kf[:, :, 1, 0, :], in0=v(xo[:, 0, :]), scalar1=mh[:, kc:kc + 1], scalar2=None, op0=ALU.mult),
                     reads=[t_eo, t_w], writes=[t_kft])
                S.op("dve", lambda e: e.tensor_scalar(out=kf[:, :, 1, 1, :], in0=v(xo[:, 1, :]), scalar1=mh[:, kc:kc + 1], scalar2=None, op0=ALU.mult),
                     reads=[t_eo, t_w], writes=[t_kft])
                S.dma(Kf[:, kc].rearrange("o p l r c -> p o l r c"), kf[:], reads=[t_kft], writes=[t_kf])
            S.barrier()
        with ExitStack() as _es:
            vt = _es.enter_context(SB(nc, "vt", [128, NCk, 256], BF16))
            zz1 = _es.enter_context(SB(nc, "zz1", [128, NCk, 256], BF16))
            Y = _es.enter_context(SB(nc, "Y", [128, 2, KC, 2, 256], BF16))
            fsl = _es.enter_context(SB(nc, "fsl", [128, 2, NCk, 2, 128], BF16))
            isl = _es.enter_context(SB(nc, "isl", [128, 2, KC, 2, 128], BF16))
            kft = _es.enter_context(SB(nc, "kft", [128, 2, 2, 2, 256], BF16))
            xo = _es.enter_context(SB(nc, "xo", [128, 2, 256], F32))
            xs_ = _es.enter_context(SB(nc, "xs_", [128, 2, 2, 256], F32))
            ta = _es.enter_context(SB(nc, "ta", [128, 4, 256], F32))
            yl = _es.enter_context(SB(nc, "yl", [128, 2, 2, 256], F32))
            xg = _es.enter_context(SB(nc, "xg", [128, 2, 256], BF16))
            zt = _es.enter_context(SB(nc, "zt", [128, 256], BF16))
            zT = _es.enter_context(SB(nc, "zT", [128, 2, 2, 256], BF16))
            psx = _es.enter_context(PS(nc, "psx", [128, 2, 4, 256], F32))
            psy = _es.enter_context(PS(nc, "psy", [128, 2, 512], F32))
            ptr = _es.enter_context(PS(nc, "ptr", [128, 2, 128], BF16))
            t_vt, t_zz1, t_Y, t_fsl, t_isl, t_kft2, t_ta, t_xg, t_zt, t_zT, t_psx, t_psy, t_ptr, t_xo, t_xs, t_yl = \
                Tok(), Tok(), Tok(), [Tok(), Tok()], [Tok(), Tok()], [Tok(), Tok()], Tok(), [Tok(), Tok()], Tok(), [Tok(), Tok()], [Tok(), Tok()], [Tok(), Tok()], Tok(), Tok(), Tok(), Tok()
            hsrc = lambda m: G.hyv[m, off:off + L, :].rearrange("(c p two) ch -> two p c ch", p=128, two=2)
            for par in range(2):
                S.dma(vt[:, par * NH:(par + 1) * NH, :], hsrc(0)[par], reads=[G.t_hyv], writes=[t_vt])
            mo = G.mixT[768:1024, :].rearrange("(c p) t -> p c t", p=128)
            for order in range(2):
                src, t_src = (vt, t_vt) if order == 0 else (zz1, t_zz1)
                for kc in range(KC):
                    b = kc % 2
                    S.dma(fsl[:, b], FT[kc], writes=[t_fsl[b]])
                    S.dma(kft[:, b], Kf[order, kc], reads=[t_kf], writes=[t_kft2[b]])
                    for eo_ in range(2):
                        for ri in range(2):
                            for c in range(NH):
                                cc = eo_ * NH + c
                                S.op("pe", lambda e: e.matmul(psx[:, b, eo_ * 2 + ri, :], lhsT=fsl[:, b, cc, ri, :], rhs=src[:, cc, :], start=(c == 0), stop=(c == NH - 1)),
                                     reads=[t_fsl[b], t_src], writes=[t_psx[b]])
                    S.op("act", lambda e: e.copy(out=xo[:], in_=psx[:, b, 2:4, :]), reads=[t_psx[b]], writes=[t_xo])
                    S.op("dve", lambda e: e.tensor_tensor(out=xs_[:, 0], in0=psx[:, b, 0:2, :], in1=xo[:], op=ALU.add), reads=[t_psx[b], t_xo], writes=[t_xs])
                    S.op("dve", lambda e: e.tensor_tensor(out=xs_[:, 1, 0, :], in0=psx[:, b, 0, :], in1=xo[:, 0, :], op=ALU.subtract), reads=[t_psx[b], t_xo], writes=[t_xs])
                    S.op("dve", lambda e: e.scalar_tensor_tensor(out=xs_[:, 1, 1, :], in0=psx[:, b, 1, :], scalar=-1.0, in1=xo[:, 1, :], op0=ALU.mult, op1=ALU.add),
                         reads=[t_psx[b], t_xo], writes=[t_xs])
                    S.op("dve", lambda e: e.tensor_tensor(out=ta[:, 0:2, :], in0=xs_[:, :, 0, :], in1=kft[:, b, :, 0, :], op=ALU.mult), reads=[t_xs, t_kft2[b]], writes=[t_ta])
                    S.op("pool", lambda e: e.tensor_tensor(out=ta[:, 2:4, :], in0=xs_[:, :, 1, :], in1=kft[:, b, :, 1, :], op=ALU.mult), reads=[t_xs, t_kft2[b]], writes=[t_ta])
                    S.op("dve", lambda e: e.tensor_tensor(out=yl[:, :, 0, :], in0=ta[:, 0:2, :], in1=ta[:, 2:4, :], op=ALU.subtract), reads=[t_ta], writes=[t_yl])
                    S.op("dve", lambda e: e.tensor_tensor(out=ta[:, 0:2, :], in0=xs_[:, :, 0, :], in1=kft[:, b, :, 1, :], op=ALU.mult), reads=[t_xs, t_kft2[b], t_yl], writes=[t_ta])
                    S.op("pool", lambda e: e.tensor_tensor(out=ta[:, 2:4, :], in0=xs_[:, :, 1, :], in1=kft[:, b, :, 0, :], op=ALU.mult), reads=[t_xs, t_kft2[b], t_yl], writes=[t_ta])
                    S.op("dve", lambda e: e.tensor_tensor(out=yl[:, :, 1, :], in0=ta[:, 0:2, :], in1=ta[:, 2:4, :], op=ALU.add), reads=[t_ta], writes=[t_yl])
                    S.op("dve", lambda e: e.tensor_tensor(out=Y[:, 0, kc, 0, :], in0=yl[:, 0, 0, :], in1=yl[:, 1, 0, :], op=ALU.add), reads=[t_yl], writes=[t_Y])
                    S.op("pool", lambda e: e.tensor_tensor(out=Y[:, 0, kc, 1, :], in0=yl[:, 0, 1, :], in1=yl[:, 1, 1, :], op=ALU.subtract), reads=[t_yl], writes=[t_Y])
                    S.op("dve", lambda e: e.tensor_tensor(out=Y[:, 1, kc, 0, :], in0=yl[:, 0, 0, :], in1=yl[:, 1, 0, :], op=ALU.subtract), reads=[t_yl], writes=[t_Y])
                    S.op("pool", lambda e: e.tensor_tensor(out=Y[:, 1, kc, 1, :], in0=yl[:, 0, 1, :], in1=yl[:, 1, 1, :], op=ALU.add), reads=[t_yl], writes=[t_Y])
                oi = 0
                for c2 in range(NH):
                    for par in range(2):
                        cc = par * NH + c2
                        b = oi % 2
                        oi += 1
                        zb = c2 % 2
                        S.dma(isl[:, b], IT[cc], writes=[t_isl[b]])
                        S.dma(xg[:, b], hsrc(1 + order)[par, :, c2, :], reads=[G.t_hyv], writes=[t_xg[b]])
                        for kc in range(KC):
                            for ri in range(2):
                                S.op("pe", lambda e: e.matmul(psy[:, b, 0:256], lhsT=isl[:, b, kc, ri, :], rhs=Y[:, par, kc, ri, :],
                                                              start=(kc == 0 and ri == 0), stop=(kc == KC - 1 and ri == 1)), reads=[t_isl[b], t_Y], writes=[t_psy[b]])
                        if order == 0:
                            S.op("dve", lambda e: e.tensor_tensor(out=zz1[:, cc, :], in0=psy[:, b, 0:256], in1=xg[:, b, :], op=ALU.mult),
                                 reads=[t_psy[b], t_xg[b]], writes=[t_zz1])
                        else:
                            S.op("dve", lambda e: e.tensor_tensor(out=zt[:], in0=psy[:, b, 0:256], in1=xg[:, b, :], op=ALU.mult),
                                 reads=[t_psy[b], t_xg[b]], writes=[t_zt])
                            for hh in range(2):
                                S.op("pe", lambda e: e.transpose(out=ptr[:, hh, :], in_=zt[:, hh * 128:(hh + 1) * 128], identity=G.identB),
                                     reads=[t_zt, G.t_c], writes=[t_ptr])
                            S.op("act", lambda e: e.copy(out=zT[:, zb].rearrange("p h (t two) -> p h t two", two=2)[:, :, :, par], in_=ptr[:]),
                                 reads=[t_ptr], writes=[t_zT[zb]])
                            if par == 1:
                                S.dma(mo[:, :, off + c2 * 256:off + (c2 + 1) * 256], zT[:, zb], reads=[t_zT[zb]], writes=[G.t_mix])
            S.barrier()
    if "hy" in G.dbg and li == 0:
        dump_bf16(G, G.mixT[768:896, 256:768], G.dbg["hy"], [G.t_mix])


def _hy_tables(L):
    N = 2 * L
    NCk = L // 128
    KC = (L // 2 + 1 + 127) // 128
    perm = np.concatenate([np.arange(0, L, 2), np.arange(1, L, 2)])
    n = perm.astype(np.int64)
    k = np.arange(KC * 128, dtype=np.int64)
    ang = ((n[:, None] * k[None, :]) % N).astype(np.float64) * (2 * np.pi / N)
    valid = (k <= L // 2).astype(np.float64)
    w = np.where(k == 0, 1.0, 2.0) / N * valid
    c, s_ = np.cos(ang), np.sin(ang)
    ft = np.stack([c * valid, -s_ * valid], axis=0)
    ft = ft.reshape(2, NCk, 128, KC, 128).transpose(3, 2, 1, 0, 4)
    it = np.stack([c * w, -s_ * w], axis=0)
    it = it.reshape(2, NCk, 128, KC, 128).transpose(1, 4, 3, 0, 2)
    f = np.float32
    nn = np.arange(L, dtype=f)
    t = nn / f(max(L - 1, 1))
    bands = np.linspace(1e-4, 15, 16, dtype=f)
    wpos = (f(2 * math.pi / L) * nn).astype(f)
    feats = np.concatenate([t[:, None], np.cos(wpos[:, None] * bands), -np.sin(wpos[:, None] * bands)], axis=-1).astype(f)
    deltas = np.abs(np.linspace(math.log(1e-2) / 1.5, math.log(1e-2) / 0.3, 256, dtype=f))
    win = np.exp(-t[:, None] * deltas).astype(f)
    winb = win.copy()
    winb[0] = 0.0
    feats = feats[perm]
    wn = np.stack([win, winb], axis=1)[perm].reshape(NCk, 128, 2, 256)
    mh = ((k != L // 2) & (k <= L // 2)).astype(f).reshape(KC, 128).T
    bf = ml_dtypes.bfloat16
    return (np.ascontiguousarray(ft).astype(bf), np.ascontiguousarray(it).astype(bf),
            np.ascontiguousarray(feats.T), np.ascontiguousarray(wn), np.ascontiguousarray(mh))
```

```python
import math
from contextlib import ExitStack
import numpy as np
import ml_dtypes
import concourse.bass as bass
import concourse.mybir as mybir
from concourse.bass_utils import run_bass_kernel_spmd

F32 = mybir.dt.float32
BF16 = mybir.dt.bfloat16
AF = mybir.ActivationFunctionType
ALU = mybir.AluOpType
AX = mybir.AxisListType

D = 1024
NCTX = 256
NLAT = 4096
NTOK = NCTX + NLAT
NT = NTOK // 128
DEPTH = 2
D_IN = 2444
EPS = 1e-6
HC = NTOK + 3
NE = 16
DFF = 256
DEBUG = False


def colof(tile):
    return 1 + 128 * tile if tile < 2 else 258 + 128 * (tile - 2)


BLOCKS = [(1, 0, 256, 0, 2)] + [(258 + 512 * j, 256 + 512 * j, 512, 2 + 4 * j, 4) for j in range(8)]


class Tok:
    __slots__ = ("w", "r")

    def __init__(self):
        self.w = None
        self.r = {}


class Sched:
    def __init__(self, nc, ndma=8, same_engine_sync=True):
        self.nc = nc
        self.eng = {"pe": nc.tensor, "act": nc.scalar, "dve": nc.vector, "pool": nc.gpsimd, "sp": nc.sync}
        self.semh = {}
        self.cnt = {}
        self.seen = {k: {} for k in self.eng}
        self.same = same_engine_sync
        for k in self.eng:
            self.semh[k] = nc.alloc_semaphore("s_" + k)
            self.cnt[k] = 0
        self.ndma = ndma
        self.dslot = {}
        self.dval = {}
        for q in ("sp", "pool"):
            self.dslot[q] = 0
            for i in range(ndma):
                key = ("dma", q, i)
                self.semh[key] = nc.alloc_semaphore("d_%s_%d" % (q, i))
                self.dval[key] = 0
        self.ninst = 0

    def _wait(self, e, deps):
        for (k, v) in sorted(deps, key=str):
            if k == e and (e == "pe" or not self.same):
                continue
            if self.seen[e].get(k, 0) >= v:
                continue
            self.eng[e].wait_ge(self.semh[k], v)
            self.seen[e][k] = v

    @staticmethod
    def _deps(reads, writes):
        deps = set()
        for t in reads:
            if t.w is not None:
                deps.add(t.w)
        for t in writes:
            if t.w is not None:
                deps.add(t.w)
            for kv in t.r.items():
                deps.add(kv)
        return deps

    @staticmethod
    def _mark(ev, reads, writes):
        k, v = ev
        for t in reads:
            if t.r.get(k, 0) < v:
                t.r[k] = v
        for t in writes:
            t.w = ev
            t.r = {}

    def op(self, e, fn, reads=(), writes=()):
        self._wait(e, self._deps(reads, writes))
        ins = fn(self.eng[e])
        self.cnt[e] += 1
        ins.then_inc(self.semh[e], 1)
        self._mark((e, self.cnt[e]), reads, writes)
        self.ninst += 1
        return ins

    def dma(self, out, in_, reads=(), writes=(), q="sp", **kw):
        i = self.dslot[q]
        self.dslot[q] = (i + 1) % self.ndma
        key = ("dma", q, i)
        deps = self._deps(reads, writes)
        if self.dval[key] > 0:
            deps.add((key, self.dval[key]))
        self._wait(q, deps)
        ins = self.eng[q].dma_start(out=out, in_=in_, **kw)
        self.dval[key] += 16
        ins.then_inc(self.semh[key], 16)
        self._mark((key, self.dval[key]), reads, writes)
        self.ninst += 1
        return ins

    def barrier(self):
        deps = set()
        for key, v in self.dval.items():
            if v > 0:
                deps.add((key, v))
        for k in self.eng:
            if self.cnt[k] > 0:
                deps.add((k, self.cnt[k]))
        for e in self.eng:
            self._wait(e, deps)


class Ctx:
    pass


_UID = [0]


def SB(nc, name, shape, dt):
    _UID[0] += 1
    return nc.sbuf_tensor("%s_%d" % (name, _UID[0]), shape, dt)


def PS(nc, name, shape, dt):
    _UID[0] += 1
    return nc.psum_tensor("%s_%d" % (name, _UID[0]), shape, dt)


def build(dbg=None):
    nc = bass.Bass("TRN2", target_bir_lowering=False)
    S = Sched(nc)
    G = Ctx()
    G.nc, G.S = nc, S

    def din(name, shape, dt=F32):
        return nc.dram_tensor(name, list(shape), dt, kind="ExternalInput").ap()

    def dscr(name, shape, dt):
        return nc.dram_tensor(name, list(shape), dt, kind="Internal").ap()

    I = {}
    I["x"] = din("x", [NLAT, D])
    I["ctx"] = din("ctx", [NCTX, D])
    I["w_mod"] = din("w_mod", [DEPTH, D, 6 * D])
    I["b_mod"] = din("b_mod", [DEPTH, 6 * D])
    I["w_in"] = din("w_in", [DEPTH, D, D_IN])
    I["w_out"] = din("w_out", [DEPTH, D, D])
    I["w_router"] = din("w_router", [D, NE])
    I["router_bias"] = din("router_bias", [NE])
    I["w_gate"] = din("w_gate", [DEPTH, NE, D, DFF])
    I["w_up"] = din("w_up", [DEPTH, NE, D, DFF])
    I["w_down"] = din("w_down", [DEPTH, NE, DFF, D])
    I["g_final"] = din("g_final", [D])
    I["cols"] = din("cols", [DEPTH, 128, NCOLS])
    I["cmat"] = din("cmat", [9, 128, 128])
    I["rope"] = din("rope", [2, 128, NLAT])
    for sname, P in SEGS.items():
        I["ft_" + sname] = din("ft_" + sname, [P["KC"], 128, P["NC"], 2, 128], BF16)
        I["it_" + sname] = din("it_" + sname, [P["NC"], 128, P["KC"], 2, 128], BF16)
        I["fe_" + sname] = din("fe_" + sname, [33, P["L"]])
        I["win_" + sname] = din("win_" + sname, [P["NC"], 128, 2, 256])
        I["mh_" + sname] = din("mh_" + sname, [128, P["KC"]])
    for nm, shp in (("hy_conv_w", [DEPTH, 3, 768]), ("hy_conv_b", [DEPTH, 768]), ("hy_w1", [DEPTH, 33, 64]), ("hy_w2", [DEPTH, 64, 64]),
                    ("hy_w3", [DEPTH, 64, 1024]), ("hy_bias", [DEPTH, 2, 256])):
        I[nm] = din(nm, shp)
    I["ssdmask"] = din("ssdmask", [2, 4, 128, 512], BF16)
    for nm, shp in (("ssd_conv_w", [DEPTH, 3, 640]), ("ssd_dt_bias", [DEPTH, 2, 6]), ("ssd_a_log", [DEPTH, 2, 6]),
                    ("ssd_d", [DEPTH, 6]), ("ssd_norm", [DEPTH, 384])):
        I[nm] = din(nm, shp)
    out = nc.dram_tensor("out", [NLAT, D], F32, kind="ExternalOutput").ap()
    G.I, G.out = I, out
    G.dbg = {}
    if dbg:
        for name, shape in dbg.items():
            G.dbg[name] = nc.dram_tensor("dbg_" + name, list(shape), F32, kind="ExternalOutput").ap()

    G.xres = dscr("xres", [NTOK, D], F32)
    G.t_xres = [Tok() for _ in range(NT)]
    G.wb_in = [dscr("wb_in%d" % i, [D, D_IN], BF16) for i in range(DEPTH)]
    G.wb_out = [dscr("wb_out%d" % i, [D, D], BF16) for i in range(DEPTH)]
    G.wb_gate = [dscr("wb_gate%d" % i, [NE, D, DFF], BF16) for i in range(DEPTH)]
    G.wb_up = [dscr("wb_up%d" % i, [NE, D, DFF], BF16) for i in range(DEPTH)]
    G.wb_down = [dscr("wb_down%d" % i, [NE, DFF, D], BF16) for i in range(DEPTH)]
    G.t_wb = Tok()
    G.mixT = dscr("mixT", [D, NTOK], BF16)
    G.hyv = dscr("hyv", [3, NTOK, 256], BF16)
    G.ssd_yb = dscr("ssd_yb", [NTOK, 384], F32)
    G.t_ssdyb = [Tok() for _ in range(NT)]
    G.t_hyv = Tok()
    G.Kf = {sn: dscr("Kf_" + sn, [2, P["KC"], 128, 2, 2, 256], BF16) for sn, P in SEGS.items()}
    G.t_mix = Tok()

    cm = nc.alloc_sbuf_tensor("cm", [128, 9, 128], F32)
    cmb = nc.alloc_sbuf_tensor("cmb", [128, 9, 128], BF16)
    ones = nc.alloc_sbuf_tensor("ones", [128, 128], F32)
    epsc = nc.alloc_sbuf_tensor("epsc", [128, 1], F32)
    G.t_c = Tok()
    S.dma(cm[:], I["cmat"].rearrange("a p c -> p a c"), writes=[G.t_c])
    S.op("dve", lambda e: e.tensor_copy(out=cmb[:], in_=cm[:]), reads=[G.t_c], writes=[G.t_c])
    S.op("dve", lambda e: e.memset(ones[:], 1.0), writes=[G.t_c])
    S.op("dve", lambda e: e.memset(epsc[:], EPS), writes=[G.t_c])
    G.cm, G.cmb, G.ones, G.epsc = cm, cmb, ones, epsc
    G.negpi = nc.alloc_sbuf_tensor("negpi", [128, 1], F32)
    S.op("dve", lambda e: e.memset(G.negpi[:], -math.pi), writes=[G.t_c])
    G.identF, G.identB = cm[:, 0, :], cmb[:, 0, :]

    S.dma(G.xres[0:NCTX, :], I["ctx"][:, :], writes=G.t_xres[0:2])
    for j in range(4):
        S.dma(G.xres[NCTX + 1024 * j:NCTX + 1024 * (j + 1), :], I["x"][1024 * j:1024 * (j + 1), :],
              writes=G.t_xres[2 + 8 * j:2 + 8 * (j + 1)])

    convert_weights(G)
    S.barrier()
    for li in range(1 if DEBUG else DEPTH):
        layer(G, li)
    S.barrier()
    return nc


def convert_weights(G):
    nc, S, I = G.nc, G.S, G.I
    CH = 4096
    with ExitStack() as _es:
        cf = _es.enter_context(SB(nc, "cv_f", [128, 2, CH], F32))
        cb = _es.enter_context(SB(nc, "cv_b", [128, 2, CH], BF16))
        tf = [Tok(), Tok()]
        tb = [Tok(), Tok()]
        n = 0
        engs = ["dve", "pool", "act"]
        for li in range(DEPTH):
            pairs = [(I["w_in"][li], G.wb_in[li], "a b -> (a b)"), (I["w_out"][li], G.wb_out[li], "a b -> (a b)"),
                     (I["w_gate"][li], G.wb_gate[li], "e a b -> (e a b)"), (I["w_up"][li], G.wb_up[li], "e a b -> (e a b)"),
                     (I["w_down"][li], G.wb_down[li], "e a b -> (e a b)")]
            for src, dst, pat in pairs:
                s1 = src.rearrange(pat).rearrange("(p m) -> p m", p=128)
                d1 = dst.rearrange(pat).rearrange("(p m) -> p m", p=128)
                M = s1.shape[1]
                for c0 in range(0, M, CH):
                    w = min(CH, M - c0)
                    k = n % 2
                    S.dma(cf[:, k, 0:w], s1[:, c0:c0 + w], writes=[tf[k]])
                    en = engs[n % 3]
                    if en == "act":
                        S.op("act", lambda e: e.copy(out=cb[:, k, 0:w], in_=cf[:, k, 0:w]), reads=[tf[k]], writes=[tb[k]])
                    else:
                        S.op(en, lambda e: e.tensor_copy(out=cb[:, k, 0:w], in_=cf[:, k, 0:w]), reads=[tf[k]], writes=[tb[k]])
                    S.dma(d1[:, c0:c0 + w], cb[:, k, 0:w], reads=[tb[k]], writes=[G.t_wb], q="pool")
                    n += 1


COLS = {}
_o = 0
for _name, _n in [("cc", 16), ("bmod", 32), ("g_mix", 8), ("g_ffn", 8), ("ssd_conv_b", 5), ("qg", 1), ("kg", 1),
                  ("ssd_d", 3), ("ssd_norm", 3), ("hy_b1", 1), ("hy_freq", 1), ("hy_b2", 1)]:
    COLS[_name] = (_o, _n)
    _o += _n
NCOLS = _o


def layer(G, li):
    nc, S, I = G.nc, G.S, G.I
    with ExitStack() as _es:
        cols = _es.enter_context(SB(nc, "cols", [128, NCOLS], F32))
        modc = _es.enter_context(SB(nc, "modc", [128, 4, 8, 2], F32))
        gtb = _es.enter_context(SB(nc, "gtb", [128, 2, 2, D], F32))
        G.cols, G.modc, G.gtb = cols, modc, gtb
        G.t_cols, G.t_modc, G.t_gtb = Tok(), Tok(), Tok()
        S.dma(cols[:], I["cols"][li], writes=[G.t_cols])
        adaln(G, li)
        S.barrier()
        with ExitStack() as _es:
            hT = _es.enter_context(SB(nc, "hT", [128, 8, HC], BF16))
            G.hT, G.t_hT = hT, Tok()
            norm_in(G, li)
            S.barrier()
            if "hy" in STAGES:
                hyena_inproj(G, li)
                S.barrier()
            if "att" in STAGES:
                attention(G, li)
                S.barrier()
            if "ssd" in STAGES:
                ssd(G, li)
                S.barrier()
        if "hy" in STAGES:
            hyena_fft(G, li)
            S.barrier()
        if "moe" in STAGES:
            with ExitStack() as _es:
                h2T = _es.enter_context(SB(nc, "h2T", [128, 8, NTOK], BF16))
                rl = _es.enter_context(SB(nc, "rl", [128, NT, NE], F32))
                G.h2T, G.t_h2T, G.rl, G.t_rl = h2T, Tok(), rl, Tok()
                outproj(G, li)
                S.barrier()
                moe(G, li)
                S.barrier()


STAGES = ("att", "ssd", "hy", "moe")


def colap(G, name, j=0, n=1, p0=0, p1=128):
    o, _ = COLS[name]
    return G.cols[p0:p1, o + j:o + j + n]


def adaln(G, li):
    nc, S, I = G.nc, G.S, G.I
    cols, modc, gtb = G.cols, G.modc, G.gtb
    with ExitStack() as _es:
        sc = _es.enter_context(SB(nc, "sc", [128, 8, 2], F32))
        screp = _es.enter_context(SB(nc, "screp", [128, 8, 2, 128], F32))
        wm = _es.enter_context(SB(nc, "wm", [128, 2, 8, 512], F32))
        brow = _es.enter_context(SB(nc, "brow", [128, 2, D], F32))
        ps_a = _es.enter_context(PS(nc, "ps_a", [128, 4, 2], F32))
        ps_g = _es.enter_context(PS(nc, "ps_g", [128, 2, 512], F32))
        t_sc, t_wm, t_pa, t_pg, t_brow = Tok(), [Tok(), Tok()], Tok(), Tok(), Tok()
        o = COLS["cc"][0]
        S.op("act", lambda e: e.activation(out=sc[:].rearrange("p k j -> p (k j)"), in_=cols[:, o:o + 16], func=AF.Silu),
             reads=[G.t_cols], writes=[t_sc])
        S.op("dve", lambda e: e.tensor_copy(out=screp[:].rearrange("p k j c -> p (k j) c"),
                                            in_=sc[:].rearrange("p k j -> p (k j)").unsqueeze(2).to_broadcast([128, 16, 128])),
             reads=[t_sc], writes=[t_sc])
        for g in range(2):
            S.dma(brow[:, g, :], I["b_mod"][li, (2 + 3 * g) * D:(3 + 3 * g) * D].partition_broadcast(128), writes=[t_brow])
        wv = I["w_mod"][li].rearrange("(k p) c -> p k c", p=128)
        ob = COLS["bmod"][0]
        for cj in range(12):
            b = cj % 2
            S.dma(wm[:, b], wv[:, :, cj * 512:(cj + 1) * 512], writes=[t_wm[b]])
            vec = cj // 2
            half = cj % 2
            if vec in (2, 5):
                g = 0 if vec == 2 else 1
                for j in range(2):
                    for kd in range(8):
                        S.op("pe", lambda e: e.matmul(ps_g[:, j, :], lhsT=screp[:, kd, j, :], rhs=wm[:, b, kd, :],
                                                      start=(kd == 0), stop=(kd == 7)), reads=[t_sc, t_wm[b]], writes=[t_pg])
                    S.op("dve", lambda e: e.tensor_tensor(out=gtb[:, g, j, half * 512:(half + 1) * 512], in0=ps_g[:, j, :],
                                                          in1=brow[:, g, half * 512:(half + 1) * 512], op=ALU.add),
                         reads=[t_pg, t_brow], writes=[G.t_gtb])
            else:
                v = {0: 0, 1: 1, 3: 2, 4: 3}[vec]
                for fc in range(4):
                    for kd in range(8):
                        S.op("pe", lambda e: e.matmul(ps_a[:, fc, :], lhsT=wm[:, b, kd, fc * 128:(fc + 1) * 128], rhs=sc[:, kd, :],
                                                      start=(kd == 0), stop=(kd == 7)), reads=[t_sc, t_wm[b]], writes=[t_pa])
                k0 = half * 4
                S.op("dve", lambda e: e.tensor_tensor(out=modc[:, v, k0:k0 + 4, :], in0=ps_a[:],
                                                      in1=cols[:, ob + v * 8 + k0:ob + v * 8 + k0 + 4].unsqueeze(2).to_broadcast([128, 4, 2]),
                                                      op=ALU.add), reads=[t_pa, G.t_cols], writes=[G.t_modc])
        for v, gname in ((1, "g_mix"), (3, "g_ffn")):
            og = COLS[gname][0]
            S.op("dve", lambda e: e.scalar_tensor_tensor(out=modc[:, v], in0=modc[:, v], scalar=1.0,
                                                         in1=cols[:, og:og + 8].unsqueeze(2).to_broadcast([128, 8, 2]),
                                                         op0=ALU.add, op1=ALU.mult), reads=[G.t_modc, G.t_cols], writes=[G.t_modc])


def rms_to_T(G, xt, t_x, tile, vA, vB, dstT, t_dst, dcol, pool):
    nc, S = G.nc, G.S
    sq, ss, xn, ps_t, toks = pool
    t_sq, t_ss, t_xn, t_ps = toks
    j = 1 if tile < 2 else 0
    S.op("act", lambda e: e.activation(out=sq[:], in_=xt, func=AF.Square, accum_out=ss[:, 0:1]), reads=[t_x], writes=[t_sq, t_ss])
    S.op("act", lambda e: e.activation(out=ss[:, 1:2], in_=ss[:, 0:1], func=AF.Sqrt, bias=G.epsc[:], scale=1.0 / D),
         reads=[t_ss, G.t_c], writes=[t_ss])
    S.op("dve", lambda e: e.reciprocal(out=ss[:, 2:3], in_=ss[:, 1:2]), reads=[t_ss], writes=[t_ss])
    S.op("dve", lambda e: e.tensor_scalar(out=xn[:], in0=xt, scalar1=ss[:, 2:3], scalar2=None, op0=ALU.mult),
         reads=[t_x, t_ss], writes=[t_xn])
    for k in range(8):
        S.op("pe", lambda e: e.transpose(out=ps_t[:, k, :], in_=xn[:, k * 128:(k + 1) * 128], identity=G.identB),
             reads=[t_xn, G.t_c], writes=[t_ps])
    S.op("dve", lambda e: e.tensor_tensor(out=sq[:].rearrange("p (k c) -> p k c", k=8), in0=ps_t[:],
                                          in1=G.modc[:, vA, :, j:j + 1].to_broadcast([128, 8, 128]), op=ALU.mult),
         reads=[t_ps, G.t_modc], writes=[t_sq])
    S.op("dve", lambda e: e.tensor_tensor(out=dstT[:, :, dcol:dcol + 128], in0=sq[:].rearrange("p (k c) -> p k c", k=8),
                                          in1=G.modc[:, vB, :, j:j + 1].to_broadcast([128, 8, 128]), op=ALU.add),
         reads=[t_sq, G.t_modc], writes=[t_dst])


def norm_in(G, li):
    nc, S = G.nc, G.S
    hT = G.hT
    with ExitStack() as _es:
        xt = _es.enter_context(SB(nc, "xt", [128, 2, D], F32))
        sq = _es.enter_context(SB(nc, "sq", [128, 2, D], F32))
        ss = _es.enter_context(SB(nc, "ss", [128, 2, 4], F32))
        xn = _es.enter_context(SB(nc, "xn", [128, 2, D], BF16))
        ps_t = _es.enter_context(PS(nc, "ps_t", [128, 2, 8, 128], BF16))
        t_x = [Tok(), Tok()]
        pools = [(sq[:, i, :], ss[:, i, :], xn[:, i, :], ps_t[:, i], (Tok(), Tok(), Tok(), Tok())) for i in range(2)]
        for c in (0, 257, HC - 1):
            S.op("pool", lambda e: e.memset(hT[:, :, c:c + 1], 0.0), writes=[G.t_hT])
        for tile in range(NT):
            b = tile % 2
            S.dma(xt[:, b, :], G.xres[tile * 128:(tile + 1) * 128, :], reads=[G.t_xres[tile]], writes=[t_x[b]])
            rms_to_T(G, xt[:, b, :], t_x[b], tile, 1, 0, hT, G.t_hT, colof(tile), pools[b])
        if "hT" in G.dbg and li == 0:
            with ExitStack() as _es:
                dh = _es.enter_context(SB(nc, "dbgh", [128, 8, 512], F32))
                t = Tok()
                S.op("dve", lambda e: e.tensor_copy(out=dh[:], in_=hT[:, :, 0:512]), reads=[G.t_hT], writes=[t])
                S.dma(G.dbg["hT"].rearrange("(k p) c -> p k c", p=128), dh[:], reads=[t])


def attention(G, li):
    nc, S, I = G.nc, G.S, G.I
    hT = G.hT
    need_ctx = li < DEPTH - 1
    wv = G.wb_in[li].rearrange("(k p) c -> p k c", p=128)
    scale = 64 ** -0.5
    with ExitStack() as _es:
        wq = _es.enter_context(SB(nc, "wqkv", [128, 8, 640], BF16))
        qT = _es.enter_context(SB(nc, "qT", [128, 6, NTOK], BF16))
        kT = _es.enter_context(SB(nc, "kT", [128, NTOK], BF16))
        vp = _es.enter_context(SB(nc, "vp", [128, NT, 2, 128], BF16))
        rp = _es.enter_context(SB(nc, "rp", [128, 2, 2, 512], F32))
        qs = _es.enter_context(SB(nc, "qs", [128, 512], F32))
        q2 = _es.enter_context(SB(nc, "q2", [128, 512], F32))
        qn = _es.enter_context(SB(nc, "qn", [128, 512], F32))
        qnb = _es.enter_context(SB(nc, "qnb", [128, 512], BF16))
        pT = _es.enter_context(SB(nc, "pT", [128, 2, 2, 512], BF16))
        rd = _es.enter_context(SB(nc, "rd", [128, 2, 512], F32))
        ao = _es.enter_context(SB(nc, "ao", [128, 2, 512], BF16))
        ps_q = _es.enter_context(PS(nc, "ps_q", [128, 512], F32))
        ps_r = _es.enter_context(PS(nc, "ps_r", [128, 512], F32))
        ps_s = _es.enter_context(PS(nc, "ps_s", [128, 2, 2, 512], F32))
        ps_o = _es.enter_context(PS(nc, "ps_o", [128, 2, 512], F32))
        t_w, t_q, t_k, t_v, t_rp = Tok(), Tok(), Tok(), Tok(), [Tok(), Tok()]
        t_qs, t_q2, t_qn, t_qnb, t_psq, t_psr = Tok(), Tok(), Tok(), Tok(), Tok(), Tok()
        t_pT, t_pss, t_pso, t_rd, t_ao = [Tok(), Tok()], [Tok(), Tok()], [Tok(), Tok()], [Tok(), Tok()], [Tok(), Tok()]
        for j in range(3):
            S.dma(wq[:, :, j * 128:j * 128 + 64], wv[:, :, j * 64:(j + 1) * 64], reads=[G.t_wb], writes=[t_w])
            S.dma(wq[:, :, j * 128 + 64:(j + 1) * 128], wv[:, :, (3 + j) * 64:(4 + j) * 64], reads=[G.t_wb], writes=[t_w])
        S.dma(wq[:, :, 384:640], wv[:, :, 384:640], reads=[G.t_wb], writes=[t_w])
        S.op("pool", lambda e: e.memset(vp[:, :, :, 64:128], 1.0), writes=[t_v])
        S.op("pool", lambda e: e.memset(qT[64:128, 0:3, :], 0.0), writes=[t_q])
        S.op("pool", lambda e: e.memset(qT[0:64, 3:6, :], 0.0), writes=[t_q])
        og = {0: COLS["qg"][0], 1: COLS["qg"][0], 2: COLS["qg"][0], 3: COLS["kg"][0]}
        for bi, (c0, t0, n, tile0, ntile) in enumerate(BLOCKS):
            if bi > 0:
                b = bi % 2
                S.dma(rp[:, b, :, :], I["rope"][:, :, t0 - NCTX:t0 - NCTX + 512].rearrange("a p c -> p a c"), writes=[t_rp[b]])
            for ch in range(4):
                for k in range(8):
                    S.op("pe", lambda e: e.matmul(ps_q[:, 0:n], lhsT=wq[:, k, ch * 128:(ch + 1) * 128], rhs=hT[:, k, c0:c0 + n],
                                                  start=(k == 0), stop=(k == 7)), reads=[t_w, G.t_hT], writes=[t_psq])
                S.op("act", lambda e: e.copy(out=qs[:, 0:n], in_=ps_q[:, 0:n]), reads=[t_psq], writes=[t_qs])
                S.op("act", lambda e: e.activation(out=q2[:, 0:n], in_=qs[:, 0:n], func=AF.Square), reads=[t_qs], writes=[t_q2])
                S.op("pe", lambda e: e.matmul(ps_r[:, 0:n], lhsT=G.cm[:, 3, :], rhs=q2[:, 0:n], start=True, stop=True),
                     reads=[t_q2, G.t_c], writes=[t_psr])
                S.op("act", lambda e: e.activation(out=q2[:, 0:n], in_=ps_r[:, 0:n], func=AF.Sqrt, bias=G.epsc[:], scale=1.0 / 64),
                     reads=[t_psr, G.t_c], writes=[t_q2])
                S.op("dve", lambda e: e.reciprocal(out=q2[:, 0:n], in_=q2[:, 0:n]), reads=[t_q2], writes=[t_q2])
                S.op("dve", lambda e: e.scalar_tensor_tensor(out=qn[:, 0:n], in0=qs[:, 0:n], scalar=G.cols[:, og[ch]:og[ch] + 1],
                                                             in1=q2[:, 0:n], op0=ALU.mult, op1=ALU.mult),
                     reads=[t_qs, t_q2, G.t_cols], writes=[t_qn])
                t_dst = t_q if ch < 3 else t_k
                halves = [(0, 64, qT[0:64, ch, t0:t0 + n]), (64, 128, qT[64:128, 3 + ch, t0:t0 + n])] if ch < 3 else [(0, 128, kT[:, t0:t0 + n])]
                if bi == 0:
                    for (p0, p1, dst) in halves:
                        S.op("dve", lambda e: e.tensor_copy(out=dst, in_=qn[p0:p1, 0:n]), reads=[t_qn], writes=[t_dst])
                else:
                    b = bi % 2
                    S.op("dve", lambda e: e.tensor_copy(out=qnb[:, 0:n], in_=qn[:, 0:n]), reads=[t_qn], writes=[t_qnb])
                    S.op("pe", lambda e: e.matmul(ps_r[:, 0:n], lhsT=G.cmb[:, 4, :], rhs=qnb[:, 0:n], start=True, stop=True),
                         reads=[t_qnb, G.t_c], writes=[t_psr])
                    S.op("dve", lambda e: e.tensor_tensor(out=qs[:, 0:n], in0=ps_r[:, 0:n], in1=rp[:, b, 1, 0:n], op=ALU.mult),
                         reads=[t_psr, t_rp[b]], writes=[t_qs])
                    S.op("dve", lambda e: e.tensor_tensor(out=qn[:, 0:n], in0=qn[:, 0:n], in1=rp[:, b, 0, 0:n], op=ALU.mult),
                         reads=[t_qn, t_rp[b]], writes=[t_qn])
                    for (p0, p1, dst) in halves:
                        S.op("dve", lambda e: e.tensor_tensor(out=dst, in0=qn[p0:p1, 0:n], in1=qs[p0:p1, 0:n], op=ALU.add),
                             reads=[t_qn, t_qs], writes=[t_dst])
            for tl in range(tile0, tile0 + ntile):
                cc = colof(tl)
                for k in range(8):
                    S.op("pe", lambda e: e.matmul(ps_q[:, 0:128], lhsT=hT[:, k, cc:cc + 128], rhs=wq[:, k, 512:640],
                                                  start=(k == 0), stop=(k == 7)), reads=[t_w, G.t_hT], writes=[t_psq])
                S.op("act", lambda e: e.copy(out=vp[:, tl, :, 0:64], in_=ps_q[:, 0:128].rearrange("p (a d) -> p a d", a=2)),
                     reads=[t_psq], writes=[t_v])
        pairs = []
        oi = 0
        for h in range(6):
            for bi, (c0, t0, n, tile0, ntile) in enumerate(BLOCKS):
                if bi == 0 and not need_ctx:
                    continue
                kcs = list(range(2)) if bi == 0 else list(range(NT))
                for ki in range(0, len(kcs), 2):
                    pairs.append((h, t0, n, kcs[ki], ki == 0, ki + 2 >= len(kcs), oi % 2))
                oi += 1

        def qk(j):
            h, t0, n, kc, first, last, ob = pairs[j]
            pb = (h // 3) * 64
            sb = j % 2
            for u in range(2):
                S.op("pe", lambda e: e.matmul(ps_s[:, sb, u, 0:n], lhsT=kT[:, (kc + u) * 128:(kc + u + 1) * 128],
                                              rhs=qT[:, h, t0:t0 + n], start=True, stop=True),
                     reads=[t_q, t_k], writes=[t_pss[sb]])

        qk(0)
        for j, (h, t0, n, kc, first, last, ob) in enumerate(pairs):
            sb = j % 2
            kv = h // 3
            if j + 1 < len(pairs):
                qk(j + 1)
            S.op("act", lambda e: e.activation(out=pT[:, sb, :, 0:n], in_=ps_s[:, sb, :, 0:n], func=AF.Exp, scale=scale),
                 reads=[t_pss[sb]], writes=[t_pT[sb]])
            for u in range(2):
                S.op("pe", lambda e: e.matmul(ps_o[:, ob, 0:n], lhsT=vp[:, kc + u, kv, :], rhs=pT[:, sb, u, 0:n],
                                              start=(first and u == 0), stop=(last and u == 1)),
                     reads=[t_v, t_pT[sb]], writes=[t_pso[ob]])
            if last:
                S.op("dve", lambda e: e.reciprocal(out=rd[0:64, ob, 0:n], in_=ps_o[64:128, ob, 0:n]), reads=[t_pso[ob]], writes=[t_rd[ob]])
                S.op("dve", lambda e: e.tensor_tensor(out=ao[0:64, ob, 0:n], in0=ps_o[0:64, ob, 0:n], in1=rd[0:64, ob, 0:n], op=ALU.mult),
                     reads=[t_pso[ob], t_rd[ob]], writes=[t_ao[ob]])
                S.dma(G.mixT[h * 64:(h + 1) * 64, t0:t0 + n], ao[0:64, ob, 0:n], reads=[t_ao[ob]], writes=[G.t_mix])
        if "att" in G.dbg and li == 0:
            dump_bf16(G, G.mixT[0:128, 256:768], G.dbg["att"], [G.t_mix])


def dump_bf16(G, src, dst, reads):
    nc, S = G.nc, G.S
    p, n = src.shape
    with ExitStack() as _es:
        a = _es.enter_context(SB(nc, "dmpb", [p, n], BF16))
        b = _es.enter_context(SB(nc, "dmpf", [p, n], F32))
        t = Tok()
        S.dma(a[:], src, reads=reads, writes=[t])
        S.op("dve", lambda e: e.tensor_copy(out=b[:], in_=a[:]), reads=[t], writes=[t])
        S.dma(dst, b[:], reads=[t])
        S.barrier()


def _cols_pack(inp, li, b):
    def colform(v, n):
        return np.ascontiguousarray(v.reshape(n, 128).T)
    parts = {}
    cc = np.zeros((128, 8, 2), np.float32)
    cc[:, :, 0] = colform(inp["c"][b], 8)
    cc[:, :, 1] = colform(inp["c_ctx"], 8)
    parts["cc"] = cc.reshape(128, 16)
    bm = inp["b_mod"][li].reshape(6, 8, 128)
    parts["bmod"] = np.concatenate([bm[v].T for v in (0, 1, 3, 4)], axis=1)
    parts["g_mix"] = colform(inp["g_mix"][li], 8)
    parts["g_ffn"] = colform(inp["g_ffn"][li], 8)
    parts["ssd_conv_b"] = colform(inp["ssd_conv_b"][li], 5)
    parts["qg"] = np.tile(inp["q_norm"][li], 2)[:, None]
    parts["kg"] = np.tile(inp["k_norm"][li], 2)[:, None]
    parts["ssd_d"] = colform(np.repeat(inp["ssd_d"][li], 64), 3)
    parts["ssd_norm"] = colform(inp["ssd_norm"][li], 3)
    for nm in ("hy_b1", "hy_freq", "hy_b2"):
        parts[nm] = np.tile(inp[nm][li], 2)[:, None]
    out = np.zeros((128, NCOLS), np.float32)
    for nm, (o, n) in COLS.items():
        out[:, o:o + n] = parts[nm]
    return out


def _consts():
    ident = np.eye(128, dtype=np.float32)
    s = np.arange(128)
    U = (s[:, None] <= s[None, :]).astype(np.float32)
    Lo = (s[:, None] >= s[None, :]).astype(np.float32)
    bo = np.kron(np.eye(2, dtype=np.float32), np.ones((64, 64), np.float32))
    rot = np.zeros((128, 128), np.float32)
    for hb in (0, 64):
        for d in range(32):
            rot[hb + d + 32, hb + d] = -1.0
            rot[hb + d, hb + d + 32] = 1.0
    top = np.zeros((128, 128), np.float32); top[:64] = 1.0
    bot = np.zeros((128, 128), np.float32); bot[64:] = 1.0
    cmat = np.stack([ident, U, Lo, bo, rot, top, bot, U - ident, Lo - ident])
    rows = NLAT // 64
    row = np.repeat(np.arange(rows), 64).astype(np.float32)
    col = np.tile(np.arange(64), rows).astype(np.float32)
    inv = (10000.0 ** (-np.arange(0, 32, 2, dtype=np.float32) / 32)).astype(np.float32)
    ang = np.concatenate([row[:, None] * inv, col[:, None] * inv], axis=-1).astype(np.float32)
    cs = np.cos(ang).astype(np.float32).T
    sn = np.sin(ang).astype(np.float32).T
    rope = np.stack([np.tile(cs, (4, 1)), np.tile(sn, (4, 1))]).astype(np.float32)
    tt = np.arange(512)[None, None, :]
    ss_ = np.arange(128)[None, :, None]
    jj = np.arange(4)[:, None, None]
    mf = (tt >= 128 * jj + ss_).astype(np.float32)
    mb = (tt <= 128 * jj + ss_).astype(np.float32)
    hyt = {}
    for sname, P in SEGS.items():
        ft, it_, fe, wn, mh = _hy_tables(P["L"])
        hyt["ft_" + sname], hyt["it_" + sname], hyt["fe_" + sname], hyt["win_" + sname], hyt["mh_" + sname] = ft, it_, fe, wn, mh
    return {**hyt, "cmat": cmat, "rope": rope, "ssdmask": np.stack([mf, mb]).astype(ml_dtypes.bfloat16)}


_CONSTS = None


def kernel(**inp):
    global _CONSTS
    inp = {k: np.asarray(v) for k, v in inp.items()}
    if _CONSTS is None:
        _CONSTS = _consts()
    dbg = kernel.dbg if hasattr(kernel, "dbg") else None
    nc = build(dbg)
    ncores = 8
    in_maps = []
    for core in range(ncores):
        b = core % 4
        m = {"x": np.ascontiguousarray(inp["x"][b]), "ctx": np.ascontiguousarray(inp["ctx"][b])}
        for k in ("w_mod", "b_mod", "w_in", "w_out", "w_router", "router_bias", "w_gate", "w_up", "w_down", "g_final",
                  "ssd_conv_w", "ssd_dt_bias", "ssd_a_log", "ssd_d", "ssd_norm", "hy_conv_w", "hy_conv_b", "hy_w1", "hy_w2", "hy_w3", "hy_bias"):
            m[k] = inp[k]
        m["cols"] = np.stack([_cols_pack(inp, li, b) for li in range(DEPTH)])
        m.update(_CONSTS)
        in_maps.append(m)
    res = run_bass_kernel_spmd(nc, in_maps, core_ids=list(range(ncores)))
    kernel.last = res
    return np.stack([res.results[b]["out"] for b in range(4)]).astype(np.float32)


def outproj(G, li):
    nc, S, I = G.nc, G.S, G.I
    need_ctx = li < DEPTH - 1
    with ExitStack() as _es:
        wo = _es.enter_context(SB(nc, "wo", [128, 8, D], BF16))
        mx = _es.enter_context(SB(nc, "mx", [128, 2, 8, 128], BF16))
        xt = _es.enter_context(SB(nc, "xt", [128, 2, D], F32))
        tmp = _es.enter_context(SB(nc, "tmp", [128, D], F32))
        sq = _es.enter_context(SB(nc, "sq", [128, D], F32))
        ss = _es.enter_context(SB(nc, "ss", [128, 4], F32))
        xn = _es.enter_context(SB(nc, "xn", [128, D], F32))
        h2f = _es.enter_context(SB(nc, "h2f", [128, 8, 128], F32))
        wr = _es.enter_context(SB(nc, "wr", [128, 8, NE], F32))
        po = _es.enter_context(PS(nc, "po", [128, 2, 512], F32))
        pt = _es.enter_context(PS(nc, "pt", [128, 8, 128], F32))
        pr = _es.enter_context(PS(nc, "pr", [128, NE], F32))
        t_wo, t_mx, t_x, t_tmp, t_po = Tok(), [Tok(), Tok()], [Tok(), Tok()], Tok(), Tok()
        t_sq, t_ss, t_xn, t_pt, t_h2f, t_wr, t_pr = Tok(), Tok(), Tok(), Tok(), Tok(), Tok(), Tok()
        S.dma(wo[:], G.wb_out[li].rearrange("(k p) c -> p k c", p=128), reads=[G.t_wb], writes=[t_wo])
        S.dma(wr[:], I["w_router"].rearrange("(k p) c -> p k c", p=128), writes=[t_wr])
        mv = G.mixT.rearrange("(k p) t -> p k t", p=128)
        tiles = [t for t in range(NT) if need_ctx or t >= 2]

        def mm(tile):
            b = tile % 2
            S.dma(mx[:, b], mv[:, :, tile * 128:(tile + 1) * 128], reads=[G.t_mix], writes=[t_mx[b]])
            S.dma(xt[:, b, :], G.xres[tile * 128:(tile + 1) * 128, :], reads=[G.t_xres[tile]], writes=[t_x[b]])
            for half in range(2):
                for k in range(8):
                    S.op("pe", lambda e: e.matmul(po[:, half, :], lhsT=mx[:, b, k, :], rhs=wo[:, k, half * 512:(half + 1) * 512],
                                                  start=(k == 0), stop=(k == 7)), reads=[t_mx[b], t_wo], writes=[t_po])

        mm(tiles[0])
        for ti, tile in enumerate(tiles):
            b = tile % 2
            j = 1 if tile < 2 else 0
            S.op("dve", lambda e: e.tensor_tensor(out=tmp[:], in0=po[:].rearrange("p a c -> p (a c)"), in1=G.gtb[:, 0, j, :], op=ALU.mult),
                 reads=[t_po, G.t_gtb], writes=[t_tmp])
            if ti + 1 < len(tiles):
                mm(tiles[ti + 1])
            S.op("dve", lambda e: e.tensor_tensor(out=xt[:, b, :], in0=tmp[:], in1=xt[:, b, :], op=ALU.add),
                 reads=[t_tmp, t_x[b]], writes=[t_x[b]])
            S.dma(G.xres[tile * 128:(tile + 1) * 128, :], xt[:, b, :], reads=[t_x[b]], writes=[G.t_xres[tile]])
            xv = xt[:, b, :]
            S.op("act", lambda e: e.activation(out=sq[:], in_=xv, func=AF.Square, accum_out=ss[:, 0:1]), reads=[t_x[b]], writes=[t_sq, t_ss])
            S.op("act", lambda e: e.activation(out=ss[:, 1:2], in_=ss[:, 0:1], func=AF.Sqrt, bias=G.epsc[:], scale=1.0 / D),
                 reads=[t_ss, G.t_c], writes=[t_ss])
            S.op("dve", lambda e: e.reciprocal(out=ss[:, 2:3], in_=ss[:, 1:2]), reads=[t_ss], writes=[t_ss])
            S.op("dve", lambda e: e.tensor_scalar(out=xn[:], in0=xv, scalar1=ss[:, 2:3], scalar2=None, op0=ALU.mult),
                 reads=[t_x[b], t_ss], writes=[t_xn])
            for k in range(8):
                S.op("pe", lambda e: e.transpose(out=pt[:, k, :], in_=xn[:, k * 128:(k + 1) * 128], identity=G.identF),
                     reads=[t_xn, G.t_c], writes=[t_pt])
            S.op("dve", lambda e: e.tensor_tensor(out=h2f[:], in0=pt[:], in1=G.modc[:, 3, :, j:j + 1].to_broadcast([128, 8, 128]), op=ALU.mult),
                 reads=[t_pt, G.t_modc], writes=[t_h2f])
            S.op("dve", lambda e: e.tensor_tensor(out=h2f[:], in0=h2f[:], in1=G.modc[:, 2, :, j:j + 1].to_broadcast([128, 8, 128]), op=ALU.add),
                 reads=[t_h2f, G.t_modc], writes=[t_h2f])
            S.op("act", lambda e: e.copy(out=G.h2T[:, :, tile * 128:(tile + 1) * 128], in_=h2f[:]), reads=[t_h2f], writes=[G.t_h2T])
            for k in range(8):
                S.op("pe", lambda e: e.matmul(pr[:], lhsT=h2f[:, k, :], rhs=wr[:, k, :], start=(k == 0), stop=(k == 7)),
                     reads=[t_h2f, t_wr], writes=[t_pr])
            S.op("dve", lambda e: e.tensor_copy(out=G.rl[:, tile, :], in_=pr[:]), reads=[t_pr], writes=[G.t_rl])


def moe(G, li):
    nc, S, I = G.nc, G.S, G.I
    need_ctx = li < DEPTH - 1
    last = li == DEPTH - 1
    h2T, rl = G.h2T, G.rl
    T0 = 0 if need_ctx else 2
    NTl = NT - T0
    BIG = 1.0e9
    with ExitStack() as _es:
        comb = _es.enter_context(SB(nc, "comb", [128, NT, NE], F32))
        t_comb = Tok()
        with ExitStack() as _es:
            sc = _es.enter_context(SB(nc, "r_sc", [128, NT, NE], F32))
            sel = _es.enter_context(SB(nc, "r_sel", [128, NT, NE], F32))
            ra = _es.enter_context(SB(nc, "r_a", [128, NT, NE], F32))
            rb = _es.enter_context(SB(nc, "r_b", [128, NT, NE], F32))
            rm = _es.enter_context(SB(nc, "r_m", [128, NT * 4], F32))
            rm2 = _es.enter_context(SB(nc, "r_m2", [128, NT * 4], F32))
            rg = _es.enter_context(SB(nc, "r_g", [128, NT], F32))
            rbias = _es.enter_context(SB(nc, "rbias", [128, NE], F32))
            t = Tok()
            if T0 > 0:
                S.op("dve", lambda e: e.memset(rl[:, 0:T0, :], 0.0), reads=[G.t_rl], writes=[G.t_rl])
            S.dma(rbias[:], I["router_bias"].partition_broadcast(128), writes=[t])
            v3 = lambda a: a[:].rearrange("p n (g x) -> p (n g) x", x=4)
            S.op("act", lambda e: e.activation(out=sc[:], in_=rl[:], func=AF.Sigmoid), reads=[G.t_rl], writes=[t])
            S.op("dve", lambda e: e.tensor_tensor(out=sel[:], in0=sc[:], in1=rbias[:].unsqueeze(1).to_broadcast([128, NT, NE]), op=ALU.add),
                 reads=[t], writes=[t])
            S.op("dve", lambda e: e.tensor_reduce(out=rm[:], in_=v3(sel), axis=AX.X, op=ALU.max), reads=[t], writes=[t])
            S.op("dve", lambda e: e.tensor_tensor(out=v3(ra), in0=v3(sel), in1=rm[:].unsqueeze(2).to_broadcast([128, NT * 4, 4]), op=ALU.is_equal),
                 reads=[t], writes=[t])
            S.op("dve", lambda e: e.scalar_tensor_tensor(out=rb[:], in0=ra[:], scalar=-BIG, in1=sel[:], op0=ALU.mult, op1=ALU.add),
                 reads=[t], writes=[t])
            S.op("dve", lambda e: e.tensor_reduce(out=rm2[:], in_=v3(rb), axis=AX.X, op=ALU.max), reads=[t], writes=[t])
            S.op("dve", lambda e: e.tensor_tensor(out=rm[:], in0=rm[:], in1=rm2[:], op=ALU.add), reads=[t], writes=[t])
            S.op("dve", lambda e: e.tensor_reduce(out=rg[:], in_=rm[:].rearrange("p (n g) -> p n g", g=4), axis=AX.X, op=ALU.max),
                 reads=[t], writes=[t])
            S.op("dve", lambda e: e.tensor_tensor(out=rm2[:].rearrange("p (n g) -> p n g", g=4), in0=rm[:].rearrange("p (n g) -> p n g", g=4),
                                                  in1=rg[:].unsqueeze(2).to_broadcast([128, NT, 4]), op=ALU.is_equal), reads=[t], writes=[t])
            S.op("dve", lambda e: e.tensor_scalar(out=rm2[:], in0=rm2[:], scalar1=1.0, scalar2=BIG, op0=ALU.subtract, op1=ALU.mult),
                 reads=[t], writes=[t])
            S.op("dve", lambda e: e.tensor_tensor(out=v3(sel), in0=v3(sel), in1=rm2[:].unsqueeze(2).to_broadcast([128, NT * 4, 4]), op=ALU.add),
                 reads=[t], writes=[t])
            S.op("dve", lambda e: e.tensor_reduce(out=rg[:], in_=sel[:], axis=AX.X, op=ALU.max), reads=[t], writes=[t])
            S.op("dve", lambda e: e.tensor_tensor(out=ra[:], in0=sel[:], in1=rg[:].unsqueeze(2).to_broadcast([128, NT, NE]), op=ALU.is_equal),
                 reads=[t], writes=[t])
            S.op("dve", lambda e: e.scalar_tensor_tensor(out=sel[:], in0=ra[:], scalar=-BIG, in1=sel[:], op0=ALU.mult, op1=ALU.add),
                 reads=[t], writes=[t])
            S.op("dve", lambda e: e.tensor_reduce(out=rg[:], in_=sel[:], axis=AX.X, op=ALU.max), reads=[t], writes=[t])
            S.op("dve", lambda e: e.tensor_tensor(out=rb[:], in0=sel[:], in1=rg[:].unsqueeze(2).to_broadcast([128, NT, NE]), op=ALU.is_equal),
                 reads=[t], writes=[t])
            S.op("dve", lambda e: e.tensor_tensor(out=ra[:], in0=ra[:], in1=rb[:], op=ALU.add), reads=[t], writes=[t])
            S.op("dve", lambda e: e.tensor_tensor(out=ra[:], in0=ra[:], in1=sc[:], op=ALU.mult), reads=[t], writes=[t])
            S.op("dve", lambda e: e.tensor_reduce(out=rg[:], in_=ra[:], axis=AX.X, op=ALU.add), reads=[t], writes=[t])
            S.op("dve", lambda e: e.reciprocal(out=rg[:], in_=rg[:]), reads=[t], writes=[t])
            S.op("dve", lambda e: e.tensor_tensor(out=comb[:], in0=ra[:], in1=rg[:].unsqueeze(2).to_broadcast([128, NT, NE]), op=ALU.mult),
                 reads=[t], writes=[t_comb])
            S.barrier()
        SGT = 12
        with ExitStack() as _es:
            acc = _es.enter_context(SB(nc, "acc", [128, SGT, D], F32))
            wg = _es.enter_context(SB(nc, "wg", [128, 2, 8, DFF], BF16))
            wu = _es.enter_context(SB(nc, "wu", [128, 2, 8, DFF], BF16))
            wd = _es.enter_context(SB(nc, "wd", [128, 2, 2, D], BF16))
            sgl = _es.enter_context(SB(nc, "sgl", [128, 2, 512], F32))
            aa2 = _es.enter_context(SB(nc, "aa", [128, 2, 2, 512], BF16))
            xt = _es.enter_context(SB(nc, "xt", [128, 2, D], F32))
            gfb = _es.enter_context(SB(nc, "gfb", [128, D], F32))
            ss = _es.enter_context(SB(nc, "ss", [128, 4], F32))
            sq = _es.enter_context(SB(nc, "sq", [128, D], F32))
            pgu = _es.enter_context(PS(nc, "pgu", [128, 4, 512], F32))
            py = _es.enter_context(PS(nc, "py", [128, 2, 2, 512], F32))
            t_pgu2, t_sgl2, t_aa2 = [Tok(), Tok()], [Tok(), Tok()], [Tok(), Tok()]
            t_acc, t_w, t_sgl, t_aa, t_pgu, t_py, t_x, t_gf, t_ss, t_sq = Tok(), [Tok(), Tok()], Tok(), Tok(), Tok(), [Tok(), Tok()], [Tok(), Tok()], Tok(), Tok(), Tok()
            if last:
                S.dma(gfb[:], I["g_final"].partition_broadcast(128), writes=[t_gf])
            yi = 0
            ui = 0
            for s0 in range(T0, NT, SGT):
                tiles = list(range(s0, min(NT, s0 + SGT)))
                units = []
                for ex in range(NE):
                    for b0 in range(0, len(tiles), 4):
                        bt = tiles[b0:b0 + 4]
                        units.append((ex, bt, len(bt) * 128, bt[0] * 128, b0 == 0))

                def gu(u, jj):
                    ex, bt, n, c0, newex = units[u]
                    wb_ = ex % 2
                    ub = (ui + u) % 2
                    if newex and jj == 0:
                        S.dma(wg[:, wb_], G.wb_gate[li][ex].rearrange("(k p) f -> p k f", p=128), reads=[G.t_wb], writes=[t_w[wb_]])
                        S.dma(wu[:, wb_], G.wb_up[li][ex].rearrange("(k p) f -> p k f", p=128), reads=[G.t_wb], writes=[t_w[wb_]])
                        S.dma(wd[:, wb_], G.wb_down[li][ex].rearrange("(j p) c -> p j c", p=128), reads=[G.t_wb], writes=[t_w[wb_]])
                    for wi, wt in enumerate((wg, wu)):
                        for k in range(8):
                            S.op("pe", lambda e: e.matmul(pgu[:, jj * 2 + wi, 0:n], lhsT=wt[:, wb_, k, jj * 128:(jj + 1) * 128],
                                                          rhs=h2T[:, k, c0:c0 + n], start=(k == 0), stop=(k == 7)),
                                 reads=[t_w[wb_], G.t_h2T], writes=[t_pgu2[jj]])
                    S.op("act", lambda e: e.activation(out=sgl[:, jj, 0:n], in_=pgu[:, jj * 2, 0:n], func=AF.Silu), reads=[t_pgu2[jj]], writes=[t_sgl2[jj]])
                    S.op("dve", lambda e: e.tensor_tensor(out=aa2[:, ub, jj, 0:n], in0=sgl[:, jj, 0:n], in1=pgu[:, jj * 2 + 1, 0:n], op=ALU.mult),
                         reads=[t_sgl2[jj], t_pgu2[jj]], writes=[t_aa2[ub]])

                def down(u):
                    nonlocal yi
                    ex, bt, n, c0, newex = units[u]
                    wb_ = ex % 2
                    ub = (ui + u) % 2
                    for ti, tl in enumerate(bt):
                        yb = yi % 2
                        yi += 1
                        for half in range(2):
                            for jj in range(2):
                                S.op("pe", lambda e: e.matmul(py[:, yb, half, :], lhsT=aa2[:, ub, jj, ti * 128:(ti + 1) * 128],
                                                              rhs=wd[:, wb_, jj, half * 512:(half + 1) * 512], start=(jj == 0), stop=(jj == 1)),
                                     reads=[t_aa2[ub], t_w[wb_]], writes=[t_py[yb]])
                        al = acc[:, tl - s0, :]
                        pyv = py[:, yb].rearrange("p a c -> p (a c)")
                        if ex == 0:
                            S.op("dve", lambda e: e.tensor_scalar(out=al, in0=pyv, scalar1=comb[:, tl, ex:ex + 1], scalar2=None, op0=ALU.mult),
                                 reads=[t_py[yb], t_comb], writes=[t_acc])
                        else:
                            S.op("dve", lambda e: e.scalar_tensor_tensor(out=al, in0=pyv, scalar=comb[:, tl, ex:ex + 1], in1=al,
                                                                         op0=ALU.mult, op1=ALU.add), reads=[t_py[yb], t_comb, t_acc], writes=[t_acc])

                gu(0, 0)
                gu(0, 1)
                for u in range(len(units)):
                    if u + 1 < len(units):
                        gu(u + 1, 0)
                    down(u)
                    if u + 1 < len(units):
                        gu(u + 1, 1)
                ui += len(units)
                for tl in tiles:
                    b = tl % 2
                    j = 1 if tl < 2 else 0
                    S.dma(xt[:, b, :], G.xres[tl * 128:(tl + 1) * 128, :], reads=[G.t_xres[tl]], writes=[t_x[b]])
                    al = acc[:, tl - s0, :]
                    S.op("dve", lambda e: e.tensor_tensor(out=al, in0=al, in1=G.gtb[:, 1, j, :], op=ALU.mult), reads=[t_acc, G.t_gtb], writes=[t_acc])
                    S.op("dve", lambda e: e.tensor_tensor(out=xt[:, b, :], in0=al, in1=xt[:, b, :], op=ALU.add), reads=[t_acc, t_x[b]], writes=[t_x[b]])
                    if not last:
                        S.dma(G.xres[tl * 128:(tl + 1) * 128, :], xt[:, b, :], reads=[t_x[b]], writes=[G.t_xres[tl]])
                    else:
                        xv = xt[:, b, :]
                        S.op("act", lambda e: e.activation(out=sq[:], in_=xv, func=AF.Square, accum_out=ss[:, 0:1]), reads=[t_x[b]], writes=[t_sq, t_ss])
                        S.op("act", lambda e: e.activation(out=ss[:, 1:2], in_=ss[:, 0:1], func=AF.Sqrt, bias=G.epsc[:], scale=1.0 / D),
                             reads=[t_ss, G.t_c], writes=[t_ss])
                        S.op("dve", lambda e: e.reciprocal(out=ss[:, 2:3], in_=ss[:, 1:2]), reads=[t_ss], writes=[t_ss])
                        S.op("dve", lambda e: e.scalar_tensor_tensor(out=xv, in0=xv, scalar=ss[:, 2:3], in1=gfb[:], op0=ALU.mult, op1=ALU.mult),
                             reads=[t_x[b], t_ss, t_gf], writes=[t_x[b]])
                        S.dma(G.out[(tl - 2) * 128:(tl - 1) * 128, :], xv, reads=[t_x[b]], writes=[])


def ssd(G, li):
    nc, S, I = G.nc, G.S, G.I
    hT = G.hT
    need_ctx = li < DEPTH - 1
    wv32 = I["w_in"][li].rearrange("(k p) c -> p k c", p=128)
    wvb = G.wb_in[li].rearrange("(k p) c -> p k c", p=128)
    XB0 = 1024
    with ExitStack() as _es:
        xbcT = _es.enter_context(SB(nc, "xbcT", [128, 5, NTOK], BF16))
        xs_tok = _es.enter_context(SB(nc, "xs_tok", [128, NT, 384], BF16))
        B_tok = _es.enter_context(SB(nc, "B_tok", [128, NT, 128], BF16))
        lndt = _es.enter_context(SB(nc, "lndt", [128, NT, 12], F32))
        dta = _es.enter_context(SB(nc, "dta", [128, NT, 12], F32))
        ea = _es.enter_context(SB(nc, "ea", [128, NT, 12], F32))
        ww = _es.enter_context(SB(nc, "ww", [128, NT, 12], F32))
        eT = _es.enter_context(SB(nc, "eT", [128, NT, 12], F32))
        t_xbc, t_xs, t_dt = Tok(), Tok(), Tok()
        with ExitStack() as _es2:
            dts = _es2.enter_context(SB(nc, "dts", [128, NT, 12], F32))
            wst = _es2.enter_context(SB(nc, "wst", [128, 8, 128], F32))
            cwb = _es2.enter_context(SB(nc, "cwb", [128, 3, 128], F32))
            wj = _es2.enter_context(SB(nc, "wj", [128, 3, 8, 128], BF16))
            wdt = _es2.enter_context(SB(nc, "wdt", [128, 8, 12], BF16))
            dtb = _es2.enter_context(SB(nc, "dtb", [128, 2, 12], F32))
            tot = _es2.enter_context(SB(nc, "tot", [128, NT, 12], F32))
            wcol = _es2.enter_context(SB(nc, "wcol", [128, NT, 12], F32))
            pp2 = _es2.enter_context(PS(nc, "pp", [128, 2, 512], F32))
            pdt = _es2.enter_context(PS(nc, "pdt", [128, NT, 12], F32))
            ptb = _es2.enter_context(PS(nc, "ptb", [128, 4, 128], BF16))
            pc = _es2.enter_context(PS(nc, "pc", [128, NT, 12], F32))
            t_wst, t_cwb, t_wj, t_pp, t_wdt, t_pdt, t_ptb, t_a, t_pc = Tok(), Tok(), Tok(), Tok(), Tok(), Tok(), Tok(), Tok(), Tok()
            ocb = COLS["ssd_conv_b"][0]
            t_pp2 = [Tok(), Tok()]
            for ch in range(5):
                S.dma(wst[:], wv32[:, :, XB0 + ch * 128:XB0 + (ch + 1) * 128], writes=[t_wst])
                for j in range(3):
                    S.dma(cwb[:, j, :], I["ssd_conv_w"][li, j, ch * 128:(ch + 1) * 128].partition_broadcast(128), writes=[t_cwb])
                for j in range(3):
                    S.op("dve", lambda e: e.tensor_tensor(out=wj[:, j], in0=wst[:], in1=cwb[:, j:j + 1, :].to_broadcast([128, 8, 128]), op=ALU.mult),
                         reads=[t_wst, t_cwb], writes=[t_wj])
                for bi_, (c0, t0, n, tile0, ntile) in enumerate(BLOCKS):
                    pb_ = (ch * len(BLOCKS) + bi_) % 2
                    pp = pp2[:, pb_, :]
                    for j in range(3):
                        for k in range(8):
                            S.op("pe", lambda e: e.matmul(pp[:, 0:n], lhsT=wj[:, j, k, :], rhs=hT[:, k, c0 + j - 1:c0 + j - 1 + n],
                                                          start=(j == 0 and k == 0), stop=(j == 2 and k == 7)), reads=[t_wj, G.t_hT], writes=[t_pp2[pb_]])
                    S.op("act", lambda e: e.activation(out=xbcT[:, ch, t0:t0 + n], in_=pp[:, 0:n], func=AF.Silu, bias=G.cols[:, ocb + ch:ocb + ch + 1]),
                         reads=[t_pp2[pb_], G.t_cols], writes=[t_xbc])
            S.dma(wdt[:], wvb[:, :, 1664:1676], reads=[G.t_wb], writes=[t_wdt])
            S.dma(dtb[:, 0, :], I["ssd_dt_bias"][li].rearrange("a h -> (a h)").partition_broadcast(128), writes=[t_wdt])
            S.dma(dtb[:, 1, :], I["ssd_a_log"][li].rearrange("a h -> (a h)").partition_broadcast(128), writes=[t_wdt])
            for tl in range(NT):
                cc = colof(tl)
                for k in range(8):
                    S.op("pe", lambda e: e.matmul(pdt[:, tl, :], lhsT=hT[:, k, cc:cc + 128], rhs=wdt[:, k, :], start=(k == 0), stop=(k == 7)),
                         reads=[t_wdt, G.t_hT], writes=[t_pdt])
            S.op("dve", lambda e: e.tensor_tensor(out=dts[:], in0=pdt[:], in1=dtb[:, 0:1, :].to_broadcast([128, NT, 12]), op=ALU.add),
                 reads=[t_pdt, t_wdt], writes=[t_dt])
            S.op("act", lambda e: e.activation(out=dts[:], in_=dts[:], func=AF.Exp), reads=[t_dt], writes=[t_dt])
            S.op("act", lambda e: e.activation(out=dts[:], in_=dts[:], func=AF.Ln, bias=1.0), reads=[t_dt], writes=[t_dt])
            S.op("act", lambda e: e.activation(out=lndt[:], in_=dts[:], func=AF.Ln), reads=[t_dt], writes=[t_dt])
            S.op("act", lambda e: e.activation(out=dtb[:, 1, :], in_=dtb[:, 1, :], func=AF.Exp), reads=[t_wdt], writes=[t_wdt])
            S.op("dve", lambda e: e.scalar_tensor_tensor(out=dta[:], in0=dts[:], scalar=-1.0, in1=dtb[:, 1:2, :].to_broadcast([128, NT, 12]),
                                                         op0=ALU.mult, op1=ALU.mult), reads=[t_dt, t_wdt], writes=[t_a])
            for tl in range(NT):
                for c in range(4):
                    S.op("pe", lambda e: e.transpose(out=ptb[:, c, :], in_=xbcT[:, c, tl * 128:(tl + 1) * 128], identity=G.identB),
                         reads=[t_xbc, G.t_c], writes=[t_ptb])
                S.op("dve", lambda e: e.tensor_copy(out=xs_tok[:, tl, :], in_=ptb[:, 0:3, :].rearrange("p c t -> p (c t)")), reads=[t_ptb], writes=[t_xs])
                S.op("dve", lambda e: e.tensor_copy(out=B_tok[:, tl, :], in_=ptb[:, 3, :]), reads=[t_ptb], writes=[t_xs])
            for dr in range(2):
                S.op("pe", lambda e: e.matmul(pc[:].rearrange("p n h -> p (n h)"), lhsT=G.cm[:, 1 + dr, :], rhs=dta[:].rearrange("p n h -> p (n h)"),
                                              start=True, stop=True), reads=[t_a, G.t_c, t_dt], writes=[t_pc])
                S.op("dve", lambda e: e.tensor_copy(out=wcol[:, :, dr * 6:(dr + 1) * 6], in_=pc[:, :, dr * 6:(dr + 1) * 6]), reads=[t_pc], writes=[t_dt])
            S.op("pe", lambda e: e.matmul(pc[:].rearrange("p n h -> p (n h)"), lhsT=G.ones[:], rhs=dta[:].rearrange("p n h -> p (n h)"), start=True, stop=True),
                 reads=[t_a, G.t_c, t_dt], writes=[t_pc])
            S.op("dve", lambda e: e.tensor_copy(out=tot[:], in_=pc[:]), reads=[t_pc], writes=[t_dt])
            S.op("act", lambda e: e.activation(out=ea[:], in_=wcol[:], func=AF.Exp), reads=[t_dt], writes=[t_dt])
            S.op("act", lambda e: e.activation(out=eT[:], in_=tot[:], func=AF.Exp), reads=[t_dt], writes=[t_dt])
            S.op("dve", lambda e: e.tensor_tensor(out=ww[:], in0=tot[:], in1=wcol[:], op=ALU.subtract), reads=[t_dt], writes=[t_dt])
            S.op("dve", lambda e: e.tensor_tensor(out=ww[:], in0=ww[:], in1=lndt[:], op=ALU.add), reads=[t_dt], writes=[t_dt])
            S.op("act", lambda e: e.activation(out=ww[:], in_=ww[:], func=AF.Exp), reads=[t_dt], writes=[t_dt])
            S.barrier()
        with ExitStack() as _es2:
            wz = _es2.enter_context(SB(nc, "wz", [128, 8, 384], BF16))
            dbc = _es2.enter_context(SB(nc, "dbc", [128, 6, 64], F32))
            d6 = _es2.enter_context(SB(nc, "d6", [128, 6], F32))
            nwb = _es2.enter_context(SB(nc, "nwb", [128, 384], F32))
            hst = _es2.enter_context(SB(nc, "hst", [128, 192], F32))
            hsb = _es2.enter_context(SB(nc, "hsb", [128, 192], BF16))
            xw = _es2.enter_context(SB(nc, "xw", [128, 2, 192], BF16))
            ybt = _es2.enter_context(SB(nc, "ybt", [128, 2, 384], F32))
            Sm = _es2.enter_context(SB(nc, "Sm", [128, 2, 2, 128], F32))
            rA = _es2.enter_context(SB(nc, "rA", [128, 4, 128], F32))
            Dd = _es2.enter_context(SB(nc, "Dd", [128, 4, 128], F32))
            Mt = _es2.enter_context(SB(nc, "Mt", [128, 4, 128], BF16))
            acc = _es2.enter_context(SB(nc, "acc", [128, 384], F32))
            tmp = _es2.enter_context(SB(nc, "tmp", [128, 384], F32))
            zs = _es2.enter_context(SB(nc, "zs", [128, 384], F32))
            ssq = _es2.enter_context(SB(nc, "ssq", [128, 4], F32))
            ob = _es2.enter_context(SB(nc, "ob", [128, 384], BF16))
            oT = _es2.enter_context(SB(nc, "oT", [128, 2, 3, 128], BF16))
            pis = _es2.enter_context(PS(nc, "pis", [128, 2, 192], F32))
            ps_st = _es2.enter_context(PS(nc, "ps_st", [128, 2, 512], F32))
            pseg = _es2.enter_context(PS(nc, "pseg", [128, 3, 512], F32))
            py = _es2.enter_context(PS(nc, "py", [128, 384], F32))
            pz = py
            ptr = _es2.enter_context(PS(nc, "ptr", [128, 3, 128], BF16))
            t_wz, t_db, t_h, t_xw, t_yb, t_Sm, t_acc, t_tmp, t_zs, t_ssq, t_ob, t_oT = Tok(), Tok(), Tok(), Tok(), [Tok(), Tok()], Tok(), Tok(), Tok(), Tok(), Tok(), Tok(), [Tok(), Tok()]
            t_rA, t_Dd, t_Mt, t_pseg = [Tok() for _ in range(4)], [Tok() for _ in range(4)], [Tok() for _ in range(4)], [Tok() for _ in range(3)]
            t_pis, t_pst, t_py, t_ptr = Tok(), Tok(), Tok(), Tok()
            t_pz = t_py
            S.dma(wz[:], wvb[:, :, 640:1024], reads=[G.t_wb], writes=[t_wz])
            S.dma(d6[:], I["ssd_d"][li].partition_broadcast(128), writes=[t_db])
            S.dma(nwb[:], I["ssd_norm"][li].partition_broadcast(128), writes=[t_db])
            S.op("dve", lambda e: e.tensor_copy(out=dbc[:], in_=d6[:].unsqueeze(2).to_broadcast([128, 6, 64])), reads=[t_db], writes=[t_db])
            yb_d = G.ssd_yb

            def carry_step(c, dr, want_out, dst):
                for g in range(2):
                    hd0 = dr * 6 + 3 * g
                    if want_out:
                        S.op("pe", lambda e: e.matmul(pis[:, 0, :], lhsT=xbcT[g * 64:(g + 1) * 64, 4, c * 128:(c + 1) * 128], rhs=hsb[g * 64:(g + 1) * 64, :],
                                                      start=True, stop=True), reads=[t_xbc, t_h], writes=[t_pis])
                        S.op("dve", lambda e: e.tensor_tensor(out=dst[:, g * 192:(g + 1) * 192].rearrange("p (a d) -> p a d", a=3),
                                                              in0=pis[:, 0, :].rearrange("p (a d) -> p a d", a=3),
                                                              in1=ea[:, c, hd0:hd0 + 3].unsqueeze(2).to_broadcast([128, 3, 64]), op=ALU.mult),
                             reads=[t_pis, t_dt], writes=[dst_tok[0]])
                    S.op("dve", lambda e: e.tensor_tensor(out=xw[:, g, :].rearrange("p (a d) -> p a d", a=3),
                                                          in0=xs_tok[:, c, g * 192:(g + 1) * 192].rearrange("p (a d) -> p a d", a=3),
                                                          in1=ww[:, c, hd0:hd0 + 3].unsqueeze(2).to_broadcast([128, 3, 64]), op=ALU.mult),
                         reads=[t_xs, t_dt], writes=[t_xw])
                    gs = slice(g * 64, (g + 1) * 64)
                    S.op("pe", lambda e: e.matmul(pis[:, 1, :], lhsT=B_tok[:, c, :], rhs=xw[:, g, :], start=True, stop=True),
                         reads=[t_xs, t_xw], writes=[t_pis])
                    S.op("dve", lambda e: e.tensor_tensor(out=hst[gs, :].rearrange("p (a d) -> p a d", a=3),
                                                          in0=hst[gs, :].rearrange("p (a d) -> p a d", a=3),
                                                          in1=eT[gs, c, hd0:hd0 + 3].unsqueeze(2).to_broadcast([64, 3, 64]), op=ALU.mult),
                         reads=[t_h, t_dt], writes=[t_h])
                    S.op("dve", lambda e: e.tensor_tensor(out=hst[gs, :], in0=hst[gs, :], in1=pis[gs, 1, :], op=ALU.add),
                         reads=[t_h, t_pis], writes=[t_h])
                    S.op("act", lambda e: e.copy(out=hsb[gs, :], in_=hst[gs, :]), reads=[t_h], writes=[t_h])

            S.op("dve", lambda e: e.memset(hst[:], 0.0), writes=[t_h])
            S.op("dve", lambda e: e.memset(hsb[:], 0.0), writes=[t_h])
            order_b = [1, 0] + list(range(NT - 1, 1, -1))
            for ci, c in enumerate(order_b):
                want = need_ctx or c >= 2
                b = ci % 2
                dst_tok = [t_yb[b]]
                carry_step(c, 1, want, ybt[:, b, :])
                if want:
                    S.dma(yb_d[c * 128:(c + 1) * 128, :], ybt[:, b, :], reads=[t_yb[b]], writes=[G.t_ssdyb[c]])
            S.op("dve", lambda e: e.memset(hst[:], 0.0), reads=[t_h], writes=[t_h])
            S.op("dve", lambda e: e.memset(hsb[:], 0.0), reads=[t_h], writes=[t_h])
            on = COLS["ssd_norm"][0]
            it = 0
            for c in range(NT):
                want = need_ctx or c >= 2
                dst_tok = [t_acc]
                carry_step(c, 0, want, acc[:])
                if not want:
                    continue
                b = c % 2
                tc0 = c * 128
                S.dma(ybt[:, b, :], yb_d[c * 128:(c + 1) * 128, :], reads=[G.t_ssdyb[c]], writes=[t_yb[b]])
                cc = colof(c)
                for k in range(8):
                    S.op("pe", lambda e: e.matmul(pz[:], lhsT=hT[:, k, cc:cc + 128], rhs=wz[:, k, :], start=(k == 0), stop=(k == 7)),
                         reads=[t_wz, G.t_hT], writes=[t_pz])
                S.op("act", lambda e: e.activation(out=zs[:], in_=pz[:], func=AF.Silu), reads=[t_pz], writes=[t_zs])
                for g in range(2):
                    S.op("pe", lambda e: e.matmul(ps_st[:, g, 0:128], lhsT=xbcT[g * 64:(g + 1) * 64, 3, tc0:tc0 + 128], rhs=xbcT[g * 64:(g + 1) * 64, 4, tc0:tc0 + 128],
                                                  start=True, stop=True), reads=[t_xbc], writes=[t_pst])
                for g in range(2):
                    for dr in range(2):
                        S.op("dve", lambda e: e.tensor_tensor(out=Sm[:, g, dr, :], in0=ps_st[:, g, 0:128], in1=G.cm[:, 1 + dr, :], op=ALU.mult),
                             reads=[t_pst, G.t_c], writes=[t_Sm])
                steps = [(h, dr) for h in range(6) for dr in range(2)]

                def st_a(i):
                    h, dr = steps[i]
                    hd = dr * 6 + h
                    r4, p3 = (it + i) % 4, (it + i) % 3
                    S.op("dve", lambda e: e.tensor_scalar(out=rA[:, r4, :], in0=G.cm[:, 1 + dr, :], scalar1=dta[:, c, hd:hd + 1], scalar2=None, op0=ALU.mult),
                         reads=[G.t_c, t_dt], writes=[t_rA[r4]])
                    S.op("pe", lambda e: e.matmul(pseg[:, p3, 0:128], lhsT=G.cm[:, 8 - dr, :], rhs=rA[:, r4, :], start=True, stop=True),
                         reads=[t_rA[r4], G.t_c], writes=[t_pseg[p3]])
                    S.op("act", lambda e: e.activation(out=Dd[:, r4, :], in_=pseg[:, p3, 0:128], func=AF.Exp, bias=lndt[:, c, hd:hd + 1]),
                         reads=[t_pseg[p3], t_dt], writes=[t_Dd[r4]])

                st_a(0)
                st_a(1)
                for i, (h, dr) in enumerate(steps):
                    r4 = (it + i) % 4
                    if i + 2 < len(steps):
                        st_a(i + 2)
                    S.op("dve", lambda e: e.tensor_tensor(out=Mt[:, r4, :], in0=Dd[:, r4, :], in1=Sm[:, h // 3, dr, :], op=ALU.mult),
                         reads=[t_Dd[r4], t_Sm], writes=[t_Mt[r4]])
                    S.op("pe", lambda e: e.matmul(py[:, h * 64:(h + 1) * 64], lhsT=Mt[:, r4, :], rhs=xs_tok[:, c, h * 64:(h + 1) * 64],
                                                  start=(dr == 0), stop=(dr == 1)), reads=[t_Mt[r4], t_xs], writes=[t_py])
                it += len(steps)
                S.op("dve", lambda e: e.tensor_tensor(out=acc[:], in0=acc[:], in1=py[:], op=ALU.add), reads=[t_acc, t_py], writes=[t_acc])
                S.op("dve", lambda e: e.tensor_tensor(out=acc[:], in0=acc[:], in1=ybt[:, b, :], op=ALU.add), reads=[t_acc, t_yb[b]], writes=[t_acc])
                S.op("dve", lambda e: e.tensor_tensor(out=tmp[:], in0=xs_tok[:, c, :], in1=dbc[:].rearrange("p a d -> p (a d)"), op=ALU.mult),
                     reads=[t_xs, t_db], writes=[t_tmp])
                S.op("dve", lambda e: e.tensor_tensor(out=acc[:], in0=acc[:], in1=tmp[:], op=ALU.add), reads=[t_acc, t_tmp], writes=[t_acc])
                S.op("dve", lambda e: e.tensor_tensor(out=acc[:], in0=acc[:], in1=zs[:], op=ALU.mult), reads=[t_acc, t_zs], writes=[t_acc])
                for g in range(2):
                    S.op("act", lambda e: e.activation(out=tmp[:, g * 192:(g + 1) * 192], in_=acc[:, g * 192:(g + 1) * 192], func=AF.Square, accum_out=ssq[:, g:g + 1]),
                         reads=[t_acc], writes=[t_tmp, t_ssq])
                S.op("act", lambda e: e.activation(out=ssq[:, 2:4], in_=ssq[:, 0:2], func=AF.Sqrt, bias=G.epsc[:], scale=1.0 / 192), reads=[t_ssq, G.t_c], writes=[t_ssq])
                S.op("dve", lambda e: e.reciprocal(out=ssq[:, 2:4], in_=ssq[:, 2:4]), reads=[t_ssq], writes=[t_ssq])
                for g in range(2):
                    S.op("dve", lambda e: e.scalar_tensor_tensor(out=ob[:, g * 192:(g + 1) * 192], in0=acc[:, g * 192:(g + 1) * 192], scalar=ssq[:, 2 + g:3 + g],
                                                                 in1=nwb[:, g * 192:(g + 1) * 192], op0=ALU.mult, op1=ALU.mult), reads=[t_acc, t_ssq, t_db], writes=[t_ob])
                for c3 in range(3):
                    S.op("pe", lambda e: e.transpose(out=ptr[:, c3, :], in_=ob[:, c3 * 128:(c3 + 1) * 128], identity=G.identB), reads=[t_ob, G.t_c], writes=[t_ptr])
                S.op("act", lambda e: e.copy(out=oT[:, b], in_=ptr[:]), reads=[t_ptr], writes=[t_oT[b]])
                S.dma(G.mixT[384:768, tc0:tc0 + 128].rearrange("(c p) t -> p c t", p=128), oT[:, b], reads=[t_oT[b]], writes=[G.t_mix])
        if "ssd" in G.dbg and li == 0:
            dump_bf16(G, G.mixT[384:512, 256:768], G.dbg["ssd"], [G.t_mix])


HY0 = 1676
SEGS = {"lat": dict(L=4096, NC=32, KC=17, off=NCTX), "ctx": dict(L=256, NC=2, KC=2, off=0)}


def hyena_inproj(G, li):
    nc, S, I = G.nc, G.S, G.I
    hT = G.hT
    need_ctx = li < DEPTH - 1
    wv32 = I["w_in"][li].rearrange("(k p) c -> p k c", p=128)
    with ExitStack() as _es:
        wst = _es.enter_context(SB(nc, "wst", [128, 8, 128], F32))
        cwb = _es.enter_context(SB(nc, "cwb", [128, 3, 128], F32))
        wj = _es.enter_context(SB(nc, "wjh", [128, 3, 8, 768], BF16))
        hb = _es.enter_context(SB(nc, "hb", [128, 768], F32))
        hv = _es.enter_context(SB(nc, "hv", [128, 2, 768], BF16))
        ph4 = _es.enter_context(PS(nc, "ph", [128, 2, 2, 512], F32))
        t_wst, t_cwb, t_wj, t_hb, t_hv, t_ph2 = Tok(), Tok(), Tok(), Tok(), [Tok(), Tok()], [Tok(), Tok()]
        S.dma(hb[:], I["hy_conv_b"][li].partition_broadcast(128), writes=[t_hb])
        for cc in range(6):
            S.dma(wst[:], wv32[:, :, HY0 + cc * 128:HY0 + (cc + 1) * 128], writes=[t_wst])
            for j in range(3):
                S.dma(cwb[:, j, :], I["hy_conv_w"][li, j, cc * 128:(cc + 1) * 128].partition_broadcast(128), writes=[t_cwb])
            for j in range(3):
                S.op("dve", lambda e: e.tensor_tensor(out=wj[:, j, :, cc * 128:(cc + 1) * 128], in0=wst[:],
                                                      in1=cwb[:, j:j + 1, :].to_broadcast([128, 8, 128]), op=ALU.mult),
                     reads=[t_wst, t_cwb], writes=[t_wj])
        dv = G.hyv.rearrange("m t c -> t m c")
        for tl in range(NT):
            if tl < 2 and not need_ctx:
                continue
            col = colof(tl)
            b = tl % 2
            ph = ph4[:, b]
            t_ph = t_ph2[b]
            for half in range(2):
                for j in range(3):
                    for k in range(8):
                        S.op("pe", lambda e: e.matmul(ph[:, half, 0:384], lhsT=hT[:, k, col + j - 1:col + j - 1 + 128],
                                                      rhs=wj[:, j, k, half * 384:(half + 1) * 384], start=(j == 0 and k == 0), stop=(j == 2 and k == 7)),
                             reads=[t_wj, G.t_hT], writes=[t_ph])
            S.op("dve", lambda e: e.tensor_tensor(out=hv[:, b, :].rearrange("p (a c) -> p a c", a=2), in0=ph[:, :, 0:384],
                                                  in1=hb[:].rearrange("p (a c) -> p a c", a=2), op=ALU.add), reads=[t_ph, t_hb], writes=[t_hv[b]])
            S.dma(dv[tl * 128:(tl + 1) * 128], hv[:, b, :].rearrange("p (m c) -> p m c", m=3), reads=[t_hv[b]], writes=[G.t_hyv])


def hyena_fft(G, li):
    nc, S, I = G.nc, G.S, G.I
    need_ctx = li < DEPTH - 1
    for sname in (("lat", "ctx") if need_ctx else ("lat",)):
        P = SEGS[sname]
        L, NCk, KC, off = P["L"], P["NC"], P["KC"], P["off"]
        NH = NCk // 2
        FT, IT, FE, WIN, MH = I["ft_" + sname], I["it_" + sname], I["fe_" + sname], I["win_" + sname], I["mh_" + sname]
        Kf = G.Kf[sname]
        t_kf = Tok()
        with ExitStack() as _es:
            hk = _es.enter_context(SB(nc, "hk", [128, NCk, 1024], BF16))
            fe = _es.enter_context(SB(nc, "fe", [33, L], F32))
            h1 = _es.enter_context(SB(nc, "h1", [64, L], F32))
            h2 = _es.enter_context(SB(nc, "h2", [64, L], BF16))
            w3b = _es.enter_context(SB(nc, "w3b", [64, 1024], BF16))
            w1 = _es.enter_context(SB(nc, "w1", [33, 64], F32))
            w2 = _es.enter_context(SB(nc, "w2", [64, 64], F32))
            w3 = _es.enter_context(SB(nc, "w3", [64, 1024], F32))
            pre = _es.enter_context(SB(nc, "pre", [64, 512], F32))
            pr2 = _es.enter_context(SB(nc, "pr2", [64, 512], F32))
            t_pr2 = Tok()
            MAGIC = 1.5 * 2 ** 23
            wint = _es.enter_context(SB(nc, "wint", [128, 2, 2, 256], F32))
            hbias = _es.enter_context(SB(nc, "hbias", [128, 512], F32))
            mh = _es.enter_context(SB(nc, "mh", [128, KC], F32))
            slab = _es.enter_context(SB(nc, "slab", [128, 2, NCk, 2, 128], BF16))
            xo = _es.enter_context(SB(nc, "xo", [128, 2, 512], F32))
            sd = _es.enter_context(SB(nc, "sd", [128, 2, 2, 2, 512], F32))
            kf = _es.enter_context(SB(nc, "kf", [128, 2, 2, 2, 256], BF16))
            _es_mlp = ExitStack()
            pm = _es_mlp.enter_context(PS(nc, "pm", [64, 512], F32))
            phh = _es_mlp.enter_context(PS(nc, "phh", [128, 2, 512], F32))
            t_hk, t_fe, t_h1, t_h2, t_w, t_pre, t_win, t_slab, t_eo, t_sd, t_kft, t_pm, t_phh, t_psk = \
                Tok(), Tok(), Tok(), Tok(), Tok(), Tok(), [Tok(), Tok()], [Tok(), Tok()], Tok(), Tok(), Tok(), Tok(), Tok(), Tok()
            S.dma(fe[:], FE[:, :], writes=[t_fe])
            S.dma(w1[:], I["hy_w1"][li], writes=[t_w])
            S.dma(w2[:], I["hy_w2"][li], writes=[t_w])
            S.dma(w3[:], I["hy_w3"][li], writes=[t_w])
            S.op("dve", lambda e: e.tensor_copy(out=w3b[:], in_=w3[:]), reads=[t_w], writes=[t_w])
            S.dma(hbias[:], I["hy_bias"][li].rearrange("o c -> (o c)").partition_broadcast(128), writes=[t_w])
            S.dma(mh[:], MH[:, :], writes=[t_w])
            ob1, ofr, ob2 = COLS["hy_b1"][0], COLS["hy_freq"][0], COLS["hy_b2"][0]
            for (src, t_src, wt, kk, bcol, dst, t_dst) in ((fe, t_fe, w1, 33, ob1, h1, t_h1), (h1, t_h1, w2, 64, ob2, h2, t_h2)):
                for c0 in range(0, L, 512):
                    n = min(512, L - c0)
                    S.op("pe", lambda e: e.matmul(pm[:, 0:n], lhsT=wt[0:kk, :], rhs=src[0:kk, c0:c0 + n], start=True, stop=True),
                         reads=[t_w, t_src], writes=[t_pm])
                    S.op("dve", lambda e: e.tensor_scalar(out=pre[:, 0:n], in0=pm[:, 0:n], scalar1=G.cols[0:64, bcol:bcol + 1],
                                                          scalar2=G.cols[0:64, ofr:ofr + 1], op0=ALU.add, op1=ALU.mult),
                         reads=[t_pm, G.t_cols], writes=[t_pre])
                    S.op("dve", lambda e: e.tensor_scalar(out=pr2[:, 0:n], in0=pre[:, 0:n], scalar1=1.0 / (2.0 * math.pi), scalar2=MAGIC, op0=ALU.mult, op1=ALU.add),
                         reads=[t_pre], writes=[t_pr2])
                    S.op("dve", lambda e: e.tensor_scalar(out=pr2[:, 0:n], in0=pr2[:, 0:n], scalar1=-MAGIC, scalar2=None, op0=ALU.add),
                         reads=[t_pr2], writes=[t_pr2])
                    S.op("dve", lambda e: e.scalar_tensor_tensor(out=pre[:, 0:n], in0=pr2[:, 0:n], scalar=-2.0 * math.pi, in1=pre[:, 0:n], op0=ALU.mult, op1=ALU.add),
                         reads=[t_pr2, t_pre], writes=[t_pre])
                    S.op("act", lambda e: e.activation(out=dst[:, c0:c0 + n], in_=pre[:, 0:n], func=AF.Sin),
                         reads=[t_pre], writes=[t_dst])
            for c in range(NCk):
                b = c % 2
                S.dma(wint[:, b], WIN[c], writes=[t_win[b]])
                for half in range(2):
                    S.op("pe", lambda e: e.matmul(phh[:, half, :], lhsT=h2[:, c * 128:(c + 1) * 128], rhs=w3b[:, half * 512:(half + 1) * 512], start=True, stop=True),
                         reads=[t_h2, t_w], writes=[t_phh])
                for dr in range(2):
                    S.op("dve", lambda e: e.tensor_tensor(out=hk[:, c, dr * 512:(dr + 1) * 512].rearrange("p (o c) -> p o c", o=2),
                                                          in0=phh[:, dr, :].rearrange("p (o c) -> p o c", o=2),
                                                          in1=wint[:, b, dr:dr + 1, :].to_broadcast([128, 2, 256]), op=ALU.mult),
                         reads=[t_phh, t_win[b]], writes=[t_hk])
            S.barrier()
            _es_mlp.close()
            psk2 = _es.enter_context(PS(nc, "psk", [128, 2, 2, 2, 512], F32))
            t_psk2 = [Tok(), Tok()]
            for kc in range(KC):
                b = kc % 2
                S.dma(slab[:, b], FT[kc], writes=[t_slab[b]])
                for dr in range(2):
                    psk = psk2[:, dr]
                    t_psk = t_psk2[dr]
                    for eo_ in range(2):
                        for ri in range(2):
                            for c in range(NH):
                                cc = eo_ * NH + c
                                S.op("pe", lambda e: e.matmul(psk[:, eo_, ri, :], lhsT=slab[:, b, cc, ri, :], rhs=hk[:, cc, dr * 512:(dr + 1) * 512],
                                                              start=(c == 0), stop=(c == NH - 1)), reads=[t_slab[b], t_hk], writes=[t_psk])
                    S.op("act", lambda e: e.copy(out=xo[:], in_=psk[:, 1]), reads=[t_psk], writes=[t_eo])
                    S.op("dve", lambda e: e.tensor_tensor(out=sd[:, dr, 0], in0=psk[:, 0], in1=xo[:], op=ALU.add), reads=[t_psk, t_eo], writes=[t_sd])
                    S.op("dve", lambda e: e.tensor_tensor(out=sd[:, dr, 1], in0=psk[:, 0], in1=xo[:], op=ALU.subtract), reads=[t_psk, t_eo], writes=[t_sd])
                v = lambda ap: ap.rearrange("p (o c) -> p o c", o=2)
                S.op("dve", lambda e: e.tensor_tensor(out=sd[:, 0, 0, 0, :], in0=sd[:, 0, 0, 0, :], in1=hbias[:], op=ALU.add), reads=[t_sd, t_w], writes=[t_sd])
                S.op("dve", lambda e: e.tensor_tensor(out=sd[:, 0, 1, 0, :], in0=sd[:, 0, 1, 0, :], in1=hbias[:], op=ALU.add), reads=[t_sd, t_w], writes=[t_sd])
                S.op("dve", lambda e: e.tensor_tensor(out=kf[:, :, 0, 0, :], in0=v(sd[:, 0, 0, 0, :]), in1=v(sd[:, 1, 0, 0, :]), op=ALU.add), reads=[t_sd], writes=[t_kft])
                S.op("dve", lambda e: e.tensor_tensor(out=kf[:, :, 0, 1, :], in0=v(sd[:, 0, 0, 1, :]), in1=v(sd[:, 1, 0, 1, :]), op=ALU.subtract), reads=[t_sd], writes=[t_kft])
                S.op("dve", lambda e: e.tensor_tensor(out=v(xo[:, 0, :]), in0=v(sd[:, 0, 1, 0, :]), in1=v(sd[:, 1, 1, 0, :]), op=ALU.add), reads=[t_sd], writes=[t_eo])
                S.op("dve", lambda e: e.tensor_tensor(out=v(xo[:, 1, :]), in0=v(sd[:, 1, 1, 1, :]), in1=v(sd[:, 0, 1, 1, :]), op=ALU.subtract), reads=[t_sd], writes=[t_eo])
                S.op("dve", lambda e: e.tensor_scalar(out=kf[:, :, 1, 0, :], in0=v(xo[:, 0, :]), scalar1=mh[:, kc:kc + 1], scalar2=None, op0=ALU.mult),
                     reads=[t_eo, t_w], writes=[t_kft])
                S.op("dve", lambda e: e.tensor_scalar(out=kf[:, :, 1, 1, :], in0=v(xo[:, 1, :]), scalar1=mh[:, kc:kc + 1], scalar2=None, op0=ALU.mult),
                     reads=[t_eo, t_w], writes=[t_kft])
                S.dma(Kf[:, kc].rearrange("o p l r c -> p o l r c"), kf[:], reads=[t_kft], writes=[t_kf])
            S.barrier()
        with ExitStack() as _es:
            vt = _es.enter_context(SB(nc, "vt", [128, NCk, 256], BF16))
            zz1 = _es.enter_context(SB(nc, "zz1", [128, NCk, 256], BF16))
            Y = _es.enter_context(SB(nc, "Y", [128, 2, KC, 2, 256], BF16))
            fsl = _es.enter_context(SB(nc, "fsl", [128, 2, NCk, 2, 128], BF16))
            isl = _es.enter_context(SB(nc, "isl", [128, 2, KC, 2, 128], BF16))
            kft = _es.enter_context(SB(nc, "kft", [128, 2, 2, 2, 256], BF16))
            xo = _es.enter_context(SB(nc, "xo", [128, 2, 256], F32))
            xs_ = _es.enter_context(SB(nc, "xs_", [128, 2, 2, 256], F32))
            ta = _es.enter_context(SB(nc, "ta", [128, 4, 256], F32))
            yl = _es.enter_context(SB(nc, "yl", [128, 2, 2, 256], F32))
            xg = _es.enter_context(SB(nc, "xg", [128, 2, 256], BF16))
            zt = _es.enter_context(SB(nc, "zt", [128, 256], BF16))
            zT = _es.enter_context(SB(nc, "zT", [128, 2, 2, 256], BF16))
            psx = _es.enter_context(PS(nc, "psx", [128, 2, 4, 256], F32))
            psy = _es.enter_context(PS(nc, "psy", [128, 2, 512], F32))
            ptr = _es.enter_context(PS(nc, "ptr", [128, 2, 128], BF16))
            t_vt, t_zz1, t_Y, t_fsl, t_isl, t_kft2, t_ta, t_xg, t_zt, t_zT, t_psx, t_psy, t_ptr, t_xo, t_xs, t_yl = \
                Tok(), Tok(), Tok(), [Tok(), Tok()], [Tok(), Tok()], [Tok(), Tok()], Tok(), [Tok(), Tok()], Tok(), [Tok(), Tok()], [Tok(), Tok()], [Tok(), Tok()], Tok(), Tok(), Tok(), Tok()
            hsrc = lambda m: G.hyv[m, off:off + L, :].rearrange("(c p two) ch -> two p c ch", p=128, two=2)
            for par in range(2):
                S.dma(vt[:, par * NH:(par + 1) * NH, :], hsrc(0)[par], reads=[G.t_hyv], writes=[t_vt])
            mo = G.mixT[768:1024, :].rearrange("(c p) t -> p c t", p=128)
            for order in range(2):
                src, t_src = (vt, t_vt) if order == 0 else (zz1, t_zz1)
                for kc in range(KC):
                    b = kc % 2
                    S.dma(fsl[:, b], FT[kc], writes=[t_fsl[b]])
                    S.dma(kft[:, b], Kf[order, kc], reads=[t_kf], writes=[t_kft2[b]])
                    for eo_ in range(2):
                        for ri in range(2):
                            for c in range(NH):
                                cc = eo_ * NH + c
                                S.op("pe", lambda e: e.matmul(psx[:, b, eo_ * 2 + ri, :], lhsT=fsl[:, b, cc, ri, :], rhs=src[:, cc, :], start=(c == 0), stop=(c == NH - 1)),
                                     reads=[t_fsl[b], t_src], writes=[t_psx[b]])
                    S.op("act", lambda e: e.copy(out=xo[:], in_=psx[:, b, 2:4, :]), reads=[t_psx[b]], writes=[t_xo])
                    S.op("dve", lambda e: e.tensor_tensor(out=xs_[:, 0], in0=psx[:, b, 0:2, :], in1=xo[:], op=ALU.add), reads=[t_psx[b], t_xo], writes=[t_xs])
                    S.op("dve", lambda e: e.tensor_tensor(out=xs_[:, 1, 0, :], in0=psx[:, b, 0, :], in1=xo[:, 0, :], op=ALU.subtract), reads=[t_psx[b], t_xo], writes=[t_xs])
                    S.op("dve", lambda e: e.scalar_tensor_tensor(out=xs_[:, 1, 1, :], in0=psx[:, b, 1, :], scalar=-1.0, in1=xo[:, 1, :], op0=ALU.mult, op1=ALU.add),
                         reads=[t_psx[b], t_xo], writes=[t_xs])
                    S.op("dve", lambda e: e.tensor_tensor(out=ta[:, 0:2, :], in0=xs_[:, :, 0, :], in1=kft[:, b, :, 0, :], op=ALU.mult), reads=[t_xs, t_kft2[b]], writes=[t_ta])
                    S.op("pool", lambda e: e.tensor_tensor(out=ta[:, 2:4, :], in0=xs_[:, :, 1, :], in1=kft[:, b, :, 1, :], op=ALU.mult), reads=[t_xs, t_kft2[b]], writes=[t_ta])
                    S.op("dve", lambda e: e.tensor_tensor(out=yl[:, :, 0, :], in0=ta[:, 0:2, :], in1=ta[:, 2:4, :], op=ALU.subtract), reads=[t_ta], writes=[t_yl])
                    S.op("dve", lambda e: e.tensor_tensor(out=ta[:, 0:2, :], in0=xs_[:, :, 0, :], in1=kft[:, b, :, 1, :], op=ALU.mult), reads=[t_xs, t_kft2[b], t_yl], writes=[t_ta])
                    S.op("pool", lambda e: e.tensor_tensor(out=ta[:, 2:4, :], in0=xs_[:, :, 1, :], in1=kft[:, b, :, 0, :], op=ALU.mult), reads=[t_xs, t_kft2[b], t_yl], writes=[t_ta])
                    S.op("dve", lambda e: e.tensor_tensor(out=yl[:, :, 1, :], in0=ta[:, 0:2, :], in1=ta[:, 2:4, :], op=ALU.add), reads=[t_ta], writes=[t_yl])
                    S.op("dve", lambda e: e.tensor_tensor(out=Y[:, 0, kc, 0, :], in0=yl[:, 0, 0, :], in1=yl[:, 1, 0, :], op=ALU.add), reads=[t_yl], writes=[t_Y])
                    S.op("pool", lambda e: e.tensor_tensor(out=Y[:, 0, kc, 1, :], in0=yl[:, 0, 1, :], in1=yl[:, 1, 1, :], op=ALU.subtract), reads=[t_yl], writes=[t_Y])
                    S.op("dve", lambda e: e.tensor_tensor(out=Y[:, 1, kc, 0, :], in0=yl[:, 0, 0, :], in1=yl[:, 1, 0, :], op=ALU.subtract), reads=[t_yl], writes=[t_Y])
                    S.op("pool", lambda e: e.tensor_tensor(out=Y[:, 1, kc, 1, :], in0=yl[:, 0, 1, :], in1=yl[:, 1, 1, :], op=ALU.add), reads=[t_yl], writes=[t_Y])
                oi = 0
                for c2 in range(NH):
                    for par in range(2):
                        cc = par * NH + c2
                        b = oi % 2
                        oi += 1
                        zb = c2 % 2
                        S.dma(isl[:, b], IT[cc], writes=[t_isl[b]])
                        S.dma(xg[:, b], hsrc(1 + order)[par, :, c2, :], reads=[G.t_hyv], writes=[t_xg[b]])
                        for kc in range(KC):
                            for ri in range(2):
                                S.op("pe", lambda e: e.matmul(psy[:, b, 0:256], lhsT=isl[:, b, kc, ri, :], rhs=Y[:, par, kc, ri, :],
                                                              start=(kc == 0 and ri == 0), stop=(kc == KC - 1 and ri == 1)), reads=[t_isl[b], t_Y], writes=[t_psy[b]])
                        if order == 0:
                            S.op("dve", lambda e: e.tensor_tensor(out=zz1[:, cc, :], in0=psy[:, b, 0:256], in1=xg[:, b, :], op=ALU.mult),
                                 reads=[t_psy[b], t_xg[b]], writes=[t_zz1])
                        else:
                            S.op("dve", lambda e: e.tensor_tensor(out=zt[:], in0=psy[:, b, 0:256], in1=xg[:, b, :], op=ALU.mult),
                                 reads=[t_psy[b], t_xg[b]], writes=[t_zt])
                            for hh in range(2):
                                S.op("pe", lambda e: e.transpose(out=ptr[:, hh, :], in_=zt[:, hh * 128:(hh + 1) * 128], identity=G.identB),
                                     reads=[t_zt, G.t_c], writes=[t_ptr])
                            S.op("act", lambda e: e.copy(out=zT[:, zb].rearrange("p h (t two) -> p h t two", two=2)[:, :, :, par], in_=ptr[:]),
                                 reads=[t_ptr], writes=[t_zT[zb]])
                            if par == 1:
                                S.dma(mo[:, :, off + c2 * 256:off + (c2 + 1) * 256], zT[:, zb], reads=[t_zT[zb]], writes=[G.t_mix])
            S.barrier()
    if "hy" in G.dbg and li == 0:
        dump_bf16(G, G.mixT[768:896, 256:768], G.dbg["hy"], [G.t_mix])


def _hy_tables(L):
    N = 2 * L
    NCk = L // 128
    KC = (L // 2 + 1 + 127) // 128
    perm = np.concatenate([np.arange(0, L, 2), np.arange(1, L, 2)])
    n = perm.astype(np.int64)
    k = np.arange(KC * 128, dtype=np.int64)
    ang = ((n[:, None] * k[None, :]) % N).astype(np.float64) * (2 * np.pi / N)
    valid = (k <= L // 2).astype(np.float64)
    w = np.where(k == 0, 1.0, 2.0) / N * valid
    c, s_ = np.cos(ang), np.sin(ang)
    ft = np.stack([c * valid, -s_ * valid], axis=0)
    ft = ft.reshape(2, NCk, 128, KC, 128).transpose(3, 2, 1, 0, 4)
    it = np.stack([c * w, -s_ * w], axis=0)
    it = it.reshape(2, NCk, 128, KC, 128).transpose(1, 4, 3, 0, 2)
    f = np.float32
    nn = np.arange(L, dtype=f)
    t = nn / f(max(L - 1, 1))
    bands = np.linspace(1e-4, 15, 16, dtype=f)
    wpos = (f(2 * math.pi / L) * nn).astype(f)
    feats = np.concatenate([t[:, None], np.cos(wpos[:, None] * bands), -np.sin(wpos[:, None] * bands)], axis=-1).astype(f)
    deltas = np.abs(np.linspace(math.log(1e-2) / 1.5, math.log(1e-2) / 0.3, 256, dtype=f))
    win = np.exp(-t[:, None] * deltas).astype(f)
    winb = win.copy()
    winb[0] = 0.0
    feats = feats[perm]
    wn = np.stack([win, winb], axis=1)[perm].reshape(NCk, 128, 2, 256)
    mh = ((k != L // 2) & (k <= L // 2)).astype(f).reshape(KC, 128).T
    bf = ml_dtypes.bfloat16
    return (np.ascontiguousarray(ft).astype(bf), np.ascontiguousarray(it).astype(bf),
            np.ascontiguousarray(feats.T), np.ascontiguousarray(wn), np.ascontiguousarray(mh))
```

```python
import math
from contextlib import ExitStack
import numpy as np
import ml_dtypes
import concourse.bass as bass
import concourse.mybir as mybir
from concourse.bass_utils import run_bass_kernel_spmd

F32 = mybir.dt.float32
BF16 = mybir.dt.bfloat16
AF = mybir.ActivationFunctionType
ALU = mybir.AluOpType
AX = mybir.AxisListType

D = 1024
NCTX = 256
NLAT = 4096
NTOK = NCTX + NLAT
NT = NTOK // 128
DEPTH = 2
D_IN = 2444
EPS = 1e-6
HC = NTOK + 3
NE = 16
DFF = 256
DEBUG = False


def colof(tile):
    return 1 + 128 * tile if tile < 2 else 258 + 128 * (tile - 2)


BLOCKS = [(1, 0, 256, 0, 2)] + [(258 + 512 * j, 256 + 512 * j, 512, 2 + 4 * j, 4) for j in range(8)]


class Tok:
    __slots__ = ("w", "r")

    def __init__(self):
        self.w = None
        self.r = {}


class Sched:
    def __init__(self, nc, ndma=8, same_engine_sync=True):
        self.nc = nc
        self.eng = {"pe": nc.tensor, "act": nc.scalar, "dve": nc.vector, "pool": nc.gpsimd, "sp": nc.sync}
        self.semh = {}
        self.cnt = {}
        self.seen = {k: {} for k in self.eng}
        self.same = same_engine_sync
        for k in self.eng:
            self.semh[k] = nc.alloc_semaphore("s_" + k)
            self.cnt[k] = 0
        self.ndma = ndma
        self.dslot = {}
        self.dval = {}
        for q in ("sp", "pool"):
            self.dslot[q] = 0
            for i in range(ndma):
                key = ("dma", q, i)
                self.semh[key] = nc.alloc_semaphore("d_%s_%d" % (q, i))
                self.dval[key] = 0
        self.ninst = 0

    def _wait(self, e, deps):
        for (k, v) in sorted(deps, key=str):
            if k == e and (e == "pe" or not self.same):
                continue
            if self.seen[e].get(k, 0) >= v:
                continue
            self.eng[e].wait_ge(self.semh[k], v)
            self.seen[e][k] = v

    @staticmethod
    def _deps(reads, writes):
        deps = set()
        for t in reads:
            if t.w is not None:
                deps.add(t.w)
        for t in writes:
            if t.w is not None:
                deps.add(t.w)
            for kv in t.r.items():
                deps.add(kv)
        return deps

    @staticmethod
    def _mark(ev, reads, writes):
        k, v = ev
        for t in reads:
            if t.r.get(k, 0) < v:
                t.r[k] = v
        for t in writes:
            t.w = ev
            t.r = {}

    def op(self, e, fn, reads=(), writes=()):
        self._wait(e, self._deps(reads, writes))
        ins = fn(self.eng[e])
        self.cnt[e] += 1
        ins.then_inc(self.semh[e], 1)
        self._mark((e, self.cnt[e]), reads, writes)
        self.ninst += 1
        return ins

    def dma(self, out, in_, reads=(), writes=(), q="sp", **kw):
        i = self.dslot[q]
        self.dslot[q] = (i + 1) % self.ndma
        key = ("dma", q, i)
        deps = self._deps(reads, writes)
        if self.dval[key] > 0:
            deps.add((key, self.dval[key]))
        self._wait(q, deps)
        ins = self.eng[q].dma_start(out=out, in_=in_, **kw)
        self.dval[key] += 16
        ins.then_inc(self.semh[key], 16)
        self._mark((key, self.dval[key]), reads, writes)
        self.ninst += 1
        return ins

    def barrier(self):
        deps = set()
        for key, v in self.dval.items():
            if v > 0:
                deps.add((key, v))
        for k in self.eng:
            if self.cnt[k] > 0:
                deps.add((k, self.cnt[k]))
        for e in self.eng:
            self._wait(e, deps)


class Ctx:
    pass


_UID = [0]


def SB(nc, name, shape, dt):
    _UID[0] += 1
    return nc.sbuf_tensor("%s_%d" % (name, _UID[0]), shape, dt)


def PS(nc, name, shape, dt):
    _UID[0] += 1
    return nc.psum_tensor("%s_%d" % (name, _UID[0]), shape, dt)


def build(dbg=None):
    nc = bass.Bass("TRN2", target_bir_lowering=False)
    S = Sched(nc)
    G = Ctx()
    G.nc, G.S = nc, S

    def din(name, shape, dt=F32):
        return nc.dram_tensor(name, list(shape), dt, kind="ExternalInput").ap()

    def dscr(name, shape, dt):
        return nc.dram_tensor(name, list(shape), dt, kind="Internal").ap()

    I = {}
    I["x"] = din("x", [NLAT, D])
    I["ctx"] = din("ctx", [NCTX, D])
    I["w_mod"] = din("w_mod", [DEPTH, D, 6 * D])
    I["b_mod"] = din("b_mod", [DEPTH, 6 * D])
    I["w_in"] = din("w_in", [DEPTH, D, D_IN])
    I["w_out"] = din("w_out", [DEPTH, D, D])
    I["w_router"] = din("w_router", [D, NE])
    I["router_bias"] = din("router_bias", [NE])
    I["w_gate"] = din("w_gate", [DEPTH, NE, D, DFF])
    I["w_up"] = din("w_up", [DEPTH, NE, D, DFF])
    I["w_down"] = din("w_down", [DEPTH, NE, DFF, D])
    I["g_final"] = din("g_final", [D])
    I["cols"] = din("cols", [DEPTH, 128, NCOLS])
    I["cmat"] = din("cmat", [9, 128, 128])
    I["rope"] = din("rope", [2, 128, NLAT])
    for sname, P in SEGS.items():
        I["ft_" + sname] = din("ft_" + sname, [P["KC"], 128, P["NC"], 2, 128], BF16)
        I["it_" + sname] = din("it_" + sname, [P["NC"], 128, P["KC"], 2, 128], BF16)
        I["fe_" + sname] = din("fe_" + sname, [33, P["L"]])
        I["win_" + sname] = din("win_" + sname, [P["NC"], 128, 2, 256])
        I["mh_" + sname] = din("mh_" + sname, [128, P["KC"]])
    for nm, shp in (("hy_conv_w", [DEPTH, 3, 768]), ("hy_conv_b", [DEPTH, 768]), ("hy_w1", [DEPTH, 33, 64]), ("hy_w2", [DEPTH, 64, 64]),
                    ("hy_w3", [DEPTH, 64, 1024]), ("hy_bias", [DEPTH, 2, 256])):
        I[nm] = din(nm, shp)
    I["ssdmask"] = din("ssdmask", [2, 4, 128, 512], BF16)
    for nm, shp in (("ssd_conv_w", [DEPTH, 3, 640]), ("ssd_dt_bias", [DEPTH, 2, 6]), ("ssd_a_log", [DEPTH, 2, 6]),
                    ("ssd_d", [DEPTH, 6]), ("ssd_norm", [DEPTH, 384])):
        I[nm] = din(nm, shp)
    out = nc.dram_tensor("out", [NLAT, D], F32, kind="ExternalOutput").ap()
    G.I, G.out = I, out
    G.dbg = {}
    if dbg:
        for name, shape in dbg.items():
            G.dbg[name] = nc.dram_tensor("dbg_" + name, list(shape), F32, kind="ExternalOutput").ap()

    G.xres = dscr("xres", [NTOK, D], F32)
    G.t_xres = [Tok() for _ in range(NT)]
    G.wb_in = [dscr("wb_in%d" % i, [D, D_IN], BF16) for i in range(DEPTH)]
    G.wb_out = [dscr("wb_out%d" % i, [D, D], BF16) for i in range(DEPTH)]
    G.wb_gate = [dscr("wb_gate%d" % i, [NE, D, DFF], BF16) for i in range(DEPTH)]
    G.wb_up = [dscr("wb_up%d" % i, [NE, D, DFF], BF16) for i in range(DEPTH)]
    G.wb_down = [dscr("wb_down%d" % i, [NE, DFF, D], BF16) for i in range(DEPTH)]
    G.t_wb = Tok()
    G.mixT = dscr("mixT", [D, NTOK], BF16)
    G.hyv = dscr("hyv", [3, NTOK, 256], BF16)
    G.ssd_yb = dscr("ssd_yb", [NTOK, 384], F32)
    G.t_ssdyb = [Tok() for _ in range(NT)]
    G.t_hyv = Tok()
    G.Kf = {sn: dscr("Kf_" + sn, [2, P["KC"], 128, 2, 2, 256], BF16) for sn, P in SEGS.items()}
    G.t_mix = Tok()

    cm = nc.alloc_sbuf_tensor("cm", [128, 9, 128], F32)
    cmb = nc.alloc_sbuf_tensor("cmb", [128, 9, 128], BF16)
    ones = nc.alloc_sbuf_tensor("ones", [128, 128], F32)
    epsc = nc.alloc_sbuf_tensor("epsc", [128, 1], F32)
    G.t_c = Tok()
    S.dma(cm[:], I["cmat"].rearrange("a p c -> p a c"), writes=[G.t_c])
    S.op("dve", lambda e: e.tensor_copy(out=cmb[:], in_=cm[:]), reads=[G.t_c], writes=[G.t_c])
    S.op("dve", lambda e: e.memset(ones[:], 1.0), writes=[G.t_c])
    S.op("dve", lambda e: e.memset(epsc[:], EPS), writes=[G.t_c])
    G.cm, G.cmb, G.ones, G.epsc = cm, cmb, ones, epsc
    G.negpi = nc.alloc_sbuf_tensor("negpi", [128, 1], F32)
    S.op("dve", lambda e: e.memset(G.negpi[:], -math.pi), writes=[G.t_c])
    G.identF, G.identB = cm[:, 0, :], cmb[:, 0, :]

    S.dma(G.xres[0:NCTX, :], I["ctx"][:, :], writes=G.t_xres[0:2])
    for j in range(4):
        S.dma(G.xres[NCTX + 1024 * j:NCTX + 1024 * (j + 1), :], I["x"][1024 * j:1024 * (j + 1), :],
              writes=G.t_xres[2 + 8 * j:2 + 8 * (j + 1)])

    convert_weights(G)
    S.barrier()
    for li in range(1 if DEBUG else DEPTH):
        layer(G, li)
    S.barrier()
    return nc


def convert_weights(G):
    nc, S, I = G.nc, G.S, G.I
    CH = 4096
    with ExitStack() as _es:
        cf = _es.enter_context(SB(nc, "cv_f", [128, 2, CH], F32))
        cb = _es.enter_context(SB(nc, "cv_b", [128, 2, CH], BF16))
        tf = [Tok(), Tok()]
        tb = [Tok(), Tok()]
        n = 0
        engs = ["dve", "pool", "act"]
        for li in range(DEPTH):
            pairs = [(I["w_in"][li], G.wb_in[li], "a b -> (a b)"), (I["w_out"][li], G.wb_out[li], "a b -> (a b)"),
                     (I["w_gate"][li], G.wb_gate[li], "e a b -> (e a b)"), (I["w_up"][li], G.wb_up[li], "e a b -> (e a b)"),
                     (I["w_down"][li], G.wb_down[li], "e a b -> (e a b)")]
            for src, dst, pat in pairs:
                s1 = src.rearrange(pat).rearrange("(p m) -> p m", p=128)
                d1 = dst.rearrange(pat).rearrange("(p m) -> p m", p=128)
                M = s1.shape[1]
                for c0 in range(0, M, CH):
                    w = min(CH, M - c0)
                    k = n % 2
                    S.dma(cf[:, k, 0:w], s1[:, c0:c0 + w], writes=[tf[k]])
                    en = engs[n % 3]
                    if en == "act":
                        S.op("act", lambda e: e.copy(out=cb[:, k, 0:w], in_=cf[:, k, 0:w]), reads=[tf[k]], writes=[tb[k]])
                    else:
                        S.op(en, lambda e: e.tensor_copy(out=cb[:, k, 0:w], in_=cf[:, k, 0:w]), reads=[tf[k]], writes=[tb[k]])
                    S.dma(d1[:, c0:c0 + w], cb[:, k, 0:w], reads=[tb[k]], writes=[G.t_wb], q="pool")
                    n += 1


COLS = {}
_o = 0
for _name, _n in [("cc", 16), ("bmod", 32), ("g_mix", 8), ("g_ffn", 8), ("ssd_conv_b", 5), ("qg", 1), ("kg", 1),
                  ("ssd_d", 3), ("ssd_norm", 3), ("hy_b1", 1), ("hy_freq", 1), ("hy_b2", 1)]:
    COLS[_name] = (_o, _n)
    _o += _n
NCOLS = _o


def layer(G, li):
    nc, S, I = G.nc, G.S, G.I
    with ExitStack() as _es:
        cols = _es.enter_context(SB(nc, "cols", [128, NCOLS], F32))
        modc = _es.enter_context(SB(nc, "modc", [128, 4, 8, 2], F32))
        gtb = _es.enter_context(SB(nc, "gtb", [128, 2, 2, D], F32))
        G.cols, G.modc, G.gtb = cols, modc, gtb
        G.t_cols, G.t_modc, G.t_gtb = Tok(), Tok(), Tok()
        S.dma(cols[:], I["cols"][li], writes=[G.t_cols])
        adaln(G, li)
        S.barrier()
        with ExitStack() as _es:
            hT = _es.enter_context(SB(nc, "hT", [128, 8, HC], BF16))
            G.hT, G.t_hT = hT, Tok()
            norm_in(G, li)
            S.barrier()
            if "hy" in STAGES:
                hyena_inproj(G, li)
                S.barrier()
            if "att" in STAGES:
                attention(G, li)
                S.barrier()
            if "ssd" in STAGES:
                ssd(G, li)
                S.barrier()
        if "hy" in STAGES:
            hyena_fft(G, li)
            S.barrier()
        if "moe" in STAGES:
            with ExitStack() as _es:
                h2T = _es.enter_context(SB(nc, "h2T", [128, 8, NTOK], BF16))
                rl = _es.enter_context(SB(nc, "rl", [128, NT, NE], F32))
                G.h2T, G.t_h2T, G.rl, G.t_rl = h2T, Tok(), rl, Tok()
                outproj(G, li)
                S.barrier()
                moe(G, li)
                S.barrier()


STAGES = ("att", "ssd", "hy", "moe")


def colap(G, name, j=0, n=1, p0=0, p1=128):
    o, _ = COLS[name]
    return G.cols[p0:p1, o + j:o + j + n]


def adaln(G, li):
    nc, S, I = G.nc, G.S, G.I
    cols, modc, gtb = G.cols, G.modc, G.gtb
    with ExitStack() as _es:
        sc = _es.enter_context(SB(nc, "sc", [128, 8, 2], F32))
        screp = _es.enter_context(SB(nc, "screp", [128, 8, 2, 128], F32))
        wm = _es.enter_context(SB(nc, "wm", [128, 2, 8, 512], F32))
        brow = _es.enter_context(SB(nc, "brow", [128, 2, D], F32))
        wmb = _es.enter_context(SB(nc, "wmb", [128, 8, 512], BF16))
        screpb = _es.enter_context(SB(nc, "screpb", [128, 8, 2, 128], BF16))
        t_wmb = Tok()
        ps_a = _es.enter_context(PS(nc, "ps_a", [128, 4, 2], F32))
        ps_g = _es.enter_context(PS(nc, "ps_g", [128, 2, 512], F32))
        t_sc, t_wm, t_pa, t_pg, t_brow = Tok(), [Tok(), Tok()], Tok(), Tok(), Tok()
        o = COLS["cc"][0]
        S.op("act", lambda e: e.activation(out=sc[:].rearrange("p k j -> p (k j)"), in_=cols[:, o:o + 16], func=AF.Silu),
             reads=[G.t_cols], writes=[t_sc])
        S.op("dve", lambda e: e.tensor_copy(out=screp[:].rearrange("p k j c -> p (k j) c"),
                                            in_=sc[:].rearrange("p k j -> p (k j)").unsqueeze(2).to_broadcast([128, 16, 128])),
             reads=[t_sc], writes=[t_sc])
        S.op("pool", lambda e: e.tensor_copy(out=screpb[:], in_=screp[:]), reads=[t_sc], writes=[t_sc])
        for g in range(2):
            S.dma(brow[:, g, :], I["b_mod"][li, (2 + 3 * g) * D:(3 + 3 * g) * D].partition_broadcast(128), writes=[t_brow])
        wv = I["w_mod"][li].rearrange("(k p) c -> p k c", p=128)
        ob = COLS["bmod"][0]
        for cj in range(12):
            b = cj % 2
            S.dma(wm[:, b], wv[:, :, cj * 512:(cj + 1) * 512], writes=[t_wm[b]])
            vec = cj // 2
            half = cj % 2
            if vec in (2, 5):
                g = 0 if vec == 2 else 1
                S.op("dve", lambda e: e.tensor_copy(out=wmb[:], in_=wm[:, b]), reads=[t_wm[b]], writes=[t_wmb])
                for j in range(2):
                    for kd in range(8):
                        S.op("pe", lambda e: e.matmul(ps_g[:, j, :], lhsT=screpb[:, kd, j, :], rhs=wmb[:, kd, :],
                                                      start=(kd == 0), stop=(kd == 7)), reads=[t_sc, t_wmb], writes=[t_pg])
                    S.op("dve", lambda e: e.tensor_tensor(out=gtb[:, g, j, half * 512:(half + 1) * 512], in0=ps_g[:, j, :],
                                                          in1=brow[:, g, half * 512:(half + 1) * 512], op=ALU.add),
                         reads=[t_pg, t_brow], writes=[G.t_gtb])
            else:
                v = {0: 0, 1: 1, 3: 2, 4: 3}[vec]
                for fc in range(4):
                    for kd in range(8):
                        S.op("pe", lambda e: e.matmul(ps_a[:, fc, :], lhsT=wm[:, b, kd, fc * 128:(fc + 1) * 128], rhs=sc[:, kd, :],
                                                      start=(kd == 0), stop=(kd == 7)), reads=[t_sc, t_wm[b]], writes=[t_pa])
                k0 = half * 4
                S.op("dve", lambda e: e.tensor_tensor(out=modc[:, v, k0:k0 + 4, :], in0=ps_a[:],
                                                      in1=cols[:, ob + v * 8 + k0:ob + v * 8 + k0 + 4].unsqueeze(2).to_broadcast([128, 4, 2]),
                                                      op=ALU.add), reads=[t_pa, G.t_cols], writes=[G.t_modc])
        for v, gname in ((1, "g_mix"), (3, "g_ffn")):
            og = COLS[gname][0]
            S.op("dve", lambda e: e.scalar_tensor_tensor(out=modc[:, v], in0=modc[:, v], scalar=1.0,
                                                         in1=cols[:, og:og + 8].unsqueeze(2).to_broadcast([128, 8, 2]),
                                                         op0=ALU.add, op1=ALU.mult), reads=[G.t_modc, G.t_cols], writes=[G.t_modc])


def rms_to_T(G, xt, t_x, tile, vA, vB, dstT, t_dst, dcol, pool):
    nc, S = G.nc, G.S
    sq, ss, xn, ps_t, toks = pool
    t_sq, t_ss, t_xn, t_ps = toks
    j = 1 if tile < 2 else 0
    S.op("act", lambda e: e.activation(out=sq[:], in_=xt, func=AF.Square, accum_out=ss[:, 0:1]), reads=[t_x], writes=[t_sq, t_ss])
    S.op("act", lambda e: e.activation(out=ss[:, 1:2], in_=ss[:, 0:1], func=AF.Sqrt, bias=G.epsc[:], scale=1.0 / D),
         reads=[t_ss, G.t_c], writes=[t_ss])
    S.op("dve", lambda e: e.reciprocal(out=ss[:, 2:3], in_=ss[:, 1:2]), reads=[t_ss], writes=[t_ss])
    S.op("dve", lambda e: e.tensor_scalar(out=xn[:], in0=xt, scalar1=ss[:, 2:3], scalar2=None, op0=ALU.mult),
         reads=[t_x, t_ss], writes=[t_xn])
    for k in range(8):
        S.op("pe", lambda e: e.transpose(out=ps_t[:, k, :], in_=xn[:, k * 128:(k + 1) * 128], identity=G.identB),
             reads=[t_xn, G.t_c], writes=[t_ps])
    S.op("dve", lambda e: e.tensor_tensor(out=sq[:].rearrange("p (k c) -> p k c", k=8), in0=ps_t[:],
                                          in1=G.modc[:, vA, :, j:j + 1].to_broadcast([128, 8, 128]), op=ALU.mult),
         reads=[t_ps, G.t_modc], writes=[t_sq])
    S.op("dve", lambda e: e.tensor_tensor(out=dstT[:, :, dcol:dcol + 128], in0=sq[:].rearrange("p (k c) -> p k c", k=8),
                                          in1=G.modc[:, vB, :, j:j + 1].to_broadcast([128, 8, 128]), op=ALU.add),
         reads=[t_sq, G.t_modc], writes=[t_dst])


def norm_in(G, li):
    nc, S = G.nc, G.S
    hT = G.hT
    with ExitStack() as _es:
        xt = _es.enter_context(SB(nc, "xt", [128, 2, D], F32))
        sq = _es.enter_context(SB(nc, "sq", [128, 2, D], F32))
        ss = _es.enter_context(SB(nc, "ss", [128, 2, 4], F32))
        xn = _es.enter_context(SB(nc, "xn", [128, 2, D], BF16))
        ps_t = _es.enter_context(PS(nc, "ps_t", [128, 2, 8, 128], BF16))
        t_x = [Tok(), Tok()]
        pools = [(sq[:, i, :], ss[:, i, :], xn[:, i, :], ps_t[:, i], (Tok(), Tok(), Tok(), Tok())) for i in range(2)]
        for c in (0, 257, HC - 1):
            S.op("pool", lambda e: e.memset(hT[:, :, c:c + 1], 0.0), writes=[G.t_hT])
        for tile in range(NT):
            b = tile % 2
            S.dma(xt[:, b, :], G.xres[tile * 128:(tile + 1) * 128, :], reads=[G.t_xres[tile]], writes=[t_x[b]])
            rms_to_T(G, xt[:, b, :], t_x[b], tile, 1, 0, hT, G.t_hT, colof(tile), pools[b])
        if "hT" in G.dbg and li == 0:
            with ExitStack() as _es:
                dh = _es.enter_context(SB(nc, "dbgh", [128, 8, 512], F32))
                t = Tok()
                S.op("dve", lambda e: e.tensor_copy(out=dh[:], in_=hT[:, :, 0:512]), reads=[G.t_hT], writes=[t])
                S.dma(G.dbg["hT"].rearrange("(k p) c -> p k c", p=128), dh[:], reads=[t])


def attention(G, li):
    nc, S, I = G.nc, G.S, G.I
    hT = G.hT
    need_ctx = li < DEPTH - 1
    wv = G.wb_in[li].rearrange("(k p) c -> p k c", p=128)
    scale = 64 ** -0.5
    with ExitStack() as _es:
        wq = _es.enter_context(SB(nc, "wqkv", [128, 8, 640], BF16))
        qT = _es.enter_context(SB(nc, "qT", [128, 6, NTOK], BF16))
        kT = _es.enter_context(SB(nc, "kT", [128, NTOK], BF16))
        vp = _es.enter_context(SB(nc, "vp", [128, NT, 2, 128], BF16))
        rp = _es.enter_context(SB(nc, "rp", [128, 2, 2, 512], F32))
        qs = _es.enter_context(SB(nc, "qs", [128, 512], F32))
        q2 = _es.enter_context(SB(nc, "q2", [128, 512], F32))
        qn = _es.enter_context(SB(nc, "qn", [128, 512], F32))
        qnb = _es.enter_context(SB(nc, "qnb", [128, 512], BF16))
        pT = _es.enter_context(SB(nc, "pT", [128, 2, 2, 512], BF16))
        rd = _es.enter_context(SB(nc, "rd", [128, 2, 512], F32))
        ao = _es.enter_context(SB(nc, "ao", [128, 2, 512], BF16))
        ps_q = _es.enter_context(PS(nc, "ps_q", [128, 512], F32))
        ps_r = _es.enter_context(PS(nc, "ps_r", [128, 512], F32))
        ps_s = _es.enter_context(PS(nc, "ps_s", [128, 2, 2, 512], F32))
        ps_o = _es.enter_context(PS(nc, "ps_o", [128, 2, 512], F32))
        t_w, t_q, t_k, t_v, t_rp = Tok(), Tok(), Tok(), Tok(), [Tok(), Tok()]
        t_qs, t_q2, t_qn, t_qnb, t_psq, t_psr = Tok(), Tok(), Tok(), Tok(), Tok(), Tok()
        t_pT, t_pss, t_pso, t_rd, t_ao = [Tok(), Tok()], [Tok(), Tok()], [Tok(), Tok()], [Tok(), Tok()], [Tok(), Tok()]
        for j in range(3):
            S.dma(wq[:, :, j * 128:j * 128 + 64], wv[:, :, j * 64:(j + 1) * 64], reads=[G.t_wb], writes=[t_w])
            S.dma(wq[:, :, j * 128 + 64:(j + 1) * 128], wv[:, :, (3 + j) * 64:(4 + j) * 64], reads=[G.t_wb], writes=[t_w])
        S.dma(wq[:, :, 384:640], wv[:, :, 384:640], reads=[G.t_wb], writes=[t_w])
        S.op("pool", lambda e: e.memset(vp[:, :, :, 64:128], 1.0), writes=[t_v])
        S.op("pool", lambda e: e.memset(qT[64:128, 0:3, :], 0.0), writes=[t_q])
        S.op("pool", lambda e: e.memset(qT[0:64, 3:6, :], 0.0), writes=[t_q])
        og = {0: COLS["qg"][0], 1: COLS["qg"][0], 2: COLS["qg"][0], 3: COLS["kg"][0]}
        for bi, (c0, t0, n, tile0, ntile) in enumerate(BLOCKS):
            if bi > 0:
                b = bi % 2
                S.dma(rp[:, b, :, :], I["rope"][:, :, t0 - NCTX:t0 - NCTX + 512].rearrange("a p c -> p a c"), writes=[t_rp[b]])
            for ch in range(4):
                for k in range(8):
                    S.op("pe", lambda e: e.matmul(ps_q[:, 0:n], lhsT=wq[:, k, ch * 128:(ch + 1) * 128], rhs=hT[:, k, c0:c0 + n],
                                                  start=(k == 0), stop=(k == 7)), reads=[t_w, G.t_hT], writes=[t_psq])
                S.op("act", lambda e: e.copy(out=qs[:, 0:n], in_=ps_q[:, 0:n]), reads=[t_psq], writes=[t_qs])
                S.op("act", lambda e: e.activation(out=q2[:, 0:n], in_=qs[:, 0:n], func=AF.Square), reads=[t_qs], writes=[t_q2])
                S.op("pe", lambda e: e.matmul(ps_r[:, 0:n], lhsT=G.cm[:, 3, :], rhs=q2[:, 0:n], start=True, stop=True),
                     reads=[t_q2, G.t_c], writes=[t_psr])
                S.op("act", lambda e: e.activation(out=q2[:, 0:n], in_=ps_r[:, 0:n], func=AF.Sqrt, bias=G.epsc[:], scale=1.0 / 64),
                     reads=[t_psr, G.t_c], writes=[t_q2])
                S.op("dve", lambda e: e.reciprocal(out=q2[:, 0:n], in_=q2[:, 0:n]), reads=[t_q2], writes=[t_q2])
                S.op("dve", lambda e: e.scalar_tensor_tensor(out=qn[:, 0:n], in0=qs[:, 0:n], scalar=G.cols[:, og[ch]:og[ch] + 1],
                                                             in1=q2[:, 0:n], op0=ALU.mult, op1=ALU.mult),
                     reads=[t_qs, t_q2, G.t_cols], writes=[t_qn])
                t_dst = t_q if ch < 3 else t_k
                halves = [(0, 64, qT[0:64, ch, t0:t0 + n]), (64, 128, qT[64:128, 3 + ch, t0:t0 + n])] if ch < 3 else [(0, 128, kT[:, t0:t0 + n])]
                if bi == 0:
                    for (p0, p1, dst) in halves:
                        S.op("dve", lambda e: e.tensor_copy(out=dst, in_=qn[p0:p1, 0:n]), reads=[t_qn], writes=[t_dst])
                else:
                    b = bi % 2
                    S.op("dve", lambda e: e.tensor_copy(out=qnb[:, 0:n], in_=qn[:, 0:n]), reads=[t_qn], writes=[t_qnb])
                    S.op("pe", lambda e: e.matmul(ps_r[:, 0:n], lhsT=G.cmb[:, 4, :], rhs=qnb[:, 0:n], start=True, stop=True),
                         reads=[t_qnb, G.t_c], writes=[t_psr])
                    S.op("dve", lambda e: e.tensor_tensor(out=qs[:, 0:n], in0=ps_r[:, 0:n], in1=rp[:, b, 1, 0:n], op=ALU.mult),
                         reads=[t_psr, t_rp[b]], writes=[t_qs])
                    S.op("dve", lambda e: e.tensor_tensor(out=qn[:, 0:n], in0=qn[:, 0:n], in1=rp[:, b, 0, 0:n], op=ALU.mult),
                         reads=[t_qn, t_rp[b]], writes=[t_qn])
                    for (p0, p1, dst) in halves:
                        S.op("dve", lambda e: e.tensor_tensor(out=dst, in0=qn[p0:p1, 0:n], in1=qs[p0:p1, 0:n], op=ALU.add),
                             reads=[t_qn, t_qs], writes=[t_dst])
            for tl in range(tile0, tile0 + ntile):
                cc = colof(tl)
                for k in range(8):
                    S.op("pe", lambda e: e.matmul(ps_q[:, 0:128], lhsT=hT[:, k, cc:cc + 128], rhs=wq[:, k, 512:640],
                                                  start=(k == 0), stop=(k == 7)), reads=[t_w, G.t_hT], writes=[t_psq])
                S.op("act", lambda e: e.copy(out=vp[:, tl, :, 0:64], in_=ps_q[:, 0:128].rearrange("p (a d) -> p a d", a=2)),
                     reads=[t_psq], writes=[t_v])
        pairs = []
        oi = 0
        for h in range(6):
            for bi, (c0, t0, n, tile0, ntile) in enumerate(BLOCKS):
                if bi == 0 and not need_ctx:
                    continue
                kcs = list(range(2)) if bi == 0 else list(range(NT))
                for ki in range(0, len(kcs), 2):
                    pairs.append((h, t0, n, kcs[ki], ki == 0, ki + 2 >= len(kcs), oi % 2))
                oi += 1

        def qk(j):
            h, t0, n, kc, first, last, ob = pairs[j]
            pb = (h // 3) * 64
            sb = j % 2
            for u in range(2):
                S.op("pe", lambda e: e.matmul(ps_s[:, sb, u, 0:n], lhsT=kT[:, (kc + u) * 128:(kc + u + 1) * 128],
                                              rhs=qT[:, h, t0:t0 + n], start=True, stop=True),
                     reads=[t_q, t_k], writes=[t_pss[sb]])

        qk(0)
        for j, (h, t0, n, kc, first, last, ob) in enumerate(pairs):
            sb = j % 2
            kv = h // 3
            if j + 1 < len(pairs):
                qk(j + 1)
            S.op("act", lambda e: e.activation(out=pT[:, sb, :, 0:n], in_=ps_s[:, sb, :, 0:n], func=AF.Exp, scale=scale),
                 reads=[t_pss[sb]], writes=[t_pT[sb]])
            for u in range(2):
                S.op("pe", lambda e: e.matmul(ps_o[:, ob, 0:n], lhsT=vp[:, kc + u, kv, :], rhs=pT[:, sb, u, 0:n],
                                              start=(first and u == 0), stop=(last and u == 1)),
                     reads=[t_v, t_pT[sb]], writes=[t_pso[ob]])
            if last:
                S.op("dve", lambda e: e.reciprocal(out=rd[0:64, ob, 0:n], in_=ps_o[64:128, ob, 0:n]), reads=[t_pso[ob]], writes=[t_rd[ob]])
                S.op("dve", lambda e: e.tensor_tensor(out=ao[0:64, ob, 0:n], in0=ps_o[0:64, ob, 0:n], in1=rd[0:64, ob, 0:n], op=ALU.mult),
                     reads=[t_pso[ob], t_rd[ob]], writes=[t_ao[ob]])
                S.dma(G.mixT[h * 64:(h + 1) * 64, t0:t0 + n], ao[0:64, ob, 0:n], reads=[t_ao[ob]], writes=[G.t_mix])
        if "att" in G.dbg and li == 0:
            dump_bf16(G, G.mixT[0:128, 256:768], G.dbg["att"], [G.t_mix])


def dump_bf16(G, src, dst, reads):
    nc, S = G.nc, G.S
    p, n = src.shape
    with ExitStack() as _es:
        a = _es.enter_context(SB(nc, "dmpb", [p, n], BF16))
        b = _es.enter_context(SB(nc, "dmpf", [p, n], F32))
        t = Tok()
        S.dma(a[:], src, reads=reads, writes=[t])
        S.op("dve", lambda e: e.tensor_copy(out=b[:], in_=a[:]), reads=[t], writes=[t])
        S.dma(dst, b[:], reads=[t])
        S.barrier()


def _cols_pack(inp, li, b):
    def colform(v, n):
        return np.ascontiguousarray(v.reshape(n, 128).T)
    parts = {}
    cc = np.zeros((128, 8, 2), np.float32)
    cc[:, :, 0] = colform(inp["c"][b], 8)
    cc[:, :, 1] = colform(inp["c_ctx"], 8)
    parts["cc"] = cc.reshape(128, 16)
    bm = inp["b_mod"][li].reshape(6, 8, 128)
    parts["bmod"] = np.concatenate([bm[v].T for v in (0, 1, 3, 4)], axis=1)
    parts["g_mix"] = colform(inp["g_mix"][li], 8)
    parts["g_ffn"] = colform(inp["g_ffn"][li], 8)
    parts["ssd_conv_b"] = colform(inp["ssd_conv_b"][li], 5)
    parts["qg"] = np.tile(inp["q_norm"][li], 2)[:, None]
    parts["kg"] = np.tile(inp["k_norm"][li], 2)[:, None]
    parts["ssd_d"] = colform(np.repeat(inp["ssd_d"][li], 64), 3)
    parts["ssd_norm"] = colform(inp["ssd_norm"][li], 3)
    for nm in ("hy_b1", "hy_freq", "hy_b2"):
        parts[nm] = np.tile(inp[nm][li], 2)[:, None]
    out = np.zeros((128, NCOLS), np.float32)
    for nm, (o, n) in COLS.items():
        out[:, o:o + n] = parts[nm]
    return out


def _consts():
    ident = np.eye(128, dtype=np.float32)
    s = np.arange(128)
    U = (s[:, None] <= s[None, :]).astype(np.float32)
    Lo = (s[:, None] >= s[None, :]).astype(np.float32)
    bo = np.kron(np.eye(2, dtype=np.float32), np.ones((64, 64), np.float32))
    rot = np.zeros((128, 128), np.float32)
    for hb in (0, 64):
        for d in range(32):
            rot[hb + d + 32, hb + d] = -1.0
            rot[hb + d, hb + d + 32] = 1.0
    top = np.zeros((128, 128), np.float32); top[:64] = 1.0
    bot = np.zeros((128, 128), np.float32); bot[64:] = 1.0
    cmat = np.stack([ident, U, Lo, bo, rot, top, bot, U - ident, Lo - ident])
    rows = NLAT // 64
    row = np.repeat(np.arange(rows), 64).astype(np.float32)
    col = np.tile(np.arange(64), rows).astype(np.float32)
    inv = (10000.0 ** (-np.arange(0, 32, 2, dtype=np.float32) / 32)).astype(np.float32)
    ang = np.concatenate([row[:, None] * inv, col[:, None] * inv], axis=-1).astype(np.float32)
    cs = np.cos(ang).astype(np.float32).T
    sn = np.sin(ang).astype(np.float32).T
    rope = np.stack([np.tile(cs, (4, 1)), np.tile(sn, (4, 1))]).astype(np.float32)
    tt = np.arange(512)[None, None, :]
    ss_ = np.arange(128)[None, :, None]
    jj = np.arange(4)[:, None, None]
    mf = (tt >= 128 * jj + ss_).astype(np.float32)
    mb = (tt <= 128 * jj + ss_).astype(np.float32)
    hyt = {}
    for sname, P in SEGS.items():
        ft, it_, fe, wn, mh = _hy_tables(P["L"])
        hyt["ft_" + sname], hyt["it_" + sname], hyt["fe_" + sname], hyt["win_" + sname], hyt["mh_" + sname] = ft, it_, fe, wn, mh
    return {**hyt, "cmat": cmat, "rope": rope, "ssdmask": np.stack([mf, mb]).astype(ml_dtypes.bfloat16)}


_CONSTS = None


def kernel(**inp):
    global _CONSTS
    inp = {k: np.asarray(v) for k, v in inp.items()}
    if _CONSTS is None:
        _CONSTS = _consts()
    dbg = kernel.dbg if hasattr(kernel, "dbg") else None
    nc = build(dbg)
    ncores = 8
    in_maps = []
    for core in range(ncores):
        b = core % 4
        m = {"x": np.ascontiguousarray(inp["x"][b]), "ctx": np.ascontiguousarray(inp["ctx"][b])}
        for k in ("w_mod", "b_mod", "w_in", "w_out", "w_router", "router_bias", "w_gate", "w_up", "w_down", "g_final",
                  "ssd_conv_w", "ssd_dt_bias", "ssd_a_log", "ssd_d", "ssd_norm", "hy_conv_w", "hy_conv_b", "hy_w1", "hy_w2", "hy_w3", "hy_bias"):
            m[k] = inp[k]
        m["cols"] = np.stack([_cols_pack(inp, li, b) for li in range(DEPTH)])
        m.update(_CONSTS)
        in_maps.append(m)
    res = run_bass_kernel_spmd(nc, in_maps, core_ids=list(range(ncores)))
    kernel.last = res
    return np.stack([res.results[b]["out"] for b in range(4)]).astype(np.float32)


def outproj(G, li):
    nc, S, I = G.nc, G.S, G.I
    need_ctx = li < DEPTH - 1
    with ExitStack() as _es:
        wo = _es.enter_context(SB(nc, "wo", [128, 8, D], BF16))
        mx = _es.enter_context(SB(nc, "mx", [128, 2, 8, 128], BF16))
        xt = _es.enter_context(SB(nc, "xt", [128, 2, D], F32))
        tmp = _es.enter_context(SB(nc, "tmp", [128, D], F32))
        sq = _es.enter_context(SB(nc, "sq", [128, D], F32))
        ss = _es.enter_context(SB(nc, "ss", [128, 4], F32))
        xn = _es.enter_context(SB(nc, "xn", [128, D], F32))
        h2f = _es.enter_context(SB(nc, "h2f", [128, 8, 128], F32))
        wr = _es.enter_context(SB(nc, "wr", [128, 8, NE], F32))
        po = _es.enter_context(PS(nc, "po", [128, 2, 512], F32))
        pt = _es.enter_context(PS(nc, "pt", [128, 8, 128], F32))
        pr = _es.enter_context(PS(nc, "pr", [128, NE], F32))
        t_wo, t_mx, t_x, t_tmp, t_po = Tok(), [Tok(), Tok()], [Tok(), Tok()], Tok(), Tok()
        t_sq, t_ss, t_xn, t_pt, t_h2f, t_wr, t_pr = Tok(), Tok(), Tok(), Tok(), Tok(), Tok(), Tok()
        S.dma(wo[:], G.wb_out[li].rearrange("(k p) c -> p k c", p=128), reads=[G.t_wb], writes=[t_wo])
        S.dma(wr[:], I["w_router"].rearrange("(k p) c -> p k c", p=128), writes=[t_wr])
        mv = G.mixT.rearrange("(k p) t -> p k t", p=128)
        tiles = [t for t in range(NT) if need_ctx or t >= 2]

        def mm(tile):
            b = tile % 2
            S.dma(mx[:, b], mv[:, :, tile * 128:(tile + 1) * 128], reads=[G.t_mix], writes=[t_mx[b]])
            S.dma(xt[:, b, :], G.xres[tile * 128:(tile + 1) * 128, :], reads=[G.t_xres[tile]], writes=[t_x[b]])
            for half in range(2):
                for k in range(8):
                    S.op("pe", lambda e: e.matmul(po[:, half, :], lhsT=mx[:, b, k, :], rhs=wo[:, k, half * 512:(half + 1) * 512],
                                                  start=(k == 0), stop=(k == 7)), reads=[t_mx[b], t_wo], writes=[t_po])

        mm(tiles[0])
        for ti, tile in enumerate(tiles):
            b = tile % 2
            j = 1 if tile < 2 else 0
            S.op("dve", lambda e: e.tensor_tensor(out=tmp[:], in0=po[:].rearrange("p a c -> p (a c)"), in1=G.gtb[:, 0, j, :], op=ALU.mult),
                 reads=[t_po, G.t_gtb], writes=[t_tmp])
            if ti + 1 < len(tiles):
                mm(tiles[ti + 1])
            S.op("dve", lambda e: e.tensor_tensor(out=xt[:, b, :], in0=tmp[:], in1=xt[:, b, :], op=ALU.add),
                 reads=[t_tmp, t_x[b]], writes=[t_x[b]])
            S.dma(G.xres[tile * 128:(tile + 1) * 128, :], xt[:, b, :], reads=[t_x[b]], writes=[G.t_xres[tile]])
            xv = xt[:, b, :]
            S.op("act", lambda e: e.activation(out=sq[:], in_=xv, func=AF.Square, accum_out=ss[:, 0:1]), reads=[t_x[b]], writes=[t_sq, t_ss])
            S.op("act", lambda e: e.activation(out=ss[:, 1:2], in_=ss[:, 0:1], func=AF.Sqrt, bias=G.epsc[:], scale=1.0 / D),
                 reads=[t_ss, G.t_c], writes=[t_ss])
            S.op("dve", lambda e: e.reciprocal(out=ss[:, 2:3], in_=ss[:, 1:2]), reads=[t_ss], writes=[t_ss])
            S.op("dve", lambda e: e.tensor_scalar(out=xn[:], in0=xv, scalar1=ss[:, 2:3], scalar2=None, op0=ALU.mult),
                 reads=[t_x[b], t_ss], writes=[t_xn])
            for k in range(8):
                S.op("pe", lambda e: e.transpose(out=pt[:, k, :], in_=xn[:, k * 128:(k + 1) * 128], identity=G.identF),
                     reads=[t_xn, G.t_c], writes=[t_pt])
            S.op("dve", lambda e: e.tensor_tensor(out=h2f[:], in0=pt[:], in1=G.modc[:, 3, :, j:j + 1].to_broadcast([128, 8, 128]), op=ALU.mult),
                 reads=[t_pt, G.t_modc], writes=[t_h2f])
            S.op("dve", lambda e: e.tensor_tensor(out=h2f[:], in0=h2f[:], in1=G.modc[:, 2, :, j:j + 1].to_broadcast([128, 8, 128]), op=ALU.add),
                 reads=[t_h2f, G.t_modc], writes=[t_h2f])
            S.op("act", lambda e: e.copy(out=G.h2T[:, :, tile * 128:(tile + 1) * 128], in_=h2f[:]), reads=[t_h2f], writes=[G.t_h2T])
            for k in range(8):
                S.op("pe", lambda e: e.matmul(pr[:], lhsT=h2f[:, k, :], rhs=wr[:, k, :], start=(k == 0), stop=(k == 7)),
                     reads=[t_h2f, t_wr], writes=[t_pr])
            S.op("dve", lambda e: e.tensor_copy(out=G.rl[:, tile, :], in_=pr[:]), reads=[t_pr], writes=[G.t_rl])


def moe(G, li):
    nc, S, I = G.nc, G.S, G.I
    need_ctx = li < DEPTH - 1
    last = li == DEPTH - 1
    h2T, rl = G.h2T, G.rl
    T0 = 0 if need_ctx else 2
    NTl = NT - T0
    BIG = 1.0e9
    with ExitStack() as _es:
        comb = _es.enter_context(SB(nc, "comb", [128, NT, NE], F32))
        t_comb = Tok()
        with ExitStack() as _es:
            sc = _es.enter_context(SB(nc, "r_sc", [128, NT, NE], F32))
            sel = _es.enter_context(SB(nc, "r_sel", [128, NT, NE], F32))
            ra = _es.enter_context(SB(nc, "r_a", [128, NT, NE], F32))
            rb = _es.enter_context(SB(nc, "r_b", [128, NT, NE], F32))
            rm = _es.enter_context(SB(nc, "r_m", [128, NT * 4], F32))
            rm2 = _es.enter_context(SB(nc, "r_m2", [128, NT * 4], F32))
            rg = _es.enter_context(SB(nc, "r_g", [128, NT], F32))
            rbias = _es.enter_context(SB(nc, "rbias", [128, NE], F32))
            t = Tok()
            if T0 > 0:
                S.op("dve", lambda e: e.memset(rl[:, 0:T0, :], 0.0), reads=[G.t_rl], writes=[G.t_rl])
            S.dma(rbias[:], I["router_bias"].partition_broadcast(128), writes=[t])
            v3 = lambda a: a[:].rearrange("p n (g x) -> p (n g) x", x=4)
            S.op("act", lambda e: e.activation(out=sc[:], in_=rl[:], func=AF.Sigmoid), reads=[G.t_rl], writes=[t])
            S.op("dve", lambda e: e.tensor_tensor(out=sel[:], in0=sc[:], in1=rbias[:].unsqueeze(1).to_broadcast([128, NT, NE]), op=ALU.add),
                 reads=[t], writes=[t])
            S.op("dve", lambda e: e.tensor_reduce(out=rm[:], in_=v3(sel), axis=AX.X, op=ALU.max), reads=[t], writes=[t])
            S.op("dve", lambda e: e.tensor_tensor(out=v3(ra), in0=v3(sel), in1=rm[:].unsqueeze(2).to_broadcast([128, NT * 4, 4]), op=ALU.is_equal),
                 reads=[t], writes=[t])
            S.op("dve", lambda e: e.scalar_tensor_tensor(out=rb[:], in0=ra[:], scalar=-BIG, in1=sel[:], op0=ALU.mult, op1=ALU.add),
                 reads=[t], writes=[t])
            S.op("dve", lambda e: e.tensor_reduce(out=rm2[:], in_=v3(rb), axis=AX.X, op=ALU.max), reads=[t], writes=[t])
            S.op("dve", lambda e: e.tensor_tensor(out=rm[:], in0=rm[:], in1=rm2[:], op=ALU.add), reads=[t], writes=[t])
            S.op("dve", lambda e: e.tensor_reduce(out=rg[:], in_=rm[:].rearrange("p (n g) -> p n g", g=4), axis=AX.X, op=ALU.max),
                 reads=[t], writes=[t])
            S.op("dve", lambda e: e.tensor_tensor(out=rm2[:].rearrange("p (n g) -> p n g", g=4), in0=rm[:].rearrange("p (n g) -> p n g", g=4),
                                                  in1=rg[:].unsqueeze(2).to_broadcast([128, NT, 4]), op=ALU.is_equal), reads=[t], writes=[t])
            S.op("dve", lambda e: e.tensor_scalar(out=rm2[:], in0=rm2[:], scalar1=1.0, scalar2=BIG, op0=ALU.subtract, op1=ALU.mult),
                 reads=[t], writes=[t])
            S.op("dve", lambda e: e.tensor_tensor(out=v3(sel), in0=v3(sel), in1=rm2[:].unsqueeze(2).to_broadcast([128, NT * 4, 4]), op=ALU.add),
                 reads=[t], writes=[t])
            S.op("dve", lambda e: e.tensor_reduce(out=rg[:], in_=sel[:], axis=AX.X, op=ALU.max), reads=[t], writes=[t])
            S.op("dve", lambda e: e.tensor_tensor(out=ra[:], in0=sel[:], in1=rg[:].unsqueeze(2).to_broadcast([128, NT, NE]), op=ALU.is_equal),
                 reads=[t], writes=[t])
            S.op("dve", lambda e: e.scalar_tensor_tensor(out=sel[:], in0=ra[:], scalar=-BIG, in1=sel[:], op0=ALU.mult, op1=ALU.add),
                 reads=[t], writes=[t])
            S.op("dve", lambda e: e.tensor_reduce(out=rg[:], in_=sel[:], axis=AX.X, op=ALU.max), reads=[t], writes=[t])
            S.op("dve", lambda e: e.tensor_tensor(out=rb[:], in0=sel[:], in1=rg[:].unsqueeze(2).to_broadcast([128, NT, NE]), op=ALU.is_equal),
                 reads=[t], writes=[t])
            S.op("dve", lambda e: e.tensor_tensor(out=ra[:], in0=ra[:], in1=rb[:], op=ALU.add), reads=[t], writes=[t])
            S.op("dve", lambda e: e.tensor_tensor(out=ra[:], in0=ra[:], in1=sc[:], op=ALU.mult), reads=[t], writes=[t])
            S.op("dve", lambda e: e.tensor_reduce(out=rg[:], in_=ra[:], axis=AX.X, op=ALU.add), reads=[t], writes=[t])
            S.op("dve", lambda e: e.reciprocal(out=rg[:], in_=rg[:]), reads=[t], writes=[t])
            S.op("dve", lambda e: e.tensor_tensor(out=comb[:], in0=ra[:], in1=rg[:].unsqueeze(2).to_broadcast([128, NT, NE]), op=ALU.mult),
                 reads=[t], writes=[t_comb])
            S.barrier()
        SGT = 12
        with ExitStack() as _es:
            acc = _es.enter_context(SB(nc, "acc", [128, SGT, D], F32))
            wg = _es.enter_context(SB(nc, "wg", [128, 2, 8, DFF], BF16))
            wu = _es.enter_context(SB(nc, "wu", [128, 2, 8, DFF], BF16))
            wd = _es.enter_context(SB(nc, "wd", [128, 2, 2, D], BF16))
            sgl = _es.enter_context(SB(nc, "sgl", [128, 2, 512], F32))
            aa2 = _es.enter_context(SB(nc, "aa", [128, 2, 2, 512], BF16))
            xt = _es.enter_context(SB(nc, "xt", [128, 2, D], F32))
            gfb = _es.enter_context(SB(nc, "gfb", [128, D], F32))
            ss = _es.enter_context(SB(nc, "ss", [128, 4], F32))
            sq = _es.enter_context(SB(nc, "sq", [128, D], F32))
            pgu = _es.enter_context(PS(nc, "pgu", [128, 4, 512], F32))
            py = _es.enter_context(PS(nc, "py", [128, 2, 2, 512], F32))
            t_pgu2, t_sgl2, t_aa2 = [Tok(), Tok()], [Tok(), Tok()], [Tok(), Tok()]
            t_acc, t_w, t_sgl, t_aa, t_pgu, t_py, t_x, t_gf, t_ss, t_sq = Tok(), [Tok(), Tok()], Tok(), Tok(), Tok(), [Tok(), Tok()], [Tok(), Tok()], Tok(), Tok(), Tok()
            if last:
                S.dma(gfb[:], I["g_final"].partition_broadcast(128), writes=[t_gf])
            yi = 0
            ui = 0
            for s0 in range(T0, NT, SGT):
                tiles = list(range(s0, min(NT, s0 + SGT)))
                units = []
                for ex in range(NE):
                    for b0 in range(0, len(tiles), 4):
                        bt = tiles[b0:b0 + 4]
                        units.append((ex, bt, len(bt) * 128, bt[0] * 128, b0 == 0))

                def gu(u, jj):
                    ex, bt, n, c0, newex = units[u]
                    wb_ = ex % 2
                    ub = (ui + u) % 2
                    if newex and jj == 0:
                        S.dma(wg[:, wb_], G.wb_gate[li][ex].rearrange("(k p) f -> p k f", p=128), reads=[G.t_wb], writes=[t_w[wb_]])
                        S.dma(wu[:, wb_], G.wb_up[li][ex].rearrange("(k p) f -> p k f", p=128), reads=[G.t_wb], writes=[t_w[wb_]])
                        S.dma(wd[:, wb_], G.wb_down[li][ex].rearrange("(j p) c -> p j c", p=128), reads=[G.t_wb], writes=[t_w[wb_]])
                    for wi, wt in enumerate((wg, wu)):
                        for k in range(8):
                            S.op("pe", lambda e: e.matmul(pgu[:, jj * 2 + wi, 0:n], lhsT=wt[:, wb_, k, jj * 128:(jj + 1) * 128],
                                                          rhs=h2T[:, k, c0:c0 + n], start=(k == 0), stop=(k == 7)),
                                 reads=[t_w[wb_], G.t_h2T], writes=[t_pgu2[jj]])
                    S.op("act", lambda e: e.activation(out=sgl[:, jj, 0:n], in_=pgu[:, jj * 2, 0:n], func=AF.Silu), reads=[t_pgu2[jj]], writes=[t_sgl2[jj]])
                    S.op("dve", lambda e: e.tensor_tensor(out=aa2[:, ub, jj, 0:n], in0=sgl[:, jj, 0:n], in1=pgu[:, jj * 2 + 1, 0:n], op=ALU.mult),
                         reads=[t_sgl2[jj], t_pgu2[jj]], writes=[t_aa2[ub]])

                def down(u):
                    nonlocal yi
                    ex, bt, n, c0, newex = units[u]
                    wb_ = ex % 2
                    ub = (ui + u) % 2
                    for ti, tl in enumerate(bt):
                        yb = yi % 2
                        yi += 1
                        for half in range(2):
                            for jj in range(2):
                                S.op("pe", lambda e: e.matmul(py[:, yb, half, :], lhsT=aa2[:, ub, jj, ti * 128:(ti + 1) * 128],
                                                              rhs=wd[:, wb_, jj, half * 512:(half + 1) * 512], start=(jj == 0), stop=(jj == 1)),
                                     reads=[t_aa2[ub], t_w[wb_]], writes=[t_py[yb]])
                        al = acc[:, tl - s0, :]
                        pyv = py[:, yb].rearrange("p a c -> p (a c)")
                        if ex == 0:
                            S.op("dve", lambda e: e.tensor_scalar(out=al, in0=pyv, scalar1=comb[:, tl, ex:ex + 1], scalar2=None, op0=ALU.mult),
                                 reads=[t_py[yb], t_comb], writes=[t_acc])
                        else:
                            S.op("dve", lambda e: e.scalar_tensor_tensor(out=al, in0=pyv, scalar=comb[:, tl, ex:ex + 1], in1=al,
                                                                         op0=ALU.mult, op1=ALU.add), reads=[t_py[yb], t_comb, t_acc], writes=[t_acc])

                gu(0, 0)
                gu(0, 1)
                for u in range(len(units)):
                    if u + 1 < len(units):
                        gu(u + 1, 0)
                    down(u)
                    if u + 1 < len(units):
                        gu(u + 1, 1)
                ui += len(units)
                for tl in tiles:
                    b = tl % 2
                    j = 1 if tl < 2 else 0
                    S.dma(xt[:, b, :], G.xres[tl * 128:(tl + 1) * 128, :], reads=[G.t_xres[tl]], writes=[t_x[b]])
                    al = acc[:, tl - s0, :]
                    S.op("dve", lambda e: e.tensor_tensor(out=al, in0=al, in1=G.gtb[:, 1, j, :], op=ALU.mult), reads=[t_acc, G.t_gtb], writes=[t_acc])
                    S.op("dve", lambda e: e.tensor_tensor(out=xt[:, b, :], in0=al, in1=xt[:, b, :], op=ALU.add), reads=[t_acc, t_x[b]], writes=[t_x[b]])
                    if not last:
                        S.dma(G.xres[tl * 128:(tl + 1) * 128, :], xt[:, b, :], reads=[t_x[b]], writes=[G.t_xres[tl]])
                    else:
                        xv = xt[:, b, :]
                        S.op("act", lambda e: e.activation(out=sq[:], in_=xv, func=AF.Square, accum_out=ss[:, 0:1]), reads=[t_x[b]], writes=[t_sq, t_ss])
                        S.op("act", lambda e: e.activation(out=ss[:, 1:2], in_=ss[:, 0:1], func=AF.Sqrt, bias=G.epsc[:], scale=1.0 / D),
                             reads=[t_ss, G.t_c], writes=[t_ss])
                        S.op("dve", lambda e: e.reciprocal(out=ss[:, 2:3], in_=ss[:, 1:2]), reads=[t_ss], writes=[t_ss])
                        S.op("dve", lambda e: e.scalar_tensor_tensor(out=xv, in0=xv, scalar=ss[:, 2:3], in1=gfb[:], op0=ALU.mult, op1=ALU.mult),
                             reads=[t_x[b], t_ss, t_gf], writes=[t_x[b]])
                        S.dma(G.out[(tl - 2) * 128:(tl - 1) * 128, :], xv, reads=[t_x[b]], writes=[])


def ssd(G, li):
    nc, S, I = G.nc, G.S, G.I
    hT = G.hT
    need_ctx = li < DEPTH - 1
    wv32 = I["w_in"][li].rearrange("(k p) c -> p k c", p=128)
    wvb = G.wb_in[li].rearrange("(k p) c -> p k c", p=128)
    XB0 = 1024
    with ExitStack() as _es:
        xbcT = _es.enter_context(SB(nc, "xbcT", [128, 5, NTOK], BF16))
        xs_tok = _es.enter_context(SB(nc, "xs_tok", [128, NT, 384], BF16))
        B_tok = _es.enter_context(SB(nc, "B_tok", [128, NT, 128], BF16))
        lndt = _es.enter_context(SB(nc, "lndt", [128, NT, 12], F32))
        dta = _es.enter_context(SB(nc, "dta", [128, NT, 12], F32))
        ea = _es.enter_context(SB(nc, "ea", [128, NT, 12], F32))
        ww = _es.enter_context(SB(nc, "ww", [128, NT, 12], F32))
        eT = _es.enter_context(SB(nc, "eT", [128, NT, 12], F32))
        t_xbc, t_xs, t_dt = Tok(), Tok(), Tok()
        with ExitStack() as _es2:
            dts = _es2.enter_context(SB(nc, "dts", [128, NT, 12], F32))
            wst = _es2.enter_context(SB(nc, "wst", [128, 8, 128], F32))
            cwb = _es2.enter_context(SB(nc, "cwb", [128, 3, 128], F32))
            wj = _es2.enter_context(SB(nc, "wj", [128, 3, 8, 128], BF16))
            wdt = _es2.enter_context(SB(nc, "wdt", [128, 8, 12], BF16))
            dtb = _es2.enter_context(SB(nc, "dtb", [128, 2, 12], F32))
            tot = _es2.enter_context(SB(nc, "tot", [128, NT, 12], F32))
            wcol = _es2.enter_context(SB(nc, "wcol", [128, NT, 12], F32))
            pp2 = _es2.enter_context(PS(nc, "pp", [128, 2, 512], F32))
            pdt = _es2.enter_context(PS(nc, "pdt", [128, NT, 12], F32))
            ptb = _es2.enter_context(PS(nc, "ptb", [128, 4, 128], BF16))
            pc = _es2.enter_context(PS(nc, "pc", [128, NT, 12], F32))
            t_wst, t_cwb, t_wj, t_pp, t_wdt, t_pdt, t_ptb, t_a, t_pc = Tok(), Tok(), Tok(), Tok(), Tok(), Tok(), Tok(), Tok(), Tok()
            ocb = COLS["ssd_conv_b"][0]
            t_pp2 = [Tok(), Tok()]
            for ch in range(5):
                S.dma(wst[:], wv32[:, :, XB0 + ch * 128:XB0 + (ch + 1) * 128], writes=[t_wst])
                for j in range(3):
                    S.dma(cwb[:, j, :], I["ssd_conv_w"][li, j, ch * 128:(ch + 1) * 128].partition_broadcast(128), writes=[t_cwb])
                for j in range(3):
                    S.op("dve", lambda e: e.tensor_tensor(out=wj[:, j], in0=wst[:], in1=cwb[:, j:j + 1, :].to_broadcast([128, 8, 128]), op=ALU.mult),
                         reads=[t_wst, t_cwb], writes=[t_wj])
                for bi_, (c0, t0, n, tile0, ntile) in enumerate(BLOCKS):
                    pb_ = (ch * len(BLOCKS) + bi_) % 2
                    pp = pp2[:, pb_, :]
                    for j in range(3):
                        for k in range(8):
                            S.op("pe", lambda e: e.matmul(pp[:, 0:n], lhsT=wj[:, j, k, :], rhs=hT[:, k, c0 + j - 1:c0 + j - 1 + n],
                                                          start=(j == 0 and k == 0), stop=(j == 2 and k == 7)), reads=[t_wj, G.t_hT], writes=[t_pp2[pb_]])
                    S.op("act", lambda e: e.activation(out=xbcT[:, ch, t0:t0 + n], in_=pp[:, 0:n], func=AF.Silu, bias=G.cols[:, ocb + ch:ocb + ch + 1]),
                         reads=[t_pp2[pb_], G.t_cols], writes=[t_xbc])
            S.dma(wdt[:], wvb[:, :, 1664:1676], reads=[G.t_wb], writes=[t_wdt])
            S.dma(dtb[:, 0, :], I["ssd_dt_bias"][li].rearrange("a h -> (a h)").partition_broadcast(128), writes=[t_wdt])
            S.dma(dtb[:, 1, :], I["ssd_a_log"][li].rearrange("a h -> (a h)").partition_broadcast(128), writes=[t_wdt])
            for tl in range(NT):
                cc = colof(tl)
                for k in range(8):
                    S.op("pe", lambda e: e.matmul(pdt[:, tl, :], lhsT=hT[:, k, cc:cc + 128], rhs=wdt[:, k, :], start=(k == 0), stop=(k == 7)),
                         reads=[t_wdt, G.t_hT], writes=[t_pdt])
            S.op("dve", lambda e: e.tensor_tensor(out=dts[:], in0=pdt[:], in1=dtb[:, 0:1, :].to_broadcast([128, NT, 12]), op=ALU.add),
                 reads=[t_pdt, t_wdt], writes=[t_dt])
            S.op("act", lambda e: e.activation(out=dts[:], in_=dts[:], func=AF.Exp), reads=[t_dt], writes=[t_dt])
            S.op("act", lambda e: e.activation(out=dts[:], in_=dts[:], func=AF.Ln, bias=1.0), reads=[t_dt], writes=[t_dt])
            S.op("act", lambda e: e.activation(out=lndt[:], in_=dts[:], func=AF.Ln), reads=[t_dt], writes=[t_dt])
            S.op("act", lambda e: e.activation(out=dtb[:, 1, :], in_=dtb[:, 1, :], func=AF.Exp), reads=[t_wdt], writes=[t_wdt])
            S.op("dve", lambda e: e.scalar_tensor_tensor(out=dta[:], in0=dts[:], scalar=-1.0, in1=dtb[:, 1:2, :].to_broadcast([128, NT, 12]),
                                                         op0=ALU.mult, op1=ALU.mult), reads=[t_dt, t_wdt], writes=[t_a])
            for tl in range(NT):
                for c in range(4):
                    S.op("pe", lambda e: e.transpose(out=ptb[:, c, :], in_=xbcT[:, c, tl * 128:(tl + 1) * 128], identity=G.identB),
                         reads=[t_xbc, G.t_c], writes=[t_ptb])
                S.op("dve", lambda e: e.tensor_copy(out=xs_tok[:, tl, :], in_=ptb[:, 0:3, :].rearrange("p c t -> p (c t)")), reads=[t_ptb], writes=[t_xs])
                S.op("dve", lambda e: e.tensor_copy(out=B_tok[:, tl, :], in_=ptb[:, 3, :]), reads=[t_ptb], writes=[t_xs])
            for dr in range(2):
                S.op("pe", lambda e: e.matmul(pc[:].rearrange("p n h -> p (n h)"), lhsT=G.cm[:, 1 + dr, :], rhs=dta[:].rearrange("p n h -> p (n h)"),
                                              start=True, stop=True), reads=[t_a, G.t_c, t_dt], writes=[t_pc])
                S.op("dve", lambda e: e.tensor_copy(out=wcol[:, :, dr * 6:(dr + 1) * 6], in_=pc[:, :, dr * 6:(dr + 1) * 6]), reads=[t_pc], writes=[t_dt])
            S.op("pe", lambda e: e.matmul(pc[:].rearrange("p n h -> p (n h)"), lhsT=G.ones[:], rhs=dta[:].rearrange("p n h -> p (n h)"), start=True, stop=True),
                 reads=[t_a, G.t_c, t_dt], writes=[t_pc])
            S.op("dve", lambda e: e.tensor_copy(out=tot[:], in_=pc[:]), reads=[t_pc], writes=[t_dt])
            S.op("act", lambda e: e.activation(out=ea[:], in_=wcol[:], func=AF.Exp), reads=[t_dt], writes=[t_dt])
            S.op("act", lambda e: e.activation(out=eT[:], in_=tot[:], func=AF.Exp), reads=[t_dt], writes=[t_dt])
            S.op("dve", lambda e: e.tensor_tensor(out=ww[:], in0=tot[:], in1=wcol[:], op=ALU.subtract), reads=[t_dt], writes=[t_dt])
            S.op("dve", lambda e: e.tensor_tensor(out=ww[:], in0=ww[:], in1=lndt[:], op=ALU.add), reads=[t_dt], writes=[t_dt])
            S.op("act", lambda e: e.activation(out=ww[:], in_=ww[:], func=AF.Exp), reads=[t_dt], writes=[t_dt])
            S.barrier()
        with ExitStack() as _es2:
            wz = _es2.enter_context(SB(nc, "wz", [128, 8, 384], BF16))
            dbc = _es2.enter_context(SB(nc, "dbc", [128, 6, 64], F32))
            d6 = _es2.enter_context(SB(nc, "d6", [128, 6], F32))
            nwb = _es2.enter_context(SB(nc, "nwb", [128, 384], F32))
            hst = _es2.enter_context(SB(nc, "hst", [128, 192], F32))
            hsb = _es2.enter_context(SB(nc, "hsb", [128, 192], BF16))
            xw = _es2.enter_context(SB(nc, "xw", [128, 2, 192], BF16))
            ybt = _es2.enter_context(SB(nc, "ybt", [128, 2, 384], F32))
            Sm = _es2.enter_context(SB(nc, "Sm", [128, 2, 2, 128], F32))
            rA = _es2.enter_context(SB(nc, "rA", [128, 4, 128], F32))
            Dd = _es2.enter_context(SB(nc, "Dd", [128, 4, 128], F32))
            Mt = _es2.enter_context(SB(nc, "Mt", [128, 4, 128], BF16))
            acc = _es2.enter_context(SB(nc, "acc", [128, 384], F32))
            tmp = _es2.enter_context(SB(nc, "tmp", [128, 384], F32))
            zs = _es2.enter_context(SB(nc, "zs", [128, 384], F32))
            ssq = _es2.enter_context(SB(nc, "ssq", [128, 4], F32))
            ob = _es2.enter_context(SB(nc, "ob", [128, 384], BF16))
            oT = _es2.enter_context(SB(nc, "oT", [128, 2, 3, 128], BF16))
            pis = _es2.enter_context(PS(nc, "pis", [128, 2, 192], F32))
            ps_st = _es2.enter_context(PS(nc, "ps_st", [128, 2, 512], F32))
            pseg = _es2.enter_context(PS(nc, "pseg", [128, 3, 512], F32))
            py = _es2.enter_context(PS(nc, "py", [128, 384], F32))
            pz = py
            ptr = _es2.enter_context(PS(nc, "ptr", [128, 3, 128], BF16))
            t_wz, t_db, t_h, t_xw, t_yb, t_Sm, t_acc, t_tmp, t_zs, t_ssq, t_ob, t_oT = Tok(), Tok(), Tok(), Tok(), [Tok(), Tok()], Tok(), Tok(), Tok(), Tok(), Tok(), Tok(), [Tok(), Tok()]
            t_rA, t_Dd, t_Mt, t_pseg = [Tok() for _ in range(4)], [Tok() for _ in range(4)], [Tok() for _ in range(4)], [Tok() for _ in range(3)]
            t_pis, t_pst, t_py, t_ptr = Tok(), Tok(), Tok(), Tok()
            t_pz = t_py
            S.dma(wz[:], wvb[:, :, 640:1024], reads=[G.t_wb], writes=[t_wz])
            S.dma(d6[:], I["ssd_d"][li].partition_broadcast(128), writes=[t_db])
            S.dma(nwb[:], I["ssd_norm"][li].partition_broadcast(128), writes=[t_db])
            S.op("dve", lambda e: e.tensor_copy(out=dbc[:], in_=d6[:].unsqueeze(2).to_broadcast([128, 6, 64])), reads=[t_db], writes=[t_db])
            yb_d = G.ssd_yb

            def carry_step(c, dr, want_out, dst):
                for g in range(2):
                    hd0 = dr * 6 + 3 * g
                    if want_out:
                        S.op("pe", lambda e: e.matmul(pis[:, 0, :], lhsT=xbcT[g * 64:(g + 1) * 64, 4, c * 128:(c + 1) * 128], rhs=hsb[g * 64:(g + 1) * 64, :],
                                                      start=True, stop=True), reads=[t_xbc, t_h], writes=[t_pis])
                        S.op("dve", lambda e: e.tensor_tensor(out=dst[:, g * 192:(g + 1) * 192].rearrange("p (a d) -> p a d", a=3),
                                                              in0=pis[:, 0, :].rearrange("p (a d) -> p a d", a=3),
                                                              in1=ea[:, c, hd0:hd0 + 3].unsqueeze(2).to_broadcast([128, 3, 64]), op=ALU.mult),
                             reads=[t_pis, t_dt], writes=[dst_tok[0]])
                    S.op("dve", lambda e: e.tensor_tensor(out=xw[:, g, :].rearrange("p (a d) -> p a d", a=3),
                                                          in0=xs_tok[:, c, g * 192:(g + 1) * 192].rearrange("p (a d) -> p a d", a=3),
                                                          in1=ww[:, c, hd0:hd0 + 3].unsqueeze(2).to_broadcast([128, 3, 64]), op=ALU.mult),
                         reads=[t_xs, t_dt], writes=[t_xw])
                    gs = slice(g * 64, (g + 1) * 64)
                    S.op("pe", lambda e: e.matmul(pis[:, 1, :], lhsT=B_tok[:, c, :], rhs=xw[:, g, :], start=True, stop=True),
                         reads=[t_xs, t_xw], writes=[t_pis])
                    S.op("dve", lambda e: e.tensor_tensor(out=hst[gs, :].rearrange("p (a d) -> p a d", a=3),
                                                          in0=hst[gs, :].rearrange("p (a d) -> p a d", a=3),
                                                          in1=eT[gs, c, hd0:hd0 + 3].unsqueeze(2).to_broadcast([64, 3, 64]), op=ALU.mult),
                         reads=[t_h, t_dt], writes=[t_h])
                    S.op("dve", lambda e: e.tensor_tensor(out=hst[gs, :], in0=hst[gs, :], in1=pis[gs, 1, :], op=ALU.add),
                         reads=[t_h, t_pis], writes=[t_h])
                    S.op("act", lambda e: e.copy(out=hsb[gs, :], in_=hst[gs, :]), reads=[t_h], writes=[t_h])

            S.op("dve", lambda e: e.memset(hst[:], 0.0), writes=[t_h])
            S.op("dve", lambda e: e.memset(hsb[:], 0.0), writes=[t_h])
            order_b = [1, 0] + list(range(NT - 1, 1, -1))
            for ci, c in enumerate(order_b):
                want = need_ctx or c >= 2
                b = ci % 2
                dst_tok = [t_yb[b]]
                carry_step(c, 1, want, ybt[:, b, :])
                if want:
                    S.dma(yb_d[c * 128:(c + 1) * 128, :], ybt[:, b, :], reads=[t_yb[b]], writes=[G.t_ssdyb[c]])
            S.op("dve", lambda e: e.memset(hst[:], 0.0), reads=[t_h], writes=[t_h])
            S.op("dve", lambda e: e.memset(hsb[:], 0.0), reads=[t_h], writes=[t_h])
            on = COLS["ssd_norm"][0]
            it = 0
            for c in range(NT):
                want = need_ctx or c >= 2
                dst_tok = [t_acc]
                carry_step(c, 0, want, acc[:])
                if not want:
                    continue
                b = c % 2
                tc0 = c * 128
                S.dma(ybt[:, b, :], yb_d[c * 128:(c + 1) * 128, :], reads=[G.t_ssdyb[c]], writes=[t_yb[b]])
                cc = colof(c)
                for k in range(8):
                    S.op("pe", lambda e: e.matmul(pz[:], lhsT=hT[:, k, cc:cc + 128], rhs=wz[:, k, :], start=(k == 0), stop=(k == 7)),
                         reads=[t_wz, G.t_hT], writes=[t_pz])
                S.op("act", lambda e: e.activation(out=zs[:], in_=pz[:], func=AF.Silu), reads=[t_pz], writes=[t_zs])
                for g in range(2):
                    S.op("pe", lambda e: e.matmul(ps_st[:, g, 0:128], lhsT=xbcT[g * 64:(g + 1) * 64, 3, tc0:tc0 + 128], rhs=xbcT[g * 64:(g + 1) * 64, 4, tc0:tc0 + 128],
                                                  start=True, stop=True), reads=[t_xbc], writes=[t_pst])
                for g in range(2):
                    for dr in range(2):
                        S.op("dve", lambda e: e.tensor_tensor(out=Sm[:, g, dr, :], in0=ps_st[:, g, 0:128], in1=G.cm[:, 1 + dr, :], op=ALU.mult),
                             reads=[t_pst, G.t_c], writes=[t_Sm])
                steps = [(h, dr) for h in range(6) for dr in range(2)]

                def st_a(i):
                    h, dr = steps[i]
                    hd = dr * 6 + h
                    r4, p3 = (it + i) % 4, (it + i) % 3
                    S.op("dve", lambda e: e.tensor_scalar(out=rA[:, r4, :], in0=G.cm[:, 1 + dr, :], scalar1=dta[:, c, hd:hd + 1], scalar2=None, op0=ALU.mult),
                         reads=[G.t_c, t_dt], writes=[t_rA[r4]])
                    S.op("pe", lambda e: e.matmul(pseg[:, p3, 0:128], lhsT=G.cm[:, 8 - dr, :], rhs=rA[:, r4, :], start=True, stop=True),
                         reads=[t_rA[r4], G.t_c], writes=[t_pseg[p3]])
                    S.op("act", lambda e: e.activation(out=Dd[:, r4, :], in_=pseg[:, p3, 0:128], func=AF.Exp, bias=lndt[:, c, hd:hd + 1]),
                         reads=[t_pseg[p3], t_dt], writes=[t_Dd[r4]])

                st_a(0)
                st_a(1)
                for i, (h, dr) in enumerate(steps):
                    r4 = (it + i) % 4
                    if i + 2 < len(steps):
                        st_a(i + 2)
                    S.op("dve", lambda e: e.tensor_tensor(out=Mt[:, r4, :], in0=Dd[:, r4, :], in1=Sm[:, h // 3, dr, :], op=ALU.mult),
                         reads=[t_Dd[r4], t_Sm], writes=[t_Mt[r4]])
                    S.op("pe", lambda e: e.matmul(py[:, h * 64:(h + 1) * 64], lhsT=Mt[:, r4, :], rhs=xs_tok[:, c, h * 64:(h + 1) * 64],
                                                  start=(dr == 0), stop=(dr == 1)), reads=[t_Mt[r4], t_xs], writes=[t_py])
                it += len(steps)
                S.op("dve", lambda e: e.tensor_tensor(out=acc[:], in0=acc[:], in1=py[:], op=ALU.add), reads=[t_acc, t_py], writes=[t_acc])
                S.op("dve", lambda e: e.tensor_tensor(out=acc[:], in0=acc[:], in1=ybt[:, b, :], op=ALU.add), reads=[t_acc, t_yb[b]], writes=[t_acc])
                S.op("dve", lambda e: e.tensor_tensor(out=tmp[:], in0=xs_tok[:, c, :], in1=dbc[:].rearrange("p a d -> p (a d)"), op=ALU.mult),
                     reads=[t_xs, t_db], writes=[t_tmp])
                S.op("dve", lambda e: e.tensor_tensor(out=acc[:], in0=acc[:], in1=tmp[:], op=ALU.add), reads=[t_acc, t_tmp], writes=[t_acc])
                S.op("dve", lambda e: e.tensor_tensor(out=acc[:], in0=acc[:], in1=zs[:], op=ALU.mult), reads=[t_acc, t_zs], writes=[t_acc])
                for g in range(2):
                    S.op("act", lambda e: e.activation(out=tmp[:, g * 192:(g + 1) * 192], in_=acc[:, g * 192:(g + 1) * 192], func=AF.Square, accum_out=ssq[:, g:g + 1]),
                         reads=[t_acc], writes=[t_tmp, t_ssq])
                S.op("act", lambda e: e.activation(out=ssq[:, 2:4], in_=ssq[:, 0:2], func=AF.Sqrt, bias=G.epsc[:], scale=1.0 / 192), reads=[t_ssq, G.t_c], writes=[t_ssq])
                S.op("dve", lambda e: e.reciprocal(out=ssq[:, 2:4], in_=ssq[:, 2:4]), reads=[t_ssq], writes=[t_ssq])
                for g in range(2):
                    S.op("dve", lambda e: e.scalar_tensor_tensor(out=ob[:, g * 192:(g + 1) * 192], in0=acc[:, g * 192:(g + 1) * 192], scalar=ssq[:, 2 + g:3 + g],
                                                                 in1=nwb[:, g * 192:(g + 1) * 192], op0=ALU.mult, op1=ALU.mult), reads=[t_acc, t_ssq, t_db], writes=[t_ob])
                for c3 in range(3):
                    S.op("pe", lambda e: e.transpose(out=ptr[:, c3, :], in_=ob[:, c3 * 128:(c3 + 1) * 128], identity=G.identB), reads=[t_ob, G.t_c], writes=[t_ptr])
                S.op("act", lambda e: e.copy(out=oT[:, b], in_=ptr[:]), reads=[t_ptr], writes=[t_oT[b]])
                S.dma(G.mixT[384:768, tc0:tc0 + 128].rearrange("(c p) t -> p c t", p=128), oT[:, b], reads=[t_oT[b]], writes=[G.t_mix])
        if "ssd" in G.dbg and li == 0:
            dump_bf16(G, G.mixT[384:512, 256:768], G.dbg["ssd"], [G.t_mix])


HY0 = 1676
SEGS = {"lat": dict(L=4096, NC=32, KC=17, off=NCTX), "ctx": dict(L=256, NC=2, KC=2, off=0)}


def hyena_inproj(G, li):
    nc, S, I = G.nc, G.S, G.I
    hT = G.hT
    need_ctx = li < DEPTH - 1
    wv32 = I["w_in"][li].rearrange("(k p) c -> p k c", p=128)
    with ExitStack() as _es:
        wst = _es.enter_context(SB(nc, "wst", [128, 8, 128], F32))
        cwb = _es.enter_context(SB(nc, "cwb", [128, 3, 128], F32))
        wj = _es.enter_context(SB(nc, "wjh", [128, 3, 8, 768], BF16))
        hb = _es.enter_context(SB(nc, "hb", [128, 768], F32))
        hv = _es.enter_context(SB(nc, "hv", [128, 2, 768], BF16))
        ph4 = _es.enter_context(PS(nc, "ph", [128, 2, 2, 512], F32))
        t_wst, t_cwb, t_wj, t_hb, t_hv, t_ph2 = Tok(), Tok(), Tok(), Tok(), [Tok(), Tok()], [Tok(), Tok()]
        S.dma(hb[:], I["hy_conv_b"][li].partition_broadcast(128), writes=[t_hb])
        for cc in range(6):
            S.dma(wst[:], wv32[:, :, HY0 + cc * 128:HY0 + (cc + 1) * 128], writes=[t_wst])
            for j in range(3):
                S.dma(cwb[:, j, :], I["hy_conv_w"][li, j, cc * 128:(cc + 1) * 128].partition_broadcast(128), writes=[t_cwb])
            for j in range(3):
                S.op("dve", lambda e: e.tensor_tensor(out=wj[:, j, :, cc * 128:(cc + 1) * 128], in0=wst[:],
                                                      in1=cwb[:, j:j + 1, :].to_broadcast([128, 8, 128]), op=ALU.mult),
                     reads=[t_wst, t_cwb], writes=[t_wj])
        dv = G.hyv.rearrange("m t c -> t m c")
        for tl in range(NT):
            if tl < 2 and not need_ctx:
                continue
            col = colof(tl)
            b = tl % 2
            ph = ph4[:, b]
            t_ph = t_ph2[b]
            for half in range(2):
                for j in range(3):
                    for k in range(8):
                        S.op("pe", lambda e: e.matmul(ph[:, half, 0:384], lhsT=hT[:, k, col + j - 1:col + j - 1 + 128],
                                                      rhs=wj[:, j, k, half * 384:(half + 1) * 384], start=(j == 0 and k == 0), stop=(j == 2 and k == 7)),
                             reads=[t_wj, G.t_hT], writes=[t_ph])
            S.op("dve", lambda e: e.tensor_tensor(out=hv[:, b, :].rearrange("p (a c) -> p a c", a=2), in0=ph[:, :, 0:384],
                                                  in1=hb[:].rearrange("p (a c) -> p a c", a=2), op=ALU.add), reads=[t_ph, t_hb], writes=[t_hv[b]])
            S.dma(dv[tl * 128:(tl + 1) * 128], hv[:, b, :].rearrange("p (m c) -> p m c", m=3), reads=[t_hv[b]], writes=[G.t_hyv])


def hyena_fft(G, li):
    nc, S, I = G.nc, G.S, G.I
    need_ctx = li < DEPTH - 1
    for sname in (("lat", "ctx") if need_ctx else ("lat",)):
        P = SEGS[sname]
        L, NCk, KC, off = P["L"], P["NC"], P["KC"], P["off"]
        NH = NCk // 2
        FT, IT, FE, WIN, MH = I["ft_" + sname], I["it_" + sname], I["fe_" + sname], I["win_" + sname], I["mh_" + sname]
        Kf = G.Kf[sname]
        t_kf = Tok()
        with ExitStack() as _es:
            hk = _es.enter_context(SB(nc, "hk", [128, NCk, 1024], BF16))
            fe = _es.enter_context(SB(nc, "fe", [33, L], F32))
            h1 = _es.enter_context(SB(nc, "h1", [64, L], F32))
            h2 = _es.enter_context(SB(nc, "h2", [64, L], BF16))
            w3b = _es.enter_context(SB(nc, "w3b", [64, 1024], BF16))
            w1 = _es.enter_context(SB(nc, "w1", [33, 64], F32))
            w2 = _es.enter_context(SB(nc, "w2", [64, 64], F32))
            w3 = _es.enter_context(SB(nc, "w3", [64, 1024], F32))
            pre = _es.enter_context(SB(nc, "pre", [64, 512], F32))
            pr2 = _es.enter_context(SB(nc, "pr2", [64, 512], F32))
            t_pr2 = Tok()
            MAGIC = 1.5 * 2 ** 23
            wint = _es.enter_context(SB(nc, "wint", [128, 2, 2, 256], F32))
            hbias = _es.enter_context(SB(nc, "hbias", [128, 512], F32))
            mh = _es.enter_context(SB(nc, "mh", [128, KC], F32))
            slab = _es.enter_context(SB(nc, "slab", [128, 2, NCk, 2, 128], BF16))
            xo = _es.enter_context(SB(nc, "xo", [128, 2, 512], F32))
            sd = _es.enter_context(SB(nc, "sd", [128, 2, 2, 2, 512], F32))
            kf = _es.enter_context(SB(nc, "kf", [128, 2, 2, 2, 256], BF16))
            _es_mlp = ExitStack()
            pm = _es_mlp.enter_context(PS(nc, "pm", [64, 512], F32))
            phh = _es_mlp.enter_context(PS(nc, "phh", [128, 2, 512], F32))
            t_hk, t_fe, t_h1, t_h2, t_w, t_pre, t_win, t_slab, t_eo, t_sd, t_kft, t_pm, t_phh, t_psk = \
                Tok(), Tok(), Tok(), Tok(), Tok(), Tok(), [Tok(), Tok()], [Tok(), Tok()], Tok(), Tok(), Tok(), Tok(), Tok(), Tok()
            S.dma(fe[:], FE[:, :], writes=[t_fe])
            S.dma(w1[:], I["hy_w1"][li], writes=[t_w])
            S.dma(w2[:], I["hy_w2"][li], writes=[t_w])
            S.dma(w3[:], I["hy_w3"][li], writes=[t_w])
            S.op("dve", lambda e: e.tensor_copy(out=w3b[:], in_=w3[:]), reads=[t_w], writes=[t_w])
            S.dma(hbias[:], I["hy_bias"][li].rearrange("o c -> (o c)").partition_broadcast(128), writes=[t_w])
            S.dma(mh[:], MH[:, :], writes=[t_w])
            ob1, ofr, ob2 = COLS["hy_b1"][0], COLS["hy_freq"][0], COLS["hy_b2"][0]
            for (src, t_src, wt, kk, bcol, dst, t_dst) in ((fe, t_fe, w1, 33, ob1, h1, t_h1), (h1, t_h1, w2, 64, ob2, h2, t_h2)):
                for c0 in range(0, L, 512):
                    n = min(512, L - c0)
                    S.op("pe", lambda e: e.matmul(pm[:, 0:n], lhsT=wt[0:kk, :], rhs=src[0:kk, c0:c0 + n], start=True, stop=True),
                         reads=[t_w, t_src], writes=[t_pm])
                    S.op("dve", lambda e: e.tensor_scalar(out=pre[:, 0:n], in0=pm[:, 0:n], scalar1=G.cols[0:64, bcol:bcol + 1],
                                                          scalar2=G.cols[0:64, ofr:ofr + 1], op0=ALU.add, op1=ALU.mult),
                         reads=[t_pm, G.t_cols], writes=[t_pre])
                    S.op("dve", lambda e: e.tensor_scalar(out=pr2[:, 0:n], in0=pre[:, 0:n], scalar1=1.0 / (2.0 * math.pi), scalar2=MAGIC, op0=ALU.mult, op1=ALU.add),
                         reads=[t_pre], writes=[t_pr2])
                    S.op("dve", lambda e: e.tensor_scalar(out=pr2[:, 0:n], in0=pr2[:, 0:n], scalar1=-MAGIC, scalar2=None, op0=ALU.add),
                         reads=[t_pr2], writes=[t_pr2])
                    S.op("dve", lambda e: e.scalar_tensor_tensor(out=pre[:, 0:n], in0=pr2[:, 0:n], scalar=-2.0 * math.pi, in1=pre[:, 0:n], op0=ALU.mult, op1=ALU.add),
                         reads=[t_pr2, t_pre], writes=[t_pre])
                    S.op("act", lambda e: e.activation(out=dst[:, c0:c0 + n], in_=pre[:, 0:n], func=AF.Sin),
                         reads=[t_pre], writes=[t_dst])
            for c in range(NCk):
                b = c % 2
                S.dma(wint[:, b], WIN[c], writes=[t_win[b]])
                for half in range(2):
                    S.op("pe", lambda e: e.matmul(phh[:, half, :], lhsT=h2[:, c * 128:(c + 1) * 128], rhs=w3b[:, half * 512:(half + 1) * 512], start=True, stop=True),
                         reads=[t_h2, t_w], writes=[t_phh])
                for dr in range(2):
                    S.op("dve", lambda e: e.tensor_tensor(out=hk[:, c, dr * 512:(dr + 1) * 512].rearrange("p (o c) -> p o c", o=2),
                                                          in0=phh[:, dr, :].rearrange("p (o c) -> p o c", o=2),
                                                          in1=wint[:, b, dr:dr + 1, :].to_broadcast([128, 2, 256]), op=ALU.mult),
                         reads=[t_phh, t_win[b]], writes=[t_hk])
            S.barrier()
            _es_mlp.close()
            psk2 = _es.enter_context(PS(nc, "psk", [128, 2, 2, 2, 512], F32))
            t_psk2 = [Tok(), Tok()]
            for kc in range(KC):
                b = kc % 2
                S.dma(slab[:, b], FT[kc], writes=[t_slab[b]])
                for dr in range(2):
                    psk = psk2[:, dr]
                    t_psk = t_psk2[dr]
                    for eo_ in range(2):
                        for ri in range(2):
                            for c in range(NH):
                                cc = eo_ * NH + c
                                S.op("pe", lambda e: e.matmul(psk[:, eo_, ri, :], lhsT=slab[:, b, cc, ri, :], rhs=hk[:, cc, dr * 512:(dr + 1) * 512],
                                                              start=(c == 0), stop=(c == NH - 1)), reads=[t_slab[b], t_hk], writes=[t_psk])
                    S.op("act", lambda e: e.copy(out=xo[:], in_=psk[:, 1]), reads=[t_psk], writes=[t_eo])
                    S.op("dve", lambda e: e.tensor_tensor(out=sd[:, dr, 0], in0=psk[:, 0], in1=xo[:], op=ALU.add), reads=[t_psk, t_eo], writes=[t_sd])
                    S.op("dve", lambda e: e.tensor_tensor(out=sd[:, dr, 1], in0=psk[:, 0], in1=xo[:], op=ALU.subtract), reads=[t_psk, t_eo], writes=[t_sd])
                v = lambda ap: ap.rearrange("p (o c) -> p o c", o=2)
                S.op("dve", lambda e: e.tensor_tensor(out=sd[:, 0, 0, 0, :], in0=sd[:, 0, 0, 0, :], in1=hbias[:], op=ALU.add), reads=[t_sd, t_w], writes=[t_sd])
                S.op("dve", lambda e: e.tensor_tensor(out=sd[:, 0, 1, 0, :], in0=sd[:, 0, 1, 0, :], in1=hbias[:], op=ALU.add), reads=[t_sd, t_w], writes=[t_sd])
                S.op("dve", lambda e: e.tensor_tensor(out=kf[:, :, 0, 0, :], in0=v(sd[:, 0, 0, 0, :]), in1=v(sd[:, 1, 0, 0, :]), op=ALU.add), reads=[t_sd], writes=[t_kft])
                S.op("dve", lambda e: e.tensor_tensor(out=kf[:, :, 0, 1, :], in0=v(sd[:, 0, 0, 1, :]), in1=v(sd[:, 1, 0, 1, :]), op=ALU.subtract), reads=[t_sd], writes=[t_kft])
                S.op("dve", lambda e: e.tensor_tensor(out=v(xo[:, 0, :]), in0=v(sd[:, 0, 1, 0, :]), in1=v(sd[:, 1, 1, 0, :]), op=ALU.add), reads=[t_sd], writes=[t_eo])
                S.op("dve", lambda e: e.tensor_tensor(out=v(xo[:, 1, :]), in0=v(sd[:, 1, 1, 1, :]), in1=v(sd[:, 0, 1, 1, :]), op=ALU.subtract), reads=[t_sd], writes=[t_eo])
                S.op("dve", lambda e: e.tensor_scalar(out=kf[:, :, 1, 0, :], in0=v(xo[:, 0, :]), scalar1=mh[:, kc:kc + 1], scalar2=None, op0=ALU.mult),
                     reads=[t_eo, t_w], writes=[t_kft])
                S.op("dve", lambda e: e.tensor_scalar(out=kf[:, :, 1, 1, :], in0=v(xo[:, 1, :]), scalar1=mh[:, kc:kc + 1], scalar2=None, op0=ALU.mult),
                     reads=[t_eo, t_w], writes=[t_kft])
                S.dma(Kf[:, kc].rearrange("o p l r c -> p o l r c"), kf[:], reads=[t_kft], writes=[t_kf])
            S.barrier()
        with ExitStack() as _es:
            vt = _es.enter_context(SB(nc, "vt", [128, NCk, 256], BF16))
            zz1 = _es.enter_context(SB(nc, "zz1", [128, NCk, 256], BF16))
            Y = _es.enter_context(SB(nc, "Y", [128, 2, KC, 2, 256], BF16))
            fsl = _es.enter_context(SB(nc, "fsl", [128, 2, NCk, 2, 128], BF16))
            isl = _es.enter_context(SB(nc, "isl", [128, 2, KC, 2, 128], BF16))
            kft = _es.enter_context(SB(nc, "kft", [128, 2, 2, 2, 256], BF16))
            xo = _es.enter_context(SB(nc, "xo", [128, 2, 256], F32))
            xs_ = _es.enter_context(SB(nc, "xs_", [128, 2, 2, 256], F32))
            ta = _es.enter_context(SB(nc, "ta", [128, 4, 256], F32))
            yl = _es.enter_context(SB(nc, "yl", [128, 2, 2, 256], F32))
            xg = _es.enter_context(SB(nc, "xg", [128, 2, 256], BF16))
            zt = _es.enter_context(SB(nc, "zt", [128, 256], BF16))
            zT = _es.enter_context(SB(nc, "zT", [128, 2, 2, 256], BF16))
            psx = _es.enter_context(PS(nc, "psx", [128, 2, 4, 256], F32))
            psy = _es.enter_context(PS(nc, "psy", [128, 2, 512], F32))
            ptr = _es.enter_context(PS(nc, "ptr", [128, 2, 128], BF16))
            t_vt, t_zz1, t_Y, t_fsl, t_isl, t_kft2, t_ta, t_xg, t_zt, t_zT, t_psx, t_psy, t_ptr, t_xo, t_xs, t_yl = \
                Tok(), Tok(), Tok(), [Tok(), Tok()], [Tok(), Tok()], [Tok(), Tok()], Tok(), [Tok(), Tok()], Tok(), [Tok(), Tok()], [Tok(), Tok()], [Tok(), Tok()], Tok(), Tok(), Tok(), Tok()
            hsrc = lambda m: G.hyv[m, off:off + L, :].rearrange("(c p two) ch -> two p c ch", p=128, two=2)
            for par in range(2):
                S.dma(vt[:, par * NH:(par + 1) * NH, :], hsrc(0)[par], reads=[G.t_hyv], writes=[t_vt])
            mo = G.mixT[768:1024, :].rearrange("(c p) t -> p c t", p=128)
            for order in range(2):
                src, t_src = (vt, t_vt) if order == 0 else (zz1, t_zz1)
                for kc in range(KC):
                    b = kc % 2
                    S.dma(fsl[:, b], FT[kc], writes=[t_fsl[b]])
                    S.dma(kft[:, b], Kf[order, kc], reads=[t_kf], writes=[t_kft2[b]])
                    for eo_ in range(2):
                        for ri in range(2):
                            for c in range(NH):
                                cc = eo_ * NH + c
                                S.op("pe", lambda e: e.matmul(psx[:, b, eo_ * 2 + ri, :], lhsT=fsl[:, b, cc, ri, :], rhs=src[:, cc, :], start=(c == 0), stop=(c == NH - 1)),
                                     reads=[t_fsl[b], t_src], writes=[t_psx[b]])
                    S.op("act", lambda e: e.copy(out=xo[:], in_=psx[:, b, 2:4, :]), reads=[t_psx[b]], writes=[t_xo])
                    S.op("dve", lambda e: e.tensor_tensor(out=xs_[:, 0], in0=psx[:, b, 0:2, :], in1=xo[:], op=ALU.add), reads=[t_psx[b], t_xo], writes=[t_xs])
                    S.op("dve", lambda e: e.tensor_tensor(out=xs_[:, 1, 0, :], in0=psx[:, b, 0, :], in1=xo[:, 0, :], op=ALU.subtract), reads=[t_psx[b], t_xo], writes=[t_xs])
                    S.op("dve", lambda e: e.scalar_tensor_tensor(out=xs_[:, 1, 1, :], in0=psx[:, b, 1, :], scalar=-1.0, in1=xo[:, 1, :], op0=ALU.mult, op1=ALU.add),
                         reads=[t_psx[b], t_xo], writes=[t_xs])
                    S.op("dve", lambda e: e.tensor_tensor(out=ta[:, 0:2, :], in0=xs_[:, :, 0, :], in1=kft[:, b, :, 0, :], op=ALU.mult), reads=[t_xs, t_kft2[b]], writes=[t_ta])
                    S.op("pool", lambda e: e.tensor_tensor(out=ta[:, 2:4, :], in0=xs_[:, :, 1, :], in1=kft[:, b, :, 1, :], op=ALU.mult), reads=[t_xs, t_kft2[b]], writes=[t_ta])
                    S.op("dve", lambda e: e.tensor_tensor(out=yl[:, :, 0, :], in0=ta[:, 0:2, :], in1=ta[:, 2:4, :], op=ALU.subtract), reads=[t_ta], writes=[t_yl])
                    S.op("dve", lambda e: e.tensor_tensor(out=ta[:, 0:2, :], in0=xs_[:, :, 0, :], in1=kft[:, b, :, 1, :], op=ALU.mult), reads=[t_xs, t_kft2[b], t_yl], writes=[t_ta])
                    S.op("pool", lambda e: e.tensor_tensor(out=ta[:, 2:4, :], in0=xs_[:, :, 1, :], in1=kft[:, b, :, 0, :], op=ALU.mult), reads=[t_xs, t_kft2[b], t_yl], writes=[t_ta])
                    S.op("dve", lambda e: e.tensor_tensor(out=yl[:, :, 1, :], in0=ta[:, 0:2, :], in1=ta[:, 2:4, :], op=ALU.add), reads=[t_ta], writes=[t_yl])
                    S.op("dve", lambda e: e.tensor_tensor(out=Y[:, 0, kc, 0, :], in0=yl[:, 0, 0, :], in1=yl[:, 1, 0, :], op=ALU.add), reads=[t_yl], writes=[t_Y])
                    S.op("pool", lambda e: e.tensor_tensor(out=Y[:, 0, kc, 1, :], in0=yl[:, 0, 1, :], in1=yl[:, 1, 1, :], op=ALU.subtract), reads=[t_yl], writes=[t_Y])
                    S.op("dve", lambda e: e.tensor_tensor(out=Y[:, 1, kc, 0, :], in0=yl[:, 0, 0, :], in1=yl[:, 1, 0, :], op=ALU.subtract), reads=[t_yl], writes=[t_Y])
                    S.op("pool", lambda e: e.tensor_tensor(out=Y[:, 1, kc, 1, :], in0=yl[:, 0, 1, :], in1=yl[:, 1, 1, :], op=ALU.add), reads=[t_yl], writes=[t_Y])
                oi = 0
                for c2 in range(NH):
                    for par in range(2):
                        cc = par * NH + c2
                        b = oi % 2
                        oi += 1
                        zb = c2 % 2
                        S.dma(isl[:, b], IT[cc], writes=[t_isl[b]])
                        S.dma(xg[:, b], hsrc(1 + order)[par, :, c2, :], reads=[G.t_hyv], writes=[t_xg[b]])
                        for kc in range(KC):
                            for ri in range(2):
                                S.op("pe", lambda e: e.matmul(psy[:, b, 0:256], lhsT=isl[:, b, kc, ri, :], rhs=Y[:, par, kc, ri, :],
                                                              start=(kc == 0 and ri == 0), stop=(kc == KC - 1 and ri == 1)), reads=[t_isl[b], t_Y], writes=[t_psy[b]])
                        if order == 0:
                            S.op("dve", lambda e: e.tensor_tensor(out=zz1[:, cc, :], in0=psy[:, b, 0:256], in1=xg[:, b, :], op=ALU.mult),
                                 reads=[t_psy[b], t_xg[b]], writes=[t_zz1])
                        else:
                            S.op("dve", lambda e: e.tensor_tensor(out=zt[:], in0=psy[:, b, 0:256], in1=xg[:, b, :], op=ALU.mult),
                                 reads=[t_psy[b], t_xg[b]], writes=[t_zt])
                            for hh in range(2):
                                S.op("pe", lambda e: e.transpose(out=ptr[:, hh, :], in_=zt[:, hh * 128:(hh + 1) * 128], identity=G.identB),
                                     reads=[t_zt, G.t_c], writes=[t_ptr])
                            S.op("act", lambda e: e.copy(out=zT[:, zb].rearrange("p h (t two) -> p h t two", two=2)[:, :, :, par], in_=ptr[:]),
                                 reads=[t_ptr], writes=[t_zT[zb]])
                            if par == 1:
                                S.dma(mo[:, :, off + c2 * 256:off + (c2 + 1) * 256], zT[:, zb], reads=[t_zT[zb]], writes=[G.t_mix])
            S.barrier()
    if "hy" in G.dbg and li == 0:
        dump_bf16(G, G.mixT[768:896, 256:768], G.dbg["hy"], [G.t_mix])


def _hy_tables(L):
    N = 2 * L
    NCk = L // 128
    KC = (L // 2 + 1 + 127) // 128
    perm = np.concatenate([np.arange(0, L, 2), np.arange(1, L, 2)])
    n = perm.astype(np.int64)
    k = np.arange(KC * 128, dtype=np.int64)
    ang = ((n[:, None] * k[None, :]) % N).astype(np.float64) * (2 * np.pi / N)
    valid = (k <= L // 2).astype(np.float64)
    w = np.where(k == 0, 1.0, 2.0) / N * valid
    c, s_ = np.cos(ang), np.sin(ang)
    ft = np.stack([c * valid, -s_ * valid], axis=0)
    ft = ft.reshape(2, NCk, 128, KC, 128).transpose(3, 2, 1, 0, 4)
    it = np.stack([c * w, -s_ * w], axis=0)
    it = it.reshape(2, NCk, 128, KC, 128).transpose(1, 4, 3, 0, 2)
    f = np.float32
    nn = np.arange(L, dtype=f)
    t = nn / f(max(L - 1, 1))
    bands = np.linspace(1e-4, 15, 16, dtype=f)
    wpos = (f(2 * math.pi / L) * nn).astype(f)
    feats = np.concatenate([t[:, None], np.cos(wpos[:, None] * bands), -np.sin(wpos[:, None] * bands)], axis=-1).astype(f)
    deltas = np.abs(np.linspace(math.log(1e-2) / 1.5, math.log(1e-2) / 0.3, 256, dtype=f))
    win = np.exp(-t[:, None] * deltas).astype(f)
    winb = win.copy()
    winb[0] = 0.0
    feats = feats[perm]
    wn = np.stack([win, winb], axis=1)[perm].reshape(NCk, 128, 2, 256)
    mh = ((k != L // 2) & (k <= L // 2)).astype(f).reshape(KC, 128).T
    bf = ml_dtypes.bfloat16
    return (np.ascontiguousarray(ft).astype(bf), np.ascontiguousarray(it).astype(bf),
            np.ascontiguousarray(feats.T), np.ascontiguousarray(wn), np.ascontiguousarray(mh))
```
